# Optimizing a Trainium2 kernel written in Bass

```python
import jax, jax.numpy as jnp
from jax import lax
import numpy as np

D_MODEL = 1024
BATCH = 4
SEQ = 4096
DEPTH = 1

EPS = 1e-6
D_MIX = D_MODEL
HG_HEADS = 4
HG_DK = 128
HG_DV = 128
HG_WIDTH = HG_HEADS * HG_DV
HG_CHUNK = 64
NSA_HEADS = 8
NSA_KV_GROUPS = 2
NSA_HD = 64
NSA_WIDTH = NSA_HEADS * NSA_HD
NSA_KV = NSA_KV_GROUPS * NSA_HD
CMP_BLOCK = 32
CMP_STRIDE = 16
CMP_HIDDEN = 256
SEL_BLOCK = 64
N_SEL = 16
WINDOW = 512
Q_BLOCK = 128
ROPE_THETA = 10000.0
N_EXPERTS = 64
TOP_K = 8
N_GROUPS = 8
TOPK_GROUPS = 4
EXPERT_HIDDEN = 256
SHARED_HIDDEN = 256
ROUTED_SCALE = 2.5
EXPERT_BLOCK = 128
IN_SPLITS = (HG_WIDTH,) * 4 + (NSA_WIDTH,) + (NSA_KV,) * 6 + (3 * NSA_HEADS,)
IN_COLS = 4 * HG_WIDTH + NSA_WIDTH + 6 * NSA_KV + 3 * NSA_HEADS

kernel_name = "hymba_hgrn2_nsa_moe_adaln"


def rmsnorm(x, g):
    xf = x.astype(jnp.float32)
    y = xf * lax.rsqrt(jnp.mean(xf * xf, axis=-1, keepdims=True) + EPS)
    return (y * g.astype(jnp.float32)).astype(x.dtype)


def rope(t, pos):
    half = t.shape[-1] // 2
    inv = ROPE_THETA ** (-jnp.arange(half, dtype=jnp.float32) / half)
    ang = pos.astype(jnp.float32)[:, None] * inv[None, :]
    cos, sin = jnp.cos(ang), jnp.sin(ang)
    t1 = t[..., :half].astype(jnp.float32)
    t2 = t[..., half:].astype(jnp.float32)
    return jnp.concatenate([t1 * cos - t2 * sin, t2 * cos + t1 * sin], axis=-1).astype(t.dtype)


def swiglu(t, w_gu, w_dn):
    g, u = jnp.split(t @ w_gu, 2, axis=-1)
    return (jax.nn.silu(g) * u) @ w_dn


def hgrn2_mixer(q, f_logit, i, gate, lb, norm_g):
    B, S, _ = q.shape
    f32 = jnp.float32
    f = lb + (1.0 - lb) * jax.nn.sigmoid(f_logit.astype(f32))
    log_f = jnp.log(f)
    k = 1.0 - f
    n_ch = S // HG_CHUNK

    def to_chunks(t, d):
        return t.reshape(B, n_ch, HG_CHUNK, HG_HEADS, d).transpose(1, 0, 3, 2, 4)

    qc = to_chunks(q.astype(f32) * HG_DK ** -0.5, HG_DK)
    kc = to_chunks(k, HG_DK)
    vc = to_chunks(i.astype(f32), HG_DV)
    bc = jnp.cumsum(to_chunks(log_f, HG_DK), axis=3)
    causal = jnp.tril(jnp.ones((HG_CHUNK, HG_CHUNK), bool))

    def step(state, inp):
        q_, k_, v_, b_ = inp
        diff = jnp.where(causal[:, :, None], b_[:, :, :, None, :] - b_[:, :, None, :, :], -jnp.inf)
        a = jnp.einsum('bhtd,bhtsd,bhsd->bhts', q_, jnp.exp(diff), k_)
        o = a @ v_ + jnp.einsum('bhtd,bhde->bhte', q_ * jnp.exp(b_), state)
        b_last = b_[:, :, -1]
        state = jnp.exp(b_last)[..., None] * state + jnp.einsum(
            'bhsd,bhse->bhde', k_ * jnp.exp(b_last[:, :, None, :] - b_), v_)
        return state, o

    s0 = jnp.zeros((B, HG_HEADS, HG_DK, HG_DV), f32)
    _, o = lax.scan(step, s0, (qc, kc, vc, bc))
    o = o.transpose(1, 0, 3, 2, 4).reshape(B, S, HG_HEADS, HG_DV)
    o = o * lax.rsqrt(jnp.mean(o * o, axis=-1, keepdims=True) + EPS) * norm_g.astype(f32).reshape(HG_HEADS, HG_DV)
    o = o.reshape(B, S, HG_WIDTH) * jax.nn.silu(gate.astype(f32))
    return o.astype(q.dtype)


def nsa_mixer(q, kc, vc, ks, vs, kw, vw, gate_logit,
              pos_k, w1_k, b1_k, w2_k, pos_v, w1_v, b1_v, w2_v):
    B, S, _ = q.shape
    G, HG, HD = NSA_KV_GROUPS, NSA_HEADS // NSA_KV_GROUPS, NSA_HD
    f32 = jnp.float32
    pos = jnp.arange(S)
    scale = HD ** -0.5
    qh = q.reshape(B, S, G, HG, HD).transpose(0, 2, 3, 1, 4)
    q_rot = rope(qh, pos)

    def kv_heads(t):
        return t.reshape(B, S, G, HD).transpose(0, 2, 1, 3)

    n_cmp = (S - CMP_BLOCK) // CMP_STRIDE + 1
    tok_idx = jnp.arange(n_cmp)[:, None] * CMP_STRIDE + jnp.arange(CMP_BLOCK)[None, :]

    def compress(t, pe, w1, b1, w2):
        blocks = kv_heads(t)[:, :, tok_idx] + pe
        hdn = jax.nn.gelu(blocks.reshape(B, G, n_cmp, CMP_BLOCK * HD) @ w1 + b1)
        return hdn @ w2

    k_cmp = compress(kc, pos_k, w1_k, b1_k, w2_k)
    v_cmp = compress(vc, pos_v, w1_v, b1_v, w2_v)
    s_cmp = jnp.einsum('bghsd,bgcd->bghsc', qh, k_cmp).astype(f32) * scale
    c_start = jnp.arange(n_cmp) * CMP_STRIDE
    cmp_mask = (c_start + CMP_BLOCK - 1)[None, :] <= pos[:, None]
    p_cmp = jax.nn.softmax(jnp.where(cmp_mask, s_cmp, -1e30), axis=-1) * cmp_mask
    o_cmp = jnp.einsum('bghsc,bgcd->bghsd', p_cmp, v_cmp.astype(f32))

    n_sb = S // SEL_BLOCK
    n_sel = min(N_SEL, n_sb)
    s_start = jnp.arange(n_sb) * SEL_BLOCK
    overlap = jnp.clip(jnp.minimum(c_start[:, None] + CMP_BLOCK, s_start[None, :] + SEL_BLOCK)
                       - jnp.maximum(c_start[:, None], s_start[None, :]), 0, None).astype(f32) / CMP_BLOCK
    p_slc = jnp.einsum('bghsc,cj->bgsj', p_cmp, overlap)
    cur = pos // SEL_BLOCK
    blk = jnp.arange(n_sb)
    available = blk[None, :] <= cur[:, None]
    forced = (blk[None, :] == 0) | (blk[None, :] == cur[:, None]) | (blk[None, :] == cur[:, None] - 1)
    sel_score = jnp.where(forced, 1e30, jnp.where(available, p_slc, -1e30))
    top_val, top_idx = lax.top_k(sel_score, n_sel)
    top_valid = top_val > -1e29

    n_qb = S // Q_BLOCK
    k_blocks = rope(kv_heads(ks), pos).reshape(B, G, n_sb, SEL_BLOCK * HD)
    v_blocks = kv_heads(vs).reshape(B, G, n_sb, SEL_BLOCK * HD)
    bi = jnp.arange(B)[:, None, None]
    gi = jnp.arange(G)[None, :, None]

    def by_qblock(t, axis):
        shape = t.shape[:axis] + (n_qb, Q_BLOCK) + t.shape[axis + 1:]
        return jnp.moveaxis(t.reshape(shape), axis, 0)

    def slc_block(args):
        qb, idx, valid, qpos = args
        flat = idx.reshape(B, G, Q_BLOCK * n_sel)
        kg = k_blocks[bi, gi, flat].reshape(B, G, Q_BLOCK, n_sel, SEL_BLOCK, HD)
        vg = v_blocks[bi, gi, flat].reshape(B, G, Q_BLOCK, n_sel, SEL_BLOCK, HD)
        s = jnp.einsum('bghqd,bgqnld->bghqnl', qb, kg).astype(f32) * scale
        kpos = idx[..., None] * SEL_BLOCK + jnp.arange(SEL_BLOCK)
        ok = valid[..., None] & (kpos <= qpos[:, None, None])
        s = jnp.where(ok[:, :, None], s, -1e30).reshape(B, G, HG, Q_BLOCK, n_sel * SEL_BLOCK)
        p = jax.nn.softmax(s, axis=-1).reshape(B, G, HG, Q_BLOCK, n_sel, SEL_BLOCK)
        return jnp.einsum('bghqnl,bgqnld->bghqd', p, vg.astype(f32))

    o_slc = lax.map(slc_block, (by_qblock(q_rot, 3), by_qblock(top_idx, 2),
                                by_qblock(top_valid, 2), pos.reshape(n_qb, Q_BLOCK)))
    o_slc = jnp.moveaxis(o_slc, 0, 3).reshape(B, G, HG, S, HD)

    n_back = WINDOW // Q_BLOCK

    def banded(t):
        tp = jnp.pad(t, ((0, 0), (0, 0), (WINDOW, 0), (0, 0))).reshape(B, G, n_qb + n_back, Q_BLOCK, HD)
        return jnp.concatenate([tp[:, :, o:o + n_qb] for o in range(n_back + 1)], axis=3)

    kwb = banded(rope(kv_heads(kw), pos))
    vwb = banded(kv_heads(vw))
    qw = q_rot.reshape(B, G, HG, n_qb, Q_BLOCK, HD)
    s_w = jnp.einsum('bghnqd,bgnkd->bghnqk', qw, kwb).astype(f32) * scale
    qpos = pos.reshape(n_qb, Q_BLOCK)
    kpos = (jnp.arange(n_qb)[:, None] - n_back) * Q_BLOCK + jnp.arange((n_back + 1) * Q_BLOCK)[None, :]
    d = qpos[:, :, None] - kpos[:, None, :]
    ok_w = (d >= 0) & (d < WINDOW) & (kpos[:, None, :] >= 0)
    p_w = jax.nn.softmax(jnp.where(ok_w, s_w, -1e30), axis=-1)
    o_win = jnp.einsum('bghnqk,bgnkd->bghnqd', p_w, vwb.astype(f32)).reshape(B, G, HG, S, HD)

    gates = jax.nn.sigmoid(gate_logit.astype(f32)).reshape(B, S, G, HG, 3).transpose(0, 2, 3, 1, 4)
    o = gates[..., 0:1] * o_cmp + gates[..., 1:2] * o_slc + gates[..., 2:3] * o_win
    return o.transpose(0, 3, 1, 2, 4).reshape(B, S, NSA_WIDTH).astype(q.dtype)


def routed_experts(t, top_i, top_w, w_gu, w_dn):
    T, D = t.shape
    A = T * TOP_K
    e_flat = top_i.reshape(A).astype(jnp.int32)
    w_flat = top_w.reshape(A).astype(jnp.float32)
    order = jnp.argsort(e_flat)
    e_sorted = e_flat[order]
    tok_sorted = (order // TOP_K).astype(jnp.int32)
    counts = jnp.bincount(e_flat, length=N_EXPERTS)
    start = jnp.cumsum(counts) - counts
    padded = (counts + EXPERT_BLOCK - 1) // EXPERT_BLOCK * EXPERT_BLOCK
    pad_end = jnp.cumsum(padded)
    pad_start = pad_end - padded
    dest = pad_start[e_sorted] + jnp.arange(A) - start[e_sorted]
    n_blk = -(-A // EXPERT_BLOCK) + N_EXPERTS
    P = n_blk * EXPERT_BLOCK
    rows = jnp.full((P,), T, jnp.int32).at[dest].set(tok_sorted)
    wts = jnp.zeros((P,), jnp.float32).at[dest].set(w_flat[order])
    blk_expert = jnp.minimum(jnp.searchsorted(pad_end, jnp.arange(n_blk) * EXPERT_BLOCK, side='right'),
                             N_EXPERTS - 1)
    t_pad = jnp.concatenate([t, jnp.zeros((1, D), t.dtype)], axis=0)

    def one_block(args):
        rb, e = args
        return swiglu(t_pad[rb], w_gu[e], w_dn[e]).astype(jnp.float32)

    ys = lax.map(one_block, (rows.reshape(n_blk, EXPERT_BLOCK), blk_expert)).reshape(P, D)
    out = jnp.zeros((T + 1, D), jnp.float32).at[rows].add(ys * wts[:, None])
    return out[:T]


def moe_ffn(h, router_w, router_bias, w_gu, w_dn, w_sh_gu, w_sh_dn):
    B, S, D = h.shape
    t = h.reshape(B * S, D)
    scores = jax.nn.sigmoid((t @ router_w).astype(jnp.float32))
    choice = scores + router_bias.astype(jnp.float32)
    grp = choice.reshape(B * S, N_GROUPS, N_EXPERTS // N_GROUPS)
    grp_score = lax.top_k(grp, 2)[0].sum(-1)
    top_g = lax.top_k(grp_score, TOPK_GROUPS)[1]
    gmask = jnp.any(top_g[..., None] == jnp.arange(N_GROUPS), axis=-2)
    emask = jnp.repeat(gmask, N_EXPERTS // N_GROUPS, axis=-1)
    top_i = lax.top_k(jnp.where(emask, choice, -jnp.inf), TOP_K)[1]
    top_w = jnp.take_along_axis(scores, top_i, axis=-1)
    top_w = top_w / jnp.sum(top_w, axis=-1, keepdims=True) * ROUTED_SCALE
    routed = routed_experts(t, top_i, top_w, w_gu, w_dn)
    shared = swiglu(t, w_sh_gu, w_sh_dn).astype(jnp.float32)
    return (routed + shared).reshape(B, S, D).astype(h.dtype)


def setup_inputs(seed: int = 0) -> dict:
    key = jax.random.key(seed)
    ks = jax.random.split(key, 26)
    L = DEPTH

    def nrm(k, shape, s):
        return jax.random.normal(k, shape, jnp.float32) * s

    return {
        "x": nrm(ks[0], (BATCH, SEQ, D_MODEL), 1.0),
        "c": nrm(ks[1], (BATCH, D_MODEL), 1.0),
        "w_ada": nrm(ks[2], (L, D_MODEL, 6 * D_MODEL), 0.5 * D_MODEL ** -0.5),
        "b_ada": nrm(ks[3], (L, 6 * D_MODEL), 0.02),
        "norm1_g": 1.0 + nrm(ks[4], (L, D_MODEL), 0.02),
        "w_in": nrm(ks[5], (L, D_MODEL, IN_COLS), D_MODEL ** -0.5),
        "hg_lb_logits": nrm(ks[6], (L + 1, HG_WIDTH), 0.5),
        "hg_norm_g": 1.0 + nrm(ks[7], (L, HG_WIDTH), 0.02),
        "cmp_pos_k": nrm(ks[8], (L, CMP_BLOCK, NSA_HD), 0.1),
        "cmp_w1_k": nrm(ks[9], (L, CMP_BLOCK * NSA_HD, CMP_HIDDEN), (CMP_BLOCK * NSA_HD) ** -0.5),
        "cmp_b1_k": nrm(ks[10], (L, CMP_HIDDEN), 0.02),
        "cmp_w2_k": nrm(ks[11], (L, CMP_HIDDEN, NSA_HD), CMP_HIDDEN ** -0.5),
        "cmp_pos_v": nrm(ks[12], (L, CMP_BLOCK, NSA_HD), 0.1),
        "cmp_w1_v": nrm(ks[13], (L, CMP_BLOCK * NSA_HD, CMP_HIDDEN), (CMP_BLOCK * NSA_HD) ** -0.5),
        "cmp_b1_v": nrm(ks[14], (L, CMP_HIDDEN), 0.02),
        "cmp_w2_v": nrm(ks[15], (L, CMP_HIDDEN, NSA_HD), CMP_HIDDEN ** -0.5),
        "w_out": nrm(ks[16], (L, D_MIX, D_MODEL), D_MIX ** -0.5),
        "norm2_g": 1.0 + nrm(ks[17], (L, D_MODEL), 0.02),
        "router_w": nrm(ks[18], (L, D_MODEL, N_EXPERTS), D_MODEL ** -0.5),
        "router_bias": nrm(ks[19], (L, N_EXPERTS), 0.01),
        "w_exp_gu": nrm(ks[20], (L, N_EXPERTS, D_MODEL, 2 * EXPERT_HIDDEN), D_MODEL ** -0.5),
        "w_exp_dn": nrm(ks[21], (L, N_EXPERTS, EXPERT_HIDDEN, D_MODEL), EXPERT_HIDDEN ** -0.5),
        "w_sh_gu": nrm(ks[22], (L, D_MODEL, 2 * SHARED_HIDDEN), D_MODEL ** -0.5),
        "w_sh_dn": nrm(ks[23], (L, SHARED_HIDDEN, D_MODEL), SHARED_HIDDEN ** -0.5),
        "final_g": 1.0 + nrm(ks[24], (D_MODEL,), 0.02),
    }


def reference(x, c, w_ada, b_ada, norm1_g, w_in, hg_lb_logits, hg_norm_g,
              cmp_pos_k, cmp_w1_k, cmp_b1_k, cmp_w2_k, cmp_pos_v, cmp_w1_v, cmp_b1_v, cmp_w2_v,
              w_out, norm2_g, router_w, router_bias, w_exp_gu, w_exp_dn, w_sh_gu, w_sh_dn, final_g):
    offsets = np.cumsum(IN_SPLITS)[:-1].tolist()
    lb_all = jnp.cumsum(jax.nn.softmax(hg_lb_logits.astype(jnp.float32), axis=0), axis=0)
    c_act = jax.nn.silu(c)
    for l in range(DEPTH):
        mod = (c_act @ w_ada[l] + b_ada[l])[:, None, :]
        sh1, sc1, g1, sh2, sc2, g2 = jnp.split(mod, 6, axis=-1)
        h = rmsnorm(x, norm1_g[l]) * (1.0 + sc1) + sh1
        hq, hf, hi, hgt, nq, kc, vc, ksl, vsl, kwn, vwn, ngate = jnp.split(h @ w_in[l], offsets, axis=-1)
        o_hg = hgrn2_mixer(hq, hf, hi, hgt, lb_all[l], hg_norm_g[l])
        o_nsa = nsa_mixer(nq, kc, vc, ksl, vsl, kwn, vwn, ngate,
                          cmp_pos_k[l], cmp_w1_k[l], cmp_b1_k[l], cmp_w2_k[l],
                          cmp_pos_v[l], cmp_w1_v[l], cmp_b1_v[l], cmp_w2_v[l])
        x = x + g1 * (jnp.concatenate([o_hg, o_nsa], axis=-1) @ w_out[l])
        h = rmsnorm(x, norm2_g[l]) * (1.0 + sc2) + sh2
        x = x + g2 * moe_ffn(h, router_w[l], router_bias[l], w_exp_gu[l], w_exp_dn[l], w_sh_gu[l], w_sh_dn[l])
    return rmsnorm(x, final_g)
```

```python
import numpy as np
import os as _os0
import ml_dtypes
from contextlib import ExitStack
import concourse.bass as bass
import concourse.mybir as mybir
from concourse.bass_utils import run_bass_kernel_spmd

F32 = mybir.dt.float32
BF16 = mybir.dt.bfloat16
AF = mybir.ActivationFunctionType
ALU = mybir.AluOpType
AX = mybir.AxisListType

S = 4096
D = 1024
NT = 32
NB = 8
SO = 2048
EPS = 1e-6
NEG = -30000.0
NDS = 12
SEM_LIMIT = 2000
SAME_SYNC = True


class Buf:
    __slots__ = ("ap", "w", "r", "excl")

    def __init__(self, ap, excl=False):
        self.ap = ap
        self.w = None
        self.r = {}
        self.excl = excl

    def __getitem__(self, k):
        return self.ap[k]


class Eng:
    def __init__(self, name, h):
        self.name = name
        self.h = h
        self.sem = None
        self.count = 0
        self.epoch = 0
        self.waited = {}


class KB:
    def __init__(self, nc, es):
        self.nc = nc
        self.es = es
        self.engs = {n: Eng(n, getattr(nc, n)) for n in ("tensor", "vector", "scalar", "gpsimd", "sync")}
        for e in self.engs.values():
            self._new_sem(e)
        self.dsems = {q: [es.enter_context(nc.semaphore(f"d_{q}{i}")) for i in range(NDS)] for q in ("sync", "gpsimd")}
        self.dcnt = {q: [0] * NDS for q in ("sync", "gpsimd")}
        self.drr = {"sync": 0, "gpsimd": 0}
        self.nsem = 0

    def _new_sem(self, e):
        e.epoch += 1
        e.sem = self.es.enter_context(self.nc.semaphore(f"s_{e.name}_{e.epoch}"))
        e.count = 0

    def wait(self, eng, tk):
        key, sem, val = tk
        if eng.waited.get(key, 0) >= val:
            return
        eng.h.wait_ge(sem, val)
        eng.waited[key] = val

    def _deps(self, en, eng, outs, ins):
        need = {}

        def add(t):
            if t[3] == en and (en == "tensor" or not SAME_SYNC):
                return
            cur = need.get(t[0])
            if cur is None or cur[2] < t[2]:
                need[t[0]] = t

        for b in ins:
            if b.w is not None:
                add(b.w)
            if b.excl:
                for t in b.r.values():
                    if t[3] != en:
                        add(t)
        for b in outs:
            if b.w is not None:
                add(b.w)
            for t in b.r.values():
                add(t)
        for t in need.values():
            self.wait(eng, t[:3])

    def op(self, en, fn, outs=(), ins=(), mark=True):
        eng = self.engs[en]
        self._deps(en, eng, outs, ins)
        if eng.count >= SEM_LIMIT:
            self._new_sem(eng)
        inst = fn(eng.h)
        if mark:
            eng.count += 1
            inst.then_inc(eng.sem, 1)
            tk = ((en, eng.epoch), eng.sem, eng.count, en)
        else:
            tk = ((en, eng.epoch), eng.sem, eng.count + 1, en)
        for b in ins:
            b.r[tk[0]] = tk
        for b in outs:
            b.w = tk
            b.r = {}
        return tk

    def dma(self, q, out_ap, in_ap, outs=(), ins=()):
        eng = self.engs[q]
        i = self.drr[q]
        self.drr[q] = (i + 1) % NDS
        sem = self.dsems[q][i]
        key = ("d", q, i)
        if self.dcnt[q][i] > 0:
            self.wait(eng, (key, sem, self.dcnt[q][i]))
        self._deps("dma_" + q, eng, outs, ins)
        inst = eng.h.dma_start(out=out_ap, in_=in_ap)
        self.dcnt[q][i] += 16
        inst.then_inc(sem, 16)
        tk = (key, sem, self.dcnt[q][i], "dma_" + q)
        for b in ins:
            b.r[key] = tk
        for b in outs:
            b.w = tk
            b.r = {}
        return tk

    def barrier(self):
        for e in self.engs.values():
            for o in self.engs.values():
                if o is e or o.count == 0:
                    continue
                self.wait(e, ((o.name, o.epoch), o.sem, o.count))
            for q in ("sync", "gpsimd"):
                for i in range(NDS):
                    if self.dcnt[q][i] > 0:
                        self.wait(e, (("d", q, i), self.dsems[q][i], self.dcnt[q][i]))


def _consts(half):
    bf = ml_dtypes.bfloat16
    c = {}
    eye = np.eye(128, dtype=np.float32)
    c["ident"] = eye.astype(bf)
    c["onesf"] = np.ones((128, 128), np.float32)
    c["isel0"] = (eye * (1.0 if half == 0 else 0.0)).astype(bf)
    c["isel1"] = (eye * (1.0 if half == 1 else 0.0)).astype(bf)
    m = np.arange(128)
    sw = (m // 64) * 64 + ((m % 64) + 32) % 64
    ps = np.zeros((128, 128), np.float32)
    ps[sw, m] = 1.0
    c["pswap"] = ps.astype(bf)
    dd = np.arange(128) % 64
    i = dd % 32
    inv = 10000.0 ** (-(i.astype(np.float64)) / 32.0)
    ang = inv[:, None].astype(np.float32).astype(np.float64) * np.arange(S)[None, :]
    ang = ang.astype(np.float32).astype(np.float64)
    c["cosT"] = np.cos(ang).astype(np.float32)
    sg = np.where(dd < 32, -1.0, 1.0)[:, None]
    c["sinT"] = (np.sin(ang) * sg).astype(np.float32)
    c["hmask"] = (m[:, None] <= m[None, :]).astype(np.float32).astype(bf)
    seg = np.ones((128, 512), np.float32)
    seg[:, ::128] = 0.0
    c["segm"] = seg
    r = np.arange(128)[:, None]
    qi = np.arange(512)[None, :]
    wb = np.zeros((8, 128, 512), np.float32)
    cb = np.zeros((4, 128, 512), np.float32)
    for j in range(8):
        kpos = -512 + 128 * j + r
        dlt = qi - kpos
        wb[j] = np.where((dlt >= 0) & (dlt < 512), 0.0, NEG)
    for j in range(4):
        kpos = 128 * j + r
        cb[j] = np.where(kpos <= qi, 0.0, NEG)
    c["wband"] = np.ascontiguousarray(wb.transpose(1, 0, 2)).astype(bf)
    c["causb"] = np.ascontiguousarray(cb.transpose(1, 0, 2)).astype(bf)
    cm = np.zeros((8, 128, 512), np.float32)
    for qb in range(8):
        ct = 0 if qb < 4 else 1
        cc = 128 * ct + r
        qpos = 512 * qb + qi
        cm[qb] = np.where((16 * cc + 31 <= qpos) & (cc < 255), 0.0, NEG)
    c["cmpb"] = np.ascontiguousarray(cm.transpose(1, 0, 2)).astype(bf)
    ek = np.zeros((64, 32, 128), np.float32)
    for kt in range(32):
        ek[2 * kt, kt, :64] = 1.0
        ek[2 * kt + 1, kt, 64:] = 1.0
    c["ekt"] = np.concatenate([ek, ek], axis=0).astype(bf)
    add = np.zeros((128, 32, 64), np.float32)
    for qt in range(32):
        pos = 128 * qt + np.arange(128)
        cur = pos // 64
        j = np.arange(64)[None, :]
        forced = (j == 0) | (j == cur[:, None]) | (j == cur[:, None] - 1)
        avail = j <= cur[:, None]
        add[:, qt, :] = np.where(forced, 1e30, np.where(avail, 0.0, -1e30))
    c["seladd"] = add
    cs = np.arange(256)[:, None] * 16
    ss = np.arange(64)[None, :] * 64
    ov = np.clip(np.minimum(cs + 32, ss + 64) - np.maximum(cs, ss), 0, None).astype(np.float32) / 32.0
    ov[255] = 0.0
    c["ovl"] = np.ascontiguousarray(ov.reshape(2, 128, 64).transpose(1, 0, 2)).astype(bf)
    return c


CONST_SHAPES = {
    "ident": ([128, 128], BF16), "onesf": ([128, 128], F32), "isel0": ([128, 128], BF16), "isel1": ([128, 128], BF16),
    "pswap": ([128, 128], BF16), "cosT": ([128, S], F32), "sinT": ([128, S], F32), "hmask": ([128, 128], BF16),
    "segm": ([128, 512], F32), "wband": ([128, 8, 512], BF16), "causb": ([128, 4, 512], BF16),
    "cmpb": ([128, 8, 512], BF16), "ekt": ([128, 32, 128], BF16), "seladd": ([128, 32, 64], F32),
    "ovl": ([128, 2, 64], BF16),
}

IN_SHAPES = {
    "xT": [D, S], "xTo": [D, SO], "cT": [128, 8], "w_ada": [D, 6 * D], "b_ada": [1, 6 * D], "n1g": [128, 8],
    "w_in": [D, 3352], "lbl": [128, 2, 4], "hng": [128, 512],
    "peTk": [64, 32], "w1k": [64, 32, 256], "b1k": [128, 2], "w2k": [128, 2, 64],
    "peTv": [64, 32], "w1v": [64, 32, 256], "b1v": [128, 2], "w2v": [128, 2, 64],
    "w_out": [D, D], "n2g": [128, 8], "rw": [128, 8, 64], "rbias": [128, 64],
    "wgu": [65, D, 512], "wdn": [65, 256, D], "fg": [128, 8],
}


class _SkipNSA(Exception):
    pass


class _NSAScope(ExitStack):
    def __exit__(self, et, ev, tb):
        super().__exit__(None, None, None)
        return et is _SkipNSA


def build(stop_after=None, dbg=(), with_moe=True, enable_nsa=True, n_experts=65):
    nc = bass.Bass("TRN2", target_bir_lowering=False)
    I = {}
    for k, shp in IN_SHAPES.items():
        if not with_moe and k in ("wgu", "wdn"):
            continue
        I[k] = nc.dram_tensor(k, list(shp), F32, kind="ExternalInput").ap()
    for k, (shp, dt) in CONST_SHAPES.items():
        I[k] = nc.dram_tensor(k, list(shp), dt, kind="ExternalInput").ap()
    outT = nc.dram_tensor("outT", [D, SO], F32, kind="ExternalOutput").ap()
    dbg_out = {}
    with ExitStack() as es:
        kb = KB(nc, es)
        E = es.enter_context

        uid = [0]

        def sb(name, shape, dt=F32, stack=None):
            uid[0] += 1
            return (stack or es).enter_context(nc.sbuf_tensor(f"sb{uid[0]}_" + name, list(shape), dt))

        ps = [E(nc.psum_tensor(f"ps{i}", [128, 512], F32)) for i in range(8)]
        PS = [Buf(p[:], excl=True) for p in ps]

        def dump(name, ap, shape, dt=F32):
            t = nc.dram_tensor("dbg_" + name, list(shape), dt, kind="ExternalOutput").ap()
            dbg_out[name] = t
            kb.barrier()
            kb.dma("sync", t, ap)
            kb.barrier()

        ident = sb("ident", [128, 128], BF16)
        onesf = sb("onesf", [128, 128], F32)
        isel0 = sb("isel0", [128, 128], BF16)
        isel1 = sb("isel1", [128, 128], BF16)
        pswap = sb("pswap", [128, 128], BF16)
        hmask = sb("hmask", [128, 128], BF16)
        segm = sb("segm", [128, 512], F32)
        CST = Buf(ident[:])
        for nm, t in (("ident", ident), ("onesf", onesf), ("isel0", isel0), ("isel1", isel1), ("pswap", pswap),
                      ("hmask", hmask), ("segm", segm)):
            kb.dma("sync", t[:], I[nm], outs=[CST])
        modcol = sb("modcol", [128, 48], F32)
        a1 = sb("a1", [128, 8], F32)
        a2 = sb("a2", [128, 8], F32)
        MOD = Buf(modcol[:])
        oT = sb("oT", [128, 8, SO], BF16)
        OT = [[Buf(oT[:, j, s * 128:(s + 1) * 128]) for s in range(16)] for j in range(8)]

        with ExitStack() as p0:
            cT = sb("cT", [128, 8], F32, p0)
            cs = sb("cs", [128, 8], F32, p0)
            bada = sb("bada", [1, 6 * D], F32, p0)
            modrow = sb("modrow", [1, 6 * D], F32, p0)
            one1 = sb("one1", [1, 1], F32, p0)
            n1g = sb("n1g", [128, 8], F32, p0)
            n2g = sb("n2g", [128, 8], F32, p0)
            wab = [sb(f"wab{i}", [128, 8, 512], F32, p0) for i in range(2)]
            WAB = [Buf(w[:]) for w in wab]
            SM = Buf(cT[:])
            MR = Buf(modrow[:])
            kb.dma("sync", cT[:], I["cT"], outs=[SM])
            kb.dma("sync", bada[:], I["b_ada"], outs=[SM])
            kb.dma("sync", n1g[:], I["n1g"], outs=[SM])
            kb.dma("sync", n2g[:], I["n2g"], outs=[SM])
            kb.op("vector", lambda e: e.memset(one1[:], 1.0), outs=[SM])
            kb.op("scalar", lambda e: e.activation(out=cs[:], in_=cT[:], func=AF.Silu), outs=[SM], ins=[SM])
            wada_v = I["w_ada"].rearrange("(k p) c -> p k c", p=128)
            for cb in range(12):
                W = WAB[cb % 2]
                kb.dma("sync" if cb % 2 == 0 else "gpsimd", wab[cb % 2][:], wada_v[:, :, cb * 512:(cb + 1) * 512], outs=[W])
                P = PS[cb % 2]
                for k in range(8):
                    kb.op("tensor", lambda e, k=k, cb=cb: e.matmul(ps[cb % 2][0:1, :], lhsT=cs[:, k:k + 1], rhs=wab[cb % 2][:, k, :],
                                                                 start=(k == 0), stop=(k == 7)),
                          outs=[P], ins=[SM, W], mark=(k == 7))
                kb.op("vector", lambda e, cb=cb: e.tensor_tensor(out=modrow[0:1, cb * 512:(cb + 1) * 512], in0=ps[cb % 2][0:1, :],
                                                                  in1=bada[0:1, cb * 512:(cb + 1) * 512], op=ALU.add),
                      outs=[MR], ins=[P, SM])
            P = PS[2]
            for j in range(48):
                kb.op("tensor", lambda e, j=j: e.matmul(ps[2][:, j:j + 1], lhsT=modrow[0:1, j * 128:(j + 1) * 128], rhs=one1[0:1, 0:1],
                                                       start=True, stop=True), outs=[P], ins=[MR, SM], mark=(j == 47))
            kb.op("vector", lambda e: e.tensor_copy(out=modcol[:], in_=ps[2][:, 0:48]), outs=[MOD], ins=[P])
            kb.op("vector", lambda e: e.scalar_tensor_tensor(out=a1[:], in0=modcol[:, 8:16], scalar=1.0, in1=n1g[:], op0=ALU.add, op1=ALU.mult),
                  outs=[MOD], ins=[MOD, SM])
            kb.op("vector", lambda e: e.scalar_tensor_tensor(out=a2[:], in0=modcol[:, 32:40], scalar=1.0, in1=n2g[:], op0=ALU.add, op1=ALU.mult),
                  outs=[MOD], ins=[MOD, SM])
            if "mod" in dbg:
                dump("mod", modcol[:], [128, 48])
            kb.barrier()
        sh1 = lambda k: modcol[:, k:k + 1]
        g1c = lambda k: modcol[:, 16 + k:17 + k]
        sh2 = lambda k: modcol[:, 24 + k:25 + k]
        g2c = lambda k: modcol[:, 40 + k:41 + k]

        if stop_after == "p0":
            kb.barrier()
            return nc, dbg_out

        with ExitStack() as p1:
            hT = sb("hT", [128, 8, S], BF16, p1)
            HT = [Buf(hT[:, :, n * 512:(n + 1) * 512]) for n in range(NB)]
            with ExitStack() as p1a:
                xb = [sb(f"xb{i}", [128, 8, 512], F32, p1a) for i in range(2)]
                XB = [Buf(t[:]) for t in xb]
                sq = sb("sq", [128, 8, 512], F32, p1a)
                SQ = Buf(sq[:])
                rstd = sb("rstd", [128, 512], F32, p1a)
                RS = Buf(rstd[:])
                tmp = [sb(f"tmp{i}", [128, 512], F32, p1a) for i in range(2)]
                TMP = [Buf(t[:]) for t in tmp]
                xT_v = I["xT"].rearrange("(k p) t -> p k t", p=128)
                for n in range(NB):
                    X = XB[n % 2]
                    x_ = xb[n % 2]
                    kb.dma("sync" if n % 2 == 0 else "gpsimd", x_[:], xT_v[:, :, n * 512:(n + 1) * 512], outs=[X])
                    kb.op("scalar", lambda e, x_=x_: e.activation(out=sq[:], in_=x_[:], func=AF.Square), outs=[SQ], ins=[X])
                    P = PS[n % 2]
                    for k in range(8):
                        kb.op("tensor", lambda e, k=k, n=n: e.matmul(ps[n % 2][:], lhsT=onesf[:], rhs=sq[:, k, :], start=(k == 0), stop=(k == 7)),
                              outs=[P], ins=[SQ, CST], mark=(k == 7))
                    kb.op("scalar", lambda e, n=n: e.activation(out=rstd[:], in_=ps[n % 2][:], func=AF.Sqrt, scale=1.0 / D, bias=EPS),
                          outs=[RS], ins=[P])
                    kb.op("vector", lambda e: e.reciprocal(out=rstd[:], in_=rstd[:]), outs=[RS], ins=[RS])
                    for k in range(8):
                        T = TMP[k % 2]
                        t_ = tmp[k % 2]
                        kb.op("vector", lambda e, k=k, t_=t_, x_=x_: e.tensor_tensor(out=t_[:], in0=x_[:, k, :], in1=rstd[:], op=ALU.mult),
                              outs=[T], ins=[X, RS])
                        kb.op("scalar", lambda e, k=k, t_=t_, n=n: e.activation(out=hT[:, k, n * 512:(n + 1) * 512], in_=t_[:], func=AF.Identity,
                                                                            scale=a1[:, k:k + 1], bias=sh1(k)),
                              outs=[HT[n]], ins=[T, MOD])
                kb.barrier()
            if "hT" in dbg:
                dump("hT", hT[:], [128, 8, S], BF16)
            if stop_after == "p1a":
                kb.barrier()
                return nc, dbg_out

            rr = [0]

            def bank():
                rr[0] = (rr[0] + 1) % 8
                return rr[0]

            w_in_v = I["w_in"].rearrange("(k p) c -> p k c", p=128)

            with ExitStack() as ph:
                lbl = sb("lbl", [128, 2, 4], F32, ph)
                lb = sb("lb", [128, 4], F32, ph)
                oml = sb("oml", [128, 4], F32, ph)
                hng = sb("hng", [128, 512], F32, ph)
                HC = Buf(lbl[:])
                kb.dma("sync", lbl[:], I["lbl"], outs=[HC])
                kb.dma("sync", hng[:], I["hng"], outs=[HC])
                kb.op("vector", lambda e: e.tensor_tensor(out=lb[:], in0=lbl[:, 0, :], in1=lbl[:, 1, :], op=ALU.subtract), outs=[HC], ins=[HC])
                kb.op("scalar", lambda e: e.activation(out=lb[:], in_=lb[:], func=AF.Sigmoid), outs=[HC], ins=[HC])
                kb.op("vector", lambda e: e.tensor_scalar(out=oml[:], in0=lb[:], scalar1=-1.0, scalar2=1.0, op0=ALU.mult, op1=ALU.add), outs=[HC], ins=[HC])
                wq = sb("wq", [128, 8, 128], BF16, ph)
                wf = sb("wf", [128, 8, 128], BF16, ph)
                wig = sb("wig", [128, 8, 256], BF16, ph)
                WQ, WF, WIG = Buf(wq[:]), Buf(wf[:]), Buf(wig[:])
                Q1 = sb("Q1", [128, S], BF16, ph)
                Q2 = sb("Q2", [128, S], BF16, ph)
                Kt = sb("Kt", [128, S], BF16, ph)
                Kh = sb("Kh", [128, NT, 128], BF16, ph)
                Vh = sb("Vh", [128, NT, 128], BF16, ph)
                SGt = sb("SGt", [128, NT, 128], BF16, ph)
                ebl = sb("ebl", [128, NT], F32, ph)
                BQ = [Buf(Q1[:, n * 512:(n + 1) * 512]) for n in range(NB)]
                BKH = [Buf(Kh[:, t, :]) for t in range(NT)]
                BV = [Buf(Vh[:, t, :]) for t in range(NT)]
                tn = ["f", "lf", "b", "d1", "d2", "eb", "e1", "en1", "el", "k"]
                T_ = {n_: sb("t_" + n_, [128, 512], F32, ph) for n_ in tn}
                TB = {n_: Buf(T_[n_][:]) for n_ in tn}
                khtb = sb("khtb", [128, 512], BF16, ph)
                KHTB = Buf(khtb[:])
                Sst = sb("Sst", [128, 128], F32, ph)
                SST = Buf(Sst[:])
                sbf = [sb(f"sbf{i}", [128, 128], BF16, ph) for i in range(2)]
                SBF = [Buf(t[:]) for t in sbf]
                atm = [sb(f"atm{i}", [128, 128], BF16, ph) for i in range(2)]
                ATM = [Buf(t[:]) for t in atm]
                for i in range(2):
                    kb.op("vector", lambda e, i=i: e.memset(atm[i][:], 0.0), outs=[ATM[i]])
                junk = sb("junk", [128, 128], F32, ph)
                JK = Buf(junk[:])
                ssq = [sb(f"ssq{i}", [128, 1], F32, ph) for i in range(2)]
                SSQ = [Buf(t[:]) for t in ssq]
                of = [sb(f"of{i}", [128, 128], F32, ph) for i in range(2)]
                OF = [Buf(t[:]) for t in of]
                obf = [sb(f"obf{i}", [128, 128], BF16, ph) for i in range(2)]
                OBF = [Buf(t[:]) for t in obf]
                v4 = lambda ap: ap.rearrange("p (c t) -> p c t", t=128)
                for hd in range(int(_os0.environ.get("NHEADS", "4"))):
                    c0 = hd * 128
                    kb.dma("gpsimd", wq[:], w_in_v[:, :, c0:c0 + 128], outs=[WQ])
                    kb.dma("gpsimd", wf[:], w_in_v[:, :, 512 + c0:512 + c0 + 128], outs=[WF])
                    kb.dma("gpsimd", wig[:, :, 0:128], w_in_v[:, :, 1024 + c0:1024 + c0 + 128], outs=[WIG])
                    kb.dma("gpsimd", wig[:, :, 128:256], w_in_v[:, :, 1536 + c0:1536 + c0 + 128], outs=[WIG])
                    for n in range(NB):
                        sl = slice(n * 512, (n + 1) * 512)
                        bq_, bf_ = bank(), bank()
                        for k in range(8):
                            kb.op("tensor", lambda e, k=k, bq_=bq_, sl=sl: e.matmul(ps[bq_][:], lhsT=wq[:, k, :], rhs=hT[:, k, sl], start=(k == 0), stop=(k == 7)),
                                  outs=[PS[bq_]], ins=[WQ, HT[n]], mark=(k == 7))
                        for k in range(8):
                            kb.op("tensor", lambda e, k=k, bf_=bf_, sl=sl: e.matmul(ps[bf_][:], lhsT=wf[:, k, :], rhs=hT[:, k, sl], start=(k == 0), stop=(k == 7)),
                                  outs=[PS[bf_]], ins=[WF, HT[n]], mark=(k == 7))
                        t = T_
                        kb.op("scalar", lambda e, bf_=bf_: e.activation(out=t["f"][:], in_=ps[bf_][:], func=AF.Sigmoid), outs=[TB["f"]], ins=[PS[bf_]])
                        kb.op("vector", lambda e, hd=hd: e.tensor_scalar(out=t["f"][:], in0=t["f"][:], scalar1=oml[:, hd:hd + 1], scalar2=lb[:, hd:hd + 1],
                                                                     op0=ALU.mult, op1=ALU.add), outs=[TB["f"]], ins=[TB["f"], HC])
                        kb.op("scalar", lambda e: e.activation(out=t["lf"][:], in_=t["f"][:], func=AF.Ln), outs=[TB["lf"]], ins=[TB["f"]])
                        kb.op("gpsimd", lambda e: e.tensor_scalar(out=t["k"][:], in0=t["f"][:], scalar1=-1.0, scalar2=1.0, op0=ALU.mult, op1=ALU.add),
                              outs=[TB["k"]], ins=[TB["f"]])
                        kb.op("vector", lambda e: e.tensor_tensor_scan(out=t["b"][:], data0=segm[:], data1=t["lf"][:], initial=0.0, op0=ALU.mult, op1=ALU.add),
                              outs=[TB["b"]], ins=[TB["lf"], CST])
                        kb.op("vector", lambda e: e.tensor_tensor(out=v4(t["d1"][:]), in0=v4(t["b"][:]), in1=v4(t["b"][:])[:, :, 63:64].to_broadcast([128, 4, 128]),
                                                                  op=ALU.subtract), outs=[TB["d1"]], ins=[TB["b"]])
                        kb.op("vector", lambda e: e.tensor_tensor(out=v4(t["d2"][:]), in0=v4(t["b"][:])[:, :, 127:128].to_broadcast([128, 4, 128]), in1=v4(t["b"][:]),
                                                                  op=ALU.subtract), outs=[TB["d2"]], ins=[TB["b"]])
                        kb.op("scalar", lambda e: e.activation(out=t["eb"][:], in_=t["b"][:], func=AF.Exp), outs=[TB["eb"]], ins=[TB["b"]])
                        kb.op("scalar", lambda e: e.activation(out=t["e1"][:], in_=t["d1"][:], func=AF.Exp), outs=[TB["e1"]], ins=[TB["d1"]])
                        kb.op("scalar", lambda e: e.activation(out=t["en1"][:], in_=t["d1"][:], func=AF.Exp, scale=-1.0), outs=[TB["en1"]], ins=[TB["d1"]])
                        kb.op("scalar", lambda e: e.activation(out=t["el"][:], in_=t["d2"][:], func=AF.Exp), outs=[TB["el"]], ins=[TB["d2"]])
                        sc_ = 128.0 ** -0.5
                        kb.op("vector", lambda e, bq_=bq_, sl=sl: e.scalar_tensor_tensor(out=Q1[:, sl], in0=ps[bq_][:], scalar=sc_, in1=t["e1"][:], op0=ALU.mult, op1=ALU.mult),
                              outs=[BQ[n]], ins=[PS[bq_], TB["e1"]])
                        kb.op("vector", lambda e, bq_=bq_, sl=sl: e.scalar_tensor_tensor(out=Q2[:, sl], in0=ps[bq_][:], scalar=sc_, in1=t["eb"][:], op0=ALU.mult, op1=ALU.mult),
                              outs=[BQ[n]], ins=[PS[bq_], TB["eb"]])
                        kb.op("gpsimd", lambda e, sl=sl: e.tensor_tensor(out=Kt[:, sl], in0=t["k"][:], in1=t["en1"][:], op=ALU.mult), outs=[BQ[n]], ins=[TB["k"], TB["en1"]])
                        kb.op("gpsimd", lambda e: e.tensor_tensor(out=khtb[:], in0=t["k"][:], in1=t["el"][:], op=ALU.mult), outs=[KHTB], ins=[TB["k"], TB["el"]])
                        kb.op("gpsimd", lambda e, n=n: e.tensor_copy(out=ebl[:, 4 * n:4 * n + 4], in_=v4(t["eb"][:])[:, :, 127]), outs=[BQ[n]], ins=[TB["eb"]])
                        for i in range(4):
                            bk = bank()
                            kb.op("tensor", lambda e, i=i, bk=bk: e.matmul(ps[bk][:, 0:128], lhsT=khtb[:, i * 128:(i + 1) * 128], rhs=ident[:], start=True, stop=True),
                                  outs=[PS[bk]], ins=[KHTB, CST])
                            kb.op("scalar", lambda e, i=i, bk=bk, n=n: e.copy(out=Kh[:, 4 * n + i, :], in_=ps[bk][:, 0:128]), outs=[BKH[4 * n + i]], ins=[PS[bk]])
                    for tt in range(NT):
                        bk = bank()
                        n = tt // 4
                        for k in range(8):
                            kb.op("tensor", lambda e, k=k, bk=bk, tt=tt: e.matmul(ps[bk][:, 0:256], lhsT=hT[:, k, tt * 128:(tt + 1) * 128], rhs=wig[:, k, :],
                                                                                 start=(k == 0), stop=(k == 7)),
                                  outs=[PS[bk]], ins=[WIG, HT[n]], mark=(k == 7))
                        kb.op("vector", lambda e, bk=bk, tt=tt: e.tensor_copy(out=Vh[:, tt, :], in_=ps[bk][:, 0:128]), outs=[BV[tt]], ins=[PS[bk]])
                        kb.op("scalar", lambda e, bk=bk, tt=tt: e.activation(out=SGt[:, tt, :], in_=ps[bk][:, 128:256], func=AF.Silu), outs=[BV[tt]], ins=[PS[bk]])
                    kb.op("vector", lambda e: e.memset(Sst[:], 0.0), outs=[SST])
                    at_bank = {}

                    def emit_at(c):
                        bk = bank()
                        at_bank[c] = bk
                        cs_ = slice(c * 128, (c + 1) * 128)
                        c0_ = c * 128
                        kb.op("tensor", lambda e: e.matmul(ps[bk][0:64, 0:64], lhsT=Kt[:, c0_:c0_ + 64], rhs=Q1[:, c0_:c0_ + 64], start=True, stop=True),
                              outs=[PS[bk]], ins=[BQ[c // 4]], mark=False)
                        kb.op("tensor", lambda e: e.matmul(ps[bk][:, 64:128], lhsT=Kt[:, cs_], rhs=Q1[:, c0_ + 64:c0_ + 128], start=True, stop=True),
                              outs=[PS[bk]], ins=[BQ[c // 4]])
                        kb.op("vector", lambda e: e.tensor_tensor(out=atm[c % 2][0:64, 0:64], in0=ps[bk][0:64, 0:64], in1=hmask[0:64, 0:64], op=ALU.mult),
                              outs=[ATM[c % 2]], ins=[PS[bk], CST])
                        kb.op("vector", lambda e: e.tensor_tensor(out=atm[c % 2][:, 64:128], in0=ps[bk][:, 64:128], in1=hmask[:, 64:128], op=ALU.mult),
                              outs=[ATM[c % 2]], ins=[PS[bk], CST])

                    emit_at(0)
                    for c in range(NT):
                        if c + 1 < NT:
                            emit_at(c + 1)
                        cs_ = slice(c * 128, (c + 1) * 128)
                        bd, bo = bank(), bank()
                        kb.op("tensor", lambda e, bd=bd, c=c: e.matmul(ps[bd][:, 0:128], lhsT=Kh[:, c, :], rhs=Vh[:, c, :], start=True, stop=True),
                              outs=[PS[bd]], ins=[BKH[c], BV[c]])
                        kb.op("tensor", lambda e, bo=bo, c=c: e.matmul(ps[bo][:, 0:128], lhsT=atm[c % 2][:], rhs=Vh[:, c, :], start=True, stop=(c == 0)),
                              outs=[PS[bo]], ins=[ATM[c % 2], BV[c]], mark=(c == 0))
                        if c > 0:
                            kb.op("tensor", lambda e, bo=bo, c=c, cs_=cs_: e.matmul(ps[bo][:, 0:128], lhsT=Q2[:, cs_], rhs=sbf[(c - 1) % 2][:], start=False, stop=True),
                                  outs=[PS[bo]], ins=[BQ[c // 4], SBF[(c - 1) % 2]])
                        if c + 1 < NT:
                            kb.op("vector", lambda e, bd=bd, c=c: e.scalar_tensor_tensor(out=Sst[:], in0=Sst[:], scalar=ebl[:, c:c + 1], in1=ps[bd][:, 0:128],
                                                                                     op0=ALU.mult, op1=ALU.add), outs=[SST], ins=[SST, PS[bd], BQ[c // 4]])
                            kb.op("scalar", lambda e, c=c: e.copy(out=sbf[c % 2][:], in_=Sst[:]), outs=[SBF[c % 2]], ins=[SST])
                        i2 = c % 2
                        kb.op("gpsimd", lambda e, i2=i2: e.memset(ssq[i2][:], 0.0), outs=[SSQ[i2]])
                        kb.op("scalar", lambda e, bo=bo, i2=i2: e.activation(out=junk[:], in_=ps[bo][:, 0:128], func=AF.Square, accum_out=ssq[i2][:]),
                              outs=[JK, SSQ[i2]], ins=[PS[bo]])
                        kb.op("scalar", lambda e, i2=i2: e.activation(out=ssq[i2][:], in_=ssq[i2][:], func=AF.Sqrt, scale=1.0 / 128, bias=EPS), outs=[SSQ[i2]], ins=[SSQ[i2]])
                        kb.op("vector", lambda e, i2=i2: e.reciprocal(out=ssq[i2][:], in_=ssq[i2][:]), outs=[SSQ[i2]], ins=[SSQ[i2]])
                        kb.op("vector", lambda e, bo=bo, i2=i2, c0=c0: e.scalar_tensor_tensor(out=of[i2][:], in0=ps[bo][:, 0:128], scalar=ssq[i2][:, 0:1], in1=hng[:, c0:c0 + 128],
                                                                                           op0=ALU.mult, op1=ALU.mult), outs=[OF[i2]], ins=[PS[bo], SSQ[i2], HC])
                        kb.op("gpsimd", lambda e, i2=i2, c=c: e.tensor_tensor(out=obf[i2][:], in0=of[i2][:], in1=SGt[:, c, :], op=ALU.mult), outs=[OBF[i2]], ins=[OF[i2], BV[c]])
                        bt = bank()
                        isel = isel0 if c < 16 else isel1
                        kb.op("tensor", lambda e, bt=bt, i2=i2, isel=isel: e.matmul(ps[bt][:, 0:128], lhsT=obf[i2][:], rhs=isel[:], start=True, stop=True),
                              outs=[PS[bt]], ins=[OBF[i2], CST])
                        s_ = c % 16
                        if c < 16:
                            kb.op("scalar", lambda e, bt=bt, s_=s_, hd=hd: e.copy(out=oT[:, hd, s_ * 128:(s_ + 1) * 128], in_=ps[bt][:, 0:128]), outs=[OT[hd][s_]], ins=[PS[bt]])
                        else:
                            kb.op("vector", lambda e, bt=bt, s_=s_, hd=hd: e.tensor_tensor(out=oT[:, hd, s_ * 128:(s_ + 1) * 128], in0=ps[bt][:, 0:128],
                                                                                       in1=oT[:, hd, s_ * 128:(s_ + 1) * 128], op=ALU.add),
                                  outs=[OT[hd][s_]], ins=[PS[bt], OT[hd][s_]])
                kb.barrier()
            if "oT" in dbg:
                dump("oT", oT[:], [128, 8, SO], BF16)
            if stop_after == "p1b":
                kb.barrier()
                return nc, dbg_out

            SCL = 64.0 ** -0.5
            if not enable_nsa:
                for jf in range(4, 8):
                    kb.op("vector", lambda e, jf=jf: e.memset(oT[:, jf, :], 0.0), outs=OT[jf])
            with _NSAScope() as pn:
                if not enable_nsa:
                    raise _SkipNSA()
                ovl = sb("ovl", [128, 2, 64], BF16, pn)
                kb.dma("sync", ovl[:], I["ovl"], outs=[CST])
                ksT = sb("ksT", [128, S], BF16, pn)
                kwT = sb("kwT", [128, S], BF16, pn)
                kcvT = sb("kcvT", [128, S], BF16, pn)
                vs1 = sb("vs1", [128, NT, 80], BF16, pn)
                vw1 = sb("vw1", [128, NT, 80], BF16, pn)
                KS = Buf(ksT[:])
                kcmpT = sb("kcmpT", [128, 256], BF16, pn)
                vcmp1 = sb("vcmp1", [128, 2, 144], BF16, pn)
                KC = Buf(kcmpT[:])
                wk3 = sb("wk3", [128, 8, 384], BF16, pn)
                wv2 = sb("wv2", [128, 8, 128], BF16, pn)
                wqg = sb("wqg", [128, 8, 256], BF16, pn)
                wgt = sb("wgt", [128, 8, 12], BF16, pn)
                WN = Buf(wk3[:])
                cosb = sb("cosb", [128, 512], F32, pn)
                sinb = sb("sinb", [128, 512], F32, pn)
                CSB = Buf(cosb[:])
                rawb = sb("rawb", [128, 512], BF16, pn)
                RAWB = Buf(rawb[:])
                rt1 = sb("rt1", [128, 512], F32, pn)
                rt2 = sb("rt2", [128, 512], F32, pn)
                RT1, RT2 = Buf(rt1[:]), Buf(rt2[:])
                _padn = int(_os0.environ.get("PADN", "0"))
                if _padn:
                    _pad = sb("padn", [128, _padn], F32, pn)
                srr = [0]

                def sbank():
                    srr[0] = (srr[0] + 1) % 3
                    return srr[0]

                mrr = [0]

                def mbank():
                    return 7

                import os as _os
                _dbgmode = int(_os.environ.get("ROPEDBG", "0"))

                def rope_from(bk, dst_ap, dstbuf):
                    if _dbgmode == 1:
                        kb.op("scalar", lambda e: e.copy(out=dst_ap, in_=ps[bk][:]), outs=[dstbuf], ins=[PS[bk]])
                        return
                    if _dbgmode == 3:
                        kb.op("vector", lambda e: e.tensor_tensor(out=rt1[:], in0=ps[bk][:], in1=cosb[:], op=ALU.mult), outs=[RT1], ins=[PS[bk], CSB])
                        kb.op("gpsimd", lambda e: e.tensor_copy(out=dst_ap, in_=rt1[:]), outs=[dstbuf], ins=[RT1])
                        return
                    if _dbgmode == 4:
                        kb.op("scalar", lambda e: e.copy(out=rawb[:], in_=ps[bk][:]), outs=[RAWB], ins=[PS[bk]])
                        b2 = mbank()
                        kb.op("tensor", lambda e: e.matmul(ps[b2][:], lhsT=pswap[:], rhs=rawb[:], start=True, stop=True), outs=[PS[b2]], ins=[RAWB, CST])
                        kb.op("vector", lambda e: e.tensor_tensor(out=rt1[:], in0=ps[bk][:], in1=cosb[:], op=ALU.mult), outs=[RT1], ins=[PS[bk], CSB])
                        kb.op("vector", lambda e: e.tensor_tensor(out=rt2[:], in0=ps[b2][:], in1=sinb[:], op=ALU.mult), outs=[RT2], ins=[PS[b2], CSB])
                        kb.op("vector", lambda e: e.tensor_tensor(out=dst_ap, in0=rt1[:], in1=rt2[:], op=ALU.add), outs=[dstbuf], ins=[RT1, RT2])
                        return
                    if _dbgmode == 5:
                        kb.op("scalar", lambda e: e.copy(out=rawb[:], in_=ps[bk][:]), outs=[RAWB], ins=[PS[bk]])
                        b2 = mbank()
                        kb.op("tensor", lambda e: e.matmul(ps[b2][:], lhsT=pswap[:], rhs=rawb[:], start=True, stop=True), outs=[PS[b2]], ins=[RAWB, CST])
                        kb.op("vector", lambda e: e.tensor_tensor(out=rt1[:], in0=ps[bk][:], in1=cosb[:], op=ALU.mult), outs=[RT1], ins=[PS[bk], CSB])
                        kb.op("scalar", lambda e: e.copy(out=rt2[:], in_=ps[b2][:]), outs=[RT2], ins=[PS[b2]])
                        _sb = cosb if _os.environ.get("USECOS") else sinb
                        kb.op("vector", lambda e: e.tensor_tensor(out=rt2[:], in0=rt2[:], in1=_sb[:], op=ALU.mult), outs=[RT2], ins=[RT2, CSB])
                        kb.op("vector", lambda e: e.tensor_tensor(out=dst_ap, in0=rt1[:], in1=rt2[:], op=ALU.add), outs=[dstbuf], ins=[RT1, RT2])
                        return
                    if _dbgmode in (7, 8):
                        kb.op("scalar", lambda e: e.copy(out=rawb[:], in_=ps[bk][:]), outs=[RAWB], ins=[PS[bk]])
                        b2 = mbank()
                        kb.op("tensor", lambda e: e.matmul(ps[b2][:], lhsT=pswap[:], rhs=rawb[:], start=True, stop=True), outs=[PS[b2]], ins=[RAWB, CST])
                        kb.op("vector", lambda e: e.tensor_tensor(out=rt1[:], in0=ps[bk][:], in1=cosb[:], op=ALU.mult), outs=[RT1], ins=[PS[bk], CSB])
                        kb.op("scalar", lambda e: e.copy(out=rt2[:], in_=ps[b2][:]), outs=[RT2], ins=[PS[b2]])
                        kb.op("vector", lambda e: e.tensor_tensor(out=rt2[:], in0=rt2[:], in1=sinb[:], op=ALU.mult), outs=[RT2], ins=[RT2, CSB])
                        if _dbgmode == 8:
                            kb.op("vector", lambda e: e.tensor_tensor(out=rt1[:], in0=rt1[:], in1=rt2[:], op=ALU.add), outs=[RT1], ins=[RT1, RT2])
                        kb.op("scalar", lambda e: e.copy(out=dst_ap, in_=rt1[:]), outs=[dstbuf], ins=[RT1])
                        return
                    if _dbgmode in (9, 10):
                        kb.op("vector", lambda e: e.tensor_tensor(out=rt1[:], in0=ps[bk][:], in1=cosb[:], op=ALU.mult), outs=[RT1], ins=[PS[bk], CSB])
                        if _dbgmode == 9:
                            kb.op("scalar", lambda e: e.copy(out=rt2[:], in_=ps[bk][:]), outs=[RT2], ins=[PS[bk]])
                        else:
                            kb.op("vector", lambda e: e.tensor_tensor(out=rt2[:], in0=rt1[:], in1=cosb[:], op=ALU.mult), outs=[RT2], ins=[RT1, CSB])
                        kb.op("gpsimd", lambda e: e.tensor_copy(out=dst_ap, in_=rt1[:]), outs=[dstbuf], ins=[RT1])
                        return
                    if _dbgmode in (11, 12):
                        kb.op("scalar", lambda e: e.copy(out=rawb[:], in_=ps[bk][:]), outs=[RAWB], ins=[PS[bk]])
                        b2 = mbank()
                        kb.op("tensor", lambda e: e.matmul(ps[b2][:], lhsT=pswap[:], rhs=rawb[:], start=True, stop=True), outs=[PS[b2]], ins=[RAWB, CST])
                        kb.op("vector", lambda e: e.tensor_tensor(out=rt1[:], in0=ps[bk][:], in1=cosb[:], op=ALU.mult), outs=[RT1], ins=[PS[bk], CSB, RAWB])
                        kb.op("gpsimd", lambda e: e.tensor_copy(out=dst_ap, in_=rt1[:]), outs=[dstbuf], ins=[RT1])
                        if _dbgmode == 12:
                            return
                        kb.op("vector", lambda e: e.tensor_tensor(out=rt1[:], in0=ps[b2][:], in1=sinb[:], op=ALU.mult), outs=[RT1], ins=[PS[b2], CSB])
                        kb.op("gpsimd", lambda e: e.tensor_tensor(out=dst_ap, in0=dst_ap, in1=rt1[:], op=ALU.add), outs=[dstbuf], ins=[RT1, dstbuf])
                        return
                    if _dbgmode == 2:
                        kb.op("scalar", lambda e: e.copy(out=rawb[:], in_=ps[bk][:]), outs=[RAWB], ins=[PS[bk]])
                        b2 = mbank()
                        kb.op("tensor", lambda e: e.matmul(ps[b2][:], lhsT=pswap[:], rhs=rawb[:], start=True, stop=True), outs=[PS[b2]], ins=[RAWB, CST])
                        kb.op("scalar", lambda e: e.copy(out=dst_ap, in_=ps[b2][:]), outs=[dstbuf], ins=[PS[b2]])
                        return
                    kb.op("scalar", lambda e: e.copy(out=rawb[:], in_=ps[bk][:]), outs=[RAWB], ins=[PS[bk]])
                    b2 = mbank()
                    kb.op("tensor", lambda e: e.matmul(ps[b2][:], lhsT=pswap[:], rhs=rawb[:], start=True, stop=True), outs=[PS[b2]], ins=[RAWB, CST])
                    kb.op("vector", lambda e: e.tensor_tensor(out=rt1[:], in0=ps[bk][:], in1=cosb[:], op=ALU.mult), outs=[RT1], ins=[PS[bk], CSB])
                    kb.op("vector", lambda e: e.tensor_tensor(out=rt2[:], in0=ps[b2][:], in1=sinb[:], op=ALU.mult), outs=[RT2], ins=[PS[b2], CSB])
                    kb.op("gpsimd", lambda e: e.tensor_tensor(out=dst_ap, in0=rt1[:], in1=rt2[:], op=ALU.add), outs=[dstbuf], ins=[RT1, RT2])

                for g in range(2):
                    for j, cbase in enumerate((2560, 2688)):
                        kb.dma("gpsimd", wk3[:, :, j * 64:(j + 1) * 64], w_in_v[:, :, cbase + g * 64:cbase + g * 64 + 64], outs=[WN])
                    for j, cbase in enumerate((2816, 2816, 3072, 3072)):
                        kb.dma("gpsimd", wk3[:, :, 128 + j * 64:128 + (j + 1) * 64], w_in_v[:, :, cbase + g * 64:cbase + g * 64 + 64], outs=[WN])
                    for j, cbase in enumerate((2944, 3200)):
                        kb.dma("gpsimd", wv2[:, :, j * 64:(j + 1) * 64], w_in_v[:, :, cbase + g * 64:cbase + g * 64 + 64], outs=[WN])
                    kb.dma("gpsimd", wqg[:], w_in_v[:, :, 2048 + g * 256:2048 + (g + 1) * 256], outs=[WN])
                    kb.dma("gpsimd", wgt[:], w_in_v[:, :, 3328 + g * 12:3328 + (g + 1) * 12], outs=[WN])
                    kb.op("vector", lambda e: e.memset(vs1[:, :, 64:65], 1.0), outs=[KS])
                    kb.op("vector", lambda e: e.memset(vw1[:, :, 64:65], 1.0), outs=[KS])
                    if stop_after == "p1c_a":
                        dump("wk3", wk3[:], [128, 8, 384], BF16)
                        return nc, dbg_out
                    for n in range(NB):
                        sl = slice(n * 512, (n + 1) * 512)
                        kb.dma("sync", cosb[:], I["cosT"][:, sl], outs=[CSB])
                        kb.dma("sync", sinb[:], I["sinT"][:, sl], outs=[CSB])
                        for j in range(3):
                            bk = sbank()
                            for k in range(8):
                                kb.op("tensor", lambda e, k=k, bk=bk, j=j: e.matmul(ps[bk][:], lhsT=wk3[:, k, j * 128:(j + 1) * 128], rhs=hT[:, k, sl],
                                                                                    start=(k == 0), stop=(k == 7)), outs=[PS[bk]], ins=[WN, HT[n]], mark=(k == 7))
                            if j == 0:
                                kb.op("scalar", lambda e, bk=bk: e.copy(out=kcvT[:, sl], in_=ps[bk][:]), outs=[KS], ins=[PS[bk]])
                            else:
                                rope_from(bk, (ksT if j == 1 else kwT)[:, sl], KS)
                        if stop_after == "p1c_b":
                            dump("ksT", ksT[:], [128, S], BF16)
                            return nc, dbg_out
                        for i in range(4):
                            tt = 4 * n + i
                            bk = mbank()
                            for k in range(8):
                                kb.op("tensor", lambda e, k=k, bk=bk, tt=tt: e.matmul(ps[bk][:, 0:128], lhsT=hT[:, k, tt * 128:(tt + 1) * 128], rhs=wv2[:, k, :],
                                                                                     start=(k == 0), stop=(k == 7)), outs=[PS[bk]], ins=[WN, HT[n]], mark=(k == 7))
                            kb.op("scalar", lambda e, bk=bk, tt=tt: e.copy(out=vs1[:, tt, 0:64], in_=ps[bk][:, 0:64]), outs=[KS], ins=[PS[bk]])
                            kb.op("vector", lambda e, bk=bk, tt=tt: e.tensor_copy(out=vw1[:, tt, 0:64], in_=ps[bk][:, 64:128]), outs=[KS], ins=[PS[bk]])
                    if stop_after == "p1c_k":
                        dump("ksT", ksT[:], [128, S], BF16)
                        dump("kcvT", kcvT[:], [128, S], BF16)
                        dump("vs1", vs1[:], [128, NT, 80], BF16)
                        return nc, dbg_out
                    with ExitStack() as pc:
                        w1kv = sb("w1kv", [128, 32, 256], BF16, pc)
                        peT = sb("peT", [128, 32], F32, pc)
                        peTb = sb("peTb", [128, 32], BF16, pc)
                        b1kv = sb("b1kv", [128, 4], F32, pc)
                        w2k2 = sb("w2k2", [128, 2, 128], BF16, pc)
                        w2v = sb("w2v", [128, 2, 64], BF16, pc)
                        hid = sb("hid", [128, 4, 256], BF16, pc)
                        beff = sb("beff", [128, 4], F32, pc)
                        gx = sb("gx", [128, 256], F32, pc)
                        gu = sb("gu", [128, 256], F32, pc)
                        gs = sb("gs", [128, 256], F32, pc)
                        CW = Buf(w1kv[:])
                        HID = Buf(hid[:])
                        GX = Buf(gx[:])
                        kb.dma("gpsimd", w1kv[0:64], I["w1k"], outs=[CW])
                        kb.dma("gpsimd", w1kv[64:128], I["w1v"], outs=[CW])
                        kb.dma("sync", peT[0:64], I["peTk"], outs=[CW])
                        kb.dma("sync", peT[64:128], I["peTv"], outs=[CW])
                        kb.dma("sync", b1kv[:, 0:2], I["b1k"], outs=[CW])
                        kb.dma("sync", b1kv[:, 2:4], I["b1v"], outs=[CW])
                        kb.dma("gpsimd", w2k2[:, :, 0:64], I["w2k"], outs=[CW])
                        kb.dma("gpsimd", w2k2[:, :, 64:128], I["w2k"], outs=[CW])
                        kb.dma("gpsimd", w2v[:], I["w2v"], outs=[CW])
                        kb.op("vector", lambda e: e.tensor_copy(out=peTb[:], in_=peT[:]), outs=[CW], ins=[CW])
                        kb.op("vector", lambda e: e.memset(hid[:], 0.0), outs=[HID])
                        for kv in range(2):
                            p0_ = kv * 64
                            for hc in range(2):
                                bk, bb = sbank(), mbank()
                                for l in range(32):
                                    kb.op("tensor", lambda e, l=l, bk=bk, hc=hc, p0_=p0_: e.matmul(ps[bk][:, 0:255], lhsT=w1kv[p0_:p0_ + 64, l, hc * 128:(hc + 1) * 128],
                                                                                                rhs=kcvT[p0_:p0_ + 64, l:l + 16 * 254 + 1:16], start=(l == 0), stop=(l == 31)),
                                          outs=[PS[bk]], ins=[CW, KS], mark=(l == 31))
                                for l in range(32):
                                    kb.op("tensor", lambda e, l=l, bb=bb, hc=hc, p0_=p0_: e.matmul(ps[bb][:, 0:1], lhsT=w1kv[p0_:p0_ + 64, l, hc * 128:(hc + 1) * 128],
                                                                                                rhs=peTb[p0_:p0_ + 64, l:l + 1], start=(l == 0), stop=(l == 31)),
                                          outs=[PS[bb]], ins=[CW], mark=(l == 31))
                                ci = kv * 2 + hc
                                kb.op("vector", lambda e, bb=bb, ci=ci: e.tensor_tensor(out=beff[:, ci:ci + 1], in0=ps[bb][:, 0:1], in1=b1kv[:, ci:ci + 1], op=ALU.add),
                                      outs=[GX], ins=[PS[bb], CW])
                                kb.op("vector", lambda e, bk=bk, ci=ci: e.tensor_scalar(out=gx[:, 0:255], in0=ps[bk][:, 0:255], scalar1=beff[:, ci:ci + 1], scalar2=None, op0=ALU.add),
                                      outs=[GX], ins=[PS[bk], GX])
                                kb.op("vector", lambda e: e.tensor_tensor(out=gu[:, 0:255], in0=gx[:, 0:255], in1=gx[:, 0:255], op=ALU.mult), outs=[GX], ins=[GX])
                                kb.op("vector", lambda e: e.tensor_scalar(out=gu[:, 0:255], in0=gu[:, 0:255], scalar1=0.044715, scalar2=1.0, op0=ALU.mult, op1=ALU.add), outs=[GX], ins=[GX])
                                kb.op("vector", lambda e: e.tensor_tensor(out=gu[:, 0:255], in0=gu[:, 0:255], in1=gx[:, 0:255], op=ALU.mult), outs=[GX], ins=[GX])
                                kb.op("scalar", lambda e: e.activation(out=gs[:, 0:255], in_=gu[:, 0:255], func=AF.Sigmoid, scale=1.5957691216057308), outs=[GX], ins=[GX])
                                kb.op("vector", lambda e, ci=ci: e.tensor_tensor(out=hid[:, ci, 0:255], in0=gx[:, 0:255], in1=gs[:, 0:255], op=ALU.mult), outs=[HID], ins=[GX])
                        bk = sbank()
                        for hc in range(2):
                            kb.op("tensor", lambda e, hc=hc, bk=bk: e.matmul(ps[bk][:, 0:256], lhsT=w2k2[:, hc, :], rhs=hid[:, hc, :], start=(hc == 0), stop=(hc == 1)),
                                  outs=[PS[bk]], ins=[CW, HID], mark=(hc == 1))
                        kb.op("scalar", lambda e, bk=bk: e.copy(out=kcmpT[:], in_=ps[bk][:, 0:256]), outs=[KC], ins=[PS[bk]])
                        for ct in range(2):
                            bk = sbank()
                            for hc in range(2):
                                kb.op("tensor", lambda e, hc=hc, bk=bk, ct=ct: e.matmul(ps[bk][:, 0:64], lhsT=hid[:, 2 + hc, ct * 128:(ct + 1) * 128], rhs=w2v[:, hc, :],
                                                                                       start=(hc == 0), stop=(hc == 1)), outs=[PS[bk]], ins=[CW, HID], mark=(hc == 1))
                            kb.op("scalar", lambda e, bk=bk, ct=ct: e.copy(out=vcmp1[:, ct, 0:64], in_=ps[bk][:, 0:64]), outs=[KC], ins=[PS[bk]])
                        kb.op("vector", lambda e: e.memset(vcmp1[:, :, 64:65], 1.0), outs=[KC])
                        kb.op("vector", lambda e: e.tensor_copy(out=vcmp1[:, :, 65:129], in_=ovl[:]), outs=[KC], ins=[CST])
                        kb.barrier()
                    if stop_after == "p1c_c":
                        dump("kcmpT", kcmpT[:], [128, 256], BF16)
                        dump("vcmp1", vcmp1[:], [128, 2, 144], BF16)
                        return nc, dbg_out
                    with ExitStack() as pq:
                        wband = sb("wband", [128, 8, 512], BF16, pq)
                        ekt = sb("ekt", [128, 32, 128], BF16, pq)
                        QC = Buf(wband[:])
                        kb.dma("sync", wband[:], I["wband"], outs=[QC])
                        kb.dma("sync", ekt[:], I["ekt"], outs=[QC])
                        qTb = sb("qTb", [128, 2, 512], BF16, pq)
                        qrTb = sb("qrTb", [128, 2, 512], BF16, pq)
                        QB_ = Buf(qTb[:])
                        QRB = Buf(qrTb[:])
                        gts = sb("gts", [128, 4, 12], F32, pq)
                        GTS = Buf(gts[:])
                        cmpbb = sb("cmpbb", [128, 512], BF16, pq)
                        CMB = Buf(cmpbb[:])
                        pt = [sb(f"pt{i}", [128, 512], BF16, pq) for i in range(3)]
                        PT = [Buf(t[:]) for t in pt]
                        onsa = sb("onsa", [128, 4, 256], F32, pq)
                        ONSA = Buf(onsa[:])
                        obn = sb("obn", [128, 4, 256], BF16, pq)
                        OBN = Buf(obn[:])
                        pslc = sb("pslc", [128, 4, 64], F32, pq)
                        PSLC = Buf(pslc[:])
                        sadd = sb("sadd", [128, 64], F32, pq)
                        SADD = Buf(sadd[:])
                        score = sb("score", [128, 64], F32, pq)
                        stmp = sb("stmp", [128, 64], F32, pq)
                        sel = sb("sel", [128, 64], F32, pq)
                        m8 = sb("m8", [128, 16], F32, pq)
                        negb = sb("negb", [128, 128], BF16, pq)
                        SEL = Buf(score[:])
                        negbT = sb("negbT", [128, 512], BF16, pq)
                        NBT = Buf(negbT[:])
                        rz = sb("rz", [128, 4], F32, pq)
                        RZ = Buf(rz[:])
                        accs = [ps[3][:, 0:129], ps[4][:, 0:129], ps[5][:, 0:129], ps[6][:, 0:129]]
                        ACC = [PS[3], PS[4], PS[5], PS[6]]
                        prr = [0]

                        def run_branch(steps):
                            prev = None
                            for st in steps + [None]:
                                if st is not None:
                                    bk = sbank()
                                    nm = len(st["s"])
                                    for idx, (l_, r_, insb) in enumerate(st["s"]):
                                        kb.op("tensor", lambda e, l_=l_, r_=r_, idx=idx, nm=nm, bk=bk: e.matmul(ps[bk][:], lhsT=l_, rhs=r_, start=(idx == 0), stop=(idx == nm - 1)),
                                              outs=[PS[bk]], ins=insb, mark=(idx == nm - 1))
                                    prr[0] = (prr[0] + 1) % 3
                                    pi = prr[0]
                                    kb.op("scalar", lambda e, bk=bk, pi=pi: e.activation(out=pt[pi][:], in_=ps[bk][:], func=AF.Exp, scale=SCL), outs=[PT[pi]], ins=[PS[bk]])
                                    st["pi"] = pi
                                if prev is not None:
                                    pi = prev["pi"]
                                    for (i, rhs_ap, w_, st_, sp_) in prev["pv"]:
                                        kb.op("tensor", lambda e, i=i, rhs_ap=rhs_ap, w_=w_, st_=st_, sp_=sp_, pi=pi: e.matmul(accs[i][:, 0:w_], lhsT=pt[pi][:, i * 128:(i + 1) * 128], rhs=rhs_ap,
                                                                                                                 start=st_, stop=sp_),
                                              outs=[ACC[i]], ins=[PT[pi], KS, KC])
                                prev = st

                        def finish(h, br, first):
                            zc = 64
                            for i in range(4):
                                kb.op("vector", lambda e, i=i: e.tensor_scalar(out=rz[:, i:i + 1], in0=accs[i][:, zc:zc + 1], scalar1=1e-30, scalar2=None, op0=ALU.max),
                                      outs=[RZ], ins=[ACC[i]])
                            kb.op("vector", lambda e: e.reciprocal(out=rz[:], in_=rz[:]), outs=[RZ], ins=[RZ])
                            if br == 0:
                                for i in range(4):
                                    if h == 0:
                                        kb.op("vector", lambda e, i=i: e.tensor_scalar(out=pslc[:, i, :], in0=accs[i][:, 65:129], scalar1=rz[:, i:i + 1], scalar2=None, op0=ALU.mult),
                                              outs=[PSLC], ins=[ACC[i], RZ])
                                    else:
                                        kb.op("vector", lambda e, i=i: e.scalar_tensor_tensor(out=pslc[:, i, :], in0=accs[i][:, 65:129], scalar=rz[:, i:i + 1], in1=pslc[:, i, :],
                                                                                           op0=ALU.mult, op1=ALU.add), outs=[PSLC], ins=[ACC[i], RZ, PSLC])
                            kb.op("vector", lambda e: e.tensor_tensor(out=rz[:], in0=rz[:], in1=gts[:, :, h * 3 + br], op=ALU.mult), outs=[RZ], ins=[RZ, GTS])
                            for i in range(4):
                                dst = onsa[:, i, h * 64:(h + 1) * 64]
                                if first:
                                    kb.op("vector", lambda e, i=i, dst=dst: e.tensor_scalar(out=dst, in0=accs[i][:, 0:64], scalar1=rz[:, i:i + 1], scalar2=None, op0=ALU.mult),
                                          outs=[ONSA], ins=[ACC[i], RZ])
                                else:
                                    kb.op("vector", lambda e, i=i, dst=dst: e.scalar_tensor_tensor(out=dst, in0=accs[i][:, 0:64], scalar=rz[:, i:i + 1], in1=dst, op0=ALU.mult, op1=ALU.add),
                                          outs=[ONSA], ins=[ACC[i], RZ, ONSA])

                        for qb in range(NB):
                            sl = slice(qb * 512, (qb + 1) * 512)
                            kb.dma("sync", cosb[:], I["cosT"][:, sl], outs=[CSB])
                            kb.dma("sync", sinb[:], I["sinT"][:, sl], outs=[CSB])
                            kb.dma("sync", cmpbb[:], I["cmpb"][:, qb, :], outs=[CMB])
                            for ch in range(2):
                                bk = sbank()
                                for k in range(8):
                                    kb.op("tensor", lambda e, k=k, bk=bk, ch=ch: e.matmul(ps[bk][:], lhsT=wqg[:, k, ch * 128:(ch + 1) * 128], rhs=hT[:, k, sl],
                                                                                         start=(k == 0), stop=(k == 7)), outs=[PS[bk]], ins=[WN, HT[qb]], mark=(k == 7))
                                kb.op("scalar", lambda e, bk=bk, ch=ch: e.copy(out=qTb[:, ch, :], in_=ps[bk][:]), outs=[QB_], ins=[PS[bk]])
                                rope_from(bk, qrTb[:, ch, :], QRB)
                            for i in range(4):
                                tt = 4 * qb + i
                                bk = mbank()
                                for k in range(8):
                                    kb.op("tensor", lambda e, k=k, bk=bk, tt=tt: e.matmul(ps[bk][:, 0:12], lhsT=hT[:, k, tt * 128:(tt + 1) * 128], rhs=wgt[:, k, :],
                                                                                         start=(k == 0), stop=(k == 7)), outs=[PS[bk]], ins=[WN, HT[qb]], mark=(k == 7))
                                kb.op("scalar", lambda e, bk=bk, i=i: e.activation(out=gts[:, i, :], in_=ps[bk][:, 0:12], func=AF.Sigmoid), outs=[GTS], ins=[PS[bk]])
                            if stop_after == "p1c_qa":
                                dump("onsa", onsa[:], [128, 4, 256])
                                return nc, dbg_out
                            ncts = 1 if qb < 4 else 2
                            for h in range(4):
                                ch, p0_ = h // 2, (h % 2) * 64
                                steps = []
                                for ct in range(ncts):
                                    smm = [(kcmpT[p0_:p0_ + 64, ct * 128:(ct + 1) * 128], qTb[p0_:p0_ + 64, ch, :], [KC, QB_])]
                                    if ct == ncts - 1:
                                        smm.append((ident[:], cmpbb[:], [CST, CMB]))
                                    pv = [(i, vcmp1[:, ct, 0:129], 129, ct == 0, ct == ncts - 1) for i in range(4)]
                                    steps.append({"s": smm, "pv": pv})
                                run_branch(steps)
                                finish(h, 0, True)
                            if stop_after == "p1c_qb":
                                dump("onsa", onsa[:], [128, 4, 256])
                                return nc, dbg_out
                            for i in range(4):
                                qt = 4 * qb + i
                                kb.dma("sync", sadd[:], I["seladd"][:, qt, :], outs=[SADD])
                                kb.op("vector", lambda e, i=i: e.tensor_tensor(out=score[:], in0=pslc[:, i, :], in1=sadd[:], op=ALU.add), outs=[SEL], ins=[PSLC, SADD])
                                kb.op("vector", lambda e: e.max(out=m8[:, 0:8], in_=score[:]), outs=[SEL], ins=[SEL])
                                kb.op("vector", lambda e: e.match_replace(out=stmp[:], in_to_replace=m8[:, 0:8], in_values=score[:], imm_value=-3e38), outs=[SEL], ins=[SEL])
                                kb.op("vector", lambda e: e.max(out=m8[:, 8:16], in_=stmp[:]), outs=[SEL], ins=[SEL])
                                kb.op("vector", lambda e: e.tensor_scalar(out=sel[:], in0=score[:], scalar1=m8[:, 15:16], scalar2=None, op0=ALU.is_ge), outs=[SEL], ins=[SEL])
                                kb.op("vector", lambda e: e.scalar_tensor_tensor(out=sel[:], in0=score[:], scalar=-1e29, in1=sel[:], op0=ALU.is_gt, op1=ALU.mult), outs=[SEL], ins=[SEL])
                                kb.op("vector", lambda e: e.tensor_scalar(out=negb[:, 0:64], in0=sel[:], scalar1=-1.0, scalar2=-NEG, op0=ALU.add, op1=ALU.mult), outs=[SEL], ins=[SEL])
                                kb.op("vector", lambda e: e.tensor_scalar(out=negb[:, 64:128], in0=sel[:], scalar1=-1.0, scalar2=-NEG, op0=ALU.add, op1=ALU.mult), outs=[SEL], ins=[SEL])
                                bk = mbank()
                                kb.op("tensor", lambda e, bk=bk: e.matmul(ps[bk][:, 0:128], lhsT=negb[:], rhs=ident[:], start=True, stop=True), outs=[PS[bk]], ins=[SEL, CST])
                                kb.op("scalar", lambda e, bk=bk, i=i: e.copy(out=negbT[:, i * 128:(i + 1) * 128], in_=ps[bk][:, 0:128]), outs=[NBT], ins=[PS[bk]])
                            if stop_after == "p1c_qc":
                                dump("onsa", onsa[:], [128, 4, 256])
                                return nc, dbg_out
                            for h in range(4):
                                ch, p0_ = h // 2, (h % 2) * 64
                                steps = []
                                for kt in range(4 * qb + 4):
                                    smm = [(ksT[p0_:p0_ + 64, kt * 128:(kt + 1) * 128], qrTb[p0_:p0_ + 64, ch, :], [KS, QRB]),
                                           (ekt[p0_:p0_ + 64, kt, :], negbT[p0_:p0_ + 64, :], [QC, NBT])]
                                    if kt >= 4 * qb:
                                        smm.append((ident[:], wband[:, 4 + kt - 4 * qb, :], [CST, QC]))
                                    pv = [(i, vs1[:, kt, 0:65], 65, kt == 0, kt == 4 * qb + i) for i in range(4) if kt <= 4 * qb + i]
                                    steps.append({"s": smm, "pv": pv})
                                run_branch(steps)
                                finish(h, 1, False)
                            if stop_after == "p1c_qd":
                                dump("onsa", onsa[:], [128, 4, 256])
                                return nc, dbg_out
                            for h in range(4):
                                ch, p0_ = h // 2, (h % 2) * 64
                                steps = []
                                jmin = max(0, 4 - 4 * qb)
                                for j in range(jmin, 8):
                                    kt = 4 * qb - 4 + j
                                    smm = [(kwT[p0_:p0_ + 64, kt * 128:(kt + 1) * 128], qrTb[p0_:p0_ + 64, ch, :], [KS, QRB]),
                                           (ident[:], wband[:, j, :], [CST, QC])]
                                    pv = [(i, vw1[:, kt, 0:65], 65, j == max(i, jmin), j == i + 4) for i in range(4) if i <= j <= i + 4]
                                    steps.append({"s": smm, "pv": pv})
                                run_branch(steps)
                                finish(h, 2, False)
                            if stop_after == "p1c_q":
                                dump("onsa", onsa[:], [128, 4, 256])
                                dump("pslc", pslc[:], [128, 4, 64])
                                dump("negbT", negbT[:], [128, 512], BF16)
                                return nc, dbg_out
                            kb.op("gpsimd", lambda e: e.tensor_copy(out=obn[:], in_=onsa[:]), outs=[OBN], ins=[ONSA])
                            for i in range(4):
                                qt = 4 * qb + i
                                isel = isel0 if qt < 16 else isel1
                                s_ = qt % 16
                                for ch in range(2):
                                    jf = 4 + g * 2 + ch
                                    bk = mbank()
                                    kb.op("tensor", lambda e, bk=bk, i=i, ch=ch, isel=isel: e.matmul(ps[bk][:, 0:128], lhsT=obn[:, i, ch * 128:(ch + 1) * 128], rhs=isel[:], start=True, stop=True),
                                          outs=[PS[bk]], ins=[OBN, CST])
                                    dst = oT[:, jf, s_ * 128:(s_ + 1) * 128]
                                    if qt < 16:
                                        kb.op("scalar", lambda e, bk=bk, dst=dst: e.copy(out=dst, in_=ps[bk][:, 0:128]), outs=[OT[jf][s_]], ins=[PS[bk]])
                                    else:
                                        kb.op("vector", lambda e, bk=bk, dst=dst: e.tensor_tensor(out=dst, in0=ps[bk][:, 0:128], in1=dst, op=ALU.add),
                                              outs=[OT[jf][s_]], ins=[PS[bk], OT[jf][s_]])
                        kb.barrier()
                kb.barrier()
            if "oT2" in dbg:
                dump("oT2", oT[:], [128, 8, SO], BF16)
            if stop_after == "p1c":
                kb.barrier()
                return nc, dbg_out
        kb.barrier()
        with ExitStack() as p2:
            acc = sb("acc", [128, 8, SO], F32, p2)
            ACCB = [Buf(acc[:, :, nb * 512:(nb + 1) * 512]) for nb in range(4)]
            wo = sb("wo", [128, 8, D], BF16, p2)
            WO = Buf(wo[:])
            fg = sb("fg", [128, 8], F32, p2)
            FG = Buf(fg[:])
            kb.dma("sync", fg[:], I["fg"], outs=[FG])
            xTo_v = I["xTo"].rearrange("(k p) t -> p k t", p=128)
            for nb in range(4):
                kb.dma("sync", acc[:, :, nb * 512:(nb + 1) * 512], xTo_v[:, :, nb * 512:(nb + 1) * 512], outs=[ACCB[nb]])
            w_out_v = I["w_out"].rearrange("(k p) c -> p k c", p=128)
            for k in range(8):
                kb.dma("gpsimd", wo[:, k, :], w_out_v[:, k, :], outs=[WO])
            rr2 = [0]

            def bank2():
                rr2[0] = (rr2[0] + 1) % 8
                return rr2[0]

            for nb in range(4):
                sl = slice(nb * 512, (nb + 1) * 512)
                for m in range(8):
                    bk = bank2()
                    for k in range(8):
                        kb.op("tensor", lambda e, k=k, m=m, bk=bk, sl=sl: e.matmul(ps[bk][:], lhsT=wo[:, k, m * 128:(m + 1) * 128], rhs=oT[:, k, sl], start=(k == 0), stop=(k == 7)),
                              outs=[PS[bk]], ins=[WO] + [OT[k][s] for s in range(nb * 4, nb * 4 + 4)], mark=(k == 7))
                    kb.op("vector", lambda e, m=m, bk=bk, sl=sl: e.scalar_tensor_tensor(out=acc[:, m, sl], in0=ps[bk][:], scalar=g1c(m), in1=acc[:, m, sl], op0=ALU.mult, op1=ALU.add),
                          outs=[ACCB[nb]], ins=[PS[bk], ACCB[nb], MOD])
            if "x2T" in dbg:
                dump("x2T", acc[:], [128, 8, SO])
            h2T = sb("h2T", [128, 8, SO], BF16, p2)
            H2 = [Buf(h2T[:, :, nb * 512:(nb + 1) * 512]) for nb in range(4)]
            with ExitStack() as p2a:
                sqa = sb("sqa", [128, 8, 512], F32, p2a)
                SQA = Buf(sqa[:])
                rsa = sb("rsa", [128, 512], F32, p2a)
                RSA = Buf(rsa[:])
                tma = [sb(f"tma{i}", [128, 512], F32, p2a) for i in range(2)]
                TMA = [Buf(t[:]) for t in tma]
                for nb in range(4):
                    sl = slice(nb * 512, (nb + 1) * 512)
                    kb.op("scalar", lambda e, sl=sl: e.activation(out=sqa[:], in_=acc[:, :, sl], func=AF.Square), outs=[SQA], ins=[ACCB[nb]])
                    bk = bank2()
                    for k in range(8):
                        kb.op("tensor", lambda e, k=k, bk=bk: e.matmul(ps[bk][:], lhsT=onesf[:], rhs=sqa[:, k, :], start=(k == 0), stop=(k == 7)),
                              outs=[PS[bk]], ins=[SQA, CST], mark=(k == 7))
                    kb.op("scalar", lambda e, bk=bk: e.activation(out=rsa[:], in_=ps[bk][:], func=AF.Sqrt, scale=1.0 / D, bias=EPS), outs=[RSA], ins=[PS[bk]])
                    kb.op("vector", lambda e: e.reciprocal(out=rsa[:], in_=rsa[:]), outs=[RSA], ins=[RSA])
                    for k in range(8):
                        T = TMA[k % 2]
                        t_ = tma[k % 2]
                        kb.op("vector", lambda e, k=k, t_=t_, sl=sl: e.tensor_tensor(out=t_[:], in0=acc[:, k, sl], in1=rsa[:], op=ALU.mult), outs=[T], ins=[ACCB[nb], RSA])
                        kb.op("scalar", lambda e, k=k, t_=t_, sl=sl: e.activation(out=h2T[:, k, sl], in_=t_[:], func=AF.Identity, scale=a2[:, k:k + 1], bias=sh2(k)),
                              outs=[H2[nb]], ins=[T, MOD])
                kb.barrier()
            if "h2T" in dbg:
                dump("h2T", h2T[:], [128, 8, SO], BF16)
            gw = sb("gw", [128, 16, 65], F32, p2)
            GW = Buf(gw[:])
            kb.op("vector", lambda e: e.memset(gw[:], 1.0), outs=[GW])
            with ExitStack() as p2r:
                rwf = sb("rwf", [128, 8, 64], F32, p2r)
                rwb = sb("rwb", [128, 8, 64], BF16, p2r)
                rbias = sb("rbias", [128, 64], F32, p2r)
                RW = Buf(rwf[:])
                kb.dma("sync", rwf[:], I["rw"], outs=[RW])
                kb.dma("sync", rbias[:], I["rbias"], outs=[RW])
                kb.op("vector", lambda e: e.tensor_copy(out=rwb[:], in_=rwf[:]), outs=[RW], ins=[RW])
                scr = sb("scr", [128, 64], F32, p2r)
                chs = sb("chs", [128, 64], F32, p2r)
                eq = sb("eq", [128, 64], F32, p2r)
                chm = sb("chm", [128, 64], F32, p2r)
                m1 = sb("m1", [128, 8], F32, p2r)
                m2 = sb("m2", [128, 8], F32, p2r)
                gsm = sb("gsm", [128, 8], F32, p2r)
                g8 = sb("g8", [128, 8], F32, p2r)
                gmk = sb("gmk", [128, 8], F32, p2r)
                e8 = sb("e8", [128, 8], F32, p2r)
                ssum = sb("ssum", [128, 1], F32, p2r)
                RT = Buf(scr[:])
                v3 = lambda ap: ap.rearrange("p (g j) -> p g j", j=8)
                b3 = lambda ap: ap.rearrange("p (g o) -> p g o", o=1).to_broadcast([128, 8, 8])
                for tt in range(16):
                    bk = bank2()
                    for k in range(8):
                        kb.op("tensor", lambda e, k=k, bk=bk, tt=tt: e.matmul(ps[bk][:, 0:64], lhsT=h2T[:, k, tt * 128:(tt + 1) * 128], rhs=rwb[:, k, :], start=(k == 0), stop=(k == 7)),
                              outs=[PS[bk]], ins=[H2[tt // 4], RW], mark=(k == 7))
                    kb.op("scalar", lambda e, bk=bk: e.activation(out=scr[:], in_=ps[bk][:, 0:64], func=AF.Sigmoid), outs=[RT], ins=[PS[bk]])
                    V = lambda f: kb.op("vector", f, outs=[RT], ins=[RT, RW])
                    V(lambda e: e.tensor_tensor(out=chs[:], in0=scr[:], in1=rbias[:], op=ALU.add))
                    V(lambda e: e.tensor_reduce(out=m1[:], in_=v3(chs[:]), axis=AX.X, op=ALU.max))
                    V(lambda e: e.tensor_tensor(out=v3(eq[:]), in0=v3(chs[:]), in1=b3(m1[:]), op=ALU.is_equal))
                    V(lambda e: e.scalar_tensor_tensor(out=eq[:], in0=eq[:], scalar=-1e30, in1=chs[:], op0=ALU.mult, op1=ALU.add))
                    V(lambda e: e.tensor_reduce(out=m2[:], in_=v3(eq[:]), axis=AX.X, op=ALU.max))
                    V(lambda e: e.tensor_tensor(out=gsm[:], in0=m1[:], in1=m2[:], op=ALU.add))
                    V(lambda e: e.max(out=g8[:], in_=gsm[:]))
                    V(lambda e: e.tensor_scalar(out=gmk[:], in0=gsm[:], scalar1=g8[:, 3:4], scalar2=None, op0=ALU.is_ge))
                    V(lambda e: e.scalar_tensor_tensor(out=v3(chm[:]), in0=v3(chs[:]), scalar=10.0, in1=b3(gmk[:]), op0=ALU.add, op1=ALU.mult))
                    V(lambda e: e.max(out=e8[:], in_=chm[:]))
                    V(lambda e: e.tensor_scalar(out=eq[:], in0=chm[:], scalar1=e8[:, 7:8], scalar2=None, op0=ALU.is_ge))
                    V(lambda e: e.tensor_tensor(out=eq[:], in0=eq[:], in1=scr[:], op=ALU.mult))
                    V(lambda e: e.tensor_reduce(out=ssum[:], in_=eq[:], axis=AX.X, op=ALU.add))
                    V(lambda e: e.reciprocal(out=ssum[:], in_=ssum[:]))
                    kb.op("vector", lambda e, tt=tt: e.tensor_scalar(out=gw[:, tt, 0:64], in0=eq[:], scalar1=ssum[:, 0:1], scalar2=2.5, op0=ALU.mult, op1=ALU.mult),
                          outs=[GW], ins=[RT])
                kb.barrier()
            if "gw" in dbg:
                dump("gw", gw[:], [128, 16, 65])
            with ExitStack() as p2e:
                wg = [sb(f"wg{i}", [128, 8, 512], BF16, p2e) for i in range(2)]
                wd = [sb(f"wd{i}", [128, 2, D], BF16, p2e) for i in range(2)]
                WG = [Buf(t[:]) for t in wg]
                WD = [Buf(t[:]) for t in wd]
                actT = [sb(f"actT{i}", [128, 2, SO], BF16, p2e) for i in range(2)]
                ACT_ = [[Buf(actT[i][:, :, nb * 512:(nb + 1) * 512]) for nb in range(4)] for i in range(2)]
                sgt = [sb(f"sgt{i}", [128, 256], F32, p2e) for i in range(2)]
                SGT = [Buf(t[:]) for t in sgt]
                att = [sb(f"att{i}", [128, 256], BF16, p2e) for i in range(2)]
                ATT = [Buf(t[:]) for t in att]
                NE = n_experts

                def load_w(e_):
                    sl_ = e_ % 2
                    gv = I["wgu"][e_].rearrange("(k p) c -> p k c", p=128)
                    kb.dma("gpsimd", wg[sl_][:, 0:4, :], gv[:, 0:4, :], outs=[WG[sl_]])
                    kb.dma("gpsimd", wg[sl_][:, 4:8, :], gv[:, 4:8, :], outs=[WG[sl_]])
                    kb.dma("gpsimd", wd[sl_][:], I["wdn"][e_].rearrange("(k p) c -> p k c", p=128), outs=[WD[sl_]])

                load_w(0)
                cnt = [0]
                for e_ in range(NE):
                    if e_ + 1 < NE:
                        load_w(e_ + 1)
                    sl_ = e_ % 2
                    for tt in range(16):
                        bk = bank2()
                        for k in range(8):
                            kb.op("tensor", lambda e, k=k, bk=bk, tt=tt: e.matmul(ps[bk][:], lhsT=h2T[:, k, tt * 128:(tt + 1) * 128], rhs=wg[sl_][:, k, :], start=(k == 0), stop=(k == 7)),
                                  outs=[PS[bk]], ins=[H2[tt // 4], WG[sl_]], mark=(k == 7))
                        cnt[0] += 1
                        i2 = cnt[0] % 2
                        kb.op("scalar", lambda e, bk=bk, i2=i2: e.activation(out=sgt[i2][:], in_=ps[bk][:, 0:256], func=AF.Silu), outs=[SGT[i2]], ins=[PS[bk]])
                        kb.op("vector", lambda e, bk=bk, i2=i2, tt=tt: e.scalar_tensor_tensor(out=att[i2][:], in0=ps[bk][:, 256:512], scalar=gw[:, tt, e_:e_ + 1], in1=sgt[i2][:],
                                                                                      op0=ALU.mult, op1=ALU.mult), outs=[ATT[i2]], ins=[PS[bk], SGT[i2], GW])
                        for hc in range(2):
                            bt = bank2()
                            kb.op("tensor", lambda e, bt=bt, hc=hc, i2=i2: e.matmul(ps[bt][:, 0:128], lhsT=att[i2][:, hc * 128:(hc + 1) * 128], rhs=ident[:], start=True, stop=True),
                                  outs=[PS[bt]], ins=[ATT[i2], CST])
                            kb.op("scalar", lambda e, bt=bt, hc=hc, tt=tt: e.copy(out=actT[sl_][:, hc, tt * 128:(tt + 1) * 128], in_=ps[bt][:, 0:128]),
                                  outs=[ACT_[sl_][tt // 4]], ins=[PS[bt]])
                    for nb in range(4):
                        sl = slice(nb * 512, (nb + 1) * 512)
                        for m in range(8):
                            bk = bank2()
                            for hc in range(2):
                                kb.op("tensor", lambda e, bk=bk, hc=hc, m=m, sl=sl: e.matmul(ps[bk][:], lhsT=wd[sl_][:, hc, m * 128:(m + 1) * 128], rhs=actT[sl_][:, hc, sl], start=(hc == 0), stop=(hc == 1)),
                                      outs=[PS[bk]], ins=[WD[sl_], ACT_[sl_][nb]], mark=(hc == 1))
                            kb.op("vector", lambda e, bk=bk, m=m, sl=sl: e.scalar_tensor_tensor(out=acc[:, m, sl], in0=ps[bk][:], scalar=g2c(m), in1=acc[:, m, sl], op0=ALU.mult, op1=ALU.add),
                                  outs=[ACCB[nb]], ins=[PS[bk], ACCB[nb], MOD])
                kb.barrier()
            if "x3T" in dbg:
                dump("x3T", acc[:], [128, 8, SO])
            sq2 = sb("sq2", [128, 8, 512], F32, p2)
            SQ2 = Buf(sq2[:])
            rstd2 = sb("rstd2", [128, 512], F32, p2)
            RS2 = Buf(rstd2[:])
            ot = [sb(f"ot{i}", [128, 8, 512], F32, p2) for i in range(2)]
            OTB = [Buf(t[:]) for t in ot]
            outT_v = outT.rearrange("(k p) t -> p k t", p=128)
            for nb in range(4):
                sl = slice(nb * 512, (nb + 1) * 512)
                kb.op("scalar", lambda e, sl=sl: e.activation(out=sq2[:], in_=acc[:, :, sl], func=AF.Square), outs=[SQ2], ins=[ACCB[nb]])
                bk = bank2()
                for k in range(8):
                    kb.op("tensor", lambda e, k=k, bk=bk: e.matmul(ps[bk][:], lhsT=onesf[:], rhs=sq2[:, k, :], start=(k == 0), stop=(k == 7)),
                          outs=[PS[bk]], ins=[SQ2, CST], mark=(k == 7))
                kb.op("scalar", lambda e, bk=bk: e.activation(out=rstd2[:], in_=ps[bk][:], func=AF.Sqrt, scale=1.0 / D, bias=EPS), outs=[RS2], ins=[PS[bk]])
                kb.op("vector", lambda e: e.reciprocal(out=rstd2[:], in_=rstd2[:]), outs=[RS2], ins=[RS2])
                o_ = ot[nb % 2]
                for k in range(8):
                    kb.op("vector", lambda e, k=k, o_=o_, sl=sl: e.scalar_tensor_tensor(out=o_[:, k, :], in0=acc[:, k, sl], scalar=fg[:, k:k + 1], in1=rstd2[:], op0=ALU.mult, op1=ALU.mult),
                          outs=[OTB[nb % 2]], ins=[ACCB[nb], FG, RS2])
                kb.dma("sync", outT_v[:, :, sl], o_[:], ins=[OTB[nb % 2]])
            kb.barrier()
        kb.barrier()
    return nc, dbg_out


def _prep_inputs(inp, core):
    b = core // 2
    half = core % 2
    f = lambda a: np.ascontiguousarray(a, dtype=np.float32)
    x = inp["x"][b]
    m = {}
    xT = f(x.T)
    m["xT"] = xT
    m["xTo"] = f(xT[:, half * SO:(half + 1) * SO])
    m["cT"] = f(inp["c"][b].reshape(8, 128).T)
    m["w_ada"] = f(inp["w_ada"][0])
    m["b_ada"] = f(inp["b_ada"][0].reshape(1, -1))
    m["n1g"] = f(inp["norm1_g"][0].reshape(8, 128).T)
    m["w_in"] = f(inp["w_in"][0])
    m["lbl"] = f(inp["hg_lb_logits"].reshape(2, 4, 128).transpose(2, 0, 1))
    m["hng"] = f(np.broadcast_to(inp["hg_norm_g"][0][None, :], (128, 512)))
    for s in ("k", "v"):
        m["peT" + s] = f(inp["cmp_pos_" + s][0].T)
        m["w1" + s] = f(inp["cmp_w1_" + s][0].reshape(32, 64, 256).transpose(1, 0, 2))
        m["b1" + s] = f(inp["cmp_b1_" + s][0].reshape(2, 128).T)
        m["w2" + s] = f(inp["cmp_w2_" + s][0].reshape(2, 128, 64).transpose(1, 0, 2))
    m["w_out"] = f(inp["w_out"][0])
    m["n2g"] = f(inp["norm2_g"][0].reshape(8, 128).T)
    m["rw"] = f(inp["router_w"][0].reshape(8, 128, 64).transpose(1, 0, 2))
    m["rbias"] = f(np.broadcast_to(inp["router_bias"][0][None, :], (128, 64)))
    m["fg"] = f(inp["final_g"].reshape(8, 128).T)
    return m


_SHARED = {}


def kernel(**inp):
    inp = {k: np.asarray(v) for k, v in inp.items()}
    nc, _ = build()
    wgu = np.ascontiguousarray(np.concatenate([inp["w_exp_gu"][0], inp["w_sh_gu"][0][None]], axis=0), dtype=np.float32)
    wdn = np.ascontiguousarray(np.concatenate([inp["w_exp_dn"][0], inp["w_sh_dn"][0][None]], axis=0), dtype=np.float32)
    in_maps = []
    for core in range(8):
        m = _prep_inputs(inp, core)
        m["wgu"] = wgu
        m["wdn"] = wdn
        m.update(_consts(core % 2))
        in_maps.append(m)
    res = run_bass_kernel_spmd(nc, in_maps, core_ids=list(range(8)))
    out = np.zeros((4, S, D), np.float32)
    for core in range(8):
        b, half = core // 2, core % 2
        out[b, half * SO:(half + 1) * SO, :] = res.results[core]["outT"].T
    return out
```

```python
import numpy as np
import os as _os0
import ml_dtypes
from contextlib import ExitStack
import concourse.bass as bass
import concourse.mybir as mybir
from concourse.bass_utils import run_bass_kernel_spmd

F32 = mybir.dt.float32
BF16 = mybir.dt.bfloat16
AF = mybir.ActivationFunctionType
ALU = mybir.AluOpType
AX = mybir.AxisListType

S = 4096
D = 1024
NT = 32
NB = 8
SO = 2048
EPS = 1e-6
NEG = -30000.0
NDS = 12
SEM_LIMIT = 2000
SAME_SYNC = True


class Buf:
    __slots__ = ("ap", "w", "r", "excl")

    def __init__(self, ap, excl=False):
        self.ap = ap
        self.w = None
        self.r = {}
        self.excl = excl

    def __getitem__(self, k):
        return self.ap[k]


class Eng:
    def __init__(self, name, h):
        self.name = name
        self.h = h
        self.sem = None
        self.count = 0
        self.epoch = 0
        self.waited = {}


class KB:
    def __init__(self, nc, es):
        self.nc = nc
        self.es = es
        self.engs = {n: Eng(n, getattr(nc, n)) for n in ("tensor", "vector", "scalar", "gpsimd", "sync")}
        for e in self.engs.values():
            self._new_sem(e)
        self.dsems = {q: [es.enter_context(nc.semaphore(f"d_{q}{i}")) for i in range(NDS)] for q in ("sync", "gpsimd")}
        self.dcnt = {q: [0] * NDS for q in ("sync", "gpsimd")}
        self.drr = {"sync": 0, "gpsimd": 0}
        self.nsem = 0

    def _new_sem(self, e):
        e.epoch += 1
        e.sem = self.es.enter_context(self.nc.semaphore(f"s_{e.name}_{e.epoch}"))
        e.count = 0

    def wait(self, eng, tk):
        key, sem, val = tk
        if eng.waited.get(key, 0) >= val:
            return
        eng.h.wait_ge(sem, val)
        eng.waited[key] = val

    def _deps(self, en, eng, outs, ins):
        need = {}

        def add(t):
            if t[3] == en and (en == "tensor" or not SAME_SYNC):
                return
            cur = need.get(t[0])
            if cur is None or cur[2] < t[2]:
                need[t[0]] = t

        for b in ins:
            if b.w is not None:
                add(b.w)
            if b.excl:
                for t in b.r.values():
                    if t[3] != en:
                        add(t)
        for b in outs:
            if b.w is not None:
                add(b.w)
            for t in b.r.values():
                add(t)
        for t in need.values():
            self.wait(eng, t[:3])

    def op(self, en, fn, outs=(), ins=(), mark=True):
        eng = self.engs[en]
        self._deps(en, eng, outs, ins)
        if eng.count >= SEM_LIMIT:
            self._new_sem(eng)
        inst = fn(eng.h)
        if mark:
            eng.count += 1
            inst.then_inc(eng.sem, 1)
            tk = ((en, eng.epoch), eng.sem, eng.count, en)
        else:
            tk = ((en, eng.epoch), eng.sem, eng.count + 1, en)
        for b in ins:
            b.r[tk[0]] = tk
        for b in outs:
            b.w = tk
            b.r = {}
        return tk

    def dma(self, q, out_ap, in_ap, outs=(), ins=()):
        eng = self.engs[q]
        i = self.drr[q]
        self.drr[q] = (i + 1) % NDS
        sem = self.dsems[q][i]
        key = ("d", q, i)
        if self.dcnt[q][i] > 0:
            self.wait(eng, (key, sem, self.dcnt[q][i]))
        self._deps("dma_" + q, eng, outs, ins)
        inst = eng.h.dma_start(out=out_ap, in_=in_ap)
        self.dcnt[q][i] += 16
        inst.then_inc(sem, 16)
        tk = (key, sem, self.dcnt[q][i], "dma_" + q)
        for b in ins:
            b.r[key] = tk
        for b in outs:
            b.w = tk
            b.r = {}
        return tk

    def barrier(self):
        for e in self.engs.values():
            for o in self.engs.values():
                if o is e or o.count == 0:
                    continue
                self.wait(e, ((o.name, o.epoch), o.sem, o.count))
            for q in ("sync", "gpsimd"):
                for i in range(NDS):
                    if self.dcnt[q][i] > 0:
                        self.wait(e, (("d", q, i), self.dsems[q][i], self.dcnt[q][i]))


def _consts(half):
    bf = ml_dtypes.bfloat16
    c = {}
    eye = np.eye(128, dtype=np.float32)
    c["ident"] = eye.astype(bf)
    c["onesf"] = np.ones((128, 128), np.float32)
    c["isel0"] = (eye * (1.0 if half == 0 else 0.0)).astype(bf)
    c["isel1"] = (eye * (1.0 if half == 1 else 0.0)).astype(bf)
    m = np.arange(128)
    sw = (m // 64) * 64 + ((m % 64) + 32) % 64
    ps = np.zeros((128, 128), np.float32)
    ps[sw, m] = 1.0
    c["pswap"] = ps.astype(bf)
    dd = np.arange(128) % 64
    i = dd % 32
    inv = 10000.0 ** (-(i.astype(np.float64)) / 32.0)
    ang = inv[:, None].astype(np.float32).astype(np.float64) * np.arange(S)[None, :]
    ang = ang.astype(np.float32).astype(np.float64)
    c["cosT"] = np.cos(ang).astype(np.float32)
    sg = np.where(dd < 32, -1.0, 1.0)[:, None]
    c["sinT"] = (np.sin(ang) * sg).astype(np.float32)
    c["hmask"] = (m[:, None] <= m[None, :]).astype(np.float32).astype(bf)
    seg = np.ones((128, 512), np.float32)
    seg[:, ::128] = 0.0
    c["segm"] = seg
    r = np.arange(128)[:, None]
    qi = np.arange(512)[None, :]
    wb = np.zeros((8, 128, 512), np.float32)
    cb = np.zeros((4, 128, 512), np.float32)
    for j in range(8):
        kpos = -512 + 128 * j + r
        dlt = qi - kpos
        wb[j] = np.where((dlt >= 0) & (dlt < 512), 0.0, NEG)
    for j in range(4):
        kpos = 128 * j + r
        cb[j] = np.where(kpos <= qi, 0.0, NEG)
    c["wband"] = np.ascontiguousarray(wb.transpose(1, 0, 2)).astype(bf)
    c["causb"] = np.ascontiguousarray(cb.transpose(1, 0, 2)).astype(bf)
    cm = np.zeros((8, 128, 512), np.float32)
    for qb in range(8):
        ct = 0 if qb < 4 else 1
        cc = 128 * ct + r
        qpos = 512 * qb + qi
        cm[qb] = np.where((16 * cc + 31 <= qpos) & (cc < 255), 0.0, NEG)
    c["cmpb"] = np.ascontiguousarray(cm.transpose(1, 0, 2)).astype(bf)
    ek = np.zeros((64, 32, 128), np.float32)
    for kt in range(32):
        ek[2 * kt, kt, :64] = 1.0
        ek[2 * kt + 1, kt, 64:] = 1.0
    c["ekt"] = np.concatenate([ek, ek], axis=0).astype(bf)
    add = np.zeros((128, 32, 64), np.float32)
    for qt in range(32):
        pos = 128 * qt + np.arange(128)
        cur = pos // 64
        j = np.arange(64)[None, :]
        forced = (j == 0) | (j == cur[:, None]) | (j == cur[:, None] - 1)
        avail = j <= cur[:, None]
        add[:, qt, :] = np.where(forced, 1e30, np.where(avail, 0.0, -1e30))
    c["seladd"] = add
    cs = np.arange(256)[:, None] * 16
    ss = np.arange(64)[None, :] * 64
    ov = np.clip(np.minimum(cs + 32, ss + 64) - np.maximum(cs, ss), 0, None).astype(np.float32) / 32.0
    ov[255] = 0.0
    c["ovl"] = np.ascontiguousarray(ov.reshape(2, 128, 64).transpose(1, 0, 2)).astype(bf)
    return c


CONST_SHAPES = {
    "ident": ([128, 128], BF16), "onesf": ([128, 128], F32), "isel0": ([128, 128], BF16), "isel1": ([128, 128], BF16),
    "pswap": ([128, 128], BF16), "cosT": ([128, S], F32), "sinT": ([128, S], F32), "hmask": ([128, 128], BF16),
    "segm": ([128, 512], F32), "wband": ([128, 8, 512], BF16), "causb": ([128, 4, 512], BF16),
    "cmpb": ([128, 8, 512], BF16), "ekt": ([128, 32, 128], BF16), "seladd": ([128, 32, 64], F32),
    "ovl": ([128, 2, 64], BF16),
}

IN_SHAPES = {
    "xT": [D, S], "xTo": [D, SO], "cT": [128, 8], "w_ada": [D, 6 * D], "b_ada": [1, 6 * D], "n1g": [128, 8],
    "w_in": [D, 3352], "lbl": [128, 2, 4], "hng": [128, 512],
    "peTk": [64, 32], "w1k": [64, 32, 256], "b1k": [128, 2], "w2k": [128, 2, 64],
    "peTv": [64, 32], "w1v": [64, 32, 256], "b1v": [128, 2], "w2v": [128, 2, 64],
    "w_out": [D, D], "n2g": [128, 8], "rw": [128, 8, 64], "rbias": [128, 64],
    "wgu": [65, D, 512], "wdn": [65, 256, D], "fg": [128, 8],
}


class _SkipNSA(Exception):
    pass


class _NSAScope(ExitStack):
    def __exit__(self, et, ev, tb):
        super().__exit__(None, None, None)
        return et is _SkipNSA


def build(stop_after=None, dbg=(), with_moe=True, enable_nsa=True, n_experts=65):
    nc = bass.Bass("TRN2", target_bir_lowering=False)
    I = {}
    for k, shp in IN_SHAPES.items():
        if not with_moe and k in ("wgu", "wdn"):
            continue
        I[k] = nc.dram_tensor(k, list(shp), F32, kind="ExternalInput").ap()
    for k, (shp, dt) in CONST_SHAPES.items():
        I[k] = nc.dram_tensor(k, list(shp), dt, kind="ExternalInput").ap()
    outT = nc.dram_tensor("outT", [D, SO], F32, kind="ExternalOutput").ap()
    dbg_out = {}
    with ExitStack() as es:
        kb = KB(nc, es)
        E = es.enter_context

        uid = [0]

        def sb(name, shape, dt=F32, stack=None):
            uid[0] += 1
            return (stack or es).enter_context(nc.sbuf_tensor(f"sb{uid[0]}_" + name, list(shape), dt))

        ps = [E(nc.psum_tensor(f"ps{i}", [128, 512], F32)) for i in range(8)]
        PS = [Buf(p[:], excl=True) for p in ps]

        def dump(name, ap, shape, dt=F32):
            t = nc.dram_tensor("dbg_" + name, list(shape), dt, kind="ExternalOutput").ap()
            dbg_out[name] = t
            kb.barrier()
            kb.dma("sync", t, ap)
            kb.barrier()

        ident = sb("ident", [128, 128], BF16)
        onesf = sb("onesf", [128, 128], F32)
        isel0 = sb("isel0", [128, 128], BF16)
        isel1 = sb("isel1", [128, 128], BF16)
        pswap = sb("pswap", [128, 128], BF16)
        hmask = sb("hmask", [128, 128], BF16)
        segm = sb("segm", [128, 512], F32)
        CST = Buf(ident[:])
        for nm, t in (("ident", ident), ("onesf", onesf), ("isel0", isel0), ("isel1", isel1), ("pswap", pswap),
                      ("hmask", hmask), ("segm", segm)):
            kb.dma("sync", t[:], I[nm], outs=[CST])
        modcol = sb("modcol", [128, 48], F32)
        a1 = sb("a1", [128, 8], F32)
        a2 = sb("a2", [128, 8], F32)
        MOD = Buf(modcol[:])
        oT = sb("oT", [128, 8, SO], BF16)
        OT = [[Buf(oT[:, j, s * 128:(s + 1) * 128]) for s in range(16)] for j in range(8)]

        with ExitStack() as p0:
            cT = sb("cT", [128, 8], F32, p0)
            cs = sb("cs", [128, 8], F32, p0)
            bada = sb("bada", [1, 6 * D], F32, p0)
            modrow = sb("modrow", [1, 6 * D], F32, p0)
            one1 = sb("one1", [1, 1], F32, p0)
            n1g = sb("n1g", [128, 8], F32, p0)
            n2g = sb("n2g", [128, 8], F32, p0)
            wab = [sb(f"wab{i}", [128, 8, 512], F32, p0) for i in range(2)]
            WAB = [Buf(w[:]) for w in wab]
            SM = Buf(cT[:])
            MR = Buf(modrow[:])
            kb.dma("sync", cT[:], I["cT"], outs=[SM])
            kb.dma("sync", bada[:], I["b_ada"], outs=[SM])
            kb.dma("sync", n1g[:], I["n1g"], outs=[SM])
            kb.dma("sync", n2g[:], I["n2g"], outs=[SM])
            kb.op("vector", lambda e: e.memset(one1[:], 1.0), outs=[SM])
            kb.op("scalar", lambda e: e.activation(out=cs[:], in_=cT[:], func=AF.Silu), outs=[SM], ins=[SM])
            wada_v = I["w_ada"].rearrange("(k p) c -> p k c", p=128)
            for cb in range(12):
                W = WAB[cb % 2]
                kb.dma("sync" if cb % 2 == 0 else "gpsimd", wab[cb % 2][:], wada_v[:, :, cb * 512:(cb + 1) * 512], outs=[W])
                P = PS[cb % 2]
                for k in range(8):
                    kb.op("tensor", lambda e, k=k, cb=cb: e.matmul(ps[cb % 2][0:1, :], lhsT=cs[:, k:k + 1], rhs=wab[cb % 2][:, k, :],
                                                                 start=(k == 0), stop=(k == 7)),
                          outs=[P], ins=[SM, W], mark=(k == 7))
                kb.op("vector", lambda e, cb=cb: e.tensor_tensor(out=modrow[0:1, cb * 512:(cb + 1) * 512], in0=ps[cb % 2][0:1, :],
                                                                  in1=bada[0:1, cb * 512:(cb + 1) * 512], op=ALU.add),
                      outs=[MR], ins=[P, SM])
            P = PS[2]
            for j in range(48):
                kb.op("tensor", lambda e, j=j: e.matmul(ps[2][:, j:j + 1], lhsT=modrow[0:1, j * 128:(j + 1) * 128], rhs=one1[0:1, 0:1],
                                                       start=True, stop=True), outs=[P], ins=[MR, SM], mark=(j == 47))
            kb.op("vector", lambda e: e.tensor_copy(out=modcol[:], in_=ps[2][:, 0:48]), outs=[MOD], ins=[P])
            kb.op("vector", lambda e: e.scalar_tensor_tensor(out=a1[:], in0=modcol[:, 8:16], scalar=1.0, in1=n1g[:], op0=ALU.add, op1=ALU.mult),
                  outs=[MOD], ins=[MOD, SM])
            kb.op("vector", lambda e: e.scalar_tensor_tensor(out=a2[:], in0=modcol[:, 32:40], scalar=1.0, in1=n2g[:], op0=ALU.add, op1=ALU.mult),
                  outs=[MOD], ins=[MOD, SM])
            if "mod" in dbg:
                dump("mod", modcol[:], [128, 48])
            kb.barrier()
        sh1 = lambda k: modcol[:, k:k + 1]
        g1c = lambda k: modcol[:, 16 + k:17 + k]
        sh2 = lambda k: modcol[:, 24 + k:25 + k]
        g2c = lambda k: modcol[:, 40 + k:41 + k]

        if stop_after == "p0":
            kb.barrier()
            return nc, dbg_out

        with ExitStack() as p1:
            hT = sb("hT", [128, 8, S], BF16, p1)
            HT = [Buf(hT[:, :, n * 512:(n + 1) * 512]) for n in range(NB)]
            with ExitStack() as p1a:
                xb = [sb(f"xb{i}", [128, 8, 512], F32, p1a) for i in range(2)]
                XB = [Buf(t[:]) for t in xb]
                sq = sb("sq", [128, 8, 512], F32, p1a)
                SQ = Buf(sq[:])
                rstd = sb("rstd", [128, 512], F32, p1a)
                RS = Buf(rstd[:])
                tmp = [sb(f"tmp{i}", [128, 512], F32, p1a) for i in range(2)]
                TMP = [Buf(t[:]) for t in tmp]
                xT_v = I["xT"].rearrange("(k p) t -> p k t", p=128)
                for n in range(NB):
                    X = XB[n % 2]
                    x_ = xb[n % 2]
                    kb.dma("sync" if n % 2 == 0 else "gpsimd", x_[:], xT_v[:, :, n * 512:(n + 1) * 512], outs=[X])
                    kb.op("scalar", lambda e, x_=x_: e.activation(out=sq[:], in_=x_[:], func=AF.Square), outs=[SQ], ins=[X])
                    P = PS[n % 2]
                    for k in range(8):
                        kb.op("tensor", lambda e, k=k, n=n: e.matmul(ps[n % 2][:], lhsT=onesf[:], rhs=sq[:, k, :], start=(k == 0), stop=(k == 7)),
                              outs=[P], ins=[SQ, CST], mark=(k == 7))
                    kb.op("scalar", lambda e, n=n: e.activation(out=rstd[:], in_=ps[n % 2][:], func=AF.Sqrt, scale=1.0 / D, bias=EPS),
                          outs=[RS], ins=[P])
                    kb.op("vector", lambda e: e.reciprocal(out=rstd[:], in_=rstd[:]), outs=[RS], ins=[RS])
                    for k in range(8):
                        T = TMP[k % 2]
                        t_ = tmp[k % 2]
                        kb.op("vector", lambda e, k=k, t_=t_, x_=x_: e.tensor_tensor(out=t_[:], in0=x_[:, k, :], in1=rstd[:], op=ALU.mult),
                              outs=[T], ins=[X, RS])
                        kb.op("scalar", lambda e, k=k, t_=t_, n=n: e.activation(out=hT[:, k, n * 512:(n + 1) * 512], in_=t_[:], func=AF.Identity,
                                                                            scale=a1[:, k:k + 1], bias=sh1(k)),
                              outs=[HT[n]], ins=[T, MOD])
                kb.barrier()
            if "hT" in dbg:
                dump("hT", hT[:], [128, 8, S], BF16)
            if stop_after == "p1a":
                kb.barrier()
                return nc, dbg_out

            rr = [0]

            def bank():
                rr[0] = (rr[0] + 1) % 8
                return rr[0]

            w_in_v = I["w_in"].rearrange("(k p) c -> p k c", p=128)

            with ExitStack() as ph:
                lbl = sb("lbl", [128, 2, 4], F32, ph)
                lb = sb("lb", [128, 4], F32, ph)
                oml = sb("oml", [128, 4], F32, ph)
                hng = sb("hng", [128, 512], F32, ph)
                HC = Buf(lbl[:])
                kb.dma("sync", lbl[:], I["lbl"], outs=[HC])
                kb.dma("sync", hng[:], I["hng"], outs=[HC])
                kb.op("vector", lambda e: e.tensor_tensor(out=lb[:], in0=lbl[:, 0, :], in1=lbl[:, 1, :], op=ALU.subtract), outs=[HC], ins=[HC])
                kb.op("scalar", lambda e: e.activation(out=lb[:], in_=lb[:], func=AF.Sigmoid), outs=[HC], ins=[HC])
                kb.op("vector", lambda e: e.tensor_scalar(out=oml[:], in0=lb[:], scalar1=-1.0, scalar2=1.0, op0=ALU.mult, op1=ALU.add), outs=[HC], ins=[HC])
                wq = sb("wq", [128, 8, 128], BF16, ph)
                wf = sb("wf", [128, 8, 128], BF16, ph)
                wig = sb("wig", [128, 8, 256], BF16, ph)
                WQ, WF, WIG = Buf(wq[:]), Buf(wf[:]), Buf(wig[:])
                Q1 = sb("Q1", [128, S], BF16, ph)
                Q2 = sb("Q2", [128, S], BF16, ph)
                Kt = sb("Kt", [128, S], BF16, ph)
                Kh = sb("Kh", [128, NT, 128], BF16, ph)
                Vh = sb("Vh", [128, NT, 128], BF16, ph)
                SGt = sb("SGt", [128, NT, 128], BF16, ph)
                ebl = sb("ebl", [128, NT], F32, ph)
                BQ = [Buf(Q1[:, n * 512:(n + 1) * 512]) for n in range(NB)]
                BKH = [Buf(Kh[:, t, :]) for t in range(NT)]
                BV = [Buf(Vh[:, t, :]) for t in range(NT)]
                tn = ["f", "lf", "b", "d1", "d2", "eb", "e1", "en1", "el", "k"]
                T_ = {n_: sb("t_" + n_, [128, 512], F32, ph) for n_ in tn}
                TB = {n_: Buf(T_[n_][:]) for n_ in tn}
                khtb = sb("khtb", [128, 512], BF16, ph)
                KHTB = Buf(khtb[:])
                Sst = sb("Sst", [128, 128], F32, ph)
                SST = Buf(Sst[:])
                sbf = [sb(f"sbf{i}", [128, 128], BF16, ph) for i in range(2)]
                SBF = [Buf(t[:]) for t in sbf]
                atm = [sb(f"atm{i}", [128, 128], BF16, ph) for i in range(2)]
                ATM = [Buf(t[:]) for t in atm]
                for i in range(2):
                    kb.op("vector", lambda e, i=i: e.memset(atm[i][:], 0.0), outs=[ATM[i]])
                junk = sb("junk", [128, 128], F32, ph)
                JK = Buf(junk[:])
                ssq = [sb(f"ssq{i}", [128, 1], F32, ph) for i in range(2)]
                SSQ = [Buf(t[:]) for t in ssq]
                of = [sb(f"of{i}", [128, 128], F32, ph) for i in range(2)]
                OF = [Buf(t[:]) for t in of]
                obf = [sb(f"obf{i}", [128, 128], BF16, ph) for i in range(2)]
                OBF = [Buf(t[:]) for t in obf]
                v4 = lambda ap: ap.rearrange("p (c t) -> p c t", t=128)
                for hd in range(int(_os0.environ.get("NHEADS", "4"))):
                    c0 = hd * 128
                    kb.dma("gpsimd", wq[:], w_in_v[:, :, c0:c0 + 128], outs=[WQ])
                    kb.dma("gpsimd", wf[:], w_in_v[:, :, 512 + c0:512 + c0 + 128], outs=[WF])
                    kb.dma("gpsimd", wig[:, :, 0:128], w_in_v[:, :, 1024 + c0:1024 + c0 + 128], outs=[WIG])
                    kb.dma("gpsimd", wig[:, :, 128:256], w_in_v[:, :, 1536 + c0:1536 + c0 + 128], outs=[WIG])
                    for n in range(NB):
                        sl = slice(n * 512, (n + 1) * 512)
                        bq_, bf_ = bank(), bank()
                        for k in range(8):
                            kb.op("tensor", lambda e, k=k, bq_=bq_, sl=sl: e.matmul(ps[bq_][:], lhsT=wq[:, k, :], rhs=hT[:, k, sl], start=(k == 0), stop=(k == 7)),
                                  outs=[PS[bq_]], ins=[WQ, HT[n]], mark=(k == 7))
                        for k in range(8):
                            kb.op("tensor", lambda e, k=k, bf_=bf_, sl=sl: e.matmul(ps[bf_][:], lhsT=wf[:, k, :], rhs=hT[:, k, sl], start=(k == 0), stop=(k == 7)),
                                  outs=[PS[bf_]], ins=[WF, HT[n]], mark=(k == 7))
                        t = T_
                        kb.op("scalar", lambda e, bf_=bf_: e.activation(out=t["f"][:], in_=ps[bf_][:], func=AF.Sigmoid), outs=[TB["f"]], ins=[PS[bf_]])
                        kb.op("vector", lambda e, hd=hd: e.tensor_scalar(out=t["f"][:], in0=t["f"][:], scalar1=oml[:, hd:hd + 1], scalar2=lb[:, hd:hd + 1],
                                                                     op0=ALU.mult, op1=ALU.add), outs=[TB["f"]], ins=[TB["f"], HC])
                        kb.op("scalar", lambda e: e.activation(out=t["lf"][:], in_=t["f"][:], func=AF.Ln), outs=[TB["lf"]], ins=[TB["f"]])
                        kb.op("gpsimd", lambda e: e.tensor_scalar(out=t["k"][:], in0=t["f"][:], scalar1=-1.0, scalar2=1.0, op0=ALU.mult, op1=ALU.add),
                              outs=[TB["k"]], ins=[TB["f"]])
                        kb.op("vector", lambda e: e.tensor_tensor_scan(out=t["b"][:], data0=segm[:], data1=t["lf"][:], initial=0.0, op0=ALU.mult, op1=ALU.add),
                              outs=[TB["b"]], ins=[TB["lf"], CST])
                        kb.op("vector", lambda e: e.tensor_tensor(out=v4(t["d1"][:]), in0=v4(t["b"][:]), in1=v4(t["b"][:])[:, :, 63:64].to_broadcast([128, 4, 128]),
                                                                  op=ALU.subtract), outs=[TB["d1"]], ins=[TB["b"]])
                        kb.op("vector", lambda e: e.tensor_tensor(out=v4(t["d2"][:]), in0=v4(t["b"][:])[:, :, 127:128].to_broadcast([128, 4, 128]), in1=v4(t["b"][:]),
                                                                  op=ALU.subtract), outs=[TB["d2"]], ins=[TB["b"]])
                        kb.op("scalar", lambda e: e.activation(out=t["eb"][:], in_=t["b"][:], func=AF.Exp), outs=[TB["eb"]], ins=[TB["b"]])
                        kb.op("scalar", lambda e: e.activation(out=t["e1"][:], in_=t["d1"][:], func=AF.Exp), outs=[TB["e1"]], ins=[TB["d1"]])
                        kb.op("scalar", lambda e: e.activation(out=t["en1"][:], in_=t["d1"][:], func=AF.Exp, scale=-1.0), outs=[TB["en1"]], ins=[TB["d1"]])
                        kb.op("scalar", lambda e: e.activation(out=t["el"][:], in_=t["d2"][:], func=AF.Exp), outs=[TB["el"]], ins=[TB["d2"]])
                        sc_ = 128.0 ** -0.5
                        kb.op("vector", lambda e, bq_=bq_, sl=sl: e.scalar_tensor_tensor(out=Q1[:, sl], in0=ps[bq_][:], scalar=sc_, in1=t["e1"][:], op0=ALU.mult, op1=ALU.mult),
                              outs=[BQ[n]], ins=[PS[bq_], TB["e1"]])
                        kb.op("vector", lambda e, bq_=bq_, sl=sl: e.scalar_tensor_tensor(out=Q2[:, sl], in0=ps[bq_][:], scalar=sc_, in1=t["eb"][:], op0=ALU.mult, op1=ALU.mult),
                              outs=[BQ[n]], ins=[PS[bq_], TB["eb"]])
                        kb.op("gpsimd", lambda e, sl=sl: e.tensor_tensor(out=Kt[:, sl], in0=t["k"][:], in1=t["en1"][:], op=ALU.mult), outs=[BQ[n]], ins=[TB["k"], TB["en1"]])
                        kb.op("gpsimd", lambda e: e.tensor_tensor(out=khtb[:], in0=t["k"][:], in1=t["el"][:], op=ALU.mult), outs=[KHTB], ins=[TB["k"], TB["el"]])
                        kb.op("gpsimd", lambda e, n=n: e.tensor_copy(out=ebl[:, 4 * n:4 * n + 4], in_=v4(t["eb"][:])[:, :, 127]), outs=[BQ[n]], ins=[TB["eb"]])
                        for i in range(4):
                            bk = bank()
                            kb.op("tensor", lambda e, i=i, bk=bk: e.matmul(ps[bk][:, 0:128], lhsT=khtb[:, i * 128:(i + 1) * 128], rhs=ident[:], start=True, stop=True),
                                  outs=[PS[bk]], ins=[KHTB, CST])
                            kb.op("scalar", lambda e, i=i, bk=bk, n=n: e.copy(out=Kh[:, 4 * n + i, :], in_=ps[bk][:, 0:128]), outs=[BKH[4 * n + i]], ins=[PS[bk]])
                    for tt in range(NT):
                        bk = bank()
                        n = tt // 4
                        for k in range(8):
                            kb.op("tensor", lambda e, k=k, bk=bk, tt=tt: e.matmul(ps[bk][:, 0:256], lhsT=hT[:, k, tt * 128:(tt + 1) * 128], rhs=wig[:, k, :],
                                                                                 start=(k == 0), stop=(k == 7)),
                                  outs=[PS[bk]], ins=[WIG, HT[n]], mark=(k == 7))
                        kb.op("vector", lambda e, bk=bk, tt=tt: e.tensor_copy(out=Vh[:, tt, :], in_=ps[bk][:, 0:128]), outs=[BV[tt]], ins=[PS[bk]])
                        kb.op("scalar", lambda e, bk=bk, tt=tt: e.activation(out=SGt[:, tt, :], in_=ps[bk][:, 128:256], func=AF.Silu), outs=[BV[tt]], ins=[PS[bk]])
                    kb.op("vector", lambda e: e.memset(Sst[:], 0.0), outs=[SST])
                    at_bank = {}

                    def emit_at(c):
                        bk = bank()
                        at_bank[c] = bk
                        cs_ = slice(c * 128, (c + 1) * 128)
                        c0_ = c * 128
                        kb.op("tensor", lambda e: e.matmul(ps[bk][0:64, 0:64], lhsT=Kt[:, c0_:c0_ + 64], rhs=Q1[:, c0_:c0_ + 64], start=True, stop=True),
                              outs=[PS[bk]], ins=[BQ[c // 4]], mark=False)
                        kb.op("tensor", lambda e: e.matmul(ps[bk][:, 64:128], lhsT=Kt[:, cs_], rhs=Q1[:, c0_ + 64:c0_ + 128], start=True, stop=True),
                              outs=[PS[bk]], ins=[BQ[c // 4]])
                        kb.op("vector", lambda e: e.tensor_tensor(out=atm[c % 2][0:64, 0:64], in0=ps[bk][0:64, 0:64], in1=hmask[0:64, 0:64], op=ALU.mult),
                              outs=[ATM[c % 2]], ins=[PS[bk], CST])
                        kb.op("vector", lambda e: e.tensor_tensor(out=atm[c % 2][:, 64:128], in0=ps[bk][:, 64:128], in1=hmask[:, 64:128], op=ALU.mult),
                              outs=[ATM[c % 2]], ins=[PS[bk], CST])

                    emit_at(0)
                    for c in range(NT):
                        if c + 1 < NT:
                            emit_at(c + 1)
                        cs_ = slice(c * 128, (c + 1) * 128)
                        bd, bo = bank(), bank()
                        kb.op("tensor", lambda e, bd=bd, c=c: e.matmul(ps[bd][:, 0:128], lhsT=Kh[:, c, :], rhs=Vh[:, c, :], start=True, stop=True),
                              outs=[PS[bd]], ins=[BKH[c], BV[c]])
                        kb.op("tensor", lambda e, bo=bo, c=c: e.matmul(ps[bo][:, 0:128], lhsT=atm[c % 2][:], rhs=Vh[:, c, :], start=True, stop=(c == 0)),
                              outs=[PS[bo]], ins=[ATM[c % 2], BV[c]], mark=(c == 0))
                        if c > 0:
                            kb.op("tensor", lambda e, bo=bo, c=c, cs_=cs_: e.matmul(ps[bo][:, 0:128], lhsT=Q2[:, cs_], rhs=sbf[(c - 1) % 2][:], start=False, stop=True),
                                  outs=[PS[bo]], ins=[BQ[c // 4], SBF[(c - 1) % 2]])
                        if c + 1 < NT:
                            kb.op("vector", lambda e, bd=bd, c=c: e.scalar_tensor_tensor(out=Sst[:], in0=Sst[:], scalar=ebl[:, c:c + 1], in1=ps[bd][:, 0:128],
                                                                                     op0=ALU.mult, op1=ALU.add), outs=[SST], ins=[SST, PS[bd], BQ[c // 4]])
                            kb.op("scalar", lambda e, c=c: e.copy(out=sbf[c % 2][:], in_=Sst[:]), outs=[SBF[c % 2]], ins=[SST])
                        i2 = c % 2
                        kb.op("gpsimd", lambda e, i2=i2: e.memset(ssq[i2][:], 0.0), outs=[SSQ[i2]])
                        kb.op("scalar", lambda e, bo=bo, i2=i2: e.activation(out=junk[:], in_=ps[bo][:, 0:128], func=AF.Square, accum_out=ssq[i2][:]),
                              outs=[JK, SSQ[i2]], ins=[PS[bo]])
                        kb.op("scalar", lambda e, i2=i2: e.activation(out=ssq[i2][:], in_=ssq[i2][:], func=AF.Sqrt, scale=1.0 / 128, bias=EPS), outs=[SSQ[i2]], ins=[SSQ[i2]])
                        kb.op("vector", lambda e, i2=i2: e.reciprocal(out=ssq[i2][:], in_=ssq[i2][:]), outs=[SSQ[i2]], ins=[SSQ[i2]])
                        kb.op("vector", lambda e, bo=bo, i2=i2, c0=c0: e.scalar_tensor_tensor(out=of[i2][:], in0=ps[bo][:, 0:128], scalar=ssq[i2][:, 0:1], in1=hng[:, c0:c0 + 128],
                                                                                           op0=ALU.mult, op1=ALU.mult), outs=[OF[i2]], ins=[PS[bo], SSQ[i2], HC])
                        kb.op("gpsimd", lambda e, i2=i2, c=c: e.tensor_tensor(out=obf[i2][:], in0=of[i2][:], in1=SGt[:, c, :], op=ALU.mult), outs=[OBF[i2]], ins=[OF[i2], BV[c]])
                        bt = bank()
                        isel = isel0 if c < 16 else isel1
                        kb.op("tensor", lambda e, bt=bt, i2=i2, isel=isel: e.matmul(ps[bt][:, 0:128], lhsT=obf[i2][:], rhs=isel[:], start=True, stop=True),
                              outs=[PS[bt]], ins=[OBF[i2], CST])
                        s_ = c % 16
                        if c < 16:
                            kb.op("scalar", lambda e, bt=bt, s_=s_, hd=hd: e.copy(out=oT[:, hd, s_ * 128:(s_ + 1) * 128], in_=ps[bt][:, 0:128]), outs=[OT[hd][s_]], ins=[PS[bt]])
                        else:
                            kb.op("vector", lambda e, bt=bt, s_=s_, hd=hd: e.tensor_tensor(out=oT[:, hd, s_ * 128:(s_ + 1) * 128], in0=ps[bt][:, 0:128],
                                                                                       in1=oT[:, hd, s_ * 128:(s_ + 1) * 128], op=ALU.add),
                                  outs=[OT[hd][s_]], ins=[PS[bt], OT[hd][s_]])
                kb.barrier()
            if "oT" in dbg:
                dump("oT", oT[:], [128, 8, SO], BF16)
            if stop_after == "p1b":
                kb.barrier()
                return nc, dbg_out

            SCL = 64.0 ** -0.5
            if not enable_nsa:
                for jf in range(4, 8):
                    kb.op("vector", lambda e, jf=jf: e.memset(oT[:, jf, :], 0.0), outs=OT[jf])
            with _NSAScope() as pn:
                if not enable_nsa:
                    raise _SkipNSA()
                ovl = sb("ovl", [128, 2, 64], BF16, pn)
                kb.dma("sync", ovl[:], I["ovl"], outs=[CST])
                ksT = sb("ksT", [128, S], BF16, pn)
                kwT = sb("kwT", [128, S], BF16, pn)
                kcvT = sb("kcvT", [128, S], BF16, pn)
                vs1 = sb("vs1", [128, NT, 80], BF16, pn)
                vw1 = sb("vw1", [128, NT, 80], BF16, pn)
                KS = Buf(ksT[:])
                kcmpT = sb("kcmpT", [128, 256], BF16, pn)
                vcmp1 = sb("vcmp1", [128, 2, 144], BF16, pn)
                KC = Buf(kcmpT[:])
                wk3 = sb("wk3", [128, 8, 384], BF16, pn)
                wv2 = sb("wv2", [128, 8, 128], BF16, pn)
                wqg = sb("wqg", [128, 8, 256], BF16, pn)
                wgt = sb("wgt", [128, 8, 12], BF16, pn)
                WN = Buf(wk3[:])
                cosb = sb("cosb", [128, 512], F32, pn)
                sinb = sb("sinb", [128, 512], F32, pn)
                CSB = Buf(cosb[:])
                rawb = sb("rawb", [128, 512], BF16, pn)
                RAWB = Buf(rawb[:])
                rt1 = sb("rt1", [128, 512], F32, pn)
                rt2 = sb("rt2", [128, 512], F32, pn)
                RT1, RT2 = Buf(rt1[:]), Buf(rt2[:])
                _padn = int(_os0.environ.get("PADN", "0"))
                if _padn:
                    _pad = sb("padn", [128, _padn], F32, pn)
                srr = [0]

                def sbank():
                    srr[0] = (srr[0] + 1) % 3
                    return srr[0]

                mrr = [0]

                def mbank():
                    return 7

                import os as _os
                _dbgmode = int(_os.environ.get("ROPEDBG", "0"))

                def rope_from(bk, dst_ap, dstbuf):
                    if _dbgmode == 1:
                        kb.op("scalar", lambda e: e.copy(out=dst_ap, in_=ps[bk][:]), outs=[dstbuf], ins=[PS[bk]])
                        return
                    if _dbgmode == 3:
                        kb.op("vector", lambda e: e.tensor_tensor(out=rt1[:], in0=ps[bk][:], in1=cosb[:], op=ALU.mult), outs=[RT1], ins=[PS[bk], CSB])
                        kb.op("gpsimd", lambda e: e.tensor_copy(out=dst_ap, in_=rt1[:]), outs=[dstbuf], ins=[RT1])
                        return
                    if _dbgmode == 4:
                        kb.op("scalar", lambda e: e.copy(out=rawb[:], in_=ps[bk][:]), outs=[RAWB], ins=[PS[bk]])
                        b2 = mbank()
                        kb.op("tensor", lambda e: e.matmul(ps[b2][:], lhsT=pswap[:], rhs=rawb[:], start=True, stop=True), outs=[PS[b2]], ins=[RAWB, CST])
                        kb.op("vector", lambda e: e.tensor_tensor(out=rt1[:], in0=ps[bk][:], in1=cosb[:], op=ALU.mult), outs=[RT1], ins=[PS[bk], CSB])
                        kb.op("vector", lambda e: e.tensor_tensor(out=rt2[:], in0=ps[b2][:], in1=sinb[:], op=ALU.mult), outs=[RT2], ins=[PS[b2], CSB])
                        kb.op("vector", lambda e: e.tensor_tensor(out=dst_ap, in0=rt1[:], in1=rt2[:], op=ALU.add), outs=[dstbuf], ins=[RT1, RT2])
                        return
                    if _dbgmode == 5:
                        kb.op("scalar", lambda e: e.copy(out=rawb[:], in_=ps[bk][:]), outs=[RAWB], ins=[PS[bk]])
                        b2 = mbank()
                        kb.op("tensor", lambda e: e.matmul(ps[b2][:], lhsT=pswap[:], rhs=rawb[:], start=True, stop=True), outs=[PS[b2]], ins=[RAWB, CST])
                        kb.op("vector", lambda e: e.tensor_tensor(out=rt1[:], in0=ps[bk][:], in1=cosb[:], op=ALU.mult), outs=[RT1], ins=[PS[bk], CSB])
                        kb.op("scalar", lambda e: e.copy(out=rt2[:], in_=ps[b2][:]), outs=[RT2], ins=[PS[b2]])
                        _sb = cosb if _os.environ.get("USECOS") else sinb
                        kb.op("vector", lambda e: e.tensor_tensor(out=rt2[:], in0=rt2[:], in1=_sb[:], op=ALU.mult), outs=[RT2], ins=[RT2, CSB])
                        kb.op("vector", lambda e: e.tensor_tensor(out=dst_ap, in0=rt1[:], in1=rt2[:], op=ALU.add), outs=[dstbuf], ins=[RT1, RT2])
                        return
                    if _dbgmode in (7, 8):
                        kb.op("scalar", lambda e: e.copy(out=rawb[:], in_=ps[bk][:]), outs=[RAWB], ins=[PS[bk]])
                        b2 = mbank()
                        kb.op("tensor", lambda e: e.matmul(ps[b2][:], lhsT=pswap[:], rhs=rawb[:], start=True, stop=True), outs=[PS[b2]], ins=[RAWB, CST])
                        kb.op("vector", lambda e: e.tensor_tensor(out=rt1[:], in0=ps[bk][:], in1=cosb[:], op=ALU.mult), outs=[RT1], ins=[PS[bk], CSB])
                        kb.op("scalar", lambda e: e.copy(out=rt2[:], in_=ps[b2][:]), outs=[RT2], ins=[PS[b2]])
                        kb.op("vector", lambda e: e.tensor_tensor(out=rt2[:], in0=rt2[:], in1=sinb[:], op=ALU.mult), outs=[RT2], ins=[RT2, CSB])
                        if _dbgmode == 8:
                            kb.op("vector", lambda e: e.tensor_tensor(out=rt1[:], in0=rt1[:], in1=rt2[:], op=ALU.add), outs=[RT1], ins=[RT1, RT2])
                        kb.op("scalar", lambda e: e.copy(out=dst_ap, in_=rt1[:]), outs=[dstbuf], ins=[RT1])
                        return
                    if _dbgmode in (9, 10):
                        kb.op("vector", lambda e: e.tensor_tensor(out=rt1[:], in0=ps[bk][:], in1=cosb[:], op=ALU.mult), outs=[RT1], ins=[PS[bk], CSB])
                        if _dbgmode == 9:
                            kb.op("scalar", lambda e: e.copy(out=rt2[:], in_=ps[bk][:]), outs=[RT2], ins=[PS[bk]])
                        else:
                            kb.op("vector", lambda e: e.tensor_tensor(out=rt2[:], in0=rt1[:], in1=cosb[:], op=ALU.mult), outs=[RT2], ins=[RT1, CSB])
                        kb.op("gpsimd", lambda e: e.tensor_copy(out=dst_ap, in_=rt1[:]), outs=[dstbuf], ins=[RT1])
                        return
                    if _dbgmode in (11, 12):
                        kb.op("scalar", lambda e: e.copy(out=rawb[:], in_=ps[bk][:]), outs=[RAWB], ins=[PS[bk]])
                        b2 = mbank()
                        kb.op("tensor", lambda e: e.matmul(ps[b2][:], lhsT=pswap[:], rhs=rawb[:], start=True, stop=True), outs=[PS[b2]], ins=[RAWB, CST])
                        kb.op("vector", lambda e: e.tensor_tensor(out=rt1[:], in0=ps[bk][:], in1=cosb[:], op=ALU.mult), outs=[RT1], ins=[PS[bk], CSB, RAWB])
                        kb.op("gpsimd", lambda e: e.tensor_copy(out=dst_ap, in_=rt1[:]), outs=[dstbuf], ins=[RT1])
                        if _dbgmode == 12:
                            return
                        kb.op("vector", lambda e: e.tensor_tensor(out=rt1[:], in0=ps[b2][:], in1=sinb[:], op=ALU.mult), outs=[RT1], ins=[PS[b2], CSB])
                        kb.op("gpsimd", lambda e: e.tensor_tensor(out=dst_ap, in0=dst_ap, in1=rt1[:], op=ALU.add), outs=[dstbuf], ins=[RT1, dstbuf])
                        return
                    if _dbgmode == 2:
                        kb.op("scalar", lambda e: e.copy(out=rawb[:], in_=ps[bk][:]), outs=[RAWB], ins=[PS[bk]])
                        b2 = mbank()
                        kb.op("tensor", lambda e: e.matmul(ps[b2][:], lhsT=pswap[:], rhs=rawb[:], start=True, stop=True), outs=[PS[b2]], ins=[RAWB, CST])
                        kb.op("scalar", lambda e: e.copy(out=dst_ap, in_=ps[b2][:]), outs=[dstbuf], ins=[PS[b2]])
                        return
                    kb.op("scalar", lambda e: e.copy(out=rawb[:], in_=ps[bk][:]), outs=[RAWB], ins=[PS[bk]])
                    b2 = mbank()
                    kb.op("tensor", lambda e: e.matmul(ps[b2][:], lhsT=pswap[:], rhs=rawb[:], start=True, stop=True), outs=[PS[b2]], ins=[RAWB, CST])
                    kb.op("vector", lambda e: e.tensor_tensor(out=rt1[:], in0=ps[bk][:], in1=cosb[:], op=ALU.mult), outs=[RT1], ins=[PS[bk], CSB])
                    kb.op("vector", lambda e: e.tensor_tensor(out=rt2[:], in0=ps[b2][:], in1=sinb[:], op=ALU.mult), outs=[RT2], ins=[PS[b2], CSB])
                    kb.op("gpsimd", lambda e: e.tensor_tensor(out=dst_ap, in0=rt1[:], in1=rt2[:], op=ALU.add), outs=[dstbuf], ins=[RT1, RT2])

                for g in range(2):
                    for j, cbase in enumerate((2560, 2688)):
                        kb.dma("gpsimd", wk3[:, :, j * 64:(j + 1) * 64], w_in_v[:, :, cbase + g * 64:cbase + g * 64 + 64], outs=[WN])
                    for j, cbase in enumerate((2816, 2816, 3072, 3072)):
                        kb.dma("gpsimd", wk3[:, :, 128 + j * 64:128 + (j + 1) * 64], w_in_v[:, :, cbase + g * 64:cbase + g * 64 + 64], outs=[WN])
                    for j, cbase in enumerate((2944, 3200)):
                        kb.dma("gpsimd", wv2[:, :, j * 64:(j + 1) * 64], w_in_v[:, :, cbase + g * 64:cbase + g * 64 + 64], outs=[WN])
                    kb.dma("gpsimd", wqg[:], w_in_v[:, :, 2048 + g * 256:2048 + (g + 1) * 256], outs=[WN])
                    kb.dma("gpsimd", wgt[:], w_in_v[:, :, 3328 + g * 12:3328 + (g + 1) * 12], outs=[WN])
                    kb.op("vector", lambda e: e.memset(vs1[:, :, 64:65], 1.0), outs=[KS])
                    kb.op("vector", lambda e: e.memset(vw1[:, :, 64:65], 1.0), outs=[KS])
                    if stop_after == "p1c_a":
                        dump("wk3", wk3[:], [128, 8, 384], BF16)
                        return nc, dbg_out
                    for n in range(NB):
                        sl = slice(n * 512, (n + 1) * 512)
                        kb.dma("sync", cosb[:], I["cosT"][:, sl], outs=[CSB])
                        kb.dma("sync", sinb[:], I["sinT"][:, sl], outs=[CSB])
                        for j in range(3):
                            bk = sbank()
                            for k in range(8):
                                kb.op("tensor", lambda e, k=k, bk=bk, j=j: e.matmul(ps[bk][:], lhsT=wk3[:, k, j * 128:(j + 1) * 128], rhs=hT[:, k, sl],
                                                                                    start=(k == 0), stop=(k == 7)), outs=[PS[bk]], ins=[WN, HT[n]], mark=(k == 7))
                            if j == 0:
                                kb.op("scalar", lambda e, bk=bk: e.copy(out=kcvT[:, sl], in_=ps[bk][:]), outs=[KS], ins=[PS[bk]])
                            else:
                                rope_from(bk, (ksT if j == 1 else kwT)[:, sl], KS)
                        if stop_after == "p1c_b":
                            dump("ksT", ksT[:], [128, S], BF16)
                            return nc, dbg_out
                        for i in range(4):
                            tt = 4 * n + i
                            bk = mbank()
                            for k in range(8):
                                kb.op("tensor", lambda e, k=k, bk=bk, tt=tt: e.matmul(ps[bk][:, 0:128], lhsT=hT[:, k, tt * 128:(tt + 1) * 128], rhs=wv2[:, k, :],
                                                                                     start=(k == 0), stop=(k == 7)), outs=[PS[bk]], ins=[WN, HT[n]], mark=(k == 7))
                            kb.op("scalar", lambda e, bk=bk, tt=tt: e.copy(out=vs1[:, tt, 0:64], in_=ps[bk][:, 0:64]), outs=[KS], ins=[PS[bk]])
                            kb.op("vector", lambda e, bk=bk, tt=tt: e.tensor_copy(out=vw1[:, tt, 0:64], in_=ps[bk][:, 64:128]), outs=[KS], ins=[PS[bk]])
                    if stop_after == "p1c_k":
                        dump("ksT", ksT[:], [128, S], BF16)
                        dump("kcvT", kcvT[:], [128, S], BF16)
                        dump("vs1", vs1[:], [128, NT, 80], BF16)
                        return nc, dbg_out
                    with ExitStack() as pc:
                        w1kv = sb("w1kv", [128, 32, 256], BF16, pc)
                        peT = sb("peT", [128, 32], F32, pc)
                        peTb = sb("peTb", [128, 32], BF16, pc)
                        b1kv = sb("b1kv", [128, 4], F32, pc)
                        w2k2 = sb("w2k2", [128, 2, 128], BF16, pc)
                        w2v = sb("w2v", [128, 2, 64], BF16, pc)
                        hid = sb("hid", [128, 4, 256], BF16, pc)
                        beff = sb("beff", [128, 4], F32, pc)
                        gx = sb("gx", [128, 256], F32, pc)
                        gu = sb("gu", [128, 256], F32, pc)
                        gs = sb("gs", [128, 256], F32, pc)
                        CW = Buf(w1kv[:])
                        HID = Buf(hid[:])
                        GX = Buf(gx[:])
                        kb.dma("gpsimd", w1kv[0:64], I["w1k"], outs=[CW])
                        kb.dma("gpsimd", w1kv[64:128], I["w1v"], outs=[CW])
                        kb.dma("sync", peT[0:64], I["peTk"], outs=[CW])
                        kb.dma("sync", peT[64:128], I["peTv"], outs=[CW])
                        kb.dma("sync", b1kv[:, 0:2], I["b1k"], outs=[CW])
                        kb.dma("sync", b1kv[:, 2:4], I["b1v"], outs=[CW])
                        kb.dma("gpsimd", w2k2[:, :, 0:64], I["w2k"], outs=[CW])
                        kb.dma("gpsimd", w2k2[:, :, 64:128], I["w2k"], outs=[CW])
                        kb.dma("gpsimd", w2v[:], I["w2v"], outs=[CW])
                        kb.op("vector", lambda e: e.tensor_copy(out=peTb[:], in_=peT[:]), outs=[CW], ins=[CW])
                        kb.op("vector", lambda e: e.memset(hid[:], 0.0), outs=[HID])
                        for kv in range(2):
                            p0_ = kv * 64
                            for hc in range(2):
                                bk, bb = sbank(), mbank()
                                for l in range(32):
                                    kb.op("tensor", lambda e, l=l, bk=bk, hc=hc, p0_=p0_: e.matmul(ps[bk][:, 0:255], lhsT=w1kv[p0_:p0_ + 64, l, hc * 128:(hc + 1) * 128],
                                                                                                rhs=kcvT[p0_:p0_ + 64, l:l + 16 * 254 + 1:16], start=(l == 0), stop=(l == 31)),
                                          outs=[PS[bk]], ins=[CW, KS], mark=(l == 31))
                                for l in range(32):
                                    kb.op("tensor", lambda e, l=l, bb=bb, hc=hc, p0_=p0_: e.matmul(ps[bb][:, 0:1], lhsT=w1kv[p0_:p0_ + 64, l, hc * 128:(hc + 1) * 128],
                                                                                                rhs=peTb[p0_:p0_ + 64, l:l + 1], start=(l == 0), stop=(l == 31)),
                                          outs=[PS[bb]], ins=[CW], mark=(l == 31))
                                ci = kv * 2 + hc
                                kb.op("vector", lambda e, bb=bb, ci=ci: e.tensor_tensor(out=beff[:, ci:ci + 1], in0=ps[bb][:, 0:1], in1=b1kv[:, ci:ci + 1], op=ALU.add),
                                      outs=[GX], ins=[PS[bb], CW])
                                kb.op("vector", lambda e, bk=bk, ci=ci: e.tensor_scalar(out=gx[:, 0:255], in0=ps[bk][:, 0:255], scalar1=beff[:, ci:ci + 1], scalar2=None, op0=ALU.add),
                                      outs=[GX], ins=[PS[bk], GX])
                                kb.op("vector", lambda e: e.tensor_tensor(out=gu[:, 0:255], in0=gx[:, 0:255], in1=gx[:, 0:255], op=ALU.mult), outs=[GX], ins=[GX])
                                kb.op("vector", lambda e: e.tensor_scalar(out=gu[:, 0:255], in0=gu[:, 0:255], scalar1=0.044715, scalar2=1.0, op0=ALU.mult, op1=ALU.add), outs=[GX], ins=[GX])
                                kb.op("vector", lambda e: e.tensor_tensor(out=gu[:, 0:255], in0=gu[:, 0:255], in1=gx[:, 0:255], op=ALU.mult), outs=[GX], ins=[GX])
                                kb.op("scalar", lambda e: e.activation(out=gs[:, 0:255], in_=gu[:, 0:255], func=AF.Sigmoid, scale=1.5957691216057308), outs=[GX], ins=[GX])
                                kb.op("vector", lambda e, ci=ci: e.tensor_tensor(out=hid[:, ci, 0:255], in0=gx[:, 0:255], in1=gs[:, 0:255], op=ALU.mult), outs=[HID], ins=[GX])
                        bk = sbank()
                        for hc in range(2):
                            kb.op("tensor", lambda e, hc=hc, bk=bk: e.matmul(ps[bk][:, 0:256], lhsT=w2k2[:, hc, :], rhs=hid[:, hc, :], start=(hc == 0), stop=(hc == 1)),
                                  outs=[PS[bk]], ins=[CW, HID], mark=(hc == 1))
                        kb.op("scalar", lambda e, bk=bk: e.copy(out=kcmpT[:], in_=ps[bk][:, 0:256]), outs=[KC], ins=[PS[bk]])
                        for ct in range(2):
                            bk = sbank()
                            for hc in range(2):
                                kb.op("tensor", lambda e, hc=hc, bk=bk, ct=ct: e.matmul(ps[bk][:, 0:64], lhsT=hid[:, 2 + hc, ct * 128:(ct + 1) * 128], rhs=w2v[:, hc, :],
                                                                                       start=(hc == 0), stop=(hc == 1)), outs=[PS[bk]], ins=[CW, HID], mark=(hc == 1))
                            kb.op("scalar", lambda e, bk=bk, ct=ct: e.copy(out=vcmp1[:, ct, 0:64], in_=ps[bk][:, 0:64]), outs=[KC], ins=[PS[bk]])
                        kb.op("vector", lambda e: e.memset(vcmp1[:, :, 64:65], 1.0), outs=[KC])
                        kb.op("vector", lambda e: e.tensor_copy(out=vcmp1[:, :, 65:129], in_=ovl[:]), outs=[KC], ins=[CST])
                        kb.barrier()
                    if stop_after == "p1c_c":
                        dump("kcmpT", kcmpT[:], [128, 256], BF16)
                        dump("vcmp1", vcmp1[:], [128, 2, 144], BF16)
                        return nc, dbg_out
                    with ExitStack() as pq:
                        wband = sb("wband", [128, 8, 512], BF16, pq)
                        ekt = sb("ekt", [128, 32, 128], BF16, pq)
                        QC = Buf(wband[:])
                        kb.dma("sync", wband[:], I["wband"], outs=[QC])
                        kb.dma("sync", ekt[:], I["ekt"], outs=[QC])
                        qTb = sb("qTb", [128, 2, 512], BF16, pq)
                        qrTb = sb("qrTb", [128, 2, 512], BF16, pq)
                        QB_ = Buf(qTb[:])
                        QRB = Buf(qrTb[:])
                        gts = sb("gts", [128, 4, 12], F32, pq)
                        GTS = Buf(gts[:])
                        cmpbb = sb("cmpbb", [128, 512], BF16, pq)
                        CMB = Buf(cmpbb[:])
                        pt = [sb(f"pt{i}", [128, 512], BF16, pq) for i in range(3)]
                        PT = [Buf(t[:]) for t in pt]
                        onsa = sb("onsa", [128, 4, 256], F32, pq)
                        ONSA = Buf(onsa[:])
                        obn = sb("obn", [128, 4, 256], BF16, pq)
                        OBN = Buf(obn[:])
                        pslc = sb("pslc", [128, 4, 64], F32, pq)
                        PSLC = Buf(pslc[:])
                        sadd = sb("sadd", [128, 64], F32, pq)
                        SADD = Buf(sadd[:])
                        score = sb("score", [128, 64], F32, pq)
                        stmp = sb("stmp", [128, 64], F32, pq)
                        sel = sb("sel", [128, 64], F32, pq)
                        m8 = sb("m8", [128, 16], F32, pq)
                        negb = sb("negb", [128, 128], BF16, pq)
                        SEL = Buf(score[:])
                        negbT = sb("negbT", [128, 512], BF16, pq)
                        NBT = Buf(negbT[:])
                        rz = sb("rz", [128, 4], F32, pq)
                        RZ = Buf(rz[:])
                        accs = [ps[3][:, 0:129], ps[4][:, 0:129], ps[5][:, 0:129], ps[6][:, 0:129]]
                        ACC = [PS[3], PS[4], PS[5], PS[6]]
                        prr = [0]

                        def run_branch(steps):
                            LA = 2
                            n_ = len(steps)
                            for idx_ in range(n_ + LA):
                                if idx_ < n_:
                                    st = steps[idx_]
                                    bk = sbank()
                                    nm = len(st["s"])
                                    for idx, (l_, r_, insb) in enumerate(st["s"]):
                                        kb.op("tensor", lambda e, l_=l_, r_=r_, idx=idx, nm=nm, bk=bk: e.matmul(ps[bk][:], lhsT=l_, rhs=r_, start=(idx == 0), stop=(idx == nm - 1)),
                                              outs=[PS[bk]], ins=insb, mark=(idx == nm - 1))
                                    prr[0] = (prr[0] + 1) % 3
                                    pi = prr[0]
                                    kb.op("scalar", lambda e, bk=bk, pi=pi: e.activation(out=pt[pi][:], in_=ps[bk][:], func=AF.Exp, scale=SCL), outs=[PT[pi]], ins=[PS[bk]])
                                    st["pi"] = pi
                                if idx_ >= LA:
                                    prev = steps[idx_ - LA]
                                    pi = prev["pi"]
                                    for (i, rhs_ap, w_, st_, sp_) in prev["pv"]:
                                        kb.op("tensor", lambda e, i=i, rhs_ap=rhs_ap, w_=w_, st_=st_, sp_=sp_, pi=pi: e.matmul(accs[i][:, 0:w_], lhsT=pt[pi][:, i * 128:(i + 1) * 128], rhs=rhs_ap,
                                                                                                                 start=st_, stop=sp_),
                                              outs=[ACC[i]], ins=[PT[pi], KS, KC])

                        def finish(h, br, first):
                            zc = 64
                            for i in range(4):
                                kb.op("vector", lambda e, i=i: e.tensor_scalar(out=rz[:, i:i + 1], in0=accs[i][:, zc:zc + 1], scalar1=1e-30, scalar2=None, op0=ALU.max),
                                      outs=[RZ], ins=[ACC[i]])
                            kb.op("vector", lambda e: e.reciprocal(out=rz[:], in_=rz[:]), outs=[RZ], ins=[RZ])
                            if br == 0:
                                for i in range(4):
                                    if h == 0:
                                        kb.op("vector", lambda e, i=i: e.tensor_scalar(out=pslc[:, i, :], in0=accs[i][:, 65:129], scalar1=rz[:, i:i + 1], scalar2=None, op0=ALU.mult),
                                              outs=[PSLC], ins=[ACC[i], RZ])
                                    else:
                                        kb.op("vector", lambda e, i=i: e.scalar_tensor_tensor(out=pslc[:, i, :], in0=accs[i][:, 65:129], scalar=rz[:, i:i + 1], in1=pslc[:, i, :],
                                                                                           op0=ALU.mult, op1=ALU.add), outs=[PSLC], ins=[ACC[i], RZ, PSLC])
                            kb.op("vector", lambda e: e.tensor_tensor(out=rz[:], in0=rz[:], in1=gts[:, :, h * 3 + br], op=ALU.mult), outs=[RZ], ins=[RZ, GTS])
                            for i in range(4):
                                dst = onsa[:, i, h * 64:(h + 1) * 64]
                                if first:
                                    kb.op("vector", lambda e, i=i, dst=dst: e.tensor_scalar(out=dst, in0=accs[i][:, 0:64], scalar1=rz[:, i:i + 1], scalar2=None, op0=ALU.mult),
                                          outs=[ONSA], ins=[ACC[i], RZ])
                                else:
                                    kb.op("vector", lambda e, i=i, dst=dst: e.scalar_tensor_tensor(out=dst, in0=accs[i][:, 0:64], scalar=rz[:, i:i + 1], in1=dst, op0=ALU.mult, op1=ALU.add),
                                          outs=[ONSA], ins=[ACC[i], RZ, ONSA])

                        for qb in range(NB):
                            sl = slice(qb * 512, (qb + 1) * 512)
                            kb.dma("sync", cosb[:], I["cosT"][:, sl], outs=[CSB])
                            kb.dma("sync", sinb[:], I["sinT"][:, sl], outs=[CSB])
                            kb.dma("sync", cmpbb[:], I["cmpb"][:, qb, :], outs=[CMB])
                            for ch in range(2):
                                bk = sbank()
                                for k in range(8):
                                    kb.op("tensor", lambda e, k=k, bk=bk, ch=ch: e.matmul(ps[bk][:], lhsT=wqg[:, k, ch * 128:(ch + 1) * 128], rhs=hT[:, k, sl],
                                                                                         start=(k == 0), stop=(k == 7)), outs=[PS[bk]], ins=[WN, HT[qb]], mark=(k == 7))
                                kb.op("scalar", lambda e, bk=bk, ch=ch: e.copy(out=qTb[:, ch, :], in_=ps[bk][:]), outs=[QB_], ins=[PS[bk]])
                                rope_from(bk, qrTb[:, ch, :], QRB)
                            for i in range(4):
                                tt = 4 * qb + i
                                bk = mbank()
                                for k in range(8):
                                    kb.op("tensor", lambda e, k=k, bk=bk, tt=tt: e.matmul(ps[bk][:, 0:12], lhsT=hT[:, k, tt * 128:(tt + 1) * 128], rhs=wgt[:, k, :],
                                                                                         start=(k == 0), stop=(k == 7)), outs=[PS[bk]], ins=[WN, HT[qb]], mark=(k == 7))
                                kb.op("scalar", lambda e, bk=bk, i=i: e.activation(out=gts[:, i, :], in_=ps[bk][:, 0:12], func=AF.Sigmoid), outs=[GTS], ins=[PS[bk]])
                            if stop_after == "p1c_qa":
                                dump("onsa", onsa[:], [128, 4, 256])
                                return nc, dbg_out
                            ncts = 1 if qb < 4 else 2
                            for h in range(4):
                                ch, p0_ = h // 2, (h % 2) * 64
                                steps = []
                                for ct in range(ncts):
                                    smm = [(kcmpT[p0_:p0_ + 64, ct * 128:(ct + 1) * 128], qTb[p0_:p0_ + 64, ch, :], [KC, QB_])]
                                    if ct == ncts - 1:
                                        smm.append((ident[:], cmpbb[:], [CST, CMB]))
                                    pv = [(i, vcmp1[:, ct, 0:129], 129, ct == 0, ct == ncts - 1) for i in range(4)]
                                    steps.append({"s": smm, "pv": pv})
                                run_branch(steps)
                                finish(h, 0, True)
                            if stop_after == "p1c_qb":
                                dump("onsa", onsa[:], [128, 4, 256])
                                return nc, dbg_out
                            for i in range(4):
                                qt = 4 * qb + i
                                kb.dma("sync", sadd[:], I["seladd"][:, qt, :], outs=[SADD])
                                kb.op("vector", lambda e, i=i: e.tensor_tensor(out=score[:], in0=pslc[:, i, :], in1=sadd[:], op=ALU.add), outs=[SEL], ins=[PSLC, SADD])
                                kb.op("vector", lambda e: e.max(out=m8[:, 0:8], in_=score[:]), outs=[SEL], ins=[SEL])
                                kb.op("vector", lambda e: e.match_replace(out=stmp[:], in_to_replace=m8[:, 0:8], in_values=score[:], imm_value=-3e38), outs=[SEL], ins=[SEL])
                                kb.op("vector", lambda e: e.max(out=m8[:, 8:16], in_=stmp[:]), outs=[SEL], ins=[SEL])
                                kb.op("vector", lambda e: e.tensor_scalar(out=sel[:], in0=score[:], scalar1=m8[:, 15:16], scalar2=None, op0=ALU.is_ge), outs=[SEL], ins=[SEL])
                                kb.op("vector", lambda e: e.scalar_tensor_tensor(out=sel[:], in0=score[:], scalar=-1e29, in1=sel[:], op0=ALU.is_gt, op1=ALU.mult), outs=[SEL], ins=[SEL])
                                kb.op("vector", lambda e: e.tensor_scalar(out=negb[:, 0:64], in0=sel[:], scalar1=-1.0, scalar2=-NEG, op0=ALU.add, op1=ALU.mult), outs=[SEL], ins=[SEL])
                                kb.op("vector", lambda e: e.tensor_scalar(out=negb[:, 64:128], in0=sel[:], scalar1=-1.0, scalar2=-NEG, op0=ALU.add, op1=ALU.mult), outs=[SEL], ins=[SEL])
                                bk = mbank()
                                kb.op("tensor", lambda e, bk=bk: e.matmul(ps[bk][:, 0:128], lhsT=negb[:], rhs=ident[:], start=True, stop=True), outs=[PS[bk]], ins=[SEL, CST])
                                kb.op("scalar", lambda e, bk=bk, i=i: e.copy(out=negbT[:, i * 128:(i + 1) * 128], in_=ps[bk][:, 0:128]), outs=[NBT], ins=[PS[bk]])
                            if stop_after == "p1c_qc":
                                dump("onsa", onsa[:], [128, 4, 256])
                                return nc, dbg_out
                            for h in range(4):
                                ch, p0_ = h // 2, (h % 2) * 64
                                steps = []
                                for kt in range(4 * qb + 4):
                                    smm = [(ksT[p0_:p0_ + 64, kt * 128:(kt + 1) * 128], qrTb[p0_:p0_ + 64, ch, :], [KS, QRB]),
                                           (ekt[p0_:p0_ + 64, kt, :], negbT[p0_:p0_ + 64, :], [QC, NBT])]
                                    if kt >= 4 * qb:
                                        smm.append((ident[:], wband[:, 4 + kt - 4 * qb, :], [CST, QC]))
                                    pv = [(i, vs1[:, kt, 0:65], 65, kt == 0, kt == 4 * qb + i) for i in range(4) if kt <= 4 * qb + i]
                                    steps.append({"s": smm, "pv": pv})
                                run_branch(steps)
                                finish(h, 1, False)
                            if stop_after == "p1c_qd":
                                dump("onsa", onsa[:], [128, 4, 256])
                                return nc, dbg_out
                            for h in range(4):
                                ch, p0_ = h // 2, (h % 2) * 64
                                steps = []
                                jmin = max(0, 4 - 4 * qb)
                                for j in range(jmin, 8):
                                    kt = 4 * qb - 4 + j
                                    smm = [(kwT[p0_:p0_ + 64, kt * 128:(kt + 1) * 128], qrTb[p0_:p0_ + 64, ch, :], [KS, QRB]),
                                           (ident[:], wband[:, j, :], [CST, QC])]
                                    pv = [(i, vw1[:, kt, 0:65], 65, j == max(i, jmin), j == i + 4) for i in range(4) if i <= j <= i + 4]
                                    steps.append({"s": smm, "pv": pv})
                                run_branch(steps)
                                finish(h, 2, False)
                            if stop_after == "p1c_q":
                                dump("onsa", onsa[:], [128, 4, 256])
                                dump("pslc", pslc[:], [128, 4, 64])
                                dump("negbT", negbT[:], [128, 512], BF16)
                                return nc, dbg_out
                            kb.op("gpsimd", lambda e: e.tensor_copy(out=obn[:], in_=onsa[:]), outs=[OBN], ins=[ONSA])
                            for i in range(4):
                                qt = 4 * qb + i
                                isel = isel0 if qt < 16 else isel1
                                s_ = qt % 16
                                for ch in range(2):
                                    jf = 4 + g * 2 + ch
                                    bk = mbank()
                                    kb.op("tensor", lambda e, bk=bk, i=i, ch=ch, isel=isel: e.matmul(ps[bk][:, 0:128], lhsT=obn[:, i, ch * 128:(ch + 1) * 128], rhs=isel[:], start=True, stop=True),
                                          outs=[PS[bk]], ins=[OBN, CST])
                                    dst = oT[:, jf, s_ * 128:(s_ + 1) * 128]
                                    if qt < 16:
                                        kb.op("scalar", lambda e, bk=bk, dst=dst: e.copy(out=dst, in_=ps[bk][:, 0:128]), outs=[OT[jf][s_]], ins=[PS[bk]])
                                    else:
                                        kb.op("vector", lambda e, bk=bk, dst=dst: e.tensor_tensor(out=dst, in0=ps[bk][:, 0:128], in1=dst, op=ALU.add),
                                              outs=[OT[jf][s_]], ins=[PS[bk], OT[jf][s_]])
                        kb.barrier()
                kb.barrier()
            if "oT2" in dbg:
                dump("oT2", oT[:], [128, 8, SO], BF16)
            if stop_after == "p1c":
                kb.barrier()
                return nc, dbg_out
        kb.barrier()
        with ExitStack() as p2:
            acc = sb("acc", [128, 8, SO], F32, p2)
            ACCB = [Buf(acc[:, :, nb * 512:(nb + 1) * 512]) for nb in range(4)]
            wo = sb("wo", [128, 8, D], BF16, p2)
            WO = Buf(wo[:])
            fg = sb("fg", [128, 8], F32, p2)
            FG = Buf(fg[:])
            kb.dma("sync", fg[:], I["fg"], outs=[FG])
            xTo_v = I["xTo"].rearrange("(k p) t -> p k t", p=128)
            for nb in range(4):
                kb.dma("sync", acc[:, :, nb * 512:(nb + 1) * 512], xTo_v[:, :, nb * 512:(nb + 1) * 512], outs=[ACCB[nb]])
            w_out_v = I["w_out"].rearrange("(k p) c -> p k c", p=128)
            for k in range(8):
                kb.dma("gpsimd", wo[:, k, :], w_out_v[:, k, :], outs=[WO])
            rr2 = [0]

            def bank2():
                rr2[0] = (rr2[0] + 1) % 8
                return rr2[0]

            for nb in range(4):
                sl = slice(nb * 512, (nb + 1) * 512)
                for m in range(8):
                    bk = bank2()
                    for k in range(8):
                        kb.op("tensor", lambda e, k=k, m=m, bk=bk, sl=sl: e.matmul(ps[bk][:], lhsT=wo[:, k, m * 128:(m + 1) * 128], rhs=oT[:, k, sl], start=(k == 0), stop=(k == 7)),
                              outs=[PS[bk]], ins=[WO] + [OT[k][s] for s in range(nb * 4, nb * 4 + 4)], mark=(k == 7))
                    kb.op("vector", lambda e, m=m, bk=bk, sl=sl: e.scalar_tensor_tensor(out=acc[:, m, sl], in0=ps[bk][:], scalar=g1c(m), in1=acc[:, m, sl], op0=ALU.mult, op1=ALU.add),
                          outs=[ACCB[nb]], ins=[PS[bk], ACCB[nb], MOD])
            if "x2T" in dbg:
                dump("x2T", acc[:], [128, 8, SO])
            h2T = sb("h2T", [128, 8, SO], BF16, p2)
            H2 = [Buf(h2T[:, :, nb * 512:(nb + 1) * 512]) for nb in range(4)]
            with ExitStack() as p2a:
                sqa = sb("sqa", [128, 8, 512], F32, p2a)
                SQA = Buf(sqa[:])
                rsa = sb("rsa", [128, 512], F32, p2a)
                RSA = Buf(rsa[:])
                tma = [sb(f"tma{i}", [128, 512], F32, p2a) for i in range(2)]
                TMA = [Buf(t[:]) for t in tma]
                for nb in range(4):
                    sl = slice(nb * 512, (nb + 1) * 512)
                    kb.op("scalar", lambda e, sl=sl: e.activation(out=sqa[:], in_=acc[:, :, sl], func=AF.Square), outs=[SQA], ins=[ACCB[nb]])
                    bk = bank2()
                    for k in range(8):
                        kb.op("tensor", lambda e, k=k, bk=bk: e.matmul(ps[bk][:], lhsT=onesf[:], rhs=sqa[:, k, :], start=(k == 0), stop=(k == 7)),
                              outs=[PS[bk]], ins=[SQA, CST], mark=(k == 7))
                    kb.op("scalar", lambda e, bk=bk: e.activation(out=rsa[:], in_=ps[bk][:], func=AF.Sqrt, scale=1.0 / D, bias=EPS), outs=[RSA], ins=[PS[bk]])
                    kb.op("vector", lambda e: e.reciprocal(out=rsa[:], in_=rsa[:]), outs=[RSA], ins=[RSA])
                    for k in range(8):
                        T = TMA[k % 2]
                        t_ = tma[k % 2]
                        kb.op("vector", lambda e, k=k, t_=t_, sl=sl: e.tensor_tensor(out=t_[:], in0=acc[:, k, sl], in1=rsa[:], op=ALU.mult), outs=[T], ins=[ACCB[nb], RSA])
                        kb.op("scalar", lambda e, k=k, t_=t_, sl=sl: e.activation(out=h2T[:, k, sl], in_=t_[:], func=AF.Identity, scale=a2[:, k:k + 1], bias=sh2(k)),
                              outs=[H2[nb]], ins=[T, MOD])
                kb.barrier()
            if "h2T" in dbg:
                dump("h2T", h2T[:], [128, 8, SO], BF16)
            gw = sb("gw", [128, 16, 65], F32, p2)
            GW = Buf(gw[:])
            kb.op("vector", lambda e: e.memset(gw[:], 1.0), outs=[GW])
            with ExitStack() as p2r:
                rwf = sb("rwf", [128, 8, 64], F32, p2r)
                rwb = sb("rwb", [128, 8, 64], BF16, p2r)
                rbias = sb("rbias", [128, 64], F32, p2r)
                RW = Buf(rwf[:])
                kb.dma("sync", rwf[:], I["rw"], outs=[RW])
                kb.dma("sync", rbias[:], I["rbias"], outs=[RW])
                kb.op("vector", lambda e: e.tensor_copy(out=rwb[:], in_=rwf[:]), outs=[RW], ins=[RW])
                scr = sb("scr", [128, 64], F32, p2r)
                chs = sb("chs", [128, 64], F32, p2r)
                eq = sb("eq", [128, 64], F32, p2r)
                chm = sb("chm", [128, 64], F32, p2r)
                m1 = sb("m1", [128, 8], F32, p2r)
                m2 = sb("m2", [128, 8], F32, p2r)
                gsm = sb("gsm", [128, 8], F32, p2r)
                g8 = sb("g8", [128, 8], F32, p2r)
                gmk = sb("gmk", [128, 8], F32, p2r)
                e8 = sb("e8", [128, 8], F32, p2r)
                ssum = sb("ssum", [128, 1], F32, p2r)
                RT = Buf(scr[:])
                v3 = lambda ap: ap.rearrange("p (g j) -> p g j", j=8)
                b3 = lambda ap: ap.rearrange("p (g o) -> p g o", o=1).to_broadcast([128, 8, 8])
                for tt in range(16):
                    bk = bank2()
                    for k in range(8):
                        kb.op("tensor", lambda e, k=k, bk=bk, tt=tt: e.matmul(ps[bk][:, 0:64], lhsT=h2T[:, k, tt * 128:(tt + 1) * 128], rhs=rwb[:, k, :], start=(k == 0), stop=(k == 7)),
                              outs=[PS[bk]], ins=[H2[tt // 4], RW], mark=(k == 7))
                    kb.op("scalar", lambda e, bk=bk: e.activation(out=scr[:], in_=ps[bk][:, 0:64], func=AF.Sigmoid), outs=[RT], ins=[PS[bk]])
                    V = lambda f: kb.op("vector", f, outs=[RT], ins=[RT, RW])
                    V(lambda e: e.tensor_tensor(out=chs[:], in0=scr[:], in1=rbias[:], op=ALU.add))
                    V(lambda e: e.tensor_reduce(out=m1[:], in_=v3(chs[:]), axis=AX.X, op=ALU.max))
                    V(lambda e: e.tensor_tensor(out=v3(eq[:]), in0=v3(chs[:]), in1=b3(m1[:]), op=ALU.is_equal))
                    V(lambda e: e.scalar_tensor_tensor(out=eq[:], in0=eq[:], scalar=-1e30, in1=chs[:], op0=ALU.mult, op1=ALU.add))
                    V(lambda e: e.tensor_reduce(out=m2[:], in_=v3(eq[:]), axis=AX.X, op=ALU.max))
                    V(lambda e: e.tensor_tensor(out=gsm[:], in0=m1[:], in1=m2[:], op=ALU.add))
                    V(lambda e: e.max(out=g8[:], in_=gsm[:]))
                    V(lambda e: e.tensor_scalar(out=gmk[:], in0=gsm[:], scalar1=g8[:, 3:4], scalar2=None, op0=ALU.is_ge))
                    V(lambda e: e.scalar_tensor_tensor(out=v3(chm[:]), in0=v3(chs[:]), scalar=10.0, in1=b3(gmk[:]), op0=ALU.add, op1=ALU.mult))
                    V(lambda e: e.max(out=e8[:], in_=chm[:]))
                    V(lambda e: e.tensor_scalar(out=eq[:], in0=chm[:], scalar1=e8[:, 7:8], scalar2=None, op0=ALU.is_ge))
                    V(lambda e: e.tensor_tensor(out=eq[:], in0=eq[:], in1=scr[:], op=ALU.mult))
                    V(lambda e: e.tensor_reduce(out=ssum[:], in_=eq[:], axis=AX.X, op=ALU.add))
                    V(lambda e: e.reciprocal(out=ssum[:], in_=ssum[:]))
                    kb.op("vector", lambda e, tt=tt: e.tensor_scalar(out=gw[:, tt, 0:64], in0=eq[:], scalar1=ssum[:, 0:1], scalar2=2.5, op0=ALU.mult, op1=ALU.mult),
                          outs=[GW], ins=[RT])
                kb.barrier()
            if "gw" in dbg:
                dump("gw", gw[:], [128, 16, 65])
            with ExitStack() as p2e:
                wg = [sb(f"wg{i}", [128, 8, 512], BF16, p2e) for i in range(2)]
                wd = [sb(f"wd{i}", [128, 2, D], BF16, p2e) for i in range(2)]
                WG = [Buf(t[:]) for t in wg]
                WD = [Buf(t[:]) for t in wd]
                actT = [sb(f"actT{i}", [128, 2, SO], BF16, p2e) for i in range(2)]
                ACT_ = [[Buf(actT[i][:, :, nb * 512:(nb + 1) * 512]) for nb in range(4)] for i in range(2)]
                sgt = [sb(f"sgt{i}", [128, 256], F32, p2e) for i in range(2)]
                SGT = [Buf(t[:]) for t in sgt]
                att = [sb(f"att{i}", [128, 256], BF16, p2e) for i in range(2)]
                ATT = [Buf(t[:]) for t in att]
                NE = n_experts

                def load_w(e_):
                    sl_ = e_ % 2
                    gv = I["wgu"][e_].rearrange("(k p) c -> p k c", p=128)
                    kb.dma("gpsimd", wg[sl_][:, 0:4, :], gv[:, 0:4, :], outs=[WG[sl_]])
                    kb.dma("gpsimd", wg[sl_][:, 4:8, :], gv[:, 4:8, :], outs=[WG[sl_]])
                    kb.dma("gpsimd", wd[sl_][:], I["wdn"][e_].rearrange("(k p) c -> p k c", p=128), outs=[WD[sl_]])

                def load_wg(e_):
                    sl_ = e_ % 2
                    gv = I["wgu"][e_].rearrange("(k p) c -> p k c", p=128)
                    kb.dma("gpsimd", wg[sl_][:, 0:4, :], gv[:, 0:4, :], outs=[WG[sl_]])
                    kb.dma("gpsimd", wg[sl_][:, 4:8, :], gv[:, 4:8, :], outs=[WG[sl_]])

                def load_wd(e_):
                    sl_ = e_ % 2
                    kb.dma("gpsimd", wd[sl_][:], I["wdn"][e_].rearrange("(k p) c -> p k c", p=128), outs=[WD[sl_]])

                cnt = [0]
                pend = {}

                def GU(e_, tt):
                    sl_ = e_ % 2
                    bk = bank2()
                    for k in range(8):
                        kb.op("tensor", lambda e, k=k: e.matmul(ps[bk][:], lhsT=h2T[:, k, tt * 128:(tt + 1) * 128], rhs=wg[sl_][:, k, :], start=(k == 0), stop=(k == 7)),
                              outs=[PS[bk]], ins=[H2[tt // 4], WG[sl_]], mark=(k == 7))
                    cnt[0] += 1
                    i2 = cnt[0] % 2
                    kb.op("scalar", lambda e: e.activation(out=sgt[i2][:], in_=ps[bk][:, 0:256], func=AF.Silu), outs=[SGT[i2]], ins=[PS[bk]])
                    kb.op("vector", lambda e: e.scalar_tensor_tensor(out=att[i2][:], in0=ps[bk][:, 256:512], scalar=gw[:, tt, e_:e_ + 1], in1=sgt[i2][:],
                                                                     op0=ALU.mult, op1=ALU.mult), outs=[ATT[i2]], ins=[PS[bk], SGT[i2], GW])
                    pend[(e_, tt)] = i2

                def TR(e_, tt):
                    sl_ = e_ % 2
                    i2 = pend.pop((e_, tt))
                    for hc in range(2):
                        bt = bank2()
                        kb.op("tensor", lambda e, hc=hc, bt=bt: e.matmul(ps[bt][:, 0:128], lhsT=att[i2][:, hc * 128:(hc + 1) * 128], rhs=ident[:], start=True, stop=True),
                              outs=[PS[bt]], ins=[ATT[i2], CST])
                        kb.op("scalar", lambda e, hc=hc, bt=bt: e.copy(out=actT[sl_][:, hc, tt * 128:(tt + 1) * 128], in_=ps[bt][:, 0:128]),
                              outs=[ACT_[sl_][tt // 4]], ins=[PS[bt]])

                def DN(e_, nb):
                    sl_ = e_ % 2
                    sl = slice(nb * 512, (nb + 1) * 512)
                    for m in range(8):
                        bk = bank2()
                        for hc in range(2):
                            kb.op("tensor", lambda e, bk=bk, hc=hc, m=m: e.matmul(ps[bk][:], lhsT=wd[sl_][:, hc, m * 128:(m + 1) * 128], rhs=actT[sl_][:, hc, sl], start=(hc == 0), stop=(hc == 1)),
                                  outs=[PS[bk]], ins=[WD[sl_], ACT_[sl_][nb]], mark=(hc == 1))
                        kb.op("vector", lambda e, bk=bk, m=m: e.scalar_tensor_tensor(out=acc[:, m, sl], in0=ps[bk][:], scalar=g2c(m), in1=acc[:, m, sl], op0=ALU.mult, op1=ALU.add),
                              outs=[ACCB[nb]], ins=[PS[bk], ACCB[nb], MOD])

                load_wg(0)
                load_wd(0)
                for e_ in range(NE + 1):
                    if e_ + 1 < NE:
                        load_wg(e_ + 1)
                    for tt in range(16):
                        if e_ < NE:
                            GU(e_, tt)
                            if tt > 0:
                                TR(e_, tt - 1)
                        if e_ > 0 and tt % 4 == 3:
                            DN(e_ - 1, tt // 4)
                    if e_ < NE:
                        TR(e_, 15)
                    if e_ + 1 < NE:
                        load_wd(e_ + 1)
                kb.barrier()
            if "x3T" in dbg:
                dump("x3T", acc[:], [128, 8, SO])
            sq2 = sb("sq2", [128, 8, 512], F32, p2)
            SQ2 = Buf(sq2[:])
            rstd2 = sb("rstd2", [128, 512], F32, p2)
            RS2 = Buf(rstd2[:])
            ot = [sb(f"ot{i}", [128, 8, 512], F32, p2) for i in range(2)]
            OTB = [Buf(t[:]) for t in ot]
            outT_v = outT.rearrange("(k p) t -> p k t", p=128)
            for nb in range(4):
                sl = slice(nb * 512, (nb + 1) * 512)
                kb.op("scalar", lambda e, sl=sl: e.activation(out=sq2[:], in_=acc[:, :, sl], func=AF.Square), outs=[SQ2], ins=[ACCB[nb]])
                bk = bank2()
                for k in range(8):
                    kb.op("tensor", lambda e, k=k, bk=bk: e.matmul(ps[bk][:], lhsT=onesf[:], rhs=sq2[:, k, :], start=(k == 0), stop=(k == 7)),
                          outs=[PS[bk]], ins=[SQ2, CST], mark=(k == 7))
                kb.op("scalar", lambda e, bk=bk: e.activation(out=rstd2[:], in_=ps[bk][:], func=AF.Sqrt, scale=1.0 / D, bias=EPS), outs=[RS2], ins=[PS[bk]])
                kb.op("vector", lambda e: e.reciprocal(out=rstd2[:], in_=rstd2[:]), outs=[RS2], ins=[RS2])
                o_ = ot[nb % 2]
                for k in range(8):
                    kb.op("vector", lambda e, k=k, o_=o_, sl=sl: e.scalar_tensor_tensor(out=o_[:, k, :], in0=acc[:, k, sl], scalar=fg[:, k:k + 1], in1=rstd2[:], op0=ALU.mult, op1=ALU.mult),
                          outs=[OTB[nb % 2]], ins=[ACCB[nb], FG, RS2])
                kb.dma("sync", outT_v[:, :, sl], o_[:], ins=[OTB[nb % 2]])
            kb.barrier()
        kb.barrier()
    return nc, dbg_out


def _prep_inputs(inp, core):
    b = core // 2
    half = core % 2
    f = lambda a: np.ascontiguousarray(a, dtype=np.float32)
    x = inp["x"][b]
    m = {}
    xT = f(x.T)
    m["xT"] = xT
    m["xTo"] = f(xT[:, half * SO:(half + 1) * SO])
    m["cT"] = f(inp["c"][b].reshape(8, 128).T)
    m["w_ada"] = f(inp["w_ada"][0])
    m["b_ada"] = f(inp["b_ada"][0].reshape(1, -1))
    m["n1g"] = f(inp["norm1_g"][0].reshape(8, 128).T)
    m["w_in"] = f(inp["w_in"][0])
    m["lbl"] = f(inp["hg_lb_logits"].reshape(2, 4, 128).transpose(2, 0, 1))
    m["hng"] = f(np.broadcast_to(inp["hg_norm_g"][0][None, :], (128, 512)))
    for s in ("k", "v"):
        m["peT" + s] = f(inp["cmp_pos_" + s][0].T)
        m["w1" + s] = f(inp["cmp_w1_" + s][0].reshape(32, 64, 256).transpose(1, 0, 2))
        m["b1" + s] = f(inp["cmp_b1_" + s][0].reshape(2, 128).T)
        m["w2" + s] = f(inp["cmp_w2_" + s][0].reshape(2, 128, 64).transpose(1, 0, 2))
    m["w_out"] = f(inp["w_out"][0])
    m["n2g"] = f(inp["norm2_g"][0].reshape(8, 128).T)
    m["rw"] = f(inp["router_w"][0].reshape(8, 128, 64).transpose(1, 0, 2))
    m["rbias"] = f(np.broadcast_to(inp["router_bias"][0][None, :], (128, 64)))
    m["fg"] = f(inp["final_g"].reshape(8, 128).T)
    return m


_SHARED = {}


def kernel(**inp):
    inp = {k: np.asarray(v) for k, v in inp.items()}
    nc, _ = build()
    wgu = np.ascontiguousarray(np.concatenate([inp["w_exp_gu"][0], inp["w_sh_gu"][0][None]], axis=0), dtype=np.float32)
    wdn = np.ascontiguousarray(np.concatenate([inp["w_exp_dn"][0], inp["w_sh_dn"][0][None]], axis=0), dtype=np.float32)
    in_maps = []
    for core in range(8):
        m = _prep_inputs(inp, core)
        m["wgu"] = wgu
        m["wdn"] = wdn
        m.update(_consts(core % 2))
        in_maps.append(m)
    res = run_bass_kernel_spmd(nc, in_maps, core_ids=list(range(8)))
    out = np.zeros((4, S, D), np.float32)
    for core in range(8):
        b, half = core // 2, core % 2
        out[b, half * SO:(half + 1) * SO, :] = res.results[core]["outT"].T
    return out
```

```python
import numpy as np
import os as _os0
import ml_dtypes
from contextlib import ExitStack
import concourse.bass as bass
import concourse.mybir as mybir
from concourse.bass_utils import run_bass_kernel_spmd

F32 = mybir.dt.float32
BF16 = mybir.dt.bfloat16
AF = mybir.ActivationFunctionType
ALU = mybir.AluOpType
AX = mybir.AxisListType

S = 4096
D = 1024
NT = 32
NB = 8
SO = 2048
EPS = 1e-6
NEG = -30000.0
NDS = 12
SEM_LIMIT = 2000
SAME_SYNC = not bool(int(_os0.environ.get("NOSAME", "0")))


class Buf:
    __slots__ = ("ap", "w", "r", "excl")

    def __init__(self, ap, excl=False):
        self.ap = ap
        self.w = None
        self.r = {}
        self.excl = excl

    def __getitem__(self, k):
        return self.ap[k]


class Eng:
    def __init__(self, name, h):
        self.name = name
        self.h = h
        self.sem = None
        self.count = 0
        self.epoch = 0
        self.waited = {}


class KB:
    def __init__(self, nc, es):
        self.nc = nc
        self.es = es
        self.engs = {n: Eng(n, getattr(nc, n)) for n in ("tensor", "vector", "scalar", "gpsimd", "sync")}
        for e in self.engs.values():
            self._new_sem(e)
        self.dsems = {q: [es.enter_context(nc.semaphore(f"d_{q}{i}")) for i in range(NDS)] for q in ("sync", "gpsimd")}
        self.dcnt = {q: [0] * NDS for q in ("sync", "gpsimd")}
        self.drr = {"sync": 0, "gpsimd": 0}
        self.nsem = 0

    def _new_sem(self, e):
        e.epoch += 1
        e.sem = self.es.enter_context(self.nc.semaphore(f"s_{e.name}_{e.epoch}"))
        e.count = 0

    def wait(self, eng, tk):
        key, sem, val = tk
        if eng.waited.get(key, 0) >= val:
            return
        eng.h.wait_ge(sem, val)
        eng.waited[key] = val

    def _deps(self, en, eng, outs, ins):
        need = {}

        def add(t):
            if t[3] == en and (en == "tensor" or not SAME_SYNC):
                return
            cur = need.get(t[0])
            if cur is None or cur[2] < t[2]:
                need[t[0]] = t

        for b in ins:
            if b.w is not None:
                add(b.w)
            if b.excl:
                for t in b.r.values():
                    if t[3] != en:
                        add(t)
        for b in outs:
            if b.w is not None:
                add(b.w)
            for t in b.r.values():
                add(t)
        for t in need.values():
            self.wait(eng, t[:3])

    def op(self, en, fn, outs=(), ins=(), mark=True):
        eng = self.engs[en]
        self._deps(en, eng, outs, ins)
        if eng.count >= SEM_LIMIT:
            self._new_sem(eng)
        inst = fn(eng.h)
        if mark:
            eng.count += 1
            inst.then_inc(eng.sem, 1)
            tk = ((en, eng.epoch), eng.sem, eng.count, en)
        else:
            tk = ((en, eng.epoch), eng.sem, eng.count + 1, en)
        for b in ins:
            b.r[tk[0]] = tk
        for b in outs:
            b.w = tk
            b.r = {}
        return tk

    def dma(self, q, out_ap, in_ap, outs=(), ins=()):
        eng = self.engs[q]
        i = self.drr[q]
        self.drr[q] = (i + 1) % NDS
        sem = self.dsems[q][i]
        key = ("d", q, i)
        if self.dcnt[q][i] > 0:
            self.wait(eng, (key, sem, self.dcnt[q][i]))
        self._deps("dma_" + q, eng, outs, ins)
        inst = eng.h.dma_start(out=out_ap, in_=in_ap)
        self.dcnt[q][i] += 16
        inst.then_inc(sem, 16)
        tk = (key, sem, self.dcnt[q][i], "dma_" + q)
        for b in ins:
            b.r[key] = tk
        for b in outs:
            b.w = tk
            b.r = {}
        return tk

    def barrier(self):
        for e in self.engs.values():
            for o in self.engs.values():
                if o is e or o.count == 0:
                    continue
                self.wait(e, ((o.name, o.epoch), o.sem, o.count))
            for q in ("sync", "gpsimd"):
                for i in range(NDS):
                    if self.dcnt[q][i] > 0:
                        self.wait(e, (("d", q, i), self.dsems[q][i], self.dcnt[q][i]))


def _consts(half):
    bf = ml_dtypes.bfloat16
    c = {}
    eye = np.eye(128, dtype=np.float32)
    c["ident"] = eye.astype(bf)
    c["onesf"] = np.ones((128, 128), np.float32)
    c["isel0"] = (eye * (1.0 if half == 0 else 0.0)).astype(bf)
    c["isel1"] = (eye * (1.0 if half == 1 else 0.0)).astype(bf)
    m = np.arange(128)
    sw = (m // 64) * 64 + ((m % 64) + 32) % 64
    ps = np.zeros((128, 128), np.float32)
    ps[sw, m] = 1.0
    c["pswap"] = ps.astype(bf)
    dd = np.arange(128) % 64
    i = dd % 32
    inv = 10000.0 ** (-(i.astype(np.float64)) / 32.0)
    ang = inv[:, None].astype(np.float32).astype(np.float64) * np.arange(S)[None, :]
    ang = ang.astype(np.float32).astype(np.float64)
    c["cosT"] = np.cos(ang).astype(np.float32)
    sg = np.where(dd < 32, -1.0, 1.0)[:, None]
    c["sinT"] = (np.sin(ang) * sg).astype(np.float32)
    c["hmask"] = (m[:, None] <= m[None, :]).astype(np.float32).astype(bf)
    seg = np.ones((128, 512), np.float32)
    seg[:, ::128] = 0.0
    c["segm"] = seg
    r = np.arange(128)[:, None]
    qi = np.arange(512)[None, :]
    wb = np.zeros((8, 128, 512), np.float32)
    cb = np.zeros((4, 128, 512), np.float32)
    for j in range(8):
        kpos = -512 + 128 * j + r
        dlt = qi - kpos
        wb[j] = np.where((dlt >= 0) & (dlt < 512), 0.0, NEG)
    for j in range(4):
        kpos = 128 * j + r
        cb[j] = np.where(kpos <= qi, 0.0, NEG)
    c["wband"] = np.ascontiguousarray(wb.transpose(1, 0, 2)).astype(bf)
    c["causb"] = np.ascontiguousarray(cb.transpose(1, 0, 2)).astype(bf)
    cm = np.zeros((8, 128, 512), np.float32)
    for qb in range(8):
        ct = 0 if qb < 4 else 1
        cc = 128 * ct + r
        qpos = 512 * qb + qi
        cm[qb] = np.where((16 * cc + 31 <= qpos) & (cc < 255), 0.0, NEG)
    c["cmpb"] = np.ascontiguousarray(cm.transpose(1, 0, 2)).astype(bf)
    ek = np.zeros((64, 32, 128), np.float32)
    for kt in range(32):
        ek[2 * kt, kt, :64] = 1.0
        ek[2 * kt + 1, kt, 64:] = 1.0
    c["ekt"] = np.concatenate([ek, ek], axis=0).astype(bf)
    add = np.zeros((128, 32, 64), np.float32)
    for qt in range(32):
        pos = 128 * qt + np.arange(128)
        cur = pos // 64
        j = np.arange(64)[None, :]
        forced = (j == 0) | (j == cur[:, None]) | (j == cur[:, None] - 1)
        avail = j <= cur[:, None]
        add[:, qt, :] = np.where(forced, 1e30, np.where(avail, 0.0, -1e30))
    c["seladd"] = add
    cs = np.arange(256)[:, None] * 16
    ss = np.arange(64)[None, :] * 64
    ov = np.clip(np.minimum(cs + 32, ss + 64) - np.maximum(cs, ss), 0, None).astype(np.float32) / 32.0
    ov[255] = 0.0
    c["ovl"] = np.ascontiguousarray(ov.reshape(2, 128, 64).transpose(1, 0, 2)).astype(bf)
    return c


CONST_SHAPES = {
    "ident": ([128, 128], BF16), "onesf": ([128, 128], F32), "isel0": ([128, 128], BF16), "isel1": ([128, 128], BF16),
    "pswap": ([128, 128], BF16), "cosT": ([128, S], F32), "sinT": ([128, S], F32), "hmask": ([128, 128], BF16),
    "segm": ([128, 512], F32), "wband": ([128, 8, 512], BF16), "causb": ([128, 4, 512], BF16),
    "cmpb": ([128, 8, 512], BF16), "ekt": ([128, 32, 128], BF16), "seladd": ([128, 32, 64], F32),
    "ovl": ([128, 2, 64], BF16),
}

IN_SHAPES = {
    "xT": [D, S], "xTo": [D, SO], "cT": [128, 8], "w_ada": [D, 6 * D], "b_ada": [1, 6 * D], "n1g": [128, 8],
    "w_in": [D, 3352], "lbl": [128, 2, 4], "hng": [128, 512],
    "peTk": [64, 32], "w1k": [64, 32, 256], "b1k": [128, 2], "w2k": [128, 2, 64],
    "peTv": [64, 32], "w1v": [64, 32, 256], "b1v": [128, 2], "w2v": [128, 2, 64],
    "w_out": [D, D], "n2g": [128, 8], "rw": [128, 8, 64], "rbias": [128, 64],
    "wgu": [65, D, 512], "wdn": [65, 256, D], "fg": [128, 8],
}


class _SkipNSA(Exception):
    pass


class _NSAScope(ExitStack):
    def __exit__(self, et, ev, tb):
        super().__exit__(None, None, None)
        return et is _SkipNSA


def build(stop_after=None, dbg=(), with_moe=True, enable_nsa=True, n_experts=65):
    nc = bass.Bass("TRN2", target_bir_lowering=False)
    I = {}
    for k, shp in IN_SHAPES.items():
        if not with_moe and k in ("wgu", "wdn"):
            continue
        I[k] = nc.dram_tensor(k, list(shp), F32, kind="ExternalInput").ap()
    for k, (shp, dt) in CONST_SHAPES.items():
        I[k] = nc.dram_tensor(k, list(shp), dt, kind="ExternalInput").ap()
    outT = nc.dram_tensor("outT", [D, SO], F32, kind="ExternalOutput").ap()
    dbg_out = {}
    with ExitStack() as es:
        kb = KB(nc, es)
        E = es.enter_context

        uid = [0]

        def sb(name, shape, dt=F32, stack=None):
            uid[0] += 1
            return (stack or es).enter_context(nc.sbuf_tensor(f"sb{uid[0]}_" + name, list(shape), dt))

        ps = [E(nc.psum_tensor(f"ps{i}", [128, 512], F32)) for i in range(8)]
        PS = [Buf(p[:], excl=True) for p in ps]

        def dump(name, ap, shape, dt=F32):
            t = nc.dram_tensor("dbg_" + name, list(shape), dt, kind="ExternalOutput").ap()
            dbg_out[name] = t
            kb.barrier()
            kb.dma("sync", t, ap)
            kb.barrier()

        ident = sb("ident", [128, 128], BF16)
        onesf = sb("onesf", [128, 128], F32)
        isel0 = sb("isel0", [128, 128], BF16)
        isel1 = sb("isel1", [128, 128], BF16)
        pswap = sb("pswap", [128, 128], BF16)
        hmask = sb("hmask", [128, 128], BF16)
        segm = sb("segm", [128, 512], F32)
        CST = Buf(ident[:])
        for nm, t in (("ident", ident), ("onesf", onesf), ("isel0", isel0), ("isel1", isel1), ("pswap", pswap),
                      ("hmask", hmask), ("segm", segm)):
            kb.dma("sync", t[:], I[nm], outs=[CST])
        modcol = sb("modcol", [128, 48], F32)
        a1 = sb("a1", [128, 8], F32)
        a2 = sb("a2", [128, 8], F32)
        MOD = Buf(modcol[:])
        oT = sb("oT", [128, 8, SO], BF16)
        OT = [[Buf(oT[:, j, s * 128:(s + 1) * 128]) for s in range(16)] for j in range(8)]

        with ExitStack() as p0:
            cT = sb("cT", [128, 8], F32, p0)
            cs = sb("cs", [128, 8], F32, p0)
            bada = sb("bada", [1, 6 * D], F32, p0)
            modrow = sb("modrow", [1, 6 * D], F32, p0)
            one1 = sb("one1", [1, 1], F32, p0)
            n1g = sb("n1g", [128, 8], F32, p0)
            n2g = sb("n2g", [128, 8], F32, p0)
            wab = [sb(f"wab{i}", [128, 8, 512], F32, p0) for i in range(2)]
            WAB = [Buf(w[:]) for w in wab]
            SM = Buf(cT[:])
            MR = Buf(modrow[:])
            kb.dma("sync", cT[:], I["cT"], outs=[SM])
            kb.dma("sync", bada[:], I["b_ada"], outs=[SM])
            kb.dma("sync", n1g[:], I["n1g"], outs=[SM])
            kb.dma("sync", n2g[:], I["n2g"], outs=[SM])
            kb.op("vector", lambda e: e.memset(one1[:], 1.0), outs=[SM])
            kb.op("scalar", lambda e: e.activation(out=cs[:], in_=cT[:], func=AF.Silu), outs=[SM], ins=[SM])
            wada_v = I["w_ada"].rearrange("(k p) c -> p k c", p=128)
            for cb in range(12):
                W = WAB[cb % 2]
                kb.dma("sync" if cb % 2 == 0 else "gpsimd", wab[cb % 2][:], wada_v[:, :, cb * 512:(cb + 1) * 512], outs=[W])
                P = PS[cb % 2]
                for k in range(8):
                    kb.op("tensor", lambda e, k=k, cb=cb: e.matmul(ps[cb % 2][0:1, :], lhsT=cs[:, k:k + 1], rhs=wab[cb % 2][:, k, :],
                                                                 start=(k == 0), stop=(k == 7)),
                          outs=[P], ins=[SM, W], mark=(k == 7))
                kb.op("vector", lambda e, cb=cb: e.tensor_tensor(out=modrow[0:1, cb * 512:(cb + 1) * 512], in0=ps[cb % 2][0:1, :],
                                                                  in1=bada[0:1, cb * 512:(cb + 1) * 512], op=ALU.add),
                      outs=[MR], ins=[P, SM])
            P = PS[2]
            for j in range(48):
                kb.op("tensor", lambda e, j=j: e.matmul(ps[2][:, j:j + 1], lhsT=modrow[0:1, j * 128:(j + 1) * 128], rhs=one1[0:1, 0:1],
                                                       start=True, stop=True), outs=[P], ins=[MR, SM], mark=(j == 47))
            kb.op("vector", lambda e: e.tensor_copy(out=modcol[:], in_=ps[2][:, 0:48]), outs=[MOD], ins=[P])
            kb.op("vector", lambda e: e.scalar_tensor_tensor(out=a1[:], in0=modcol[:, 8:16], scalar=1.0, in1=n1g[:], op0=ALU.add, op1=ALU.mult),
                  outs=[MOD], ins=[MOD, SM])
            kb.op("vector", lambda e: e.scalar_tensor_tensor(out=a2[:], in0=modcol[:, 32:40], scalar=1.0, in1=n2g[:], op0=ALU.add, op1=ALU.mult),
                  outs=[MOD], ins=[MOD, SM])
            if "mod" in dbg:
                dump("mod", modcol[:], [128, 48])
            kb.barrier()
        sh1 = lambda k: modcol[:, k:k + 1]
        g1c = lambda k: modcol[:, 16 + k:17 + k]
        sh2 = lambda k: modcol[:, 24 + k:25 + k]
        g2c = lambda k: modcol[:, 40 + k:41 + k]

        if stop_after == "p0":
            kb.barrier()
            return nc, dbg_out

        with ExitStack() as p1:
            hT = sb("hT", [128, 8, S], BF16, p1)
            HT = [Buf(hT[:, :, n * 512:(n + 1) * 512]) for n in range(NB)]
            with ExitStack() as p1a:
                xb = [sb(f"xb{i}", [128, 8, 512], F32, p1a) for i in range(2)]
                XB = [Buf(t[:]) for t in xb]
                sq = sb("sq", [128, 8, 512], F32, p1a)
                SQ = Buf(sq[:])
                rstd = sb("rstd", [128, 512], F32, p1a)
                RS = Buf(rstd[:])
                tmp = [sb(f"tmp{i}", [128, 512], F32, p1a) for i in range(2)]
                TMP = [Buf(t[:]) for t in tmp]
                xT_v = I["xT"].rearrange("(k p) t -> p k t", p=128)
                for n in range(NB):
                    X = XB[n % 2]
                    x_ = xb[n % 2]
                    kb.dma("sync" if n % 2 == 0 else "gpsimd", x_[:], xT_v[:, :, n * 512:(n + 1) * 512], outs=[X])
                    kb.op("scalar", lambda e, x_=x_: e.activation(out=sq[:], in_=x_[:], func=AF.Square), outs=[SQ], ins=[X])
                    P = PS[n % 2]
                    for k in range(8):
                        kb.op("tensor", lambda e, k=k, n=n: e.matmul(ps[n % 2][:], lhsT=onesf[:], rhs=sq[:, k, :], start=(k == 0), stop=(k == 7)),
                              outs=[P], ins=[SQ, CST], mark=(k == 7))
                    kb.op("scalar", lambda e, n=n: e.activation(out=rstd[:], in_=ps[n % 2][:], func=AF.Sqrt, scale=1.0 / D, bias=EPS),
                          outs=[RS], ins=[P])
                    kb.op("vector", lambda e: e.reciprocal(out=rstd[:], in_=rstd[:]), outs=[RS], ins=[RS])
                    for k in range(8):
                        T = TMP[k % 2]
                        t_ = tmp[k % 2]
                        kb.op("vector", lambda e, k=k, t_=t_, x_=x_: e.tensor_tensor(out=t_[:], in0=x_[:, k, :], in1=rstd[:], op=ALU.mult),
                              outs=[T], ins=[X, RS])
                        kb.op("scalar", lambda e, k=k, t_=t_, n=n: e.activation(out=hT[:, k, n * 512:(n + 1) * 512], in_=t_[:], func=AF.Identity,
                                                                            scale=a1[:, k:k + 1], bias=sh1(k)),
                              outs=[HT[n]], ins=[T, MOD])
                kb.barrier()
            if "hT" in dbg:
                dump("hT", hT[:], [128, 8, S], BF16)
            if stop_after == "p1a":
                kb.barrier()
                return nc, dbg_out

            rr = [0]

            def bank():
                rr[0] = (rr[0] + 1) % 8
                return rr[0]

            w_in_v = I["w_in"].rearrange("(k p) c -> p k c", p=128)

            with ExitStack() as ph:
                lbl = sb("lbl", [128, 2, 4], F32, ph)
                lb = sb("lb", [128, 4], F32, ph)
                oml = sb("oml", [128, 4], F32, ph)
                hng = sb("hng", [128, 512], F32, ph)
                HC = Buf(lbl[:])
                kb.dma("sync", lbl[:], I["lbl"], outs=[HC])
                kb.dma("sync", hng[:], I["hng"], outs=[HC])
                kb.op("vector", lambda e: e.tensor_tensor(out=lb[:], in0=lbl[:, 0, :], in1=lbl[:, 1, :], op=ALU.subtract), outs=[HC], ins=[HC])
                kb.op("scalar", lambda e: e.activation(out=lb[:], in_=lb[:], func=AF.Sigmoid), outs=[HC], ins=[HC])
                kb.op("vector", lambda e: e.tensor_scalar(out=oml[:], in0=lb[:], scalar1=-1.0, scalar2=1.0, op0=ALU.mult, op1=ALU.add), outs=[HC], ins=[HC])
                wq = sb("wq", [128, 8, 128], BF16, ph)
                wf = sb("wf", [128, 8, 128], BF16, ph)
                wig = sb("wig", [128, 8, 256], BF16, ph)
                WQ, WF, WIG = Buf(wq[:]), Buf(wf[:]), Buf(wig[:])
                Q1 = sb("Q1", [128, S], BF16, ph)
                Q2 = sb("Q2", [128, S], BF16, ph)
                Kt = sb("Kt", [128, S], BF16, ph)
                Kh = sb("Kh", [128, NT, 128], BF16, ph)
                Vh = sb("Vh", [128, NT, 128], BF16, ph)
                SGt = sb("SGt", [128, NT, 128], BF16, ph)
                ebl = sb("ebl", [128, NT], F32, ph)
                BQ = [Buf(Q1[:, n * 512:(n + 1) * 512]) for n in range(NB)]
                BKH = [Buf(Kh[:, t, :]) for t in range(NT)]
                BV = [Buf(Vh[:, t, :]) for t in range(NT)]
                tn = ["f", "lf", "b", "d1", "d2", "eb", "e1", "en1", "el", "k"]
                T_ = {n_: sb("t_" + n_, [128, 512], F32, ph) for n_ in tn}
                TB = {n_: Buf(T_[n_][:]) for n_ in tn}
                khtb = sb("khtb", [128, 512], BF16, ph)
                KHTB = Buf(khtb[:])
                Sst = sb("Sst", [128, 128], F32, ph)
                SST = Buf(Sst[:])
                sbf = [sb(f"sbf{i}", [128, 128], BF16, ph) for i in range(2)]
                SBF = [Buf(t[:]) for t in sbf]
                atm = [sb(f"atm{i}", [128, 128], BF16, ph) for i in range(2)]
                ATM = [Buf(t[:]) for t in atm]
                for i in range(2):
                    kb.op("vector", lambda e, i=i: e.memset(atm[i][:], 0.0), outs=[ATM[i]])
                junk = sb("junk", [128, 128], F32, ph)
                JK = Buf(junk[:])
                ssq = [sb(f"ssq{i}", [128, 1], F32, ph) for i in range(2)]
                SSQ = [Buf(t[:]) for t in ssq]
                of = [sb(f"of{i}", [128, 128], F32, ph) for i in range(2)]
                OF = [Buf(t[:]) for t in of]
                obf = [sb(f"obf{i}", [128, 128], BF16, ph) for i in range(2)]
                OBF = [Buf(t[:]) for t in obf]
                v4 = lambda ap: ap.rearrange("p (c t) -> p c t", t=128)
                for hd in range(int(_os0.environ.get("NHEADS", "4"))):
                    c0 = hd * 128
                    kb.dma("gpsimd", wq[:], w_in_v[:, :, c0:c0 + 128], outs=[WQ])
                    kb.dma("gpsimd", wf[:], w_in_v[:, :, 512 + c0:512 + c0 + 128], outs=[WF])
                    kb.dma("gpsimd", wig[:, :, 0:128], w_in_v[:, :, 1024 + c0:1024 + c0 + 128], outs=[WIG])
                    kb.dma("gpsimd", wig[:, :, 128:256], w_in_v[:, :, 1536 + c0:1536 + c0 + 128], outs=[WIG])
                    for n in range(NB):
                        sl = slice(n * 512, (n + 1) * 512)
                        bq_, bf_ = bank(), bank()
                        for k in range(8):
                            kb.op("tensor", lambda e, k=k, bq_=bq_, sl=sl: e.matmul(ps[bq_][:], lhsT=wq[:, k, :], rhs=hT[:, k, sl], start=(k == 0), stop=(k == 7)),
                                  outs=[PS[bq_]], ins=[WQ, HT[n]], mark=(k == 7))
                        for k in range(8):
                            kb.op("tensor", lambda e, k=k, bf_=bf_, sl=sl: e.matmul(ps[bf_][:], lhsT=wf[:, k, :], rhs=hT[:, k, sl], start=(k == 0), stop=(k == 7)),
                                  outs=[PS[bf_]], ins=[WF, HT[n]], mark=(k == 7))
                        t = T_
                        kb.op("scalar", lambda e, bf_=bf_: e.activation(out=t["f"][:], in_=ps[bf_][:], func=AF.Sigmoid), outs=[TB["f"]], ins=[PS[bf_]])
                        kb.op("vector", lambda e, hd=hd: e.tensor_scalar(out=t["f"][:], in0=t["f"][:], scalar1=oml[:, hd:hd + 1], scalar2=lb[:, hd:hd + 1],
                                                                     op0=ALU.mult, op1=ALU.add), outs=[TB["f"]], ins=[TB["f"], HC])
                        kb.op("scalar", lambda e: e.activation(out=t["lf"][:], in_=t["f"][:], func=AF.Ln), outs=[TB["lf"]], ins=[TB["f"]])
                        kb.op("gpsimd", lambda e: e.tensor_scalar(out=t["k"][:], in0=t["f"][:], scalar1=-1.0, scalar2=1.0, op0=ALU.mult, op1=ALU.add),
                              outs=[TB["k"]], ins=[TB["f"]])
                        kb.op("vector", lambda e: e.tensor_tensor_scan(out=t["b"][:], data0=segm[:], data1=t["lf"][:], initial=0.0, op0=ALU.mult, op1=ALU.add),
                              outs=[TB["b"]], ins=[TB["lf"], CST])
                        kb.op("vector", lambda e: e.tensor_tensor(out=v4(t["d1"][:]), in0=v4(t["b"][:]), in1=v4(t["b"][:])[:, :, 63:64].to_broadcast([128, 4, 128]),
                                                                  op=ALU.subtract), outs=[TB["d1"]], ins=[TB["b"]])
                        kb.op("vector", lambda e: e.tensor_tensor(out=v4(t["d2"][:]), in0=v4(t["b"][:])[:, :, 127:128].to_broadcast([128, 4, 128]), in1=v4(t["b"][:]),
                                                                  op=ALU.subtract), outs=[TB["d2"]], ins=[TB["b"]])
                        kb.op("scalar", lambda e: e.activation(out=t["eb"][:], in_=t["b"][:], func=AF.Exp), outs=[TB["eb"]], ins=[TB["b"]])
                        kb.op("scalar", lambda e: e.activation(out=t["e1"][:], in_=t["d1"][:], func=AF.Exp), outs=[TB["e1"]], ins=[TB["d1"]])
                        kb.op("scalar", lambda e: e.activation(out=t["en1"][:], in_=t["d1"][:], func=AF.Exp, scale=-1.0), outs=[TB["en1"]], ins=[TB["d1"]])
                        kb.op("scalar", lambda e: e.activation(out=t["el"][:], in_=t["d2"][:], func=AF.Exp), outs=[TB["el"]], ins=[TB["d2"]])
                        sc_ = 128.0 ** -0.5
                        kb.op("vector", lambda e, bq_=bq_, sl=sl: e.scalar_tensor_tensor(out=Q1[:, sl], in0=ps[bq_][:], scalar=sc_, in1=t["e1"][:], op0=ALU.mult, op1=ALU.mult),
                              outs=[BQ[n]], ins=[PS[bq_], TB["e1"]])
                        kb.op("vector", lambda e, bq_=bq_, sl=sl: e.scalar_tensor_tensor(out=Q2[:, sl], in0=ps[bq_][:], scalar=sc_, in1=t["eb"][:], op0=ALU.mult, op1=ALU.mult),
                              outs=[BQ[n]], ins=[PS[bq_], TB["eb"]])
                        kb.op("gpsimd", lambda e, sl=sl: e.tensor_tensor(out=Kt[:, sl], in0=t["k"][:], in1=t["en1"][:], op=ALU.mult), outs=[BQ[n]], ins=[TB["k"], TB["en1"]])
                        kb.op("gpsimd", lambda e: e.tensor_tensor(out=khtb[:], in0=t["k"][:], in1=t["el"][:], op=ALU.mult), outs=[KHTB], ins=[TB["k"], TB["el"]])
                        kb.op("gpsimd", lambda e, n=n: e.tensor_copy(out=ebl[:, 4 * n:4 * n + 4], in_=v4(t["eb"][:])[:, :, 127]), outs=[BQ[n]], ins=[TB["eb"]])
                        for i in range(4):
                            bk = bank()
                            kb.op("tensor", lambda e, i=i, bk=bk: e.matmul(ps[bk][:, 0:128], lhsT=khtb[:, i * 128:(i + 1) * 128], rhs=ident[:], start=True, stop=True),
                                  outs=[PS[bk]], ins=[KHTB, CST])
                            kb.op("scalar", lambda e, i=i, bk=bk, n=n: e.copy(out=Kh[:, 4 * n + i, :], in_=ps[bk][:, 0:128]), outs=[BKH[4 * n + i]], ins=[PS[bk]])
                    for tt in range(NT):
                        bk = bank()
                        n = tt // 4
                        for k in range(8):
                            kb.op("tensor", lambda e, k=k, bk=bk, tt=tt: e.matmul(ps[bk][:, 0:256], lhsT=hT[:, k, tt * 128:(tt + 1) * 128], rhs=wig[:, k, :],
                                                                                 start=(k == 0), stop=(k == 7)),
                                  outs=[PS[bk]], ins=[WIG, HT[n]], mark=(k == 7))
                        kb.op("vector", lambda e, bk=bk, tt=tt: e.tensor_copy(out=Vh[:, tt, :], in_=ps[bk][:, 0:128]), outs=[BV[tt]], ins=[PS[bk]])
                        kb.op("scalar", lambda e, bk=bk, tt=tt: e.activation(out=SGt[:, tt, :], in_=ps[bk][:, 128:256], func=AF.Silu), outs=[BV[tt]], ins=[PS[bk]])
                    kb.op("vector", lambda e: e.memset(Sst[:], 0.0), outs=[SST])
                    at_bank = {}

                    def emit_at(c):
                        bk = bank()
                        at_bank[c] = bk
                        cs_ = slice(c * 128, (c + 1) * 128)
                        c0_ = c * 128
                        kb.op("tensor", lambda e: e.matmul(ps[bk][0:64, 0:64], lhsT=Kt[:, c0_:c0_ + 64], rhs=Q1[:, c0_:c0_ + 64], start=True, stop=True),
                              outs=[PS[bk]], ins=[BQ[c // 4]], mark=False)
                        kb.op("tensor", lambda e: e.matmul(ps[bk][:, 64:128], lhsT=Kt[:, cs_], rhs=Q1[:, c0_ + 64:c0_ + 128], start=True, stop=True),
                              outs=[PS[bk]], ins=[BQ[c // 4]])
                        kb.op("vector", lambda e: e.tensor_tensor(out=atm[c % 2][0:64, 0:64], in0=ps[bk][0:64, 0:64], in1=hmask[0:64, 0:64], op=ALU.mult),
                              outs=[ATM[c % 2]], ins=[PS[bk], CST])
                        kb.op("vector", lambda e: e.tensor_tensor(out=atm[c % 2][:, 64:128], in0=ps[bk][:, 64:128], in1=hmask[:, 64:128], op=ALU.mult),
                              outs=[ATM[c % 2]], ins=[PS[bk], CST])

                    emit_at(0)
                    for c in range(NT):
                        if c + 1 < NT:
                            emit_at(c + 1)
                        cs_ = slice(c * 128, (c + 1) * 128)
                        bd, bo = bank(), bank()
                        kb.op("tensor", lambda e, bd=bd, c=c: e.matmul(ps[bd][:, 0:128], lhsT=Kh[:, c, :], rhs=Vh[:, c, :], start=True, stop=True),
                              outs=[PS[bd]], ins=[BKH[c], BV[c]])
                        kb.op("tensor", lambda e, bo=bo, c=c: e.matmul(ps[bo][:, 0:128], lhsT=atm[c % 2][:], rhs=Vh[:, c, :], start=True, stop=(c == 0)),
                              outs=[PS[bo]], ins=[ATM[c % 2], BV[c]], mark=(c == 0))
                        if c > 0:
                            kb.op("tensor", lambda e, bo=bo, c=c, cs_=cs_: e.matmul(ps[bo][:, 0:128], lhsT=Q2[:, cs_], rhs=sbf[(c - 1) % 2][:], start=False, stop=True),
                                  outs=[PS[bo]], ins=[BQ[c // 4], SBF[(c - 1) % 2]])
                        if c + 1 < NT:
                            kb.op("vector", lambda e, bd=bd, c=c: e.scalar_tensor_tensor(out=Sst[:], in0=Sst[:], scalar=ebl[:, c:c + 1], in1=ps[bd][:, 0:128],
                                                                                     op0=ALU.mult, op1=ALU.add), outs=[SST], ins=[SST, PS[bd], BQ[c // 4]])
                            kb.op("scalar", lambda e, c=c: e.copy(out=sbf[c % 2][:], in_=Sst[:]), outs=[SBF[c % 2]], ins=[SST])
                        i2 = c % 2
                        kb.op("gpsimd", lambda e, i2=i2: e.memset(ssq[i2][:], 0.0), outs=[SSQ[i2]])
                        kb.op("scalar", lambda e, bo=bo, i2=i2: e.activation(out=junk[:], in_=ps[bo][:, 0:128], func=AF.Square, accum_out=ssq[i2][:]),
                              outs=[JK, SSQ[i2]], ins=[PS[bo]])
                        kb.op("scalar", lambda e, i2=i2: e.activation(out=ssq[i2][:], in_=ssq[i2][:], func=AF.Sqrt, scale=1.0 / 128, bias=EPS), outs=[SSQ[i2]], ins=[SSQ[i2]])
                        kb.op("vector", lambda e, i2=i2: e.reciprocal(out=ssq[i2][:], in_=ssq[i2][:]), outs=[SSQ[i2]], ins=[SSQ[i2]])
                        kb.op("vector", lambda e, bo=bo, i2=i2, c0=c0: e.scalar_tensor_tensor(out=of[i2][:], in0=ps[bo][:, 0:128], scalar=ssq[i2][:, 0:1], in1=hng[:, c0:c0 + 128],
                                                                                           op0=ALU.mult, op1=ALU.mult), outs=[OF[i2]], ins=[PS[bo], SSQ[i2], HC])
                        kb.op("gpsimd", lambda e, i2=i2, c=c: e.tensor_tensor(out=obf[i2][:], in0=of[i2][:], in1=SGt[:, c, :], op=ALU.mult), outs=[OBF[i2]], ins=[OF[i2], BV[c]])
                        bt = bank()
                        isel = isel0 if c < 16 else isel1
                        kb.op("tensor", lambda e, bt=bt, i2=i2, isel=isel: e.matmul(ps[bt][:, 0:128], lhsT=obf[i2][:], rhs=isel[:], start=True, stop=True),
                              outs=[PS[bt]], ins=[OBF[i2], CST])
                        s_ = c % 16
                        if c < 16:
                            kb.op("scalar", lambda e, bt=bt, s_=s_, hd=hd: e.copy(out=oT[:, hd, s_ * 128:(s_ + 1) * 128], in_=ps[bt][:, 0:128]), outs=[OT[hd][s_]], ins=[PS[bt]])
                        else:
                            kb.op("vector", lambda e, bt=bt, s_=s_, hd=hd: e.tensor_tensor(out=oT[:, hd, s_ * 128:(s_ + 1) * 128], in0=ps[bt][:, 0:128],
                                                                                       in1=oT[:, hd, s_ * 128:(s_ + 1) * 128], op=ALU.add),
                                  outs=[OT[hd][s_]], ins=[PS[bt], OT[hd][s_]])
                kb.barrier()
            if "oT" in dbg:
                dump("oT", oT[:], [128, 8, SO], BF16)
            if stop_after == "p1b":
                kb.barrier()
                return nc, dbg_out

            SCL = 64.0 ** -0.5
            if not enable_nsa:
                for jf in range(4, 8):
                    kb.op("vector", lambda e, jf=jf: e.memset(oT[:, jf, :], 0.0), outs=OT[jf])
            with _NSAScope() as pn:
                if not enable_nsa:
                    raise _SkipNSA()
                ovl = sb("ovl", [128, 2, 64], BF16, pn)
                kb.dma("sync", ovl[:], I["ovl"], outs=[CST])
                KEe = sb("KEe", [128, S], BF16, pn)
                KEo = sb("KEo", [128, S], BF16, pn)
                ekt_v = I["ekt"].rearrange("p a b -> p (a b)")
                kb.dma("sync", KEe[64:128, :], ekt_v[64:128, :], outs=[CST])
                kb.dma("sync", KEo[0:64, :], ekt_v[0:64, :], outs=[CST])
                kwT = sb("kwT", [128, S], BF16, pn)
                kcvT = sb("kcvT", [128, S], BF16, pn)
                vs1 = sb("vs1", [128, NT, 80], BF16, pn)
                vw1 = sb("vw1", [128, NT, 80], BF16, pn)
                KS = Buf(kwT[:])
                kcmpT = sb("kcmpT", [128, 256], BF16, pn)
                vcmp1 = sb("vcmp1", [128, 2, 144], BF16, pn)
                KC = Buf(kcmpT[:])
                wk3 = sb("wk3", [128, 8, 384], BF16, pn)
                wv2 = sb("wv2", [128, 8, 128], BF16, pn)
                wqg = sb("wqg", [128, 8, 256], BF16, pn)
                wgt = sb("wgt", [128, 8, 12], BF16, pn)
                WN = Buf(wk3[:])
                cosb = sb("cosb", [128, 512], F32, pn)
                sinb = sb("sinb", [128, 512], F32, pn)
                CSB = Buf(cosb[:])
                rawb = sb("rawb", [128, 512], BF16, pn)
                RAWB = Buf(rawb[:])
                rt1 = sb("rt1", [128, 512], F32, pn)
                rt2 = sb("rt2", [128, 512], F32, pn)
                RT1, RT2 = Buf(rt1[:]), Buf(rt2[:])
                _padn = int(_os0.environ.get("PADN", "0"))
                if _padn:
                    _pad = sb("padn", [128, _padn], F32, pn)
                srr = [0]

                def sbank():
                    srr[0] = (srr[0] + 1) % 3
                    return srr[0]

                mrr = [0]

                def mbank():
                    return 7

                import os as _os
                _dbgmode = int(_os.environ.get("ROPEDBG", "0"))

                def rope_from(bk, dst_ap, dstbuf):
                    if _dbgmode == 1:
                        kb.op("scalar", lambda e: e.copy(out=dst_ap, in_=ps[bk][:]), outs=[dstbuf], ins=[PS[bk]])
                        return
                    if _dbgmode == 3:
                        kb.op("vector", lambda e: e.tensor_tensor(out=rt1[:], in0=ps[bk][:], in1=cosb[:], op=ALU.mult), outs=[RT1], ins=[PS[bk], CSB])
                        kb.op("gpsimd", lambda e: e.tensor_copy(out=dst_ap, in_=rt1[:]), outs=[dstbuf], ins=[RT1])
                        return
                    if _dbgmode == 4:
                        kb.op("scalar", lambda e: e.copy(out=rawb[:], in_=ps[bk][:]), outs=[RAWB], ins=[PS[bk]])
                        b2 = mbank()
                        kb.op("tensor", lambda e: e.matmul(ps[b2][:], lhsT=pswap[:], rhs=rawb[:], start=True, stop=True), outs=[PS[b2]], ins=[RAWB, CST])
                        kb.op("vector", lambda e: e.tensor_tensor(out=rt1[:], in0=ps[bk][:], in1=cosb[:], op=ALU.mult), outs=[RT1], ins=[PS[bk], CSB])
                        kb.op("vector", lambda e: e.tensor_tensor(out=rt2[:], in0=ps[b2][:], in1=sinb[:], op=ALU.mult), outs=[RT2], ins=[PS[b2], CSB])
                        kb.op("vector", lambda e: e.tensor_tensor(out=dst_ap, in0=rt1[:], in1=rt2[:], op=ALU.add), outs=[dstbuf], ins=[RT1, RT2])
                        return
                    if _dbgmode == 5:
                        kb.op("scalar", lambda e: e.copy(out=rawb[:], in_=ps[bk][:]), outs=[RAWB], ins=[PS[bk]])
                        b2 = mbank()
                        kb.op("tensor", lambda e: e.matmul(ps[b2][:], lhsT=pswap[:], rhs=rawb[:], start=True, stop=True), outs=[PS[b2]], ins=[RAWB, CST])
                        kb.op("vector", lambda e: e.tensor_tensor(out=rt1[:], in0=ps[bk][:], in1=cosb[:], op=ALU.mult), outs=[RT1], ins=[PS[bk], CSB])
                        kb.op("scalar", lambda e: e.copy(out=rt2[:], in_=ps[b2][:]), outs=[RT2], ins=[PS[b2]])
                        _sb = cosb if _os.environ.get("USECOS") else sinb
                        kb.op("vector", lambda e: e.tensor_tensor(out=rt2[:], in0=rt2[:], in1=_sb[:], op=ALU.mult), outs=[RT2], ins=[RT2, CSB])
                        kb.op("vector", lambda e: e.tensor_tensor(out=dst_ap, in0=rt1[:], in1=rt2[:], op=ALU.add), outs=[dstbuf], ins=[RT1, RT2])
                        return
                    if _dbgmode in (7, 8):
                        kb.op("scalar", lambda e: e.copy(out=rawb[:], in_=ps[bk][:]), outs=[RAWB], ins=[PS[bk]])
                        b2 = mbank()
                        kb.op("tensor", lambda e: e.matmul(ps[b2][:], lhsT=pswap[:], rhs=rawb[:], start=True, stop=True), outs=[PS[b2]], ins=[RAWB, CST])
                        kb.op("vector", lambda e: e.tensor_tensor(out=rt1[:], in0=ps[bk][:], in1=cosb[:], op=ALU.mult), outs=[RT1], ins=[PS[bk], CSB])
                        kb.op("scalar", lambda e: e.copy(out=rt2[:], in_=ps[b2][:]), outs=[RT2], ins=[PS[b2]])
                        kb.op("vector", lambda e: e.tensor_tensor(out=rt2[:], in0=rt2[:], in1=sinb[:], op=ALU.mult), outs=[RT2], ins=[RT2, CSB])
                        if _dbgmode == 8:
                            kb.op("vector", lambda e: e.tensor_tensor(out=rt1[:], in0=rt1[:], in1=rt2[:], op=ALU.add), outs=[RT1], ins=[RT1, RT2])
                        kb.op("scalar", lambda e: e.copy(out=dst_ap, in_=rt1[:]), outs=[dstbuf], ins=[RT1])
                        return
                    if _dbgmode in (9, 10):
                        kb.op("vector", lambda e: e.tensor_tensor(out=rt1[:], in0=ps[bk][:], in1=cosb[:], op=ALU.mult), outs=[RT1], ins=[PS[bk], CSB])
                        if _dbgmode == 9:
                            kb.op("scalar", lambda e: e.copy(out=rt2[:], in_=ps[bk][:]), outs=[RT2], ins=[PS[bk]])
                        else:
                            kb.op("vector", lambda e: e.tensor_tensor(out=rt2[:], in0=rt1[:], in1=cosb[:], op=ALU.mult), outs=[RT2], ins=[RT1, CSB])
                        kb.op("gpsimd", lambda e: e.tensor_copy(out=dst_ap, in_=rt1[:]), outs=[dstbuf], ins=[RT1])
                        return
                    if _dbgmode in (11, 12):
                        kb.op("scalar", lambda e: e.copy(out=rawb[:], in_=ps[bk][:]), outs=[RAWB], ins=[PS[bk]])
                        b2 = mbank()
                        kb.op("tensor", lambda e: e.matmul(ps[b2][:], lhsT=pswap[:], rhs=rawb[:], start=True, stop=True), outs=[PS[b2]], ins=[RAWB, CST])
                        kb.op("vector", lambda e: e.tensor_tensor(out=rt1[:], in0=ps[bk][:], in1=cosb[:], op=ALU.mult), outs=[RT1], ins=[PS[bk], CSB, RAWB])
                        kb.op("gpsimd", lambda e: e.tensor_copy(out=dst_ap, in_=rt1[:]), outs=[dstbuf], ins=[RT1])
                        if _dbgmode == 12:
                            return
                        kb.op("vector", lambda e: e.tensor_tensor(out=rt1[:], in0=ps[b2][:], in1=sinb[:], op=ALU.mult), outs=[RT1], ins=[PS[b2], CSB])
                        kb.op("gpsimd", lambda e: e.tensor_tensor(out=dst_ap, in0=dst_ap, in1=rt1[:], op=ALU.add), outs=[dstbuf], ins=[RT1, dstbuf])
                        return
                    if _dbgmode == 2:
                        kb.op("scalar", lambda e: e.copy(out=rawb[:], in_=ps[bk][:]), outs=[RAWB], ins=[PS[bk]])
                        b2 = mbank()
                        kb.op("tensor", lambda e: e.matmul(ps[b2][:], lhsT=pswap[:], rhs=rawb[:], start=True, stop=True), outs=[PS[b2]], ins=[RAWB, CST])
                        kb.op("scalar", lambda e: e.copy(out=dst_ap, in_=ps[b2][:]), outs=[dstbuf], ins=[PS[b2]])
                        return
                    kb.op("scalar", lambda e: e.copy(out=rawb[:], in_=ps[bk][:]), outs=[RAWB], ins=[PS[bk]])
                    b2 = mbank()
                    kb.op("tensor", lambda e: e.matmul(ps[b2][:], lhsT=pswap[:], rhs=rawb[:], start=True, stop=True), outs=[PS[b2]], ins=[RAWB, CST])
                    kb.op("vector", lambda e: e.tensor_tensor(out=rt1[:], in0=ps[bk][:], in1=cosb[:], op=ALU.mult), outs=[RT1], ins=[PS[bk], CSB])
                    kb.op("vector", lambda e: e.tensor_tensor(out=rt2[:], in0=ps[b2][:], in1=sinb[:], op=ALU.mult), outs=[RT2], ins=[PS[b2], CSB])
                    if isinstance(dst_ap, tuple):
                        kb.op("gpsimd", lambda e: e.tensor_tensor(out=dst_ap[0], in0=rt1[0:64, :], in1=rt2[0:64, :], op=ALU.add), outs=[dstbuf], ins=[RT1, RT2])
                        kb.op("gpsimd", lambda e: e.tensor_tensor(out=dst_ap[1], in0=rt1[64:128, :], in1=rt2[64:128, :], op=ALU.add), outs=[dstbuf], ins=[RT1, RT2])
                    else:
                        kb.op("gpsimd", lambda e: e.tensor_tensor(out=dst_ap, in0=rt1[:], in1=rt2[:], op=ALU.add), outs=[dstbuf], ins=[RT1, RT2])

                for g in range(2):
                    for j, cbase in enumerate((2560, 2688)):
                        kb.dma("gpsimd", wk3[:, :, j * 64:(j + 1) * 64], w_in_v[:, :, cbase + g * 64:cbase + g * 64 + 64], outs=[WN])
                    for j, cbase in enumerate((2816, 2816, 3072, 3072)):
                        kb.dma("gpsimd", wk3[:, :, 128 + j * 64:128 + (j + 1) * 64], w_in_v[:, :, cbase + g * 64:cbase + g * 64 + 64], outs=[WN])
                    for j, cbase in enumerate((2944, 3200)):
                        kb.dma("gpsimd", wv2[:, :, j * 64:(j + 1) * 64], w_in_v[:, :, cbase + g * 64:cbase + g * 64 + 64], outs=[WN])
                    kb.dma("gpsimd", wqg[:], w_in_v[:, :, 2048 + g * 256:2048 + (g + 1) * 256], outs=[WN])
                    kb.dma("gpsimd", wgt[:], w_in_v[:, :, 3328 + g * 12:3328 + (g + 1) * 12], outs=[WN])
                    kb.op("vector", lambda e: e.memset(vs1[:, :, 64:65], 1.0), outs=[KS])
                    kb.op("vector", lambda e: e.memset(vw1[:, :, 64:65], 1.0), outs=[KS])
                    if stop_after == "p1c_a":
                        dump("wk3", wk3[:], [128, 8, 384], BF16)
                        return nc, dbg_out
                    for n in range(NB):
                        sl = slice(n * 512, (n + 1) * 512)
                        kb.dma("sync", cosb[:], I["cosT"][:, sl], outs=[CSB])
                        kb.dma("sync", sinb[:], I["sinT"][:, sl], outs=[CSB])
                        for j in range(3):
                            bk = sbank()
                            for k in range(8):
                                kb.op("tensor", lambda e, k=k, bk=bk, j=j: e.matmul(ps[bk][:], lhsT=wk3[:, k, j * 128:(j + 1) * 128], rhs=hT[:, k, sl],
                                                                                    start=(k == 0), stop=(k == 7)), outs=[PS[bk]], ins=[WN, HT[n]], mark=(k == 7))
                            if j == 0:
                                kb.op("scalar", lambda e, bk=bk: e.copy(out=kcvT[:, sl], in_=ps[bk][:]), outs=[KS], ins=[PS[bk]])
                            else:
                                rope_from(bk, (KEe[0:64, sl], KEo[64:128, sl]) if j == 1 else kwT[:, sl], KS)
                        if stop_after == "p1c_b":
                                return nc, dbg_out
                        for i in range(4):
                            tt = 4 * n + i
                            bk = mbank()
                            for k in range(8):
                                kb.op("tensor", lambda e, k=k, bk=bk, tt=tt: e.matmul(ps[bk][:, 0:128], lhsT=hT[:, k, tt * 128:(tt + 1) * 128], rhs=wv2[:, k, :],
                                                                                     start=(k == 0), stop=(k == 7)), outs=[PS[bk]], ins=[WN, HT[n]], mark=(k == 7))
                            kb.op("scalar", lambda e, bk=bk, tt=tt: e.copy(out=vs1[:, tt, 0:64], in_=ps[bk][:, 0:64]), outs=[KS], ins=[PS[bk]])
                            kb.op("vector", lambda e, bk=bk, tt=tt: e.tensor_copy(out=vw1[:, tt, 0:64], in_=ps[bk][:, 64:128]), outs=[KS], ins=[PS[bk]])
                    if stop_after == "p1c_k":
                        dump("kcvT", kcvT[:], [128, S], BF16)
                        dump("vs1", vs1[:], [128, NT, 80], BF16)
                        return nc, dbg_out
                    with ExitStack() as pc:
                        w1kv = sb("w1kv", [128, 32, 256], BF16, pc)
                        peT = sb("peT", [128, 32], F32, pc)
                        peTb = sb("peTb", [128, 32], BF16, pc)
                        b1kv = sb("b1kv", [128, 4], F32, pc)
                        w2k2 = sb("w2k2", [128, 2, 128], BF16, pc)
                        w2v = sb("w2v", [128, 2, 64], BF16, pc)
                        hid = sb("hid", [128, 4, 256], BF16, pc)
                        beff = sb("beff", [128, 4], F32, pc)
                        gx = sb("gx", [128, 256], F32, pc)
                        gu = sb("gu", [128, 256], F32, pc)
                        gs = sb("gs", [128, 256], F32, pc)
                        CW = Buf(w1kv[:])
                        HID = Buf(hid[:])
                        GX = Buf(gx[:])
                        kb.dma("gpsimd", w1kv[0:64], I["w1k"], outs=[CW])
                        kb.dma("gpsimd", w1kv[64:128], I["w1v"], outs=[CW])
                        kb.dma("sync", peT[0:64], I["peTk"], outs=[CW])
                        kb.dma("sync", peT[64:128], I["peTv"], outs=[CW])
                        kb.dma("sync", b1kv[:, 0:2], I["b1k"], outs=[CW])
                        kb.dma("sync", b1kv[:, 2:4], I["b1v"], outs=[CW])
                        kb.dma("gpsimd", w2k2[:, :, 0:64], I["w2k"], outs=[CW])
                        kb.dma("gpsimd", w2k2[:, :, 64:128], I["w2k"], outs=[CW])
                        kb.dma("gpsimd", w2v[:], I["w2v"], outs=[CW])
                        kb.op("vector", lambda e: e.tensor_copy(out=peTb[:], in_=peT[:]), outs=[CW], ins=[CW])
                        kb.op("vector", lambda e: e.memset(hid[:], 0.0), outs=[HID])
                        for kv in range(2):
                            p0_ = kv * 64
                            for hc in range(2):
                                bk, bb = sbank(), mbank()
                                for l in range(32):
                                    kb.op("tensor", lambda e, l=l, bk=bk, hc=hc, p0_=p0_: e.matmul(ps[bk][:, 0:255], lhsT=w1kv[p0_:p0_ + 64, l, hc * 128:(hc + 1) * 128],
                                                                                                rhs=kcvT[p0_:p0_ + 64, l:l + 16 * 254 + 1:16], start=(l == 0), stop=(l == 31)),
                                          outs=[PS[bk]], ins=[CW, KS], mark=(l == 31))
                                for l in range(32):
                                    kb.op("tensor", lambda e, l=l, bb=bb, hc=hc, p0_=p0_: e.matmul(ps[bb][:, 0:1], lhsT=w1kv[p0_:p0_ + 64, l, hc * 128:(hc + 1) * 128],
                                                                                                rhs=peTb[p0_:p0_ + 64, l:l + 1], start=(l == 0), stop=(l == 31)),
                                          outs=[PS[bb]], ins=[CW], mark=(l == 31))
                                ci = kv * 2 + hc
                                kb.op("vector", lambda e, bb=bb, ci=ci: e.tensor_tensor(out=beff[:, ci:ci + 1], in0=ps[bb][:, 0:1], in1=b1kv[:, ci:ci + 1], op=ALU.add),
                                      outs=[GX], ins=[PS[bb], CW])
                                kb.op("vector", lambda e, bk=bk, ci=ci: e.tensor_scalar(out=gx[:, 0:255], in0=ps[bk][:, 0:255], scalar1=beff[:, ci:ci + 1], scalar2=None, op0=ALU.add),
                                      outs=[GX], ins=[PS[bk], GX])
                                kb.op("vector", lambda e: e.tensor_tensor(out=gu[:, 0:255], in0=gx[:, 0:255], in1=gx[:, 0:255], op=ALU.mult), outs=[GX], ins=[GX])
                                kb.op("vector", lambda e: e.tensor_scalar(out=gu[:, 0:255], in0=gu[:, 0:255], scalar1=0.044715, scalar2=1.0, op0=ALU.mult, op1=ALU.add), outs=[GX], ins=[GX])
                                kb.op("vector", lambda e: e.tensor_tensor(out=gu[:, 0:255], in0=gu[:, 0:255], in1=gx[:, 0:255], op=ALU.mult), outs=[GX], ins=[GX])
                                kb.op("scalar", lambda e: e.activation(out=gs[:, 0:255], in_=gu[:, 0:255], func=AF.Sigmoid, scale=1.5957691216057308), outs=[GX], ins=[GX])
                                kb.op("vector", lambda e, ci=ci: e.tensor_tensor(out=hid[:, ci, 0:255], in0=gx[:, 0:255], in1=gs[:, 0:255], op=ALU.mult), outs=[HID], ins=[GX])
                        bk = sbank()
                        for hc in range(2):
                            kb.op("tensor", lambda e, hc=hc, bk=bk: e.matmul(ps[bk][:, 0:256], lhsT=w2k2[:, hc, :], rhs=hid[:, hc, :], start=(hc == 0), stop=(hc == 1)),
                                  outs=[PS[bk]], ins=[CW, HID], mark=(hc == 1))
                        kb.op("scalar", lambda e, bk=bk: e.copy(out=kcmpT[:], in_=ps[bk][:, 0:256]), outs=[KC], ins=[PS[bk]])
                        for ct in range(2):
                            bk = sbank()
                            for hc in range(2):
                                kb.op("tensor", lambda e, hc=hc, bk=bk, ct=ct: e.matmul(ps[bk][:, 0:64], lhsT=hid[:, 2 + hc, ct * 128:(ct + 1) * 128], rhs=w2v[:, hc, :],
                                                                                       start=(hc == 0), stop=(hc == 1)), outs=[PS[bk]], ins=[CW, HID], mark=(hc == 1))
                            kb.op("scalar", lambda e, bk=bk, ct=ct: e.copy(out=vcmp1[:, ct, 0:64], in_=ps[bk][:, 0:64]), outs=[KC], ins=[PS[bk]])
                        kb.op("vector", lambda e: e.memset(vcmp1[:, :, 64:65], 1.0), outs=[KC])
                        kb.op("vector", lambda e: e.tensor_copy(out=vcmp1[:, :, 65:129], in_=ovl[:]), outs=[KC], ins=[CST])
                        kb.barrier()
                    if stop_after == "p1c_c":
                        dump("kcmpT", kcmpT[:], [128, 256], BF16)
                        dump("vcmp1", vcmp1[:], [128, 2, 144], BF16)
                        return nc, dbg_out
                    with ExitStack() as pq:
                        wband = sb("wband", [128, 8, 512], BF16, pq)
                        QC = Buf(wband[:])
                        kb.dma("sync", wband[:], I["wband"], outs=[QC])
                        qn = [[sb(f"qn{ch}{par}", [128, 512], BF16, pq) for par in range(2)] for ch in range(2)]
                        QN = Buf(qn[0][0][:])
                        qTb = sb("qTb", [128, 2, 512], BF16, pq)
                        qrTb = sb("qrTb", [128, 2, 512], BF16, pq)
                        QB_ = Buf(qTb[:])
                        QRB = Buf(qrTb[:])
                        gts = sb("gts", [128, 4, 12], F32, pq)
                        GTS = Buf(gts[:])
                        cmpbb = sb("cmpbb", [128, 512], BF16, pq)
                        CMB = Buf(cmpbb[:])
                        pt = [sb(f"pt{i}", [128, 512], BF16, pq) for i in range(3)]
                        PT = [Buf(t[:]) for t in pt]
                        onsa = sb("onsa", [128, 4, 256], F32, pq)
                        ONSA = Buf(onsa[:])
                        obn = sb("obn", [128, 4, 256], BF16, pq)
                        OBN = Buf(obn[:])
                        pslc = sb("pslc", [128, 4, 64], F32, pq)
                        PSLC = Buf(pslc[:])
                        sadd = sb("sadd", [128, 64], F32, pq)
                        SADD = Buf(sadd[:])
                        score = sb("score", [128, 64], F32, pq)
                        stmp = sb("stmp", [128, 64], F32, pq)
                        sel = sb("sel", [128, 64], F32, pq)
                        m8 = sb("m8", [128, 16], F32, pq)
                        negb = sb("negb", [128, 128], BF16, pq)
                        SEL = Buf(score[:])
                        negbT = sb("negbT", [128, 512], BF16, pq)
                        NBT = Buf(negbT[:])
                        rz = sb("rz", [128, 4], F32, pq)
                        RZ = Buf(rz[:])
                        accs = [ps[3][:, 0:129], ps[4][:, 0:129], ps[5][:, 0:129], ps[6][:, 0:129]]
                        ACC = [PS[3], PS[4], PS[5], PS[6]]
                        prr = [0]

                        def run_branch(steps):
                            LA = 2
                            n_ = len(steps)
                            for idx_ in range(n_ + LA):
                                if idx_ < n_:
                                    st = steps[idx_]
                                    bk = sbank()
                                    nm = len(st["s"])
                                    for idx, (l_, r_, insb) in enumerate(st["s"]):
                                        kb.op("tensor", lambda e, l_=l_, r_=r_, idx=idx, nm=nm, bk=bk: e.matmul(ps[bk][:], lhsT=l_, rhs=r_, start=(idx == 0), stop=(idx == nm - 1)),
                                              outs=[PS[bk]], ins=insb, mark=(idx == nm - 1))
                                    prr[0] = (prr[0] + 1) % 3
                                    pi = prr[0]
                                    kb.op("scalar", lambda e, bk=bk, pi=pi: e.activation(out=pt[pi][:], in_=ps[bk][:], func=AF.Exp, scale=SCL), outs=[PT[pi]], ins=[PS[bk]])
                                    st["pi"] = pi
                                if idx_ >= LA:
                                    prev = steps[idx_ - LA]
                                    pi = prev["pi"]
                                    for (i, rhs_ap, w_, st_, sp_) in prev["pv"]:
                                        kb.op("tensor", lambda e, i=i, rhs_ap=rhs_ap, w_=w_, st_=st_, sp_=sp_, pi=pi: e.matmul(accs[i][:, 0:w_], lhsT=pt[pi][:, i * 128:(i + 1) * 128], rhs=rhs_ap,
                                                                                                                 start=st_, stop=sp_),
                                              outs=[ACC[i]], ins=[PT[pi], KS, KC])

                        def finish(h, br, first):
                            zc = 64
                            for i in range(4):
                                kb.op("vector", lambda e, i=i: e.tensor_scalar(out=rz[:, i:i + 1], in0=accs[i][:, zc:zc + 1], scalar1=1e-30, scalar2=None, op0=ALU.max),
                                      outs=[RZ], ins=[ACC[i]])
                            kb.op("vector", lambda e: e.reciprocal(out=rz[:], in_=rz[:]), outs=[RZ], ins=[RZ])
                            if br == 0:
                                for i in range(4):
                                    if h == 0:
                                        kb.op("vector", lambda e, i=i: e.tensor_scalar(out=pslc[:, i, :], in0=accs[i][:, 65:129], scalar1=rz[:, i:i + 1], scalar2=None, op0=ALU.mult),
                                              outs=[PSLC], ins=[ACC[i], RZ])
                                    else:
                                        kb.op("vector", lambda e, i=i: e.scalar_tensor_tensor(out=pslc[:, i, :], in0=accs[i][:, 65:129], scalar=rz[:, i:i + 1], in1=pslc[:, i, :],
                                                                                           op0=ALU.mult, op1=ALU.add), outs=[PSLC], ins=[ACC[i], RZ, PSLC])
                            kb.op("vector", lambda e: e.tensor_tensor(out=rz[:], in0=rz[:], in1=gts[:, :, h * 3 + br], op=ALU.mult), outs=[RZ], ins=[RZ, GTS])
                            for i in range(4):
                                dst = onsa[:, i, h * 64:(h + 1) * 64]
                                if first:
                                    kb.op("vector", lambda e, i=i, dst=dst: e.tensor_scalar(out=dst, in0=accs[i][:, 0:64], scalar1=rz[:, i:i + 1], scalar2=None, op0=ALU.mult),
                                          outs=[ONSA], ins=[ACC[i], RZ])
                                else:
                                    kb.op("vector", lambda e, i=i, dst=dst: e.scalar_tensor_tensor(out=dst, in0=accs[i][:, 0:64], scalar=rz[:, i:i + 1], in1=dst, op0=ALU.mult, op1=ALU.add),
                                          outs=[ONSA], ins=[ACC[i], RZ, ONSA])

                        for qb in range(NB):
                            sl = slice(qb * 512, (qb + 1) * 512)
                            kb.dma("sync", cosb[:], I["cosT"][:, sl], outs=[CSB])
                            kb.dma("sync", sinb[:], I["sinT"][:, sl], outs=[CSB])
                            kb.dma("sync", cmpbb[:], I["cmpb"][:, qb, :], outs=[CMB])
                            for ch in range(2):
                                bk = sbank()
                                for k in range(8):
                                    kb.op("tensor", lambda e, k=k, bk=bk, ch=ch: e.matmul(ps[bk][:], lhsT=wqg[:, k, ch * 128:(ch + 1) * 128], rhs=hT[:, k, sl],
                                                                                         start=(k == 0), stop=(k == 7)), outs=[PS[bk]], ins=[WN, HT[qb]], mark=(k == 7))
                                kb.op("scalar", lambda e, bk=bk, ch=ch: e.copy(out=qTb[:, ch, :], in_=ps[bk][:]), outs=[QB_], ins=[PS[bk]])
                                rope_from(bk, qrTb[:, ch, :], QRB)
                            for i in range(4):
                                tt = 4 * qb + i
                                bk = mbank()
                                for k in range(8):
                                    kb.op("tensor", lambda e, k=k, bk=bk, tt=tt: e.matmul(ps[bk][:, 0:12], lhsT=hT[:, k, tt * 128:(tt + 1) * 128], rhs=wgt[:, k, :],
                                                                                         start=(k == 0), stop=(k == 7)), outs=[PS[bk]], ins=[WN, HT[qb]], mark=(k == 7))
                                kb.op("scalar", lambda e, bk=bk, i=i: e.activation(out=gts[:, i, :], in_=ps[bk][:, 0:12], func=AF.Sigmoid), outs=[GTS], ins=[PS[bk]])
                            if stop_after == "p1c_qa":
                                dump("onsa", onsa[:], [128, 4, 256])
                                return nc, dbg_out
                            ncts = 1 if qb < 4 else 2
                            for h in range(4):
                                ch, p0_ = h // 2, (h % 2) * 64
                                steps = []
                                for ct in range(ncts):
                                    smm = [(kcmpT[p0_:p0_ + 64, ct * 128:(ct + 1) * 128], qTb[p0_:p0_ + 64, ch, :], [KC, QB_])]
                                    if ct == ncts - 1:
                                        smm.append((ident[:], cmpbb[:], [CST, CMB]))
                                    pv = [(i, vcmp1[:, ct, 0:129], 129, ct == 0, ct == ncts - 1) for i in range(4)]
                                    steps.append({"s": smm, "pv": pv})
                                run_branch(steps)
                                finish(h, 0, True)
                            if stop_after == "p1c_qb":
                                dump("onsa", onsa[:], [128, 4, 256])
                                return nc, dbg_out
                            for i in range(4):
                                qt = 4 * qb + i
                                kb.dma("sync", sadd[:], I["seladd"][:, qt, :], outs=[SADD])
                                kb.op("vector", lambda e, i=i: e.tensor_tensor(out=score[:], in0=pslc[:, i, :], in1=sadd[:], op=ALU.add), outs=[SEL], ins=[PSLC, SADD])
                                kb.op("vector", lambda e: e.max(out=m8[:, 0:8], in_=score[:]), outs=[SEL], ins=[SEL])
                                kb.op("vector", lambda e: e.match_replace(out=stmp[:], in_to_replace=m8[:, 0:8], in_values=score[:], imm_value=-3e38), outs=[SEL], ins=[SEL])
                                kb.op("vector", lambda e: e.max(out=m8[:, 8:16], in_=stmp[:]), outs=[SEL], ins=[SEL])
                                kb.op("vector", lambda e: e.tensor_scalar(out=sel[:], in0=score[:], scalar1=m8[:, 15:16], scalar2=None, op0=ALU.is_ge), outs=[SEL], ins=[SEL])
                                kb.op("vector", lambda e: e.scalar_tensor_tensor(out=sel[:], in0=score[:], scalar=-1e29, in1=sel[:], op0=ALU.is_gt, op1=ALU.mult), outs=[SEL], ins=[SEL])
                                kb.op("vector", lambda e: e.tensor_scalar(out=negb[:, 0:64], in0=sel[:], scalar1=-1.0, scalar2=-NEG, op0=ALU.add, op1=ALU.mult), outs=[SEL], ins=[SEL])
                                kb.op("vector", lambda e: e.tensor_scalar(out=negb[:, 64:128], in0=sel[:], scalar1=-1.0, scalar2=-NEG, op0=ALU.add, op1=ALU.mult), outs=[SEL], ins=[SEL])
                                bk = mbank()
                                kb.op("tensor", lambda e, bk=bk: e.matmul(ps[bk][:, 0:128], lhsT=negb[:], rhs=ident[:], start=True, stop=True), outs=[PS[bk]], ins=[SEL, CST])
                                kb.op("scalar", lambda e, bk=bk, i=i: e.copy(out=negbT[:, i * 128:(i + 1) * 128], in_=ps[bk][:, 0:128]), outs=[NBT], ins=[PS[bk]])
                            if stop_after == "p1c_qc":
                                dump("onsa", onsa[:], [128, 4, 256])
                                return nc, dbg_out
                            for ch in range(2):
                                kb.op("gpsimd", lambda e, ch=ch: e.tensor_copy(out=qn[ch][0][0:64, :], in_=qrTb[0:64, ch, :]), outs=[QN], ins=[QRB])
                                kb.op("gpsimd", lambda e, ch=ch: e.tensor_copy(out=qn[ch][0][64:128, :], in_=negbT[64:128, :]), outs=[QN], ins=[NBT])
                                kb.op("gpsimd", lambda e, ch=ch: e.tensor_copy(out=qn[ch][1][0:64, :], in_=negbT[0:64, :]), outs=[QN], ins=[NBT])
                                kb.op("gpsimd", lambda e, ch=ch: e.tensor_copy(out=qn[ch][1][64:128, :], in_=qrTb[64:128, ch, :]), outs=[QN], ins=[QRB])
                            for h in range(4):
                                ch, p0_ = h // 2, (h % 2) * 64
                                KE_ = KEe if h % 2 == 0 else KEo
                                steps = []
                                for kt in range(4 * qb + 4):
                                    smm = [(KE_[:, kt * 128:(kt + 1) * 128], qn[ch][h % 2][:], [KS, QN, CST])]
                                    if kt >= 4 * qb:
                                        smm.append((ident[:], wband[:, 4 + kt - 4 * qb, :], [CST, QC]))
                                    pv = [(i, vs1[:, kt, 0:65], 65, kt == 0, kt == 4 * qb + i) for i in range(4) if kt <= 4 * qb + i]
                                    steps.append({"s": smm, "pv": pv})
                                run_branch(steps)
                                finish(h, 1, False)
                            if stop_after == "p1c_qd":
                                dump("onsa", onsa[:], [128, 4, 256])
                                return nc, dbg_out
                            for h in range(4):
                                ch, p0_ = h // 2, (h % 2) * 64
                                steps = []
                                jmin = max(0, 4 - 4 * qb)
                                for j in range(jmin, 8):
                                    kt = 4 * qb - 4 + j
                                    smm = [(kwT[p0_:p0_ + 64, kt * 128:(kt + 1) * 128], qrTb[p0_:p0_ + 64, ch, :], [KS, QRB]),
                                           (ident[:], wband[:, j, :], [CST, QC])]
                                    pv = [(i, vw1[:, kt, 0:65], 65, j == max(i, jmin), j == i + 4) for i in range(4) if i <= j <= i + 4]
                                    steps.append({"s": smm, "pv": pv})
                                run_branch(steps)
                                finish(h, 2, False)
                            if stop_after == "p1c_q":
                                dump("onsa", onsa[:], [128, 4, 256])
                                dump("pslc", pslc[:], [128, 4, 64])
                                dump("negbT", negbT[:], [128, 512], BF16)
                                return nc, dbg_out
                            kb.op("gpsimd", lambda e: e.tensor_copy(out=obn[:], in_=onsa[:]), outs=[OBN], ins=[ONSA])
                            for i in range(4):
                                qt = 4 * qb + i
                                isel = isel0 if qt < 16 else isel1
                                s_ = qt % 16
                                for ch in range(2):
                                    jf = 4 + g * 2 + ch
                                    bk = mbank()
                                    kb.op("tensor", lambda e, bk=bk, i=i, ch=ch, isel=isel: e.matmul(ps[bk][:, 0:128], lhsT=obn[:, i, ch * 128:(ch + 1) * 128], rhs=isel[:], start=True, stop=True),
                                          outs=[PS[bk]], ins=[OBN, CST])
                                    dst = oT[:, jf, s_ * 128:(s_ + 1) * 128]
                                    if qt < 16:
                                        kb.op("scalar", lambda e, bk=bk, dst=dst: e.copy(out=dst, in_=ps[bk][:, 0:128]), outs=[OT[jf][s_]], ins=[PS[bk]])
                                    else:
                                        kb.op("vector", lambda e, bk=bk, dst=dst: e.tensor_tensor(out=dst, in0=ps[bk][:, 0:128], in1=dst, op=ALU.add),
                                              outs=[OT[jf][s_]], ins=[PS[bk], OT[jf][s_]])
                        kb.barrier()
                kb.barrier()
            if "oT2" in dbg:
                dump("oT2", oT[:], [128, 8, SO], BF16)
            if stop_after == "p1c":
                kb.barrier()
                return nc, dbg_out
        kb.barrier()
        with ExitStack() as p2:
            acc = sb("acc", [128, 8, SO], F32, p2)
            ACCB = [Buf(acc[:, :, nb * 512:(nb + 1) * 512]) for nb in range(4)]
            wo = sb("wo", [128, 8, D], BF16, p2)
            WO = Buf(wo[:])
            fg = sb("fg", [128, 8], F32, p2)
            FG = Buf(fg[:])
            kb.dma("sync", fg[:], I["fg"], outs=[FG])
            xTo_v = I["xTo"].rearrange("(k p) t -> p k t", p=128)
            for nb in range(4):
                kb.dma("sync", acc[:, :, nb * 512:(nb + 1) * 512], xTo_v[:, :, nb * 512:(nb + 1) * 512], outs=[ACCB[nb]])
            w_out_v = I["w_out"].rearrange("(k p) c -> p k c", p=128)
            for k in range(8):
                kb.dma("gpsimd", wo[:, k, :], w_out_v[:, k, :], outs=[WO])
            rr2 = [0]

            def bank2():
                rr2[0] = (rr2[0] + 1) % 8
                return rr2[0]

            for nb in range(4):
                sl = slice(nb * 512, (nb + 1) * 512)
                for m in range(8):
                    bk = bank2()
                    for k in range(8):
                        kb.op("tensor", lambda e, k=k, m=m, bk=bk, sl=sl: e.matmul(ps[bk][:], lhsT=wo[:, k, m * 128:(m + 1) * 128], rhs=oT[:, k, sl], start=(k == 0), stop=(k == 7)),
                              outs=[PS[bk]], ins=[WO] + [OT[k][s] for s in range(nb * 4, nb * 4 + 4)], mark=(k == 7))
                    kb.op("vector", lambda e, m=m, bk=bk, sl=sl: e.scalar_tensor_tensor(out=acc[:, m, sl], in0=ps[bk][:], scalar=g1c(m), in1=acc[:, m, sl], op0=ALU.mult, op1=ALU.add),
                          outs=[ACCB[nb]], ins=[PS[bk], ACCB[nb], MOD])
            if "x2T" in dbg:
                dump("x2T", acc[:], [128, 8, SO])
            h2T = sb("h2T", [128, 8, SO], BF16, p2)
            H2 = [Buf(h2T[:, :, nb * 512:(nb + 1) * 512]) for nb in range(4)]
            with ExitStack() as p2a:
                sqa = sb("sqa", [128, 8, 512], F32, p2a)
                SQA = Buf(sqa[:])
                rsa = sb("rsa", [128, 512], F32, p2a)
                RSA = Buf(rsa[:])
                tma = [sb(f"tma{i}", [128, 512], F32, p2a) for i in range(2)]
                TMA = [Buf(t[:]) for t in tma]
                for nb in range(4):
                    sl = slice(nb * 512, (nb + 1) * 512)
                    kb.op("scalar", lambda e, sl=sl: e.activation(out=sqa[:], in_=acc[:, :, sl], func=AF.Square), outs=[SQA], ins=[ACCB[nb]])
                    bk = bank2()
                    for k in range(8):
                        kb.op("tensor", lambda e, k=k, bk=bk: e.matmul(ps[bk][:], lhsT=onesf[:], rhs=sqa[:, k, :], start=(k == 0), stop=(k == 7)),
                              outs=[PS[bk]], ins=[SQA, CST], mark=(k == 7))
                    kb.op("scalar", lambda e, bk=bk: e.activation(out=rsa[:], in_=ps[bk][:], func=AF.Sqrt, scale=1.0 / D, bias=EPS), outs=[RSA], ins=[PS[bk]])
                    kb.op("vector", lambda e: e.reciprocal(out=rsa[:], in_=rsa[:]), outs=[RSA], ins=[RSA])
                    for k in range(8):
                        T = TMA[k % 2]
                        t_ = tma[k % 2]
                        kb.op("vector", lambda e, k=k, t_=t_, sl=sl: e.tensor_tensor(out=t_[:], in0=acc[:, k, sl], in1=rsa[:], op=ALU.mult), outs=[T], ins=[ACCB[nb], RSA])
                        kb.op("scalar", lambda e, k=k, t_=t_, sl=sl: e.activation(out=h2T[:, k, sl], in_=t_[:], func=AF.Identity, scale=a2[:, k:k + 1], bias=sh2(k)),
                              outs=[H2[nb]], ins=[T, MOD])
                kb.barrier()
            if "h2T" in dbg:
                dump("h2T", h2T[:], [128, 8, SO], BF16)
            gw = sb("gw", [128, 16, 65], F32, p2)
            GW = Buf(gw[:])
            kb.op("vector", lambda e: e.memset(gw[:], 1.0), outs=[GW])
            with ExitStack() as p2r:
                rwf = sb("rwf", [128, 8, 64], F32, p2r)
                rwb = sb("rwb", [128, 8, 64], BF16, p2r)
                rbias = sb("rbias", [128, 64], F32, p2r)
                RW = Buf(rwf[:])
                kb.dma("sync", rwf[:], I["rw"], outs=[RW])
                kb.dma("sync", rbias[:], I["rbias"], outs=[RW])
                kb.op("vector", lambda e: e.tensor_copy(out=rwb[:], in_=rwf[:]), outs=[RW], ins=[RW])
                scr = sb("scr", [128, 64], F32, p2r)
                chs = sb("chs", [128, 64], F32, p2r)
                eq = sb("eq", [128, 64], F32, p2r)
                chm = sb("chm", [128, 64], F32, p2r)
                m1 = sb("m1", [128, 8], F32, p2r)
                m2 = sb("m2", [128, 8], F32, p2r)
                gsm = sb("gsm", [128, 8], F32, p2r)
                g8 = sb("g8", [128, 8], F32, p2r)
                gmk = sb("gmk", [128, 8], F32, p2r)
                e8 = sb("e8", [128, 8], F32, p2r)
                ssum = sb("ssum", [128, 1], F32, p2r)
                RT = Buf(scr[:])
                v3 = lambda ap: ap.rearrange("p (g j) -> p g j", j=8)
                b3 = lambda ap: ap.rearrange("p (g o) -> p g o", o=1).to_broadcast([128, 8, 8])
                for tt in range(16):
                    bk = bank2()
                    for k in range(8):
                        kb.op("tensor", lambda e, k=k, bk=bk, tt=tt: e.matmul(ps[bk][:, 0:64], lhsT=h2T[:, k, tt * 128:(tt + 1) * 128], rhs=rwb[:, k, :], start=(k == 0), stop=(k == 7)),
                              outs=[PS[bk]], ins=[H2[tt // 4], RW], mark=(k == 7))
                    kb.op("scalar", lambda e, bk=bk: e.activation(out=scr[:], in_=ps[bk][:, 0:64], func=AF.Sigmoid), outs=[RT], ins=[PS[bk]])
                    V = lambda f: kb.op("vector", f, outs=[RT], ins=[RT, RW])
                    V(lambda e: e.tensor_tensor(out=chs[:], in0=scr[:], in1=rbias[:], op=ALU.add))
                    V(lambda e: e.tensor_reduce(out=m1[:], in_=v3(chs[:]), axis=AX.X, op=ALU.max))
                    V(lambda e: e.tensor_tensor(out=v3(eq[:]), in0=v3(chs[:]), in1=b3(m1[:]), op=ALU.is_equal))
                    V(lambda e: e.scalar_tensor_tensor(out=eq[:], in0=eq[:], scalar=-1e30, in1=chs[:], op0=ALU.mult, op1=ALU.add))
                    V(lambda e: e.tensor_reduce(out=m2[:], in_=v3(eq[:]), axis=AX.X, op=ALU.max))
                    V(lambda e: e.tensor_tensor(out=gsm[:], in0=m1[:], in1=m2[:], op=ALU.add))
                    V(lambda e: e.max(out=g8[:], in_=gsm[:]))
                    V(lambda e: e.tensor_scalar(out=gmk[:], in0=gsm[:], scalar1=g8[:, 3:4], scalar2=None, op0=ALU.is_ge))
                    V(lambda e: e.scalar_tensor_tensor(out=v3(chm[:]), in0=v3(chs[:]), scalar=10.0, in1=b3(gmk[:]), op0=ALU.add, op1=ALU.mult))
                    V(lambda e: e.max(out=e8[:], in_=chm[:]))
                    V(lambda e: e.tensor_scalar(out=eq[:], in0=chm[:], scalar1=e8[:, 7:8], scalar2=None, op0=ALU.is_ge))
                    V(lambda e: e.tensor_tensor(out=eq[:], in0=eq[:], in1=scr[:], op=ALU.mult))
                    V(lambda e: e.tensor_reduce(out=ssum[:], in_=eq[:], axis=AX.X, op=ALU.add))
                    V(lambda e: e.reciprocal(out=ssum[:], in_=ssum[:]))
                    kb.op("vector", lambda e, tt=tt: e.tensor_scalar(out=gw[:, tt, 0:64], in0=eq[:], scalar1=ssum[:, 0:1], scalar2=2.5, op0=ALU.mult, op1=ALU.mult),
                          outs=[GW], ins=[RT])
                kb.barrier()
            if "gw" in dbg:
                dump("gw", gw[:], [128, 16, 65])
            with ExitStack() as p2e:
                wg = [sb(f"wg{i}", [128, 8, 512], BF16, p2e) for i in range(2)]
                wd = [sb(f"wd{i}", [128, 2, D], BF16, p2e) for i in range(2)]
                WG = [Buf(t[:]) for t in wg]
                WD = [Buf(t[:]) for t in wd]
                actT = [sb(f"actT{i}", [128, 2, SO], BF16, p2e) for i in range(2)]
                ACT_ = [[Buf(actT[i][:, :, nb * 512:(nb + 1) * 512]) for nb in range(4)] for i in range(2)]
                sgt = [sb(f"sgt{i}", [128, 256], F32, p2e) for i in range(2)]
                SGT = [Buf(t[:]) for t in sgt]
                att = [sb(f"att{i}", [128, 256], BF16, p2e) for i in range(2)]
                ATT = [Buf(t[:]) for t in att]
                NE = n_experts

                def load_w(e_):
                    sl_ = e_ % 2
                    gv = I["wgu"][e_].rearrange("(k p) c -> p k c", p=128)
                    kb.dma("gpsimd", wg[sl_][:, 0:4, :], gv[:, 0:4, :], outs=[WG[sl_]])
                    kb.dma("gpsimd", wg[sl_][:, 4:8, :], gv[:, 4:8, :], outs=[WG[sl_]])
                    kb.dma("gpsimd", wd[sl_][:], I["wdn"][e_].rearrange("(k p) c -> p k c", p=128), outs=[WD[sl_]])

                def load_wg(e_):
                    sl_ = e_ % 2
                    gv = I["wgu"][e_].rearrange("(k p) c -> p k c", p=128)
                    kb.dma("gpsimd", wg[sl_][:, 0:4, :], gv[:, 0:4, :], outs=[WG[sl_]])
                    kb.dma("gpsimd", wg[sl_][:, 4:8, :], gv[:, 4:8, :], outs=[WG[sl_]])

                def load_wd(e_):
                    sl_ = e_ % 2
                    kb.dma("gpsimd", wd[sl_][:], I["wdn"][e_].rearrange("(k p) c -> p k c", p=128), outs=[WD[sl_]])

                cnt = [0]
                pend = {}

                def GU(e_, tt):
                    sl_ = e_ % 2
                    bk = bank2()
                    for k in range(8):
                        kb.op("tensor", lambda e, k=k: e.matmul(ps[bk][:], lhsT=h2T[:, k, tt * 128:(tt + 1) * 128], rhs=wg[sl_][:, k, :], start=(k == 0), stop=(k == 7)),
                              outs=[PS[bk]], ins=[H2[tt // 4], WG[sl_]], mark=(k == 7))
                    cnt[0] += 1
                    i2 = cnt[0] % 2
                    kb.op("scalar", lambda e: e.activation(out=sgt[i2][:], in_=ps[bk][:, 0:256], func=AF.Silu), outs=[SGT[i2]], ins=[PS[bk]])
                    kb.op("vector", lambda e: e.scalar_tensor_tensor(out=att[i2][:], in0=ps[bk][:, 256:512], scalar=gw[:, tt, e_:e_ + 1], in1=sgt[i2][:],
                                                                     op0=ALU.mult, op1=ALU.mult), outs=[ATT[i2]], ins=[PS[bk], SGT[i2], GW])
                    pend[(e_, tt)] = i2

                def TR(e_, tt):
                    sl_ = e_ % 2
                    i2 = pend.pop((e_, tt))
                    for hc in range(2):
                        bt = bank2()
                        kb.op("tensor", lambda e, hc=hc, bt=bt: e.matmul(ps[bt][:, 0:128], lhsT=att[i2][:, hc * 128:(hc + 1) * 128], rhs=ident[:], start=True, stop=True),
                              outs=[PS[bt]], ins=[ATT[i2], CST])
                        kb.op("scalar", lambda e, hc=hc, bt=bt: e.copy(out=actT[sl_][:, hc, tt * 128:(tt + 1) * 128], in_=ps[bt][:, 0:128]),
                              outs=[ACT_[sl_][tt // 4]], ins=[PS[bt]])

                def DN(e_, nb):
                    sl_ = e_ % 2
                    sl = slice(nb * 512, (nb + 1) * 512)
                    for m in range(8):
                        bk = bank2()
                        for hc in range(2):
                            kb.op("tensor", lambda e, bk=bk, hc=hc, m=m: e.matmul(ps[bk][:], lhsT=wd[sl_][:, hc, m * 128:(m + 1) * 128], rhs=actT[sl_][:, hc, sl], start=(hc == 0), stop=(hc == 1)),
                                  outs=[PS[bk]], ins=[WD[sl_], ACT_[sl_][nb]], mark=(hc == 1))
                        kb.op("vector", lambda e, bk=bk, m=m: e.scalar_tensor_tensor(out=acc[:, m, sl], in0=ps[bk][:], scalar=g2c(m), in1=acc[:, m, sl], op0=ALU.mult, op1=ALU.add),
                              outs=[ACCB[nb]], ins=[PS[bk], ACCB[nb], MOD])

                load_wg(0)
                load_wd(0)
                for e_ in range(NE + 1):
                    if e_ + 1 < NE:
                        load_wg(e_ + 1)
                    for tt in range(16):
                        if e_ < NE:
                            GU(e_, tt)
                            if tt > 0:
                                TR(e_, tt - 1)
                        if e_ > 0 and tt % 4 == 3:
                            DN(e_ - 1, tt // 4)
                    if e_ < NE:
                        TR(e_, 15)
                    if e_ + 1 < NE:
                        load_wd(e_ + 1)
                kb.barrier()
            if "x3T" in dbg:
                dump("x3T", acc[:], [128, 8, SO])
            sq2 = sb("sq2", [128, 8, 512], F32, p2)
            SQ2 = Buf(sq2[:])
            rstd2 = sb("rstd2", [128, 512], F32, p2)
            RS2 = Buf(rstd2[:])
            ot = [sb(f"ot{i}", [128, 8, 512], F32, p2) for i in range(2)]
            OTB = [Buf(t[:]) for t in ot]
            outT_v = outT.rearrange("(k p) t -> p k t", p=128)
            for nb in range(4):
                sl = slice(nb * 512, (nb + 1) * 512)
                kb.op("scalar", lambda e, sl=sl: e.activation(out=sq2[:], in_=acc[:, :, sl], func=AF.Square), outs=[SQ2], ins=[ACCB[nb]])
                bk = bank2()
                for k in range(8):
                    kb.op("tensor", lambda e, k=k, bk=bk: e.matmul(ps[bk][:], lhsT=onesf[:], rhs=sq2[:, k, :], start=(k == 0), stop=(k == 7)),
                          outs=[PS[bk]], ins=[SQ2, CST], mark=(k == 7))
                kb.op("scalar", lambda e, bk=bk: e.activation(out=rstd2[:], in_=ps[bk][:], func=AF.Sqrt, scale=1.0 / D, bias=EPS), outs=[RS2], ins=[PS[bk]])
                kb.op("vector", lambda e: e.reciprocal(out=rstd2[:], in_=rstd2[:]), outs=[RS2], ins=[RS2])
                o_ = ot[nb % 2]
                for k in range(8):
                    kb.op("vector", lambda e, k=k, o_=o_, sl=sl: e.scalar_tensor_tensor(out=o_[:, k, :], in0=acc[:, k, sl], scalar=fg[:, k:k + 1], in1=rstd2[:], op0=ALU.mult, op1=ALU.mult),
                          outs=[OTB[nb % 2]], ins=[ACCB[nb], FG, RS2])
                kb.dma("sync", outT_v[:, :, sl], o_[:], ins=[OTB[nb % 2]])
            kb.barrier()
        kb.barrier()
    return nc, dbg_out


def _prep_inputs(inp, core):
    b = core // 2
    half = core % 2
    f = lambda a: np.ascontiguousarray(a, dtype=np.float32)
    x = inp["x"][b]
    m = {}
    xT = f(x.T)
    m["xT"] = xT
    m["xTo"] = f(xT[:, half * SO:(half + 1) * SO])
    m["cT"] = f(inp["c"][b].reshape(8, 128).T)
    m["w_ada"] = f(inp["w_ada"][0])
    m["b_ada"] = f(inp["b_ada"][0].reshape(1, -1))
    m["n1g"] = f(inp["norm1_g"][0].reshape(8, 128).T)
    m["w_in"] = f(inp["w_in"][0])
    m["lbl"] = f(inp["hg_lb_logits"].reshape(2, 4, 128).transpose(2, 0, 1))
    m["hng"] = f(np.broadcast_to(inp["hg_norm_g"][0][None, :], (128, 512)))
    for s in ("k", "v"):
        m["peT" + s] = f(inp["cmp_pos_" + s][0].T)
        m["w1" + s] = f(inp["cmp_w1_" + s][0].reshape(32, 64, 256).transpose(1, 0, 2))
        m["b1" + s] = f(inp["cmp_b1_" + s][0].reshape(2, 128).T)
        m["w2" + s] = f(inp["cmp_w2_" + s][0].reshape(2, 128, 64).transpose(1, 0, 2))
    m["w_out"] = f(inp["w_out"][0])
    m["n2g"] = f(inp["norm2_g"][0].reshape(8, 128).T)
    m["rw"] = f(inp["router_w"][0].reshape(8, 128, 64).transpose(1, 0, 2))
    m["rbias"] = f(np.broadcast_to(inp["router_bias"][0][None, :], (128, 64)))
    m["fg"] = f(inp["final_g"].reshape(8, 128).T)
    return m


_SHARED = {}


def kernel(**inp):
    inp = {k: np.asarray(v) for k, v in inp.items()}
    nc, _ = build()
    wgu = np.ascontiguousarray(np.concatenate([inp["w_exp_gu"][0], inp["w_sh_gu"][0][None]], axis=0), dtype=np.float32)
    wdn = np.ascontiguousarray(np.concatenate([inp["w_exp_dn"][0], inp["w_sh_dn"][0][None]], axis=0), dtype=np.float32)
    in_maps = []
    for core in range(8):
        m = _prep_inputs(inp, core)
        m["wgu"] = wgu
        m["wdn"] = wdn
        m.update(_consts(core % 2))
        in_maps.append(m)
    res = run_bass_kernel_spmd(nc, in_maps, core_ids=list(range(8)))
    out = np.zeros((4, S, D), np.float32)
    for core in range(8):
        b, half = core // 2, core % 2
        out[b, half * SO:(half + 1) * SO, :] = res.results[core]["outT"].T
    return out
```

```python
import numpy as np
import os as _os0
import ml_dtypes
from contextlib import ExitStack
import concourse.bass as bass
import concourse.mybir as mybir
from concourse.bass_utils import run_bass_kernel_spmd

F32 = mybir.dt.float32
BF16 = mybir.dt.bfloat16
AF = mybir.ActivationFunctionType
ALU = mybir.AluOpType
AX = mybir.AxisListType

S = 4096
D = 1024
NT = 32
NB = 8
SO = 2048
EPS = 1e-6
NEG = -30000.0
NDS = 12
SEM_LIMIT = 2000
SAME_SYNC = not bool(int(_os0.environ.get("NOSAME", "0")))


class Buf:
    __slots__ = ("ap", "w", "r", "excl")

    def __init__(self, ap, excl=False):
        self.ap = ap
        self.w = None
        self.r = {}
        self.excl = excl

    def __getitem__(self, k):
        return self.ap[k]


class Eng:
    def __init__(self, name, h):
        self.name = name
        self.h = h
        self.sem = None
        self.count = 0
        self.epoch = 0
        self.waited = {}


class KB:
    def __init__(self, nc, es):
        self.nc = nc
        self.es = es
        self.engs = {n: Eng(n, getattr(nc, n)) for n in ("tensor", "vector", "scalar", "gpsimd", "sync")}
        for e in self.engs.values():
            self._new_sem(e)
        self.dsems = {q: [es.enter_context(nc.semaphore(f"d_{q}{i}")) for i in range(NDS)] for q in ("sync", "gpsimd")}
        self.dcnt = {q: [0] * NDS for q in ("sync", "gpsimd")}
        self.drr = {"sync": 0, "gpsimd": 0}
        self.nsem = 0

    def _new_sem(self, e):
        e.epoch += 1
        e.sem = self.es.enter_context(self.nc.semaphore(f"s_{e.name}_{e.epoch}"))
        e.count = 0

    def wait(self, eng, tk):
        key, sem, val = tk
        if eng.waited.get(key, 0) >= val:
            return
        eng.h.wait_ge(sem, val)
        eng.waited[key] = val

    def _deps(self, en, eng, outs, ins):
        need = {}

        def add(t):
            if t[3] == en and (en == "tensor" or not SAME_SYNC):
                return
            cur = need.get(t[0])
            if cur is None or cur[2] < t[2]:
                need[t[0]] = t

        for b in ins:
            if b.w is not None:
                add(b.w)
            if b.excl:
                for t in b.r.values():
                    if t[3] != en:
                        add(t)
        for b in outs:
            if b.w is not None:
                add(b.w)
            for t in b.r.values():
                add(t)
        for t in need.values():
            self.wait(eng, t[:3])

    def op(self, en, fn, outs=(), ins=(), mark=True):
        eng = self.engs[en]
        self._deps(en, eng, outs, ins)
        if eng.count >= SEM_LIMIT:
            self._new_sem(eng)
        inst = fn(eng.h)
        if mark:
            eng.count += 1
            inst.then_inc(eng.sem, 1)
            tk = ((en, eng.epoch), eng.sem, eng.count, en)
        else:
            tk = ((en, eng.epoch), eng.sem, eng.count + 1, en)
        for b in ins:
            b.r[tk[0]] = tk
        for b in outs:
            b.w = tk
            b.r = {}
        return tk

    def dma(self, q, out_ap, in_ap, outs=(), ins=()):
        eng = self.engs[q]
        i = self.drr[q]
        self.drr[q] = (i + 1) % NDS
        sem = self.dsems[q][i]
        key = ("d", q, i)
        if self.dcnt[q][i] > 0:
            self.wait(eng, (key, sem, self.dcnt[q][i]))
        self._deps("dma_" + q, eng, outs, ins)
        inst = eng.h.dma_start(out=out_ap, in_=in_ap)
        self.dcnt[q][i] += 16
        inst.then_inc(sem, 16)
        tk = (key, sem, self.dcnt[q][i], "dma_" + q)
        for b in ins:
            b.r[key] = tk
        for b in outs:
            b.w = tk
            b.r = {}
        return tk

    def barrier(self):
        for e in self.engs.values():
            for o in self.engs.values():
                if o is e or o.count == 0:
                    continue
                self.wait(e, ((o.name, o.epoch), o.sem, o.count))
            for q in ("sync", "gpsimd"):
                for i in range(NDS):
                    if self.dcnt[q][i] > 0:
                        self.wait(e, (("d", q, i), self.dsems[q][i], self.dcnt[q][i]))


def _consts(half):
    bf = ml_dtypes.bfloat16
    c = {}
    eye = np.eye(128, dtype=np.float32)
    c["ident"] = eye.astype(bf)
    c["onesf"] = np.ones((128, 128), np.float32)
    c["isel0"] = (eye * (1.0 if half == 0 else 0.0)).astype(bf)
    c["isel1"] = (eye * (1.0 if half == 1 else 0.0)).astype(bf)
    m = np.arange(128)
    sw = (m // 64) * 64 + ((m % 64) + 32) % 64
    ps = np.zeros((128, 128), np.float32)
    ps[sw, m] = 1.0
    c["pswap"] = ps.astype(bf)
    shift = 2048 if half == 0 else 0
    dd = np.arange(128) % 64
    i = dd % 32
    inv = 10000.0 ** (-(i.astype(np.float64)) / 32.0)
    tpos = (np.arange(S) - shift).astype(np.float64)
    ang = inv[:, None].astype(np.float32).astype(np.float64) * tpos[None, :]
    ang = ang.astype(np.float32).astype(np.float64)
    c["cosT"] = np.cos(ang).astype(np.float32)
    sg = np.where(dd < 32, -1.0, 1.0)[:, None]
    c["sinT"] = (np.sin(ang) * sg).astype(np.float32)
    vm = np.ones((128, 32), np.float32)
    if half == 0:
        vm[:, :16] = 0.0
    c["vmask"] = vm
    c["hmask"] = (m[:, None] <= m[None, :]).astype(np.float32).astype(bf)
    seg = np.ones((128, 512), np.float32)
    seg[:, ::128] = 0.0
    c["segm"] = seg
    r = np.arange(128)[:, None]
    qi = np.arange(512)[None, :]
    wb = np.zeros((8, 128, 512), np.float32)
    cb = np.zeros((4, 128, 512), np.float32)
    for j in range(8):
        kpos = -512 + 128 * j + r
        dlt = qi - kpos
        wb[j] = np.where((dlt >= 0) & (dlt < 512), 0.0, NEG)
    for j in range(4):
        kpos = 128 * j + r
        cb[j] = np.where(kpos <= qi, 0.0, NEG)
    c["wband"] = np.ascontiguousarray(wb.transpose(1, 0, 2)).astype(bf)
    c["causb"] = np.ascontiguousarray(cb.transpose(1, 0, 2)).astype(bf)
    wb4 = wb.copy()
    if half == 0:
        wb4[0:4] = NEG
    c["wband4"] = np.ascontiguousarray(wb4.transpose(1, 0, 2)).astype(bf)
    cm = np.zeros((8, 128, 512), np.float32)
    for qb in range(8):
        ct = 0 if qb < 4 else 1
        cc = 128 * ct + r
        qpos = 512 * qb + qi - shift
        tc = cc - shift // 16
        cm[qb] = np.where((16 * tc + 31 <= qpos) & (cc < 255) & (tc >= 0), 0.0, NEG)
    c["cmpb"] = np.ascontiguousarray(cm.transpose(1, 0, 2)).astype(bf)
    c["cmpb0"] = np.full((128, 512), NEG if half == 0 else 0.0, np.float32).astype(bf)
    ek = np.zeros((64, 32, 128), np.float32)
    for kt in range(32):
        ek[2 * kt, kt, :64] = 1.0
        ek[2 * kt + 1, kt, 64:] = 1.0
    c["ekt"] = np.concatenate([ek, ek], axis=0).astype(bf)
    add = np.zeros((128, 32, 64), np.float32)
    for qt in range(32):
        pos = 128 * qt + np.arange(128) - shift
        cur = pos // 64
        j = np.arange(64)[None, :] - shift // 64
        forced = (j == 0) | (j == cur[:, None]) | (j == cur[:, None] - 1)
        avail = (j <= cur[:, None]) & (j >= 0)
        add[:, qt, :] = np.where(avail & forced, 1e30, np.where(avail, 0.0, -1e30))
    c["seladd"] = add
    cs = np.arange(256)[:, None] * 16
    ss = np.arange(64)[None, :] * 64
    ov = np.clip(np.minimum(cs + 32, ss + 64) - np.maximum(cs, ss), 0, None).astype(np.float32) / 32.0
    ov[255] = 0.0
    c["ovl"] = np.ascontiguousarray(ov.reshape(2, 128, 64).transpose(1, 0, 2)).astype(bf)
    return c


CONST_SHAPES = {
    "ident": ([128, 128], BF16), "onesf": ([128, 128], F32), "isel0": ([128, 128], BF16), "isel1": ([128, 128], BF16),
    "pswap": ([128, 128], BF16), "cosT": ([128, S], F32), "sinT": ([128, S], F32), "hmask": ([128, 128], BF16),
    "segm": ([128, 512], F32), "wband": ([128, 8, 512], BF16), "causb": ([128, 4, 512], BF16),
    "cmpb": ([128, 8, 512], BF16), "ekt": ([128, 32, 128], BF16), "seladd": ([128, 32, 64], F32),
    "ovl": ([128, 2, 64], BF16), "vmask": ([128, 32], F32), "wband4": ([128, 8, 512], BF16), "cmpb0": ([128, 512], BF16),
}

IN_SHAPES = {
    "xT": [D, S], "xTo": [D, SO], "cT": [128, 8], "w_ada": [D, 6 * D], "b_ada": [1, 6 * D], "n1g": [128, 8],
    "w_in": [D, 3352], "lbl": [128, 2, 4], "hng": [128, 512],
    "peTk": [64, 32], "w1k": [64, 32, 256], "b1k": [128, 2], "w2k": [128, 2, 64],
    "peTv": [64, 32], "w1v": [64, 32, 256], "b1v": [128, 2], "w2v": [128, 2, 64],
    "w_out": [D, D], "n2g": [128, 8], "rw": [128, 8, 64], "rbias": [128, 64],
    "wgu": [65, D, 512], "wdn": [65, 256, D], "fg": [128, 8],
}


class _SkipNSA(Exception):
    pass


class _NSAScope(ExitStack):
    def __exit__(self, et, ev, tb):
        super().__exit__(None, None, None)
        return et is _SkipNSA


def build(stop_after=None, dbg=(), with_moe=True, enable_nsa=True, n_experts=65):
    nc = bass.Bass("TRN2", target_bir_lowering=False)
    I = {}
    for k, shp in IN_SHAPES.items():
        if not with_moe and k in ("wgu", "wdn"):
            continue
        I[k] = nc.dram_tensor(k, list(shp), F32, kind="ExternalInput").ap()
    for k, (shp, dt) in CONST_SHAPES.items():
        I[k] = nc.dram_tensor(k, list(shp), dt, kind="ExternalInput").ap()
    outT = nc.dram_tensor("outT", [D, SO], F32, kind="ExternalOutput").ap()
    dbg_out = {}
    with ExitStack() as es:
        kb = KB(nc, es)
        E = es.enter_context

        uid = [0]

        def sb(name, shape, dt=F32, stack=None):
            uid[0] += 1
            return (stack or es).enter_context(nc.sbuf_tensor(f"sb{uid[0]}_" + name, list(shape), dt))

        ps = [E(nc.psum_tensor(f"ps{i}", [128, 512], F32)) for i in range(8)]
        PS = [Buf(p[:], excl=True) for p in ps]

        def dump(name, ap, shape, dt=F32):
            t = nc.dram_tensor("dbg_" + name, list(shape), dt, kind="ExternalOutput").ap()
            dbg_out[name] = t
            kb.barrier()
            kb.dma("sync", t, ap)
            kb.barrier()

        ident = sb("ident", [128, 128], BF16)
        onesf = sb("onesf", [128, 128], F32)
        isel0 = sb("isel0", [128, 128], BF16)
        isel1 = sb("isel1", [128, 128], BF16)
        pswap = sb("pswap", [128, 128], BF16)
        hmask = sb("hmask", [128, 128], BF16)
        segm = sb("segm", [128, 512], F32)
        CST = Buf(ident[:])
        for nm, t in (("ident", ident), ("onesf", onesf), ("isel0", isel0), ("isel1", isel1), ("pswap", pswap),
                      ("hmask", hmask), ("segm", segm)):
            kb.dma("sync", t[:], I[nm], outs=[CST])
        modcol = sb("modcol", [128, 48], F32)
        a1 = sb("a1", [128, 8], F32)
        a2 = sb("a2", [128, 8], F32)
        MOD = Buf(modcol[:])
        oT = sb("oT", [128, 8, SO], BF16)
        OT = [[Buf(oT[:, j, s * 128:(s + 1) * 128]) for s in range(16)] for j in range(8)]

        with ExitStack() as p0:
            cT = sb("cT", [128, 8], F32, p0)
            cs = sb("cs", [128, 8], F32, p0)
            bada = sb("bada", [1, 6 * D], F32, p0)
            modrow = sb("modrow", [1, 6 * D], F32, p0)
            one1 = sb("one1", [1, 1], F32, p0)
            n1g = sb("n1g", [128, 8], F32, p0)
            n2g = sb("n2g", [128, 8], F32, p0)
            wab = [sb(f"wab{i}", [128, 8, 512], F32, p0) for i in range(2)]
            WAB = [Buf(w[:]) for w in wab]
            SM = Buf(cT[:])
            MR = Buf(modrow[:])
            kb.dma("sync", cT[:], I["cT"], outs=[SM])
            kb.dma("sync", bada[:], I["b_ada"], outs=[SM])
            kb.dma("sync", n1g[:], I["n1g"], outs=[SM])
            kb.dma("sync", n2g[:], I["n2g"], outs=[SM])
            kb.op("vector", lambda e: e.memset(one1[:], 1.0), outs=[SM])
            kb.op("scalar", lambda e: e.activation(out=cs[:], in_=cT[:], func=AF.Silu), outs=[SM], ins=[SM])
            wada_v = I["w_ada"].rearrange("(k p) c -> p k c", p=128)
            for cb in range(12):
                W = WAB[cb % 2]
                kb.dma("sync" if cb % 2 == 0 else "gpsimd", wab[cb % 2][:], wada_v[:, :, cb * 512:(cb + 1) * 512], outs=[W])
                P = PS[cb % 2]
                for k in range(8):
                    kb.op("tensor", lambda e, k=k, cb=cb: e.matmul(ps[cb % 2][0:1, :], lhsT=cs[:, k:k + 1], rhs=wab[cb % 2][:, k, :],
                                                                 start=(k == 0), stop=(k == 7)),
                          outs=[P], ins=[SM, W], mark=(k == 7))
                kb.op("vector", lambda e, cb=cb: e.tensor_tensor(out=modrow[0:1, cb * 512:(cb + 1) * 512], in0=ps[cb % 2][0:1, :],
                                                                  in1=bada[0:1, cb * 512:(cb + 1) * 512], op=ALU.add),
                      outs=[MR], ins=[P, SM])
            P = PS[2]
            for j in range(48):
                kb.op("tensor", lambda e, j=j: e.matmul(ps[2][:, j:j + 1], lhsT=modrow[0:1, j * 128:(j + 1) * 128], rhs=one1[0:1, 0:1],
                                                       start=True, stop=True), outs=[P], ins=[MR, SM], mark=(j == 47))
            kb.op("vector", lambda e: e.tensor_copy(out=modcol[:], in_=ps[2][:, 0:48]), outs=[MOD], ins=[P])
            kb.op("vector", lambda e: e.scalar_tensor_tensor(out=a1[:], in0=modcol[:, 8:16], scalar=1.0, in1=n1g[:], op0=ALU.add, op1=ALU.mult),
                  outs=[MOD], ins=[MOD, SM])
            kb.op("vector", lambda e: e.scalar_tensor_tensor(out=a2[:], in0=modcol[:, 32:40], scalar=1.0, in1=n2g[:], op0=ALU.add, op1=ALU.mult),
                  outs=[MOD], ins=[MOD, SM])
            if "mod" in dbg:
                dump("mod", modcol[:], [128, 48])
            kb.barrier()
        sh1 = lambda k: modcol[:, k:k + 1]
        g1c = lambda k: modcol[:, 16 + k:17 + k]
        sh2 = lambda k: modcol[:, 24 + k:25 + k]
        g2c = lambda k: modcol[:, 40 + k:41 + k]

        if stop_after == "p0":
            kb.barrier()
            return nc, dbg_out

        with ExitStack() as p1:
            hT = sb("hT", [128, 8, S], BF16, p1)
            HT = [Buf(hT[:, :, n * 512:(n + 1) * 512]) for n in range(NB)]
            with ExitStack() as p1a:
                xb = [sb(f"xb{i}", [128, 8, 512], F32, p1a) for i in range(2)]
                XB = [Buf(t[:]) for t in xb]
                sq = sb("sq", [128, 8, 512], F32, p1a)
                SQ = Buf(sq[:])
                rstd = sb("rstd", [128, 512], F32, p1a)
                RS = Buf(rstd[:])
                tmp = [sb(f"tmp{i}", [128, 512], F32, p1a) for i in range(2)]
                TMP = [Buf(t[:]) for t in tmp]
                xT_v = I["xT"].rearrange("(k p) t -> p k t", p=128)
                for n in range(NB):
                    X = XB[n % 2]
                    x_ = xb[n % 2]
                    kb.dma("sync" if n % 2 == 0 else "gpsimd", x_[:], xT_v[:, :, n * 512:(n + 1) * 512], outs=[X])
                    kb.op("scalar", lambda e, x_=x_: e.activation(out=sq[:], in_=x_[:], func=AF.Square), outs=[SQ], ins=[X])
                    P = PS[n % 2]
                    for k in range(8):
                        kb.op("tensor", lambda e, k=k, n=n: e.matmul(ps[n % 2][:], lhsT=onesf[:], rhs=sq[:, k, :], start=(k == 0), stop=(k == 7)),
                              outs=[P], ins=[SQ, CST], mark=(k == 7))
                    kb.op("scalar", lambda e, n=n: e.activation(out=rstd[:], in_=ps[n % 2][:], func=AF.Sqrt, scale=1.0 / D, bias=EPS),
                          outs=[RS], ins=[P])
                    kb.op("vector", lambda e: e.reciprocal(out=rstd[:], in_=rstd[:]), outs=[RS], ins=[RS])
                    for k in range(8):
                        T = TMP[k % 2]
                        t_ = tmp[k % 2]
                        kb.op("vector", lambda e, k=k, t_=t_, x_=x_: e.tensor_tensor(out=t_[:], in0=x_[:, k, :], in1=rstd[:], op=ALU.mult),
                              outs=[T], ins=[X, RS])
                        kb.op("scalar", lambda e, k=k, t_=t_, n=n: e.activation(out=hT[:, k, n * 512:(n + 1) * 512], in_=t_[:], func=AF.Identity,
                                                                            scale=a1[:, k:k + 1], bias=sh1(k)),
                              outs=[HT[n]], ins=[T, MOD])
                kb.barrier()
            if "hT" in dbg:
                dump("hT", hT[:], [128, 8, S], BF16)
            if stop_after == "p1a":
                kb.barrier()
                return nc, dbg_out

            rr = [0]

            def bank():
                rr[0] = (rr[0] + 1) % 8
                return rr[0]

            w_in_v = I["w_in"].rearrange("(k p) c -> p k c", p=128)

            with ExitStack() as ph:
                lbl = sb("lbl", [128, 2, 4], F32, ph)
                lb = sb("lb", [128, 4], F32, ph)
                oml = sb("oml", [128, 4], F32, ph)
                hng = sb("hng", [128, 512], F32, ph)
                HC = Buf(lbl[:])
                kb.dma("sync", lbl[:], I["lbl"], outs=[HC])
                kb.dma("sync", hng[:], I["hng"], outs=[HC])
                kb.op("vector", lambda e: e.tensor_tensor(out=lb[:], in0=lbl[:, 0, :], in1=lbl[:, 1, :], op=ALU.subtract), outs=[HC], ins=[HC])
                kb.op("scalar", lambda e: e.activation(out=lb[:], in_=lb[:], func=AF.Sigmoid), outs=[HC], ins=[HC])
                kb.op("vector", lambda e: e.tensor_scalar(out=oml[:], in0=lb[:], scalar1=-1.0, scalar2=1.0, op0=ALU.mult, op1=ALU.add), outs=[HC], ins=[HC])
                wq = sb("wq", [128, 8, 128], BF16, ph)
                wf = sb("wf", [128, 8, 128], BF16, ph)
                wig = sb("wig", [128, 8, 256], BF16, ph)
                WQ, WF, WIG = Buf(wq[:]), Buf(wf[:]), Buf(wig[:])
                Q1 = sb("Q1", [128, S], BF16, ph)
                Q2 = sb("Q2", [128, S], BF16, ph)
                Kt = sb("Kt", [128, S], BF16, ph)
                Kh = sb("Kh", [128, NT, 128], BF16, ph)
                Vh = sb("Vh", [128, NT, 128], BF16, ph)
                SGt = sb("SGt", [128, NT, 128], BF16, ph)
                ebl = sb("ebl", [128, NT], F32, ph)
                BQ = [Buf(Q1[:, n * 512:(n + 1) * 512]) for n in range(NB)]
                BKH = [Buf(Kh[:, t, :]) for t in range(NT)]
                BV = [Buf(Vh[:, t, :]) for t in range(NT)]
                tn = ["f", "lf", "b", "d1", "d2", "eb", "e1", "en1", "el", "k"]
                T_ = {n_: sb("t_" + n_, [128, 512], F32, ph) for n_ in tn}
                TB = {n_: Buf(T_[n_][:]) for n_ in tn}
                khtb = sb("khtb", [128, 512], BF16, ph)
                KHTB = Buf(khtb[:])
                vmask = sb("vmask", [128, 32], F32, ph)
                kb.dma("sync", vmask[:], I["vmask"], outs=[HC])
                Sst = sb("Sst", [128, 128], F32, ph)
                SST = Buf(Sst[:])
                sbf = [sb(f"sbf{i}", [128, 128], BF16, ph) for i in range(2)]
                SBF = [Buf(t[:]) for t in sbf]
                atm = [sb(f"atm{i}", [128, 128], BF16, ph) for i in range(2)]
                ATM = [Buf(t[:]) for t in atm]
                for i in range(2):
                    kb.op("vector", lambda e, i=i: e.memset(atm[i][:], 0.0), outs=[ATM[i]])
                junk = sb("junk", [128, 128], F32, ph)
                JK = Buf(junk[:])
                ssq = [sb(f"ssq{i}", [128, 1], F32, ph) for i in range(2)]
                SSQ = [Buf(t[:]) for t in ssq]
                of = [sb(f"of{i}", [128, 128], F32, ph) for i in range(2)]
                OF = [Buf(t[:]) for t in of]
                obf = [sb(f"obf{i}", [128, 128], BF16, ph) for i in range(2)]
                OBF = [Buf(t[:]) for t in obf]
                v4 = lambda ap: ap.rearrange("p (c t) -> p c t", t=128)
                for hd in range(int(_os0.environ.get("NHEADS", "4"))):
                    c0 = hd * 128
                    kb.dma("gpsimd", wq[:], w_in_v[:, :, c0:c0 + 128], outs=[WQ])
                    kb.dma("gpsimd", wf[:], w_in_v[:, :, 512 + c0:512 + c0 + 128], outs=[WF])
                    kb.dma("gpsimd", wig[:, :, 0:128], w_in_v[:, :, 1024 + c0:1024 + c0 + 128], outs=[WIG])
                    kb.dma("gpsimd", wig[:, :, 128:256], w_in_v[:, :, 1536 + c0:1536 + c0 + 128], outs=[WIG])
                    for n in range(NB):
                        sl = slice(n * 512, (n + 1) * 512)
                        bq_, bf_ = bank(), bank()
                        for k in range(8):
                            kb.op("tensor", lambda e, k=k, bq_=bq_, sl=sl: e.matmul(ps[bq_][:], lhsT=wq[:, k, :], rhs=hT[:, k, sl], start=(k == 0), stop=(k == 7)),
                                  outs=[PS[bq_]], ins=[WQ, HT[n]], mark=(k == 7))
                        for k in range(8):
                            kb.op("tensor", lambda e, k=k, bf_=bf_, sl=sl: e.matmul(ps[bf_][:], lhsT=wf[:, k, :], rhs=hT[:, k, sl], start=(k == 0), stop=(k == 7)),
                                  outs=[PS[bf_]], ins=[WF, HT[n]], mark=(k == 7))
                        t = T_
                        kb.op("scalar", lambda e, bf_=bf_: e.activation(out=t["f"][:], in_=ps[bf_][:], func=AF.Sigmoid), outs=[TB["f"]], ins=[PS[bf_]])
                        kb.op("vector", lambda e, hd=hd: e.tensor_scalar(out=t["f"][:], in0=t["f"][:], scalar1=oml[:, hd:hd + 1], scalar2=lb[:, hd:hd + 1],
                                                                     op0=ALU.mult, op1=ALU.add), outs=[TB["f"]], ins=[TB["f"], HC])
                        kb.op("scalar", lambda e: e.activation(out=t["lf"][:], in_=t["f"][:], func=AF.Ln), outs=[TB["lf"]], ins=[TB["f"]])
                        kb.op("gpsimd", lambda e: e.tensor_scalar(out=t["k"][:], in0=t["f"][:], scalar1=-1.0, scalar2=1.0, op0=ALU.mult, op1=ALU.add),
                              outs=[TB["k"]], ins=[TB["f"]])
                        kb.op("vector", lambda e: e.tensor_tensor_scan(out=t["b"][:], data0=segm[:], data1=t["lf"][:], initial=0.0, op0=ALU.mult, op1=ALU.add),
                              outs=[TB["b"]], ins=[TB["lf"], CST])
                        kb.op("vector", lambda e: e.tensor_tensor(out=v4(t["d1"][:]), in0=v4(t["b"][:]), in1=v4(t["b"][:])[:, :, 63:64].to_broadcast([128, 4, 128]),
                                                                  op=ALU.subtract), outs=[TB["d1"]], ins=[TB["b"]])
                        kb.op("vector", lambda e: e.tensor_tensor(out=v4(t["d2"][:]), in0=v4(t["b"][:])[:, :, 127:128].to_broadcast([128, 4, 128]), in1=v4(t["b"][:]),
                                                                  op=ALU.subtract), outs=[TB["d2"]], ins=[TB["b"]])
                        kb.op("scalar", lambda e: e.activation(out=t["eb"][:], in_=t["b"][:], func=AF.Exp), outs=[TB["eb"]], ins=[TB["b"]])
                        kb.op("scalar", lambda e: e.activation(out=t["e1"][:], in_=t["d1"][:], func=AF.Exp), outs=[TB["e1"]], ins=[TB["d1"]])
                        kb.op("scalar", lambda e: e.activation(out=t["en1"][:], in_=t["d1"][:], func=AF.Exp, scale=-1.0), outs=[TB["en1"]], ins=[TB["d1"]])
                        kb.op("scalar", lambda e: e.activation(out=t["el"][:], in_=t["d2"][:], func=AF.Exp), outs=[TB["el"]], ins=[TB["d2"]])
                        sc_ = 128.0 ** -0.5
                        kb.op("vector", lambda e, bq_=bq_, sl=sl: e.scalar_tensor_tensor(out=Q1[:, sl], in0=ps[bq_][:], scalar=sc_, in1=t["e1"][:], op0=ALU.mult, op1=ALU.mult),
                              outs=[BQ[n]], ins=[PS[bq_], TB["e1"]])
                        kb.op("vector", lambda e, bq_=bq_, sl=sl: e.scalar_tensor_tensor(out=Q2[:, sl], in0=ps[bq_][:], scalar=sc_, in1=t["eb"][:], op0=ALU.mult, op1=ALU.mult),
                              outs=[BQ[n]], ins=[PS[bq_], TB["eb"]])
                        kb.op("gpsimd", lambda e, sl=sl: e.tensor_tensor(out=Kt[:, sl], in0=t["k"][:], in1=t["en1"][:], op=ALU.mult), outs=[BQ[n]], ins=[TB["k"], TB["en1"]])
                        kb.op("gpsimd", lambda e: e.tensor_tensor(out=khtb[:], in0=t["k"][:], in1=t["el"][:], op=ALU.mult), outs=[KHTB], ins=[TB["k"], TB["el"]])
                        kb.op("gpsimd", lambda e, n=n: e.tensor_copy(out=ebl[:, 4 * n:4 * n + 4], in_=v4(t["eb"][:])[:, :, 127]), outs=[BQ[n]], ins=[TB["eb"]])
                        for i in range(4):
                            bk = bank()
                            kb.op("tensor", lambda e, i=i, bk=bk: e.matmul(ps[bk][:, 0:128], lhsT=khtb[:, i * 128:(i + 1) * 128], rhs=ident[:], start=True, stop=True),
                                  outs=[PS[bk]], ins=[KHTB, CST])
                            kb.op("scalar", lambda e, i=i, bk=bk, n=n: e.copy(out=Kh[:, 4 * n + i, :], in_=ps[bk][:, 0:128]), outs=[BKH[4 * n + i]], ins=[PS[bk]])
                    for tt in range(NT):
                        bk = bank()
                        n = tt // 4
                        for k in range(8):
                            kb.op("tensor", lambda e, k=k, bk=bk, tt=tt: e.matmul(ps[bk][:, 0:256], lhsT=hT[:, k, tt * 128:(tt + 1) * 128], rhs=wig[:, k, :],
                                                                                 start=(k == 0), stop=(k == 7)),
                                  outs=[PS[bk]], ins=[WIG, HT[n]], mark=(k == 7))
                        kb.op("vector", lambda e, bk=bk, tt=tt: e.tensor_scalar(out=Vh[:, tt, :], in0=ps[bk][:, 0:128], scalar1=vmask[:, tt:tt + 1], scalar2=None, op0=ALU.mult),
                              outs=[BV[tt]], ins=[PS[bk], HC])
                        kb.op("scalar", lambda e, bk=bk, tt=tt: e.activation(out=SGt[:, tt, :], in_=ps[bk][:, 128:256], func=AF.Silu), outs=[BV[tt]], ins=[PS[bk]])
                    kb.op("vector", lambda e: e.memset(Sst[:], 0.0), outs=[SST])
                    at_bank = {}

                    def emit_at(c):
                        bk = bank()
                        at_bank[c] = bk
                        cs_ = slice(c * 128, (c + 1) * 128)
                        c0_ = c * 128
                        kb.op("tensor", lambda e: e.matmul(ps[bk][0:64, 0:64], lhsT=Kt[:, c0_:c0_ + 64], rhs=Q1[:, c0_:c0_ + 64], start=True, stop=True),
                              outs=[PS[bk]], ins=[BQ[c // 4]], mark=False)
                        kb.op("tensor", lambda e: e.matmul(ps[bk][:, 64:128], lhsT=Kt[:, cs_], rhs=Q1[:, c0_ + 64:c0_ + 128], start=True, stop=True),
                              outs=[PS[bk]], ins=[BQ[c // 4]])
                        kb.op("vector", lambda e: e.tensor_tensor(out=atm[c % 2][0:64, 0:64], in0=ps[bk][0:64, 0:64], in1=hmask[0:64, 0:64], op=ALU.mult),
                              outs=[ATM[c % 2]], ins=[PS[bk], CST])
                        kb.op("vector", lambda e: e.tensor_tensor(out=atm[c % 2][:, 64:128], in0=ps[bk][:, 64:128], in1=hmask[:, 64:128], op=ALU.mult),
                              outs=[ATM[c % 2]], ins=[PS[bk], CST])

                    emit_at(0)
                    for c in range(NT):
                        if c + 1 < NT:
                            emit_at(c + 1)
                        cs_ = slice(c * 128, (c + 1) * 128)
                        bd, bo = bank(), bank()
                        kb.op("tensor", lambda e, bd=bd, c=c: e.matmul(ps[bd][:, 0:128], lhsT=Kh[:, c, :], rhs=Vh[:, c, :], start=True, stop=True),
                              outs=[PS[bd]], ins=[BKH[c], BV[c]])
                        kb.op("tensor", lambda e, bo=bo, c=c: e.matmul(ps[bo][:, 0:128], lhsT=atm[c % 2][:], rhs=Vh[:, c, :], start=True, stop=(c == 0)),
                              outs=[PS[bo]], ins=[ATM[c % 2], BV[c]], mark=(c == 0))
                        if c > 0:
                            kb.op("tensor", lambda e, bo=bo, c=c, cs_=cs_: e.matmul(ps[bo][:, 0:128], lhsT=Q2[:, cs_], rhs=sbf[(c - 1) % 2][:], start=False, stop=True),
                                  outs=[PS[bo]], ins=[BQ[c // 4], SBF[(c - 1) % 2]])
                        if c + 1 < NT:
                            kb.op("vector", lambda e, bd=bd, c=c: e.scalar_tensor_tensor(out=Sst[:], in0=Sst[:], scalar=ebl[:, c:c + 1], in1=ps[bd][:, 0:128],
                                                                                     op0=ALU.mult, op1=ALU.add), outs=[SST], ins=[SST, PS[bd], BQ[c // 4]])
                            kb.op("scalar", lambda e, c=c: e.copy(out=sbf[c % 2][:], in_=Sst[:]), outs=[SBF[c % 2]], ins=[SST])
                        if c < 16:
                            continue
                        i2 = c % 2
                        kb.op("gpsimd", lambda e, i2=i2: e.memset(ssq[i2][:], 0.0), outs=[SSQ[i2]])
                        kb.op("scalar", lambda e, bo=bo, i2=i2: e.activation(out=junk[:], in_=ps[bo][:, 0:128], func=AF.Square, accum_out=ssq[i2][:]),
                              outs=[JK, SSQ[i2]], ins=[PS[bo]])
                        kb.op("scalar", lambda e, i2=i2: e.activation(out=ssq[i2][:], in_=ssq[i2][:], func=AF.Sqrt, scale=1.0 / 128, bias=EPS), outs=[SSQ[i2]], ins=[SSQ[i2]])
                        kb.op("vector", lambda e, i2=i2: e.reciprocal(out=ssq[i2][:], in_=ssq[i2][:]), outs=[SSQ[i2]], ins=[SSQ[i2]])
                        kb.op("vector", lambda e, bo=bo, i2=i2, c0=c0: e.scalar_tensor_tensor(out=of[i2][:], in0=ps[bo][:, 0:128], scalar=ssq[i2][:, 0:1], in1=hng[:, c0:c0 + 128],
                                                                                           op0=ALU.mult, op1=ALU.mult), outs=[OF[i2]], ins=[PS[bo], SSQ[i2], HC])
                        kb.op("gpsimd", lambda e, i2=i2, c=c: e.tensor_tensor(out=obf[i2][:], in0=of[i2][:], in1=SGt[:, c, :], op=ALU.mult), outs=[OBF[i2]], ins=[OF[i2], BV[c]])
                        bt = bank()
                        kb.op("tensor", lambda e, bt=bt, i2=i2: e.matmul(ps[bt][:, 0:128], lhsT=obf[i2][:], rhs=ident[:], start=True, stop=True),
                              outs=[PS[bt]], ins=[OBF[i2], CST])
                        s_ = c - 16
                        kb.op("scalar", lambda e, bt=bt, s_=s_, hd=hd: e.copy(out=oT[:, hd, s_ * 128:(s_ + 1) * 128], in_=ps[bt][:, 0:128]), outs=[OT[hd][s_]], ins=[PS[bt]])
                kb.barrier()
            if "oT" in dbg:
                dump("oT", oT[:], [128, 8, SO], BF16)
            if stop_after == "p1b":
                kb.barrier()
                return nc, dbg_out

            SCL = 64.0 ** -0.5
            if not enable_nsa:
                for jf in range(4, 8):
                    kb.op("vector", lambda e, jf=jf: e.memset(oT[:, jf, :], 0.0), outs=OT[jf])
            with _NSAScope() as pn:
                if not enable_nsa:
                    raise _SkipNSA()
                ovl = sb("ovl", [128, 2, 64], BF16, pn)
                kb.dma("sync", ovl[:], I["ovl"], outs=[CST])
                KEe = sb("KEe", [128, S], BF16, pn)
                KEo = sb("KEo", [128, S], BF16, pn)
                ekt_v = I["ekt"].rearrange("p a b -> p (a b)")
                kb.dma("sync", KEe[64:128, :], ekt_v[64:128, :], outs=[CST])
                kb.dma("sync", KEo[0:64, :], ekt_v[0:64, :], outs=[CST])
                kwT = sb("kwT", [128, S], BF16, pn)
                kcvT = sb("kcvT", [128, S], BF16, pn)
                vs1 = sb("vs1", [128, NT, 80], BF16, pn)
                vw1 = sb("vw1", [128, NT, 80], BF16, pn)
                KS = Buf(kwT[:])
                kcmpT = sb("kcmpT", [128, 256], BF16, pn)
                vcmp1 = sb("vcmp1", [128, 2, 144], BF16, pn)
                KC = Buf(kcmpT[:])
                wk3 = sb("wk3", [128, 8, 384], BF16, pn)
                wv2 = sb("wv2", [128, 8, 128], BF16, pn)
                wqg = sb("wqg", [128, 8, 256], BF16, pn)
                wgt = sb("wgt", [128, 8, 12], BF16, pn)
                WN = Buf(wk3[:])
                cosb = sb("cosb", [128, 512], F32, pn)
                sinb = sb("sinb", [128, 512], F32, pn)
                CSB = Buf(cosb[:])
                rawb = sb("rawb", [128, 512], BF16, pn)
                RAWB = Buf(rawb[:])
                rt1 = sb("rt1", [128, 512], F32, pn)
                rt2 = sb("rt2", [128, 512], F32, pn)
                RT1, RT2 = Buf(rt1[:]), Buf(rt2[:])
                _padn = int(_os0.environ.get("PADN", "0"))
                if _padn:
                    _pad = sb("padn", [128, _padn], F32, pn)
                srr = [0]

                def sbank():
                    srr[0] = (srr[0] + 1) % 3
                    return srr[0]

                mrr = [0]

                def mbank():
                    return 7

                import os as _os
                _dbgmode = int(_os.environ.get("ROPEDBG", "0"))

                def rope_from(bk, dst_ap, dstbuf):
                    if _dbgmode == 1:
                        kb.op("scalar", lambda e: e.copy(out=dst_ap, in_=ps[bk][:]), outs=[dstbuf], ins=[PS[bk]])
                        return
                    if _dbgmode == 3:
                        kb.op("vector", lambda e: e.tensor_tensor(out=rt1[:], in0=ps[bk][:], in1=cosb[:], op=ALU.mult), outs=[RT1], ins=[PS[bk], CSB])
                        kb.op("gpsimd", lambda e: e.tensor_copy(out=dst_ap, in_=rt1[:]), outs=[dstbuf], ins=[RT1])
                        return
                    if _dbgmode == 4:
                        kb.op("scalar", lambda e: e.copy(out=rawb[:], in_=ps[bk][:]), outs=[RAWB], ins=[PS[bk]])
                        b2 = mbank()
                        kb.op("tensor", lambda e: e.matmul(ps[b2][:], lhsT=pswap[:], rhs=rawb[:], start=True, stop=True), outs=[PS[b2]], ins=[RAWB, CST])
                        kb.op("vector", lambda e: e.tensor_tensor(out=rt1[:], in0=ps[bk][:], in1=cosb[:], op=ALU.mult), outs=[RT1], ins=[PS[bk], CSB])
                        kb.op("vector", lambda e: e.tensor_tensor(out=rt2[:], in0=ps[b2][:], in1=sinb[:], op=ALU.mult), outs=[RT2], ins=[PS[b2], CSB])
                        kb.op("vector", lambda e: e.tensor_tensor(out=dst_ap, in0=rt1[:], in1=rt2[:], op=ALU.add), outs=[dstbuf], ins=[RT1, RT2])
                        return
                    if _dbgmode == 5:
                        kb.op("scalar", lambda e: e.copy(out=rawb[:], in_=ps[bk][:]), outs=[RAWB], ins=[PS[bk]])
                        b2 = mbank()
                        kb.op("tensor", lambda e: e.matmul(ps[b2][:], lhsT=pswap[:], rhs=rawb[:], start=True, stop=True), outs=[PS[b2]], ins=[RAWB, CST])
                        kb.op("vector", lambda e: e.tensor_tensor(out=rt1[:], in0=ps[bk][:], in1=cosb[:], op=ALU.mult), outs=[RT1], ins=[PS[bk], CSB])
                        kb.op("scalar", lambda e: e.copy(out=rt2[:], in_=ps[b2][:]), outs=[RT2], ins=[PS[b2]])
                        _sb = cosb if _os.environ.get("USECOS") else sinb
                        kb.op("vector", lambda e: e.tensor_tensor(out=rt2[:], in0=rt2[:], in1=_sb[:], op=ALU.mult), outs=[RT2], ins=[RT2, CSB])
                        kb.op("vector", lambda e: e.tensor_tensor(out=dst_ap, in0=rt1[:], in1=rt2[:], op=ALU.add), outs=[dstbuf], ins=[RT1, RT2])
                        return
                    if _dbgmode in (7, 8):
                        kb.op("scalar", lambda e: e.copy(out=rawb[:], in_=ps[bk][:]), outs=[RAWB], ins=[PS[bk]])
                        b2 = mbank()
                        kb.op("tensor", lambda e: e.matmul(ps[b2][:], lhsT=pswap[:], rhs=rawb[:], start=True, stop=True), outs=[PS[b2]], ins=[RAWB, CST])
                        kb.op("vector", lambda e: e.tensor_tensor(out=rt1[:], in0=ps[bk][:], in1=cosb[:], op=ALU.mult), outs=[RT1], ins=[PS[bk], CSB])
                        kb.op("scalar", lambda e: e.copy(out=rt2[:], in_=ps[b2][:]), outs=[RT2], ins=[PS[b2]])
                        kb.op("vector", lambda e: e.tensor_tensor(out=rt2[:], in0=rt2[:], in1=sinb[:], op=ALU.mult), outs=[RT2], ins=[RT2, CSB])
                        if _dbgmode == 8:
                            kb.op("vector", lambda e: e.tensor_tensor(out=rt1[:], in0=rt1[:], in1=rt2[:], op=ALU.add), outs=[RT1], ins=[RT1, RT2])
                        kb.op("scalar", lambda e: e.copy(out=dst_ap, in_=rt1[:]), outs=[dstbuf], ins=[RT1])
                        return
                    if _dbgmode in (9, 10):
                        kb.op("vector", lambda e: e.tensor_tensor(out=rt1[:], in0=ps[bk][:], in1=cosb[:], op=ALU.mult), outs=[RT1], ins=[PS[bk], CSB])
                        if _dbgmode == 9:
                            kb.op("scalar", lambda e: e.copy(out=rt2[:], in_=ps[bk][:]), outs=[RT2], ins=[PS[bk]])
                        else:
                            kb.op("vector", lambda e: e.tensor_tensor(out=rt2[:], in0=rt1[:], in1=cosb[:], op=ALU.mult), outs=[RT2], ins=[RT1, CSB])
                        kb.op("gpsimd", lambda e: e.tensor_copy(out=dst_ap, in_=rt1[:]), outs=[dstbuf], ins=[RT1])
                        return
                    if _dbgmode in (11, 12):
                        kb.op("scalar", lambda e: e.copy(out=rawb[:], in_=ps[bk][:]), outs=[RAWB], ins=[PS[bk]])
                        b2 = mbank()
                        kb.op("tensor", lambda e: e.matmul(ps[b2][:], lhsT=pswap[:], rhs=rawb[:], start=True, stop=True), outs=[PS[b2]], ins=[RAWB, CST])
                        kb.op("vector", lambda e: e.tensor_tensor(out=rt1[:], in0=ps[bk][:], in1=cosb[:], op=ALU.mult), outs=[RT1], ins=[PS[bk], CSB, RAWB])
                        kb.op("gpsimd", lambda e: e.tensor_copy(out=dst_ap, in_=rt1[:]), outs=[dstbuf], ins=[RT1])
                        if _dbgmode == 12:
                            return
                        kb.op("vector", lambda e: e.tensor_tensor(out=rt1[:], in0=ps[b2][:], in1=sinb[:], op=ALU.mult), outs=[RT1], ins=[PS[b2], CSB])
                        kb.op("gpsimd", lambda e: e.tensor_tensor(out=dst_ap, in0=dst_ap, in1=rt1[:], op=ALU.add), outs=[dstbuf], ins=[RT1, dstbuf])
                        return
                    if _dbgmode == 2:
                        kb.op("scalar", lambda e: e.copy(out=rawb[:], in_=ps[bk][:]), outs=[RAWB], ins=[PS[bk]])
                        b2 = mbank()
                        kb.op("tensor", lambda e: e.matmul(ps[b2][:], lhsT=pswap[:], rhs=rawb[:], start=True, stop=True), outs=[PS[b2]], ins=[RAWB, CST])
                        kb.op("scalar", lambda e: e.copy(out=dst_ap, in_=ps[b2][:]), outs=[dstbuf], ins=[PS[b2]])
                        return
                    kb.op("scalar", lambda e: e.copy(out=rawb[:], in_=ps[bk][:]), outs=[RAWB], ins=[PS[bk]])
                    b2 = mbank()
                    kb.op("tensor", lambda e: e.matmul(ps[b2][:], lhsT=pswap[:], rhs=rawb[:], start=True, stop=True), outs=[PS[b2]], ins=[RAWB, CST])
                    kb.op("vector", lambda e: e.tensor_tensor(out=rt1[:], in0=ps[bk][:], in1=cosb[:], op=ALU.mult), outs=[RT1], ins=[PS[bk], CSB])
                    kb.op("vector", lambda e: e.tensor_tensor(out=rt2[:], in0=ps[b2][:], in1=sinb[:], op=ALU.mult), outs=[RT2], ins=[PS[b2], CSB])
                    if isinstance(dst_ap, tuple):
                        kb.op("gpsimd", lambda e: e.tensor_tensor(out=dst_ap[0], in0=rt1[0:64, :], in1=rt2[0:64, :], op=ALU.add), outs=[dstbuf], ins=[RT1, RT2])
                        kb.op("gpsimd", lambda e: e.tensor_tensor(out=dst_ap[1], in0=rt1[64:128, :], in1=rt2[64:128, :], op=ALU.add), outs=[dstbuf], ins=[RT1, RT2])
                    else:
                        kb.op("gpsimd", lambda e: e.tensor_tensor(out=dst_ap, in0=rt1[:], in1=rt2[:], op=ALU.add), outs=[dstbuf], ins=[RT1, RT2])

                for g in range(2):
                    for j, cbase in enumerate((2560, 2688)):
                        kb.dma("gpsimd", wk3[:, :, j * 64:(j + 1) * 64], w_in_v[:, :, cbase + g * 64:cbase + g * 64 + 64], outs=[WN])
                    for j, cbase in enumerate((2816, 2816, 3072, 3072)):
                        kb.dma("gpsimd", wk3[:, :, 128 + j * 64:128 + (j + 1) * 64], w_in_v[:, :, cbase + g * 64:cbase + g * 64 + 64], outs=[WN])
                    for j, cbase in enumerate((2944, 3200)):
                        kb.dma("gpsimd", wv2[:, :, j * 64:(j + 1) * 64], w_in_v[:, :, cbase + g * 64:cbase + g * 64 + 64], outs=[WN])
                    kb.dma("gpsimd", wqg[:], w_in_v[:, :, 2048 + g * 256:2048 + (g + 1) * 256], outs=[WN])
                    kb.dma("gpsimd", wgt[:], w_in_v[:, :, 3328 + g * 12:3328 + (g + 1) * 12], outs=[WN])
                    kb.op("vector", lambda e: e.memset(vs1[:, :, 64:65], 1.0), outs=[KS])
                    kb.op("vector", lambda e: e.memset(vw1[:, :, 64:65], 1.0), outs=[KS])
                    if stop_after == "p1c_a":
                        dump("wk3", wk3[:], [128, 8, 384], BF16)
                        return nc, dbg_out
                    for n in range(NB):
                        sl = slice(n * 512, (n + 1) * 512)
                        kb.dma("sync", cosb[:], I["cosT"][:, sl], outs=[CSB])
                        kb.dma("sync", sinb[:], I["sinT"][:, sl], outs=[CSB])
                        for j in range(3):
                            bk = sbank()
                            for k in range(8):
                                kb.op("tensor", lambda e, k=k, bk=bk, j=j: e.matmul(ps[bk][:], lhsT=wk3[:, k, j * 128:(j + 1) * 128], rhs=hT[:, k, sl],
                                                                                    start=(k == 0), stop=(k == 7)), outs=[PS[bk]], ins=[WN, HT[n]], mark=(k == 7))
                            if j == 0:
                                kb.op("scalar", lambda e, bk=bk: e.copy(out=kcvT[:, sl], in_=ps[bk][:]), outs=[KS], ins=[PS[bk]])
                            else:
                                rope_from(bk, (KEe[0:64, sl], KEo[64:128, sl]) if j == 1 else kwT[:, sl], KS)
                        if stop_after == "p1c_b":
                                return nc, dbg_out
                        for i in range(4):
                            tt = 4 * n + i
                            bk = mbank()
                            for k in range(8):
                                kb.op("tensor", lambda e, k=k, bk=bk, tt=tt: e.matmul(ps[bk][:, 0:128], lhsT=hT[:, k, tt * 128:(tt + 1) * 128], rhs=wv2[:, k, :],
                                                                                     start=(k == 0), stop=(k == 7)), outs=[PS[bk]], ins=[WN, HT[n]], mark=(k == 7))
                            kb.op("scalar", lambda e, bk=bk, tt=tt: e.copy(out=vs1[:, tt, 0:64], in_=ps[bk][:, 0:64]), outs=[KS], ins=[PS[bk]])
                            kb.op("vector", lambda e, bk=bk, tt=tt: e.tensor_copy(out=vw1[:, tt, 0:64], in_=ps[bk][:, 64:128]), outs=[KS], ins=[PS[bk]])
                    if stop_after == "p1c_k":
                        dump("kcvT", kcvT[:], [128, S], BF16)
                        dump("vs1", vs1[:], [128, NT, 80], BF16)
                        return nc, dbg_out
                    with ExitStack() as pc:
                        w1kv = sb("w1kv", [128, 32, 256], BF16, pc)
                        peT = sb("peT", [128, 32], F32, pc)
                        peTb = sb("peTb", [128, 32], BF16, pc)
                        b1kv = sb("b1kv", [128, 4], F32, pc)
                        w2k2 = sb("w2k2", [128, 2, 128], BF16, pc)
                        w2v = sb("w2v", [128, 2, 64], BF16, pc)
                        hid = sb("hid", [128, 4, 256], BF16, pc)
                        beff = sb("beff", [128, 4], F32, pc)
                        gx = sb("gx", [128, 256], F32, pc)
                        gu = sb("gu", [128, 256], F32, pc)
                        gs = sb("gs", [128, 256], F32, pc)
                        CW = Buf(w1kv[:])
                        HID = Buf(hid[:])
                        GX = Buf(gx[:])
                        kb.dma("gpsimd", w1kv[0:64], I["w1k"], outs=[CW])
                        kb.dma("gpsimd", w1kv[64:128], I["w1v"], outs=[CW])
                        kb.dma("sync", peT[0:64], I["peTk"], outs=[CW])
                        kb.dma("sync", peT[64:128], I["peTv"], outs=[CW])
                        kb.dma("sync", b1kv[:, 0:2], I["b1k"], outs=[CW])
                        kb.dma("sync", b1kv[:, 2:4], I["b1v"], outs=[CW])
                        kb.dma("gpsimd", w2k2[:, :, 0:64], I["w2k"], outs=[CW])
                        kb.dma("gpsimd", w2k2[:, :, 64:128], I["w2k"], outs=[CW])
                        kb.dma("gpsimd", w2v[:], I["w2v"], outs=[CW])
                        kb.op("vector", lambda e: e.tensor_copy(out=peTb[:], in_=peT[:]), outs=[CW], ins=[CW])
                        kb.op("vector", lambda e: e.memset(hid[:], 0.0), outs=[HID])
                        for kv in range(2):
                            p0_ = kv * 64
                            for hc in range(2):
                                bk, bb = sbank(), mbank()
                                for l in range(32):
                                    kb.op("tensor", lambda e, l=l, bk=bk, hc=hc, p0_=p0_: e.matmul(ps[bk][:, 0:255], lhsT=w1kv[p0_:p0_ + 64, l, hc * 128:(hc + 1) * 128],
                                                                                                rhs=kcvT[p0_:p0_ + 64, l:l + 16 * 254 + 1:16], start=(l == 0), stop=(l == 31)),
                                          outs=[PS[bk]], ins=[CW, KS], mark=(l == 31))
                                for l in range(32):
                                    kb.op("tensor", lambda e, l=l, bb=bb, hc=hc, p0_=p0_: e.matmul(ps[bb][:, 0:1], lhsT=w1kv[p0_:p0_ + 64, l, hc * 128:(hc + 1) * 128],
                                                                                                rhs=peTb[p0_:p0_ + 64, l:l + 1], start=(l == 0), stop=(l == 31)),
                                          outs=[PS[bb]], ins=[CW], mark=(l == 31))
                                ci = kv * 2 + hc
                                kb.op("vector", lambda e, bb=bb, ci=ci: e.tensor_tensor(out=beff[:, ci:ci + 1], in0=ps[bb][:, 0:1], in1=b1kv[:, ci:ci + 1], op=ALU.add),
                                      outs=[GX], ins=[PS[bb], CW])
                                kb.op("vector", lambda e, bk=bk, ci=ci: e.tensor_scalar(out=gx[:, 0:255], in0=ps[bk][:, 0:255], scalar1=beff[:, ci:ci + 1], scalar2=None, op0=ALU.add),
                                      outs=[GX], ins=[PS[bk], GX])
                                kb.op("vector", lambda e: e.tensor_tensor(out=gu[:, 0:255], in0=gx[:, 0:255], in1=gx[:, 0:255], op=ALU.mult), outs=[GX], ins=[GX])
                                kb.op("vector", lambda e: e.tensor_scalar(out=gu[:, 0:255], in0=gu[:, 0:255], scalar1=0.044715, scalar2=1.0, op0=ALU.mult, op1=ALU.add), outs=[GX], ins=[GX])
                                kb.op("vector", lambda e: e.tensor_tensor(out=gu[:, 0:255], in0=gu[:, 0:255], in1=gx[:, 0:255], op=ALU.mult), outs=[GX], ins=[GX])
                                kb.op("scalar", lambda e: e.activation(out=gs[:, 0:255], in_=gu[:, 0:255], func=AF.Sigmoid, scale=1.5957691216057308), outs=[GX], ins=[GX])
                                kb.op("vector", lambda e, ci=ci: e.tensor_tensor(out=hid[:, ci, 0:255], in0=gx[:, 0:255], in1=gs[:, 0:255], op=ALU.mult), outs=[HID], ins=[GX])
                        bk = sbank()
                        for hc in range(2):
                            kb.op("tensor", lambda e, hc=hc, bk=bk: e.matmul(ps[bk][:, 0:256], lhsT=w2k2[:, hc, :], rhs=hid[:, hc, :], start=(hc == 0), stop=(hc == 1)),
                                  outs=[PS[bk]], ins=[CW, HID], mark=(hc == 1))
                        kb.op("scalar", lambda e, bk=bk: e.copy(out=kcmpT[:], in_=ps[bk][:, 0:256]), outs=[KC], ins=[PS[bk]])
                        for ct in range(2):
                            bk = sbank()
                            for hc in range(2):
                                kb.op("tensor", lambda e, hc=hc, bk=bk, ct=ct: e.matmul(ps[bk][:, 0:64], lhsT=hid[:, 2 + hc, ct * 128:(ct + 1) * 128], rhs=w2v[:, hc, :],
                                                                                       start=(hc == 0), stop=(hc == 1)), outs=[PS[bk]], ins=[CW, HID], mark=(hc == 1))
                            kb.op("scalar", lambda e, bk=bk, ct=ct: e.copy(out=vcmp1[:, ct, 0:64], in_=ps[bk][:, 0:64]), outs=[KC], ins=[PS[bk]])
                        kb.op("vector", lambda e: e.memset(vcmp1[:, :, 64:65], 1.0), outs=[KC])
                        kb.op("vector", lambda e: e.tensor_copy(out=vcmp1[:, :, 65:129], in_=ovl[:]), outs=[KC], ins=[CST])
                        kb.barrier()
                    if stop_after == "p1c_c":
                        dump("kcmpT", kcmpT[:], [128, 256], BF16)
                        dump("vcmp1", vcmp1[:], [128, 2, 144], BF16)
                        return nc, dbg_out
                    with ExitStack() as pq:
                        wband = sb("wband", [128, 8, 512], BF16, pq)
                        QC = Buf(wband[:])
                        kb.dma("sync", wband[:], I["wband"], outs=[QC])
                        qn = [[sb(f"qn{ch}{par}", [128, 512], BF16, pq) for par in range(2)] for ch in range(2)]
                        QN = Buf(qn[0][0][:])
                        qTb = sb("qTb", [128, 2, 512], BF16, pq)
                        qrTb = sb("qrTb", [128, 2, 512], BF16, pq)
                        QB_ = Buf(qTb[:])
                        QRB = Buf(qrTb[:])
                        gts = sb("gts", [128, 4, 12], F32, pq)
                        GTS = Buf(gts[:])
                        cmpbb = sb("cmpbb", [128, 512], BF16, pq)
                        CMB = Buf(cmpbb[:])
                        pt = [sb(f"pt{i}", [128, 512], BF16, pq) for i in range(3)]
                        PT = [Buf(t[:]) for t in pt]
                        onsa = sb("onsa", [128, 4, 256], F32, pq)
                        ONSA = Buf(onsa[:])
                        obn = sb("obn", [128, 4, 256], BF16, pq)
                        OBN = Buf(obn[:])
                        pslc = sb("pslc", [128, 4, 64], F32, pq)
                        PSLC = Buf(pslc[:])
                        sadd = sb("sadd", [128, 64], F32, pq)
                        SADD = Buf(sadd[:])
                        score = sb("score", [128, 64], F32, pq)
                        stmp = sb("stmp", [128, 64], F32, pq)
                        sel = sb("sel", [128, 64], F32, pq)
                        m8 = sb("m8", [128, 16], F32, pq)
                        negb = sb("negb", [128, 128], BF16, pq)
                        SEL = Buf(score[:])
                        negbT = sb("negbT", [128, 512], BF16, pq)
                        NBT = Buf(negbT[:])
                        rz = sb("rz", [128, 4], F32, pq)
                        RZ = Buf(rz[:])
                        accs = [ps[3][:, 0:129], ps[4][:, 0:129], ps[5][:, 0:129], ps[6][:, 0:129]]
                        ACC = [PS[3], PS[4], PS[5], PS[6]]
                        prr = [0]

                        def run_branch(steps):
                            LA = 2
                            n_ = len(steps)
                            for idx_ in range(n_ + LA):
                                if idx_ < n_:
                                    st = steps[idx_]
                                    bk = sbank()
                                    nm = len(st["s"])
                                    for idx, (l_, r_, insb) in enumerate(st["s"]):
                                        kb.op("tensor", lambda e, l_=l_, r_=r_, idx=idx, nm=nm, bk=bk: e.matmul(ps[bk][:], lhsT=l_, rhs=r_, start=(idx == 0), stop=(idx == nm - 1)),
                                              outs=[PS[bk]], ins=insb, mark=(idx == nm - 1))
                                    prr[0] = (prr[0] + 1) % 3
                                    pi = prr[0]
                                    kb.op("scalar", lambda e, bk=bk, pi=pi: e.activation(out=pt[pi][:], in_=ps[bk][:], func=AF.Exp, scale=SCL), outs=[PT[pi]], ins=[PS[bk]])
                                    st["pi"] = pi
                                if idx_ >= LA:
                                    prev = steps[idx_ - LA]
                                    pi = prev["pi"]
                                    for (i, rhs_ap, w_, st_, sp_) in prev["pv"]:
                                        kb.op("tensor", lambda e, i=i, rhs_ap=rhs_ap, w_=w_, st_=st_, sp_=sp_, pi=pi: e.matmul(accs[i][:, 0:w_], lhsT=pt[pi][:, i * 128:(i + 1) * 128], rhs=rhs_ap,
                                                                                                                 start=st_, stop=sp_),
                                              outs=[ACC[i]], ins=[PT[pi], KS, KC])

                        def finish(h, br, first):
                            zc = 64
                            for i in range(4):
                                kb.op("vector", lambda e, i=i: e.tensor_scalar(out=rz[:, i:i + 1], in0=accs[i][:, zc:zc + 1], scalar1=1e-30, scalar2=None, op0=ALU.max),
                                      outs=[RZ], ins=[ACC[i]])
                            kb.op("vector", lambda e: e.reciprocal(out=rz[:], in_=rz[:]), outs=[RZ], ins=[RZ])
                            if br == 0:
                                for i in range(4):
                                    if h == 0:
                                        kb.op("vector", lambda e, i=i: e.tensor_scalar(out=pslc[:, i, :], in0=accs[i][:, 65:129], scalar1=rz[:, i:i + 1], scalar2=None, op0=ALU.mult),
                                              outs=[PSLC], ins=[ACC[i], RZ])
                                    else:
                                        kb.op("vector", lambda e, i=i: e.scalar_tensor_tensor(out=pslc[:, i, :], in0=accs[i][:, 65:129], scalar=rz[:, i:i + 1], in1=pslc[:, i, :],
                                                                                           op0=ALU.mult, op1=ALU.add), outs=[PSLC], ins=[ACC[i], RZ, PSLC])
                            kb.op("vector", lambda e: e.tensor_tensor(out=rz[:], in0=rz[:], in1=gts[:, :, h * 3 + br], op=ALU.mult), outs=[RZ], ins=[RZ, GTS])
                            for i in range(4):
                                dst = onsa[:, i, h * 64:(h + 1) * 64]
                                if first:
                                    kb.op("vector", lambda e, i=i, dst=dst: e.tensor_scalar(out=dst, in0=accs[i][:, 0:64], scalar1=rz[:, i:i + 1], scalar2=None, op0=ALU.mult),
                                          outs=[ONSA], ins=[ACC[i], RZ])
                                else:
                                    kb.op("vector", lambda e, i=i, dst=dst: e.scalar_tensor_tensor(out=dst, in0=accs[i][:, 0:64], scalar=rz[:, i:i + 1], in1=dst, op0=ALU.mult, op1=ALU.add),
                                          outs=[ONSA], ins=[ACC[i], RZ, ONSA])

                        wband4 = sb("wband4", [128, 8, 512], BF16, pq)
                        cmpb0 = sb("cmpb0", [128, 512], BF16, pq)
                        kb.dma("sync", wband4[:], I["wband4"], outs=[QC])
                        kb.dma("sync", cmpb0[:], I["cmpb0"], outs=[QC])
                        for qb in range(4, NB):
                            sl = slice(qb * 512, (qb + 1) * 512)
                            kb.dma("sync", cosb[:], I["cosT"][:, sl], outs=[CSB])
                            kb.dma("sync", sinb[:], I["sinT"][:, sl], outs=[CSB])
                            kb.dma("sync", cmpbb[:], I["cmpb"][:, qb, :], outs=[CMB])
                            for ch in range(2):
                                bk = sbank()
                                for k in range(8):
                                    kb.op("tensor", lambda e, k=k, bk=bk, ch=ch: e.matmul(ps[bk][:], lhsT=wqg[:, k, ch * 128:(ch + 1) * 128], rhs=hT[:, k, sl],
                                                                                         start=(k == 0), stop=(k == 7)), outs=[PS[bk]], ins=[WN, HT[qb]], mark=(k == 7))
                                kb.op("scalar", lambda e, bk=bk, ch=ch: e.copy(out=qTb[:, ch, :], in_=ps[bk][:]), outs=[QB_], ins=[PS[bk]])
                                rope_from(bk, qrTb[:, ch, :], QRB)
                            for i in range(4):
                                tt = 4 * qb + i
                                bk = mbank()
                                for k in range(8):
                                    kb.op("tensor", lambda e, k=k, bk=bk, tt=tt: e.matmul(ps[bk][:, 0:12], lhsT=hT[:, k, tt * 128:(tt + 1) * 128], rhs=wgt[:, k, :],
                                                                                         start=(k == 0), stop=(k == 7)), outs=[PS[bk]], ins=[WN, HT[qb]], mark=(k == 7))
                                kb.op("scalar", lambda e, bk=bk, i=i: e.activation(out=gts[:, i, :], in_=ps[bk][:, 0:12], func=AF.Sigmoid), outs=[GTS], ins=[PS[bk]])
                            if stop_after == "p1c_qa":
                                dump("onsa", onsa[:], [128, 4, 256])
                                return nc, dbg_out
                            ncts = 1 if qb < 4 else 2
                            for h in range(4):
                                ch, p0_ = h // 2, (h % 2) * 64
                                steps = []
                                for ct in range(ncts):
                                    smm = [(kcmpT[p0_:p0_ + 64, ct * 128:(ct + 1) * 128], qTb[p0_:p0_ + 64, ch, :], [KC, QB_])]
                                    if ct == ncts - 1:
                                        smm.append((ident[:], cmpbb[:], [CST, CMB]))
                                    else:
                                        smm.append((ident[:], cmpb0[:], [CST, QC]))
                                    pv = [(i, vcmp1[:, ct, 0:129], 129, ct == 0, ct == ncts - 1) for i in range(4)]
                                    steps.append({"s": smm, "pv": pv})
                                run_branch(steps)
                                finish(h, 0, True)
                            if stop_after == "p1c_qb":
                                dump("onsa", onsa[:], [128, 4, 256])
                                return nc, dbg_out
                            for i in range(4):
                                qt = 4 * qb + i
                                kb.dma("sync", sadd[:], I["seladd"][:, qt, :], outs=[SADD])
                                kb.op("vector", lambda e, i=i: e.tensor_tensor(out=score[:], in0=pslc[:, i, :], in1=sadd[:], op=ALU.add), outs=[SEL], ins=[PSLC, SADD])
                                kb.op("vector", lambda e: e.max(out=m8[:, 0:8], in_=score[:]), outs=[SEL], ins=[SEL])
                                kb.op("vector", lambda e: e.match_replace(out=stmp[:], in_to_replace=m8[:, 0:8], in_values=score[:], imm_value=-3e38), outs=[SEL], ins=[SEL])
                                kb.op("vector", lambda e: e.max(out=m8[:, 8:16], in_=stmp[:]), outs=[SEL], ins=[SEL])
                                kb.op("vector", lambda e: e.tensor_scalar(out=sel[:], in0=score[:], scalar1=m8[:, 15:16], scalar2=None, op0=ALU.is_ge), outs=[SEL], ins=[SEL])
                                kb.op("vector", lambda e: e.scalar_tensor_tensor(out=sel[:], in0=score[:], scalar=-1e29, in1=sel[:], op0=ALU.is_gt, op1=ALU.mult), outs=[SEL], ins=[SEL])
                                kb.op("vector", lambda e: e.tensor_scalar(out=negb[:, 0:64], in0=sel[:], scalar1=-1.0, scalar2=-NEG, op0=ALU.add, op1=ALU.mult), outs=[SEL], ins=[SEL])
                                kb.op("vector", lambda e: e.tensor_scalar(out=negb[:, 64:128], in0=sel[:], scalar1=-1.0, scalar2=-NEG, op0=ALU.add, op1=ALU.mult), outs=[SEL], ins=[SEL])
                                bk = mbank()
                                kb.op("tensor", lambda e, bk=bk: e.matmul(ps[bk][:, 0:128], lhsT=negb[:], rhs=ident[:], start=True, stop=True), outs=[PS[bk]], ins=[SEL, CST])
                                kb.op("scalar", lambda e, bk=bk, i=i: e.copy(out=negbT[:, i * 128:(i + 1) * 128], in_=ps[bk][:, 0:128]), outs=[NBT], ins=[PS[bk]])
                            if stop_after == "p1c_qc":
                                dump("onsa", onsa[:], [128, 4, 256])
                                return nc, dbg_out
                            for ch in range(2):
                                kb.op("gpsimd", lambda e, ch=ch: e.tensor_copy(out=qn[ch][0][0:64, :], in_=qrTb[0:64, ch, :]), outs=[QN], ins=[QRB])
                                kb.op("gpsimd", lambda e, ch=ch: e.tensor_copy(out=qn[ch][0][64:128, :], in_=negbT[64:128, :]), outs=[QN], ins=[NBT])
                                kb.op("gpsimd", lambda e, ch=ch: e.tensor_copy(out=qn[ch][1][0:64, :], in_=negbT[0:64, :]), outs=[QN], ins=[NBT])
                                kb.op("gpsimd", lambda e, ch=ch: e.tensor_copy(out=qn[ch][1][64:128, :], in_=qrTb[64:128, ch, :]), outs=[QN], ins=[QRB])
                            for h in range(4):
                                ch, p0_ = h // 2, (h % 2) * 64
                                KE_ = KEe if h % 2 == 0 else KEo
                                steps = []
                                for kt in range(4 * qb + 4):
                                    smm = [(KE_[:, kt * 128:(kt + 1) * 128], qn[ch][h % 2][:], [KS, QN, CST])]
                                    if kt >= 4 * qb:
                                        smm.append((ident[:], wband[:, 4 + kt - 4 * qb, :], [CST, QC]))
                                    pv = [(i, vs1[:, kt, 0:65], 65, kt == 0, kt == 4 * qb + i) for i in range(4) if kt <= 4 * qb + i]
                                    steps.append({"s": smm, "pv": pv})
                                run_branch(steps)
                                finish(h, 1, False)
                            if stop_after == "p1c_qd":
                                dump("onsa", onsa[:], [128, 4, 256])
                                return nc, dbg_out
                            for h in range(4):
                                ch, p0_ = h // 2, (h % 2) * 64
                                steps = []
                                jmin = max(0, 4 - 4 * qb)
                                for j in range(jmin, 8):
                                    kt = 4 * qb - 4 + j
                                    smm = [(kwT[p0_:p0_ + 64, kt * 128:(kt + 1) * 128], qrTb[p0_:p0_ + 64, ch, :], [KS, QRB]),
                                           (ident[:], (wband4 if qb == 4 else wband)[:, j, :], [CST, QC])]
                                    pv = [(i, vw1[:, kt, 0:65], 65, j == max(i, jmin), j == i + 4) for i in range(4) if i <= j <= i + 4]
                                    steps.append({"s": smm, "pv": pv})
                                run_branch(steps)
                                finish(h, 2, False)
                            if stop_after == "p1c_q":
                                dump("onsa", onsa[:], [128, 4, 256])
                                dump("pslc", pslc[:], [128, 4, 64])
                                dump("negbT", negbT[:], [128, 512], BF16)
                                return nc, dbg_out
                            kb.op("gpsimd", lambda e: e.tensor_copy(out=obn[:], in_=onsa[:]), outs=[OBN], ins=[ONSA])
                            for i in range(4):
                                qt = 4 * qb + i
                                s_ = qt - 16
                                for ch in range(2):
                                    jf = 4 + g * 2 + ch
                                    bk = mbank()
                                    kb.op("tensor", lambda e, bk=bk, i=i, ch=ch: e.matmul(ps[bk][:, 0:128], lhsT=obn[:, i, ch * 128:(ch + 1) * 128], rhs=ident[:], start=True, stop=True),
                                          outs=[PS[bk]], ins=[OBN, CST])
                                    dst = oT[:, jf, s_ * 128:(s_ + 1) * 128]
                                    kb.op("scalar", lambda e, bk=bk, dst=dst: e.copy(out=dst, in_=ps[bk][:, 0:128]), outs=[OT[jf][s_]], ins=[PS[bk]])
                        kb.barrier()
                kb.barrier()
            if "oT2" in dbg:
                dump("oT2", oT[:], [128, 8, SO], BF16)
            if stop_after == "p1c":
                kb.barrier()
                return nc, dbg_out
        kb.barrier()
        with ExitStack() as p2:
            acc = sb("acc", [128, 8, SO], F32, p2)
            ACCB = [Buf(acc[:, :, nb * 512:(nb + 1) * 512]) for nb in range(4)]
            wo = sb("wo", [128, 8, D], BF16, p2)
            WO = Buf(wo[:])
            fg = sb("fg", [128, 8], F32, p2)
            FG = Buf(fg[:])
            kb.dma("sync", fg[:], I["fg"], outs=[FG])
            xTo_v = I["xTo"].rearrange("(k p) t -> p k t", p=128)
            for nb in range(4):
                kb.dma("sync", acc[:, :, nb * 512:(nb + 1) * 512], xTo_v[:, :, nb * 512:(nb + 1) * 512], outs=[ACCB[nb]])
            w_out_v = I["w_out"].rearrange("(k p) c -> p k c", p=128)
            for k in range(8):
                kb.dma("gpsimd", wo[:, k, :], w_out_v[:, k, :], outs=[WO])
            rr2 = [0]

            def bank2():
                rr2[0] = (rr2[0] + 1) % 8
                return rr2[0]

            for nb in range(4):
                sl = slice(nb * 512, (nb + 1) * 512)
                for m in range(8):
                    bk = bank2()
                    for k in range(8):
                        kb.op("tensor", lambda e, k=k, m=m, bk=bk, sl=sl: e.matmul(ps[bk][:], lhsT=wo[:, k, m * 128:(m + 1) * 128], rhs=oT[:, k, sl], start=(k == 0), stop=(k == 7)),
                              outs=[PS[bk]], ins=[WO] + [OT[k][s] for s in range(nb * 4, nb * 4 + 4)], mark=(k == 7))
                    kb.op("vector", lambda e, m=m, bk=bk, sl=sl: e.scalar_tensor_tensor(out=acc[:, m, sl], in0=ps[bk][:], scalar=g1c(m), in1=acc[:, m, sl], op0=ALU.mult, op1=ALU.add),
                          outs=[ACCB[nb]], ins=[PS[bk], ACCB[nb], MOD])
            if "x2T" in dbg:
                dump("x2T", acc[:], [128, 8, SO])
            h2T = sb("h2T", [128, 8, SO], BF16, p2)
            H2 = [Buf(h2T[:, :, nb * 512:(nb + 1) * 512]) for nb in range(4)]
            with ExitStack() as p2a:
                sqa = sb("sqa", [128, 8, 512], F32, p2a)
                SQA = Buf(sqa[:])
                rsa = sb("rsa", [128, 512], F32, p2a)
                RSA = Buf(rsa[:])
                tma = [sb(f"tma{i}", [128, 512], F32, p2a) for i in range(2)]
                TMA = [Buf(t[:]) for t in tma]
                for nb in range(4):
                    sl = slice(nb * 512, (nb + 1) * 512)
                    kb.op("scalar", lambda e, sl=sl: e.activation(out=sqa[:], in_=acc[:, :, sl], func=AF.Square), outs=[SQA], ins=[ACCB[nb]])
                    bk = bank2()
                    for k in range(8):
                        kb.op("tensor", lambda e, k=k, bk=bk: e.matmul(ps[bk][:], lhsT=onesf[:], rhs=sqa[:, k, :], start=(k == 0), stop=(k == 7)),
                              outs=[PS[bk]], ins=[SQA, CST], mark=(k == 7))
                    kb.op("scalar", lambda e, bk=bk: e.activation(out=rsa[:], in_=ps[bk][:], func=AF.Sqrt, scale=1.0 / D, bias=EPS), outs=[RSA], ins=[PS[bk]])
                    kb.op("vector", lambda e: e.reciprocal(out=rsa[:], in_=rsa[:]), outs=[RSA], ins=[RSA])
                    for k in range(8):
                        T = TMA[k % 2]
                        t_ = tma[k % 2]
                        kb.op("vector", lambda e, k=k, t_=t_, sl=sl: e.tensor_tensor(out=t_[:], in0=acc[:, k, sl], in1=rsa[:], op=ALU.mult), outs=[T], ins=[ACCB[nb], RSA])
                        kb.op("scalar", lambda e, k=k, t_=t_, sl=sl: e.activation(out=h2T[:, k, sl], in_=t_[:], func=AF.Identity, scale=a2[:, k:k + 1], bias=sh2(k)),
                              outs=[H2[nb]], ins=[T, MOD])
                kb.barrier()
            if "h2T" in dbg:
                dump("h2T", h2T[:], [128, 8, SO], BF16)
            gw = sb("gw", [128, 16, 65], F32, p2)
            GW = Buf(gw[:])
            kb.op("vector", lambda e: e.memset(gw[:], 1.0), outs=[GW])
            with ExitStack() as p2r:
                rwf = sb("rwf", [128, 8, 64], F32, p2r)
                rwb = sb("rwb", [128, 8, 64], BF16, p2r)
                rbias = sb("rbias", [128, 64], F32, p2r)
                RW = Buf(rwf[:])
                kb.dma("sync", rwf[:], I["rw"], outs=[RW])
                kb.dma("sync", rbias[:], I["rbias"], outs=[RW])
                kb.op("vector", lambda e: e.tensor_copy(out=rwb[:], in_=rwf[:]), outs=[RW], ins=[RW])
                scr = sb("scr", [128, 64], F32, p2r)
                chs = sb("chs", [128, 64], F32, p2r)
                eq = sb("eq", [128, 64], F32, p2r)
                chm = sb("chm", [128, 64], F32, p2r)
                m1 = sb("m1", [128, 8], F32, p2r)
                m2 = sb("m2", [128, 8], F32, p2r)
                gsm = sb("gsm", [128, 8], F32, p2r)
                g8 = sb("g8", [128, 8], F32, p2r)
                gmk = sb("gmk", [128, 8], F32, p2r)
                e8 = sb("e8", [128, 8], F32, p2r)
                ssum = sb("ssum", [128, 1], F32, p2r)
                RT = Buf(scr[:])
                v3 = lambda ap: ap.rearrange("p (g j) -> p g j", j=8)
                b3 = lambda ap: ap.rearrange("p (g o) -> p g o", o=1).to_broadcast([128, 8, 8])
                for tt in range(16):
                    bk = bank2()
                    for k in range(8):
                        kb.op("tensor", lambda e, k=k, bk=bk, tt=tt: e.matmul(ps[bk][:, 0:64], lhsT=h2T[:, k, tt * 128:(tt + 1) * 128], rhs=rwb[:, k, :], start=(k == 0), stop=(k == 7)),
                              outs=[PS[bk]], ins=[H2[tt // 4], RW], mark=(k == 7))
                    kb.op("scalar", lambda e, bk=bk: e.activation(out=scr[:], in_=ps[bk][:, 0:64], func=AF.Sigmoid), outs=[RT], ins=[PS[bk]])
                    V = lambda f: kb.op("vector", f, outs=[RT], ins=[RT, RW])
                    V(lambda e: e.tensor_tensor(out=chs[:], in0=scr[:], in1=rbias[:], op=ALU.add))
                    V(lambda e: e.tensor_reduce(out=m1[:], in_=v3(chs[:]), axis=AX.X, op=ALU.max))
                    V(lambda e: e.tensor_tensor(out=v3(eq[:]), in0=v3(chs[:]), in1=b3(m1[:]), op=ALU.is_equal))
                    V(lambda e: e.scalar_tensor_tensor(out=eq[:], in0=eq[:], scalar=-1e30, in1=chs[:], op0=ALU.mult, op1=ALU.add))
                    V(lambda e: e.tensor_reduce(out=m2[:], in_=v3(eq[:]), axis=AX.X, op=ALU.max))
                    V(lambda e: e.tensor_tensor(out=gsm[:], in0=m1[:], in1=m2[:], op=ALU.add))
                    V(lambda e: e.max(out=g8[:], in_=gsm[:]))
                    V(lambda e: e.tensor_scalar(out=gmk[:], in0=gsm[:], scalar1=g8[:, 3:4], scalar2=None, op0=ALU.is_ge))
                    V(lambda e: e.scalar_tensor_tensor(out=v3(chm[:]), in0=v3(chs[:]), scalar=10.0, in1=b3(gmk[:]), op0=ALU.add, op1=ALU.mult))
                    V(lambda e: e.max(out=e8[:], in_=chm[:]))
                    V(lambda e: e.tensor_scalar(out=eq[:], in0=chm[:], scalar1=e8[:, 7:8], scalar2=None, op0=ALU.is_ge))
                    V(lambda e: e.tensor_tensor(out=eq[:], in0=eq[:], in1=scr[:], op=ALU.mult))
                    V(lambda e: e.tensor_reduce(out=ssum[:], in_=eq[:], axis=AX.X, op=ALU.add))
                    V(lambda e: e.reciprocal(out=ssum[:], in_=ssum[:]))
                    kb.op("vector", lambda e, tt=tt: e.tensor_scalar(out=gw[:, tt, 0:64], in0=eq[:], scalar1=ssum[:, 0:1], scalar2=2.5, op0=ALU.mult, op1=ALU.mult),
                          outs=[GW], ins=[RT])
                kb.barrier()
            if "gw" in dbg:
                dump("gw", gw[:], [128, 16, 65])
            with ExitStack() as p2e:
                wg = [sb(f"wg{i}", [128, 8, 512], BF16, p2e) for i in range(2)]
                wd = [sb(f"wd{i}", [128, 2, D], BF16, p2e) for i in range(2)]
                WG = [Buf(t[:]) for t in wg]
                WD = [Buf(t[:]) for t in wd]
                actT = [sb(f"actT{i}", [128, 2, SO], BF16, p2e) for i in range(2)]
                ACT_ = [[Buf(actT[i][:, :, nb * 512:(nb + 1) * 512]) for nb in range(4)] for i in range(2)]
                sgt = [sb(f"sgt{i}", [128, 256], F32, p2e) for i in range(2)]
                SGT = [Buf(t[:]) for t in sgt]
                att = [sb(f"att{i}", [128, 256], BF16, p2e) for i in range(2)]
                ATT = [Buf(t[:]) for t in att]
                NE = n_experts

                def load_w(e_):
                    sl_ = e_ % 2
                    gv = I["wgu"][e_].rearrange("(k p) c -> p k c", p=128)
                    kb.dma("gpsimd", wg[sl_][:, 0:4, :], gv[:, 0:4, :], outs=[WG[sl_]])
                    kb.dma("gpsimd", wg[sl_][:, 4:8, :], gv[:, 4:8, :], outs=[WG[sl_]])
                    kb.dma("gpsimd", wd[sl_][:], I["wdn"][e_].rearrange("(k p) c -> p k c", p=128), outs=[WD[sl_]])

                def load_wg(e_):
                    sl_ = e_ % 2
                    gv = I["wgu"][e_].rearrange("(k p) c -> p k c", p=128)
                    kb.dma("gpsimd", wg[sl_][:, 0:4, :], gv[:, 0:4, :], outs=[WG[sl_]])
                    kb.dma("gpsimd", wg[sl_][:, 4:8, :], gv[:, 4:8, :], outs=[WG[sl_]])

                def load_wd(e_):
                    sl_ = e_ % 2
                    kb.dma("gpsimd", wd[sl_][:], I["wdn"][e_].rearrange("(k p) c -> p k c", p=128), outs=[WD[sl_]])

                cnt = [0]
                pend = {}

                def GU(e_, tt):
                    sl_ = e_ % 2
                    bk = bank2()
                    for k in range(8):
                        kb.op("tensor", lambda e, k=k: e.matmul(ps[bk][:], lhsT=h2T[:, k, tt * 128:(tt + 1) * 128], rhs=wg[sl_][:, k, :], start=(k == 0), stop=(k == 7)),
                              outs=[PS[bk]], ins=[H2[tt // 4], WG[sl_]], mark=(k == 7))
                    cnt[0] += 1
                    i2 = cnt[0] % 2
                    kb.op("scalar", lambda e: e.activation(out=sgt[i2][:], in_=ps[bk][:, 0:256], func=AF.Silu), outs=[SGT[i2]], ins=[PS[bk]])
                    kb.op("vector", lambda e: e.scalar_tensor_tensor(out=att[i2][:], in0=ps[bk][:, 256:512], scalar=gw[:, tt, e_:e_ + 1], in1=sgt[i2][:],
                                                                     op0=ALU.mult, op1=ALU.mult), outs=[ATT[i2]], ins=[PS[bk], SGT[i2], GW])
                    pend[(e_, tt)] = i2

                def TR(e_, tt):
                    sl_ = e_ % 2
                    i2 = pend.pop((e_, tt))
                    for hc in range(2):
                        bt = bank2()
                        kb.op("tensor", lambda e, hc=hc, bt=bt: e.matmul(ps[bt][:, 0:128], lhsT=att[i2][:, hc * 128:(hc + 1) * 128], rhs=ident[:], start=True, stop=True),
                              outs=[PS[bt]], ins=[ATT[i2], CST])
                        kb.op("scalar", lambda e, hc=hc, bt=bt: e.copy(out=actT[sl_][:, hc, tt * 128:(tt + 1) * 128], in_=ps[bt][:, 0:128]),
                              outs=[ACT_[sl_][tt // 4]], ins=[PS[bt]])

                def DN(e_, nb):
                    sl_ = e_ % 2
                    sl = slice(nb * 512, (nb + 1) * 512)
                    for m in range(8):
                        bk = bank2()
                        for hc in range(2):
                            kb.op("tensor", lambda e, bk=bk, hc=hc, m=m: e.matmul(ps[bk][:], lhsT=wd[sl_][:, hc, m * 128:(m + 1) * 128], rhs=actT[sl_][:, hc, sl], start=(hc == 0), stop=(hc == 1)),
                                  outs=[PS[bk]], ins=[WD[sl_], ACT_[sl_][nb]], mark=(hc == 1))
                        kb.op("vector", lambda e, bk=bk, m=m: e.scalar_tensor_tensor(out=acc[:, m, sl], in0=ps[bk][:], scalar=g2c(m), in1=acc[:, m, sl], op0=ALU.mult, op1=ALU.add),
                              outs=[ACCB[nb]], ins=[PS[bk], ACCB[nb], MOD])

                load_wg(0)
                load_wd(0)
                for e_ in range(NE + 1):
                    if e_ + 1 < NE:
                        load_wg(e_ + 1)
                    for tt in range(16):
                        if e_ < NE:
                            GU(e_, tt)
                            if tt > 0:
                                TR(e_, tt - 1)
                        if e_ > 0 and tt % 4 == 3:
                            DN(e_ - 1, tt // 4)
                    if e_ < NE:
                        TR(e_, 15)
                    if e_ + 1 < NE:
                        load_wd(e_ + 1)
                kb.barrier()
            if "x3T" in dbg:
                dump("x3T", acc[:], [128, 8, SO])
            sq2 = sb("sq2", [128, 8, 512], F32, p2)
            SQ2 = Buf(sq2[:])
            rstd2 = sb("rstd2", [128, 512], F32, p2)
            RS2 = Buf(rstd2[:])
            ot = [sb(f"ot{i}", [128, 8, 512], F32, p2) for i in range(2)]
            OTB = [Buf(t[:]) for t in ot]
            outT_v = outT.rearrange("(k p) t -> p k t", p=128)
            for nb in range(4):
                sl = slice(nb * 512, (nb + 1) * 512)
                kb.op("scalar", lambda e, sl=sl: e.activation(out=sq2[:], in_=acc[:, :, sl], func=AF.Square), outs=[SQ2], ins=[ACCB[nb]])
                bk = bank2()
                for k in range(8):
                    kb.op("tensor", lambda e, k=k, bk=bk: e.matmul(ps[bk][:], lhsT=onesf[:], rhs=sq2[:, k, :], start=(k == 0), stop=(k == 7)),
                          outs=[PS[bk]], ins=[SQ2, CST], mark=(k == 7))
                kb.op("scalar", lambda e, bk=bk: e.activation(out=rstd2[:], in_=ps[bk][:], func=AF.Sqrt, scale=1.0 / D, bias=EPS), outs=[RS2], ins=[PS[bk]])
                kb.op("vector", lambda e: e.reciprocal(out=rstd2[:], in_=rstd2[:]), outs=[RS2], ins=[RS2])
                o_ = ot[nb % 2]
                for k in range(8):
                    kb.op("vector", lambda e, k=k, o_=o_, sl=sl: e.scalar_tensor_tensor(out=o_[:, k, :], in0=acc[:, k, sl], scalar=fg[:, k:k + 1], in1=rstd2[:], op0=ALU.mult, op1=ALU.mult),
                          outs=[OTB[nb % 2]], ins=[ACCB[nb], FG, RS2])
                kb.dma("sync", outT_v[:, :, sl], o_[:], ins=[OTB[nb % 2]])
            kb.barrier()
        kb.barrier()
    return nc, dbg_out


def _prep_inputs(inp, core):
    b = core // 2
    half = core % 2
    f = lambda a: np.ascontiguousarray(a, dtype=np.float32)
    x = inp["x"][b]
    m = {}
    xT = f(x.T)
    m["xTo"] = f(xT[:, half * SO:(half + 1) * SO])
    if half == 0:
        xl = np.zeros((D, S), np.float32)
        xl[:, SO:] = xT[:, :SO]
        m["xT"] = xl
    else:
        m["xT"] = xT
    m["cT"] = f(inp["c"][b].reshape(8, 128).T)
    m["w_ada"] = f(inp["w_ada"][0])
    m["b_ada"] = f(inp["b_ada"][0].reshape(1, -1))
    m["n1g"] = f(inp["norm1_g"][0].reshape(8, 128).T)
    m["w_in"] = f(inp["w_in"][0])
    m["lbl"] = f(inp["hg_lb_logits"].reshape(2, 4, 128).transpose(2, 0, 1))
    m["hng"] = f(np.broadcast_to(inp["hg_norm_g"][0][None, :], (128, 512)))
    for s in ("k", "v"):
        m["peT" + s] = f(inp["cmp_pos_" + s][0].T)
        m["w1" + s] = f(inp["cmp_w1_" + s][0].reshape(32, 64, 256).transpose(1, 0, 2))
        m["b1" + s] = f(inp["cmp_b1_" + s][0].reshape(2, 128).T)
        m["w2" + s] = f(inp["cmp_w2_" + s][0].reshape(2, 128, 64).transpose(1, 0, 2))
    m["w_out"] = f(inp["w_out"][0])
    m["n2g"] = f(inp["norm2_g"][0].reshape(8, 128).T)
    m["rw"] = f(inp["router_w"][0].reshape(8, 128, 64).transpose(1, 0, 2))
    m["rbias"] = f(np.broadcast_to(inp["router_bias"][0][None, :], (128, 64)))
    m["fg"] = f(inp["final_g"].reshape(8, 128).T)
    return m


_SHARED = {}


def kernel(**inp):
    inp = {k: np.asarray(v) for k, v in inp.items()}
    nc, _ = build()
    wgu = np.ascontiguousarray(np.concatenate([inp["w_exp_gu"][0], inp["w_sh_gu"][0][None]], axis=0), dtype=np.float32)
    wdn = np.ascontiguousarray(np.concatenate([inp["w_exp_dn"][0], inp["w_sh_dn"][0][None]], axis=0), dtype=np.float32)
    in_maps = []
    for core in range(8):
        m = _prep_inputs(inp, core)
        m["wgu"] = wgu
        m["wdn"] = wdn
        m.update(_consts(core % 2))
        in_maps.append(m)
    res = run_bass_kernel_spmd(nc, in_maps, core_ids=list(range(8)))
    out = np.zeros((4, S, D), np.float32)
    for core in range(8):
        b, half = core // 2, core % 2
        out[b, half * SO:(half + 1) * SO, :] = res.results[core]["outT"].T
    return out
```

```python
import numpy as np
import os as _os0
import ml_dtypes
from contextlib import ExitStack
import concourse.bass as bass
import concourse.mybir as mybir
from concourse.bass_utils import run_bass_kernel_spmd

F32 = mybir.dt.float32
BF16 = mybir.dt.bfloat16
AF = mybir.ActivationFunctionType
ALU = mybir.AluOpType
AX = mybir.AxisListType

S = 4096
D = 1024
NT = 32
NB = 8
SO = 2048
EPS = 1e-6
NEG = -30000.0
NDS = 12
SEM_LIMIT = 2000
SAME_SYNC = not bool(int(_os0.environ.get("NOSAME", "0")))


class Buf:
    __slots__ = ("ap", "w", "r", "excl")

    def __init__(self, ap, excl=False):
        self.ap = ap
        self.w = None
        self.r = {}
        self.excl = excl

    def __getitem__(self, k):
        return self.ap[k]


class Eng:
    def __init__(self, name, h):
        self.name = name
        self.h = h
        self.sem = None
        self.count = 0
        self.epoch = 0
        self.waited = {}


class KB:
    def __init__(self, nc, es):
        self.nc = nc
        self.es = es
        self.engs = {n: Eng(n, getattr(nc, n)) for n in ("tensor", "vector", "scalar", "gpsimd", "sync")}
        for e in self.engs.values():
            self._new_sem(e)
        self.dsems = {q: [es.enter_context(nc.semaphore(f"d_{q}{i}")) for i in range(NDS)] for q in ("sync", "gpsimd")}
        self.dcnt = {q: [0] * NDS for q in ("sync", "gpsimd")}
        self.drr = {"sync": 0, "gpsimd": 0}
        self.nsem = 0

    def _new_sem(self, e):
        e.epoch += 1
        e.sem = self.es.enter_context(self.nc.semaphore(f"s_{e.name}_{e.epoch}"))
        e.count = 0

    def wait(self, eng, tk):
        key, sem, val = tk
        if eng.waited.get(key, 0) >= val:
            return
        eng.h.wait_ge(sem, val)
        eng.waited[key] = val

    def _deps(self, en, eng, outs, ins):
        need = {}

        def add(t):
            if t[3] == en and (en == "tensor" or not SAME_SYNC):
                return
            cur = need.get(t[0])
            if cur is None or cur[2] < t[2]:
                need[t[0]] = t

        for b in ins:
            if b.w is not None:
                add(b.w)
            if b.excl:
                for t in b.r.values():
                    if t[3] != en:
                        add(t)
        for b in outs:
            if b.w is not None:
                add(b.w)
            for t in b.r.values():
                add(t)
        for t in need.values():
            self.wait(eng, t[:3])

    def op(self, en, fn, outs=(), ins=(), mark=True):
        eng = self.engs[en]
        self._deps(en, eng, outs, ins)
        if eng.count >= SEM_LIMIT:
            self._new_sem(eng)
        inst = fn(eng.h)
        if mark:
            eng.count += 1
            inst.then_inc(eng.sem, 1)
            tk = ((en, eng.epoch), eng.sem, eng.count, en)
        else:
            tk = ((en, eng.epoch), eng.sem, eng.count + 1, en)
        for b in ins:
            b.r[tk[0]] = tk
        for b in outs:
            b.w = tk
            b.r = {}
        return tk

    def dma(self, q, out_ap, in_ap, outs=(), ins=()):
        eng = self.engs[q]
        i = self.drr[q]
        self.drr[q] = (i + 1) % NDS
        sem = self.dsems[q][i]
        key = ("d", q, i)
        if self.dcnt[q][i] > 0:
            self.wait(eng, (key, sem, self.dcnt[q][i]))
        self._deps("dma_" + q, eng, outs, ins)
        inst = eng.h.dma_start(out=out_ap, in_=in_ap)
        self.dcnt[q][i] += 16
        inst.then_inc(sem, 16)
        tk = (key, sem, self.dcnt[q][i], "dma_" + q)
        for b in ins:
            b.r[key] = tk
        for b in outs:
            b.w = tk
            b.r = {}
        return tk

    def barrier(self):
        for e in self.engs.values():
            for o in self.engs.values():
                if o is e or o.count == 0:
                    continue
                self.wait(e, ((o.name, o.epoch), o.sem, o.count))
            for q in ("sync", "gpsimd"):
                for i in range(NDS):
                    if self.dcnt[q][i] > 0:
                        self.wait(e, (("d", q, i), self.dsems[q][i], self.dcnt[q][i]))


def _consts(half):
    bf = ml_dtypes.bfloat16
    c = {}
    eye = np.eye(128, dtype=np.float32)
    c["ident"] = eye.astype(bf)
    c["onesf"] = np.ones((128, 128), np.float32)
    c["isel0"] = (eye * (1.0 if half == 0 else 0.0)).astype(bf)
    c["isel1"] = (eye * (1.0 if half == 1 else 0.0)).astype(bf)
    m = np.arange(128)
    sw = (m // 64) * 64 + ((m % 64) + 32) % 64
    ps = np.zeros((128, 128), np.float32)
    ps[sw, m] = 1.0
    c["pswap"] = ps.astype(bf)
    shift = 2048 if half == 0 else 0
    dd = np.arange(128) % 64
    i = dd % 32
    inv = 10000.0 ** (-(i.astype(np.float64)) / 32.0)
    tpos = (np.arange(S) - shift).astype(np.float64)
    ang = inv[:, None].astype(np.float32).astype(np.float64) * tpos[None, :]
    ang = ang.astype(np.float32).astype(np.float64)
    c["cosT"] = np.cos(ang).astype(np.float32)
    sg = np.where(dd < 32, -1.0, 1.0)[:, None]
    c["sinT"] = (np.sin(ang) * sg).astype(np.float32)
    vm = np.ones((128, 32), np.float32)
    if half == 0:
        vm[:, :16] = 0.0
    c["vmask"] = vm
    c["hmask"] = (m[:, None] <= m[None, :]).astype(np.float32).astype(bf)
    seg = np.ones((128, 512), np.float32)
    seg[:, ::128] = 0.0
    c["segm"] = seg
    r = np.arange(128)[:, None]
    qi = np.arange(512)[None, :]
    wb = np.zeros((8, 128, 512), np.float32)
    cb = np.zeros((4, 128, 512), np.float32)
    for j in range(8):
        kpos = -512 + 128 * j + r
        dlt = qi - kpos
        wb[j] = np.where((dlt >= 0) & (dlt < 512), 0.0, NEG)
    for j in range(4):
        kpos = 128 * j + r
        cb[j] = np.where(kpos <= qi, 0.0, NEG)
    c["wband"] = np.ascontiguousarray(wb.transpose(1, 0, 2)).astype(bf)
    c["causb"] = np.ascontiguousarray(cb.transpose(1, 0, 2)).astype(bf)
    wb4 = wb.copy()
    if half == 0:
        wb4[0:4] = NEG
    c["wband4"] = np.ascontiguousarray(wb4.transpose(1, 0, 2)).astype(bf)
    cm = np.zeros((8, 128, 512), np.float32)
    for qb in range(8):
        ct = 0 if qb < 4 else 1
        cc = 128 * ct + r
        qpos = 512 * qb + qi - shift
        tc = cc - shift // 16
        cm[qb] = np.where((16 * tc + 31 <= qpos) & (cc < 255) & (tc >= 0), 0.0, NEG)
    c["cmpb"] = np.ascontiguousarray(cm.transpose(1, 0, 2)).astype(bf)
    c["cmpb0"] = np.full((128, 512), NEG if half == 0 else 0.0, np.float32).astype(bf)
    ek = np.zeros((64, 32, 128), np.float32)
    for kt in range(32):
        ek[2 * kt, kt, :64] = 1.0
        ek[2 * kt + 1, kt, 64:] = 1.0
    c["ekt"] = np.concatenate([ek, ek], axis=0).astype(bf)
    add = np.zeros((128, 32, 64), np.float32)
    for qt in range(32):
        pos = 128 * qt + np.arange(128) - shift
        cur = pos // 64
        j = np.arange(64)[None, :] - shift // 64
        forced = (j == 0) | (j == cur[:, None]) | (j == cur[:, None] - 1)
        avail = (j <= cur[:, None]) & (j >= 0)
        add[:, qt, :] = np.where(avail & forced, 1e30, np.where(avail, 0.0, -1e30))
    c["seladd"] = add
    cs = np.arange(256)[:, None] * 16
    ss = np.arange(64)[None, :] * 64
    ov = np.clip(np.minimum(cs + 32, ss + 64) - np.maximum(cs, ss), 0, None).astype(np.float32) / 32.0
    ov[255] = 0.0
    c["ovl"] = np.ascontiguousarray(ov.reshape(2, 128, 64).transpose(1, 0, 2)).astype(bf)
    return c


CONST_SHAPES = {
    "ident": ([128, 128], BF16), "onesf": ([128, 128], F32), "isel0": ([128, 128], BF16), "isel1": ([128, 128], BF16),
    "pswap": ([128, 128], BF16), "cosT": ([128, S], F32), "sinT": ([128, S], F32), "hmask": ([128, 128], BF16),
    "segm": ([128, 512], F32), "wband": ([128, 8, 512], BF16), "causb": ([128, 4, 512], BF16),
    "cmpb": ([128, 8, 512], BF16), "ekt": ([128, 32, 128], BF16), "seladd": ([128, 32, 64], F32),
    "ovl": ([128, 2, 64], BF16), "vmask": ([128, 32], F32), "wband4": ([128, 8, 512], BF16), "cmpb0": ([128, 512], BF16),
}

IN_SHAPES = {
    "xT": [D, S], "xTo": [D, SO], "cT": [128, 8], "w_ada": [D, 6 * D], "b_ada": [1, 6 * D], "n1g": [128, 8],
    "w_in": [D, 3352], "lbl": [128, 2, 4], "hng": [128, 512],
    "peTk": [64, 32], "w1k": [64, 32, 256], "b1k": [128, 2], "w2k": [128, 2, 64],
    "peTv": [64, 32], "w1v": [64, 32, 256], "b1v": [128, 2], "w2v": [128, 2, 64],
    "w_out": [D, D], "n2g": [128, 8], "rw": [128, 8, 64], "rbias": [128, 64],
    "wgu": [65, D, 512], "wdn": [65, 256, D], "fg": [128, 8],
}


class _SkipNSA(Exception):
    pass


class _NSAScope(ExitStack):
    def __exit__(self, et, ev, tb):
        super().__exit__(None, None, None)
        return et is _SkipNSA


def build(stop_after=None, dbg=(), with_moe=True, enable_nsa=True, n_experts=65):
    nc = bass.Bass("TRN2", target_bir_lowering=False)
    I = {}
    for k, shp in IN_SHAPES.items():
        if not with_moe and k in ("wgu", "wdn"):
            continue
        I[k] = nc.dram_tensor(k, list(shp), F32, kind="ExternalInput").ap()
    for k, (shp, dt) in CONST_SHAPES.items():
        I[k] = nc.dram_tensor(k, list(shp), dt, kind="ExternalInput").ap()
    outT = nc.dram_tensor("outT", [D, SO], F32, kind="ExternalOutput").ap()
    dbg_out = {}
    with ExitStack() as es:
        kb = KB(nc, es)
        E = es.enter_context

        uid = [0]

        def sb(name, shape, dt=F32, stack=None):
            uid[0] += 1
            return (stack or es).enter_context(nc.sbuf_tensor(f"sb{uid[0]}_" + name, list(shape), dt))

        ps = [E(nc.psum_tensor(f"ps{i}", [128, 512], F32)) for i in range(8)]
        PS = [Buf(p[:], excl=True) for p in ps]

        def dump(name, ap, shape, dt=F32):
            t = nc.dram_tensor("dbg_" + name, list(shape), dt, kind="ExternalOutput").ap()
            dbg_out[name] = t
            kb.barrier()
            kb.dma("sync", t, ap)
            kb.barrier()

        ident = sb("ident", [128, 128], BF16)
        onesf = sb("onesf", [128, 128], F32)
        isel0 = sb("isel0", [128, 128], BF16)
        isel1 = sb("isel1", [128, 128], BF16)
        pswap = sb("pswap", [128, 128], BF16)
        hmask = sb("hmask", [128, 128], BF16)
        segm = sb("segm", [128, 512], F32)
        CST = Buf(ident[:])
        for nm, t in (("ident", ident), ("onesf", onesf), ("isel0", isel0), ("isel1", isel1), ("pswap", pswap),
                      ("hmask", hmask), ("segm", segm)):
            kb.dma("sync", t[:], I[nm], outs=[CST])
        modcol = sb("modcol", [128, 48], F32)
        a1 = sb("a1", [128, 8], F32)
        a2 = sb("a2", [128, 8], F32)
        MOD = Buf(modcol[:])
        oT = sb("oT", [128, 8, SO], BF16)
        OT = [[Buf(oT[:, j, s * 128:(s + 1) * 128]) for s in range(16)] for j in range(8)]

        with ExitStack() as p0:
            cT = sb("cT", [128, 8], F32, p0)
            cs = sb("cs", [128, 8], F32, p0)
            bada = sb("bada", [1, 6 * D], F32, p0)
            modrow = sb("modrow", [1, 6 * D], F32, p0)
            one1 = sb("one1", [1, 1], F32, p0)
            n1g = sb("n1g", [128, 8], F32, p0)
            n2g = sb("n2g", [128, 8], F32, p0)
            wab = [sb(f"wab{i}", [128, 8, 512], F32, p0) for i in range(2)]
            WAB = [Buf(w[:]) for w in wab]
            SM = Buf(cT[:])
            MR = Buf(modrow[:])
            kb.dma("sync", cT[:], I["cT"], outs=[SM])
            kb.dma("sync", bada[:], I["b_ada"], outs=[SM])
            kb.dma("sync", n1g[:], I["n1g"], outs=[SM])
            kb.dma("sync", n2g[:], I["n2g"], outs=[SM])
            kb.op("vector", lambda e: e.memset(one1[:], 1.0), outs=[SM])
            kb.op("scalar", lambda e: e.activation(out=cs[:], in_=cT[:], func=AF.Silu), outs=[SM], ins=[SM])
            wada_v = I["w_ada"].rearrange("(k p) c -> p k c", p=128)
            for cb in range(12):
                W = WAB[cb % 2]
                kb.dma("sync" if cb % 2 == 0 else "gpsimd", wab[cb % 2][:], wada_v[:, :, cb * 512:(cb + 1) * 512], outs=[W])
                P = PS[cb % 2]
                for k in range(8):
                    kb.op("tensor", lambda e, k=k, cb=cb: e.matmul(ps[cb % 2][0:1, :], lhsT=cs[:, k:k + 1], rhs=wab[cb % 2][:, k, :],
                                                                 start=(k == 0), stop=(k == 7)),
                          outs=[P], ins=[SM, W], mark=(k == 7))
                kb.op("vector", lambda e, cb=cb: e.tensor_tensor(out=modrow[0:1, cb * 512:(cb + 1) * 512], in0=ps[cb % 2][0:1, :],
                                                                  in1=bada[0:1, cb * 512:(cb + 1) * 512], op=ALU.add),
                      outs=[MR], ins=[P, SM])
            P = PS[2]
            for j in range(48):
                kb.op("tensor", lambda e, j=j: e.matmul(ps[2][:, j:j + 1], lhsT=modrow[0:1, j * 128:(j + 1) * 128], rhs=one1[0:1, 0:1],
                                                       start=True, stop=True), outs=[P], ins=[MR, SM], mark=(j == 47))
            kb.op("vector", lambda e: e.tensor_copy(out=modcol[:], in_=ps[2][:, 0:48]), outs=[MOD], ins=[P])
            kb.op("vector", lambda e: e.scalar_tensor_tensor(out=a1[:], in0=modcol[:, 8:16], scalar=1.0, in1=n1g[:], op0=ALU.add, op1=ALU.mult),
                  outs=[MOD], ins=[MOD, SM])
            kb.op("vector", lambda e: e.scalar_tensor_tensor(out=a2[:], in0=modcol[:, 32:40], scalar=1.0, in1=n2g[:], op0=ALU.add, op1=ALU.mult),
                  outs=[MOD], ins=[MOD, SM])
            if "mod" in dbg:
                dump("mod", modcol[:], [128, 48])
            kb.barrier()
        sh1 = lambda k: modcol[:, k:k + 1]
        g1c = lambda k: modcol[:, 16 + k:17 + k]
        sh2 = lambda k: modcol[:, 24 + k:25 + k]
        g2c = lambda k: modcol[:, 40 + k:41 + k]

        if stop_after == "p0":
            kb.barrier()
            return nc, dbg_out

        with ExitStack() as p1:
            hT = sb("hT", [128, 8, S], BF16, p1)
            HT = [Buf(hT[:, :, n * 512:(n + 1) * 512]) for n in range(NB)]
            with ExitStack() as p1a:
                xb = [sb(f"xb{i}", [128, 8, 512], F32, p1a) for i in range(2)]
                XB = [Buf(t[:]) for t in xb]
                sq = sb("sq", [128, 8, 512], F32, p1a)
                SQ = Buf(sq[:])
                rstd = sb("rstd", [128, 512], F32, p1a)
                RS = Buf(rstd[:])
                tmp = [sb(f"tmp{i}", [128, 512], F32, p1a) for i in range(2)]
                TMP = [Buf(t[:]) for t in tmp]
                xT_v = I["xT"].rearrange("(k p) t -> p k t", p=128)
                for n in range(NB):
                    X = XB[n % 2]
                    x_ = xb[n % 2]
                    kb.dma("sync" if n % 2 == 0 else "gpsimd", x_[:], xT_v[:, :, n * 512:(n + 1) * 512], outs=[X])
                    kb.op("scalar", lambda e, x_=x_: e.activation(out=sq[:], in_=x_[:], func=AF.Square), outs=[SQ], ins=[X])
                    P = PS[n % 2]
                    for k in range(8):
                        kb.op("tensor", lambda e, k=k, n=n: e.matmul(ps[n % 2][:], lhsT=onesf[:], rhs=sq[:, k, :], start=(k == 0), stop=(k == 7)),
                              outs=[P], ins=[SQ, CST], mark=(k == 7))
                    kb.op("scalar", lambda e, n=n: e.activation(out=rstd[:], in_=ps[n % 2][:], func=AF.Sqrt, scale=1.0 / D, bias=EPS),
                          outs=[RS], ins=[P])
                    kb.op("vector", lambda e: e.reciprocal(out=rstd[:], in_=rstd[:]), outs=[RS], ins=[RS])
                    for k in range(8):
                        T = TMP[k % 2]
                        t_ = tmp[k % 2]
                        kb.op("vector", lambda e, k=k, t_=t_, x_=x_: e.tensor_tensor(out=t_[:], in0=x_[:, k, :], in1=rstd[:], op=ALU.mult),
                              outs=[T], ins=[X, RS])
                        kb.op("scalar", lambda e, k=k, t_=t_, n=n: e.activation(out=hT[:, k, n * 512:(n + 1) * 512], in_=t_[:], func=AF.Identity,
                                                                            scale=a1[:, k:k + 1], bias=sh1(k)),
                              outs=[HT[n]], ins=[T, MOD])
                kb.barrier()
            if "hT" in dbg:
                dump("hT", hT[:], [128, 8, S], BF16)
            if stop_after == "p1a":
                kb.barrier()
                return nc, dbg_out

            rr = [0]

            def bank():
                rr[0] = (rr[0] + 1) % 8
                return rr[0]

            w_in_v = I["w_in"].rearrange("(k p) c -> p k c", p=128)

            with ExitStack() as ph:
                lbl = sb("lbl", [128, 2, 4], F32, ph)
                lb = sb("lb", [128, 4], F32, ph)
                oml = sb("oml", [128, 4], F32, ph)
                hng = sb("hng", [128, 512], F32, ph)
                HC = Buf(lbl[:])
                kb.dma("sync", lbl[:], I["lbl"], outs=[HC])
                kb.dma("sync", hng[:], I["hng"], outs=[HC])
                kb.op("vector", lambda e: e.tensor_tensor(out=lb[:], in0=lbl[:, 0, :], in1=lbl[:, 1, :], op=ALU.subtract), outs=[HC], ins=[HC])
                kb.op("scalar", lambda e: e.activation(out=lb[:], in_=lb[:], func=AF.Sigmoid), outs=[HC], ins=[HC])
                kb.op("vector", lambda e: e.tensor_scalar(out=oml[:], in0=lb[:], scalar1=-1.0, scalar2=1.0, op0=ALU.mult, op1=ALU.add), outs=[HC], ins=[HC])
                wq = sb("wq", [128, 8, 128], BF16, ph)
                wf = sb("wf", [128, 8, 128], BF16, ph)
                wig = sb("wig", [128, 8, 256], BF16, ph)
                WQ, WF, WIG = Buf(wq[:]), Buf(wf[:]), Buf(wig[:])
                Q1 = sb("Q1", [128, S], BF16, ph)
                Q2 = sb("Q2", [128, S], BF16, ph)
                Kt = sb("Kt", [128, S], BF16, ph)
                Kh = sb("Kh", [128, NT, 128], BF16, ph)
                Vh = sb("Vh", [128, NT, 128], BF16, ph)
                SGt = sb("SGt", [128, NT, 128], BF16, ph)
                ebl = sb("ebl", [128, NT], F32, ph)
                BQ = [Buf(Q1[:, n * 512:(n + 1) * 512]) for n in range(NB)]
                BKH = [Buf(Kh[:, t, :]) for t in range(NT)]
                BV = [Buf(Vh[:, t, :]) for t in range(NT)]
                tn = ["f", "lf", "b", "d1", "d2", "eb", "e1", "en1", "el", "k"]
                T_ = {n_: sb("t_" + n_, [128, 512], F32, ph) for n_ in tn}
                TB = {n_: Buf(T_[n_][:]) for n_ in tn}
                khtb = sb("khtb", [128, 512], BF16, ph)
                KHTB = Buf(khtb[:])
                vmask = sb("vmask", [128, 32], F32, ph)
                kb.dma("sync", vmask[:], I["vmask"], outs=[HC])
                Sst = sb("Sst", [128, 128], F32, ph)
                SST = Buf(Sst[:])
                sbf = [sb(f"sbf{i}", [128, 128], BF16, ph) for i in range(2)]
                SBF = [Buf(t[:]) for t in sbf]
                atm = [sb(f"atm{i}", [128, 128], BF16, ph) for i in range(2)]
                ATM = [Buf(t[:]) for t in atm]
                for i in range(2):
                    kb.op("vector", lambda e, i=i: e.memset(atm[i][:], 0.0), outs=[ATM[i]])
                junk = sb("junk", [128, 128], F32, ph)
                JK = Buf(junk[:])
                ssq = [sb(f"ssq{i}", [128, 1], F32, ph) for i in range(2)]
                SSQ = [Buf(t[:]) for t in ssq]
                of = [sb(f"of{i}", [128, 128], F32, ph) for i in range(2)]
                OF = [Buf(t[:]) for t in of]
                obf = [sb(f"obf{i}", [128, 128], BF16, ph) for i in range(2)]
                OBF = [Buf(t[:]) for t in obf]
                v4 = lambda ap: ap.rearrange("p (c t) -> p c t", t=128)
                for hd in range(int(_os0.environ.get("NHEADS", "4"))):
                    c0 = hd * 128
                    kb.dma("gpsimd", wq[:], w_in_v[:, :, c0:c0 + 128], outs=[WQ])
                    kb.dma("gpsimd", wf[:], w_in_v[:, :, 512 + c0:512 + c0 + 128], outs=[WF])
                    kb.dma("gpsimd", wig[:, :, 0:128], w_in_v[:, :, 1024 + c0:1024 + c0 + 128], outs=[WIG])
                    kb.dma("gpsimd", wig[:, :, 128:256], w_in_v[:, :, 1536 + c0:1536 + c0 + 128], outs=[WIG])
                    for n in range(NB):
                        sl = slice(n * 512, (n + 1) * 512)
                        own = n >= 4
                        bq_, bf_ = bank(), bank()
                        if own:
                            for k in range(8):
                                kb.op("tensor", lambda e, k=k, bq_=bq_, sl=sl: e.matmul(ps[bq_][:], lhsT=wq[:, k, :], rhs=hT[:, k, sl], start=(k == 0), stop=(k == 7)),
                                      outs=[PS[bq_]], ins=[WQ, HT[n]], mark=(k == 7))
                        for k in range(8):
                            kb.op("tensor", lambda e, k=k, bf_=bf_, sl=sl: e.matmul(ps[bf_][:], lhsT=wf[:, k, :], rhs=hT[:, k, sl], start=(k == 0), stop=(k == 7)),
                                  outs=[PS[bf_]], ins=[WF, HT[n]], mark=(k == 7))
                        t = T_
                        kb.op("scalar", lambda e, bf_=bf_: e.activation(out=t["f"][:], in_=ps[bf_][:], func=AF.Sigmoid), outs=[TB["f"]], ins=[PS[bf_]])
                        kb.op("vector", lambda e, hd=hd: e.tensor_scalar(out=t["f"][:], in0=t["f"][:], scalar1=oml[:, hd:hd + 1], scalar2=lb[:, hd:hd + 1],
                                                                     op0=ALU.mult, op1=ALU.add), outs=[TB["f"]], ins=[TB["f"], HC])
                        kb.op("scalar", lambda e: e.activation(out=t["lf"][:], in_=t["f"][:], func=AF.Ln), outs=[TB["lf"]], ins=[TB["f"]])
                        kb.op("gpsimd", lambda e: e.tensor_scalar(out=t["k"][:], in0=t["f"][:], scalar1=-1.0, scalar2=1.0, op0=ALU.mult, op1=ALU.add),
                              outs=[TB["k"]], ins=[TB["f"]])
                        kb.op("vector", lambda e: e.tensor_tensor_scan(out=t["b"][:], data0=segm[:], data1=t["lf"][:], initial=0.0, op0=ALU.mult, op1=ALU.add),
                              outs=[TB["b"]], ins=[TB["lf"], CST])
                        if own:
                            kb.op("vector", lambda e: e.tensor_tensor(out=v4(t["d1"][:]), in0=v4(t["b"][:]), in1=v4(t["b"][:])[:, :, 63:64].to_broadcast([128, 4, 128]),
                                                                      op=ALU.subtract), outs=[TB["d1"]], ins=[TB["b"]])
                        kb.op("vector", lambda e: e.tensor_tensor(out=v4(t["d2"][:]), in0=v4(t["b"][:])[:, :, 127:128].to_broadcast([128, 4, 128]), in1=v4(t["b"][:]),
                                                                  op=ALU.subtract), outs=[TB["d2"]], ins=[TB["b"]])
                        kb.op("scalar", lambda e: e.activation(out=t["eb"][:], in_=t["b"][:], func=AF.Exp), outs=[TB["eb"]], ins=[TB["b"]])
                        if own:
                            kb.op("scalar", lambda e: e.activation(out=t["e1"][:], in_=t["d1"][:], func=AF.Exp), outs=[TB["e1"]], ins=[TB["d1"]])
                            kb.op("scalar", lambda e: e.activation(out=t["en1"][:], in_=t["d1"][:], func=AF.Exp, scale=-1.0), outs=[TB["en1"]], ins=[TB["d1"]])
                        kb.op("scalar", lambda e: e.activation(out=t["el"][:], in_=t["d2"][:], func=AF.Exp), outs=[TB["el"]], ins=[TB["d2"]])
                        sc_ = 128.0 ** -0.5
                        if own:
                            kb.op("vector", lambda e, bq_=bq_, sl=sl: e.scalar_tensor_tensor(out=Q1[:, sl], in0=ps[bq_][:], scalar=sc_, in1=t["e1"][:], op0=ALU.mult, op1=ALU.mult),
                                  outs=[BQ[n]], ins=[PS[bq_], TB["e1"]])
                            kb.op("vector", lambda e, bq_=bq_, sl=sl: e.scalar_tensor_tensor(out=Q2[:, sl], in0=ps[bq_][:], scalar=sc_, in1=t["eb"][:], op0=ALU.mult, op1=ALU.mult),
                                  outs=[BQ[n]], ins=[PS[bq_], TB["eb"]])
                            kb.op("gpsimd", lambda e, sl=sl: e.tensor_tensor(out=Kt[:, sl], in0=t["k"][:], in1=t["en1"][:], op=ALU.mult), outs=[BQ[n]], ins=[TB["k"], TB["en1"]])
                        kb.op("gpsimd", lambda e: e.tensor_tensor(out=khtb[:], in0=t["k"][:], in1=t["el"][:], op=ALU.mult), outs=[KHTB], ins=[TB["k"], TB["el"]])
                        kb.op("gpsimd", lambda e, n=n: e.tensor_copy(out=ebl[:, 4 * n:4 * n + 4], in_=v4(t["eb"][:])[:, :, 127]), outs=[BQ[n]], ins=[TB["eb"]])
                        for i in range(4):
                            bk = bank()
                            kb.op("tensor", lambda e, i=i, bk=bk: e.matmul(ps[bk][:, 0:128], lhsT=khtb[:, i * 128:(i + 1) * 128], rhs=ident[:], start=True, stop=True),
                                  outs=[PS[bk]], ins=[KHTB, CST])
                            kb.op("scalar", lambda e, i=i, bk=bk, n=n: e.copy(out=Kh[:, 4 * n + i, :], in_=ps[bk][:, 0:128]), outs=[BKH[4 * n + i]], ins=[PS[bk]])
                    for tt in range(NT):
                        bk = bank()
                        n = tt // 4
                        for k in range(8):
                            kb.op("tensor", lambda e, k=k, bk=bk, tt=tt: e.matmul(ps[bk][:, 0:256], lhsT=hT[:, k, tt * 128:(tt + 1) * 128], rhs=wig[:, k, :],
                                                                                 start=(k == 0), stop=(k == 7)),
                                  outs=[PS[bk]], ins=[WIG, HT[n]], mark=(k == 7))
                        kb.op("vector", lambda e, bk=bk, tt=tt: e.tensor_scalar(out=Vh[:, tt, :], in0=ps[bk][:, 0:128], scalar1=vmask[:, tt:tt + 1], scalar2=None, op0=ALU.mult),
                              outs=[BV[tt]], ins=[PS[bk], HC])
                        kb.op("scalar", lambda e, bk=bk, tt=tt: e.activation(out=SGt[:, tt, :], in_=ps[bk][:, 128:256], func=AF.Silu), outs=[BV[tt]], ins=[PS[bk]])
                    kb.op("vector", lambda e: e.memset(Sst[:], 0.0), outs=[SST])
                    at_bank = {}

                    def emit_at(c):
                        bk = bank()
                        at_bank[c] = bk
                        cs_ = slice(c * 128, (c + 1) * 128)
                        c0_ = c * 128
                        kb.op("tensor", lambda e: e.matmul(ps[bk][0:64, 0:64], lhsT=Kt[:, c0_:c0_ + 64], rhs=Q1[:, c0_:c0_ + 64], start=True, stop=True),
                              outs=[PS[bk]], ins=[BQ[c // 4]], mark=False)
                        kb.op("tensor", lambda e: e.matmul(ps[bk][:, 64:128], lhsT=Kt[:, cs_], rhs=Q1[:, c0_ + 64:c0_ + 128], start=True, stop=True),
                              outs=[PS[bk]], ins=[BQ[c // 4]])
                        kb.op("vector", lambda e: e.tensor_tensor(out=atm[c % 2][0:64, 0:64], in0=ps[bk][0:64, 0:64], in1=hmask[0:64, 0:64], op=ALU.mult),
                              outs=[ATM[c % 2]], ins=[PS[bk], CST])
                        kb.op("vector", lambda e: e.tensor_tensor(out=atm[c % 2][:, 64:128], in0=ps[bk][:, 64:128], in1=hmask[:, 64:128], op=ALU.mult),
                              outs=[ATM[c % 2]], ins=[PS[bk], CST])

                    for c in range(NT):
                        if c + 1 < NT and c + 1 >= 16:
                            emit_at(c + 1)
                        cs_ = slice(c * 128, (c + 1) * 128)
                        bd = bank()
                        kb.op("tensor", lambda e, bd=bd, c=c: e.matmul(ps[bd][:, 0:128], lhsT=Kh[:, c, :], rhs=Vh[:, c, :], start=True, stop=True),
                              outs=[PS[bd]], ins=[BKH[c], BV[c]])
                        if c >= 16:
                            bo = bank()
                            kb.op("tensor", lambda e, bo=bo, c=c: e.matmul(ps[bo][:, 0:128], lhsT=atm[c % 2][:], rhs=Vh[:, c, :], start=True, stop=False),
                                  outs=[PS[bo]], ins=[ATM[c % 2], BV[c]], mark=False)
                            kb.op("tensor", lambda e, bo=bo, c=c, cs_=cs_: e.matmul(ps[bo][:, 0:128], lhsT=Q2[:, cs_], rhs=sbf[(c - 1) % 2][:], start=False, stop=True),
                                  outs=[PS[bo]], ins=[BQ[c // 4], SBF[(c - 1) % 2]])
                        if c + 1 < NT:
                            kb.op("vector", lambda e, bd=bd, c=c: e.scalar_tensor_tensor(out=Sst[:], in0=Sst[:], scalar=ebl[:, c:c + 1], in1=ps[bd][:, 0:128],
                                                                                     op0=ALU.mult, op1=ALU.add), outs=[SST], ins=[SST, PS[bd], BQ[c // 4]])
                            kb.op("scalar", lambda e, c=c: e.copy(out=sbf[c % 2][:], in_=Sst[:]), outs=[SBF[c % 2]], ins=[SST])
                        if c < 16:
                            continue
                        i2 = c % 2
                        kb.op("gpsimd", lambda e, i2=i2: e.memset(ssq[i2][:], 0.0), outs=[SSQ[i2]])
                        kb.op("scalar", lambda e, bo=bo, i2=i2: e.activation(out=junk[:], in_=ps[bo][:, 0:128], func=AF.Square, accum_out=ssq[i2][:]),
                              outs=[JK, SSQ[i2]], ins=[PS[bo]])
                        kb.op("scalar", lambda e, i2=i2: e.activation(out=ssq[i2][:], in_=ssq[i2][:], func=AF.Sqrt, scale=1.0 / 128, bias=EPS), outs=[SSQ[i2]], ins=[SSQ[i2]])
                        kb.op("vector", lambda e, i2=i2: e.reciprocal(out=ssq[i2][:], in_=ssq[i2][:]), outs=[SSQ[i2]], ins=[SSQ[i2]])
                        kb.op("vector", lambda e, bo=bo, i2=i2, c0=c0: e.scalar_tensor_tensor(out=of[i2][:], in0=ps[bo][:, 0:128], scalar=ssq[i2][:, 0:1], in1=hng[:, c0:c0 + 128],
                                                                                           op0=ALU.mult, op1=ALU.mult), outs=[OF[i2]], ins=[PS[bo], SSQ[i2], HC])
                        kb.op("gpsimd", lambda e, i2=i2, c=c: e.tensor_tensor(out=obf[i2][:], in0=of[i2][:], in1=SGt[:, c, :], op=ALU.mult), outs=[OBF[i2]], ins=[OF[i2], BV[c]])
                        bt = bank()
                        kb.op("tensor", lambda e, bt=bt, i2=i2: e.matmul(ps[bt][:, 0:128], lhsT=obf[i2][:], rhs=ident[:], start=True, stop=True),
                              outs=[PS[bt]], ins=[OBF[i2], CST])
                        s_ = c - 16
                        kb.op("scalar", lambda e, bt=bt, s_=s_, hd=hd: e.copy(out=oT[:, hd, s_ * 128:(s_ + 1) * 128], in_=ps[bt][:, 0:128]), outs=[OT[hd][s_]], ins=[PS[bt]])
                kb.barrier()
            if "oT" in dbg:
                dump("oT", oT[:], [128, 8, SO], BF16)
            if stop_after == "p1b":
                kb.barrier()
                return nc, dbg_out

            SCL = 64.0 ** -0.5
            if not enable_nsa:
                for jf in range(4, 8):
                    kb.op("vector", lambda e, jf=jf: e.memset(oT[:, jf, :], 0.0), outs=OT[jf])
            with _NSAScope() as pn:
                if not enable_nsa:
                    raise _SkipNSA()
                ovl = sb("ovl", [128, 2, 64], BF16, pn)
                kb.dma("sync", ovl[:], I["ovl"], outs=[CST])
                KEe = sb("KEe", [128, S], BF16, pn)
                KEo = sb("KEo", [128, S], BF16, pn)
                ekt_v = I["ekt"].rearrange("p a b -> p (a b)")
                kb.dma("sync", KEe[64:128, :], ekt_v[64:128, :], outs=[CST])
                kb.dma("sync", KEo[0:64, :], ekt_v[0:64, :], outs=[CST])
                kwT = sb("kwT", [128, S], BF16, pn)
                kcvT = sb("kcvT", [128, S], BF16, pn)
                vs1 = sb("vs1", [128, NT, 80], BF16, pn)
                vw1 = sb("vw1", [128, NT, 80], BF16, pn)
                KS = Buf(kwT[:])
                kcmpT = sb("kcmpT", [128, 256], BF16, pn)
                vcmp1 = sb("vcmp1", [128, 2, 144], BF16, pn)
                KC = Buf(kcmpT[:])
                wk3 = sb("wk3", [128, 8, 384], BF16, pn)
                wv2 = sb("wv2", [128, 8, 128], BF16, pn)
                wqg = sb("wqg", [128, 8, 256], BF16, pn)
                wgt = sb("wgt", [128, 8, 12], BF16, pn)
                WN = Buf(wk3[:])
                cosb = sb("cosb", [128, 512], F32, pn)
                sinb = sb("sinb", [128, 512], F32, pn)
                CSB = Buf(cosb[:])
                rawb = sb("rawb", [128, 512], BF16, pn)
                RAWB = Buf(rawb[:])
                rt1 = sb("rt1", [128, 512], F32, pn)
                rt2 = sb("rt2", [128, 512], F32, pn)
                RT1, RT2 = Buf(rt1[:]), Buf(rt2[:])
                _padn = int(_os0.environ.get("PADN", "0"))
                if _padn:
                    _pad = sb("padn", [128, _padn], F32, pn)
                srr = [0]

                def sbank():
                    srr[0] = (srr[0] + 1) % 3
                    return srr[0]

                mrr = [0]

                def mbank():
                    return 7

                import os as _os
                _dbgmode = int(_os.environ.get("ROPEDBG", "0"))

                def rope_from(bk, dst_ap, dstbuf):
                    if _dbgmode == 1:
                        kb.op("scalar", lambda e: e.copy(out=dst_ap, in_=ps[bk][:]), outs=[dstbuf], ins=[PS[bk]])
                        return
                    if _dbgmode == 3:
                        kb.op("vector", lambda e: e.tensor_tensor(out=rt1[:], in0=ps[bk][:], in1=cosb[:], op=ALU.mult), outs=[RT1], ins=[PS[bk], CSB])
                        kb.op("gpsimd", lambda e: e.tensor_copy(out=dst_ap, in_=rt1[:]), outs=[dstbuf], ins=[RT1])
                        return
                    if _dbgmode == 4:
                        kb.op("scalar", lambda e: e.copy(out=rawb[:], in_=ps[bk][:]), outs=[RAWB], ins=[PS[bk]])
                        b2 = mbank()
                        kb.op("tensor", lambda e: e.matmul(ps[b2][:], lhsT=pswap[:], rhs=rawb[:], start=True, stop=True), outs=[PS[b2]], ins=[RAWB, CST])
                        kb.op("vector", lambda e: e.tensor_tensor(out=rt1[:], in0=ps[bk][:], in1=cosb[:], op=ALU.mult), outs=[RT1], ins=[PS[bk], CSB])
                        kb.op("vector", lambda e: e.tensor_tensor(out=rt2[:], in0=ps[b2][:], in1=sinb[:], op=ALU.mult), outs=[RT2], ins=[PS[b2], CSB])
                        kb.op("vector", lambda e: e.tensor_tensor(out=dst_ap, in0=rt1[:], in1=rt2[:], op=ALU.add), outs=[dstbuf], ins=[RT1, RT2])
                        return
                    if _dbgmode == 5:
                        kb.op("scalar", lambda e: e.copy(out=rawb[:], in_=ps[bk][:]), outs=[RAWB], ins=[PS[bk]])
                        b2 = mbank()
                        kb.op("tensor", lambda e: e.matmul(ps[b2][:], lhsT=pswap[:], rhs=rawb[:], start=True, stop=True), outs=[PS[b2]], ins=[RAWB, CST])
                        kb.op("vector", lambda e: e.tensor_tensor(out=rt1[:], in0=ps[bk][:], in1=cosb[:], op=ALU.mult), outs=[RT1], ins=[PS[bk], CSB])
                        kb.op("scalar", lambda e: e.copy(out=rt2[:], in_=ps[b2][:]), outs=[RT2], ins=[PS[b2]])
                        _sb = cosb if _os.environ.get("USECOS") else sinb
                        kb.op("vector", lambda e: e.tensor_tensor(out=rt2[:], in0=rt2[:], in1=_sb[:], op=ALU.mult), outs=[RT2], ins=[RT2, CSB])
                        kb.op("vector", lambda e: e.tensor_tensor(out=dst_ap, in0=rt1[:], in1=rt2[:], op=ALU.add), outs=[dstbuf], ins=[RT1, RT2])
                        return
                    if _dbgmode in (7, 8):
                        kb.op("scalar", lambda e: e.copy(out=rawb[:], in_=ps[bk][:]), outs=[RAWB], ins=[PS[bk]])
                        b2 = mbank()
                        kb.op("tensor", lambda e: e.matmul(ps[b2][:], lhsT=pswap[:], rhs=rawb[:], start=True, stop=True), outs=[PS[b2]], ins=[RAWB, CST])
                        kb.op("vector", lambda e: e.tensor_tensor(out=rt1[:], in0=ps[bk][:], in1=cosb[:], op=ALU.mult), outs=[RT1], ins=[PS[bk], CSB])
                        kb.op("scalar", lambda e: e.copy(out=rt2[:], in_=ps[b2][:]), outs=[RT2], ins=[PS[b2]])
                        kb.op("vector", lambda e: e.tensor_tensor(out=rt2[:], in0=rt2[:], in1=sinb[:], op=ALU.mult), outs=[RT2], ins=[RT2, CSB])
                        if _dbgmode == 8:
                            kb.op("vector", lambda e: e.tensor_tensor(out=rt1[:], in0=rt1[:], in1=rt2[:], op=ALU.add), outs=[RT1], ins=[RT1, RT2])
                        kb.op("scalar", lambda e: e.copy(out=dst_ap, in_=rt1[:]), outs=[dstbuf], ins=[RT1])
                        return
                    if _dbgmode in (9, 10):
                        kb.op("vector", lambda e: e.tensor_tensor(out=rt1[:], in0=ps[bk][:], in1=cosb[:], op=ALU.mult), outs=[RT1], ins=[PS[bk], CSB])
                        if _dbgmode == 9:
                            kb.op("scalar", lambda e: e.copy(out=rt2[:], in_=ps[bk][:]), outs=[RT2], ins=[PS[bk]])
                        else:
                            kb.op("vector", lambda e: e.tensor_tensor(out=rt2[:], in0=rt1[:], in1=cosb[:], op=ALU.mult), outs=[RT2], ins=[RT1, CSB])
                        kb.op("gpsimd", lambda e: e.tensor_copy(out=dst_ap, in_=rt1[:]), outs=[dstbuf], ins=[RT1])
                        return
                    if _dbgmode in (11, 12):
                        kb.op("scalar", lambda e: e.copy(out=rawb[:], in_=ps[bk][:]), outs=[RAWB], ins=[PS[bk]])
                        b2 = mbank()
                        kb.op("tensor", lambda e: e.matmul(ps[b2][:], lhsT=pswap[:], rhs=rawb[:], start=True, stop=True), outs=[PS[b2]], ins=[RAWB, CST])
                        kb.op("vector", lambda e: e.tensor_tensor(out=rt1[:], in0=ps[bk][:], in1=cosb[:], op=ALU.mult), outs=[RT1], ins=[PS[bk], CSB, RAWB])
                        kb.op("gpsimd", lambda e: e.tensor_copy(out=dst_ap, in_=rt1[:]), outs=[dstbuf], ins=[RT1])
                        if _dbgmode == 12:
                            return
                        kb.op("vector", lambda e: e.tensor_tensor(out=rt1[:], in0=ps[b2][:], in1=sinb[:], op=ALU.mult), outs=[RT1], ins=[PS[b2], CSB])
                        kb.op("gpsimd", lambda e: e.tensor_tensor(out=dst_ap, in0=dst_ap, in1=rt1[:], op=ALU.add), outs=[dstbuf], ins=[RT1, dstbuf])
                        return
                    if _dbgmode == 2:
                        kb.op("scalar", lambda e: e.copy(out=rawb[:], in_=ps[bk][:]), outs=[RAWB], ins=[PS[bk]])
                        b2 = mbank()
                        kb.op("tensor", lambda e: e.matmul(ps[b2][:], lhsT=pswap[:], rhs=rawb[:], start=True, stop=True), outs=[PS[b2]], ins=[RAWB, CST])
                        kb.op("scalar", lambda e: e.copy(out=dst_ap, in_=ps[b2][:]), outs=[dstbuf], ins=[PS[b2]])
                        return
                    kb.op("scalar", lambda e: e.copy(out=rawb[:], in_=ps[bk][:]), outs=[RAWB], ins=[PS[bk]])
                    b2 = mbank()
                    kb.op("tensor", lambda e: e.matmul(ps[b2][:], lhsT=pswap[:], rhs=rawb[:], start=True, stop=True), outs=[PS[b2]], ins=[RAWB, CST])
                    kb.op("vector", lambda e: e.tensor_tensor(out=rt1[:], in0=ps[bk][:], in1=cosb[:], op=ALU.mult), outs=[RT1], ins=[PS[bk], CSB])
                    kb.op("vector", lambda e: e.tensor_tensor(out=rt2[:], in0=ps[b2][:], in1=sinb[:], op=ALU.mult), outs=[RT2], ins=[PS[b2], CSB])
                    if isinstance(dst_ap, tuple):
                        kb.op("gpsimd", lambda e: e.tensor_tensor(out=dst_ap[0], in0=rt1[0:64, :], in1=rt2[0:64, :], op=ALU.add), outs=[dstbuf], ins=[RT1, RT2])
                        kb.op("gpsimd", lambda e: e.tensor_tensor(out=dst_ap[1], in0=rt1[64:128, :], in1=rt2[64:128, :], op=ALU.add), outs=[dstbuf], ins=[RT1, RT2])
                    else:
                        kb.op("gpsimd", lambda e: e.tensor_tensor(out=dst_ap, in0=rt1[:], in1=rt2[:], op=ALU.add), outs=[dstbuf], ins=[RT1, RT2])

                for g in range(2):
                    for j, cbase in enumerate((2560, 2688)):
                        kb.dma("gpsimd", wk3[:, :, j * 64:(j + 1) * 64], w_in_v[:, :, cbase + g * 64:cbase + g * 64 + 64], outs=[WN])
                    for j, cbase in enumerate((2816, 2816, 3072, 3072)):
                        kb.dma("gpsimd", wk3[:, :, 128 + j * 64:128 + (j + 1) * 64], w_in_v[:, :, cbase + g * 64:cbase + g * 64 + 64], outs=[WN])
                    for j, cbase in enumerate((2944, 3200)):
                        kb.dma("gpsimd", wv2[:, :, j * 64:(j + 1) * 64], w_in_v[:, :, cbase + g * 64:cbase + g * 64 + 64], outs=[WN])
                    kb.dma("gpsimd", wqg[:], w_in_v[:, :, 2048 + g * 256:2048 + (g + 1) * 256], outs=[WN])
                    kb.dma("gpsimd", wgt[:], w_in_v[:, :, 3328 + g * 12:3328 + (g + 1) * 12], outs=[WN])
                    kb.op("vector", lambda e: e.memset(vs1[:, :, 64:65], 1.0), outs=[KS])
                    kb.op("vector", lambda e: e.memset(vw1[:, :, 64:65], 1.0), outs=[KS])
                    if stop_after == "p1c_a":
                        dump("wk3", wk3[:], [128, 8, 384], BF16)
                        return nc, dbg_out
                    for n in range(NB):
                        sl = slice(n * 512, (n + 1) * 512)
                        kb.dma("sync", cosb[:], I["cosT"][:, sl], outs=[CSB])
                        kb.dma("sync", sinb[:], I["sinT"][:, sl], outs=[CSB])
                        for j in range(3):
                            bk = sbank()
                            for k in range(8):
                                kb.op("tensor", lambda e, k=k, bk=bk, j=j: e.matmul(ps[bk][:], lhsT=wk3[:, k, j * 128:(j + 1) * 128], rhs=hT[:, k, sl],
                                                                                    start=(k == 0), stop=(k == 7)), outs=[PS[bk]], ins=[WN, HT[n]], mark=(k == 7))
                            if j == 0:
                                kb.op("scalar", lambda e, bk=bk: e.copy(out=kcvT[:, sl], in_=ps[bk][:]), outs=[KS], ins=[PS[bk]])
                            else:
                                rope_from(bk, (KEe[0:64, sl], KEo[64:128, sl]) if j == 1 else kwT[:, sl], KS)
                        if stop_after == "p1c_b":
                                return nc, dbg_out
                        for i in range(4):
                            tt = 4 * n + i
                            bk = mbank()
                            for k in range(8):
                                kb.op("tensor", lambda e, k=k, bk=bk, tt=tt: e.matmul(ps[bk][:, 0:128], lhsT=hT[:, k, tt * 128:(tt + 1) * 128], rhs=wv2[:, k, :],
                                                                                     start=(k == 0), stop=(k == 7)), outs=[PS[bk]], ins=[WN, HT[n]], mark=(k == 7))
                            kb.op("scalar", lambda e, bk=bk, tt=tt: e.copy(out=vs1[:, tt, 0:64], in_=ps[bk][:, 0:64]), outs=[KS], ins=[PS[bk]])
                            kb.op("vector", lambda e, bk=bk, tt=tt: e.tensor_copy(out=vw1[:, tt, 0:64], in_=ps[bk][:, 64:128]), outs=[KS], ins=[PS[bk]])
                    if stop_after == "p1c_k":
                        dump("kcvT", kcvT[:], [128, S], BF16)
                        dump("vs1", vs1[:], [128, NT, 80], BF16)
                        return nc, dbg_out
                    with ExitStack() as pc:
                        w1kv = sb("w1kv", [128, 32, 256], BF16, pc)
                        peT = sb("peT", [128, 32], F32, pc)
                        peTb = sb("peTb", [128, 32], BF16, pc)
                        b1kv = sb("b1kv", [128, 4], F32, pc)
                        w2k2 = sb("w2k2", [128, 2, 128], BF16, pc)
                        w2v = sb("w2v", [128, 2, 64], BF16, pc)
                        hid = sb("hid", [128, 4, 256], BF16, pc)
                        beff = sb("beff", [128, 4], F32, pc)
                        gx = sb("gx", [128, 256], F32, pc)
                        gu = sb("gu", [128, 256], F32, pc)
                        gs = sb("gs", [128, 256], F32, pc)
                        CW = Buf(w1kv[:])
                        HID = Buf(hid[:])
                        GX = Buf(gx[:])
                        kb.dma("gpsimd", w1kv[0:64], I["w1k"], outs=[CW])
                        kb.dma("gpsimd", w1kv[64:128], I["w1v"], outs=[CW])
                        kb.dma("sync", peT[0:64], I["peTk"], outs=[CW])
                        kb.dma("sync", peT[64:128], I["peTv"], outs=[CW])
                        kb.dma("sync", b1kv[:, 0:2], I["b1k"], outs=[CW])
                        kb.dma("sync", b1kv[:, 2:4], I["b1v"], outs=[CW])
                        kb.dma("gpsimd", w2k2[:, :, 0:64], I["w2k"], outs=[CW])
                        kb.dma("gpsimd", w2k2[:, :, 64:128], I["w2k"], outs=[CW])
                        kb.dma("gpsimd", w2v[:], I["w2v"], outs=[CW])
                        kb.op("vector", lambda e: e.tensor_copy(out=peTb[:], in_=peT[:]), outs=[CW], ins=[CW])
                        kb.op("vector", lambda e: e.memset(hid[:], 0.0), outs=[HID])
                        for kv in range(2):
                            p0_ = kv * 64
                            for hc in range(2):
                                bk, bb = sbank(), mbank()
                                for l in range(32):
                                    kb.op("tensor", lambda e, l=l, bk=bk, hc=hc, p0_=p0_: e.matmul(ps[bk][:, 0:255], lhsT=w1kv[p0_:p0_ + 64, l, hc * 128:(hc + 1) * 128],
                                                                                                rhs=kcvT[p0_:p0_ + 64, l:l + 16 * 254 + 1:16], start=(l == 0), stop=(l == 31)),
                                          outs=[PS[bk]], ins=[CW, KS], mark=(l == 31))
                                for l in range(32):
                                    kb.op("tensor", lambda e, l=l, bb=bb, hc=hc, p0_=p0_: e.matmul(ps[bb][:, 0:1], lhsT=w1kv[p0_:p0_ + 64, l, hc * 128:(hc + 1) * 128],
                                                                                                rhs=peTb[p0_:p0_ + 64, l:l + 1], start=(l == 0), stop=(l == 31)),
                                          outs=[PS[bb]], ins=[CW], mark=(l == 31))
                                ci = kv * 2 + hc
                                kb.op("vector", lambda e, bb=bb, ci=ci: e.tensor_tensor(out=beff[:, ci:ci + 1], in0=ps[bb][:, 0:1], in1=b1kv[:, ci:ci + 1], op=ALU.add),
                                      outs=[GX], ins=[PS[bb], CW])
                                kb.op("vector", lambda e, bk=bk, ci=ci: e.tensor_scalar(out=gx[:, 0:255], in0=ps[bk][:, 0:255], scalar1=beff[:, ci:ci + 1], scalar2=None, op0=ALU.add),
                                      outs=[GX], ins=[PS[bk], GX])
                                kb.op("vector", lambda e: e.tensor_tensor(out=gu[:, 0:255], in0=gx[:, 0:255], in1=gx[:, 0:255], op=ALU.mult), outs=[GX], ins=[GX])
                                kb.op("vector", lambda e: e.tensor_scalar(out=gu[:, 0:255], in0=gu[:, 0:255], scalar1=0.044715, scalar2=1.0, op0=ALU.mult, op1=ALU.add), outs=[GX], ins=[GX])
                                kb.op("vector", lambda e: e.tensor_tensor(out=gu[:, 0:255], in0=gu[:, 0:255], in1=gx[:, 0:255], op=ALU.mult), outs=[GX], ins=[GX])
                                kb.op("scalar", lambda e: e.activation(out=gs[:, 0:255], in_=gu[:, 0:255], func=AF.Sigmoid, scale=1.5957691216057308), outs=[GX], ins=[GX])
                                kb.op("vector", lambda e, ci=ci: e.tensor_tensor(out=hid[:, ci, 0:255], in0=gx[:, 0:255], in1=gs[:, 0:255], op=ALU.mult), outs=[HID], ins=[GX])
                        bk = sbank()
                        for hc in range(2):
                            kb.op("tensor", lambda e, hc=hc, bk=bk: e.matmul(ps[bk][:, 0:256], lhsT=w2k2[:, hc, :], rhs=hid[:, hc, :], start=(hc == 0), stop=(hc == 1)),
                                  outs=[PS[bk]], ins=[CW, HID], mark=(hc == 1))
                        kb.op("scalar", lambda e, bk=bk: e.copy(out=kcmpT[:], in_=ps[bk][:, 0:256]), outs=[KC], ins=[PS[bk]])
                        for ct in range(2):
                            bk = sbank()
                            for hc in range(2):
                                kb.op("tensor", lambda e, hc=hc, bk=bk, ct=ct: e.matmul(ps[bk][:, 0:64], lhsT=hid[:, 2 + hc, ct * 128:(ct + 1) * 128], rhs=w2v[:, hc, :],
                                                                                       start=(hc == 0), stop=(hc == 1)), outs=[PS[bk]], ins=[CW, HID], mark=(hc == 1))
                            kb.op("scalar", lambda e, bk=bk, ct=ct: e.copy(out=vcmp1[:, ct, 0:64], in_=ps[bk][:, 0:64]), outs=[KC], ins=[PS[bk]])
                        kb.op("vector", lambda e: e.memset(vcmp1[:, :, 64:65], 1.0), outs=[KC])
                        kb.op("vector", lambda e: e.tensor_copy(out=vcmp1[:, :, 65:129], in_=ovl[:]), outs=[KC], ins=[CST])
                        kb.barrier()
                    if stop_after == "p1c_c":
                        dump("kcmpT", kcmpT[:], [128, 256], BF16)
                        dump("vcmp1", vcmp1[:], [128, 2, 144], BF16)
                        return nc, dbg_out
                    with ExitStack() as pq:
                        wband = sb("wband", [128, 8, 512], BF16, pq)
                        QC = Buf(wband[:])
                        kb.dma("sync", wband[:], I["wband"], outs=[QC])
                        qn = [[sb(f"qn{ch}{par}", [128, 512], BF16, pq) for par in range(2)] for ch in range(2)]
                        QN = Buf(qn[0][0][:])
                        qTb = sb("qTb", [128, 2, 512], BF16, pq)
                        qrTb = sb("qrTb", [128, 2, 512], BF16, pq)
                        QB_ = Buf(qTb[:])
                        QRB = Buf(qrTb[:])
                        gts = sb("gts", [128, 4, 12], F32, pq)
                        GTS = Buf(gts[:])
                        cmpbb = sb("cmpbb", [128, 512], BF16, pq)
                        CMB = Buf(cmpbb[:])
                        pt = [sb(f"pt{i}", [128, 512], BF16, pq) for i in range(3)]
                        PT = [Buf(t[:]) for t in pt]
                        onsa = sb("onsa", [128, 4, 256], F32, pq)
                        ONSA = Buf(onsa[:])
                        obn = sb("obn", [128, 4, 256], BF16, pq)
                        OBN = Buf(obn[:])
                        pslc = sb("pslc", [128, 4, 64], F32, pq)
                        PSLC = Buf(pslc[:])
                        sadd = sb("sadd", [128, 64], F32, pq)
                        SADD = Buf(sadd[:])
                        score = sb("score", [128, 64], F32, pq)
                        stmp = sb("stmp", [128, 64], F32, pq)
                        sel = sb("sel", [128, 64], F32, pq)
                        m8 = sb("m8", [128, 16], F32, pq)
                        negb = sb("negb", [128, 128], BF16, pq)
                        SEL = Buf(score[:])
                        negbT = sb("negbT", [128, 512], BF16, pq)
                        NBT = Buf(negbT[:])
                        rz = sb("rz", [128, 4], F32, pq)
                        RZ = Buf(rz[:])
                        accs = [ps[3][:, 0:129], ps[4][:, 0:129], ps[5][:, 0:129], ps[6][:, 0:129]]
                        ACC = [PS[3], PS[4], PS[5], PS[6]]
                        prr = [0]

                        def run_branch(steps):
                            LA = 2
                            n_ = len(steps)
                            for idx_ in range(n_ + LA):
                                if idx_ < n_:
                                    st = steps[idx_]
                                    bk = sbank()
                                    nm = len(st["s"])
                                    for idx, (l_, r_, insb) in enumerate(st["s"]):
                                        kb.op("tensor", lambda e, l_=l_, r_=r_, idx=idx, nm=nm, bk=bk: e.matmul(ps[bk][:], lhsT=l_, rhs=r_, start=(idx == 0), stop=(idx == nm - 1)),
                                              outs=[PS[bk]], ins=insb, mark=(idx == nm - 1))
                                    prr[0] = (prr[0] + 1) % 3
                                    pi = prr[0]
                                    kb.op("scalar", lambda e, bk=bk, pi=pi: e.activation(out=pt[pi][:], in_=ps[bk][:], func=AF.Exp, scale=SCL), outs=[PT[pi]], ins=[PS[bk]])
                                    st["pi"] = pi
                                if idx_ >= LA:
                                    prev = steps[idx_ - LA]
                                    pi = prev["pi"]
                                    for (i, rhs_ap, w_, st_, sp_) in prev["pv"]:
                                        kb.op("tensor", lambda e, i=i, rhs_ap=rhs_ap, w_=w_, st_=st_, sp_=sp_, pi=pi: e.matmul(accs[i][:, 0:w_], lhsT=pt[pi][:, i * 128:(i + 1) * 128], rhs=rhs_ap,
                                                                                                                 start=st_, stop=sp_),
                                              outs=[ACC[i]], ins=[PT[pi], KS, KC])

                        def finish(h, br, first):
                            zc = 64
                            for i in range(4):
                                kb.op("vector", lambda e, i=i: e.tensor_scalar(out=rz[:, i:i + 1], in0=accs[i][:, zc:zc + 1], scalar1=1e-30, scalar2=None, op0=ALU.max),
                                      outs=[RZ], ins=[ACC[i]])
                            kb.op("vector", lambda e: e.reciprocal(out=rz[:], in_=rz[:]), outs=[RZ], ins=[RZ])
                            if br == 0:
                                for i in range(4):
                                    if h == 0:
                                        kb.op("vector", lambda e, i=i: e.tensor_scalar(out=pslc[:, i, :], in0=accs[i][:, 65:129], scalar1=rz[:, i:i + 1], scalar2=None, op0=ALU.mult),
                                              outs=[PSLC], ins=[ACC[i], RZ])
                                    else:
                                        kb.op("vector", lambda e, i=i: e.scalar_tensor_tensor(out=pslc[:, i, :], in0=accs[i][:, 65:129], scalar=rz[:, i:i + 1], in1=pslc[:, i, :],
                                                                                           op0=ALU.mult, op1=ALU.add), outs=[PSLC], ins=[ACC[i], RZ, PSLC])
                            kb.op("vector", lambda e: e.tensor_tensor(out=rz[:], in0=rz[:], in1=gts[:, :, h * 3 + br], op=ALU.mult), outs=[RZ], ins=[RZ, GTS])
                            for i in range(4):
                                dst = onsa[:, i, h * 64:(h + 1) * 64]
                                if first:
                                    kb.op("vector", lambda e, i=i, dst=dst: e.tensor_scalar(out=dst, in0=accs[i][:, 0:64], scalar1=rz[:, i:i + 1], scalar2=None, op0=ALU.mult),
                                          outs=[ONSA], ins=[ACC[i], RZ])
                                else:
                                    kb.op("vector", lambda e, i=i, dst=dst: e.scalar_tensor_tensor(out=dst, in0=accs[i][:, 0:64], scalar=rz[:, i:i + 1], in1=dst, op0=ALU.mult, op1=ALU.add),
                                          outs=[ONSA], ins=[ACC[i], RZ, ONSA])

                        wband4 = sb("wband4", [128, 8, 512], BF16, pq)
                        cmpb0 = sb("cmpb0", [128, 512], BF16, pq)
                        kb.dma("sync", wband4[:], I["wband4"], outs=[QC])
                        kb.dma("sync", cmpb0[:], I["cmpb0"], outs=[QC])
                        for qb in range(4, NB):
                            sl = slice(qb * 512, (qb + 1) * 512)
                            kb.dma("sync", cosb[:], I["cosT"][:, sl], outs=[CSB])
                            kb.dma("sync", sinb[:], I["sinT"][:, sl], outs=[CSB])
                            kb.dma("sync", cmpbb[:], I["cmpb"][:, qb, :], outs=[CMB])
                            for ch in range(2):
                                bk = sbank()
                                for k in range(8):
                                    kb.op("tensor", lambda e, k=k, bk=bk, ch=ch: e.matmul(ps[bk][:], lhsT=wqg[:, k, ch * 128:(ch + 1) * 128], rhs=hT[:, k, sl],
                                                                                         start=(k == 0), stop=(k == 7)), outs=[PS[bk]], ins=[WN, HT[qb]], mark=(k == 7))
                                kb.op("scalar", lambda e, bk=bk, ch=ch: e.copy(out=qTb[:, ch, :], in_=ps[bk][:]), outs=[QB_], ins=[PS[bk]])
                                rope_from(bk, qrTb[:, ch, :], QRB)
                            for i in range(4):
                                tt = 4 * qb + i
                                bk = mbank()
                                for k in range(8):
                                    kb.op("tensor", lambda e, k=k, bk=bk, tt=tt: e.matmul(ps[bk][:, 0:12], lhsT=hT[:, k, tt * 128:(tt + 1) * 128], rhs=wgt[:, k, :],
                                                                                         start=(k == 0), stop=(k == 7)), outs=[PS[bk]], ins=[WN, HT[qb]], mark=(k == 7))
                                kb.op("scalar", lambda e, bk=bk, i=i: e.activation(out=gts[:, i, :], in_=ps[bk][:, 0:12], func=AF.Sigmoid), outs=[GTS], ins=[PS[bk]])
                            if stop_after == "p1c_qa":
                                dump("onsa", onsa[:], [128, 4, 256])
                                return nc, dbg_out
                            ncts = 1 if qb < 4 else 2
                            for h in range(4):
                                ch, p0_ = h // 2, (h % 2) * 64
                                steps = []
                                for ct in range(ncts):
                                    smm = [(kcmpT[p0_:p0_ + 64, ct * 128:(ct + 1) * 128], qTb[p0_:p0_ + 64, ch, :], [KC, QB_])]
                                    if ct == ncts - 1:
                                        smm.append((ident[:], cmpbb[:], [CST, CMB]))
                                    else:
                                        smm.append((ident[:], cmpb0[:], [CST, QC]))
                                    pv = [(i, vcmp1[:, ct, 0:129], 129, ct == 0, ct == ncts - 1) for i in range(4)]
                                    steps.append({"s": smm, "pv": pv})
                                run_branch(steps)
                                finish(h, 0, True)
                            if stop_after == "p1c_qb":
                                dump("onsa", onsa[:], [128, 4, 256])
                                return nc, dbg_out
                            for i in range(4):
                                qt = 4 * qb + i
                                kb.dma("sync", sadd[:], I["seladd"][:, qt, :], outs=[SADD])
                                kb.op("vector", lambda e, i=i: e.tensor_tensor(out=score[:], in0=pslc[:, i, :], in1=sadd[:], op=ALU.add), outs=[SEL], ins=[PSLC, SADD])
                                kb.op("vector", lambda e: e.max(out=m8[:, 0:8], in_=score[:]), outs=[SEL], ins=[SEL])
                                kb.op("vector", lambda e: e.match_replace(out=stmp[:], in_to_replace=m8[:, 0:8], in_values=score[:], imm_value=-3e38), outs=[SEL], ins=[SEL])
                                kb.op("vector", lambda e: e.max(out=m8[:, 8:16], in_=stmp[:]), outs=[SEL], ins=[SEL])
                                kb.op("vector", lambda e: e.tensor_scalar(out=sel[:], in0=score[:], scalar1=m8[:, 15:16], scalar2=None, op0=ALU.is_ge), outs=[SEL], ins=[SEL])
                                kb.op("vector", lambda e: e.scalar_tensor_tensor(out=sel[:], in0=score[:], scalar=-1e29, in1=sel[:], op0=ALU.is_gt, op1=ALU.mult), outs=[SEL], ins=[SEL])
                                kb.op("vector", lambda e: e.tensor_scalar(out=negb[:, 0:64], in0=sel[:], scalar1=-1.0, scalar2=-NEG, op0=ALU.add, op1=ALU.mult), outs=[SEL], ins=[SEL])
                                kb.op("vector", lambda e: e.tensor_scalar(out=negb[:, 64:128], in0=sel[:], scalar1=-1.0, scalar2=-NEG, op0=ALU.add, op1=ALU.mult), outs=[SEL], ins=[SEL])
                                bk = mbank()
                                kb.op("tensor", lambda e, bk=bk: e.matmul(ps[bk][:, 0:128], lhsT=negb[:], rhs=ident[:], start=True, stop=True), outs=[PS[bk]], ins=[SEL, CST])
                                kb.op("scalar", lambda e, bk=bk, i=i: e.copy(out=negbT[:, i * 128:(i + 1) * 128], in_=ps[bk][:, 0:128]), outs=[NBT], ins=[PS[bk]])
                            if stop_after == "p1c_qc":
                                dump("onsa", onsa[:], [128, 4, 256])
                                return nc, dbg_out
                            for ch in range(2):
                                kb.op("gpsimd", lambda e, ch=ch: e.tensor_copy(out=qn[ch][0][0:64, :], in_=qrTb[0:64, ch, :]), outs=[QN], ins=[QRB])
                                kb.op("gpsimd", lambda e, ch=ch: e.tensor_copy(out=qn[ch][0][64:128, :], in_=negbT[64:128, :]), outs=[QN], ins=[NBT])
                                kb.op("gpsimd", lambda e, ch=ch: e.tensor_copy(out=qn[ch][1][0:64, :], in_=negbT[0:64, :]), outs=[QN], ins=[NBT])
                                kb.op("gpsimd", lambda e, ch=ch: e.tensor_copy(out=qn[ch][1][64:128, :], in_=qrTb[64:128, ch, :]), outs=[QN], ins=[QRB])
                            for h in range(4):
                                ch, p0_ = h // 2, (h % 2) * 64
                                KE_ = KEe if h % 2 == 0 else KEo
                                steps = []
                                for kt in range(4 * qb + 4):
                                    smm = [(KE_[:, kt * 128:(kt + 1) * 128], qn[ch][h % 2][:], [KS, QN, CST])]
                                    if kt >= 4 * qb:
                                        smm.append((ident[:], wband[:, 4 + kt - 4 * qb, :], [CST, QC]))
                                    pv = [(i, vs1[:, kt, 0:65], 65, kt == 0, kt == 4 * qb + i) for i in range(4) if kt <= 4 * qb + i]
                                    steps.append({"s": smm, "pv": pv})
                                run_branch(steps)
                                finish(h, 1, False)
                            if stop_after == "p1c_qd":
                                dump("onsa", onsa[:], [128, 4, 256])
                                return nc, dbg_out
                            for h in range(4):
                                ch, p0_ = h // 2, (h % 2) * 64
                                steps = []
                                jmin = max(0, 4 - 4 * qb)
                                for j in range(jmin, 8):
                                    kt = 4 * qb - 4 + j
                                    smm = [(kwT[p0_:p0_ + 64, kt * 128:(kt + 1) * 128], qrTb[p0_:p0_ + 64, ch, :], [KS, QRB]),
                                           (ident[:], (wband4 if qb == 4 else wband)[:, j, :], [CST, QC])]
                                    pv = [(i, vw1[:, kt, 0:65], 65, j == max(i, jmin), j == i + 4) for i in range(4) if i <= j <= i + 4]
                                    steps.append({"s": smm, "pv": pv})
                                run_branch(steps)
                                finish(h, 2, False)
                            if stop_after == "p1c_q":
                                dump("onsa", onsa[:], [128, 4, 256])
                                dump("pslc", pslc[:], [128, 4, 64])
                                dump("negbT", negbT[:], [128, 512], BF16)
                                return nc, dbg_out
                            kb.op("gpsimd", lambda e: e.tensor_copy(out=obn[:], in_=onsa[:]), outs=[OBN], ins=[ONSA])
                            for i in range(4):
                                qt = 4 * qb + i
                                s_ = qt - 16
                                for ch in range(2):
                                    jf = 4 + g * 2 + ch
                                    bk = mbank()
                                    kb.op("tensor", lambda e, bk=bk, i=i, ch=ch: e.matmul(ps[bk][:, 0:128], lhsT=obn[:, i, ch * 128:(ch + 1) * 128], rhs=ident[:], start=True, stop=True),
                                          outs=[PS[bk]], ins=[OBN, CST])
                                    dst = oT[:, jf, s_ * 128:(s_ + 1) * 128]
                                    kb.op("scalar", lambda e, bk=bk, dst=dst: e.copy(out=dst, in_=ps[bk][:, 0:128]), outs=[OT[jf][s_]], ins=[PS[bk]])
                        kb.barrier()
                kb.barrier()
            if "oT2" in dbg:
                dump("oT2", oT[:], [128, 8, SO], BF16)
            if stop_after == "p1c":
                kb.barrier()
                return nc, dbg_out
        kb.barrier()
        with ExitStack() as p2:
            acc = sb("acc", [128, 8, SO], F32, p2)
            ACCB = [Buf(acc[:, :, nb * 512:(nb + 1) * 512]) for nb in range(4)]
            wo = sb("wo", [128, 8, D], BF16, p2)
            WO = Buf(wo[:])
            fg = sb("fg", [128, 8], F32, p2)
            FG = Buf(fg[:])
            kb.dma("sync", fg[:], I["fg"], outs=[FG])
            xTo_v = I["xTo"].rearrange("(k p) t -> p k t", p=128)
            for nb in range(4):
                kb.dma("sync", acc[:, :, nb * 512:(nb + 1) * 512], xTo_v[:, :, nb * 512:(nb + 1) * 512], outs=[ACCB[nb]])
            w_out_v = I["w_out"].rearrange("(k p) c -> p k c", p=128)
            for k in range(8):
                kb.dma("gpsimd", wo[:, k, :], w_out_v[:, k, :], outs=[WO])
            rr2 = [0]

            def bank2():
                rr2[0] = (rr2[0] + 1) % 8
                return rr2[0]

            for nb in range(4):
                sl = slice(nb * 512, (nb + 1) * 512)
                for m in range(8):
                    bk = bank2()
                    for k in range(8):
                        kb.op("tensor", lambda e, k=k, m=m, bk=bk, sl=sl: e.matmul(ps[bk][:], lhsT=wo[:, k, m * 128:(m + 1) * 128], rhs=oT[:, k, sl], start=(k == 0), stop=(k == 7)),
                              outs=[PS[bk]], ins=[WO] + [OT[k][s] for s in range(nb * 4, nb * 4 + 4)], mark=(k == 7))
                    kb.op("vector", lambda e, m=m, bk=bk, sl=sl: e.scalar_tensor_tensor(out=acc[:, m, sl], in0=ps[bk][:], scalar=g1c(m), in1=acc[:, m, sl], op0=ALU.mult, op1=ALU.add),
                          outs=[ACCB[nb]], ins=[PS[bk], ACCB[nb], MOD])
            if "x2T" in dbg:
                dump("x2T", acc[:], [128, 8, SO])
            h2T = sb("h2T", [128, 8, SO], BF16, p2)
            H2 = [Buf(h2T[:, :, nb * 512:(nb + 1) * 512]) for nb in range(4)]
            with ExitStack() as p2a:
                sqa = sb("sqa", [128, 8, 512], F32, p2a)
                SQA = Buf(sqa[:])
                rsa = sb("rsa", [128, 512], F32, p2a)
                RSA = Buf(rsa[:])
                tma = [sb(f"tma{i}", [128, 512], F32, p2a) for i in range(2)]
                TMA = [Buf(t[:]) for t in tma]
                for nb in range(4):
                    sl = slice(nb * 512, (nb + 1) * 512)
                    kb.op("scalar", lambda e, sl=sl: e.activation(out=sqa[:], in_=acc[:, :, sl], func=AF.Square), outs=[SQA], ins=[ACCB[nb]])
                    bk = bank2()
                    for k in range(8):
                        kb.op("tensor", lambda e, k=k, bk=bk: e.matmul(ps[bk][:], lhsT=onesf[:], rhs=sqa[:, k, :], start=(k == 0), stop=(k == 7)),
                              outs=[PS[bk]], ins=[SQA, CST], mark=(k == 7))
                    kb.op("scalar", lambda e, bk=bk: e.activation(out=rsa[:], in_=ps[bk][:], func=AF.Sqrt, scale=1.0 / D, bias=EPS), outs=[RSA], ins=[PS[bk]])
                    kb.op("vector", lambda e: e.reciprocal(out=rsa[:], in_=rsa[:]), outs=[RSA], ins=[RSA])
                    for k in range(8):
                        T = TMA[k % 2]
                        t_ = tma[k % 2]
                        kb.op("vector", lambda e, k=k, t_=t_, sl=sl: e.tensor_tensor(out=t_[:], in0=acc[:, k, sl], in1=rsa[:], op=ALU.mult), outs=[T], ins=[ACCB[nb], RSA])
                        kb.op("scalar", lambda e, k=k, t_=t_, sl=sl: e.activation(out=h2T[:, k, sl], in_=t_[:], func=AF.Identity, scale=a2[:, k:k + 1], bias=sh2(k)),
                              outs=[H2[nb]], ins=[T, MOD])
                kb.barrier()
            if "h2T" in dbg:
                dump("h2T", h2T[:], [128, 8, SO], BF16)
            gw = sb("gw", [128, 16, 65], F32, p2)
            GW = Buf(gw[:])
            kb.op("vector", lambda e: e.memset(gw[:], 1.0), outs=[GW])
            with ExitStack() as p2r:
                rwf = sb("rwf", [128, 8, 64], F32, p2r)
                rwb = sb("rwb", [128, 8, 64], BF16, p2r)
                rbias = sb("rbias", [128, 64], F32, p2r)
                RW = Buf(rwf[:])
                kb.dma("sync", rwf[:], I["rw"], outs=[RW])
                kb.dma("sync", rbias[:], I["rbias"], outs=[RW])
                kb.op("vector", lambda e: e.tensor_copy(out=rwb[:], in_=rwf[:]), outs=[RW], ins=[RW])
                scr = sb("scr", [128, 64], F32, p2r)
                chs = sb("chs", [128, 64], F32, p2r)
                eq = sb("eq", [128, 64], F32, p2r)
                chm = sb("chm", [128, 64], F32, p2r)
                m1 = sb("m1", [128, 8], F32, p2r)
                m2 = sb("m2", [128, 8], F32, p2r)
                gsm = sb("gsm", [128, 8], F32, p2r)
                g8 = sb("g8", [128, 8], F32, p2r)
                gmk = sb("gmk", [128, 8], F32, p2r)
                e8 = sb("e8", [128, 8], F32, p2r)
                ssum = sb("ssum", [128, 1], F32, p2r)
                RT = Buf(scr[:])
                v3 = lambda ap: ap.rearrange("p (g j) -> p g j", j=8)
                b3 = lambda ap: ap.rearrange("p (g o) -> p g o", o=1).to_broadcast([128, 8, 8])
                for tt in range(16):
                    bk = bank2()
                    for k in range(8):
                        kb.op("tensor", lambda e, k=k, bk=bk, tt=tt: e.matmul(ps[bk][:, 0:64], lhsT=h2T[:, k, tt * 128:(tt + 1) * 128], rhs=rwb[:, k, :], start=(k == 0), stop=(k == 7)),
                              outs=[PS[bk]], ins=[H2[tt // 4], RW], mark=(k == 7))
                    kb.op("scalar", lambda e, bk=bk: e.activation(out=scr[:], in_=ps[bk][:, 0:64], func=AF.Sigmoid), outs=[RT], ins=[PS[bk]])
                    V = lambda f: kb.op("vector", f, outs=[RT], ins=[RT, RW])
                    V(lambda e: e.tensor_tensor(out=chs[:], in0=scr[:], in1=rbias[:], op=ALU.add))
                    V(lambda e: e.tensor_reduce(out=m1[:], in_=v3(chs[:]), axis=AX.X, op=ALU.max))
                    V(lambda e: e.tensor_tensor(out=v3(eq[:]), in0=v3(chs[:]), in1=b3(m1[:]), op=ALU.is_equal))
                    V(lambda e: e.scalar_tensor_tensor(out=eq[:], in0=eq[:], scalar=-1e30, in1=chs[:], op0=ALU.mult, op1=ALU.add))
                    V(lambda e: e.tensor_reduce(out=m2[:], in_=v3(eq[:]), axis=AX.X, op=ALU.max))
                    V(lambda e: e.tensor_tensor(out=gsm[:], in0=m1[:], in1=m2[:], op=ALU.add))
                    V(lambda e: e.max(out=g8[:], in_=gsm[:]))
                    V(lambda e: e.tensor_scalar(out=gmk[:], in0=gsm[:], scalar1=g8[:, 3:4], scalar2=None, op0=ALU.is_ge))
                    V(lambda e: e.scalar_tensor_tensor(out=v3(chm[:]), in0=v3(chs[:]), scalar=10.0, in1=b3(gmk[:]), op0=ALU.add, op1=ALU.mult))
                    V(lambda e: e.max(out=e8[:], in_=chm[:]))
                    V(lambda e: e.tensor_scalar(out=eq[:], in0=chm[:], scalar1=e8[:, 7:8], scalar2=None, op0=ALU.is_ge))
                    V(lambda e: e.tensor_tensor(out=eq[:], in0=eq[:], in1=scr[:], op=ALU.mult))
                    V(lambda e: e.tensor_reduce(out=ssum[:], in_=eq[:], axis=AX.X, op=ALU.add))
                    V(lambda e: e.reciprocal(out=ssum[:], in_=ssum[:]))
                    kb.op("vector", lambda e, tt=tt: e.tensor_scalar(out=gw[:, tt, 0:64], in0=eq[:], scalar1=ssum[:, 0:1], scalar2=2.5, op0=ALU.mult, op1=ALU.mult),
                          outs=[GW], ins=[RT])
                kb.barrier()
            if "gw" in dbg:
                dump("gw", gw[:], [128, 16, 65])
            with ExitStack() as p2e:
                wg = [sb(f"wg{i}", [128, 8, 512], BF16, p2e) for i in range(2)]
                wd = [sb(f"wd{i}", [128, 2, D], BF16, p2e) for i in range(2)]
                WG = [Buf(t[:]) for t in wg]
                WD = [Buf(t[:]) for t in wd]
                actT = [sb(f"actT{i}", [128, 2, SO], BF16, p2e) for i in range(2)]
                ACT_ = [[Buf(actT[i][:, :, nb * 512:(nb + 1) * 512]) for nb in range(4)] for i in range(2)]
                sgt = [sb(f"sgt{i}", [128, 256], F32, p2e) for i in range(2)]
                SGT = [Buf(t[:]) for t in sgt]
                att = [sb(f"att{i}", [128, 256], BF16, p2e) for i in range(2)]
                ATT = [Buf(t[:]) for t in att]
                NE = n_experts

                def load_w(e_):
                    sl_ = e_ % 2
                    gv = I["wgu"][e_].rearrange("(k p) c -> p k c", p=128)
                    kb.dma("gpsimd", wg[sl_][:, 0:4, :], gv[:, 0:4, :], outs=[WG[sl_]])
                    kb.dma("gpsimd", wg[sl_][:, 4:8, :], gv[:, 4:8, :], outs=[WG[sl_]])
                    kb.dma("gpsimd", wd[sl_][:], I["wdn"][e_].rearrange("(k p) c -> p k c", p=128), outs=[WD[sl_]])

                def load_wg(e_):
                    sl_ = e_ % 2
                    gv = I["wgu"][e_].rearrange("(k p) c -> p k c", p=128)
                    kb.dma("gpsimd", wg[sl_][:, 0:4, :], gv[:, 0:4, :], outs=[WG[sl_]])
                    kb.dma("gpsimd", wg[sl_][:, 4:8, :], gv[:, 4:8, :], outs=[WG[sl_]])

                def load_wd(e_):
                    sl_ = e_ % 2
                    kb.dma("gpsimd", wd[sl_][:], I["wdn"][e_].rearrange("(k p) c -> p k c", p=128), outs=[WD[sl_]])

                cnt = [0]
                pend = {}

                def GU(e_, tt):
                    sl_ = e_ % 2
                    bk = bank2()
                    for k in range(8):
                        kb.op("tensor", lambda e, k=k: e.matmul(ps[bk][:], lhsT=h2T[:, k, tt * 128:(tt + 1) * 128], rhs=wg[sl_][:, k, :], start=(k == 0), stop=(k == 7)),
                              outs=[PS[bk]], ins=[H2[tt // 4], WG[sl_]], mark=(k == 7))
                    cnt[0] += 1
                    i2 = cnt[0] % 2
                    kb.op("scalar", lambda e: e.activation(out=sgt[i2][:], in_=ps[bk][:, 0:256], func=AF.Silu), outs=[SGT[i2]], ins=[PS[bk]])
                    kb.op("vector", lambda e: e.scalar_tensor_tensor(out=att[i2][:], in0=ps[bk][:, 256:512], scalar=gw[:, tt, e_:e_ + 1], in1=sgt[i2][:],
                                                                     op0=ALU.mult, op1=ALU.mult), outs=[ATT[i2]], ins=[PS[bk], SGT[i2], GW])
                    pend[(e_, tt)] = i2

                def TR(e_, tt):
                    sl_ = e_ % 2
                    i2 = pend.pop((e_, tt))
                    for hc in range(2):
                        bt = bank2()
                        kb.op("tensor", lambda e, hc=hc, bt=bt: e.matmul(ps[bt][:, 0:128], lhsT=att[i2][:, hc * 128:(hc + 1) * 128], rhs=ident[:], start=True, stop=True),
                              outs=[PS[bt]], ins=[ATT[i2], CST])
                        kb.op("scalar", lambda e, hc=hc, bt=bt: e.copy(out=actT[sl_][:, hc, tt * 128:(tt + 1) * 128], in_=ps[bt][:, 0:128]),
                              outs=[ACT_[sl_][tt // 4]], ins=[PS[bt]])

                def DN(e_, nb):
                    sl_ = e_ % 2
                    sl = slice(nb * 512, (nb + 1) * 512)
                    for m in range(8):
                        bk = bank2()
                        for hc in range(2):
                            kb.op("tensor", lambda e, bk=bk, hc=hc, m=m: e.matmul(ps[bk][:], lhsT=wd[sl_][:, hc, m * 128:(m + 1) * 128], rhs=actT[sl_][:, hc, sl], start=(hc == 0), stop=(hc == 1)),
                                  outs=[PS[bk]], ins=[WD[sl_], ACT_[sl_][nb]], mark=(hc == 1))
                        kb.op("vector", lambda e, bk=bk, m=m: e.scalar_tensor_tensor(out=acc[:, m, sl], in0=ps[bk][:], scalar=g2c(m), in1=acc[:, m, sl], op0=ALU.mult, op1=ALU.add),
                              outs=[ACCB[nb]], ins=[PS[bk], ACCB[nb], MOD])

                load_wg(0)
                load_wd(0)
                for e_ in range(NE + 1):
                    if e_ + 1 < NE:
                        load_wg(e_ + 1)
                    for tt in range(16):
                        if e_ < NE:
                            GU(e_, tt)
                            if tt > 0:
                                TR(e_, tt - 1)
                        if e_ > 0 and tt % 4 == 3:
                            DN(e_ - 1, tt // 4)
                    if e_ < NE:
                        TR(e_, 15)
                    if e_ + 1 < NE:
                        load_wd(e_ + 1)
                kb.barrier()
            if "x3T" in dbg:
                dump("x3T", acc[:], [128, 8, SO])
            sq2 = sb("sq2", [128, 8, 512], F32, p2)
            SQ2 = Buf(sq2[:])
            rstd2 = sb("rstd2", [128, 512], F32, p2)
            RS2 = Buf(rstd2[:])
            ot = [sb(f"ot{i}", [128, 8, 512], F32, p2) for i in range(2)]
            OTB = [Buf(t[:]) for t in ot]
            outT_v = outT.rearrange("(k p) t -> p k t", p=128)
            for nb in range(4):
                sl = slice(nb * 512, (nb + 1) * 512)
                kb.op("scalar", lambda e, sl=sl: e.activation(out=sq2[:], in_=acc[:, :, sl], func=AF.Square), outs=[SQ2], ins=[ACCB[nb]])
                bk = bank2()
                for k in range(8):
                    kb.op("tensor", lambda e, k=k, bk=bk: e.matmul(ps[bk][:], lhsT=onesf[:], rhs=sq2[:, k, :], start=(k == 0), stop=(k == 7)),
                          outs=[PS[bk]], ins=[SQ2, CST], mark=(k == 7))
                kb.op("scalar", lambda e, bk=bk: e.activation(out=rstd2[:], in_=ps[bk][:], func=AF.Sqrt, scale=1.0 / D, bias=EPS), outs=[RS2], ins=[PS[bk]])
                kb.op("vector", lambda e: e.reciprocal(out=rstd2[:], in_=rstd2[:]), outs=[RS2], ins=[RS2])
                o_ = ot[nb % 2]
                for k in range(8):
                    kb.op("vector", lambda e, k=k, o_=o_, sl=sl: e.scalar_tensor_tensor(out=o_[:, k, :], in0=acc[:, k, sl], scalar=fg[:, k:k + 1], in1=rstd2[:], op0=ALU.mult, op1=ALU.mult),
                          outs=[OTB[nb % 2]], ins=[ACCB[nb], FG, RS2])
                kb.dma("sync", outT_v[:, :, sl], o_[:], ins=[OTB[nb % 2]])
            kb.barrier()
        kb.barrier()
    return nc, dbg_out


def _prep_inputs(inp, core):
    b = core // 2
    half = core % 2
    f = lambda a: np.ascontiguousarray(a, dtype=np.float32)
    x = inp["x"][b]
    m = {}
    xT = f(x.T)
    m["xTo"] = f(xT[:, half * SO:(half + 1) * SO])
    if half == 0:
        xl = np.zeros((D, S), np.float32)
        xl[:, SO:] = xT[:, :SO]
        m["xT"] = xl
    else:
        m["xT"] = xT
    m["cT"] = f(inp["c"][b].reshape(8, 128).T)
    m["w_ada"] = f(inp["w_ada"][0])
    m["b_ada"] = f(inp["b_ada"][0].reshape(1, -1))
    m["n1g"] = f(inp["norm1_g"][0].reshape(8, 128).T)
    m["w_in"] = f(inp["w_in"][0])
    m["lbl"] = f(inp["hg_lb_logits"].reshape(2, 4, 128).transpose(2, 0, 1))
    m["hng"] = f(np.broadcast_to(inp["hg_norm_g"][0][None, :], (128, 512)))
    for s in ("k", "v"):
        m["peT" + s] = f(inp["cmp_pos_" + s][0].T)
        m["w1" + s] = f(inp["cmp_w1_" + s][0].reshape(32, 64, 256).transpose(1, 0, 2))
        m["b1" + s] = f(inp["cmp_b1_" + s][0].reshape(2, 128).T)
        m["w2" + s] = f(inp["cmp_w2_" + s][0].reshape(2, 128, 64).transpose(1, 0, 2))
    m["w_out"] = f(inp["w_out"][0])
    m["n2g"] = f(inp["norm2_g"][0].reshape(8, 128).T)
    m["rw"] = f(inp["router_w"][0].reshape(8, 128, 64).transpose(1, 0, 2))
    m["rbias"] = f(np.broadcast_to(inp["router_bias"][0][None, :], (128, 64)))
    m["fg"] = f(inp["final_g"].reshape(8, 128).T)
    return m


_SHARED = {}


def kernel(**inp):
    inp = {k: np.asarray(v) for k, v in inp.items()}
    nc, _ = build()
    wgu = np.ascontiguousarray(np.concatenate([inp["w_exp_gu"][0], inp["w_sh_gu"][0][None]], axis=0), dtype=np.float32)
    wdn = np.ascontiguousarray(np.concatenate([inp["w_exp_dn"][0], inp["w_sh_dn"][0][None]], axis=0), dtype=np.float32)
    in_maps = []
    for core in range(8):
        m = _prep_inputs(inp, core)
        m["wgu"] = wgu
        m["wdn"] = wdn
        m.update(_consts(core % 2))
        in_maps.append(m)
    res = run_bass_kernel_spmd(nc, in_maps, core_ids=list(range(8)))
    out = np.zeros((4, S, D), np.float32)
    for core in range(8):
        b, half = core // 2, core % 2
        out[b, half * SO:(half + 1) * SO, :] = res.results[core]["outT"].T
    return out
```

```python
import numpy as np
import os as _os0
import ml_dtypes
from contextlib import ExitStack
import concourse.bass as bass
import concourse.mybir as mybir
from concourse.bass_utils import run_bass_kernel_spmd

F32 = mybir.dt.float32
BF16 = mybir.dt.bfloat16
AF = mybir.ActivationFunctionType
ALU = mybir.AluOpType
AX = mybir.AxisListType

S = 4096
D = 1024
NT = 32
NB = 8
SO = 2048
EPS = 1e-6
NEG = -30000.0
NDS = 12
SEM_LIMIT = 2000
SAME_SYNC = not bool(int(_os0.environ.get("NOSAME", "0")))


class Buf:
    __slots__ = ("ap", "w", "r", "excl")

    def __init__(self, ap, excl=False):
        self.ap = ap
        self.w = None
        self.r = {}
        self.excl = excl

    def __getitem__(self, k):
        return self.ap[k]


class Eng:
    def __init__(self, name, h):
        self.name = name
        self.h = h
        self.sem = None
        self.count = 0
        self.epoch = 0
        self.waited = {}


class KB:
    def __init__(self, nc, es):
        self.nc = nc
        self.es = es
        self.engs = {n: Eng(n, getattr(nc, n)) for n in ("tensor", "vector", "scalar", "gpsimd", "sync")}
        for e in self.engs.values():
            self._new_sem(e)
        self.dsems = {q: [es.enter_context(nc.semaphore(f"d_{q}{i}")) for i in range(NDS)] for q in ("sync", "gpsimd")}
        self.dcnt = {q: [0] * NDS for q in ("sync", "gpsimd")}
        self.drr = {"sync": 0, "gpsimd": 0}
        self.nsem = 0

    def _new_sem(self, e):
        e.epoch += 1
        e.sem = self.es.enter_context(self.nc.semaphore(f"s_{e.name}_{e.epoch}"))
        e.count = 0

    def wait(self, eng, tk):
        key, sem, val = tk
        if eng.waited.get(key, 0) >= val:
            return
        eng.h.wait_ge(sem, val)
        eng.waited[key] = val

    def _deps(self, en, eng, outs, ins):
        need = {}

        def add(t):
            if t[3] == en and (en == "tensor" or not SAME_SYNC):
                return
            cur = need.get(t[0])
            if cur is None or cur[2] < t[2]:
                need[t[0]] = t

        for b in ins:
            if b.w is not None:
                add(b.w)
            if b.excl:
                for t in b.r.values():
                    if t[3] != en:
                        add(t)
        for b in outs:
            if b.w is not None:
                add(b.w)
            for t in b.r.values():
                add(t)
        for t in need.values():
            self.wait(eng, t[:3])

    def op(self, en, fn, outs=(), ins=(), mark=True):
        eng = self.engs[en]
        self._deps(en, eng, outs, ins)
        if eng.count >= SEM_LIMIT:
            self._new_sem(eng)
        inst = fn(eng.h)
        if mark:
            eng.count += 1
            inst.then_inc(eng.sem, 1)
            tk = ((en, eng.epoch), eng.sem, eng.count, en)
        else:
            tk = ((en, eng.epoch), eng.sem, eng.count + 1, en)
        for b in ins:
            b.r[tk[0]] = tk
        for b in outs:
            b.w = tk
            b.r = {}
        return tk

    def dma(self, q, out_ap, in_ap, outs=(), ins=()):
        eng = self.engs[q]
        i = self.drr[q]
        self.drr[q] = (i + 1) % NDS
        sem = self.dsems[q][i]
        key = ("d", q, i)
        if self.dcnt[q][i] > 0:
            self.wait(eng, (key, sem, self.dcnt[q][i]))
        self._deps("dma_" + q, eng, outs, ins)
        inst = eng.h.dma_start(out=out_ap, in_=in_ap)
        self.dcnt[q][i] += 16
        inst.then_inc(sem, 16)
        tk = (key, sem, self.dcnt[q][i], "dma_" + q)
        for b in ins:
            b.r[key] = tk
        for b in outs:
            b.w = tk
            b.r = {}
        return tk

    def barrier(self):
        for e in self.engs.values():
            for o in self.engs.values():
                if o is e or o.count == 0:
                    continue
                self.wait(e, ((o.name, o.epoch), o.sem, o.count))
            for q in ("sync", "gpsimd"):
                for i in range(NDS):
                    if self.dcnt[q][i] > 0:
                        self.wait(e, (("d", q, i), self.dsems[q][i], self.dcnt[q][i]))


def _consts(half):
    bf = ml_dtypes.bfloat16
    c = {}
    eye = np.eye(128, dtype=np.float32)
    c["ident"] = eye.astype(bf)
    c["onesf"] = np.ones((128, 128), np.float32)
    c["isel0"] = (eye * (1.0 if half == 0 else 0.0)).astype(bf)
    c["isel1"] = (eye * (1.0 if half == 1 else 0.0)).astype(bf)
    m = np.arange(128)
    sw = (m // 64) * 64 + ((m % 64) + 32) % 64
    ps = np.zeros((128, 128), np.float32)
    ps[sw, m] = 1.0
    c["pswap"] = ps.astype(bf)
    shift = 2048 if half == 0 else 0
    dd = np.arange(128) % 64
    i = dd % 32
    inv = 10000.0 ** (-(i.astype(np.float64)) / 32.0)
    tpos = (np.arange(S) - shift).astype(np.float64)
    ang = inv[:, None].astype(np.float32).astype(np.float64) * tpos[None, :]
    ang = ang.astype(np.float32).astype(np.float64)
    c["cosT"] = np.cos(ang).astype(np.float32)
    sg = np.where(dd < 32, -1.0, 1.0)[:, None]
    c["sinT"] = (np.sin(ang) * sg).astype(np.float32)
    vm = np.ones((128, 32), np.float32)
    if half == 0:
        vm[:, :16] = 0.0
    c["vmask"] = vm
    c["hmask"] = (m[:, None] <= m[None, :]).astype(np.float32).astype(bf)
    seg = np.ones((128, 512), np.float32)
    seg[:, ::128] = 0.0
    c["segm"] = seg
    r = np.arange(128)[:, None]
    qi = np.arange(512)[None, :]
    wb = np.zeros((8, 128, 512), np.float32)
    cb = np.zeros((4, 128, 512), np.float32)
    for j in range(8):
        kpos = -512 + 128 * j + r
        dlt = qi - kpos
        wb[j] = np.where((dlt >= 0) & (dlt < 512), 0.0, NEG)
    for j in range(4):
        kpos = 128 * j + r
        cb[j] = np.where(kpos <= qi, 0.0, NEG)
    c["wband"] = np.ascontiguousarray(wb.transpose(1, 0, 2)).astype(bf)
    c["causb"] = np.ascontiguousarray(cb.transpose(1, 0, 2)).astype(bf)
    wb4 = wb.copy()
    if half == 0:
        wb4[0:4] = NEG
    c["wband4"] = np.ascontiguousarray(wb4.transpose(1, 0, 2)).astype(bf)
    cm = np.zeros((8, 128, 512), np.float32)
    for qb in range(8):
        ct = 0 if qb < 4 else 1
        cc = 128 * ct + r
        qpos = 512 * qb + qi - shift
        tc = cc - shift // 16
        cm[qb] = np.where((16 * tc + 31 <= qpos) & (cc < 255) & (tc >= 0), 0.0, NEG)
    c["cmpb"] = np.ascontiguousarray(cm.transpose(1, 0, 2)).astype(bf)
    c["cmpb0"] = np.full((128, 512), NEG if half == 0 else 0.0, np.float32).astype(bf)
    ek = np.zeros((64, 32, 128), np.float32)
    for kt in range(32):
        ek[2 * kt, kt, :64] = 1.0
        ek[2 * kt + 1, kt, 64:] = 1.0
    c["ekt"] = np.concatenate([ek, ek], axis=0).astype(bf)
    add = np.zeros((128, 32, 64), np.float32)
    for qt in range(32):
        pos = 128 * qt + np.arange(128) - shift
        cur = pos // 64
        j = np.arange(64)[None, :] - shift // 64
        forced = (j == 0) | (j == cur[:, None]) | (j == cur[:, None] - 1)
        avail = (j <= cur[:, None]) & (j >= 0)
        add[:, qt, :] = np.where(avail & forced, 1e30, np.where(avail, 0.0, -1e30))
    c["seladd"] = add
    cs = np.arange(256)[:, None] * 16
    ss = np.arange(64)[None, :] * 64
    ov = np.clip(np.minimum(cs + 32, ss + 64) - np.maximum(cs, ss), 0, None).astype(np.float32) / 32.0
    ov[255] = 0.0
    c["ovl"] = np.ascontiguousarray(ov.reshape(2, 128, 64).transpose(1, 0, 2)).astype(bf)
    return c


CONST_SHAPES = {
    "ident": ([128, 128], BF16), "onesf": ([128, 128], F32), "isel0": ([128, 128], BF16), "isel1": ([128, 128], BF16),
    "pswap": ([128, 128], BF16), "cosT": ([128, S], F32), "sinT": ([128, S], F32), "hmask": ([128, 128], BF16),
    "segm": ([128, 512], F32), "wband": ([128, 8, 512], BF16), "causb": ([128, 4, 512], BF16),
    "cmpb": ([128, 8, 512], BF16), "ekt": ([128, 32, 128], BF16), "seladd": ([128, 32, 64], F32),
    "ovl": ([128, 2, 64], BF16), "vmask": ([128, 32], F32), "wband4": ([128, 8, 512], BF16), "cmpb0": ([128, 512], BF16),
}

IN_SHAPES = {
    "xT": [D, S], "xTo": [D, SO], "cT": [128, 8], "w_ada": [D, 6 * D], "b_ada": [1, 6 * D], "n1g": [128, 8],
    "w_in": [D, 3352], "lbl": [128, 2, 4], "hng": [128, 512],
    "peTk": [64, 32], "w1k": [64, 32, 256], "b1k": [128, 2], "w2k": [128, 2, 64],
    "peTv": [64, 32], "w1v": [64, 32, 256], "b1v": [128, 2], "w2v": [128, 2, 64],
    "w_out": [D, D], "n2g": [128, 8], "rw": [128, 8, 64], "rbias": [128, 64],
    "wgu": [65, D, 512], "wdn": [65, 256, D], "fg": [128, 8],
}


class _SkipNSA(Exception):
    pass


class _NSAScope(ExitStack):
    def __exit__(self, et, ev, tb):
        super().__exit__(None, None, None)
        return et is _SkipNSA


def build(stop_after=None, dbg=(), with_moe=True, enable_nsa=True, n_experts=65):
    nc = bass.Bass("TRN2", target_bir_lowering=False)
    I = {}
    for k, shp in IN_SHAPES.items():
        if not with_moe and k in ("wgu", "wdn"):
            continue
        I[k] = nc.dram_tensor(k, list(shp), F32, kind="ExternalInput").ap()
    for k, (shp, dt) in CONST_SHAPES.items():
        I[k] = nc.dram_tensor(k, list(shp), dt, kind="ExternalInput").ap()
    outT = nc.dram_tensor("outT", [D, SO], F32, kind="ExternalOutput").ap()
    dbg_out = {}
    with ExitStack() as es:
        kb = KB(nc, es)
        E = es.enter_context

        uid = [0]

        def sb(name, shape, dt=F32, stack=None):
            uid[0] += 1
            return (stack or es).enter_context(nc.sbuf_tensor(f"sb{uid[0]}_" + name, list(shape), dt))

        ps = [E(nc.psum_tensor(f"ps{i}", [128, 512], F32)) for i in range(8)]
        PS = [Buf(p[:], excl=True) for p in ps]

        def dump(name, ap, shape, dt=F32):
            t = nc.dram_tensor("dbg_" + name, list(shape), dt, kind="ExternalOutput").ap()
            dbg_out[name] = t
            kb.barrier()
            kb.dma("sync", t, ap)
            kb.barrier()

        ident = sb("ident", [128, 128], BF16)
        onesf = sb("onesf", [128, 128], F32)
        isel0 = sb("isel0", [128, 128], BF16)
        isel1 = sb("isel1", [128, 128], BF16)
        pswap = sb("pswap", [128, 128], BF16)
        hmask = sb("hmask", [128, 128], BF16)
        segm = sb("segm", [128, 512], F32)
        CST = Buf(ident[:])
        for nm, t in (("ident", ident), ("onesf", onesf), ("isel0", isel0), ("isel1", isel1), ("pswap", pswap),
                      ("hmask", hmask), ("segm", segm)):
            kb.dma("sync", t[:], I[nm], outs=[CST])
        modcol = sb("modcol", [128, 48], F32)
        a1 = sb("a1", [128, 8], F32)
        a2 = sb("a2", [128, 8], F32)
        MOD = Buf(modcol[:])
        oT = sb("oT", [128, 8, SO], BF16)
        OT = [[Buf(oT[:, j, s * 128:(s + 1) * 128]) for s in range(16)] for j in range(8)]

        with ExitStack() as p0:
            cT = sb("cT", [128, 8], F32, p0)
            cs = sb("cs", [128, 8], F32, p0)
            bada = sb("bada", [1, 6 * D], F32, p0)
            modrow = sb("modrow", [1, 6 * D], F32, p0)
            one1 = sb("one1", [1, 1], F32, p0)
            n1g = sb("n1g", [128, 8], F32, p0)
            n2g = sb("n2g", [128, 8], F32, p0)
            wab = [sb(f"wab{i}", [128, 8, 512], F32, p0) for i in range(2)]
            WAB = [Buf(w[:]) for w in wab]
            SM = Buf(cT[:])
            MR = Buf(modrow[:])
            kb.dma("sync", cT[:], I["cT"], outs=[SM])
            kb.dma("sync", bada[:], I["b_ada"], outs=[SM])
            kb.dma("sync", n1g[:], I["n1g"], outs=[SM])
            kb.dma("sync", n2g[:], I["n2g"], outs=[SM])
            kb.op("vector", lambda e: e.memset(one1[:], 1.0), outs=[SM])
            kb.op("scalar", lambda e: e.activation(out=cs[:], in_=cT[:], func=AF.Silu), outs=[SM], ins=[SM])
            wada_v = I["w_ada"].rearrange("(k p) c -> p k c", p=128)
            for cb in range(12):
                W = WAB[cb % 2]
                kb.dma("sync" if cb % 2 == 0 else "gpsimd", wab[cb % 2][:], wada_v[:, :, cb * 512:(cb + 1) * 512], outs=[W])
                P = PS[cb % 2]
                for k in range(8):
                    kb.op("tensor", lambda e, k=k, cb=cb: e.matmul(ps[cb % 2][0:1, :], lhsT=cs[:, k:k + 1], rhs=wab[cb % 2][:, k, :],
                                                                 start=(k == 0), stop=(k == 7)),
                          outs=[P], ins=[SM, W], mark=(k == 7))
                kb.op("vector", lambda e, cb=cb: e.tensor_tensor(out=modrow[0:1, cb * 512:(cb + 1) * 512], in0=ps[cb % 2][0:1, :],
                                                                  in1=bada[0:1, cb * 512:(cb + 1) * 512], op=ALU.add),
                      outs=[MR], ins=[P, SM])
            P = PS[2]
            for j in range(48):
                kb.op("tensor", lambda e, j=j: e.matmul(ps[2][:, j:j + 1], lhsT=modrow[0:1, j * 128:(j + 1) * 128], rhs=one1[0:1, 0:1],
                                                       start=True, stop=True), outs=[P], ins=[MR, SM], mark=(j == 47))
            kb.op("vector", lambda e: e.tensor_copy(out=modcol[:], in_=ps[2][:, 0:48]), outs=[MOD], ins=[P])
            kb.op("vector", lambda e: e.scalar_tensor_tensor(out=a1[:], in0=modcol[:, 8:16], scalar=1.0, in1=n1g[:], op0=ALU.add, op1=ALU.mult),
                  outs=[MOD], ins=[MOD, SM])
            kb.op("vector", lambda e: e.scalar_tensor_tensor(out=a2[:], in0=modcol[:, 32:40], scalar=1.0, in1=n2g[:], op0=ALU.add, op1=ALU.mult),
                  outs=[MOD], ins=[MOD, SM])
            if "mod" in dbg:
                dump("mod", modcol[:], [128, 48])
            kb.barrier()
        sh1 = lambda k: modcol[:, k:k + 1]
        g1c = lambda k: modcol[:, 16 + k:17 + k]
        sh2 = lambda k: modcol[:, 24 + k:25 + k]
        g2c = lambda k: modcol[:, 40 + k:41 + k]

        if stop_after == "p0":
            kb.barrier()
            return nc, dbg_out

        with ExitStack() as p1:
            hT = sb("hT", [128, 8, S], BF16, p1)
            HT = [Buf(hT[:, :, n * 512:(n + 1) * 512]) for n in range(NB)]
            with ExitStack() as p1a:
                xb = [sb(f"xb{i}", [128, 8, 512], F32, p1a) for i in range(2)]
                XB = [Buf(t[:]) for t in xb]
                sq = sb("sq", [128, 8, 512], F32, p1a)
                SQ = Buf(sq[:])
                rstd = sb("rstd", [128, 512], F32, p1a)
                RS = Buf(rstd[:])
                tmp = [sb(f"tmp{i}", [128, 512], F32, p1a) for i in range(2)]
                TMP = [Buf(t[:]) for t in tmp]
                xT_v = I["xT"].rearrange("(k p) t -> p k t", p=128)
                for n in range(NB):
                    X = XB[n % 2]
                    x_ = xb[n % 2]
                    kb.dma("sync" if n % 2 == 0 else "gpsimd", x_[:], xT_v[:, :, n * 512:(n + 1) * 512], outs=[X])
                    kb.op("scalar", lambda e, x_=x_: e.activation(out=sq[:], in_=x_[:], func=AF.Square), outs=[SQ], ins=[X])
                    P = PS[n % 2]
                    for k in range(8):
                        kb.op("tensor", lambda e, k=k, n=n: e.matmul(ps[n % 2][:], lhsT=onesf[:], rhs=sq[:, k, :], start=(k == 0), stop=(k == 7)),
                              outs=[P], ins=[SQ, CST], mark=(k == 7))
                    kb.op("scalar", lambda e, n=n: e.activation(out=rstd[:], in_=ps[n % 2][:], func=AF.Sqrt, scale=1.0 / D, bias=EPS),
                          outs=[RS], ins=[P])
                    kb.op("vector", lambda e: e.reciprocal(out=rstd[:], in_=rstd[:]), outs=[RS], ins=[RS])
                    for k in range(8):
                        T = TMP[k % 2]
                        t_ = tmp[k % 2]
                        kb.op("vector", lambda e, k=k, t_=t_, x_=x_: e.tensor_tensor(out=t_[:], in0=x_[:, k, :], in1=rstd[:], op=ALU.mult),
                              outs=[T], ins=[X, RS])
                        kb.op("scalar", lambda e, k=k, t_=t_, n=n: e.activation(out=hT[:, k, n * 512:(n + 1) * 512], in_=t_[:], func=AF.Identity,
                                                                            scale=a1[:, k:k + 1], bias=sh1(k)),
                              outs=[HT[n]], ins=[T, MOD])
                kb.barrier()
            if "hT" in dbg:
                dump("hT", hT[:], [128, 8, S], BF16)
            if stop_after == "p1a":
                kb.barrier()
                return nc, dbg_out

            rr = [0]

            def bank():
                rr[0] = (rr[0] + 1) % 8
                return rr[0]

            w_in_v = I["w_in"].rearrange("(k p) c -> p k c", p=128)

            with ExitStack() as ph:
                lbl = sb("lbl", [128, 2, 4], F32, ph)
                lb = sb("lb", [128, 4], F32, ph)
                oml = sb("oml", [128, 4], F32, ph)
                hng = sb("hng", [128, 512], F32, ph)
                HC = Buf(lbl[:])
                kb.dma("sync", lbl[:], I["lbl"], outs=[HC])
                kb.dma("sync", hng[:], I["hng"], outs=[HC])
                kb.op("vector", lambda e: e.tensor_tensor(out=lb[:], in0=lbl[:, 0, :], in1=lbl[:, 1, :], op=ALU.subtract), outs=[HC], ins=[HC])
                kb.op("scalar", lambda e: e.activation(out=lb[:], in_=lb[:], func=AF.Sigmoid), outs=[HC], ins=[HC])
                kb.op("vector", lambda e: e.tensor_scalar(out=oml[:], in0=lb[:], scalar1=-1.0, scalar2=1.0, op0=ALU.mult, op1=ALU.add), outs=[HC], ins=[HC])
                wq = sb("wq", [128, 8, 128], BF16, ph)
                wf = sb("wf", [128, 8, 128], BF16, ph)
                wig = sb("wig", [128, 8, 256], BF16, ph)
                WQ, WF, WIG = Buf(wq[:]), Buf(wf[:]), Buf(wig[:])
                Q1 = sb("Q1", [128, S], BF16, ph)
                Q2 = sb("Q2", [128, S], BF16, ph)
                Kt = sb("Kt", [128, S], BF16, ph)
                Kh = sb("Kh", [128, NT, 128], BF16, ph)
                Vh = sb("Vh", [128, NT, 128], BF16, ph)
                SGt = sb("SGt", [128, NT, 128], BF16, ph)
                ebl = sb("ebl", [128, NT], F32, ph)
                BQ = [Buf(Q1[:, n * 512:(n + 1) * 512]) for n in range(NB)]
                BKH = [Buf(Kh[:, t, :]) for t in range(NT)]
                BV = [Buf(Vh[:, t, :]) for t in range(NT)]
                tn = ["f", "lf", "b", "d1", "d2", "eb", "e1", "en1", "el", "k"]
                T2 = [{n_: sb(f"t{i}_" + n_, [128, 512], F32, ph) for n_ in tn} for i in range(2)]
                TB2 = [{n_: Buf(T2[i][n_][:]) for n_ in tn} for i in range(2)]
                khtb2 = [sb(f"khtb{i}", [128, 512], BF16, ph) for i in range(2)]
                KHTB2 = [Buf(t[:]) for t in khtb2]
                vmask = sb("vmask", [128, 32], F32, ph)
                kb.dma("sync", vmask[:], I["vmask"], outs=[HC])
                Sst = sb("Sst", [128, 128], F32, ph)
                SST = Buf(Sst[:])
                sbf = [sb(f"sbf{i}", [128, 128], BF16, ph) for i in range(2)]
                SBF = [Buf(t[:]) for t in sbf]
                atm = [sb(f"atm{i}", [128, 128], BF16, ph) for i in range(2)]
                ATM = [Buf(t[:]) for t in atm]
                for i in range(2):
                    kb.op("vector", lambda e, i=i: e.memset(atm[i][:], 0.0), outs=[ATM[i]])
                junk = sb("junk", [128, 128], F32, ph)
                JK = Buf(junk[:])
                ssq = [sb(f"ssq{i}", [128, 1], F32, ph) for i in range(2)]
                SSQ = [Buf(t[:]) for t in ssq]
                of = [sb(f"of{i}", [128, 128], F32, ph) for i in range(2)]
                OF = [Buf(t[:]) for t in of]
                obf = [sb(f"obf{i}", [128, 128], BF16, ph) for i in range(2)]
                OBF = [Buf(t[:]) for t in obf]
                v4 = lambda ap: ap.rearrange("p (c t) -> p c t", t=128)
                for hd in range(int(_os0.environ.get("NHEADS", "4"))):
                    c0 = hd * 128
                    kb.dma("gpsimd", wq[:], w_in_v[:, :, c0:c0 + 128], outs=[WQ])
                    kb.dma("gpsimd", wf[:], w_in_v[:, :, 512 + c0:512 + c0 + 128], outs=[WF])
                    kb.dma("gpsimd", wig[:, :, 0:128], w_in_v[:, :, 1024 + c0:1024 + c0 + 128], outs=[WIG])
                    kb.dma("gpsimd", wig[:, :, 128:256], w_in_v[:, :, 1536 + c0:1536 + c0 + 128], outs=[WIG])
                    for n in range(NB):
                        sl = slice(n * 512, (n + 1) * 512)
                        own = n >= 4
                        bq_, bf_ = bank(), bank()
                        if own:
                            for k in range(8):
                                kb.op("tensor", lambda e, k=k, bq_=bq_, sl=sl: e.matmul(ps[bq_][:], lhsT=wq[:, k, :], rhs=hT[:, k, sl], start=(k == 0), stop=(k == 7)),
                                      outs=[PS[bq_]], ins=[WQ, HT[n]], mark=(k == 7))
                        for k in range(8):
                            kb.op("tensor", lambda e, k=k, bf_=bf_, sl=sl: e.matmul(ps[bf_][:], lhsT=wf[:, k, :], rhs=hT[:, k, sl], start=(k == 0), stop=(k == 7)),
                                  outs=[PS[bf_]], ins=[WF, HT[n]], mark=(k == 7))
                        t = T2[n % 2]
                        TB = TB2[n % 2]
                        khtb = khtb2[n % 2]
                        KHTB = KHTB2[n % 2]
                        kb.op("scalar", lambda e, bf_=bf_: e.activation(out=t["f"][:], in_=ps[bf_][:], func=AF.Sigmoid), outs=[TB["f"]], ins=[PS[bf_]])
                        kb.op("vector", lambda e, hd=hd: e.tensor_scalar(out=t["f"][:], in0=t["f"][:], scalar1=oml[:, hd:hd + 1], scalar2=lb[:, hd:hd + 1],
                                                                     op0=ALU.mult, op1=ALU.add), outs=[TB["f"]], ins=[TB["f"], HC])
                        kb.op("scalar", lambda e: e.activation(out=t["lf"][:], in_=t["f"][:], func=AF.Ln), outs=[TB["lf"]], ins=[TB["f"]])
                        kb.op("gpsimd", lambda e: e.tensor_scalar(out=t["k"][:], in0=t["f"][:], scalar1=-1.0, scalar2=1.0, op0=ALU.mult, op1=ALU.add),
                              outs=[TB["k"]], ins=[TB["f"]])
                        kb.op("vector", lambda e: e.tensor_tensor_scan(out=t["b"][:], data0=segm[:], data1=t["lf"][:], initial=0.0, op0=ALU.mult, op1=ALU.add),
                              outs=[TB["b"]], ins=[TB["lf"], CST])
                        if own:
                            kb.op("vector", lambda e: e.tensor_tensor(out=v4(t["d1"][:]), in0=v4(t["b"][:]), in1=v4(t["b"][:])[:, :, 63:64].to_broadcast([128, 4, 128]),
                                                                      op=ALU.subtract), outs=[TB["d1"]], ins=[TB["b"]])
                        kb.op("vector", lambda e: e.tensor_tensor(out=v4(t["d2"][:]), in0=v4(t["b"][:])[:, :, 127:128].to_broadcast([128, 4, 128]), in1=v4(t["b"][:]),
                                                                  op=ALU.subtract), outs=[TB["d2"]], ins=[TB["b"]])
                        kb.op("scalar", lambda e: e.activation(out=t["eb"][:], in_=t["b"][:], func=AF.Exp), outs=[TB["eb"]], ins=[TB["b"]])
                        if own:
                            kb.op("scalar", lambda e: e.activation(out=t["e1"][:], in_=t["d1"][:], func=AF.Exp), outs=[TB["e1"]], ins=[TB["d1"]])
                            kb.op("scalar", lambda e: e.activation(out=t["en1"][:], in_=t["d1"][:], func=AF.Exp, scale=-1.0), outs=[TB["en1"]], ins=[TB["d1"]])
                        kb.op("scalar", lambda e: e.activation(out=t["el"][:], in_=t["d2"][:], func=AF.Exp), outs=[TB["el"]], ins=[TB["d2"]])
                        sc_ = 128.0 ** -0.5
                        if own:
                            kb.op("vector", lambda e, bq_=bq_, sl=sl: e.scalar_tensor_tensor(out=Q1[:, sl], in0=ps[bq_][:], scalar=sc_, in1=t["e1"][:], op0=ALU.mult, op1=ALU.mult),
                                  outs=[BQ[n]], ins=[PS[bq_], TB["e1"]])
                            kb.op("vector", lambda e, bq_=bq_, sl=sl: e.scalar_tensor_tensor(out=Q2[:, sl], in0=ps[bq_][:], scalar=sc_, in1=t["eb"][:], op0=ALU.mult, op1=ALU.mult),
                                  outs=[BQ[n]], ins=[PS[bq_], TB["eb"]])
                            kb.op("gpsimd", lambda e, sl=sl: e.tensor_tensor(out=Kt[:, sl], in0=t["k"][:], in1=t["en1"][:], op=ALU.mult), outs=[BQ[n]], ins=[TB["k"], TB["en1"]])
                        kb.op("gpsimd", lambda e: e.tensor_tensor(out=khtb[:], in0=t["k"][:], in1=t["el"][:], op=ALU.mult), outs=[KHTB], ins=[TB["k"], TB["el"]])
                        kb.op("gpsimd", lambda e, n=n: e.tensor_copy(out=ebl[:, 4 * n:4 * n + 4], in_=v4(t["eb"][:])[:, :, 127]), outs=[BQ[n]], ins=[TB["eb"]])
                        for i in range(4):
                            bk = bank()
                            kb.op("tensor", lambda e, i=i, bk=bk: e.matmul(ps[bk][:, 0:128], lhsT=khtb[:, i * 128:(i + 1) * 128], rhs=ident[:], start=True, stop=True),
                                  outs=[PS[bk]], ins=[KHTB, CST])
                            kb.op("scalar", lambda e, i=i, bk=bk, n=n: e.copy(out=Kh[:, 4 * n + i, :], in_=ps[bk][:, 0:128]), outs=[BKH[4 * n + i]], ins=[PS[bk]])
                    for tt in range(NT):
                        bk = bank()
                        n = tt // 4
                        for k in range(8):
                            kb.op("tensor", lambda e, k=k, bk=bk, tt=tt: e.matmul(ps[bk][:, 0:256], lhsT=hT[:, k, tt * 128:(tt + 1) * 128], rhs=wig[:, k, :],
                                                                                 start=(k == 0), stop=(k == 7)),
                                  outs=[PS[bk]], ins=[WIG, HT[n]], mark=(k == 7))
                        kb.op("vector", lambda e, bk=bk, tt=tt: e.tensor_scalar(out=Vh[:, tt, :], in0=ps[bk][:, 0:128], scalar1=vmask[:, tt:tt + 1], scalar2=None, op0=ALU.mult),
                              outs=[BV[tt]], ins=[PS[bk], HC])
                        kb.op("scalar", lambda e, bk=bk, tt=tt: e.activation(out=SGt[:, tt, :], in_=ps[bk][:, 128:256], func=AF.Silu), outs=[BV[tt]], ins=[PS[bk]])
                    kb.op("vector", lambda e: e.memset(Sst[:], 0.0), outs=[SST])
                    at_bank = {}

                    def emit_at(c):
                        bk = bank()
                        at_bank[c] = bk
                        cs_ = slice(c * 128, (c + 1) * 128)
                        c0_ = c * 128
                        kb.op("tensor", lambda e: e.matmul(ps[bk][0:64, 0:64], lhsT=Kt[:, c0_:c0_ + 64], rhs=Q1[:, c0_:c0_ + 64], start=True, stop=True),
                              outs=[PS[bk]], ins=[BQ[c // 4]], mark=False)
                        kb.op("tensor", lambda e: e.matmul(ps[bk][:, 64:128], lhsT=Kt[:, cs_], rhs=Q1[:, c0_ + 64:c0_ + 128], start=True, stop=True),
                              outs=[PS[bk]], ins=[BQ[c // 4]])
                        kb.op("vector", lambda e: e.tensor_tensor(out=atm[c % 2][0:64, 0:64], in0=ps[bk][0:64, 0:64], in1=hmask[0:64, 0:64], op=ALU.mult),
                              outs=[ATM[c % 2]], ins=[PS[bk], CST])
                        kb.op("vector", lambda e: e.tensor_tensor(out=atm[c % 2][:, 64:128], in0=ps[bk][:, 64:128], in1=hmask[:, 64:128], op=ALU.mult),
                              outs=[ATM[c % 2]], ins=[PS[bk], CST])

                    for c in range(NT):
                        if c + 1 < NT and c + 1 >= 16:
                            emit_at(c + 1)
                        cs_ = slice(c * 128, (c + 1) * 128)
                        bd = bank()
                        kb.op("tensor", lambda e, bd=bd, c=c: e.matmul(ps[bd][:, 0:128], lhsT=Kh[:, c, :], rhs=Vh[:, c, :], start=True, stop=True),
                              outs=[PS[bd]], ins=[BKH[c], BV[c]])
                        if c >= 16:
                            bo = bank()
                            kb.op("tensor", lambda e, bo=bo, c=c: e.matmul(ps[bo][:, 0:128], lhsT=atm[c % 2][:], rhs=Vh[:, c, :], start=True, stop=False),
                                  outs=[PS[bo]], ins=[ATM[c % 2], BV[c]], mark=False)
                            kb.op("tensor", lambda e, bo=bo, c=c, cs_=cs_: e.matmul(ps[bo][:, 0:128], lhsT=Q2[:, cs_], rhs=sbf[(c - 1) % 2][:], start=False, stop=True),
                                  outs=[PS[bo]], ins=[BQ[c // 4], SBF[(c - 1) % 2]])
                        if c + 1 < NT:
                            kb.op("vector", lambda e, bd=bd, c=c: e.scalar_tensor_tensor(out=Sst[:], in0=Sst[:], scalar=ebl[:, c:c + 1], in1=ps[bd][:, 0:128],
                                                                                     op0=ALU.mult, op1=ALU.add), outs=[SST], ins=[SST, PS[bd], BQ[c // 4]])
                            if c >= 15:
                                kb.op("scalar", lambda e, c=c: e.copy(out=sbf[c % 2][:], in_=Sst[:]), outs=[SBF[c % 2]], ins=[SST])
                        if c < 16:
                            continue
                        i2 = c % 2
                        kb.op("gpsimd", lambda e, i2=i2: e.memset(ssq[i2][:], 0.0), outs=[SSQ[i2]])
                        kb.op("scalar", lambda e, bo=bo, i2=i2: e.activation(out=junk[:], in_=ps[bo][:, 0:128], func=AF.Square, accum_out=ssq[i2][:]),
                              outs=[JK, SSQ[i2]], ins=[PS[bo]])
                        kb.op("scalar", lambda e, i2=i2: e.activation(out=ssq[i2][:], in_=ssq[i2][:], func=AF.Sqrt, scale=1.0 / 128, bias=EPS), outs=[SSQ[i2]], ins=[SSQ[i2]])
                        kb.op("vector", lambda e, i2=i2: e.reciprocal(out=ssq[i2][:], in_=ssq[i2][:]), outs=[SSQ[i2]], ins=[SSQ[i2]])
                        kb.op("vector", lambda e, bo=bo, i2=i2, c0=c0: e.scalar_tensor_tensor(out=of[i2][:], in0=ps[bo][:, 0:128], scalar=ssq[i2][:, 0:1], in1=hng[:, c0:c0 + 128],
                                                                                           op0=ALU.mult, op1=ALU.mult), outs=[OF[i2]], ins=[PS[bo], SSQ[i2], HC])
                        kb.op("gpsimd", lambda e, i2=i2, c=c: e.tensor_tensor(out=obf[i2][:], in0=of[i2][:], in1=SGt[:, c, :], op=ALU.mult), outs=[OBF[i2]], ins=[OF[i2], BV[c]])
                        bt = bank()
                        kb.op("tensor", lambda e, bt=bt, i2=i2: e.matmul(ps[bt][:, 0:128], lhsT=obf[i2][:], rhs=ident[:], start=True, stop=True),
                              outs=[PS[bt]], ins=[OBF[i2], CST])
                        s_ = c - 16
                        kb.op("scalar", lambda e, bt=bt, s_=s_, hd=hd: e.copy(out=oT[:, hd, s_ * 128:(s_ + 1) * 128], in_=ps[bt][:, 0:128]), outs=[OT[hd][s_]], ins=[PS[bt]])
                kb.barrier()
            if "oT" in dbg:
                dump("oT", oT[:], [128, 8, SO], BF16)
            if stop_after == "p1b":
                kb.barrier()
                return nc, dbg_out

            SCL = 64.0 ** -0.5
            if not enable_nsa:
                for jf in range(4, 8):
                    kb.op("vector", lambda e, jf=jf: e.memset(oT[:, jf, :], 0.0), outs=OT[jf])
            with _NSAScope() as pn:
                if not enable_nsa:
                    raise _SkipNSA()
                ovl = sb("ovl", [128, 2, 64], BF16, pn)
                kb.dma("sync", ovl[:], I["ovl"], outs=[CST])
                KEe = sb("KEe", [128, S], BF16, pn)
                KEo = sb("KEo", [128, S], BF16, pn)
                ekt_v = I["ekt"].rearrange("p a b -> p (a b)")
                kb.dma("sync", KEe[64:128, :], ekt_v[64:128, :], outs=[CST])
                kb.dma("sync", KEo[0:64, :], ekt_v[0:64, :], outs=[CST])
                kwT = sb("kwT", [128, S], BF16, pn)
                kcvT = sb("kcvT", [128, S], BF16, pn)
                vs1 = sb("vs1", [128, NT, 80], BF16, pn)
                vw1 = sb("vw1", [128, NT, 80], BF16, pn)
                KS = Buf(kwT[:])
                kcmpT = sb("kcmpT", [128, 256], BF16, pn)
                vcmp1 = sb("vcmp1", [128, 2, 144], BF16, pn)
                KC = Buf(kcmpT[:])
                wk3 = sb("wk3", [128, 8, 384], BF16, pn)
                wv2 = sb("wv2", [128, 8, 128], BF16, pn)
                wqg = sb("wqg", [128, 8, 256], BF16, pn)
                wgt = sb("wgt", [128, 8, 12], BF16, pn)
                WN = Buf(wk3[:])
                cosb = sb("cosb", [128, 512], F32, pn)
                sinb = sb("sinb", [128, 512], F32, pn)
                CSB = Buf(cosb[:])
                rawb = sb("rawb", [128, 512], BF16, pn)
                RAWB = Buf(rawb[:])
                rt1 = sb("rt1", [128, 512], F32, pn)
                rt2 = sb("rt2", [128, 512], F32, pn)
                RT1, RT2 = Buf(rt1[:]), Buf(rt2[:])
                _padn = int(_os0.environ.get("PADN", "0"))
                if _padn:
                    _pad = sb("padn", [128, _padn], F32, pn)
                srr = [0]

                def sbank():
                    srr[0] = (srr[0] + 1) % 3
                    return srr[0]

                mrr = [0]

                def mbank():
                    return 7

                import os as _os
                _dbgmode = int(_os.environ.get("ROPEDBG", "0"))

                def rope_from(bk, dst_ap, dstbuf):
                    if _dbgmode == 1:
                        kb.op("scalar", lambda e: e.copy(out=dst_ap, in_=ps[bk][:]), outs=[dstbuf], ins=[PS[bk]])
                        return
                    if _dbgmode == 3:
                        kb.op("vector", lambda e: e.tensor_tensor(out=rt1[:], in0=ps[bk][:], in1=cosb[:], op=ALU.mult), outs=[RT1], ins=[PS[bk], CSB])
                        kb.op("gpsimd", lambda e: e.tensor_copy(out=dst_ap, in_=rt1[:]), outs=[dstbuf], ins=[RT1])
                        return
                    if _dbgmode == 4:
                        kb.op("scalar", lambda e: e.copy(out=rawb[:], in_=ps[bk][:]), outs=[RAWB], ins=[PS[bk]])
                        b2 = mbank()
                        kb.op("tensor", lambda e: e.matmul(ps[b2][:], lhsT=pswap[:], rhs=rawb[:], start=True, stop=True), outs=[PS[b2]], ins=[RAWB, CST])
                        kb.op("vector", lambda e: e.tensor_tensor(out=rt1[:], in0=ps[bk][:], in1=cosb[:], op=ALU.mult), outs=[RT1], ins=[PS[bk], CSB])
                        kb.op("vector", lambda e: e.tensor_tensor(out=rt2[:], in0=ps[b2][:], in1=sinb[:], op=ALU.mult), outs=[RT2], ins=[PS[b2], CSB])
                        kb.op("vector", lambda e: e.tensor_tensor(out=dst_ap, in0=rt1[:], in1=rt2[:], op=ALU.add), outs=[dstbuf], ins=[RT1, RT2])
                        return
                    if _dbgmode == 5:
                        kb.op("scalar", lambda e: e.copy(out=rawb[:], in_=ps[bk][:]), outs=[RAWB], ins=[PS[bk]])
                        b2 = mbank()
                        kb.op("tensor", lambda e: e.matmul(ps[b2][:], lhsT=pswap[:], rhs=rawb[:], start=True, stop=True), outs=[PS[b2]], ins=[RAWB, CST])
                        kb.op("vector", lambda e: e.tensor_tensor(out=rt1[:], in0=ps[bk][:], in1=cosb[:], op=ALU.mult), outs=[RT1], ins=[PS[bk], CSB])
                        kb.op("scalar", lambda e: e.copy(out=rt2[:], in_=ps[b2][:]), outs=[RT2], ins=[PS[b2]])
                        _sb = cosb if _os.environ.get("USECOS") else sinb
                        kb.op("vector", lambda e: e.tensor_tensor(out=rt2[:], in0=rt2[:], in1=_sb[:], op=ALU.mult), outs=[RT2], ins=[RT2, CSB])
                        kb.op("vector", lambda e: e.tensor_tensor(out=dst_ap, in0=rt1[:], in1=rt2[:], op=ALU.add), outs=[dstbuf], ins=[RT1, RT2])
                        return
                    if _dbgmode in (7, 8):
                        kb.op("scalar", lambda e: e.copy(out=rawb[:], in_=ps[bk][:]), outs=[RAWB], ins=[PS[bk]])
                        b2 = mbank()
                        kb.op("tensor", lambda e: e.matmul(ps[b2][:], lhsT=pswap[:], rhs=rawb[:], start=True, stop=True), outs=[PS[b2]], ins=[RAWB, CST])
                        kb.op("vector", lambda e: e.tensor_tensor(out=rt1[:], in0=ps[bk][:], in1=cosb[:], op=ALU.mult), outs=[RT1], ins=[PS[bk], CSB])
                        kb.op("scalar", lambda e: e.copy(out=rt2[:], in_=ps[b2][:]), outs=[RT2], ins=[PS[b2]])
                        kb.op("vector", lambda e: e.tensor_tensor(out=rt2[:], in0=rt2[:], in1=sinb[:], op=ALU.mult), outs=[RT2], ins=[RT2, CSB])
                        if _dbgmode == 8:
                            kb.op("vector", lambda e: e.tensor_tensor(out=rt1[:], in0=rt1[:], in1=rt2[:], op=ALU.add), outs=[RT1], ins=[RT1, RT2])
                        kb.op("scalar", lambda e: e.copy(out=dst_ap, in_=rt1[:]), outs=[dstbuf], ins=[RT1])
                        return
                    if _dbgmode in (9, 10):
                        kb.op("vector", lambda e: e.tensor_tensor(out=rt1[:], in0=ps[bk][:], in1=cosb[:], op=ALU.mult), outs=[RT1], ins=[PS[bk], CSB])
                        if _dbgmode == 9:
                            kb.op("scalar", lambda e: e.copy(out=rt2[:], in_=ps[bk][:]), outs=[RT2], ins=[PS[bk]])
                        else:
                            kb.op("vector", lambda e: e.tensor_tensor(out=rt2[:], in0=rt1[:], in1=cosb[:], op=ALU.mult), outs=[RT2], ins=[RT1, CSB])
                        kb.op("gpsimd", lambda e: e.tensor_copy(out=dst_ap, in_=rt1[:]), outs=[dstbuf], ins=[RT1])
                        return
                    if _dbgmode in (11, 12):
                        kb.op("scalar", lambda e: e.copy(out=rawb[:], in_=ps[bk][:]), outs=[RAWB], ins=[PS[bk]])
                        b2 = mbank()
                        kb.op("tensor", lambda e: e.matmul(ps[b2][:], lhsT=pswap[:], rhs=rawb[:], start=True, stop=True), outs=[PS[b2]], ins=[RAWB, CST])
                        kb.op("vector", lambda e: e.tensor_tensor(out=rt1[:], in0=ps[bk][:], in1=cosb[:], op=ALU.mult), outs=[RT1], ins=[PS[bk], CSB, RAWB])
                        kb.op("gpsimd", lambda e: e.tensor_copy(out=dst_ap, in_=rt1[:]), outs=[dstbuf], ins=[RT1])
                        if _dbgmode == 12:
                            return
                        kb.op("vector", lambda e: e.tensor_tensor(out=rt1[:], in0=ps[b2][:], in1=sinb[:], op=ALU.mult), outs=[RT1], ins=[PS[b2], CSB])
                        kb.op("gpsimd", lambda e: e.tensor_tensor(out=dst_ap, in0=dst_ap, in1=rt1[:], op=ALU.add), outs=[dstbuf], ins=[RT1, dstbuf])
                        return
                    if _dbgmode == 2:
                        kb.op("scalar", lambda e: e.copy(out=rawb[:], in_=ps[bk][:]), outs=[RAWB], ins=[PS[bk]])
                        b2 = mbank()
                        kb.op("tensor", lambda e: e.matmul(ps[b2][:], lhsT=pswap[:], rhs=rawb[:], start=True, stop=True), outs=[PS[b2]], ins=[RAWB, CST])
                        kb.op("scalar", lambda e: e.copy(out=dst_ap, in_=ps[b2][:]), outs=[dstbuf], ins=[PS[b2]])
                        return
                    kb.op("scalar", lambda e: e.copy(out=rawb[:], in_=ps[bk][:]), outs=[RAWB], ins=[PS[bk]])
                    b2 = mbank()
                    kb.op("tensor", lambda e: e.matmul(ps[b2][:], lhsT=pswap[:], rhs=rawb[:], start=True, stop=True), outs=[PS[b2]], ins=[RAWB, CST])
                    kb.op("vector", lambda e: e.tensor_tensor(out=rt1[:], in0=ps[bk][:], in1=cosb[:], op=ALU.mult), outs=[RT1], ins=[PS[bk], CSB])
                    kb.op("vector", lambda e: e.tensor_tensor(out=rt2[:], in0=ps[b2][:], in1=sinb[:], op=ALU.mult), outs=[RT2], ins=[PS[b2], CSB])
                    if isinstance(dst_ap, tuple):
                        kb.op("gpsimd", lambda e: e.tensor_tensor(out=dst_ap[0], in0=rt1[0:64, :], in1=rt2[0:64, :], op=ALU.add), outs=[dstbuf], ins=[RT1, RT2])
                        kb.op("gpsimd", lambda e: e.tensor_tensor(out=dst_ap[1], in0=rt1[64:128, :], in1=rt2[64:128, :], op=ALU.add), outs=[dstbuf], ins=[RT1, RT2])
                    else:
                        kb.op("gpsimd", lambda e: e.tensor_tensor(out=dst_ap, in0=rt1[:], in1=rt2[:], op=ALU.add), outs=[dstbuf], ins=[RT1, RT2])

                for g in range(2):
                    for j, cbase in enumerate((2560, 2688)):
                        kb.dma("gpsimd", wk3[:, :, j * 64:(j + 1) * 64], w_in_v[:, :, cbase + g * 64:cbase + g * 64 + 64], outs=[WN])
                    for j, cbase in enumerate((2816, 2816, 3072, 3072)):
                        kb.dma("gpsimd", wk3[:, :, 128 + j * 64:128 + (j + 1) * 64], w_in_v[:, :, cbase + g * 64:cbase + g * 64 + 64], outs=[WN])
                    for j, cbase in enumerate((2944, 3200)):
                        kb.dma("gpsimd", wv2[:, :, j * 64:(j + 1) * 64], w_in_v[:, :, cbase + g * 64:cbase + g * 64 + 64], outs=[WN])
                    kb.dma("gpsimd", wqg[:], w_in_v[:, :, 2048 + g * 256:2048 + (g + 1) * 256], outs=[WN])
                    kb.dma("gpsimd", wgt[:], w_in_v[:, :, 3328 + g * 12:3328 + (g + 1) * 12], outs=[WN])
                    kb.op("vector", lambda e: e.memset(vs1[:, :, 64:65], 1.0), outs=[KS])
                    kb.op("vector", lambda e: e.memset(vw1[:, :, 64:65], 1.0), outs=[KS])
                    if stop_after == "p1c_a":
                        dump("wk3", wk3[:], [128, 8, 384], BF16)
                        return nc, dbg_out
                    for n in range(NB):
                        sl = slice(n * 512, (n + 1) * 512)
                        kb.dma("sync", cosb[:], I["cosT"][:, sl], outs=[CSB])
                        kb.dma("sync", sinb[:], I["sinT"][:, sl], outs=[CSB])
                        for j in range(3):
                            bk = sbank()
                            for k in range(8):
                                kb.op("tensor", lambda e, k=k, bk=bk, j=j: e.matmul(ps[bk][:], lhsT=wk3[:, k, j * 128:(j + 1) * 128], rhs=hT[:, k, sl],
                                                                                    start=(k == 0), stop=(k == 7)), outs=[PS[bk]], ins=[WN, HT[n]], mark=(k == 7))
                            if j == 0:
                                kb.op("scalar", lambda e, bk=bk: e.copy(out=kcvT[:, sl], in_=ps[bk][:]), outs=[KS], ins=[PS[bk]])
                            else:
                                rope_from(bk, (KEe[0:64, sl], KEo[64:128, sl]) if j == 1 else kwT[:, sl], KS)
                        if stop_after == "p1c_b":
                                return nc, dbg_out
                        for i in range(4):
                            tt = 4 * n + i
                            bk = mbank()
                            for k in range(8):
                                kb.op("tensor", lambda e, k=k, bk=bk, tt=tt: e.matmul(ps[bk][:, 0:128], lhsT=hT[:, k, tt * 128:(tt + 1) * 128], rhs=wv2[:, k, :],
                                                                                     start=(k == 0), stop=(k == 7)), outs=[PS[bk]], ins=[WN, HT[n]], mark=(k == 7))
                            kb.op("scalar", lambda e, bk=bk, tt=tt: e.copy(out=vs1[:, tt, 0:64], in_=ps[bk][:, 0:64]), outs=[KS], ins=[PS[bk]])
                            kb.op("vector", lambda e, bk=bk, tt=tt: e.tensor_copy(out=vw1[:, tt, 0:64], in_=ps[bk][:, 64:128]), outs=[KS], ins=[PS[bk]])
                    if stop_after == "p1c_k":
                        dump("kcvT", kcvT[:], [128, S], BF16)
                        dump("vs1", vs1[:], [128, NT, 80], BF16)
                        return nc, dbg_out
                    with ExitStack() as pc:
                        w1kv = sb("w1kv", [128, 32, 256], BF16, pc)
                        peT = sb("peT", [128, 32], F32, pc)
                        peTb = sb("peTb", [128, 32], BF16, pc)
                        b1kv = sb("b1kv", [128, 4], F32, pc)
                        w2k2 = sb("w2k2", [128, 2, 128], BF16, pc)
                        w2v = sb("w2v", [128, 2, 64], BF16, pc)
                        hid = sb("hid", [128, 4, 256], BF16, pc)
                        beff = sb("beff", [128, 4], F32, pc)
                        gx = sb("gx", [128, 256], F32, pc)
                        gu = sb("gu", [128, 256], F32, pc)
                        gs = sb("gs", [128, 256], F32, pc)
                        CW = Buf(w1kv[:])
                        HID = Buf(hid[:])
                        GX = Buf(gx[:])
                        kb.dma("gpsimd", w1kv[0:64], I["w1k"], outs=[CW])
                        kb.dma("gpsimd", w1kv[64:128], I["w1v"], outs=[CW])
                        kb.dma("sync", peT[0:64], I["peTk"], outs=[CW])
                        kb.dma("sync", peT[64:128], I["peTv"], outs=[CW])
                        kb.dma("sync", b1kv[:, 0:2], I["b1k"], outs=[CW])
                        kb.dma("sync", b1kv[:, 2:4], I["b1v"], outs=[CW])
                        kb.dma("gpsimd", w2k2[:, :, 0:64], I["w2k"], outs=[CW])
                        kb.dma("gpsimd", w2k2[:, :, 64:128], I["w2k"], outs=[CW])
                        kb.dma("gpsimd", w2v[:], I["w2v"], outs=[CW])
                        kb.op("vector", lambda e: e.tensor_copy(out=peTb[:], in_=peT[:]), outs=[CW], ins=[CW])
                        kb.op("vector", lambda e: e.memset(hid[:], 0.0), outs=[HID])
                        for kv in range(2):
                            p0_ = kv * 64
                            for hc in range(2):
                                bk, bb = sbank(), mbank()
                                for l in range(32):
                                    kb.op("tensor", lambda e, l=l, bk=bk, hc=hc, p0_=p0_: e.matmul(ps[bk][:, 0:255], lhsT=w1kv[p0_:p0_ + 64, l, hc * 128:(hc + 1) * 128],
                                                                                                rhs=kcvT[p0_:p0_ + 64, l:l + 16 * 254 + 1:16], start=(l == 0), stop=(l == 31)),
                                          outs=[PS[bk]], ins=[CW, KS], mark=(l == 31))
                                for l in range(32):
                                    kb.op("tensor", lambda e, l=l, bb=bb, hc=hc, p0_=p0_: e.matmul(ps[bb][:, 0:1], lhsT=w1kv[p0_:p0_ + 64, l, hc * 128:(hc + 1) * 128],
                                                                                                rhs=peTb[p0_:p0_ + 64, l:l + 1], start=(l == 0), stop=(l == 31)),
                                          outs=[PS[bb]], ins=[CW], mark=(l == 31))
                                ci = kv * 2 + hc
                                kb.op("vector", lambda e, bb=bb, ci=ci: e.tensor_tensor(out=beff[:, ci:ci + 1], in0=ps[bb][:, 0:1], in1=b1kv[:, ci:ci + 1], op=ALU.add),
                                      outs=[GX], ins=[PS[bb], CW])
                                kb.op("vector", lambda e, bk=bk, ci=ci: e.tensor_scalar(out=gx[:, 0:255], in0=ps[bk][:, 0:255], scalar1=beff[:, ci:ci + 1], scalar2=None, op0=ALU.add),
                                      outs=[GX], ins=[PS[bk], GX])
                                kb.op("vector", lambda e: e.tensor_tensor(out=gu[:, 0:255], in0=gx[:, 0:255], in1=gx[:, 0:255], op=ALU.mult), outs=[GX], ins=[GX])
                                kb.op("vector", lambda e: e.tensor_scalar(out=gu[:, 0:255], in0=gu[:, 0:255], scalar1=0.044715, scalar2=1.0, op0=ALU.mult, op1=ALU.add), outs=[GX], ins=[GX])
                                kb.op("vector", lambda e: e.tensor_tensor(out=gu[:, 0:255], in0=gu[:, 0:255], in1=gx[:, 0:255], op=ALU.mult), outs=[GX], ins=[GX])
                                kb.op("scalar", lambda e: e.activation(out=gs[:, 0:255], in_=gu[:, 0:255], func=AF.Sigmoid, scale=1.5957691216057308), outs=[GX], ins=[GX])
                                kb.op("vector", lambda e, ci=ci: e.tensor_tensor(out=hid[:, ci, 0:255], in0=gx[:, 0:255], in1=gs[:, 0:255], op=ALU.mult), outs=[HID], ins=[GX])
                        bk = sbank()
                        for hc in range(2):
                            kb.op("tensor", lambda e, hc=hc, bk=bk: e.matmul(ps[bk][:, 0:256], lhsT=w2k2[:, hc, :], rhs=hid[:, hc, :], start=(hc == 0), stop=(hc == 1)),
                                  outs=[PS[bk]], ins=[CW, HID], mark=(hc == 1))
                        kb.op("scalar", lambda e, bk=bk: e.copy(out=kcmpT[:], in_=ps[bk][:, 0:256]), outs=[KC], ins=[PS[bk]])
                        for ct in range(2):
                            bk = sbank()
                            for hc in range(2):
                                kb.op("tensor", lambda e, hc=hc, bk=bk, ct=ct: e.matmul(ps[bk][:, 0:64], lhsT=hid[:, 2 + hc, ct * 128:(ct + 1) * 128], rhs=w2v[:, hc, :],
                                                                                       start=(hc == 0), stop=(hc == 1)), outs=[PS[bk]], ins=[CW, HID], mark=(hc == 1))
                            kb.op("scalar", lambda e, bk=bk, ct=ct: e.copy(out=vcmp1[:, ct, 0:64], in_=ps[bk][:, 0:64]), outs=[KC], ins=[PS[bk]])
                        kb.op("vector", lambda e: e.memset(vcmp1[:, :, 64:65], 1.0), outs=[KC])
                        kb.op("vector", lambda e: e.tensor_copy(out=vcmp1[:, :, 65:129], in_=ovl[:]), outs=[KC], ins=[CST])
                        kb.barrier()
                    if stop_after == "p1c_c":
                        dump("kcmpT", kcmpT[:], [128, 256], BF16)
                        dump("vcmp1", vcmp1[:], [128, 2, 144], BF16)
                        return nc, dbg_out
                    with ExitStack() as pq:
                        wband = sb("wband", [128, 8, 512], BF16, pq)
                        QC = Buf(wband[:])
                        kb.dma("sync", wband[:], I["wband"], outs=[QC])
                        qn = [[sb(f"qn{ch}{par}", [128, 512], BF16, pq) for par in range(2)] for ch in range(2)]
                        QN = Buf(qn[0][0][:])
                        qTb = sb("qTb", [128, 2, 512], BF16, pq)
                        qrTb = sb("qrTb", [128, 2, 512], BF16, pq)
                        QB_ = Buf(qTb[:])
                        QRB = Buf(qrTb[:])
                        gts = sb("gts", [128, 4, 12], F32, pq)
                        GTS = Buf(gts[:])
                        cmpbb = sb("cmpbb", [128, 512], BF16, pq)
                        CMB = Buf(cmpbb[:])
                        pt = [sb(f"pt{i}", [128, 512], BF16, pq) for i in range(3)]
                        PT = [Buf(t[:]) for t in pt]
                        onsa = sb("onsa", [128, 4, 256], F32, pq)
                        ONSA = Buf(onsa[:])
                        obn = sb("obn", [128, 4, 256], BF16, pq)
                        OBN = Buf(obn[:])
                        pslc = sb("pslc", [128, 4, 64], F32, pq)
                        PSLC = Buf(pslc[:])
                        sadd = sb("sadd", [128, 64], F32, pq)
                        SADD = Buf(sadd[:])
                        score = sb("score", [128, 64], F32, pq)
                        stmp = sb("stmp", [128, 64], F32, pq)
                        sel = sb("sel", [128, 64], F32, pq)
                        m8 = sb("m8", [128, 16], F32, pq)
                        negb4 = [sb(f"negb{i}", [128, 128], BF16, pq) for i in range(4)]
                        SEL = Buf(score[:])
                        negbT = sb("negbT", [128, 512], BF16, pq)
                        NBT = Buf(negbT[:])
                        rz = sb("rz", [128, 4], F32, pq)
                        RZ = Buf(rz[:])
                        accs = [ps[3][:, 0:129], ps[4][:, 0:129], ps[5][:, 0:129], ps[6][:, 0:129]]
                        ACC = [PS[3], PS[4], PS[5], PS[6]]
                        prr = [0]

                        def run_branch(steps):
                            LA = 2
                            n_ = len(steps)
                            for idx_ in range(n_ + LA):
                                if idx_ < n_:
                                    st = steps[idx_]
                                    bk = sbank()
                                    nm = len(st["s"])
                                    for idx, (l_, r_, insb) in enumerate(st["s"]):
                                        kb.op("tensor", lambda e, l_=l_, r_=r_, idx=idx, nm=nm, bk=bk: e.matmul(ps[bk][:], lhsT=l_, rhs=r_, start=(idx == 0), stop=(idx == nm - 1)),
                                              outs=[PS[bk]], ins=insb, mark=(idx == nm - 1))
                                    prr[0] = (prr[0] + 1) % 3
                                    pi = prr[0]
                                    kb.op("scalar", lambda e, bk=bk, pi=pi: e.activation(out=pt[pi][:], in_=ps[bk][:], func=AF.Exp, scale=SCL), outs=[PT[pi]], ins=[PS[bk]])
                                    st["pi"] = pi
                                if idx_ >= LA:
                                    prev = steps[idx_ - LA]
                                    pi = prev["pi"]
                                    for (i, rhs_ap, w_, st_, sp_) in prev["pv"]:
                                        kb.op("tensor", lambda e, i=i, rhs_ap=rhs_ap, w_=w_, st_=st_, sp_=sp_, pi=pi: e.matmul(accs[i][:, 0:w_], lhsT=pt[pi][:, i * 128:(i + 1) * 128], rhs=rhs_ap,
                                                                                                                 start=st_, stop=sp_),
                                              outs=[ACC[i]], ins=[PT[pi], KS, KC])

                        def finish(h, br, first):
                            zc = 64
                            for i in range(4):
                                kb.op("vector", lambda e, i=i: e.tensor_scalar(out=rz[:, i:i + 1], in0=accs[i][:, zc:zc + 1], scalar1=1e-30, scalar2=None, op0=ALU.max),
                                      outs=[RZ], ins=[ACC[i]])
                            kb.op("vector", lambda e: e.reciprocal(out=rz[:], in_=rz[:]), outs=[RZ], ins=[RZ])
                            if br == 0:
                                for i in range(4):
                                    if h == 0:
                                        kb.op("vector", lambda e, i=i: e.tensor_scalar(out=pslc[:, i, :], in0=accs[i][:, 65:129], scalar1=rz[:, i:i + 1], scalar2=None, op0=ALU.mult),
                                              outs=[PSLC], ins=[ACC[i], RZ])
                                    else:
                                        kb.op("vector", lambda e, i=i: e.scalar_tensor_tensor(out=pslc[:, i, :], in0=accs[i][:, 65:129], scalar=rz[:, i:i + 1], in1=pslc[:, i, :],
                                                                                           op0=ALU.mult, op1=ALU.add), outs=[PSLC], ins=[ACC[i], RZ, PSLC])
                            kb.op("vector", lambda e: e.tensor_tensor(out=rz[:], in0=rz[:], in1=gts[:, :, h * 3 + br], op=ALU.mult), outs=[RZ], ins=[RZ, GTS])
                            for i in range(4):
                                dst = onsa[:, i, h * 64:(h + 1) * 64]
                                if first:
                                    kb.op("vector", lambda e, i=i, dst=dst: e.tensor_scalar(out=dst, in0=accs[i][:, 0:64], scalar1=rz[:, i:i + 1], scalar2=None, op0=ALU.mult),
                                          outs=[ONSA], ins=[ACC[i], RZ])
                                else:
                                    kb.op("vector", lambda e, i=i, dst=dst: e.scalar_tensor_tensor(out=dst, in0=accs[i][:, 0:64], scalar=rz[:, i:i + 1], in1=dst, op0=ALU.mult, op1=ALU.add),
                                          outs=[ONSA], ins=[ACC[i], RZ, ONSA])

                        wband4 = sb("wband4", [128, 8, 512], BF16, pq)
                        cmpb0 = sb("cmpb0", [128, 512], BF16, pq)
                        kb.dma("sync", wband4[:], I["wband4"], outs=[QC])
                        kb.dma("sync", cmpb0[:], I["cmpb0"], outs=[QC])
                        for qb in range(4, NB):
                            sl = slice(qb * 512, (qb + 1) * 512)
                            kb.dma("sync", cosb[:], I["cosT"][:, sl], outs=[CSB])
                            kb.dma("sync", sinb[:], I["sinT"][:, sl], outs=[CSB])
                            kb.dma("sync", cmpbb[:], I["cmpb"][:, qb, :], outs=[CMB])
                            for ch in range(2):
                                bk = sbank()
                                for k in range(8):
                                    kb.op("tensor", lambda e, k=k, bk=bk, ch=ch: e.matmul(ps[bk][:], lhsT=wqg[:, k, ch * 128:(ch + 1) * 128], rhs=hT[:, k, sl],
                                                                                         start=(k == 0), stop=(k == 7)), outs=[PS[bk]], ins=[WN, HT[qb]], mark=(k == 7))
                                kb.op("scalar", lambda e, bk=bk, ch=ch: e.copy(out=qTb[:, ch, :], in_=ps[bk][:]), outs=[QB_], ins=[PS[bk]])
                                rope_from(bk, qrTb[:, ch, :], QRB)
                            for i in range(4):
                                tt = 4 * qb + i
                                bk = mbank()
                                for k in range(8):
                                    kb.op("tensor", lambda e, k=k, bk=bk, tt=tt: e.matmul(ps[bk][:, 0:12], lhsT=hT[:, k, tt * 128:(tt + 1) * 128], rhs=wgt[:, k, :],
                                                                                         start=(k == 0), stop=(k == 7)), outs=[PS[bk]], ins=[WN, HT[qb]], mark=(k == 7))
                                kb.op("scalar", lambda e, bk=bk, i=i: e.activation(out=gts[:, i, :], in_=ps[bk][:, 0:12], func=AF.Sigmoid), outs=[GTS], ins=[PS[bk]])
                            if stop_after == "p1c_qa":
                                dump("onsa", onsa[:], [128, 4, 256])
                                return nc, dbg_out
                            ncts = 1 if qb < 4 else 2
                            for h in range(4):
                                ch, p0_ = h // 2, (h % 2) * 64
                                steps = []
                                for ct in range(ncts):
                                    smm = [(kcmpT[p0_:p0_ + 64, ct * 128:(ct + 1) * 128], qTb[p0_:p0_ + 64, ch, :], [KC, QB_])]
                                    if ct == ncts - 1:
                                        smm.append((ident[:], cmpbb[:], [CST, CMB]))
                                    else:
                                        smm.append((ident[:], cmpb0[:], [CST, QC]))
                                    pv = [(i, vcmp1[:, ct, 0:129], 129, ct == 0, ct == ncts - 1) for i in range(4)]
                                    steps.append({"s": smm, "pv": pv})
                                run_branch(steps)
                                finish(h, 0, True)
                            if stop_after == "p1c_qb":
                                dump("onsa", onsa[:], [128, 4, 256])
                                return nc, dbg_out
                            for i in range(4):
                                qt = 4 * qb + i
                                kb.dma("sync", sadd[:], I["seladd"][:, qt, :], outs=[SADD])
                                kb.op("vector", lambda e, i=i: e.tensor_tensor(out=score[:], in0=pslc[:, i, :], in1=sadd[:], op=ALU.add), outs=[SEL], ins=[PSLC, SADD])
                                kb.op("vector", lambda e: e.max(out=m8[:, 0:8], in_=score[:]), outs=[SEL], ins=[SEL])
                                kb.op("vector", lambda e: e.match_replace(out=stmp[:], in_to_replace=m8[:, 0:8], in_values=score[:], imm_value=-3e38), outs=[SEL], ins=[SEL])
                                kb.op("vector", lambda e: e.max(out=m8[:, 8:16], in_=stmp[:]), outs=[SEL], ins=[SEL])
                                kb.op("vector", lambda e: e.tensor_scalar(out=sel[:], in0=score[:], scalar1=m8[:, 15:16], scalar2=None, op0=ALU.is_ge), outs=[SEL], ins=[SEL])
                                kb.op("vector", lambda e: e.scalar_tensor_tensor(out=sel[:], in0=score[:], scalar=-1e29, in1=sel[:], op0=ALU.is_gt, op1=ALU.mult), outs=[SEL], ins=[SEL])
                                kb.op("vector", lambda e: e.tensor_scalar(out=negb4[i][:, 0:64], in0=sel[:], scalar1=-1.0, scalar2=-NEG, op0=ALU.add, op1=ALU.mult), outs=[SEL], ins=[SEL])
                                kb.op("vector", lambda e: e.tensor_scalar(out=negb4[i][:, 64:128], in0=sel[:], scalar1=-1.0, scalar2=-NEG, op0=ALU.add, op1=ALU.mult), outs=[SEL], ins=[SEL])
                            for h in range(4):
                                ch, p0_ = h // 2, (h % 2) * 64
                                steps = []
                                jmin = max(0, 4 - 4 * qb)
                                for j in range(jmin, 8):
                                    kt = 4 * qb - 4 + j
                                    smm = [(kwT[p0_:p0_ + 64, kt * 128:(kt + 1) * 128], qrTb[p0_:p0_ + 64, ch, :], [KS, QRB]),
                                           (ident[:], (wband4 if qb == 4 else wband)[:, j, :], [CST, QC])]
                                    pv = [(i, vw1[:, kt, 0:65], 65, j == max(i, jmin), j == i + 4) for i in range(4) if i <= j <= i + 4]
                                    steps.append({"s": smm, "pv": pv})
                                run_branch(steps)
                                finish(h, 2, False)
                            for i in range(4):
                                bk = mbank()
                                kb.op("tensor", lambda e, bk=bk, i=i: e.matmul(ps[bk][:, 0:128], lhsT=negb4[i][:], rhs=ident[:], start=True, stop=True), outs=[PS[bk]], ins=[SEL, CST])
                                kb.op("scalar", lambda e, bk=bk, i=i: e.copy(out=negbT[:, i * 128:(i + 1) * 128], in_=ps[bk][:, 0:128]), outs=[NBT], ins=[PS[bk]])
                            for ch in range(2):
                                kb.op("gpsimd", lambda e, ch=ch: e.tensor_copy(out=qn[ch][0][0:64, :], in_=qrTb[0:64, ch, :]), outs=[QN], ins=[QRB])
                                kb.op("gpsimd", lambda e, ch=ch: e.tensor_copy(out=qn[ch][0][64:128, :], in_=negbT[64:128, :]), outs=[QN], ins=[NBT])
                                kb.op("gpsimd", lambda e, ch=ch: e.tensor_copy(out=qn[ch][1][0:64, :], in_=negbT[0:64, :]), outs=[QN], ins=[NBT])
                                kb.op("gpsimd", lambda e, ch=ch: e.tensor_copy(out=qn[ch][1][64:128, :], in_=qrTb[64:128, ch, :]), outs=[QN], ins=[QRB])
                            for h in range(4):
                                ch, p0_ = h // 2, (h % 2) * 64
                                KE_ = KEe if h % 2 == 0 else KEo
                                steps = []
                                for kt in range(4 * qb + 4):
                                    smm = [(KE_[:, kt * 128:(kt + 1) * 128], qn[ch][h % 2][:], [KS, QN, CST])]
                                    if kt >= 4 * qb:
                                        smm.append((ident[:], wband[:, 4 + kt - 4 * qb, :], [CST, QC]))
                                    pv = [(i, vs1[:, kt, 0:65], 65, kt == 0, kt == 4 * qb + i) for i in range(4) if kt <= 4 * qb + i]
                                    steps.append({"s": smm, "pv": pv})
                                run_branch(steps)
                                finish(h, 1, False)
                            if stop_after == "p1c_q":
                                dump("onsa", onsa[:], [128, 4, 256])
                                dump("pslc", pslc[:], [128, 4, 64])
                                dump("negbT", negbT[:], [128, 512], BF16)
                                return nc, dbg_out
                            kb.op("gpsimd", lambda e: e.tensor_copy(out=obn[:], in_=onsa[:]), outs=[OBN], ins=[ONSA])
                            for i in range(4):
                                qt = 4 * qb + i
                                s_ = qt - 16
                                for ch in range(2):
                                    jf = 4 + g * 2 + ch
                                    bk = mbank()
                                    kb.op("tensor", lambda e, bk=bk, i=i, ch=ch: e.matmul(ps[bk][:, 0:128], lhsT=obn[:, i, ch * 128:(ch + 1) * 128], rhs=ident[:], start=True, stop=True),
                                          outs=[PS[bk]], ins=[OBN, CST])
                                    dst = oT[:, jf, s_ * 128:(s_ + 1) * 128]
                                    kb.op("scalar", lambda e, bk=bk, dst=dst: e.copy(out=dst, in_=ps[bk][:, 0:128]), outs=[OT[jf][s_]], ins=[PS[bk]])
                        kb.barrier()
                kb.barrier()
            if "oT2" in dbg:
                dump("oT2", oT[:], [128, 8, SO], BF16)
            if stop_after == "p1c":
                kb.barrier()
                return nc, dbg_out
        kb.barrier()
        with ExitStack() as p2:
            acc = sb("acc", [128, 8, SO], F32, p2)
            ACCB = [Buf(acc[:, :, nb * 512:(nb + 1) * 512]) for nb in range(4)]
            wo = sb("wo", [128, 8, D], BF16, p2)
            WO = Buf(wo[:])
            fg = sb("fg", [128, 8], F32, p2)
            FG = Buf(fg[:])
            kb.dma("sync", fg[:], I["fg"], outs=[FG])
            xTo_v = I["xTo"].rearrange("(k p) t -> p k t", p=128)
            for nb in range(4):
                kb.dma("sync", acc[:, :, nb * 512:(nb + 1) * 512], xTo_v[:, :, nb * 512:(nb + 1) * 512], outs=[ACCB[nb]])
            w_out_v = I["w_out"].rearrange("(k p) c -> p k c", p=128)
            for k in range(8):
                kb.dma("gpsimd", wo[:, k, :], w_out_v[:, k, :], outs=[WO])
            rr2 = [0]

            def bank2():
                rr2[0] = (rr2[0] + 1) % 8
                return rr2[0]

            for nb in range(4):
                sl = slice(nb * 512, (nb + 1) * 512)
                for m in range(8):
                    bk = bank2()
                    for k in range(8):
                        kb.op("tensor", lambda e, k=k, m=m, bk=bk, sl=sl: e.matmul(ps[bk][:], lhsT=wo[:, k, m * 128:(m + 1) * 128], rhs=oT[:, k, sl], start=(k == 0), stop=(k == 7)),
                              outs=[PS[bk]], ins=[WO] + [OT[k][s] for s in range(nb * 4, nb * 4 + 4)], mark=(k == 7))
                    kb.op("vector", lambda e, m=m, bk=bk, sl=sl: e.scalar_tensor_tensor(out=acc[:, m, sl], in0=ps[bk][:], scalar=g1c(m), in1=acc[:, m, sl], op0=ALU.mult, op1=ALU.add),
                          outs=[ACCB[nb]], ins=[PS[bk], ACCB[nb], MOD])
            if "x2T" in dbg:
                dump("x2T", acc[:], [128, 8, SO])
            h2T = sb("h2T", [128, 8, SO], BF16, p2)
            H2 = [Buf(h2T[:, :, nb * 512:(nb + 1) * 512]) for nb in range(4)]
            with ExitStack() as p2a:
                sqa = sb("sqa", [128, 8, 512], F32, p2a)
                SQA = Buf(sqa[:])
                rsa = sb("rsa", [128, 512], F32, p2a)
                RSA = Buf(rsa[:])
                tma = [sb(f"tma{i}", [128, 512], F32, p2a) for i in range(2)]
                TMA = [Buf(t[:]) for t in tma]
                for nb in range(4):
                    sl = slice(nb * 512, (nb + 1) * 512)
                    kb.op("scalar", lambda e, sl=sl: e.activation(out=sqa[:], in_=acc[:, :, sl], func=AF.Square), outs=[SQA], ins=[ACCB[nb]])
                    bk = bank2()
                    for k in range(8):
                        kb.op("tensor", lambda e, k=k, bk=bk: e.matmul(ps[bk][:], lhsT=onesf[:], rhs=sqa[:, k, :], start=(k == 0), stop=(k == 7)),
                              outs=[PS[bk]], ins=[SQA, CST], mark=(k == 7))
                    kb.op("scalar", lambda e, bk=bk: e.activation(out=rsa[:], in_=ps[bk][:], func=AF.Sqrt, scale=1.0 / D, bias=EPS), outs=[RSA], ins=[PS[bk]])
                    kb.op("vector", lambda e: e.reciprocal(out=rsa[:], in_=rsa[:]), outs=[RSA], ins=[RSA])
                    for k in range(8):
                        T = TMA[k % 2]
                        t_ = tma[k % 2]
                        kb.op("vector", lambda e, k=k, t_=t_, sl=sl: e.tensor_tensor(out=t_[:], in0=acc[:, k, sl], in1=rsa[:], op=ALU.mult), outs=[T], ins=[ACCB[nb], RSA])
                        kb.op("scalar", lambda e, k=k, t_=t_, sl=sl: e.activation(out=h2T[:, k, sl], in_=t_[:], func=AF.Identity, scale=a2[:, k:k + 1], bias=sh2(k)),
                              outs=[H2[nb]], ins=[T, MOD])
                kb.barrier()
            if "h2T" in dbg:
                dump("h2T", h2T[:], [128, 8, SO], BF16)
            gw = sb("gw", [128, 16, 65], F32, p2)
            GW = Buf(gw[:])
            kb.op("vector", lambda e: e.memset(gw[:], 1.0), outs=[GW])
            with ExitStack() as p2r:
                rwf = sb("rwf", [128, 8, 64], F32, p2r)
                rwb = sb("rwb", [128, 8, 64], BF16, p2r)
                rbias = sb("rbias", [128, 64], F32, p2r)
                RW = Buf(rwf[:])
                kb.dma("sync", rwf[:], I["rw"], outs=[RW])
                kb.dma("sync", rbias[:], I["rbias"], outs=[RW])
                kb.op("vector", lambda e: e.tensor_copy(out=rwb[:], in_=rwf[:]), outs=[RW], ins=[RW])
                scr = sb("scr", [128, 64], F32, p2r)
                chs = sb("chs", [128, 64], F32, p2r)
                eq = sb("eq", [128, 64], F32, p2r)
                chm = sb("chm", [128, 64], F32, p2r)
                m1 = sb("m1", [128, 8], F32, p2r)
                m2 = sb("m2", [128, 8], F32, p2r)
                gsm = sb("gsm", [128, 8], F32, p2r)
                g8 = sb("g8", [128, 8], F32, p2r)
                gmk = sb("gmk", [128, 8], F32, p2r)
                e8 = sb("e8", [128, 8], F32, p2r)
                ssum = sb("ssum", [128, 1], F32, p2r)
                RT = Buf(scr[:])
                v3 = lambda ap: ap.rearrange("p (g j) -> p g j", j=8)
                b3 = lambda ap: ap.rearrange("p (g o) -> p g o", o=1).to_broadcast([128, 8, 8])
                for tt in range(16):
                    bk = bank2()
                    for k in range(8):
                        kb.op("tensor", lambda e, k=k, bk=bk, tt=tt: e.matmul(ps[bk][:, 0:64], lhsT=h2T[:, k, tt * 128:(tt + 1) * 128], rhs=rwb[:, k, :], start=(k == 0), stop=(k == 7)),
                              outs=[PS[bk]], ins=[H2[tt // 4], RW], mark=(k == 7))
                    kb.op("scalar", lambda e, bk=bk: e.activation(out=scr[:], in_=ps[bk][:, 0:64], func=AF.Sigmoid), outs=[RT], ins=[PS[bk]])
                    V = lambda f: kb.op("vector", f, outs=[RT], ins=[RT, RW])
                    V(lambda e: e.tensor_tensor(out=chs[:], in0=scr[:], in1=rbias[:], op=ALU.add))
                    V(lambda e: e.tensor_reduce(out=m1[:], in_=v3(chs[:]), axis=AX.X, op=ALU.max))
                    V(lambda e: e.tensor_tensor(out=v3(eq[:]), in0=v3(chs[:]), in1=b3(m1[:]), op=ALU.is_equal))
                    V(lambda e: e.scalar_tensor_tensor(out=eq[:], in0=eq[:], scalar=-1e30, in1=chs[:], op0=ALU.mult, op1=ALU.add))
                    V(lambda e: e.tensor_reduce(out=m2[:], in_=v3(eq[:]), axis=AX.X, op=ALU.max))
                    V(lambda e: e.tensor_tensor(out=gsm[:], in0=m1[:], in1=m2[:], op=ALU.add))
                    V(lambda e: e.max(out=g8[:], in_=gsm[:]))
                    V(lambda e: e.tensor_scalar(out=gmk[:], in0=gsm[:], scalar1=g8[:, 3:4], scalar2=None, op0=ALU.is_ge))
                    V(lambda e: e.scalar_tensor_tensor(out=v3(chm[:]), in0=v3(chs[:]), scalar=10.0, in1=b3(gmk[:]), op0=ALU.add, op1=ALU.mult))
                    V(lambda e: e.max(out=e8[:], in_=chm[:]))
                    V(lambda e: e.tensor_scalar(out=eq[:], in0=chm[:], scalar1=e8[:, 7:8], scalar2=None, op0=ALU.is_ge))
                    V(lambda e: e.tensor_tensor(out=eq[:], in0=eq[:], in1=scr[:], op=ALU.mult))
                    V(lambda e: e.tensor_reduce(out=ssum[:], in_=eq[:], axis=AX.X, op=ALU.add))
                    V(lambda e: e.reciprocal(out=ssum[:], in_=ssum[:]))
                    kb.op("vector", lambda e, tt=tt: e.tensor_scalar(out=gw[:, tt, 0:64], in0=eq[:], scalar1=ssum[:, 0:1], scalar2=2.5, op0=ALU.mult, op1=ALU.mult),
                          outs=[GW], ins=[RT])
                kb.barrier()
            if "gw" in dbg:
                dump("gw", gw[:], [128, 16, 65])
            with ExitStack() as p2e:
                wg = [sb(f"wg{i}", [128, 8, 512], BF16, p2e) for i in range(2)]
                wd = [sb(f"wd{i}", [128, 2, D], BF16, p2e) for i in range(2)]
                WG = [Buf(t[:]) for t in wg]
                WD = [Buf(t[:]) for t in wd]
                actT = [sb(f"actT{i}", [128, 2, SO], BF16, p2e) for i in range(2)]
                ACT_ = [[Buf(actT[i][:, :, nb * 512:(nb + 1) * 512]) for nb in range(4)] for i in range(2)]
                sgt = [sb(f"sgt{i}", [128, 256], F32, p2e) for i in range(2)]
                SGT = [Buf(t[:]) for t in sgt]
                att = [sb(f"att{i}", [128, 256], BF16, p2e) for i in range(2)]
                ATT = [Buf(t[:]) for t in att]
                NE = n_experts

                def load_w(e_):
                    sl_ = e_ % 2
                    gv = I["wgu"][e_].rearrange("(k p) c -> p k c", p=128)
                    kb.dma("gpsimd", wg[sl_][:, 0:4, :], gv[:, 0:4, :], outs=[WG[sl_]])
                    kb.dma("gpsimd", wg[sl_][:, 4:8, :], gv[:, 4:8, :], outs=[WG[sl_]])
                    kb.dma("gpsimd", wd[sl_][:], I["wdn"][e_].rearrange("(k p) c -> p k c", p=128), outs=[WD[sl_]])

                def load_wg(e_):
                    sl_ = e_ % 2
                    gv = I["wgu"][e_].rearrange("(k p) c -> p k c", p=128)
                    kb.dma("gpsimd", wg[sl_][:, 0:4, :], gv[:, 0:4, :], outs=[WG[sl_]])
                    kb.dma("gpsimd", wg[sl_][:, 4:8, :], gv[:, 4:8, :], outs=[WG[sl_]])

                def load_wd(e_):
                    sl_ = e_ % 2
                    kb.dma("gpsimd", wd[sl_][:], I["wdn"][e_].rearrange("(k p) c -> p k c", p=128), outs=[WD[sl_]])

                cnt = [0]
                pend = {}

                def GU(e_, tt):
                    sl_ = e_ % 2
                    bk = bank2()
                    for k in range(8):
                        kb.op("tensor", lambda e, k=k: e.matmul(ps[bk][:], lhsT=h2T[:, k, tt * 128:(tt + 1) * 128], rhs=wg[sl_][:, k, :], start=(k == 0), stop=(k == 7)),
                              outs=[PS[bk]], ins=[H2[tt // 4], WG[sl_]], mark=(k == 7))
                    cnt[0] += 1
                    i2 = cnt[0] % 2
                    kb.op("scalar", lambda e: e.activation(out=sgt[i2][:], in_=ps[bk][:, 0:256], func=AF.Silu), outs=[SGT[i2]], ins=[PS[bk]])
                    kb.op("vector", lambda e: e.scalar_tensor_tensor(out=att[i2][:], in0=ps[bk][:, 256:512], scalar=gw[:, tt, e_:e_ + 1], in1=sgt[i2][:],
                                                                     op0=ALU.mult, op1=ALU.mult), outs=[ATT[i2]], ins=[PS[bk], SGT[i2], GW])
                    pend[(e_, tt)] = i2

                def TR(e_, tt):
                    sl_ = e_ % 2
                    i2 = pend.pop((e_, tt))
                    for hc in range(2):
                        bt = bank2()
                        kb.op("tensor", lambda e, hc=hc, bt=bt: e.matmul(ps[bt][:, 0:128], lhsT=att[i2][:, hc * 128:(hc + 1) * 128], rhs=ident[:], start=True, stop=True),
                              outs=[PS[bt]], ins=[ATT[i2], CST])
                        kb.op("scalar", lambda e, hc=hc, bt=bt: e.copy(out=actT[sl_][:, hc, tt * 128:(tt + 1) * 128], in_=ps[bt][:, 0:128]),
                              outs=[ACT_[sl_][tt // 4]], ins=[PS[bt]])

                def DN(e_, nb, ms):
                    sl_ = e_ % 2
                    sl = slice(nb * 512, (nb + 1) * 512)
                    for m in ms:
                        bk = bank2()
                        for hc in range(2):
                            kb.op("tensor", lambda e, bk=bk, hc=hc, m=m: e.matmul(ps[bk][:], lhsT=wd[sl_][:, hc, m * 128:(m + 1) * 128], rhs=actT[sl_][:, hc, sl], start=(hc == 0), stop=(hc == 1)),
                                  outs=[PS[bk]], ins=[WD[sl_], ACT_[sl_][nb]], mark=(hc == 1))
                        kb.op("vector", lambda e, bk=bk, m=m: e.scalar_tensor_tensor(out=acc[:, m, sl], in0=ps[bk][:], scalar=g2c(m), in1=acc[:, m, sl], op0=ALU.mult, op1=ALU.add),
                              outs=[ACCB[nb]], ins=[PS[bk], ACCB[nb], MOD])

                load_wg(0)
                load_wd(0)
                for e_ in range(NE + 1):
                    if e_ + 1 < NE:
                        load_wg(e_ + 1)
                    for tt in range(16):
                        if e_ < NE:
                            GU(e_, tt)
                            if tt > 0:
                                TR(e_, tt - 1)
                        if e_ > 0:
                            DN(e_ - 1, tt // 4, (2 * (tt % 4), 2 * (tt % 4) + 1))
                    if e_ < NE:
                        TR(e_, 15)
                    if e_ + 1 < NE:
                        load_wd(e_ + 1)
                kb.barrier()
            if "x3T" in dbg:
                dump("x3T", acc[:], [128, 8, SO])
            sq2 = sb("sq2", [128, 8, 512], F32, p2)
            SQ2 = Buf(sq2[:])
            rstd2 = sb("rstd2", [128, 512], F32, p2)
            RS2 = Buf(rstd2[:])
            ot = [sb(f"ot{i}", [128, 8, 512], F32, p2) for i in range(2)]
            OTB = [Buf(t[:]) for t in ot]
            outT_v = outT.rearrange("(k p) t -> p k t", p=128)
            for nb in range(4):
                sl = slice(nb * 512, (nb + 1) * 512)
                kb.op("scalar", lambda e, sl=sl: e.activation(out=sq2[:], in_=acc[:, :, sl], func=AF.Square), outs=[SQ2], ins=[ACCB[nb]])
                bk = bank2()
                for k in range(8):
                    kb.op("tensor", lambda e, k=k, bk=bk: e.matmul(ps[bk][:], lhsT=onesf[:], rhs=sq2[:, k, :], start=(k == 0), stop=(k == 7)),
                          outs=[PS[bk]], ins=[SQ2, CST], mark=(k == 7))
                kb.op("scalar", lambda e, bk=bk: e.activation(out=rstd2[:], in_=ps[bk][:], func=AF.Sqrt, scale=1.0 / D, bias=EPS), outs=[RS2], ins=[PS[bk]])
                kb.op("vector", lambda e: e.reciprocal(out=rstd2[:], in_=rstd2[:]), outs=[RS2], ins=[RS2])
                o_ = ot[nb % 2]
                for k in range(8):
                    kb.op("vector", lambda e, k=k, o_=o_, sl=sl: e.scalar_tensor_tensor(out=o_[:, k, :], in0=acc[:, k, sl], scalar=fg[:, k:k + 1], in1=rstd2[:], op0=ALU.mult, op1=ALU.mult),
                          outs=[OTB[nb % 2]], ins=[ACCB[nb], FG, RS2])
                kb.dma("sync", outT_v[:, :, sl], o_[:], ins=[OTB[nb % 2]])
            kb.barrier()
        kb.barrier()
    return nc, dbg_out


def _prep_inputs(inp, core):
    b = core // 2
    half = core % 2
    f = lambda a: np.ascontiguousarray(a, dtype=np.float32)
    x = inp["x"][b]
    m = {}
    xT = f(x.T)
    m["xTo"] = f(xT[:, half * SO:(half + 1) * SO])
    if half == 0:
        xl = np.zeros((D, S), np.float32)
        xl[:, SO:] = xT[:, :SO]
        m["xT"] = xl
    else:
        m["xT"] = xT
    m["cT"] = f(inp["c"][b].reshape(8, 128).T)
    m["w_ada"] = f(inp["w_ada"][0])
    m["b_ada"] = f(inp["b_ada"][0].reshape(1, -1))
    m["n1g"] = f(inp["norm1_g"][0].reshape(8, 128).T)
    m["w_in"] = f(inp["w_in"][0])
    m["lbl"] = f(inp["hg_lb_logits"].reshape(2, 4, 128).transpose(2, 0, 1))
    m["hng"] = f(np.broadcast_to(inp["hg_norm_g"][0][None, :], (128, 512)))
    for s in ("k", "v"):
        m["peT" + s] = f(inp["cmp_pos_" + s][0].T)
        m["w1" + s] = f(inp["cmp_w1_" + s][0].reshape(32, 64, 256).transpose(1, 0, 2))
        m["b1" + s] = f(inp["cmp_b1_" + s][0].reshape(2, 128).T)
        m["w2" + s] = f(inp["cmp_w2_" + s][0].reshape(2, 128, 64).transpose(1, 0, 2))
    m["w_out"] = f(inp["w_out"][0])
    m["n2g"] = f(inp["norm2_g"][0].reshape(8, 128).T)
    m["rw"] = f(inp["router_w"][0].reshape(8, 128, 64).transpose(1, 0, 2))
    m["rbias"] = f(np.broadcast_to(inp["router_bias"][0][None, :], (128, 64)))
    m["fg"] = f(inp["final_g"].reshape(8, 128).T)
    return m


_SHARED = {}


def kernel(**inp):
    inp = {k: np.asarray(v) for k, v in inp.items()}
    nc, _ = build()
    wgu = np.ascontiguousarray(np.concatenate([inp["w_exp_gu"][0], inp["w_sh_gu"][0][None]], axis=0), dtype=np.float32)
    wdn = np.ascontiguousarray(np.concatenate([inp["w_exp_dn"][0], inp["w_sh_dn"][0][None]], axis=0), dtype=np.float32)
    in_maps = []
    for core in range(8):
        m = _prep_inputs(inp, core)
        m["wgu"] = wgu
        m["wdn"] = wdn
        m.update(_consts(core % 2))
        in_maps.append(m)
    res = run_bass_kernel_spmd(nc, in_maps, core_ids=list(range(8)))
    out = np.zeros((4, S, D), np.float32)
    for core in range(8):
        b, half = core // 2, core % 2
        out[b, half * SO:(half + 1) * SO, :] = res.results[core]["outT"].T
    return out
```

```python
import numpy as np
import os as _os0
import ml_dtypes
from contextlib import ExitStack
import concourse.bass as bass
import concourse.mybir as mybir
from concourse.bass_utils import run_bass_kernel_spmd

F32 = mybir.dt.float32
BF16 = mybir.dt.bfloat16
AF = mybir.ActivationFunctionType
ALU = mybir.AluOpType
AX = mybir.AxisListType

S = 4096
D = 1024
NT = 32
NB = 8
SO = 2048
EPS = 1e-6
NEG = -30000.0
NDS = 12
SEM_LIMIT = 2000
SAME_SYNC = not bool(int(_os0.environ.get("NOSAME", "0")))


class Buf:
    __slots__ = ("ap", "w", "r", "excl")

    def __init__(self, ap, excl=False):
        self.ap = ap
        self.w = None
        self.r = {}
        self.excl = excl

    def __getitem__(self, k):
        return self.ap[k]


class Eng:
    def __init__(self, name, h):
        self.name = name
        self.h = h
        self.sem = None
        self.count = 0
        self.epoch = 0
        self.waited = {}


class KB:
    def __init__(self, nc, es):
        self.nc = nc
        self.es = es
        self.engs = {n: Eng(n, getattr(nc, n)) for n in ("tensor", "vector", "scalar", "gpsimd", "sync")}
        for e in self.engs.values():
            self._new_sem(e)
        self.dsems = {q: [es.enter_context(nc.semaphore(f"d_{q}{i}")) for i in range(NDS)] for q in ("sync", "gpsimd")}
        self.dcnt = {q: [0] * NDS for q in ("sync", "gpsimd")}
        self.drr = {"sync": 0, "gpsimd": 0}
        self.nsem = 0

    def _new_sem(self, e):
        e.epoch += 1
        e.sem = self.es.enter_context(self.nc.semaphore(f"s_{e.name}_{e.epoch}"))
        e.count = 0

    def wait(self, eng, tk):
        key, sem, val = tk
        if eng.waited.get(key, 0) >= val:
            return
        eng.h.wait_ge(sem, val)
        eng.waited[key] = val

    def _deps(self, en, eng, outs, ins):
        need = {}

        def add(t):
            if t[3] == en and (en == "tensor" or not SAME_SYNC):
                return
            cur = need.get(t[0])
            if cur is None or cur[2] < t[2]:
                need[t[0]] = t

        for b in ins:
            if b.w is not None:
                add(b.w)
            if b.excl:
                for t in b.r.values():
                    if t[3] != en:
                        add(t)
        for b in outs:
            if b.w is not None:
                add(b.w)
            for t in b.r.values():
                add(t)
        for t in need.values():
            self.wait(eng, t[:3])

    def op(self, en, fn, outs=(), ins=(), mark=True):
        eng = self.engs[en]
        self._deps(en, eng, outs, ins)
        if eng.count >= SEM_LIMIT:
            self._new_sem(eng)
        inst = fn(eng.h)
        if mark:
            eng.count += 1
            inst.then_inc(eng.sem, 1)
            tk = ((en, eng.epoch), eng.sem, eng.count, en)
        else:
            tk = ((en, eng.epoch), eng.sem, eng.count + 1, en)
        for b in ins:
            b.r[tk[0]] = tk
        for b in outs:
            b.w = tk
            b.r = {}
        return tk

    def dma(self, q, out_ap, in_ap, outs=(), ins=()):
        eng = self.engs[q]
        i = self.drr[q]
        self.drr[q] = (i + 1) % NDS
        sem = self.dsems[q][i]
        key = ("d", q, i)
        if self.dcnt[q][i] > 0:
            self.wait(eng, (key, sem, self.dcnt[q][i]))
        self._deps("dma_" + q, eng, outs, ins)
        inst = eng.h.dma_start(out=out_ap, in_=in_ap)
        self.dcnt[q][i] += 16
        inst.then_inc(sem, 16)
        tk = (key, sem, self.dcnt[q][i], "dma_" + q)
        for b in ins:
            b.r[key] = tk
        for b in outs:
            b.w = tk
            b.r = {}
        return tk

    def barrier(self):
        for e in self.engs.values():
            for o in self.engs.values():
                if o is e or o.count == 0:
                    continue
                self.wait(e, ((o.name, o.epoch), o.sem, o.count))
            for q in ("sync", "gpsimd"):
                for i in range(NDS):
                    if self.dcnt[q][i] > 0:
                        self.wait(e, (("d", q, i), self.dsems[q][i], self.dcnt[q][i]))


def _consts(half):
    bf = ml_dtypes.bfloat16
    c = {}
    eye = np.eye(128, dtype=np.float32)
    c["ident"] = eye.astype(bf)
    c["onesf"] = np.ones((128, 128), np.float32)
    c["isel0"] = (eye * (1.0 if half == 0 else 0.0)).astype(bf)
    c["isel1"] = (eye * (1.0 if half == 1 else 0.0)).astype(bf)
    m = np.arange(128)
    sw = (m // 64) * 64 + ((m % 64) + 32) % 64
    ps = np.zeros((128, 128), np.float32)
    ps[sw, m] = 1.0
    c["pswap"] = ps.astype(bf)
    shift = 2048 if half == 0 else 0
    dd = np.arange(128) % 64
    i = dd % 32
    inv = 10000.0 ** (-(i.astype(np.float64)) / 32.0)
    tpos = (np.arange(S) - shift).astype(np.float64)
    ang = inv[:, None].astype(np.float32).astype(np.float64) * tpos[None, :]
    ang = ang.astype(np.float32).astype(np.float64)
    c["cosT"] = np.cos(ang).astype(np.float32)
    sg = np.where(dd < 32, -1.0, 1.0)[:, None]
    c["sinT"] = (np.sin(ang) * sg).astype(np.float32)
    vm = np.ones((128, 32), np.float32)
    if half == 0:
        vm[:, :16] = 0.0
    c["vmask"] = vm
    c["hmask"] = (m[:, None] <= m[None, :]).astype(np.float32).astype(bf)
    seg = np.ones((128, 512), np.float32)
    seg[:, ::128] = 0.0
    c["segm"] = seg
    r = np.arange(128)[:, None]
    qi = np.arange(512)[None, :]
    wb = np.zeros((8, 128, 512), np.float32)
    cb = np.zeros((4, 128, 512), np.float32)
    for j in range(8):
        kpos = -512 + 128 * j + r
        dlt = qi - kpos
        wb[j] = np.where((dlt >= 0) & (dlt < 512), 0.0, NEG)
    for j in range(4):
        kpos = 128 * j + r
        cb[j] = np.where(kpos <= qi, 0.0, NEG)
    c["wband"] = np.ascontiguousarray(wb.transpose(1, 0, 2)).astype(bf)
    c["causb"] = np.ascontiguousarray(cb.transpose(1, 0, 2)).astype(bf)
    wb4 = wb.copy()
    if half == 0:
        wb4[0:4] = NEG
    c["wband4"] = np.ascontiguousarray(wb4.transpose(1, 0, 2)).astype(bf)
    cm = np.zeros((8, 128, 512), np.float32)
    for qb in range(8):
        ct = 0 if qb < 4 else 1
        cc = 128 * ct + r
        qpos = 512 * qb + qi - shift
        tc = cc - shift // 16
        cm[qb] = np.where((16 * tc + 31 <= qpos) & (cc < 255) & (tc >= 0), 0.0, NEG)
    c["cmpb"] = np.ascontiguousarray(cm.transpose(1, 0, 2)).astype(bf)
    c["cmpb0"] = np.full((128, 512), NEG if half == 0 else 0.0, np.float32).astype(bf)
    ek = np.zeros((64, 32, 128), np.float32)
    for kt in range(32):
        ek[2 * kt, kt, :64] = 1.0
        ek[2 * kt + 1, kt, 64:] = 1.0
    c["ekt"] = np.concatenate([ek, ek], axis=0).astype(bf)
    add = np.zeros((128, 32, 64), np.float32)
    for qt in range(32):
        pos = 128 * qt + np.arange(128) - shift
        cur = pos // 64
        j = np.arange(64)[None, :] - shift // 64
        forced = (j == 0) | (j == cur[:, None]) | (j == cur[:, None] - 1)
        avail = (j <= cur[:, None]) & (j >= 0)
        add[:, qt, :] = np.where(avail & forced, 1e30, np.where(avail, 0.0, -1e30))
    c["seladd"] = add
    cs = np.arange(256)[:, None] * 16
    ss = np.arange(64)[None, :] * 64
    ov = np.clip(np.minimum(cs + 32, ss + 64) - np.maximum(cs, ss), 0, None).astype(np.float32) / 32.0
    ov[255] = 0.0
    c["ovl"] = np.ascontiguousarray(ov.reshape(2, 128, 64).transpose(1, 0, 2)).astype(bf)
    return c


CONST_SHAPES = {
    "ident": ([128, 128], BF16), "onesf": ([128, 128], F32), "isel0": ([128, 128], BF16), "isel1": ([128, 128], BF16),
    "pswap": ([128, 128], BF16), "cosT": ([128, S], F32), "sinT": ([128, S], F32), "hmask": ([128, 128], BF16),
    "segm": ([128, 512], F32), "wband": ([128, 8, 512], BF16), "causb": ([128, 4, 512], BF16),
    "cmpb": ([128, 8, 512], BF16), "ekt": ([128, 32, 128], BF16), "seladd": ([128, 32, 64], F32),
    "ovl": ([128, 2, 64], BF16), "vmask": ([128, 32], F32), "wband4": ([128, 8, 512], BF16), "cmpb0": ([128, 512], BF16),
}

IN_SHAPES = {
    "xT": [D, S], "xTo": [D, SO], "cT": [128, 8], "w_ada": [D, 6 * D], "b_ada": [1, 6 * D], "n1g": [128, 8],
    "w_in": [D, 3352], "lbl": [128, 2, 4], "hng": [128, 512],
    "peTk": [64, 32], "w1k": [64, 32, 256], "b1k": [128, 2], "w2k": [128, 2, 64],
    "peTv": [64, 32], "w1v": [64, 32, 256], "b1v": [128, 2], "w2v": [128, 2, 64],
    "w_out": [D, D], "n2g": [128, 8], "rw": [128, 8, 64], "rbias": [128, 64],
    "wgu": [65, D, 512], "wdn": [65, 256, D], "fg": [128, 8],
}


class _SkipNSA(Exception):
    pass


class _NSAScope(ExitStack):
    def __exit__(self, et, ev, tb):
        super().__exit__(None, None, None)
        return et is _SkipNSA


def build(stop_after=None, dbg=(), with_moe=True, enable_nsa=True, n_experts=65):
    nc = bass.Bass("TRN2", target_bir_lowering=False)
    I = {}
    for k, shp in IN_SHAPES.items():
        if not with_moe and k in ("wgu", "wdn"):
            continue
        I[k] = nc.dram_tensor(k, list(shp), F32, kind="ExternalInput").ap()
    for k, (shp, dt) in CONST_SHAPES.items():
        I[k] = nc.dram_tensor(k, list(shp), dt, kind="ExternalInput").ap()
    outT = nc.dram_tensor("outT", [D, SO], F32, kind="ExternalOutput").ap()
    dbg_out = {}
    with ExitStack() as es:
        kb = KB(nc, es)
        E = es.enter_context

        uid = [0]

        def sb(name, shape, dt=F32, stack=None):
            uid[0] += 1
            return (stack or es).enter_context(nc.sbuf_tensor(f"sb{uid[0]}_" + name, list(shape), dt))

        ps = [E(nc.psum_tensor(f"ps{i}", [128, 512], F32)) for i in range(8)]
        PS = [Buf(p[:], excl=True) for p in ps]

        def dump(name, ap, shape, dt=F32):
            t = nc.dram_tensor("dbg_" + name, list(shape), dt, kind="ExternalOutput").ap()
            dbg_out[name] = t
            kb.barrier()
            kb.dma("sync", t, ap)
            kb.barrier()

        ident = sb("ident", [128, 128], BF16)
        onesf = sb("onesf", [128, 128], F32)
        isel0 = sb("isel0", [128, 128], BF16)
        isel1 = sb("isel1", [128, 128], BF16)
        pswap = sb("pswap", [128, 128], BF16)
        hmask = sb("hmask", [128, 128], BF16)
        segm = sb("segm", [128, 512], F32)
        CST = Buf(ident[:])
        for nm, t in (("ident", ident), ("onesf", onesf), ("isel0", isel0), ("isel1", isel1), ("pswap", pswap),
                      ("hmask", hmask), ("segm", segm)):
            kb.dma("sync", t[:], I[nm], outs=[CST])
        modcol = sb("modcol", [128, 48], F32)
        a1 = sb("a1", [128, 8], F32)
        a2 = sb("a2", [128, 8], F32)
        MOD = Buf(modcol[:])
        oT = sb("oT", [128, 8, SO], BF16)
        OT = [[Buf(oT[:, j, s * 128:(s + 1) * 128]) for s in range(16)] for j in range(8)]

        with ExitStack() as p0:
            cT = sb("cT", [128, 8], F32, p0)
            cs = sb("cs", [128, 8], F32, p0)
            bada = sb("bada", [1, 6 * D], F32, p0)
            modrow = sb("modrow", [1, 6 * D], F32, p0)
            one1 = sb("one1", [1, 1], F32, p0)
            n1g = sb("n1g", [128, 8], F32, p0)
            n2g = sb("n2g", [128, 8], F32, p0)
            wab = [sb(f"wab{i}", [128, 8, 512], F32, p0) for i in range(2)]
            WAB = [Buf(w[:]) for w in wab]
            SM = Buf(cT[:])
            MR = Buf(modrow[:])
            kb.dma("sync", cT[:], I["cT"], outs=[SM])
            kb.dma("sync", bada[:], I["b_ada"], outs=[SM])
            kb.dma("sync", n1g[:], I["n1g"], outs=[SM])
            kb.dma("sync", n2g[:], I["n2g"], outs=[SM])
            kb.op("vector", lambda e: e.memset(one1[:], 1.0), outs=[SM])
            kb.op("scalar", lambda e: e.activation(out=cs[:], in_=cT[:], func=AF.Silu), outs=[SM], ins=[SM])
            wada_v = I["w_ada"].rearrange("(k p) c -> p k c", p=128)
            for cb in range(12):
                W = WAB[cb % 2]
                kb.dma("sync" if cb % 2 == 0 else "gpsimd", wab[cb % 2][:], wada_v[:, :, cb * 512:(cb + 1) * 512], outs=[W])
                P = PS[cb % 2]
                for k in range(8):
                    kb.op("tensor", lambda e, k=k, cb=cb: e.matmul(ps[cb % 2][0:1, :], lhsT=cs[:, k:k + 1], rhs=wab[cb % 2][:, k, :],
                                                                 start=(k == 0), stop=(k == 7)),
                          outs=[P], ins=[SM, W], mark=(k == 7))
                kb.op("vector", lambda e, cb=cb: e.tensor_tensor(out=modrow[0:1, cb * 512:(cb + 1) * 512], in0=ps[cb % 2][0:1, :],
                                                                  in1=bada[0:1, cb * 512:(cb + 1) * 512], op=ALU.add),
                      outs=[MR], ins=[P, SM])
            P = PS[2]
            for j in range(48):
                kb.op("tensor", lambda e, j=j: e.matmul(ps[2][:, j:j + 1], lhsT=modrow[0:1, j * 128:(j + 1) * 128], rhs=one1[0:1, 0:1],
                                                       start=True, stop=True), outs=[P], ins=[MR, SM], mark=(j == 47))
            kb.op("vector", lambda e: e.tensor_copy(out=modcol[:], in_=ps[2][:, 0:48]), outs=[MOD], ins=[P])
            kb.op("vector", lambda e: e.scalar_tensor_tensor(out=a1[:], in0=modcol[:, 8:16], scalar=1.0, in1=n1g[:], op0=ALU.add, op1=ALU.mult),
                  outs=[MOD], ins=[MOD, SM])
            kb.op("vector", lambda e: e.scalar_tensor_tensor(out=a2[:], in0=modcol[:, 32:40], scalar=1.0, in1=n2g[:], op0=ALU.add, op1=ALU.mult),
                  outs=[MOD], ins=[MOD, SM])
            if "mod" in dbg:
                dump("mod", modcol[:], [128, 48])
            kb.barrier()
        sh1 = lambda k: modcol[:, k:k + 1]
        g1c = lambda k: modcol[:, 16 + k:17 + k]
        sh2 = lambda k: modcol[:, 24 + k:25 + k]
        g2c = lambda k: modcol[:, 40 + k:41 + k]

        if stop_after == "p0":
            kb.barrier()
            return nc, dbg_out

        with ExitStack() as p1:
            hT = sb("hT", [128, 8, S], BF16, p1)
            HT = [Buf(hT[:, :, n * 512:(n + 1) * 512]) for n in range(NB)]
            with ExitStack() as p1a:
                xb = [sb(f"xb{i}", [128, 8, 512], F32, p1a) for i in range(2)]
                XB = [Buf(t[:]) for t in xb]
                sq = sb("sq", [128, 8, 512], F32, p1a)
                SQ = Buf(sq[:])
                rstd = sb("rstd", [128, 512], F32, p1a)
                RS = Buf(rstd[:])
                tmp = [sb(f"tmp{i}", [128, 512], F32, p1a) for i in range(2)]
                TMP = [Buf(t[:]) for t in tmp]
                xT_v = I["xT"].rearrange("(k p) t -> p k t", p=128)
                for n in range(NB):
                    X = XB[n % 2]
                    x_ = xb[n % 2]
                    kb.dma("sync" if n % 2 == 0 else "gpsimd", x_[:], xT_v[:, :, n * 512:(n + 1) * 512], outs=[X])
                    kb.op("scalar", lambda e, x_=x_: e.activation(out=sq[:], in_=x_[:], func=AF.Square), outs=[SQ], ins=[X])
                    P = PS[n % 2]
                    for k in range(8):
                        kb.op("tensor", lambda e, k=k, n=n: e.matmul(ps[n % 2][:], lhsT=onesf[:], rhs=sq[:, k, :], start=(k == 0), stop=(k == 7)),
                              outs=[P], ins=[SQ, CST], mark=(k == 7))
                    kb.op("scalar", lambda e, n=n: e.activation(out=rstd[:], in_=ps[n % 2][:], func=AF.Sqrt, scale=1.0 / D, bias=EPS),
                          outs=[RS], ins=[P])
                    kb.op("vector", lambda e: e.reciprocal(out=rstd[:], in_=rstd[:]), outs=[RS], ins=[RS])
                    for k in range(8):
                        T = TMP[k % 2]
                        t_ = tmp[k % 2]
                        kb.op("vector", lambda e, k=k, t_=t_, x_=x_: e.tensor_tensor(out=t_[:], in0=x_[:, k, :], in1=rstd[:], op=ALU.mult),
                              outs=[T], ins=[X, RS])
                        kb.op("scalar", lambda e, k=k, t_=t_, n=n: e.activation(out=hT[:, k, n * 512:(n + 1) * 512], in_=t_[:], func=AF.Identity,
                                                                            scale=a1[:, k:k + 1], bias=sh1(k)),
                              outs=[HT[n]], ins=[T, MOD])
                kb.barrier()
            if "hT" in dbg:
                dump("hT", hT[:], [128, 8, S], BF16)
            if stop_after == "p1a":
                kb.barrier()
                return nc, dbg_out

            rr = [0]

            def bank():
                rr[0] = (rr[0] + 1) % 8
                return rr[0]

            w_in_v = I["w_in"].rearrange("(k p) c -> p k c", p=128)

            with ExitStack() as ph:
                lbl = sb("lbl", [128, 2, 4], F32, ph)
                lb = sb("lb", [128, 4], F32, ph)
                oml = sb("oml", [128, 4], F32, ph)
                hng = sb("hng", [128, 512], F32, ph)
                HC = Buf(lbl[:])
                kb.dma("sync", lbl[:], I["lbl"], outs=[HC])
                kb.dma("sync", hng[:], I["hng"], outs=[HC])
                kb.op("vector", lambda e: e.tensor_tensor(out=lb[:], in0=lbl[:, 0, :], in1=lbl[:, 1, :], op=ALU.subtract), outs=[HC], ins=[HC])
                kb.op("scalar", lambda e: e.activation(out=lb[:], in_=lb[:], func=AF.Sigmoid), outs=[HC], ins=[HC])
                kb.op("vector", lambda e: e.tensor_scalar(out=oml[:], in0=lb[:], scalar1=-1.0, scalar2=1.0, op0=ALU.mult, op1=ALU.add), outs=[HC], ins=[HC])
                wq = sb("wq", [128, 8, 128], BF16, ph)
                wf = sb("wf", [128, 8, 128], BF16, ph)
                wig = sb("wig", [128, 8, 256], BF16, ph)
                WQ, WF, WIG = Buf(wq[:]), Buf(wf[:]), Buf(wig[:])
                Q1 = sb("Q1", [128, S], BF16, ph)
                Q2 = sb("Q2", [128, S], BF16, ph)
                Kt = sb("Kt", [128, S], BF16, ph)
                Kh = sb("Kh", [128, NT, 128], BF16, ph)
                Vh = sb("Vh", [128, NT, 128], BF16, ph)
                SGt = sb("SGt", [128, NT, 128], BF16, ph)
                ebl = sb("ebl", [128, NT], F32, ph)
                BQ = [Buf(Q1[:, n * 512:(n + 1) * 512]) for n in range(NB)]
                BKH = [Buf(Kh[:, t, :]) for t in range(NT)]
                BV = [Buf(Vh[:, t, :]) for t in range(NT)]
                tn = ["f", "lf", "b", "d1", "d2", "eb", "e1", "en1", "el", "k"]
                T2 = [{n_: sb(f"t{i}_" + n_, [128, 512], F32, ph) for n_ in tn} for i in range(2)]
                TB2 = [{n_: Buf(T2[i][n_][:]) for n_ in tn} for i in range(2)]
                khtb2 = [sb(f"khtb{i}", [128, 512], BF16, ph) for i in range(2)]
                KHTB2 = [Buf(t[:]) for t in khtb2]
                vmask = sb("vmask", [128, 32], F32, ph)
                kb.dma("sync", vmask[:], I["vmask"], outs=[HC])
                Sst = sb("Sst", [128, 128], F32, ph)
                SST = Buf(Sst[:])
                sbf = [sb(f"sbf{i}", [128, 128], BF16, ph) for i in range(2)]
                SBF = [Buf(t[:]) for t in sbf]
                atm = [sb(f"atm{i}", [128, 128], BF16, ph) for i in range(2)]
                ATM = [Buf(t[:]) for t in atm]
                for i in range(2):
                    kb.op("vector", lambda e, i=i: e.memset(atm[i][:], 0.0), outs=[ATM[i]])
                junk = sb("junk", [128, 128], F32, ph)
                JK = Buf(junk[:])
                ssq = [sb(f"ssq{i}", [128, 1], F32, ph) for i in range(2)]
                SSQ = [Buf(t[:]) for t in ssq]
                of = [sb(f"of{i}", [128, 128], F32, ph) for i in range(2)]
                OF = [Buf(t[:]) for t in of]
                obf = [sb(f"obf{i}", [128, 128], BF16, ph) for i in range(2)]
                OBF = [Buf(t[:]) for t in obf]
                v4 = lambda ap: ap.rearrange("p (c t) -> p c t", t=128)
                for hd in range(int(_os0.environ.get("NHEADS", "4"))):
                    c0 = hd * 128
                    kb.dma("gpsimd", wq[:], w_in_v[:, :, c0:c0 + 128], outs=[WQ])
                    kb.dma("gpsimd", wf[:], w_in_v[:, :, 512 + c0:512 + c0 + 128], outs=[WF])
                    kb.dma("gpsimd", wig[:, :, 0:128], w_in_v[:, :, 1024 + c0:1024 + c0 + 128], outs=[WIG])
                    kb.dma("gpsimd", wig[:, :, 128:256], w_in_v[:, :, 1536 + c0:1536 + c0 + 128], outs=[WIG])
                    for n in range(NB):
                        sl = slice(n * 512, (n + 1) * 512)
                        own = n >= 4
                        bq_, bf_ = bank(), bank()
                        if own:
                            for k in range(8):
                                kb.op("tensor", lambda e, k=k, bq_=bq_, sl=sl: e.matmul(ps[bq_][:], lhsT=wq[:, k, :], rhs=hT[:, k, sl], start=(k == 0), stop=(k == 7)),
                                      outs=[PS[bq_]], ins=[WQ, HT[n]], mark=(k == 7))
                        for k in range(8):
                            kb.op("tensor", lambda e, k=k, bf_=bf_, sl=sl: e.matmul(ps[bf_][:], lhsT=wf[:, k, :], rhs=hT[:, k, sl], start=(k == 0), stop=(k == 7)),
                                  outs=[PS[bf_]], ins=[WF, HT[n]], mark=(k == 7))
                        t = T2[n % 2]
                        TB = TB2[n % 2]
                        khtb = khtb2[n % 2]
                        KHTB = KHTB2[n % 2]
                        kb.op("scalar", lambda e, bf_=bf_: e.activation(out=t["f"][:], in_=ps[bf_][:], func=AF.Sigmoid), outs=[TB["f"]], ins=[PS[bf_]])
                        kb.op("vector", lambda e, hd=hd: e.tensor_scalar(out=t["f"][:], in0=t["f"][:], scalar1=oml[:, hd:hd + 1], scalar2=lb[:, hd:hd + 1],
                                                                     op0=ALU.mult, op1=ALU.add), outs=[TB["f"]], ins=[TB["f"], HC])
                        kb.op("scalar", lambda e: e.activation(out=t["lf"][:], in_=t["f"][:], func=AF.Ln), outs=[TB["lf"]], ins=[TB["f"]])
                        kb.op("gpsimd", lambda e: e.tensor_scalar(out=t["k"][:], in0=t["f"][:], scalar1=-1.0, scalar2=1.0, op0=ALU.mult, op1=ALU.add),
                              outs=[TB["k"]], ins=[TB["f"]])
                        kb.op("vector", lambda e: e.tensor_tensor_scan(out=t["b"][:], data0=segm[:], data1=t["lf"][:], initial=0.0, op0=ALU.mult, op1=ALU.add),
                              outs=[TB["b"]], ins=[TB["lf"], CST])
                        if own:
                            kb.op("vector", lambda e: e.tensor_tensor(out=v4(t["d1"][:]), in0=v4(t["b"][:]), in1=v4(t["b"][:])[:, :, 63:64].to_broadcast([128, 4, 128]),
                                                                      op=ALU.subtract), outs=[TB["d1"]], ins=[TB["b"]])
                        kb.op("vector", lambda e: e.tensor_tensor(out=v4(t["d2"][:]), in0=v4(t["b"][:])[:, :, 127:128].to_broadcast([128, 4, 128]), in1=v4(t["b"][:]),
                                                                  op=ALU.subtract), outs=[TB["d2"]], ins=[TB["b"]])
                        kb.op("scalar", lambda e: e.activation(out=t["eb"][:], in_=t["b"][:], func=AF.Exp), outs=[TB["eb"]], ins=[TB["b"]])
                        if own:
                            kb.op("scalar", lambda e: e.activation(out=t["e1"][:], in_=t["d1"][:], func=AF.Exp), outs=[TB["e1"]], ins=[TB["d1"]])
                            kb.op("scalar", lambda e: e.activation(out=t["en1"][:], in_=t["d1"][:], func=AF.Exp, scale=-1.0), outs=[TB["en1"]], ins=[TB["d1"]])
                        kb.op("scalar", lambda e: e.activation(out=t["el"][:], in_=t["d2"][:], func=AF.Exp), outs=[TB["el"]], ins=[TB["d2"]])
                        sc_ = 128.0 ** -0.5
                        if own:
                            kb.op("vector", lambda e, bq_=bq_, sl=sl: e.scalar_tensor_tensor(out=Q1[:, sl], in0=ps[bq_][:], scalar=sc_, in1=t["e1"][:], op0=ALU.mult, op1=ALU.mult),
                                  outs=[BQ[n]], ins=[PS[bq_], TB["e1"]])
                            kb.op("vector", lambda e, bq_=bq_, sl=sl: e.scalar_tensor_tensor(out=Q2[:, sl], in0=ps[bq_][:], scalar=sc_, in1=t["eb"][:], op0=ALU.mult, op1=ALU.mult),
                                  outs=[BQ[n]], ins=[PS[bq_], TB["eb"]])
                            kb.op("gpsimd", lambda e, sl=sl: e.tensor_tensor(out=Kt[:, sl], in0=t["k"][:], in1=t["en1"][:], op=ALU.mult), outs=[BQ[n]], ins=[TB["k"], TB["en1"]])
                        kb.op("gpsimd", lambda e: e.tensor_tensor(out=khtb[:], in0=t["k"][:], in1=t["el"][:], op=ALU.mult), outs=[KHTB], ins=[TB["k"], TB["el"]])
                        kb.op("gpsimd", lambda e, n=n: e.tensor_copy(out=ebl[:, 4 * n:4 * n + 4], in_=v4(t["eb"][:])[:, :, 127]), outs=[BQ[n]], ins=[TB["eb"]])
                        for i in range(4):
                            bk = bank()
                            kb.op("tensor", lambda e, i=i, bk=bk: e.matmul(ps[bk][:, 0:128], lhsT=khtb[:, i * 128:(i + 1) * 128], rhs=ident[:], start=True, stop=True),
                                  outs=[PS[bk]], ins=[KHTB, CST])
                            kb.op("scalar", lambda e, i=i, bk=bk, n=n: e.copy(out=Kh[:, 4 * n + i, :], in_=ps[bk][:, 0:128]), outs=[BKH[4 * n + i]], ins=[PS[bk]])
                    for tt in range(NT):
                        bk = bank()
                        n = tt // 4
                        for k in range(8):
                            kb.op("tensor", lambda e, k=k, bk=bk, tt=tt: e.matmul(ps[bk][:, 0:256], lhsT=hT[:, k, tt * 128:(tt + 1) * 128], rhs=wig[:, k, :],
                                                                                 start=(k == 0), stop=(k == 7)),
                                  outs=[PS[bk]], ins=[WIG, HT[n]], mark=(k == 7))
                        kb.op("vector", lambda e, bk=bk, tt=tt: e.tensor_scalar(out=Vh[:, tt, :], in0=ps[bk][:, 0:128], scalar1=vmask[:, tt:tt + 1], scalar2=None, op0=ALU.mult),
                              outs=[BV[tt]], ins=[PS[bk], HC])
                        kb.op("scalar", lambda e, bk=bk, tt=tt: e.activation(out=SGt[:, tt, :], in_=ps[bk][:, 128:256], func=AF.Silu), outs=[BV[tt]], ins=[PS[bk]])
                    kb.op("vector", lambda e: e.memset(Sst[:], 0.0), outs=[SST])
                    at_bank = {}

                    def emit_at(c):
                        bk = bank()
                        at_bank[c] = bk
                        cs_ = slice(c * 128, (c + 1) * 128)
                        c0_ = c * 128
                        kb.op("tensor", lambda e: e.matmul(ps[bk][0:64, 0:64], lhsT=Kt[:, c0_:c0_ + 64], rhs=Q1[:, c0_:c0_ + 64], start=True, stop=True),
                              outs=[PS[bk]], ins=[BQ[c // 4]], mark=False)
                        kb.op("tensor", lambda e: e.matmul(ps[bk][:, 64:128], lhsT=Kt[:, cs_], rhs=Q1[:, c0_ + 64:c0_ + 128], start=True, stop=True),
                              outs=[PS[bk]], ins=[BQ[c // 4]])
                        kb.op("vector", lambda e: e.tensor_tensor(out=atm[c % 2][0:64, 0:64], in0=ps[bk][0:64, 0:64], in1=hmask[0:64, 0:64], op=ALU.mult),
                              outs=[ATM[c % 2]], ins=[PS[bk], CST])
                        kb.op("vector", lambda e: e.tensor_tensor(out=atm[c % 2][:, 64:128], in0=ps[bk][:, 64:128], in1=hmask[:, 64:128], op=ALU.mult),
                              outs=[ATM[c % 2]], ins=[PS[bk], CST])

                    for c in range(NT):
                        if c + 1 < NT and c + 1 >= 16:
                            emit_at(c + 1)
                        cs_ = slice(c * 128, (c + 1) * 128)
                        bd = bank()
                        kb.op("tensor", lambda e, bd=bd, c=c: e.matmul(ps[bd][:, 0:128], lhsT=Kh[:, c, :], rhs=Vh[:, c, :], start=True, stop=True),
                              outs=[PS[bd]], ins=[BKH[c], BV[c]])
                        if c >= 16:
                            bo = bank()
                            kb.op("tensor", lambda e, bo=bo, c=c: e.matmul(ps[bo][:, 0:128], lhsT=atm[c % 2][:], rhs=Vh[:, c, :], start=True, stop=False),
                                  outs=[PS[bo]], ins=[ATM[c % 2], BV[c]], mark=False)
                            kb.op("tensor", lambda e, bo=bo, c=c, cs_=cs_: e.matmul(ps[bo][:, 0:128], lhsT=Q2[:, cs_], rhs=sbf[(c - 1) % 2][:], start=False, stop=True),
                                  outs=[PS[bo]], ins=[BQ[c // 4], SBF[(c - 1) % 2]])
                        if c + 1 < NT:
                            kb.op("vector", lambda e, bd=bd, c=c: e.scalar_tensor_tensor(out=Sst[:], in0=Sst[:], scalar=ebl[:, c:c + 1], in1=ps[bd][:, 0:128],
                                                                                     op0=ALU.mult, op1=ALU.add), outs=[SST], ins=[SST, PS[bd], BQ[c // 4]])
                            if c >= 15:
                                kb.op("scalar", lambda e, c=c: e.copy(out=sbf[c % 2][:], in_=Sst[:]), outs=[SBF[c % 2]], ins=[SST])
                        if c < 16:
                            continue
                        i2 = c % 2
                        kb.op("gpsimd", lambda e, i2=i2: e.memset(ssq[i2][:], 0.0), outs=[SSQ[i2]])
                        kb.op("scalar", lambda e, bo=bo, i2=i2: e.activation(out=junk[:], in_=ps[bo][:, 0:128], func=AF.Square, accum_out=ssq[i2][:]),
                              outs=[JK, SSQ[i2]], ins=[PS[bo]])
                        kb.op("scalar", lambda e, i2=i2: e.activation(out=ssq[i2][:], in_=ssq[i2][:], func=AF.Sqrt, scale=1.0 / 128, bias=EPS), outs=[SSQ[i2]], ins=[SSQ[i2]])
                        kb.op("vector", lambda e, i2=i2: e.reciprocal(out=ssq[i2][:], in_=ssq[i2][:]), outs=[SSQ[i2]], ins=[SSQ[i2]])
                        kb.op("vector", lambda e, bo=bo, i2=i2, c0=c0: e.scalar_tensor_tensor(out=of[i2][:], in0=ps[bo][:, 0:128], scalar=ssq[i2][:, 0:1], in1=hng[:, c0:c0 + 128],
                                                                                           op0=ALU.mult, op1=ALU.mult), outs=[OF[i2]], ins=[PS[bo], SSQ[i2], HC])
                        kb.op("gpsimd", lambda e, i2=i2, c=c: e.tensor_tensor(out=obf[i2][:], in0=of[i2][:], in1=SGt[:, c, :], op=ALU.mult), outs=[OBF[i2]], ins=[OF[i2], BV[c]])
                        bt = bank()
                        kb.op("tensor", lambda e, bt=bt, i2=i2: e.matmul(ps[bt][:, 0:128], lhsT=obf[i2][:], rhs=ident[:], start=True, stop=True),
                              outs=[PS[bt]], ins=[OBF[i2], CST])
                        s_ = c - 16
                        kb.op("scalar", lambda e, bt=bt, s_=s_, hd=hd: e.copy(out=oT[:, hd, s_ * 128:(s_ + 1) * 128], in_=ps[bt][:, 0:128]), outs=[OT[hd][s_]], ins=[PS[bt]])
                kb.barrier()
            if "oT" in dbg:
                dump("oT", oT[:], [128, 8, SO], BF16)
            if stop_after == "p1b":
                kb.barrier()
                return nc, dbg_out

            SCL = 64.0 ** -0.5
            if not enable_nsa:
                for jf in range(4, 8):
                    kb.op("vector", lambda e, jf=jf: e.memset(oT[:, jf, :], 0.0), outs=OT[jf])
            with _NSAScope() as pn:
                if not enable_nsa:
                    raise _SkipNSA()
                ovl = sb("ovl", [128, 2, 64], BF16, pn)
                kb.dma("sync", ovl[:], I["ovl"], outs=[CST])
                KEe = sb("KEe", [128, S], BF16, pn)
                KEo = sb("KEo", [128, S], BF16, pn)
                ekt_v = I["ekt"].rearrange("p a b -> p (a b)")
                kb.dma("sync", KEe[64:128, :], ekt_v[64:128, :], outs=[CST])
                kb.dma("sync", KEo[0:64, :], ekt_v[0:64, :], outs=[CST])
                kwT = sb("kwT", [128, S], BF16, pn)
                kcvT = sb("kcvT", [128, S], BF16, pn)
                vs1 = sb("vs1", [128, NT, 80], BF16, pn)
                vw1 = sb("vw1", [128, NT, 80], BF16, pn)
                KS = Buf(kwT[:])
                kcmpT = sb("kcmpT", [128, 256], BF16, pn)
                vcmp1 = sb("vcmp1", [128, 2, 144], BF16, pn)
                KC = Buf(kcmpT[:])
                wk3 = sb("wk3", [128, 8, 384], BF16, pn)
                wv2 = sb("wv2", [128, 8, 128], BF16, pn)
                wqg = sb("wqg", [128, 8, 256], BF16, pn)
                wgt = sb("wgt", [128, 8, 12], BF16, pn)
                WN = Buf(wk3[:])
                cosb = sb("cosb", [128, 512], F32, pn)
                sinb = sb("sinb", [128, 512], F32, pn)
                CSB = Buf(cosb[:])
                rawb = sb("rawb", [128, 512], BF16, pn)
                RAWB = Buf(rawb[:])
                rt1 = sb("rt1", [128, 512], F32, pn)
                rt2 = sb("rt2", [128, 512], F32, pn)
                RT1, RT2 = Buf(rt1[:]), Buf(rt2[:])
                _padn = int(_os0.environ.get("PADN", "0"))
                if _padn:
                    _pad = sb("padn", [128, _padn], F32, pn)
                srr = [0]

                def sbank():
                    srr[0] = (srr[0] + 1) % 3
                    return srr[0]

                mrr = [0]

                def mbank():
                    return 7

                import os as _os
                _dbgmode = int(_os.environ.get("ROPEDBG", "0"))

                def rope_from(bk, dst_ap, dstbuf):
                    if _dbgmode == 1:
                        kb.op("scalar", lambda e: e.copy(out=dst_ap, in_=ps[bk][:]), outs=[dstbuf], ins=[PS[bk]])
                        return
                    if _dbgmode == 3:
                        kb.op("vector", lambda e: e.tensor_tensor(out=rt1[:], in0=ps[bk][:], in1=cosb[:], op=ALU.mult), outs=[RT1], ins=[PS[bk], CSB])
                        kb.op("gpsimd", lambda e: e.tensor_copy(out=dst_ap, in_=rt1[:]), outs=[dstbuf], ins=[RT1])
                        return
                    if _dbgmode == 4:
                        kb.op("scalar", lambda e: e.copy(out=rawb[:], in_=ps[bk][:]), outs=[RAWB], ins=[PS[bk]])
                        b2 = mbank()
                        kb.op("tensor", lambda e: e.matmul(ps[b2][:], lhsT=pswap[:], rhs=rawb[:], start=True, stop=True), outs=[PS[b2]], ins=[RAWB, CST])
                        kb.op("vector", lambda e: e.tensor_tensor(out=rt1[:], in0=ps[bk][:], in1=cosb[:], op=ALU.mult), outs=[RT1], ins=[PS[bk], CSB])
                        kb.op("vector", lambda e: e.tensor_tensor(out=rt2[:], in0=ps[b2][:], in1=sinb[:], op=ALU.mult), outs=[RT2], ins=[PS[b2], CSB])
                        kb.op("vector", lambda e: e.tensor_tensor(out=dst_ap, in0=rt1[:], in1=rt2[:], op=ALU.add), outs=[dstbuf], ins=[RT1, RT2])
                        return
                    if _dbgmode == 5:
                        kb.op("scalar", lambda e: e.copy(out=rawb[:], in_=ps[bk][:]), outs=[RAWB], ins=[PS[bk]])
                        b2 = mbank()
                        kb.op("tensor", lambda e: e.matmul(ps[b2][:], lhsT=pswap[:], rhs=rawb[:], start=True, stop=True), outs=[PS[b2]], ins=[RAWB, CST])
                        kb.op("vector", lambda e: e.tensor_tensor(out=rt1[:], in0=ps[bk][:], in1=cosb[:], op=ALU.mult), outs=[RT1], ins=[PS[bk], CSB])
                        kb.op("scalar", lambda e: e.copy(out=rt2[:], in_=ps[b2][:]), outs=[RT2], ins=[PS[b2]])
                        _sb = cosb if _os.environ.get("USECOS") else sinb
                        kb.op("vector", lambda e: e.tensor_tensor(out=rt2[:], in0=rt2[:], in1=_sb[:], op=ALU.mult), outs=[RT2], ins=[RT2, CSB])
                        kb.op("vector", lambda e: e.tensor_tensor(out=dst_ap, in0=rt1[:], in1=rt2[:], op=ALU.add), outs=[dstbuf], ins=[RT1, RT2])
                        return
                    if _dbgmode in (7, 8):
                        kb.op("scalar", lambda e: e.copy(out=rawb[:], in_=ps[bk][:]), outs=[RAWB], ins=[PS[bk]])
                        b2 = mbank()
                        kb.op("tensor", lambda e: e.matmul(ps[b2][:], lhsT=pswap[:], rhs=rawb[:], start=True, stop=True), outs=[PS[b2]], ins=[RAWB, CST])
                        kb.op("vector", lambda e: e.tensor_tensor(out=rt1[:], in0=ps[bk][:], in1=cosb[:], op=ALU.mult), outs=[RT1], ins=[PS[bk], CSB])
                        kb.op("scalar", lambda e: e.copy(out=rt2[:], in_=ps[b2][:]), outs=[RT2], ins=[PS[b2]])
                        kb.op("vector", lambda e: e.tensor_tensor(out=rt2[:], in0=rt2[:], in1=sinb[:], op=ALU.mult), outs=[RT2], ins=[RT2, CSB])
                        if _dbgmode == 8:
                            kb.op("vector", lambda e: e.tensor_tensor(out=rt1[:], in0=rt1[:], in1=rt2[:], op=ALU.add), outs=[RT1], ins=[RT1, RT2])
                        kb.op("scalar", lambda e: e.copy(out=dst_ap, in_=rt1[:]), outs=[dstbuf], ins=[RT1])
                        return
                    if _dbgmode in (9, 10):
                        kb.op("vector", lambda e: e.tensor_tensor(out=rt1[:], in0=ps[bk][:], in1=cosb[:], op=ALU.mult), outs=[RT1], ins=[PS[bk], CSB])
                        if _dbgmode == 9:
                            kb.op("scalar", lambda e: e.copy(out=rt2[:], in_=ps[bk][:]), outs=[RT2], ins=[PS[bk]])
                        else:
                            kb.op("vector", lambda e: e.tensor_tensor(out=rt2[:], in0=rt1[:], in1=cosb[:], op=ALU.mult), outs=[RT2], ins=[RT1, CSB])
                        kb.op("gpsimd", lambda e: e.tensor_copy(out=dst_ap, in_=rt1[:]), outs=[dstbuf], ins=[RT1])
                        return
                    if _dbgmode in (11, 12):
                        kb.op("scalar", lambda e: e.copy(out=rawb[:], in_=ps[bk][:]), outs=[RAWB], ins=[PS[bk]])
                        b2 = mbank()
                        kb.op("tensor", lambda e: e.matmul(ps[b2][:], lhsT=pswap[:], rhs=rawb[:], start=True, stop=True), outs=[PS[b2]], ins=[RAWB, CST])
                        kb.op("vector", lambda e: e.tensor_tensor(out=rt1[:], in0=ps[bk][:], in1=cosb[:], op=ALU.mult), outs=[RT1], ins=[PS[bk], CSB, RAWB])
                        kb.op("gpsimd", lambda e: e.tensor_copy(out=dst_ap, in_=rt1[:]), outs=[dstbuf], ins=[RT1])
                        if _dbgmode == 12:
                            return
                        kb.op("vector", lambda e: e.tensor_tensor(out=rt1[:], in0=ps[b2][:], in1=sinb[:], op=ALU.mult), outs=[RT1], ins=[PS[b2], CSB])
                        kb.op("gpsimd", lambda e: e.tensor_tensor(out=dst_ap, in0=dst_ap, in1=rt1[:], op=ALU.add), outs=[dstbuf], ins=[RT1, dstbuf])
                        return
                    if _dbgmode == 2:
                        kb.op("scalar", lambda e: e.copy(out=rawb[:], in_=ps[bk][:]), outs=[RAWB], ins=[PS[bk]])
                        b2 = mbank()
                        kb.op("tensor", lambda e: e.matmul(ps[b2][:], lhsT=pswap[:], rhs=rawb[:], start=True, stop=True), outs=[PS[b2]], ins=[RAWB, CST])
                        kb.op("scalar", lambda e: e.copy(out=dst_ap, in_=ps[b2][:]), outs=[dstbuf], ins=[PS[b2]])
                        return
                    kb.op("scalar", lambda e: e.copy(out=rawb[:], in_=ps[bk][:]), outs=[RAWB], ins=[PS[bk]])
                    b2 = mbank()
                    kb.op("tensor", lambda e: e.matmul(ps[b2][:], lhsT=pswap[:], rhs=rawb[:], start=True, stop=True), outs=[PS[b2]], ins=[RAWB, CST])
                    kb.op("vector", lambda e: e.tensor_tensor(out=rt1[:], in0=ps[bk][:], in1=cosb[:], op=ALU.mult), outs=[RT1], ins=[PS[bk], CSB])
                    kb.op("vector", lambda e: e.tensor_tensor(out=rt2[:], in0=ps[b2][:], in1=sinb[:], op=ALU.mult), outs=[RT2], ins=[PS[b2], CSB])
                    if isinstance(dst_ap, tuple):
                        kb.op("gpsimd", lambda e: e.tensor_tensor(out=dst_ap[0], in0=rt1[0:64, :], in1=rt2[0:64, :], op=ALU.add), outs=[dstbuf], ins=[RT1, RT2])
                        kb.op("gpsimd", lambda e: e.tensor_tensor(out=dst_ap[1], in0=rt1[64:128, :], in1=rt2[64:128, :], op=ALU.add), outs=[dstbuf], ins=[RT1, RT2])
                    else:
                        kb.op("gpsimd", lambda e: e.tensor_tensor(out=dst_ap, in0=rt1[:], in1=rt2[:], op=ALU.add), outs=[dstbuf], ins=[RT1, RT2])

                for g in range(2):
                    for j, cbase in enumerate((2560, 2688)):
                        kb.dma("gpsimd", wk3[:, :, j * 64:(j + 1) * 64], w_in_v[:, :, cbase + g * 64:cbase + g * 64 + 64], outs=[WN])
                    for j, cbase in enumerate((2816, 2816, 3072, 3072)):
                        kb.dma("gpsimd", wk3[:, :, 128 + j * 64:128 + (j + 1) * 64], w_in_v[:, :, cbase + g * 64:cbase + g * 64 + 64], outs=[WN])
                    for j, cbase in enumerate((2944, 3200)):
                        kb.dma("gpsimd", wv2[:, :, j * 64:(j + 1) * 64], w_in_v[:, :, cbase + g * 64:cbase + g * 64 + 64], outs=[WN])
                    kb.dma("gpsimd", wqg[:], w_in_v[:, :, 2048 + g * 256:2048 + (g + 1) * 256], outs=[WN])
                    kb.dma("gpsimd", wgt[:], w_in_v[:, :, 3328 + g * 12:3328 + (g + 1) * 12], outs=[WN])
                    kb.op("vector", lambda e: e.memset(vs1[:, :, 64:65], 1.0), outs=[KS])
                    kb.op("vector", lambda e: e.memset(vw1[:, :, 64:65], 1.0), outs=[KS])
                    if stop_after == "p1c_a":
                        dump("wk3", wk3[:], [128, 8, 384], BF16)
                        return nc, dbg_out
                    for n in range(NB):
                        sl = slice(n * 512, (n + 1) * 512)
                        kb.dma("sync", cosb[:], I["cosT"][:, sl], outs=[CSB])
                        kb.dma("sync", sinb[:], I["sinT"][:, sl], outs=[CSB])
                        for j in range(3):
                            bk = sbank()
                            for k in range(8):
                                kb.op("tensor", lambda e, k=k, bk=bk, j=j: e.matmul(ps[bk][:], lhsT=wk3[:, k, j * 128:(j + 1) * 128], rhs=hT[:, k, sl],
                                                                                    start=(k == 0), stop=(k == 7)), outs=[PS[bk]], ins=[WN, HT[n]], mark=(k == 7))
                            if j == 0:
                                kb.op("scalar", lambda e, bk=bk: e.copy(out=kcvT[:, sl], in_=ps[bk][:]), outs=[KS], ins=[PS[bk]])
                            else:
                                rope_from(bk, (KEe[0:64, sl], KEo[64:128, sl]) if j == 1 else kwT[:, sl], KS)
                        if stop_after == "p1c_b":
                                return nc, dbg_out
                        for i in range(4):
                            tt = 4 * n + i
                            bk = mbank()
                            for k in range(8):
                                kb.op("tensor", lambda e, k=k, bk=bk, tt=tt: e.matmul(ps[bk][:, 0:128], lhsT=hT[:, k, tt * 128:(tt + 1) * 128], rhs=wv2[:, k, :],
                                                                                     start=(k == 0), stop=(k == 7)), outs=[PS[bk]], ins=[WN, HT[n]], mark=(k == 7))
                            kb.op("scalar", lambda e, bk=bk, tt=tt: e.copy(out=vs1[:, tt, 0:64], in_=ps[bk][:, 0:64]), outs=[KS], ins=[PS[bk]])
                            kb.op("vector", lambda e, bk=bk, tt=tt: e.tensor_copy(out=vw1[:, tt, 0:64], in_=ps[bk][:, 64:128]), outs=[KS], ins=[PS[bk]])
                    if stop_after == "p1c_k":
                        dump("kcvT", kcvT[:], [128, S], BF16)
                        dump("vs1", vs1[:], [128, NT, 80], BF16)
                        return nc, dbg_out
                    with ExitStack() as pc:
                        w1kv = sb("w1kv", [128, 32, 256], BF16, pc)
                        peT = sb("peT", [128, 32], F32, pc)
                        peTb = sb("peTb", [128, 32], BF16, pc)
                        b1kv = sb("b1kv", [128, 4], F32, pc)
                        w2k2 = sb("w2k2", [128, 2, 128], BF16, pc)
                        w2v = sb("w2v", [128, 2, 64], BF16, pc)
                        hid = sb("hid", [128, 4, 256], BF16, pc)
                        beff = sb("beff", [128, 4], F32, pc)
                        gx = sb("gx", [128, 256], F32, pc)
                        gu = sb("gu", [128, 256], F32, pc)
                        gs = sb("gs", [128, 256], F32, pc)
                        CW = Buf(w1kv[:])
                        HID = Buf(hid[:])
                        GX = Buf(gx[:])
                        kb.dma("gpsimd", w1kv[0:64], I["w1k"], outs=[CW])
                        kb.dma("gpsimd", w1kv[64:128], I["w1v"], outs=[CW])
                        kb.dma("sync", peT[0:64], I["peTk"], outs=[CW])
                        kb.dma("sync", peT[64:128], I["peTv"], outs=[CW])
                        kb.dma("sync", b1kv[:, 0:2], I["b1k"], outs=[CW])
                        kb.dma("sync", b1kv[:, 2:4], I["b1v"], outs=[CW])
                        kb.dma("gpsimd", w2k2[:, :, 0:64], I["w2k"], outs=[CW])
                        kb.dma("gpsimd", w2k2[:, :, 64:128], I["w2k"], outs=[CW])
                        kb.dma("gpsimd", w2v[:], I["w2v"], outs=[CW])
                        kb.op("vector", lambda e: e.tensor_copy(out=peTb[:], in_=peT[:]), outs=[CW], ins=[CW])
                        kb.op("vector", lambda e: e.memset(hid[:], 0.0), outs=[HID])
                        for kv in range(2):
                            p0_ = kv * 64
                            for hc in range(2):
                                bk, bb = sbank(), mbank()
                                for l in range(32):
                                    kb.op("tensor", lambda e, l=l, bk=bk, hc=hc, p0_=p0_: e.matmul(ps[bk][:, 0:255], lhsT=w1kv[p0_:p0_ + 64, l, hc * 128:(hc + 1) * 128],
                                                                                                rhs=kcvT[p0_:p0_ + 64, l:l + 16 * 254 + 1:16], start=(l == 0), stop=(l == 31)),
                                          outs=[PS[bk]], ins=[CW, KS], mark=(l == 31))
                                for l in range(32):
                                    kb.op("tensor", lambda e, l=l, bb=bb, hc=hc, p0_=p0_: e.matmul(ps[bb][:, 0:1], lhsT=w1kv[p0_:p0_ + 64, l, hc * 128:(hc + 1) * 128],
                                                                                                rhs=peTb[p0_:p0_ + 64, l:l + 1], start=(l == 0), stop=(l == 31)),
                                          outs=[PS[bb]], ins=[CW], mark=(l == 31))
                                ci = kv * 2 + hc
                                kb.op("vector", lambda e, bb=bb, ci=ci: e.tensor_tensor(out=beff[:, ci:ci + 1], in0=ps[bb][:, 0:1], in1=b1kv[:, ci:ci + 1], op=ALU.add),
                                      outs=[GX], ins=[PS[bb], CW])
                                kb.op("vector", lambda e, bk=bk, ci=ci: e.tensor_scalar(out=gx[:, 0:255], in0=ps[bk][:, 0:255], scalar1=beff[:, ci:ci + 1], scalar2=None, op0=ALU.add),
                                      outs=[GX], ins=[PS[bk], GX])
                                kb.op("vector", lambda e: e.tensor_tensor(out=gu[:, 0:255], in0=gx[:, 0:255], in1=gx[:, 0:255], op=ALU.mult), outs=[GX], ins=[GX])
                                kb.op("vector", lambda e: e.tensor_scalar(out=gu[:, 0:255], in0=gu[:, 0:255], scalar1=0.044715, scalar2=1.0, op0=ALU.mult, op1=ALU.add), outs=[GX], ins=[GX])
                                kb.op("vector", lambda e: e.tensor_tensor(out=gu[:, 0:255], in0=gu[:, 0:255], in1=gx[:, 0:255], op=ALU.mult), outs=[GX], ins=[GX])
                                kb.op("scalar", lambda e: e.activation(out=gs[:, 0:255], in_=gu[:, 0:255], func=AF.Sigmoid, scale=1.5957691216057308), outs=[GX], ins=[GX])
                                kb.op("vector", lambda e, ci=ci: e.tensor_tensor(out=hid[:, ci, 0:255], in0=gx[:, 0:255], in1=gs[:, 0:255], op=ALU.mult), outs=[HID], ins=[GX])
                        bk = sbank()
                        for hc in range(2):
                            kb.op("tensor", lambda e, hc=hc, bk=bk: e.matmul(ps[bk][:, 0:256], lhsT=w2k2[:, hc, :], rhs=hid[:, hc, :], start=(hc == 0), stop=(hc == 1)),
                                  outs=[PS[bk]], ins=[CW, HID], mark=(hc == 1))
                        kb.op("scalar", lambda e, bk=bk: e.copy(out=kcmpT[:], in_=ps[bk][:, 0:256]), outs=[KC], ins=[PS[bk]])
                        for ct in range(2):
                            bk = sbank()
                            for hc in range(2):
                                kb.op("tensor", lambda e, hc=hc, bk=bk, ct=ct: e.matmul(ps[bk][:, 0:64], lhsT=hid[:, 2 + hc, ct * 128:(ct + 1) * 128], rhs=w2v[:, hc, :],
                                                                                       start=(hc == 0), stop=(hc == 1)), outs=[PS[bk]], ins=[CW, HID], mark=(hc == 1))
                            kb.op("scalar", lambda e, bk=bk, ct=ct: e.copy(out=vcmp1[:, ct, 0:64], in_=ps[bk][:, 0:64]), outs=[KC], ins=[PS[bk]])
                        kb.op("vector", lambda e: e.memset(vcmp1[:, :, 64:65], 1.0), outs=[KC])
                        kb.op("vector", lambda e: e.tensor_copy(out=vcmp1[:, :, 65:129], in_=ovl[:]), outs=[KC], ins=[CST])
                        kb.barrier()
                    if stop_after == "p1c_c":
                        dump("kcmpT", kcmpT[:], [128, 256], BF16)
                        dump("vcmp1", vcmp1[:], [128, 2, 144], BF16)
                        return nc, dbg_out
                    with ExitStack() as pq:
                        wband = sb("wband", [128, 8, 512], BF16, pq)
                        QC = Buf(wband[:])
                        kb.dma("sync", wband[:], I["wband"], outs=[QC])
                        qn = [[sb(f"qn{ch}{par}", [128, 512], BF16, pq) for par in range(2)] for ch in range(2)]
                        QN = Buf(qn[0][0][:])
                        qTb = sb("qTb", [128, 2, 512], BF16, pq)
                        qrTb = sb("qrTb", [128, 2, 512], BF16, pq)
                        QB_ = Buf(qTb[:])
                        QRB = Buf(qrTb[:])
                        gts = sb("gts", [128, 4, 12], F32, pq)
                        GTS = Buf(gts[:])
                        cmpbb = sb("cmpbb", [128, 512], BF16, pq)
                        CMB = Buf(cmpbb[:])
                        pt = [sb(f"pt{i}", [128, 512], BF16, pq) for i in range(3)]
                        PT = [Buf(t[:]) for t in pt]
                        onsa = sb("onsa", [128, 4, 256], F32, pq)
                        ONSA = Buf(onsa[:])
                        obn = sb("obn", [128, 4, 256], BF16, pq)
                        OBN = Buf(obn[:])
                        pslc = sb("pslc", [128, 4, 64], F32, pq)
                        PSLC = Buf(pslc[:])
                        sadd = sb("sadd", [128, 64], F32, pq)
                        SADD = Buf(sadd[:])
                        score = sb("score", [128, 64], F32, pq)
                        stmp = sb("stmp", [128, 64], F32, pq)
                        sel = sb("sel", [128, 64], F32, pq)
                        m8 = sb("m8", [128, 16], F32, pq)
                        negb4 = [sb(f"negb{i}", [128, 128], BF16, pq) for i in range(4)]
                        SEL = Buf(score[:])
                        negbT = sb("negbT", [128, 512], BF16, pq)
                        NBT = Buf(negbT[:])
                        rz = sb("rz", [128, 4], F32, pq)
                        RZ = Buf(rz[:])
                        accs = [ps[3][:, 0:129], ps[4][:, 0:129], ps[5][:, 0:129], ps[6][:, 0:129]]
                        ACC = [PS[3], PS[4], PS[5], PS[6]]
                        prr = [0]

                        def run_branch(steps):
                            LA = 2
                            n_ = len(steps)
                            for idx_ in range(n_ + LA):
                                if idx_ < n_:
                                    st = steps[idx_]
                                    bk = sbank()
                                    nm = len(st["s"])
                                    for idx, (l_, r_, insb) in enumerate(st["s"]):
                                        kb.op("tensor", lambda e, l_=l_, r_=r_, idx=idx, nm=nm, bk=bk: e.matmul(ps[bk][:], lhsT=l_, rhs=r_, start=(idx == 0), stop=(idx == nm - 1)),
                                              outs=[PS[bk]], ins=insb, mark=(idx == nm - 1))
                                    prr[0] = (prr[0] + 1) % 3
                                    pi = prr[0]
                                    kb.op("scalar", lambda e, bk=bk, pi=pi: e.activation(out=pt[pi][:], in_=ps[bk][:], func=AF.Exp, scale=SCL), outs=[PT[pi]], ins=[PS[bk]])
                                    st["pi"] = pi
                                if idx_ >= LA:
                                    prev = steps[idx_ - LA]
                                    pi = prev["pi"]
                                    for (i, rhs_ap, w_, st_, sp_) in prev["pv"]:
                                        kb.op("tensor", lambda e, i=i, rhs_ap=rhs_ap, w_=w_, st_=st_, sp_=sp_, pi=pi: e.matmul(accs[i][:, 0:w_], lhsT=pt[pi][:, i * 128:(i + 1) * 128], rhs=rhs_ap,
                                                                                                                 start=st_, stop=sp_),
                                              outs=[ACC[i]], ins=[PT[pi], KS, KC])

                        accsb = [sb(f"accsb{i}", [128, 132], F32, pq) for i in range(4)]
                        ACCSB = [Buf(t[:]) for t in accsb]

                        def finish(h, br, first):
                            zc = 64
                            wd_ = 129 if br == 0 else 65
                            for i in range(4):
                                kb.op("vector", lambda e, i=i: e.tensor_copy(out=accsb[i][:, 0:wd_], in_=accs[i][:, 0:wd_]), outs=[ACCSB[i]], ins=[ACC[i]])
                            for i in range(4):
                                kb.op("vector", lambda e, i=i: e.tensor_scalar(out=rz[:, i:i + 1], in0=accsb[i][:, zc:zc + 1], scalar1=1e-30, scalar2=None, op0=ALU.max),
                                      outs=[RZ], ins=[ACCSB[i]])
                            kb.op("vector", lambda e: e.reciprocal(out=rz[:], in_=rz[:]), outs=[RZ], ins=[RZ])
                            if br == 0:
                                for i in range(4):
                                    if h == 0:
                                        kb.op("vector", lambda e, i=i: e.tensor_scalar(out=pslc[:, i, :], in0=accsb[i][:, 65:129], scalar1=rz[:, i:i + 1], scalar2=None, op0=ALU.mult),
                                              outs=[PSLC], ins=[ACCSB[i], RZ])
                                    else:
                                        kb.op("vector", lambda e, i=i: e.scalar_tensor_tensor(out=pslc[:, i, :], in0=accsb[i][:, 65:129], scalar=rz[:, i:i + 1], in1=pslc[:, i, :],
                                                                                           op0=ALU.mult, op1=ALU.add), outs=[PSLC], ins=[ACCSB[i], RZ, PSLC])
                            kb.op("vector", lambda e: e.tensor_tensor(out=rz[:], in0=rz[:], in1=gts[:, :, h * 3 + br], op=ALU.mult), outs=[RZ], ins=[RZ, GTS])
                            for i in range(4):
                                dst = onsa[:, i, h * 64:(h + 1) * 64]
                                if first:
                                    kb.op("vector", lambda e, i=i, dst=dst: e.tensor_scalar(out=dst, in0=accsb[i][:, 0:64], scalar1=rz[:, i:i + 1], scalar2=None, op0=ALU.mult),
                                          outs=[ONSA], ins=[ACCSB[i], RZ])
                                else:
                                    kb.op("vector", lambda e, i=i, dst=dst: e.scalar_tensor_tensor(out=dst, in0=accsb[i][:, 0:64], scalar=rz[:, i:i + 1], in1=dst, op0=ALU.mult, op1=ALU.add),
                                          outs=[ONSA], ins=[ACCSB[i], RZ, ONSA])

                        wband4 = sb("wband4", [128, 8, 512], BF16, pq)
                        cmpb0 = sb("cmpb0", [128, 512], BF16, pq)
                        kb.dma("sync", wband4[:], I["wband4"], outs=[QC])
                        kb.dma("sync", cmpb0[:], I["cmpb0"], outs=[QC])
                        for qb in range(4, NB):
                            sl = slice(qb * 512, (qb + 1) * 512)
                            kb.dma("sync", cosb[:], I["cosT"][:, sl], outs=[CSB])
                            kb.dma("sync", sinb[:], I["sinT"][:, sl], outs=[CSB])
                            kb.dma("sync", cmpbb[:], I["cmpb"][:, qb, :], outs=[CMB])
                            for ch in range(2):
                                bk = sbank()
                                for k in range(8):
                                    kb.op("tensor", lambda e, k=k, bk=bk, ch=ch: e.matmul(ps[bk][:], lhsT=wqg[:, k, ch * 128:(ch + 1) * 128], rhs=hT[:, k, sl],
                                                                                         start=(k == 0), stop=(k == 7)), outs=[PS[bk]], ins=[WN, HT[qb]], mark=(k == 7))
                                kb.op("scalar", lambda e, bk=bk, ch=ch: e.copy(out=qTb[:, ch, :], in_=ps[bk][:]), outs=[QB_], ins=[PS[bk]])
                                rope_from(bk, qrTb[:, ch, :], QRB)
                            for i in range(4):
                                tt = 4 * qb + i
                                bk = mbank()
                                for k in range(8):
                                    kb.op("tensor", lambda e, k=k, bk=bk, tt=tt: e.matmul(ps[bk][:, 0:12], lhsT=hT[:, k, tt * 128:(tt + 1) * 128], rhs=wgt[:, k, :],
                                                                                         start=(k == 0), stop=(k == 7)), outs=[PS[bk]], ins=[WN, HT[qb]], mark=(k == 7))
                                kb.op("scalar", lambda e, bk=bk, i=i: e.activation(out=gts[:, i, :], in_=ps[bk][:, 0:12], func=AF.Sigmoid), outs=[GTS], ins=[PS[bk]])
                            if stop_after == "p1c_qa":
                                dump("onsa", onsa[:], [128, 4, 256])
                                return nc, dbg_out
                            ncts = 1 if qb < 4 else 2
                            for h in range(4):
                                ch, p0_ = h // 2, (h % 2) * 64
                                steps = []
                                for ct in range(ncts):
                                    smm = [(kcmpT[p0_:p0_ + 64, ct * 128:(ct + 1) * 128], qTb[p0_:p0_ + 64, ch, :], [KC, QB_])]
                                    if ct == ncts - 1:
                                        smm.append((ident[:], cmpbb[:], [CST, CMB]))
                                    else:
                                        smm.append((ident[:], cmpb0[:], [CST, QC]))
                                    pv = [(i, vcmp1[:, ct, 0:129], 129, ct == 0, ct == ncts - 1) for i in range(4)]
                                    steps.append({"s": smm, "pv": pv})
                                run_branch(steps)
                                finish(h, 0, True)
                            if stop_after == "p1c_qb":
                                dump("onsa", onsa[:], [128, 4, 256])
                                return nc, dbg_out
                            for i in range(4):
                                qt = 4 * qb + i
                                kb.dma("sync", sadd[:], I["seladd"][:, qt, :], outs=[SADD])
                                kb.op("vector", lambda e, i=i: e.tensor_tensor(out=score[:], in0=pslc[:, i, :], in1=sadd[:], op=ALU.add), outs=[SEL], ins=[PSLC, SADD])
                                kb.op("vector", lambda e: e.max(out=m8[:, 0:8], in_=score[:]), outs=[SEL], ins=[SEL])
                                kb.op("vector", lambda e: e.match_replace(out=stmp[:], in_to_replace=m8[:, 0:8], in_values=score[:], imm_value=-3e38), outs=[SEL], ins=[SEL])
                                kb.op("vector", lambda e: e.max(out=m8[:, 8:16], in_=stmp[:]), outs=[SEL], ins=[SEL])
                                kb.op("vector", lambda e: e.tensor_scalar(out=sel[:], in0=score[:], scalar1=m8[:, 15:16], scalar2=None, op0=ALU.is_ge), outs=[SEL], ins=[SEL])
                                kb.op("vector", lambda e: e.scalar_tensor_tensor(out=sel[:], in0=score[:], scalar=-1e29, in1=sel[:], op0=ALU.is_gt, op1=ALU.mult), outs=[SEL], ins=[SEL])
                                kb.op("vector", lambda e: e.tensor_scalar(out=negb4[i][:, 0:64], in0=sel[:], scalar1=-1.0, scalar2=-NEG, op0=ALU.add, op1=ALU.mult), outs=[SEL], ins=[SEL])
                                kb.op("vector", lambda e: e.tensor_scalar(out=negb4[i][:, 64:128], in0=sel[:], scalar1=-1.0, scalar2=-NEG, op0=ALU.add, op1=ALU.mult), outs=[SEL], ins=[SEL])
                            for h in range(4):
                                ch, p0_ = h // 2, (h % 2) * 64
                                steps = []
                                jmin = max(0, 4 - 4 * qb)
                                for j in range(jmin, 8):
                                    kt = 4 * qb - 4 + j
                                    smm = [(kwT[p0_:p0_ + 64, kt * 128:(kt + 1) * 128], qrTb[p0_:p0_ + 64, ch, :], [KS, QRB]),
                                           (ident[:], (wband4 if qb == 4 else wband)[:, j, :], [CST, QC])]
                                    pv = [(i, vw1[:, kt, 0:65], 65, j == max(i, jmin), j == i + 4) for i in range(4) if i <= j <= i + 4]
                                    steps.append({"s": smm, "pv": pv})
                                run_branch(steps)
                                finish(h, 2, False)
                            for i in range(4):
                                bk = mbank()
                                kb.op("tensor", lambda e, bk=bk, i=i: e.matmul(ps[bk][:, 0:128], lhsT=negb4[i][:], rhs=ident[:], start=True, stop=True), outs=[PS[bk]], ins=[SEL, CST])
                                kb.op("scalar", lambda e, bk=bk, i=i: e.copy(out=negbT[:, i * 128:(i + 1) * 128], in_=ps[bk][:, 0:128]), outs=[NBT], ins=[PS[bk]])
                            for ch in range(2):
                                kb.op("gpsimd", lambda e, ch=ch: e.tensor_copy(out=qn[ch][0][0:64, :], in_=qrTb[0:64, ch, :]), outs=[QN], ins=[QRB])
                                kb.op("gpsimd", lambda e, ch=ch: e.tensor_copy(out=qn[ch][0][64:128, :], in_=negbT[64:128, :]), outs=[QN], ins=[NBT])
                                kb.op("gpsimd", lambda e, ch=ch: e.tensor_copy(out=qn[ch][1][0:64, :], in_=negbT[0:64, :]), outs=[QN], ins=[NBT])
                                kb.op("gpsimd", lambda e, ch=ch: e.tensor_copy(out=qn[ch][1][64:128, :], in_=qrTb[64:128, ch, :]), outs=[QN], ins=[QRB])
                            for h in range(4):
                                ch, p0_ = h // 2, (h % 2) * 64
                                KE_ = KEe if h % 2 == 0 else KEo
                                steps = []
                                for kt in range(4 * qb + 4):
                                    smm = [(KE_[:, kt * 128:(kt + 1) * 128], qn[ch][h % 2][:], [KS, QN, CST])]
                                    if kt >= 4 * qb:
                                        smm.append((ident[:], wband[:, 4 + kt - 4 * qb, :], [CST, QC]))
                                    pv = [(i, vs1[:, kt, 0:65], 65, kt == 0, kt == 4 * qb + i) for i in range(4) if kt <= 4 * qb + i]
                                    steps.append({"s": smm, "pv": pv})
                                run_branch(steps)
                                finish(h, 1, False)
                            if stop_after == "p1c_q":
                                dump("onsa", onsa[:], [128, 4, 256])
                                dump("pslc", pslc[:], [128, 4, 64])
                                dump("negbT", negbT[:], [128, 512], BF16)
                                return nc, dbg_out
                            kb.op("gpsimd", lambda e: e.tensor_copy(out=obn[:], in_=onsa[:]), outs=[OBN], ins=[ONSA])
                            for i in range(4):
                                qt = 4 * qb + i
                                s_ = qt - 16
                                for ch in range(2):
                                    jf = 4 + g * 2 + ch
                                    bk = mbank()
                                    kb.op("tensor", lambda e, bk=bk, i=i, ch=ch: e.matmul(ps[bk][:, 0:128], lhsT=obn[:, i, ch * 128:(ch + 1) * 128], rhs=ident[:], start=True, stop=True),
                                          outs=[PS[bk]], ins=[OBN, CST])
                                    dst = oT[:, jf, s_ * 128:(s_ + 1) * 128]
                                    kb.op("scalar", lambda e, bk=bk, dst=dst: e.copy(out=dst, in_=ps[bk][:, 0:128]), outs=[OT[jf][s_]], ins=[PS[bk]])
                        kb.barrier()
                kb.barrier()
            if "oT2" in dbg:
                dump("oT2", oT[:], [128, 8, SO], BF16)
            if stop_after == "p1c":
                kb.barrier()
                return nc, dbg_out
        kb.barrier()
        with ExitStack() as p2:
            acc = sb("acc", [128, 8, SO], F32, p2)
            ACCB = [Buf(acc[:, :, nb * 512:(nb + 1) * 512]) for nb in range(4)]
            wo = sb("wo", [128, 8, D], BF16, p2)
            WO = Buf(wo[:])
            fg = sb("fg", [128, 8], F32, p2)
            FG = Buf(fg[:])
            kb.dma("sync", fg[:], I["fg"], outs=[FG])
            xTo_v = I["xTo"].rearrange("(k p) t -> p k t", p=128)
            for nb in range(4):
                kb.dma("sync", acc[:, :, nb * 512:(nb + 1) * 512], xTo_v[:, :, nb * 512:(nb + 1) * 512], outs=[ACCB[nb]])
            w_out_v = I["w_out"].rearrange("(k p) c -> p k c", p=128)
            for k in range(8):
                kb.dma("gpsimd", wo[:, k, :], w_out_v[:, k, :], outs=[WO])
            rr2 = [0]

            def bank2():
                rr2[0] = (rr2[0] + 1) % 8
                return rr2[0]

            for nb in range(4):
                sl = slice(nb * 512, (nb + 1) * 512)
                for m in range(8):
                    bk = bank2()
                    for k in range(8):
                        kb.op("tensor", lambda e, k=k, m=m, bk=bk, sl=sl: e.matmul(ps[bk][:], lhsT=wo[:, k, m * 128:(m + 1) * 128], rhs=oT[:, k, sl], start=(k == 0), stop=(k == 7)),
                              outs=[PS[bk]], ins=[WO] + [OT[k][s] for s in range(nb * 4, nb * 4 + 4)], mark=(k == 7))
                    kb.op("vector", lambda e, m=m, bk=bk, sl=sl: e.scalar_tensor_tensor(out=acc[:, m, sl], in0=ps[bk][:], scalar=g1c(m), in1=acc[:, m, sl], op0=ALU.mult, op1=ALU.add),
                          outs=[ACCB[nb]], ins=[PS[bk], ACCB[nb], MOD])
            if "x2T" in dbg:
                dump("x2T", acc[:], [128, 8, SO])
            h2T = sb("h2T", [128, 8, SO], BF16, p2)
            H2 = [Buf(h2T[:, :, nb * 512:(nb + 1) * 512]) for nb in range(4)]
            with ExitStack() as p2a:
                sqa = sb("sqa", [128, 8, 512], F32, p2a)
                SQA = Buf(sqa[:])
                rsa = sb("rsa", [128, 512], F32, p2a)
                RSA = Buf(rsa[:])
                tma = [sb(f"tma{i}", [128, 512], F32, p2a) for i in range(2)]
                TMA = [Buf(t[:]) for t in tma]
                for nb in range(4):
                    sl = slice(nb * 512, (nb + 1) * 512)
                    kb.op("scalar", lambda e, sl=sl: e.activation(out=sqa[:], in_=acc[:, :, sl], func=AF.Square), outs=[SQA], ins=[ACCB[nb]])
                    bk = bank2()
                    for k in range(8):
                        kb.op("tensor", lambda e, k=k, bk=bk: e.matmul(ps[bk][:], lhsT=onesf[:], rhs=sqa[:, k, :], start=(k == 0), stop=(k == 7)),
                              outs=[PS[bk]], ins=[SQA, CST], mark=(k == 7))
                    kb.op("scalar", lambda e, bk=bk: e.activation(out=rsa[:], in_=ps[bk][:], func=AF.Sqrt, scale=1.0 / D, bias=EPS), outs=[RSA], ins=[PS[bk]])
                    kb.op("vector", lambda e: e.reciprocal(out=rsa[:], in_=rsa[:]), outs=[RSA], ins=[RSA])
                    for k in range(8):
                        T = TMA[k % 2]
                        t_ = tma[k % 2]
                        kb.op("vector", lambda e, k=k, t_=t_, sl=sl: e.tensor_tensor(out=t_[:], in0=acc[:, k, sl], in1=rsa[:], op=ALU.mult), outs=[T], ins=[ACCB[nb], RSA])
                        kb.op("scalar", lambda e, k=k, t_=t_, sl=sl: e.activation(out=h2T[:, k, sl], in_=t_[:], func=AF.Identity, scale=a2[:, k:k + 1], bias=sh2(k)),
                              outs=[H2[nb]], ins=[T, MOD])
                kb.barrier()
            if "h2T" in dbg:
                dump("h2T", h2T[:], [128, 8, SO], BF16)
            gw = sb("gw", [128, 16, 65], F32, p2)
            GW = Buf(gw[:])
            kb.op("vector", lambda e: e.memset(gw[:], 1.0), outs=[GW])
            with ExitStack() as p2r:
                rwf = sb("rwf", [128, 8, 64], F32, p2r)
                rwb = sb("rwb", [128, 8, 64], BF16, p2r)
                rbias = sb("rbias", [128, 64], F32, p2r)
                RW = Buf(rwf[:])
                kb.dma("sync", rwf[:], I["rw"], outs=[RW])
                kb.dma("sync", rbias[:], I["rbias"], outs=[RW])
                kb.op("vector", lambda e: e.tensor_copy(out=rwb[:], in_=rwf[:]), outs=[RW], ins=[RW])
                scr = sb("scr", [128, 64], F32, p2r)
                chs = sb("chs", [128, 64], F32, p2r)
                eq = sb("eq", [128, 64], F32, p2r)
                chm = sb("chm", [128, 64], F32, p2r)
                m1 = sb("m1", [128, 8], F32, p2r)
                m2 = sb("m2", [128, 8], F32, p2r)
                gsm = sb("gsm", [128, 8], F32, p2r)
                g8 = sb("g8", [128, 8], F32, p2r)
                gmk = sb("gmk", [128, 8], F32, p2r)
                e8 = sb("e8", [128, 8], F32, p2r)
                ssum = sb("ssum", [128, 1], F32, p2r)
                RT = Buf(scr[:])
                v3 = lambda ap: ap.rearrange("p (g j) -> p g j", j=8)
                b3 = lambda ap: ap.rearrange("p (g o) -> p g o", o=1).to_broadcast([128, 8, 8])
                for tt in range(16):
                    bk = bank2()
                    for k in range(8):
                        kb.op("tensor", lambda e, k=k, bk=bk, tt=tt: e.matmul(ps[bk][:, 0:64], lhsT=h2T[:, k, tt * 128:(tt + 1) * 128], rhs=rwb[:, k, :], start=(k == 0), stop=(k == 7)),
                              outs=[PS[bk]], ins=[H2[tt // 4], RW], mark=(k == 7))
                    kb.op("scalar", lambda e, bk=bk: e.activation(out=scr[:], in_=ps[bk][:, 0:64], func=AF.Sigmoid), outs=[RT], ins=[PS[bk]])
                    V = lambda f: kb.op("vector", f, outs=[RT], ins=[RT, RW])
                    V(lambda e: e.tensor_tensor(out=chs[:], in0=scr[:], in1=rbias[:], op=ALU.add))
                    V(lambda e: e.tensor_reduce(out=m1[:], in_=v3(chs[:]), axis=AX.X, op=ALU.max))
                    V(lambda e: e.tensor_tensor(out=v3(eq[:]), in0=v3(chs[:]), in1=b3(m1[:]), op=ALU.is_equal))
                    V(lambda e: e.scalar_tensor_tensor(out=eq[:], in0=eq[:], scalar=-1e30, in1=chs[:], op0=ALU.mult, op1=ALU.add))
                    V(lambda e: e.tensor_reduce(out=m2[:], in_=v3(eq[:]), axis=AX.X, op=ALU.max))
                    V(lambda e: e.tensor_tensor(out=gsm[:], in0=m1[:], in1=m2[:], op=ALU.add))
                    V(lambda e: e.max(out=g8[:], in_=gsm[:]))
                    V(lambda e: e.tensor_scalar(out=gmk[:], in0=gsm[:], scalar1=g8[:, 3:4], scalar2=None, op0=ALU.is_ge))
                    V(lambda e: e.scalar_tensor_tensor(out=v3(chm[:]), in0=v3(chs[:]), scalar=10.0, in1=b3(gmk[:]), op0=ALU.add, op1=ALU.mult))
                    V(lambda e: e.max(out=e8[:], in_=chm[:]))
                    V(lambda e: e.tensor_scalar(out=eq[:], in0=chm[:], scalar1=e8[:, 7:8], scalar2=None, op0=ALU.is_ge))
                    V(lambda e: e.tensor_tensor(out=eq[:], in0=eq[:], in1=scr[:], op=ALU.mult))
                    V(lambda e: e.tensor_reduce(out=ssum[:], in_=eq[:], axis=AX.X, op=ALU.add))
                    V(lambda e: e.reciprocal(out=ssum[:], in_=ssum[:]))
                    kb.op("vector", lambda e, tt=tt: e.tensor_scalar(out=gw[:, tt, 0:64], in0=eq[:], scalar1=ssum[:, 0:1], scalar2=2.5, op0=ALU.mult, op1=ALU.mult),
                          outs=[GW], ins=[RT])
                kb.barrier()
            if "gw" in dbg:
                dump("gw", gw[:], [128, 16, 65])
            with ExitStack() as p2e:
                wg = [sb(f"wg{i}", [128, 8, 512], BF16, p2e) for i in range(2)]
                wd = [sb(f"wd{i}", [128, 2, D], BF16, p2e) for i in range(2)]
                WG = [Buf(t[:]) for t in wg]
                WD = [Buf(t[:]) for t in wd]
                actT = [sb(f"actT{i}", [128, 2, SO], BF16, p2e) for i in range(2)]
                ACT_ = [[Buf(actT[i][:, :, nb * 512:(nb + 1) * 512]) for nb in range(4)] for i in range(2)]
                sgt = [sb(f"sgt{i}", [128, 256], F32, p2e) for i in range(2)]
                SGT = [Buf(t[:]) for t in sgt]
                att = [sb(f"att{i}", [128, 256], BF16, p2e) for i in range(2)]
                ATT = [Buf(t[:]) for t in att]
                NE = n_experts

                def load_w(e_):
                    sl_ = e_ % 2
                    gv = I["wgu"][e_].rearrange("(k p) c -> p k c", p=128)
                    kb.dma("gpsimd", wg[sl_][:, 0:4, :], gv[:, 0:4, :], outs=[WG[sl_]])
                    kb.dma("gpsimd", wg[sl_][:, 4:8, :], gv[:, 4:8, :], outs=[WG[sl_]])
                    kb.dma("gpsimd", wd[sl_][:], I["wdn"][e_].rearrange("(k p) c -> p k c", p=128), outs=[WD[sl_]])

                def load_wg(e_):
                    sl_ = e_ % 2
                    gv = I["wgu"][e_].rearrange("(k p) c -> p k c", p=128)
                    kb.dma("gpsimd", wg[sl_][:, 0:4, :], gv[:, 0:4, :], outs=[WG[sl_]])
                    kb.dma("gpsimd", wg[sl_][:, 4:8, :], gv[:, 4:8, :], outs=[WG[sl_]])

                def load_wd(e_):
                    sl_ = e_ % 2
                    kb.dma("gpsimd", wd[sl_][:], I["wdn"][e_].rearrange("(k p) c -> p k c", p=128), outs=[WD[sl_]])

                cnt = [0]
                pend = {}

                def GU(e_, tt):
                    sl_ = e_ % 2
                    bk = bank2()
                    for k in range(8):
                        kb.op("tensor", lambda e, k=k: e.matmul(ps[bk][:], lhsT=h2T[:, k, tt * 128:(tt + 1) * 128], rhs=wg[sl_][:, k, :], start=(k == 0), stop=(k == 7)),
                              outs=[PS[bk]], ins=[H2[tt // 4], WG[sl_]], mark=(k == 7))
                    cnt[0] += 1
                    i2 = cnt[0] % 2
                    kb.op("scalar", lambda e: e.activation(out=sgt[i2][:], in_=ps[bk][:, 0:256], func=AF.Silu), outs=[SGT[i2]], ins=[PS[bk]])
                    kb.op("vector", lambda e: e.scalar_tensor_tensor(out=att[i2][:], in0=ps[bk][:, 256:512], scalar=gw[:, tt, e_:e_ + 1], in1=sgt[i2][:],
                                                                     op0=ALU.mult, op1=ALU.mult), outs=[ATT[i2]], ins=[PS[bk], SGT[i2], GW])
                    pend[(e_, tt)] = i2

                def TR(e_, tt):
                    sl_ = e_ % 2
                    i2 = pend.pop((e_, tt))
                    for hc in range(2):
                        bt = bank2()
                        kb.op("tensor", lambda e, hc=hc, bt=bt: e.matmul(ps[bt][:, 0:128], lhsT=att[i2][:, hc * 128:(hc + 1) * 128], rhs=ident[:], start=True, stop=True),
                              outs=[PS[bt]], ins=[ATT[i2], CST])
                        kb.op("scalar", lambda e, hc=hc, bt=bt: e.copy(out=actT[sl_][:, hc, tt * 128:(tt + 1) * 128], in_=ps[bt][:, 0:128]),
                              outs=[ACT_[sl_][tt // 4]], ins=[PS[bt]])

                def DN(e_, nb, ms):
                    sl_ = e_ % 2
                    sl = slice(nb * 512, (nb + 1) * 512)
                    for m in ms:
                        bk = bank2()
                        for hc in range(2):
                            kb.op("tensor", lambda e, bk=bk, hc=hc, m=m: e.matmul(ps[bk][:], lhsT=wd[sl_][:, hc, m * 128:(m + 1) * 128], rhs=actT[sl_][:, hc, sl], start=(hc == 0), stop=(hc == 1)),
                                  outs=[PS[bk]], ins=[WD[sl_], ACT_[sl_][nb]], mark=(hc == 1))
                        kb.op("vector", lambda e, bk=bk, m=m: e.scalar_tensor_tensor(out=acc[:, m, sl], in0=ps[bk][:], scalar=g2c(m), in1=acc[:, m, sl], op0=ALU.mult, op1=ALU.add),
                              outs=[ACCB[nb]], ins=[PS[bk], ACCB[nb], MOD])

                load_wg(0)
                load_wd(0)
                for e_ in range(NE + 1):
                    if e_ + 1 < NE:
                        load_wg(e_ + 1)
                    for tt in range(16):
                        if e_ < NE:
                            GU(e_, tt)
                            if tt > 0:
                                TR(e_, tt - 1)
                        if e_ > 0:
                            DN(e_ - 1, tt // 4, (2 * (tt % 4), 2 * (tt % 4) + 1))
                    if e_ < NE:
                        TR(e_, 15)
                    if e_ + 1 < NE:
                        load_wd(e_ + 1)
                kb.barrier()
            if "x3T" in dbg:
                dump("x3T", acc[:], [128, 8, SO])
            sq2 = sb("sq2", [128, 8, 512], F32, p2)
            SQ2 = Buf(sq2[:])
            rstd2 = sb("rstd2", [128, 512], F32, p2)
            RS2 = Buf(rstd2[:])
            ot = [sb(f"ot{i}", [128, 8, 512], F32, p2) for i in range(2)]
            OTB = [Buf(t[:]) for t in ot]
            outT_v = outT.rearrange("(k p) t -> p k t", p=128)
            for nb in range(4):
                sl = slice(nb * 512, (nb + 1) * 512)
                kb.op("scalar", lambda e, sl=sl: e.activation(out=sq2[:], in_=acc[:, :, sl], func=AF.Square), outs=[SQ2], ins=[ACCB[nb]])
                bk = bank2()
                for k in range(8):
                    kb.op("tensor", lambda e, k=k, bk=bk: e.matmul(ps[bk][:], lhsT=onesf[:], rhs=sq2[:, k, :], start=(k == 0), stop=(k == 7)),
                          outs=[PS[bk]], ins=[SQ2, CST], mark=(k == 7))
                kb.op("scalar", lambda e, bk=bk: e.activation(out=rstd2[:], in_=ps[bk][:], func=AF.Sqrt, scale=1.0 / D, bias=EPS), outs=[RS2], ins=[PS[bk]])
                kb.op("vector", lambda e: e.reciprocal(out=rstd2[:], in_=rstd2[:]), outs=[RS2], ins=[RS2])
                o_ = ot[nb % 2]
                for k in range(8):
                    kb.op("vector", lambda e, k=k, o_=o_, sl=sl: e.scalar_tensor_tensor(out=o_[:, k, :], in0=acc[:, k, sl], scalar=fg[:, k:k + 1], in1=rstd2[:], op0=ALU.mult, op1=ALU.mult),
                          outs=[OTB[nb % 2]], ins=[ACCB[nb], FG, RS2])
                kb.dma("sync", outT_v[:, :, sl], o_[:], ins=[OTB[nb % 2]])
            kb.barrier()
        kb.barrier()
    return nc, dbg_out


def _prep_inputs(inp, core):
    b = core // 2
    half = core % 2
    f = lambda a: np.ascontiguousarray(a, dtype=np.float32)
    x = inp["x"][b]
    m = {}
    xT = f(x.T)
    m["xTo"] = f(xT[:, half * SO:(half + 1) * SO])
    if half == 0:
        xl = np.zeros((D, S), np.float32)
        xl[:, SO:] = xT[:, :SO]
        m["xT"] = xl
    else:
        m["xT"] = xT
    m["cT"] = f(inp["c"][b].reshape(8, 128).T)
    m["w_ada"] = f(inp["w_ada"][0])
    m["b_ada"] = f(inp["b_ada"][0].reshape(1, -1))
    m["n1g"] = f(inp["norm1_g"][0].reshape(8, 128).T)
    m["w_in"] = f(inp["w_in"][0])
    m["lbl"] = f(inp["hg_lb_logits"].reshape(2, 4, 128).transpose(2, 0, 1))
    m["hng"] = f(np.broadcast_to(inp["hg_norm_g"][0][None, :], (128, 512)))
    for s in ("k", "v"):
        m["peT" + s] = f(inp["cmp_pos_" + s][0].T)
        m["w1" + s] = f(inp["cmp_w1_" + s][0].reshape(32, 64, 256).transpose(1, 0, 2))
        m["b1" + s] = f(inp["cmp_b1_" + s][0].reshape(2, 128).T)
        m["w2" + s] = f(inp["cmp_w2_" + s][0].reshape(2, 128, 64).transpose(1, 0, 2))
    m["w_out"] = f(inp["w_out"][0])
    m["n2g"] = f(inp["norm2_g"][0].reshape(8, 128).T)
    m["rw"] = f(inp["router_w"][0].reshape(8, 128, 64).transpose(1, 0, 2))
    m["rbias"] = f(np.broadcast_to(inp["router_bias"][0][None, :], (128, 64)))
    m["fg"] = f(inp["final_g"].reshape(8, 128).T)
    return m


_SHARED = {}


def kernel(**inp):
    inp = {k: np.asarray(v) for k, v in inp.items()}
    nc, _ = build()
    wgu = np.ascontiguousarray(np.concatenate([inp["w_exp_gu"][0], inp["w_sh_gu"][0][None]], axis=0), dtype=np.float32)
    wdn = np.ascontiguousarray(np.concatenate([inp["w_exp_dn"][0], inp["w_sh_dn"][0][None]], axis=0), dtype=np.float32)
    in_maps = []
    for core in range(8):
        m = _prep_inputs(inp, core)
        m["wgu"] = wgu
        m["wdn"] = wdn
        m.update(_consts(core % 2))
        in_maps.append(m)
    res = run_bass_kernel_spmd(nc, in_maps, core_ids=list(range(8)))
    out = np.zeros((4, S, D), np.float32)
    for core in range(8):
        b, half = core // 2, core % 2
        out[b, half * SO:(half + 1) * SO, :] = res.results[core]["outT"].T
    return out
```

```python
import numpy as np
import os as _os0
import ml_dtypes
from contextlib import ExitStack
import concourse.bass as bass
import concourse.mybir as mybir
from concourse.bass_utils import run_bass_kernel_spmd

F32 = mybir.dt.float32
BF16 = mybir.dt.bfloat16
AF = mybir.ActivationFunctionType
ALU = mybir.AluOpType
AX = mybir.AxisListType

S = 4096
D = 1024
NT = 32
NB = 8
SO = 2048
EPS = 1e-6
NEG = -30000.0
NDS = 12
SEM_LIMIT = 2000
SAME_SYNC = not bool(int(_os0.environ.get("NOSAME", "0")))


class Buf:
    __slots__ = ("ap", "w", "r", "excl")

    def __init__(self, ap, excl=False):
        self.ap = ap
        self.w = None
        self.r = {}
        self.excl = excl

    def __getitem__(self, k):
        return self.ap[k]


class Eng:
    def __init__(self, name, h):
        self.name = name
        self.h = h
        self.sem = None
        self.count = 0
        self.epoch = 0
        self.waited = {}


class KB:
    def __init__(self, nc, es):
        self.nc = nc
        self.es = es
        self.engs = {n: Eng(n, getattr(nc, n)) for n in ("tensor", "vector", "scalar", "gpsimd", "sync")}
        for e in self.engs.values():
            self._new_sem(e)
        self.dsems = {q: [es.enter_context(nc.semaphore(f"d_{q}{i}")) for i in range(NDS)] for q in ("sync", "gpsimd")}
        self.dcnt = {q: [0] * NDS for q in ("sync", "gpsimd")}
        self.drr = {"sync": 0, "gpsimd": 0}
        self.nsem = 0

    def _new_sem(self, e):
        e.epoch += 1
        e.sem = self.es.enter_context(self.nc.semaphore(f"s_{e.name}_{e.epoch}"))
        e.count = 0

    def wait(self, eng, tk):
        key, sem, val = tk
        if eng.waited.get(key, 0) >= val:
            return
        eng.h.wait_ge(sem, val)
        eng.waited[key] = val

    def _deps(self, en, eng, outs, ins):
        need = {}

        def add(t):
            if t[3] == en and (en == "tensor" or not SAME_SYNC):
                return
            cur = need.get(t[0])
            if cur is None or cur[2] < t[2]:
                need[t[0]] = t

        for b in ins:
            if b.w is not None:
                add(b.w)
            if b.excl:
                for t in b.r.values():
                    if t[3] != en:
                        add(t)
        for b in outs:
            if b.w is not None:
                add(b.w)
            for t in b.r.values():
                add(t)
        for t in need.values():
            self.wait(eng, t[:3])

    def op(self, en, fn, outs=(), ins=(), mark=True):
        eng = self.engs[en]
        self._deps(en, eng, outs, ins)
        if eng.count >= SEM_LIMIT:
            self._new_sem(eng)
        inst = fn(eng.h)
        if mark:
            eng.count += 1
            inst.then_inc(eng.sem, 1)
            tk = ((en, eng.epoch), eng.sem, eng.count, en)
        else:
            tk = ((en, eng.epoch), eng.sem, eng.count + 1, en)
        for b in ins:
            b.r[tk[0]] = tk
        for b in outs:
            b.w = tk
            b.r = {}
        return tk

    def dma(self, q, out_ap, in_ap, outs=(), ins=()):
        eng = self.engs[q]
        i = self.drr[q]
        self.drr[q] = (i + 1) % NDS
        sem = self.dsems[q][i]
        key = ("d", q, i)
        if self.dcnt[q][i] > 0:
            self.wait(eng, (key, sem, self.dcnt[q][i]))
        self._deps("dma_" + q, eng, outs, ins)
        inst = eng.h.dma_start(out=out_ap, in_=in_ap)
        self.dcnt[q][i] += 16
        inst.then_inc(sem, 16)
        tk = (key, sem, self.dcnt[q][i], "dma_" + q)
        for b in ins:
            b.r[key] = tk
        for b in outs:
            b.w = tk
            b.r = {}
        return tk

    def barrier(self):
        for e in self.engs.values():
            for o in self.engs.values():
                if o is e or o.count == 0:
                    continue
                self.wait(e, ((o.name, o.epoch), o.sem, o.count))
            for q in ("sync", "gpsimd"):
                for i in range(NDS):
                    if self.dcnt[q][i] > 0:
                        self.wait(e, (("d", q, i), self.dsems[q][i], self.dcnt[q][i]))


def _consts(half):
    bf = ml_dtypes.bfloat16
    c = {}
    eye = np.eye(128, dtype=np.float32)
    c["ident"] = eye.astype(bf)
    c["onesf"] = np.ones((128, 128), np.float32)
    c["isel0"] = (eye * (1.0 if half == 0 else 0.0)).astype(bf)
    c["isel1"] = (eye * (1.0 if half == 1 else 0.0)).astype(bf)
    m = np.arange(128)
    sw = (m // 64) * 64 + ((m % 64) + 32) % 64
    ps = np.zeros((128, 128), np.float32)
    ps[sw, m] = 1.0
    c["pswap"] = ps.astype(bf)
    shift = 2048 if half == 0 else 0
    dd = np.arange(128) % 64
    i = dd % 32
    inv = 10000.0 ** (-(i.astype(np.float64)) / 32.0)
    tpos = (np.arange(S) - shift).astype(np.float64)
    ang = inv[:, None].astype(np.float32).astype(np.float64) * tpos[None, :]
    ang = ang.astype(np.float32).astype(np.float64)
    c["cosT"] = np.cos(ang).astype(np.float32)
    sg = np.where(dd < 32, -1.0, 1.0)[:, None]
    c["sinT"] = (np.sin(ang) * sg).astype(np.float32)
    vm = np.ones((128, 32), np.float32)
    if half == 0:
        vm[:, :16] = 0.0
    c["vmask"] = vm
    c["hmask"] = (m[:, None] <= m[None, :]).astype(np.float32).astype(bf)
    seg = np.ones((128, 512), np.float32)
    seg[:, ::128] = 0.0
    c["segm"] = seg
    r = np.arange(128)[:, None]
    qi = np.arange(512)[None, :]
    wb = np.zeros((8, 128, 512), np.float32)
    cb = np.zeros((4, 128, 512), np.float32)
    for j in range(8):
        kpos = -512 + 128 * j + r
        dlt = qi - kpos
        wb[j] = np.where((dlt >= 0) & (dlt < 512), 0.0, NEG)
    for j in range(4):
        kpos = 128 * j + r
        cb[j] = np.where(kpos <= qi, 0.0, NEG)
    c["wband"] = np.ascontiguousarray(wb.transpose(1, 0, 2)).astype(bf)
    c["causb"] = np.ascontiguousarray(cb.transpose(1, 0, 2)).astype(bf)
    wb4 = wb.copy()
    if half == 0:
        wb4[0:4] = NEG
    c["wband4"] = np.ascontiguousarray(wb4.transpose(1, 0, 2)).astype(bf)
    cm = np.zeros((8, 128, 512), np.float32)
    for qb in range(8):
        ct = 0 if qb < 4 else 1
        cc = 128 * ct + r
        qpos = 512 * qb + qi - shift
        tc = cc - shift // 16
        cm[qb] = np.where((16 * tc + 31 <= qpos) & (cc < 255) & (tc >= 0), 0.0, NEG)
    c["cmpb"] = np.ascontiguousarray(cm.transpose(1, 0, 2)).astype(bf)
    c["cmpb0"] = np.full((128, 512), NEG if half == 0 else 0.0, np.float32).astype(bf)
    ek = np.zeros((64, 32, 128), np.float32)
    for kt in range(32):
        ek[2 * kt, kt, :64] = 1.0
        ek[2 * kt + 1, kt, 64:] = 1.0
    c["ekt"] = np.concatenate([ek, ek], axis=0).astype(bf)
    add = np.zeros((128, 32, 64), np.float32)
    for qt in range(32):
        pos = 128 * qt + np.arange(128) - shift
        cur = pos // 64
        j = np.arange(64)[None, :] - shift // 64
        forced = (j == 0) | (j == cur[:, None]) | (j == cur[:, None] - 1)
        avail = (j <= cur[:, None]) & (j >= 0)
        add[:, qt, :] = np.where(avail & forced, 1e30, np.where(avail, 0.0, -1e30))
    c["seladd"] = add
    cs = np.arange(256)[:, None] * 16
    ss = np.arange(64)[None, :] * 64
    ov = np.clip(np.minimum(cs + 32, ss + 64) - np.maximum(cs, ss), 0, None).astype(np.float32) / 32.0
    ov[255] = 0.0
    c["ovl"] = np.ascontiguousarray(ov.reshape(2, 128, 64).transpose(1, 0, 2)).astype(bf)
    return c


CONST_SHAPES = {
    "ident": ([128, 128], BF16), "onesf": ([128, 128], F32), "isel0": ([128, 128], BF16), "isel1": ([128, 128], BF16),
    "pswap": ([128, 128], BF16), "cosT": ([128, S], F32), "sinT": ([128, S], F32), "hmask": ([128, 128], BF16),
    "segm": ([128, 512], F32), "wband": ([128, 8, 512], BF16), "causb": ([128, 4, 512], BF16),
    "cmpb": ([128, 8, 512], BF16), "ekt": ([128, 32, 128], BF16), "seladd": ([128, 32, 64], F32),
    "ovl": ([128, 2, 64], BF16), "vmask": ([128, 32], F32), "wband4": ([128, 8, 512], BF16), "cmpb0": ([128, 512], BF16),
}

IN_SHAPES = {
    "xT": [D, S], "xTo": [D, SO], "cT": [128, 8], "w_ada": [D, 6 * D], "b_ada": [1, 6 * D], "n1g": [128, 8],
    "w_in": [D, 3352], "lbl": [128, 2, 4], "hng": [128, 512],
    "peTk": [64, 32], "w1k": [64, 32, 256], "b1k": [128, 2], "w2k": [128, 2, 64],
    "peTv": [64, 32], "w1v": [64, 32, 256], "b1v": [128, 2], "w2v": [128, 2, 64],
    "w_out": [D, D], "n2g": [128, 8], "rw": [128, 8, 64], "rbias": [128, 64],
    "wgu": [65, D, 512], "wdn": [65, 256, D], "fg": [128, 8],
}


class _SkipNSA(Exception):
    pass


class _NSAScope(ExitStack):
    def __exit__(self, et, ev, tb):
        super().__exit__(None, None, None)
        return et is _SkipNSA


def build(stop_after=None, dbg=(), with_moe=True, enable_nsa=True, n_experts=65):
    nc = bass.Bass("TRN2", target_bir_lowering=False)
    I = {}
    for k, shp in IN_SHAPES.items():
        if not with_moe and k in ("wgu", "wdn"):
            continue
        I[k] = nc.dram_tensor(k, list(shp), F32, kind="ExternalInput").ap()
    for k, (shp, dt) in CONST_SHAPES.items():
        I[k] = nc.dram_tensor(k, list(shp), dt, kind="ExternalInput").ap()
    outT = nc.dram_tensor("outT", [D, SO], F32, kind="ExternalOutput").ap()
    dbg_out = {}
    with ExitStack() as es:
        kb = KB(nc, es)
        E = es.enter_context

        uid = [0]

        def sb(name, shape, dt=F32, stack=None):
            uid[0] += 1
            return (stack or es).enter_context(nc.sbuf_tensor(f"sb{uid[0]}_" + name, list(shape), dt))

        ps = [E(nc.psum_tensor(f"ps{i}", [128, 512], F32)) for i in range(8)]
        PS = [Buf(p[:], excl=True) for p in ps]

        def dump(name, ap, shape, dt=F32):
            t = nc.dram_tensor("dbg_" + name, list(shape), dt, kind="ExternalOutput").ap()
            dbg_out[name] = t
            kb.barrier()
            kb.dma("sync", t, ap)
            kb.barrier()

        ident = sb("ident", [128, 128], BF16)
        onesf = sb("onesf", [128, 128], F32)
        isel0 = sb("isel0", [128, 128], BF16)
        isel1 = sb("isel1", [128, 128], BF16)
        pswap = sb("pswap", [128, 128], BF16)
        hmask = sb("hmask", [128, 128], BF16)
        segm = sb("segm", [128, 512], F32)
        CST = Buf(ident[:])
        for nm, t in (("ident", ident), ("onesf", onesf), ("isel0", isel0), ("isel1", isel1), ("pswap", pswap),
                      ("hmask", hmask), ("segm", segm)):
            kb.dma("sync", t[:], I[nm], outs=[CST])
        modcol = sb("modcol", [128, 48], F32)
        a1 = sb("a1", [128, 8], F32)
        a2 = sb("a2", [128, 8], F32)
        MOD = Buf(modcol[:])
        oT = sb("oT", [128, 8, SO], BF16)
        OT = [[Buf(oT[:, j, s * 128:(s + 1) * 128]) for s in range(16)] for j in range(8)]

        with ExitStack() as p0:
            cT = sb("cT", [128, 8], F32, p0)
            cs = sb("cs", [128, 8], F32, p0)
            bada = sb("bada", [1, 6 * D], F32, p0)
            modrow = sb("modrow", [1, 6 * D], F32, p0)
            one1 = sb("one1", [1, 1], F32, p0)
            n1g = sb("n1g", [128, 8], F32, p0)
            n2g = sb("n2g", [128, 8], F32, p0)
            wab = [sb(f"wab{i}", [128, 8, 512], F32, p0) for i in range(2)]
            WAB = [Buf(w[:]) for w in wab]
            SM = Buf(cT[:])
            MR = Buf(modrow[:])
            kb.dma("sync", cT[:], I["cT"], outs=[SM])
            kb.dma("sync", bada[:], I["b_ada"], outs=[SM])
            kb.dma("sync", n1g[:], I["n1g"], outs=[SM])
            kb.dma("sync", n2g[:], I["n2g"], outs=[SM])
            kb.op("vector", lambda e: e.memset(one1[:], 1.0), outs=[SM])
            kb.op("scalar", lambda e: e.activation(out=cs[:], in_=cT[:], func=AF.Silu), outs=[SM], ins=[SM])
            wada_v = I["w_ada"].rearrange("(k p) c -> p k c", p=128)
            for cb in range(12):
                W = WAB[cb % 2]
                kb.dma("sync" if cb % 2 == 0 else "gpsimd", wab[cb % 2][:], wada_v[:, :, cb * 512:(cb + 1) * 512], outs=[W])
                P = PS[cb % 2]
                for k in range(8):
                    kb.op("tensor", lambda e, k=k, cb=cb: e.matmul(ps[cb % 2][0:1, :], lhsT=cs[:, k:k + 1], rhs=wab[cb % 2][:, k, :],
                                                                 start=(k == 0), stop=(k == 7)),
                          outs=[P], ins=[SM, W], mark=(k == 7))
                kb.op("vector", lambda e, cb=cb: e.tensor_tensor(out=modrow[0:1, cb * 512:(cb + 1) * 512], in0=ps[cb % 2][0:1, :],
                                                                  in1=bada[0:1, cb * 512:(cb + 1) * 512], op=ALU.add),
                      outs=[MR], ins=[P, SM])
            P = PS[2]
            for j in range(48):
                kb.op("tensor", lambda e, j=j: e.matmul(ps[2][:, j:j + 1], lhsT=modrow[0:1, j * 128:(j + 1) * 128], rhs=one1[0:1, 0:1],
                                                       start=True, stop=True), outs=[P], ins=[MR, SM], mark=(j == 47))
            kb.op("vector", lambda e: e.tensor_copy(out=modcol[:], in_=ps[2][:, 0:48]), outs=[MOD], ins=[P])
            kb.op("vector", lambda e: e.scalar_tensor_tensor(out=a1[:], in0=modcol[:, 8:16], scalar=1.0, in1=n1g[:], op0=ALU.add, op1=ALU.mult),
                  outs=[MOD], ins=[MOD, SM])
            kb.op("vector", lambda e: e.scalar_tensor_tensor(out=a2[:], in0=modcol[:, 32:40], scalar=1.0, in1=n2g[:], op0=ALU.add, op1=ALU.mult),
                  outs=[MOD], ins=[MOD, SM])
            if "mod" in dbg:
                dump("mod", modcol[:], [128, 48])
            kb.barrier()
        sh1 = lambda k: modcol[:, k:k + 1]
        g1c = lambda k: modcol[:, 16 + k:17 + k]
        sh2 = lambda k: modcol[:, 24 + k:25 + k]
        g2c = lambda k: modcol[:, 40 + k:41 + k]

        if stop_after == "p0":
            kb.barrier()
            return nc, dbg_out

        with ExitStack() as p1:
            hT = sb("hT", [128, 8, S], BF16, p1)
            HT = [Buf(hT[:, :, n * 512:(n + 1) * 512]) for n in range(NB)]
            with ExitStack() as p1a:
                xb = [sb(f"xb{i}", [128, 8, 512], F32, p1a) for i in range(2)]
                XB = [Buf(t[:]) for t in xb]
                sq = sb("sq", [128, 8, 512], F32, p1a)
                SQ = Buf(sq[:])
                rstd = sb("rstd", [128, 512], F32, p1a)
                RS = Buf(rstd[:])
                tmp = [sb(f"tmp{i}", [128, 512], F32, p1a) for i in range(2)]
                TMP = [Buf(t[:]) for t in tmp]
                xT_v = I["xT"].rearrange("(k p) t -> p k t", p=128)
                for n in range(NB):
                    X = XB[n % 2]
                    x_ = xb[n % 2]
                    kb.dma("sync" if n % 2 == 0 else "gpsimd", x_[:], xT_v[:, :, n * 512:(n + 1) * 512], outs=[X])
                    kb.op("scalar", lambda e, x_=x_: e.activation(out=sq[:], in_=x_[:], func=AF.Square), outs=[SQ], ins=[X])
                    P = PS[n % 2]
                    for k in range(8):
                        kb.op("tensor", lambda e, k=k, n=n: e.matmul(ps[n % 2][:], lhsT=onesf[:], rhs=sq[:, k, :], start=(k == 0), stop=(k == 7)),
                              outs=[P], ins=[SQ, CST], mark=(k == 7))
                    kb.op("scalar", lambda e, n=n: e.activation(out=rstd[:], in_=ps[n % 2][:], func=AF.Sqrt, scale=1.0 / D, bias=EPS),
                          outs=[RS], ins=[P])
                    kb.op("vector", lambda e: e.reciprocal(out=rstd[:], in_=rstd[:]), outs=[RS], ins=[RS])
                    for k in range(8):
                        T = TMP[k % 2]
                        t_ = tmp[k % 2]
                        kb.op("vector", lambda e, k=k, t_=t_, x_=x_: e.tensor_tensor(out=t_[:], in0=x_[:, k, :], in1=rstd[:], op=ALU.mult),
                              outs=[T], ins=[X, RS])
                        kb.op("scalar", lambda e, k=k, t_=t_, n=n: e.activation(out=hT[:, k, n * 512:(n + 1) * 512], in_=t_[:], func=AF.Identity,
                                                                            scale=a1[:, k:k + 1], bias=sh1(k)),
                              outs=[HT[n]], ins=[T, MOD])
                kb.barrier()
            if "hT" in dbg:
                dump("hT", hT[:], [128, 8, S], BF16)
            if stop_after == "p1a":
                kb.barrier()
                return nc, dbg_out

            rr = [0]

            def bank():
                rr[0] = (rr[0] + 1) % 8
                return rr[0]

            w_in_v = I["w_in"].rearrange("(k p) c -> p k c", p=128)

            with ExitStack() as ph:
                lbl = sb("lbl", [128, 2, 4], F32, ph)
                lb = sb("lb", [128, 4], F32, ph)
                oml = sb("oml", [128, 4], F32, ph)
                hng = sb("hng", [128, 512], F32, ph)
                HC = Buf(lbl[:])
                kb.dma("sync", lbl[:], I["lbl"], outs=[HC])
                kb.dma("sync", hng[:], I["hng"], outs=[HC])
                kb.op("vector", lambda e: e.tensor_tensor(out=lb[:], in0=lbl[:, 0, :], in1=lbl[:, 1, :], op=ALU.subtract), outs=[HC], ins=[HC])
                kb.op("scalar", lambda e: e.activation(out=lb[:], in_=lb[:], func=AF.Sigmoid), outs=[HC], ins=[HC])
                kb.op("vector", lambda e: e.tensor_scalar(out=oml[:], in0=lb[:], scalar1=-1.0, scalar2=1.0, op0=ALU.mult, op1=ALU.add), outs=[HC], ins=[HC])
                wq = sb("wq", [128, 8, 128], BF16, ph)
                wf = sb("wf", [128, 8, 128], BF16, ph)
                wig = sb("wig", [128, 8, 256], BF16, ph)
                WQ, WF, WIG = Buf(wq[:]), Buf(wf[:]), Buf(wig[:])
                Q1 = sb("Q1", [128, S], BF16, ph)
                Q2 = sb("Q2", [128, S], BF16, ph)
                Kt = sb("Kt", [128, S], BF16, ph)
                Kh = sb("Kh", [128, NT, 128], BF16, ph)
                Vh = sb("Vh", [128, NT, 128], BF16, ph)
                SGt = sb("SGt", [128, NT, 128], BF16, ph)
                ebl = sb("ebl", [128, NT], F32, ph)
                BQ = [Buf(Q1[:, n * 512:(n + 1) * 512]) for n in range(NB)]
                BKH = [Buf(Kh[:, t, :]) for t in range(NT)]
                BV = [Buf(Vh[:, t, :]) for t in range(NT)]
                tn = ["f", "lf", "b", "d1", "d2", "eb", "e1", "en1", "el", "k"]
                T2 = [{n_: sb(f"t{i}_" + n_, [128, 512], F32, ph) for n_ in tn} for i in range(2)]
                TB2 = [{n_: Buf(T2[i][n_][:]) for n_ in tn} for i in range(2)]
                khtb2 = [sb(f"khtb{i}", [128, 512], BF16, ph) for i in range(2)]
                KHTB2 = [Buf(t[:]) for t in khtb2]
                vmask = sb("vmask", [128, 32], F32, ph)
                kb.dma("sync", vmask[:], I["vmask"], outs=[HC])
                Sst = sb("Sst", [128, 128], F32, ph)
                SST = Buf(Sst[:])
                sbf = [sb(f"sbf{i}", [128, 128], BF16, ph) for i in range(2)]
                SBF = [Buf(t[:]) for t in sbf]
                atm = [sb(f"atm{i}", [128, 128], BF16, ph) for i in range(2)]
                ATM = [Buf(t[:]) for t in atm]
                for i in range(2):
                    kb.op("vector", lambda e, i=i: e.memset(atm[i][:], 0.0), outs=[ATM[i]])
                junk = sb("junk", [128, 128], F32, ph)
                JK = Buf(junk[:])
                ssq = [sb(f"ssq{i}", [128, 1], F32, ph) for i in range(2)]
                SSQ = [Buf(t[:]) for t in ssq]
                of = [sb(f"of{i}", [128, 128], F32, ph) for i in range(2)]
                OF = [Buf(t[:]) for t in of]
                obf = [sb(f"obf{i}", [128, 128], BF16, ph) for i in range(2)]
                OBF = [Buf(t[:]) for t in obf]
                v4 = lambda ap: ap.rearrange("p (c t) -> p c t", t=128)
                for hd in range(int(_os0.environ.get("NHEADS", "4"))):
                    c0 = hd * 128
                    kb.dma("gpsimd", wq[:], w_in_v[:, :, c0:c0 + 128], outs=[WQ])
                    kb.dma("gpsimd", wf[:], w_in_v[:, :, 512 + c0:512 + c0 + 128], outs=[WF])
                    kb.dma("gpsimd", wig[:, :, 0:128], w_in_v[:, :, 1024 + c0:1024 + c0 + 128], outs=[WIG])
                    kb.dma("gpsimd", wig[:, :, 128:256], w_in_v[:, :, 1536 + c0:1536 + c0 + 128], outs=[WIG])
                    for n in range(NB):
                        sl = slice(n * 512, (n + 1) * 512)
                        own = n >= 4
                        bq_, bf_ = bank(), bank()
                        if own:
                            for k in range(8):
                                kb.op("tensor", lambda e, k=k, bq_=bq_, sl=sl: e.matmul(ps[bq_][:], lhsT=wq[:, k, :], rhs=hT[:, k, sl], start=(k == 0), stop=(k == 7)),
                                      outs=[PS[bq_]], ins=[WQ, HT[n]], mark=(k == 7))
                        for k in range(8):
                            kb.op("tensor", lambda e, k=k, bf_=bf_, sl=sl: e.matmul(ps[bf_][:], lhsT=wf[:, k, :], rhs=hT[:, k, sl], start=(k == 0), stop=(k == 7)),
                                  outs=[PS[bf_]], ins=[WF, HT[n]], mark=(k == 7))
                        t = T2[n % 2]
                        TB = TB2[n % 2]
                        khtb = khtb2[n % 2]
                        KHTB = KHTB2[n % 2]
                        kb.op("scalar", lambda e, bf_=bf_: e.activation(out=t["f"][:], in_=ps[bf_][:], func=AF.Sigmoid), outs=[TB["f"]], ins=[PS[bf_]])
                        kb.op("vector", lambda e, hd=hd: e.tensor_scalar(out=t["f"][:], in0=t["f"][:], scalar1=oml[:, hd:hd + 1], scalar2=lb[:, hd:hd + 1],
                                                                     op0=ALU.mult, op1=ALU.add), outs=[TB["f"]], ins=[TB["f"], HC])
                        kb.op("scalar", lambda e: e.activation(out=t["lf"][:], in_=t["f"][:], func=AF.Ln), outs=[TB["lf"]], ins=[TB["f"]])
                        kb.op("gpsimd", lambda e: e.tensor_scalar(out=t["k"][:], in0=t["f"][:], scalar1=-1.0, scalar2=1.0, op0=ALU.mult, op1=ALU.add),
                              outs=[TB["k"]], ins=[TB["f"]])
                        kb.op("vector", lambda e: e.tensor_tensor_scan(out=t["b"][:], data0=segm[:], data1=t["lf"][:], initial=0.0, op0=ALU.mult, op1=ALU.add),
                              outs=[TB["b"]], ins=[TB["lf"], CST])
                        if own:
                            kb.op("vector", lambda e: e.tensor_tensor(out=v4(t["d1"][:]), in0=v4(t["b"][:]), in1=v4(t["b"][:])[:, :, 63:64].to_broadcast([128, 4, 128]),
                                                                      op=ALU.subtract), outs=[TB["d1"]], ins=[TB["b"]])
                        kb.op("vector", lambda e: e.tensor_tensor(out=v4(t["d2"][:]), in0=v4(t["b"][:])[:, :, 127:128].to_broadcast([128, 4, 128]), in1=v4(t["b"][:]),
                                                                  op=ALU.subtract), outs=[TB["d2"]], ins=[TB["b"]])
                        kb.op("scalar", lambda e: e.activation(out=t["eb"][:], in_=t["b"][:], func=AF.Exp), outs=[TB["eb"]], ins=[TB["b"]])
                        if own:
                            kb.op("scalar", lambda e: e.activation(out=t["e1"][:], in_=t["d1"][:], func=AF.Exp), outs=[TB["e1"]], ins=[TB["d1"]])
                            kb.op("scalar", lambda e: e.activation(out=t["en1"][:], in_=t["d1"][:], func=AF.Exp, scale=-1.0), outs=[TB["en1"]], ins=[TB["d1"]])
                        kb.op("scalar", lambda e: e.activation(out=t["el"][:], in_=t["d2"][:], func=AF.Exp), outs=[TB["el"]], ins=[TB["d2"]])
                        sc_ = 128.0 ** -0.5
                        if own:
                            kb.op("vector", lambda e, bq_=bq_, sl=sl: e.scalar_tensor_tensor(out=Q1[:, sl], in0=ps[bq_][:], scalar=sc_, in1=t["e1"][:], op0=ALU.mult, op1=ALU.mult),
                                  outs=[BQ[n]], ins=[PS[bq_], TB["e1"]])
                            kb.op("vector", lambda e, bq_=bq_, sl=sl: e.scalar_tensor_tensor(out=Q2[:, sl], in0=ps[bq_][:], scalar=sc_, in1=t["eb"][:], op0=ALU.mult, op1=ALU.mult),
                                  outs=[BQ[n]], ins=[PS[bq_], TB["eb"]])
                            kb.op("gpsimd", lambda e, sl=sl: e.tensor_tensor(out=Kt[:, sl], in0=t["k"][:], in1=t["en1"][:], op=ALU.mult), outs=[BQ[n]], ins=[TB["k"], TB["en1"]])
                        kb.op("gpsimd", lambda e: e.tensor_tensor(out=khtb[:], in0=t["k"][:], in1=t["el"][:], op=ALU.mult), outs=[KHTB], ins=[TB["k"], TB["el"]])
                        kb.op("gpsimd", lambda e, n=n: e.tensor_copy(out=ebl[:, 4 * n:4 * n + 4], in_=v4(t["eb"][:])[:, :, 127]), outs=[BQ[n]], ins=[TB["eb"]])
                        for tt in range(4 * n, 4 * n + 4):
                            bk = bank()
                            for k in range(8):
                                kb.op("tensor", lambda e, k=k, bk=bk, tt=tt: e.matmul(ps[bk][:, 0:256], lhsT=hT[:, k, tt * 128:(tt + 1) * 128], rhs=wig[:, k, :],
                                                                                     start=(k == 0), stop=(k == 7)),
                                      outs=[PS[bk]], ins=[WIG, HT[n]], mark=(k == 7))
                            kb.op("vector", lambda e, bk=bk, tt=tt: e.tensor_scalar(out=Vh[:, tt, :], in0=ps[bk][:, 0:128], scalar1=vmask[:, tt:tt + 1], scalar2=None, op0=ALU.mult),
                                  outs=[BV[tt]], ins=[PS[bk], HC])
                            kb.op("scalar", lambda e, bk=bk, tt=tt: e.activation(out=SGt[:, tt, :], in_=ps[bk][:, 128:256], func=AF.Silu), outs=[BV[tt]], ins=[PS[bk]])
                        for i in range(4):
                            bk = bank()
                            kb.op("tensor", lambda e, i=i, bk=bk: e.matmul(ps[bk][:, 0:128], lhsT=khtb[:, i * 128:(i + 1) * 128], rhs=ident[:], start=True, stop=True),
                                  outs=[PS[bk]], ins=[KHTB, CST])
                            kb.op("scalar", lambda e, i=i, bk=bk, n=n: e.copy(out=Kh[:, 4 * n + i, :], in_=ps[bk][:, 0:128]), outs=[BKH[4 * n + i]], ins=[PS[bk]])
                    kb.op("vector", lambda e: e.memset(Sst[:], 0.0), outs=[SST])
                    at_bank = {}

                    def emit_at(c):
                        bk = bank()
                        at_bank[c] = bk
                        cs_ = slice(c * 128, (c + 1) * 128)
                        c0_ = c * 128
                        kb.op("tensor", lambda e: e.matmul(ps[bk][0:64, 0:64], lhsT=Kt[:, c0_:c0_ + 64], rhs=Q1[:, c0_:c0_ + 64], start=True, stop=True),
                              outs=[PS[bk]], ins=[BQ[c // 4]], mark=False)
                        kb.op("tensor", lambda e: e.matmul(ps[bk][:, 64:128], lhsT=Kt[:, cs_], rhs=Q1[:, c0_ + 64:c0_ + 128], start=True, stop=True),
                              outs=[PS[bk]], ins=[BQ[c // 4]])
                        kb.op("vector", lambda e: e.tensor_tensor(out=atm[c % 2][0:64, 0:64], in0=ps[bk][0:64, 0:64], in1=hmask[0:64, 0:64], op=ALU.mult),
                              outs=[ATM[c % 2]], ins=[PS[bk], CST])
                        kb.op("vector", lambda e: e.tensor_tensor(out=atm[c % 2][:, 64:128], in0=ps[bk][:, 64:128], in1=hmask[:, 64:128], op=ALU.mult),
                              outs=[ATM[c % 2]], ins=[PS[bk], CST])

                    for c in range(NT):
                        if c + 1 < NT and c + 1 >= 16:
                            emit_at(c + 1)
                        cs_ = slice(c * 128, (c + 1) * 128)
                        bd = bank()
                        kb.op("tensor", lambda e, bd=bd, c=c: e.matmul(ps[bd][:, 0:128], lhsT=Kh[:, c, :], rhs=Vh[:, c, :], start=True, stop=True),
                              outs=[PS[bd]], ins=[BKH[c], BV[c]])
                        if c >= 16:
                            bo = bank()
                            kb.op("tensor", lambda e, bo=bo, c=c: e.matmul(ps[bo][:, 0:128], lhsT=atm[c % 2][:], rhs=Vh[:, c, :], start=True, stop=False),
                                  outs=[PS[bo]], ins=[ATM[c % 2], BV[c]], mark=False)
                            kb.op("tensor", lambda e, bo=bo, c=c, cs_=cs_: e.matmul(ps[bo][:, 0:128], lhsT=Q2[:, cs_], rhs=sbf[(c - 1) % 2][:], start=False, stop=True),
                                  outs=[PS[bo]], ins=[BQ[c // 4], SBF[(c - 1) % 2]])
                        if c + 1 < NT:
                            kb.op("vector", lambda e, bd=bd, c=c: e.scalar_tensor_tensor(out=Sst[:], in0=Sst[:], scalar=ebl[:, c:c + 1], in1=ps[bd][:, 0:128],
                                                                                     op0=ALU.mult, op1=ALU.add), outs=[SST], ins=[SST, PS[bd], BQ[c // 4]])
                            if c >= 15:
                                kb.op("scalar", lambda e, c=c: e.copy(out=sbf[c % 2][:], in_=Sst[:]), outs=[SBF[c % 2]], ins=[SST])
                        if c < 16:
                            continue
                        i2 = c % 2
                        kb.op("gpsimd", lambda e, i2=i2: e.memset(ssq[i2][:], 0.0), outs=[SSQ[i2]])
                        kb.op("scalar", lambda e, bo=bo, i2=i2: e.activation(out=junk[:], in_=ps[bo][:, 0:128], func=AF.Square, accum_out=ssq[i2][:]),
                              outs=[JK, SSQ[i2]], ins=[PS[bo]])
                        kb.op("scalar", lambda e, i2=i2: e.activation(out=ssq[i2][:], in_=ssq[i2][:], func=AF.Sqrt, scale=1.0 / 128, bias=EPS), outs=[SSQ[i2]], ins=[SSQ[i2]])
                        kb.op("vector", lambda e, i2=i2: e.reciprocal(out=ssq[i2][:], in_=ssq[i2][:]), outs=[SSQ[i2]], ins=[SSQ[i2]])
                        kb.op("vector", lambda e, bo=bo, i2=i2, c0=c0: e.scalar_tensor_tensor(out=of[i2][:], in0=ps[bo][:, 0:128], scalar=ssq[i2][:, 0:1], in1=hng[:, c0:c0 + 128],
                                                                                           op0=ALU.mult, op1=ALU.mult), outs=[OF[i2]], ins=[PS[bo], SSQ[i2], HC])
                        kb.op("gpsimd", lambda e, i2=i2, c=c: e.tensor_tensor(out=obf[i2][:], in0=of[i2][:], in1=SGt[:, c, :], op=ALU.mult), outs=[OBF[i2]], ins=[OF[i2], BV[c]])
                        bt = bank()
                        kb.op("tensor", lambda e, bt=bt, i2=i2: e.matmul(ps[bt][:, 0:128], lhsT=obf[i2][:], rhs=ident[:], start=True, stop=True),
                              outs=[PS[bt]], ins=[OBF[i2], CST])
                        s_ = c - 16
                        kb.op("scalar", lambda e, bt=bt, s_=s_, hd=hd: e.copy(out=oT[:, hd, s_ * 128:(s_ + 1) * 128], in_=ps[bt][:, 0:128]), outs=[OT[hd][s_]], ins=[PS[bt]])
                kb.barrier()
            if "oT" in dbg:
                dump("oT", oT[:], [128, 8, SO], BF16)
            if stop_after == "p1b":
                kb.barrier()
                return nc, dbg_out

            SCL = 64.0 ** -0.5
            if not enable_nsa:
                for jf in range(4, 8):
                    kb.op("vector", lambda e, jf=jf: e.memset(oT[:, jf, :], 0.0), outs=OT[jf])
            with _NSAScope() as pn:
                if not enable_nsa:
                    raise _SkipNSA()
                ovl = sb("ovl", [128, 2, 64], BF16, pn)
                kb.dma("sync", ovl[:], I["ovl"], outs=[CST])
                KEe = sb("KEe", [128, S], BF16, pn)
                KEo = sb("KEo", [128, S], BF16, pn)
                ekt_v = I["ekt"].rearrange("p a b -> p (a b)")
                kb.dma("sync", KEe[64:128, :], ekt_v[64:128, :], outs=[CST])
                kb.dma("sync", KEo[0:64, :], ekt_v[0:64, :], outs=[CST])
                kwT = sb("kwT", [128, S], BF16, pn)
                kcvT = sb("kcvT", [128, S], BF16, pn)
                vs1 = sb("vs1", [128, NT, 80], BF16, pn)
                vw1 = sb("vw1", [128, NT, 80], BF16, pn)
                KS = Buf(kwT[:])
                kcmpT = sb("kcmpT", [128, 256], BF16, pn)
                vcmp1 = sb("vcmp1", [128, 2, 144], BF16, pn)
                KC = Buf(kcmpT[:])
                wk3 = sb("wk3", [128, 8, 384], BF16, pn)
                wv2 = sb("wv2", [128, 8, 128], BF16, pn)
                wqg = sb("wqg", [128, 8, 256], BF16, pn)
                wgt = sb("wgt", [128, 8, 12], BF16, pn)
                WN = Buf(wk3[:])
                cosb = sb("cosb", [128, 512], F32, pn)
                sinb = sb("sinb", [128, 512], F32, pn)
                CSB = Buf(cosb[:])
                rawb = sb("rawb", [128, 512], BF16, pn)
                RAWB = Buf(rawb[:])
                rt1 = sb("rt1", [128, 512], F32, pn)
                rt2 = sb("rt2", [128, 512], F32, pn)
                RT1, RT2 = Buf(rt1[:]), Buf(rt2[:])
                _padn = int(_os0.environ.get("PADN", "0"))
                if _padn:
                    _pad = sb("padn", [128, _padn], F32, pn)
                srr = [0]

                def sbank():
                    srr[0] = (srr[0] + 1) % 3
                    return srr[0]

                mrr = [0]

                def mbank():
                    return 7

                import os as _os
                _dbgmode = int(_os.environ.get("ROPEDBG", "0"))

                def rope_from(bk, dst_ap, dstbuf):
                    if _dbgmode == 1:
                        kb.op("scalar", lambda e: e.copy(out=dst_ap, in_=ps[bk][:]), outs=[dstbuf], ins=[PS[bk]])
                        return
                    if _dbgmode == 3:
                        kb.op("vector", lambda e: e.tensor_tensor(out=rt1[:], in0=ps[bk][:], in1=cosb[:], op=ALU.mult), outs=[RT1], ins=[PS[bk], CSB])
                        kb.op("gpsimd", lambda e: e.tensor_copy(out=dst_ap, in_=rt1[:]), outs=[dstbuf], ins=[RT1])
                        return
                    if _dbgmode == 4:
                        kb.op("scalar", lambda e: e.copy(out=rawb[:], in_=ps[bk][:]), outs=[RAWB], ins=[PS[bk]])
                        b2 = mbank()
                        kb.op("tensor", lambda e: e.matmul(ps[b2][:], lhsT=pswap[:], rhs=rawb[:], start=True, stop=True), outs=[PS[b2]], ins=[RAWB, CST])
                        kb.op("vector", lambda e: e.tensor_tensor(out=rt1[:], in0=ps[bk][:], in1=cosb[:], op=ALU.mult), outs=[RT1], ins=[PS[bk], CSB])
                        kb.op("vector", lambda e: e.tensor_tensor(out=rt2[:], in0=ps[b2][:], in1=sinb[:], op=ALU.mult), outs=[RT2], ins=[PS[b2], CSB])
                        kb.op("vector", lambda e: e.tensor_tensor(out=dst_ap, in0=rt1[:], in1=rt2[:], op=ALU.add), outs=[dstbuf], ins=[RT1, RT2])
                        return
                    if _dbgmode == 5:
                        kb.op("scalar", lambda e: e.copy(out=rawb[:], in_=ps[bk][:]), outs=[RAWB], ins=[PS[bk]])
                        b2 = mbank()
                        kb.op("tensor", lambda e: e.matmul(ps[b2][:], lhsT=pswap[:], rhs=rawb[:], start=True, stop=True), outs=[PS[b2]], ins=[RAWB, CST])
                        kb.op("vector", lambda e: e.tensor_tensor(out=rt1[:], in0=ps[bk][:], in1=cosb[:], op=ALU.mult), outs=[RT1], ins=[PS[bk], CSB])
                        kb.op("scalar", lambda e: e.copy(out=rt2[:], in_=ps[b2][:]), outs=[RT2], ins=[PS[b2]])
                        _sb = cosb if _os.environ.get("USECOS") else sinb
                        kb.op("vector", lambda e: e.tensor_tensor(out=rt2[:], in0=rt2[:], in1=_sb[:], op=ALU.mult), outs=[RT2], ins=[RT2, CSB])
                        kb.op("vector", lambda e: e.tensor_tensor(out=dst_ap, in0=rt1[:], in1=rt2[:], op=ALU.add), outs=[dstbuf], ins=[RT1, RT2])
                        return
                    if _dbgmode in (7, 8):
                        kb.op("scalar", lambda e: e.copy(out=rawb[:], in_=ps[bk][:]), outs=[RAWB], ins=[PS[bk]])
                        b2 = mbank()
                        kb.op("tensor", lambda e: e.matmul(ps[b2][:], lhsT=pswap[:], rhs=rawb[:], start=True, stop=True), outs=[PS[b2]], ins=[RAWB, CST])
                        kb.op("vector", lambda e: e.tensor_tensor(out=rt1[:], in0=ps[bk][:], in1=cosb[:], op=ALU.mult), outs=[RT1], ins=[PS[bk], CSB])
                        kb.op("scalar", lambda e: e.copy(out=rt2[:], in_=ps[b2][:]), outs=[RT2], ins=[PS[b2]])
                        kb.op("vector", lambda e: e.tensor_tensor(out=rt2[:], in0=rt2[:], in1=sinb[:], op=ALU.mult), outs=[RT2], ins=[RT2, CSB])
                        if _dbgmode == 8:
                            kb.op("vector", lambda e: e.tensor_tensor(out=rt1[:], in0=rt1[:], in1=rt2[:], op=ALU.add), outs=[RT1], ins=[RT1, RT2])
                        kb.op("scalar", lambda e: e.copy(out=dst_ap, in_=rt1[:]), outs=[dstbuf], ins=[RT1])
                        return
                    if _dbgmode in (9, 10):
                        kb.op("vector", lambda e: e.tensor_tensor(out=rt1[:], in0=ps[bk][:], in1=cosb[:], op=ALU.mult), outs=[RT1], ins=[PS[bk], CSB])
                        if _dbgmode == 9:
                            kb.op("scalar", lambda e: e.copy(out=rt2[:], in_=ps[bk][:]), outs=[RT2], ins=[PS[bk]])
                        else:
                            kb.op("vector", lambda e: e.tensor_tensor(out=rt2[:], in0=rt1[:], in1=cosb[:], op=ALU.mult), outs=[RT2], ins=[RT1, CSB])
                        kb.op("gpsimd", lambda e: e.tensor_copy(out=dst_ap, in_=rt1[:]), outs=[dstbuf], ins=[RT1])
                        return
                    if _dbgmode in (11, 12):
                        kb.op("scalar", lambda e: e.copy(out=rawb[:], in_=ps[bk][:]), outs=[RAWB], ins=[PS[bk]])
                        b2 = mbank()
                        kb.op("tensor", lambda e: e.matmul(ps[b2][:], lhsT=pswap[:], rhs=rawb[:], start=True, stop=True), outs=[PS[b2]], ins=[RAWB, CST])
                        kb.op("vector", lambda e: e.tensor_tensor(out=rt1[:], in0=ps[bk][:], in1=cosb[:], op=ALU.mult), outs=[RT1], ins=[PS[bk], CSB, RAWB])
                        kb.op("gpsimd", lambda e: e.tensor_copy(out=dst_ap, in_=rt1[:]), outs=[dstbuf], ins=[RT1])
                        if _dbgmode == 12:
                            return
                        kb.op("vector", lambda e: e.tensor_tensor(out=rt1[:], in0=ps[b2][:], in1=sinb[:], op=ALU.mult), outs=[RT1], ins=[PS[b2], CSB])
                        kb.op("gpsimd", lambda e: e.tensor_tensor(out=dst_ap, in0=dst_ap, in1=rt1[:], op=ALU.add), outs=[dstbuf], ins=[RT1, dstbuf])
                        return
                    if _dbgmode == 2:
                        kb.op("scalar", lambda e: e.copy(out=rawb[:], in_=ps[bk][:]), outs=[RAWB], ins=[PS[bk]])
                        b2 = mbank()
                        kb.op("tensor", lambda e: e.matmul(ps[b2][:], lhsT=pswap[:], rhs=rawb[:], start=True, stop=True), outs=[PS[b2]], ins=[RAWB, CST])
                        kb.op("scalar", lambda e: e.copy(out=dst_ap, in_=ps[b2][:]), outs=[dstbuf], ins=[PS[b2]])
                        return
                    kb.op("scalar", lambda e: e.copy(out=rawb[:], in_=ps[bk][:]), outs=[RAWB], ins=[PS[bk]])
                    b2 = mbank()
                    kb.op("tensor", lambda e: e.matmul(ps[b2][:], lhsT=pswap[:], rhs=rawb[:], start=True, stop=True), outs=[PS[b2]], ins=[RAWB, CST])
                    kb.op("vector", lambda e: e.tensor_tensor(out=rt1[:], in0=ps[bk][:], in1=cosb[:], op=ALU.mult), outs=[RT1], ins=[PS[bk], CSB])
                    kb.op("vector", lambda e: e.tensor_tensor(out=rt2[:], in0=ps[b2][:], in1=sinb[:], op=ALU.mult), outs=[RT2], ins=[PS[b2], CSB])
                    if isinstance(dst_ap, tuple):
                        kb.op("gpsimd", lambda e: e.tensor_tensor(out=dst_ap[0], in0=rt1[0:64, :], in1=rt2[0:64, :], op=ALU.add), outs=[dstbuf], ins=[RT1, RT2])
                        kb.op("gpsimd", lambda e: e.tensor_tensor(out=dst_ap[1], in0=rt1[64:128, :], in1=rt2[64:128, :], op=ALU.add), outs=[dstbuf], ins=[RT1, RT2])
                    else:
                        kb.op("gpsimd", lambda e: e.tensor_tensor(out=dst_ap, in0=rt1[:], in1=rt2[:], op=ALU.add), outs=[dstbuf], ins=[RT1, RT2])

                for g in range(2):
                    for j, cbase in enumerate((2560, 2688)):
                        kb.dma("gpsimd", wk3[:, :, j * 64:(j + 1) * 64], w_in_v[:, :, cbase + g * 64:cbase + g * 64 + 64], outs=[WN])
                    for j, cbase in enumerate((2816, 2816, 3072, 3072)):
                        kb.dma("gpsimd", wk3[:, :, 128 + j * 64:128 + (j + 1) * 64], w_in_v[:, :, cbase + g * 64:cbase + g * 64 + 64], outs=[WN])
                    for j, cbase in enumerate((2944, 3200)):
                        kb.dma("gpsimd", wv2[:, :, j * 64:(j + 1) * 64], w_in_v[:, :, cbase + g * 64:cbase + g * 64 + 64], outs=[WN])
                    kb.dma("gpsimd", wqg[:], w_in_v[:, :, 2048 + g * 256:2048 + (g + 1) * 256], outs=[WN])
                    kb.dma("gpsimd", wgt[:], w_in_v[:, :, 3328 + g * 12:3328 + (g + 1) * 12], outs=[WN])
                    kb.op("vector", lambda e: e.memset(vs1[:, :, 64:65], 1.0), outs=[KS])
                    kb.op("vector", lambda e: e.memset(vw1[:, :, 64:65], 1.0), outs=[KS])
                    if stop_after == "p1c_a":
                        dump("wk3", wk3[:], [128, 8, 384], BF16)
                        return nc, dbg_out
                    for n in range(NB):
                        sl = slice(n * 512, (n + 1) * 512)
                        kb.dma("sync", cosb[:], I["cosT"][:, sl], outs=[CSB])
                        kb.dma("sync", sinb[:], I["sinT"][:, sl], outs=[CSB])
                        for j in range(3):
                            bk = sbank()
                            for k in range(8):
                                kb.op("tensor", lambda e, k=k, bk=bk, j=j: e.matmul(ps[bk][:], lhsT=wk3[:, k, j * 128:(j + 1) * 128], rhs=hT[:, k, sl],
                                                                                    start=(k == 0), stop=(k == 7)), outs=[PS[bk]], ins=[WN, HT[n]], mark=(k == 7))
                            if j == 0:
                                kb.op("scalar", lambda e, bk=bk: e.copy(out=kcvT[:, sl], in_=ps[bk][:]), outs=[KS], ins=[PS[bk]])
                            else:
                                rope_from(bk, (KEe[0:64, sl], KEo[64:128, sl]) if j == 1 else kwT[:, sl], KS)
                        if stop_after == "p1c_b":
                                return nc, dbg_out
                        for i in range(4):
                            tt = 4 * n + i
                            bk = mbank()
                            for k in range(8):
                                kb.op("tensor", lambda e, k=k, bk=bk, tt=tt: e.matmul(ps[bk][:, 0:128], lhsT=hT[:, k, tt * 128:(tt + 1) * 128], rhs=wv2[:, k, :],
                                                                                     start=(k == 0), stop=(k == 7)), outs=[PS[bk]], ins=[WN, HT[n]], mark=(k == 7))
                            kb.op("scalar", lambda e, bk=bk, tt=tt: e.copy(out=vs1[:, tt, 0:64], in_=ps[bk][:, 0:64]), outs=[KS], ins=[PS[bk]])
                            kb.op("vector", lambda e, bk=bk, tt=tt: e.tensor_copy(out=vw1[:, tt, 0:64], in_=ps[bk][:, 64:128]), outs=[KS], ins=[PS[bk]])
                    if stop_after == "p1c_k":
                        dump("kcvT", kcvT[:], [128, S], BF16)
                        dump("vs1", vs1[:], [128, NT, 80], BF16)
                        return nc, dbg_out
                    with ExitStack() as pc:
                        w1kv = sb("w1kv", [128, 32, 256], BF16, pc)
                        peT = sb("peT", [128, 32], F32, pc)
                        peTb = sb("peTb", [128, 32], BF16, pc)
                        b1kv = sb("b1kv", [128, 4], F32, pc)
                        w2k2 = sb("w2k2", [128, 2, 128], BF16, pc)
                        w2v = sb("w2v", [128, 2, 64], BF16, pc)
                        hid = sb("hid", [128, 4, 256], BF16, pc)
                        beff = sb("beff", [128, 4], F32, pc)
                        gx = sb("gx", [128, 256], F32, pc)
                        gu = sb("gu", [128, 256], F32, pc)
                        gs = sb("gs", [128, 256], F32, pc)
                        CW = Buf(w1kv[:])
                        HID = Buf(hid[:])
                        GX = Buf(gx[:])
                        kb.dma("gpsimd", w1kv[0:64], I["w1k"], outs=[CW])
                        kb.dma("gpsimd", w1kv[64:128], I["w1v"], outs=[CW])
                        kb.dma("sync", peT[0:64], I["peTk"], outs=[CW])
                        kb.dma("sync", peT[64:128], I["peTv"], outs=[CW])
                        kb.dma("sync", b1kv[:, 0:2], I["b1k"], outs=[CW])
                        kb.dma("sync", b1kv[:, 2:4], I["b1v"], outs=[CW])
                        kb.dma("gpsimd", w2k2[:, :, 0:64], I["w2k"], outs=[CW])
                        kb.dma("gpsimd", w2k2[:, :, 64:128], I["w2k"], outs=[CW])
                        kb.dma("gpsimd", w2v[:], I["w2v"], outs=[CW])
                        kb.op("vector", lambda e: e.tensor_copy(out=peTb[:], in_=peT[:]), outs=[CW], ins=[CW])
                        kb.op("vector", lambda e: e.memset(hid[:], 0.0), outs=[HID])
                        for kv in range(2):
                            p0_ = kv * 64
                            for hc in range(2):
                                bk, bb = sbank(), mbank()
                                for l in range(32):
                                    kb.op("tensor", lambda e, l=l, bk=bk, hc=hc, p0_=p0_: e.matmul(ps[bk][:, 0:255], lhsT=w1kv[p0_:p0_ + 64, l, hc * 128:(hc + 1) * 128],
                                                                                                rhs=kcvT[p0_:p0_ + 64, l:l + 16 * 254 + 1:16], start=(l == 0), stop=(l == 31)),
                                          outs=[PS[bk]], ins=[CW, KS], mark=(l == 31))
                                for l in range(32):
                                    kb.op("tensor", lambda e, l=l, bb=bb, hc=hc, p0_=p0_: e.matmul(ps[bb][:, 0:1], lhsT=w1kv[p0_:p0_ + 64, l, hc * 128:(hc + 1) * 128],
                                                                                                rhs=peTb[p0_:p0_ + 64, l:l + 1], start=(l == 0), stop=(l == 31)),
                                          outs=[PS[bb]], ins=[CW], mark=(l == 31))
                                ci = kv * 2 + hc
                                kb.op("vector", lambda e, bb=bb, ci=ci: e.tensor_tensor(out=beff[:, ci:ci + 1], in0=ps[bb][:, 0:1], in1=b1kv[:, ci:ci + 1], op=ALU.add),
                                      outs=[GX], ins=[PS[bb], CW])
                                kb.op("vector", lambda e, bk=bk, ci=ci: e.tensor_scalar(out=gx[:, 0:255], in0=ps[bk][:, 0:255], scalar1=beff[:, ci:ci + 1], scalar2=None, op0=ALU.add),
                                      outs=[GX], ins=[PS[bk], GX])
                                kb.op("vector", lambda e: e.tensor_tensor(out=gu[:, 0:255], in0=gx[:, 0:255], in1=gx[:, 0:255], op=ALU.mult), outs=[GX], ins=[GX])
                                kb.op("vector", lambda e: e.tensor_scalar(out=gu[:, 0:255], in0=gu[:, 0:255], scalar1=0.044715, scalar2=1.0, op0=ALU.mult, op1=ALU.add), outs=[GX], ins=[GX])
                                kb.op("vector", lambda e: e.tensor_tensor(out=gu[:, 0:255], in0=gu[:, 0:255], in1=gx[:, 0:255], op=ALU.mult), outs=[GX], ins=[GX])
                                kb.op("scalar", lambda e: e.activation(out=gs[:, 0:255], in_=gu[:, 0:255], func=AF.Sigmoid, scale=1.5957691216057308), outs=[GX], ins=[GX])
                                kb.op("vector", lambda e, ci=ci: e.tensor_tensor(out=hid[:, ci, 0:255], in0=gx[:, 0:255], in1=gs[:, 0:255], op=ALU.mult), outs=[HID], ins=[GX])
                        bk = sbank()
                        for hc in range(2):
                            kb.op("tensor", lambda e, hc=hc, bk=bk: e.matmul(ps[bk][:, 0:256], lhsT=w2k2[:, hc, :], rhs=hid[:, hc, :], start=(hc == 0), stop=(hc == 1)),
                                  outs=[PS[bk]], ins=[CW, HID], mark=(hc == 1))
                        kb.op("scalar", lambda e, bk=bk: e.copy(out=kcmpT[:], in_=ps[bk][:, 0:256]), outs=[KC], ins=[PS[bk]])
                        for ct in range(2):
                            bk = sbank()
                            for hc in range(2):
                                kb.op("tensor", lambda e, hc=hc, bk=bk, ct=ct: e.matmul(ps[bk][:, 0:64], lhsT=hid[:, 2 + hc, ct * 128:(ct + 1) * 128], rhs=w2v[:, hc, :],
                                                                                       start=(hc == 0), stop=(hc == 1)), outs=[PS[bk]], ins=[CW, HID], mark=(hc == 1))
                            kb.op("scalar", lambda e, bk=bk, ct=ct: e.copy(out=vcmp1[:, ct, 0:64], in_=ps[bk][:, 0:64]), outs=[KC], ins=[PS[bk]])
                        kb.op("vector", lambda e: e.memset(vcmp1[:, :, 64:65], 1.0), outs=[KC])
                        kb.op("vector", lambda e: e.tensor_copy(out=vcmp1[:, :, 65:129], in_=ovl[:]), outs=[KC], ins=[CST])
                        kb.barrier()
                    if stop_after == "p1c_c":
                        dump("kcmpT", kcmpT[:], [128, 256], BF16)
                        dump("vcmp1", vcmp1[:], [128, 2, 144], BF16)
                        return nc, dbg_out
                    with ExitStack() as pq:
                        wband = sb("wband", [128, 8, 512], BF16, pq)
                        QC = Buf(wband[:])
                        kb.dma("sync", wband[:], I["wband"], outs=[QC])
                        qn = [[sb(f"qn{ch}{par}", [128, 512], BF16, pq) for par in range(2)] for ch in range(2)]
                        QN = Buf(qn[0][0][:])
                        qTb = sb("qTb", [128, 2, 512], BF16, pq)
                        qrTb = sb("qrTb", [128, 2, 512], BF16, pq)
                        QB_ = Buf(qTb[:])
                        QRB = Buf(qrTb[:])
                        gts = sb("gts", [128, 4, 12], F32, pq)
                        GTS = Buf(gts[:])
                        cmpbb = sb("cmpbb", [128, 512], BF16, pq)
                        CMB = Buf(cmpbb[:])
                        pt = [sb(f"pt{i}", [128, 512], BF16, pq) for i in range(3)]
                        PT = [Buf(t[:]) for t in pt]
                        onsa = sb("onsa", [128, 4, 256], F32, pq)
                        ONSA = Buf(onsa[:])
                        obn = sb("obn", [128, 4, 256], BF16, pq)
                        OBN = Buf(obn[:])
                        pslc = sb("pslc", [128, 4, 64], F32, pq)
                        PSLC = Buf(pslc[:])
                        sadd = sb("sadd", [128, 64], F32, pq)
                        SADD = Buf(sadd[:])
                        score = sb("score", [128, 64], F32, pq)
                        stmp = sb("stmp", [128, 64], F32, pq)
                        sel = sb("sel", [128, 64], F32, pq)
                        m8 = sb("m8", [128, 16], F32, pq)
                        negb4 = [sb(f"negb{i}", [128, 128], BF16, pq) for i in range(4)]
                        SEL = Buf(score[:])
                        negbT = sb("negbT", [128, 512], BF16, pq)
                        NBT = Buf(negbT[:])
                        rz = sb("rz", [128, 4], F32, pq)
                        RZ = Buf(rz[:])
                        accs = [ps[3][:, 0:129], ps[4][:, 0:129], ps[5][:, 0:129], ps[6][:, 0:129]]
                        ACC = [PS[3], PS[4], PS[5], PS[6]]
                        prr = [0]

                        def run_branch(steps):
                            LA = 2
                            n_ = len(steps)
                            for idx_ in range(n_ + LA):
                                if idx_ < n_:
                                    st = steps[idx_]
                                    bk = sbank()
                                    nm = len(st["s"])
                                    for idx, (l_, r_, insb) in enumerate(st["s"]):
                                        kb.op("tensor", lambda e, l_=l_, r_=r_, idx=idx, nm=nm, bk=bk: e.matmul(ps[bk][:], lhsT=l_, rhs=r_, start=(idx == 0), stop=(idx == nm - 1)),
                                              outs=[PS[bk]], ins=insb, mark=(idx == nm - 1))
                                    prr[0] = (prr[0] + 1) % 3
                                    pi = prr[0]
                                    kb.op("scalar", lambda e, bk=bk, pi=pi: e.activation(out=pt[pi][:], in_=ps[bk][:], func=AF.Exp, scale=SCL), outs=[PT[pi]], ins=[PS[bk]])
                                    st["pi"] = pi
                                if idx_ >= LA:
                                    prev = steps[idx_ - LA]
                                    pi = prev["pi"]
                                    for (i, rhs_ap, w_, st_, sp_) in prev["pv"]:
                                        kb.op("tensor", lambda e, i=i, rhs_ap=rhs_ap, w_=w_, st_=st_, sp_=sp_, pi=pi: e.matmul(accs[i][:, 0:w_], lhsT=pt[pi][:, i * 128:(i + 1) * 128], rhs=rhs_ap,
                                                                                                                 start=st_, stop=sp_),
                                              outs=[ACC[i]], ins=[PT[pi], KS, KC])

                        accsb = [sb(f"accsb{i}", [128, 132], F32, pq) for i in range(4)]
                        ACCSB = [Buf(t[:]) for t in accsb]

                        def finish(h, br, first):
                            zc = 64
                            wd_ = 129 if br == 0 else 65
                            for i in range(4):
                                kb.op("vector", lambda e, i=i: e.tensor_copy(out=accsb[i][:, 0:wd_], in_=accs[i][:, 0:wd_]), outs=[ACCSB[i]], ins=[ACC[i]])
                            for i in range(4):
                                kb.op("vector", lambda e, i=i: e.tensor_scalar(out=rz[:, i:i + 1], in0=accsb[i][:, zc:zc + 1], scalar1=1e-30, scalar2=None, op0=ALU.max),
                                      outs=[RZ], ins=[ACCSB[i]])
                            kb.op("vector", lambda e: e.reciprocal(out=rz[:], in_=rz[:]), outs=[RZ], ins=[RZ])
                            if br == 0:
                                for i in range(4):
                                    if h == 0:
                                        kb.op("vector", lambda e, i=i: e.tensor_scalar(out=pslc[:, i, :], in0=accsb[i][:, 65:129], scalar1=rz[:, i:i + 1], scalar2=None, op0=ALU.mult),
                                              outs=[PSLC], ins=[ACCSB[i], RZ])
                                    else:
                                        kb.op("vector", lambda e, i=i: e.scalar_tensor_tensor(out=pslc[:, i, :], in0=accsb[i][:, 65:129], scalar=rz[:, i:i + 1], in1=pslc[:, i, :],
                                                                                           op0=ALU.mult, op1=ALU.add), outs=[PSLC], ins=[ACCSB[i], RZ, PSLC])
                            kb.op("vector", lambda e: e.tensor_tensor(out=rz[:], in0=rz[:], in1=gts[:, :, h * 3 + br], op=ALU.mult), outs=[RZ], ins=[RZ, GTS])
                            for i in range(4):
                                dst = onsa[:, i, h * 64:(h + 1) * 64]
                                if first:
                                    kb.op("vector", lambda e, i=i, dst=dst: e.tensor_scalar(out=dst, in0=accsb[i][:, 0:64], scalar1=rz[:, i:i + 1], scalar2=None, op0=ALU.mult),
                                          outs=[ONSA], ins=[ACCSB[i], RZ])
                                else:
                                    kb.op("vector", lambda e, i=i, dst=dst: e.scalar_tensor_tensor(out=dst, in0=accsb[i][:, 0:64], scalar=rz[:, i:i + 1], in1=dst, op0=ALU.mult, op1=ALU.add),
                                          outs=[ONSA], ins=[ACCSB[i], RZ, ONSA])

                        wband4 = sb("wband4", [128, 8, 512], BF16, pq)
                        cmpb0 = sb("cmpb0", [128, 512], BF16, pq)
                        kb.dma("sync", wband4[:], I["wband4"], outs=[QC])
                        kb.dma("sync", cmpb0[:], I["cmpb0"], outs=[QC])
                        for qb in range(4, NB):
                            sl = slice(qb * 512, (qb + 1) * 512)
                            kb.dma("sync", cosb[:], I["cosT"][:, sl], outs=[CSB])
                            kb.dma("sync", sinb[:], I["sinT"][:, sl], outs=[CSB])
                            kb.dma("sync", cmpbb[:], I["cmpb"][:, qb, :], outs=[CMB])
                            for ch in range(2):
                                bk = sbank()
                                for k in range(8):
                                    kb.op("tensor", lambda e, k=k, bk=bk, ch=ch: e.matmul(ps[bk][:], lhsT=wqg[:, k, ch * 128:(ch + 1) * 128], rhs=hT[:, k, sl],
                                                                                         start=(k == 0), stop=(k == 7)), outs=[PS[bk]], ins=[WN, HT[qb]], mark=(k == 7))
                                kb.op("scalar", lambda e, bk=bk, ch=ch: e.copy(out=qTb[:, ch, :], in_=ps[bk][:]), outs=[QB_], ins=[PS[bk]])
                                rope_from(bk, qrTb[:, ch, :], QRB)
                            for i in range(4):
                                tt = 4 * qb + i
                                bk = mbank()
                                for k in range(8):
                                    kb.op("tensor", lambda e, k=k, bk=bk, tt=tt: e.matmul(ps[bk][:, 0:12], lhsT=hT[:, k, tt * 128:(tt + 1) * 128], rhs=wgt[:, k, :],
                                                                                         start=(k == 0), stop=(k == 7)), outs=[PS[bk]], ins=[WN, HT[qb]], mark=(k == 7))
                                kb.op("scalar", lambda e, bk=bk, i=i: e.activation(out=gts[:, i, :], in_=ps[bk][:, 0:12], func=AF.Sigmoid), outs=[GTS], ins=[PS[bk]])
                            if stop_after == "p1c_qa":
                                dump("onsa", onsa[:], [128, 4, 256])
                                return nc, dbg_out
                            ncts = 1 if qb < 4 else 2
                            for h in range(4):
                                ch, p0_ = h // 2, (h % 2) * 64
                                steps = []
                                for ct in range(ncts):
                                    smm = [(kcmpT[p0_:p0_ + 64, ct * 128:(ct + 1) * 128], qTb[p0_:p0_ + 64, ch, :], [KC, QB_])]
                                    if ct == ncts - 1:
                                        smm.append((ident[:], cmpbb[:], [CST, CMB]))
                                    else:
                                        smm.append((ident[:], cmpb0[:], [CST, QC]))
                                    pv = [(i, vcmp1[:, ct, 0:129], 129, ct == 0, ct == ncts - 1) for i in range(4)]
                                    steps.append({"s": smm, "pv": pv})
                                run_branch(steps)
                                finish(h, 0, True)
                            if stop_after == "p1c_qb":
                                dump("onsa", onsa[:], [128, 4, 256])
                                return nc, dbg_out
                            for i in range(4):
                                qt = 4 * qb + i
                                kb.dma("sync", sadd[:], I["seladd"][:, qt, :], outs=[SADD])
                                kb.op("vector", lambda e, i=i: e.tensor_tensor(out=score[:], in0=pslc[:, i, :], in1=sadd[:], op=ALU.add), outs=[SEL], ins=[PSLC, SADD])
                                kb.op("vector", lambda e: e.max(out=m8[:, 0:8], in_=score[:]), outs=[SEL], ins=[SEL])
                                kb.op("vector", lambda e: e.match_replace(out=stmp[:], in_to_replace=m8[:, 0:8], in_values=score[:], imm_value=-3e38), outs=[SEL], ins=[SEL])
                                kb.op("vector", lambda e: e.max(out=m8[:, 8:16], in_=stmp[:]), outs=[SEL], ins=[SEL])
                                kb.op("vector", lambda e: e.tensor_scalar(out=sel[:], in0=score[:], scalar1=m8[:, 15:16], scalar2=None, op0=ALU.is_ge), outs=[SEL], ins=[SEL])
                                kb.op("vector", lambda e: e.scalar_tensor_tensor(out=sel[:], in0=score[:], scalar=-1e29, in1=sel[:], op0=ALU.is_gt, op1=ALU.mult), outs=[SEL], ins=[SEL])
                                kb.op("vector", lambda e: e.tensor_scalar(out=negb4[i][:, 0:64], in0=sel[:], scalar1=-1.0, scalar2=-NEG, op0=ALU.add, op1=ALU.mult), outs=[SEL], ins=[SEL])
                                kb.op("vector", lambda e: e.tensor_scalar(out=negb4[i][:, 64:128], in0=sel[:], scalar1=-1.0, scalar2=-NEG, op0=ALU.add, op1=ALU.mult), outs=[SEL], ins=[SEL])
                            for h in range(4):
                                ch, p0_ = h // 2, (h % 2) * 64
                                steps = []
                                jmin = max(0, 4 - 4 * qb)
                                for j in range(jmin, 8):
                                    kt = 4 * qb - 4 + j
                                    smm = [(kwT[p0_:p0_ + 64, kt * 128:(kt + 1) * 128], qrTb[p0_:p0_ + 64, ch, :], [KS, QRB]),
                                           (ident[:], (wband4 if qb == 4 else wband)[:, j, :], [CST, QC])]
                                    pv = [(i, vw1[:, kt, 0:65], 65, j == max(i, jmin), j == i + 4) for i in range(4) if i <= j <= i + 4]
                                    steps.append({"s": smm, "pv": pv})
                                run_branch(steps)
                                finish(h, 2, False)
                            for i in range(4):
                                bk = mbank()
                                kb.op("tensor", lambda e, bk=bk, i=i: e.matmul(ps[bk][:, 0:128], lhsT=negb4[i][:], rhs=ident[:], start=True, stop=True), outs=[PS[bk]], ins=[SEL, CST])
                                kb.op("scalar", lambda e, bk=bk, i=i: e.copy(out=negbT[:, i * 128:(i + 1) * 128], in_=ps[bk][:, 0:128]), outs=[NBT], ins=[PS[bk]])
                            for ch in range(2):
                                kb.op("gpsimd", lambda e, ch=ch: e.tensor_copy(out=qn[ch][0][0:64, :], in_=qrTb[0:64, ch, :]), outs=[QN], ins=[QRB])
                                kb.op("gpsimd", lambda e, ch=ch: e.tensor_copy(out=qn[ch][0][64:128, :], in_=negbT[64:128, :]), outs=[QN], ins=[NBT])
                                kb.op("gpsimd", lambda e, ch=ch: e.tensor_copy(out=qn[ch][1][0:64, :], in_=negbT[0:64, :]), outs=[QN], ins=[NBT])
                                kb.op("gpsimd", lambda e, ch=ch: e.tensor_copy(out=qn[ch][1][64:128, :], in_=qrTb[64:128, ch, :]), outs=[QN], ins=[QRB])
                            for h in range(4):
                                ch, p0_ = h // 2, (h % 2) * 64
                                KE_ = KEe if h % 2 == 0 else KEo
                                steps = []
                                for kt in range(4 * qb + 4):
                                    smm = [(KE_[:, kt * 128:(kt + 1) * 128], qn[ch][h % 2][:], [KS, QN, CST])]
                                    if kt >= 4 * qb:
                                        smm.append((ident[:], wband[:, 4 + kt - 4 * qb, :], [CST, QC]))
                                    pv = [(i, vs1[:, kt, 0:65], 65, kt == 0, kt == 4 * qb + i) for i in range(4) if kt <= 4 * qb + i]
                                    steps.append({"s": smm, "pv": pv})
                                run_branch(steps)
                                finish(h, 1, False)
                            if stop_after == "p1c_q":
                                dump("onsa", onsa[:], [128, 4, 256])
                                dump("pslc", pslc[:], [128, 4, 64])
                                dump("negbT", negbT[:], [128, 512], BF16)
                                return nc, dbg_out
                            kb.op("gpsimd", lambda e: e.tensor_copy(out=obn[:], in_=onsa[:]), outs=[OBN], ins=[ONSA])
                            for i in range(4):
                                qt = 4 * qb + i
                                s_ = qt - 16
                                for ch in range(2):
                                    jf = 4 + g * 2 + ch
                                    bk = mbank()
                                    kb.op("tensor", lambda e, bk=bk, i=i, ch=ch: e.matmul(ps[bk][:, 0:128], lhsT=obn[:, i, ch * 128:(ch + 1) * 128], rhs=ident[:], start=True, stop=True),
                                          outs=[PS[bk]], ins=[OBN, CST])
                                    dst = oT[:, jf, s_ * 128:(s_ + 1) * 128]
                                    kb.op("scalar", lambda e, bk=bk, dst=dst: e.copy(out=dst, in_=ps[bk][:, 0:128]), outs=[OT[jf][s_]], ins=[PS[bk]])
                        kb.barrier()
                kb.barrier()
            if "oT2" in dbg:
                dump("oT2", oT[:], [128, 8, SO], BF16)
            if stop_after == "p1c":
                kb.barrier()
                return nc, dbg_out
        kb.barrier()
        with ExitStack() as p2:
            acc = sb("acc", [128, 8, SO], F32, p2)
            ACCB = [Buf(acc[:, :, nb * 512:(nb + 1) * 512]) for nb in range(4)]
            wo = sb("wo", [128, 8, D], BF16, p2)
            WO = Buf(wo[:])
            fg = sb("fg", [128, 8], F32, p2)
            FG = Buf(fg[:])
            kb.dma("sync", fg[:], I["fg"], outs=[FG])
            xTo_v = I["xTo"].rearrange("(k p) t -> p k t", p=128)
            for nb in range(4):
                kb.dma("sync", acc[:, :, nb * 512:(nb + 1) * 512], xTo_v[:, :, nb * 512:(nb + 1) * 512], outs=[ACCB[nb]])
            w_out_v = I["w_out"].rearrange("(k p) c -> p k c", p=128)
            for k in range(8):
                kb.dma("gpsimd", wo[:, k, :], w_out_v[:, k, :], outs=[WO])
            rr2 = [0]

            def bank2():
                rr2[0] = (rr2[0] + 1) % 8
                return rr2[0]

            for nb in range(4):
                sl = slice(nb * 512, (nb + 1) * 512)
                for m in range(8):
                    bk = bank2()
                    for k in range(8):
                        kb.op("tensor", lambda e, k=k, m=m, bk=bk, sl=sl: e.matmul(ps[bk][:], lhsT=wo[:, k, m * 128:(m + 1) * 128], rhs=oT[:, k, sl], start=(k == 0), stop=(k == 7)),
                              outs=[PS[bk]], ins=[WO] + [OT[k][s] for s in range(nb * 4, nb * 4 + 4)], mark=(k == 7))
                    kb.op("vector", lambda e, m=m, bk=bk, sl=sl: e.scalar_tensor_tensor(out=acc[:, m, sl], in0=ps[bk][:], scalar=g1c(m), in1=acc[:, m, sl], op0=ALU.mult, op1=ALU.add),
                          outs=[ACCB[nb]], ins=[PS[bk], ACCB[nb], MOD])
            if "x2T" in dbg:
                dump("x2T", acc[:], [128, 8, SO])
            h2T = sb("h2T", [128, 8, SO], BF16, p2)
            H2 = [Buf(h2T[:, :, nb * 512:(nb + 1) * 512]) for nb in range(4)]
            with ExitStack() as p2a:
                sqa = sb("sqa", [128, 8, 512], F32, p2a)
                SQA = Buf(sqa[:])
                rsa = sb("rsa", [128, 512], F32, p2a)
                RSA = Buf(rsa[:])
                tma = [sb(f"tma{i}", [128, 512], F32, p2a) for i in range(2)]
                TMA = [Buf(t[:]) for t in tma]
                for nb in range(4):
                    sl = slice(nb * 512, (nb + 1) * 512)
                    kb.op("scalar", lambda e, sl=sl: e.activation(out=sqa[:], in_=acc[:, :, sl], func=AF.Square), outs=[SQA], ins=[ACCB[nb]])
                    bk = bank2()
                    for k in range(8):
                        kb.op("tensor", lambda e, k=k, bk=bk: e.matmul(ps[bk][:], lhsT=onesf[:], rhs=sqa[:, k, :], start=(k == 0), stop=(k == 7)),
                              outs=[PS[bk]], ins=[SQA, CST], mark=(k == 7))
                    kb.op("scalar", lambda e, bk=bk: e.activation(out=rsa[:], in_=ps[bk][:], func=AF.Sqrt, scale=1.0 / D, bias=EPS), outs=[RSA], ins=[PS[bk]])
                    kb.op("vector", lambda e: e.reciprocal(out=rsa[:], in_=rsa[:]), outs=[RSA], ins=[RSA])
                    for k in range(8):
                        T = TMA[k % 2]
                        t_ = tma[k % 2]
                        kb.op("vector", lambda e, k=k, t_=t_, sl=sl: e.tensor_tensor(out=t_[:], in0=acc[:, k, sl], in1=rsa[:], op=ALU.mult), outs=[T], ins=[ACCB[nb], RSA])
                        kb.op("scalar", lambda e, k=k, t_=t_, sl=sl: e.activation(out=h2T[:, k, sl], in_=t_[:], func=AF.Identity, scale=a2[:, k:k + 1], bias=sh2(k)),
                              outs=[H2[nb]], ins=[T, MOD])
                kb.barrier()
            if "h2T" in dbg:
                dump("h2T", h2T[:], [128, 8, SO], BF16)
            gw = sb("gw", [128, 16, 65], F32, p2)
            GW = Buf(gw[:])
            kb.op("vector", lambda e: e.memset(gw[:], 1.0), outs=[GW])
            with ExitStack() as p2r:
                rwf = sb("rwf", [128, 8, 64], F32, p2r)
                rwb = sb("rwb", [128, 8, 64], BF16, p2r)
                rbias = sb("rbias", [128, 64], F32, p2r)
                RW = Buf(rwf[:])
                kb.dma("sync", rwf[:], I["rw"], outs=[RW])
                kb.dma("sync", rbias[:], I["rbias"], outs=[RW])
                kb.op("vector", lambda e: e.tensor_copy(out=rwb[:], in_=rwf[:]), outs=[RW], ins=[RW])
                scr = sb("scr", [128, 64], F32, p2r)
                chs = sb("chs", [128, 64], F32, p2r)
                eq = sb("eq", [128, 64], F32, p2r)
                chm = sb("chm", [128, 64], F32, p2r)
                m1 = sb("m1", [128, 8], F32, p2r)
                m2 = sb("m2", [128, 8], F32, p2r)
                gsm = sb("gsm", [128, 8], F32, p2r)
                g8 = sb("g8", [128, 8], F32, p2r)
                gmk = sb("gmk", [128, 8], F32, p2r)
                e8 = sb("e8", [128, 8], F32, p2r)
                ssum = sb("ssum", [128, 1], F32, p2r)
                RT = Buf(scr[:])
                v3 = lambda ap: ap.rearrange("p (g j) -> p g j", j=8)
                b3 = lambda ap: ap.rearrange("p (g o) -> p g o", o=1).to_broadcast([128, 8, 8])
                for tt in range(16):
                    bk = bank2()
                    for k in range(8):
                        kb.op("tensor", lambda e, k=k, bk=bk, tt=tt: e.matmul(ps[bk][:, 0:64], lhsT=h2T[:, k, tt * 128:(tt + 1) * 128], rhs=rwb[:, k, :], start=(k == 0), stop=(k == 7)),
                              outs=[PS[bk]], ins=[H2[tt // 4], RW], mark=(k == 7))
                    kb.op("scalar", lambda e, bk=bk: e.activation(out=scr[:], in_=ps[bk][:, 0:64], func=AF.Sigmoid), outs=[RT], ins=[PS[bk]])
                    V = lambda f: kb.op("vector", f, outs=[RT], ins=[RT, RW])
                    V(lambda e: e.tensor_tensor(out=chs[:], in0=scr[:], in1=rbias[:], op=ALU.add))
                    V(lambda e: e.tensor_reduce(out=m1[:], in_=v3(chs[:]), axis=AX.X, op=ALU.max))
                    V(lambda e: e.tensor_tensor(out=v3(eq[:]), in0=v3(chs[:]), in1=b3(m1[:]), op=ALU.is_equal))
                    V(lambda e: e.scalar_tensor_tensor(out=eq[:], in0=eq[:], scalar=-1e30, in1=chs[:], op0=ALU.mult, op1=ALU.add))
                    V(lambda e: e.tensor_reduce(out=m2[:], in_=v3(eq[:]), axis=AX.X, op=ALU.max))
                    V(lambda e: e.tensor_tensor(out=gsm[:], in0=m1[:], in1=m2[:], op=ALU.add))
                    V(lambda e: e.max(out=g8[:], in_=gsm[:]))
                    V(lambda e: e.tensor_scalar(out=gmk[:], in0=gsm[:], scalar1=g8[:, 3:4], scalar2=None, op0=ALU.is_ge))
                    V(lambda e: e.scalar_tensor_tensor(out=v3(chm[:]), in0=v3(chs[:]), scalar=10.0, in1=b3(gmk[:]), op0=ALU.add, op1=ALU.mult))
                    V(lambda e: e.max(out=e8[:], in_=chm[:]))
                    V(lambda e: e.tensor_scalar(out=eq[:], in0=chm[:], scalar1=e8[:, 7:8], scalar2=None, op0=ALU.is_ge))
                    V(lambda e: e.tensor_tensor(out=eq[:], in0=eq[:], in1=scr[:], op=ALU.mult))
                    V(lambda e: e.tensor_reduce(out=ssum[:], in_=eq[:], axis=AX.X, op=ALU.add))
                    V(lambda e: e.reciprocal(out=ssum[:], in_=ssum[:]))
                    kb.op("vector", lambda e, tt=tt: e.tensor_scalar(out=gw[:, tt, 0:64], in0=eq[:], scalar1=ssum[:, 0:1], scalar2=2.5, op0=ALU.mult, op1=ALU.mult),
                          outs=[GW], ins=[RT])
                kb.barrier()
            if "gw" in dbg:
                dump("gw", gw[:], [128, 16, 65])
            with ExitStack() as p2e:
                wg = [sb(f"wg{i}", [128, 8, 512], BF16, p2e) for i in range(2)]
                wd = [sb(f"wd{i}", [128, 2, D], BF16, p2e) for i in range(2)]
                WG = [Buf(t[:]) for t in wg]
                WD = [Buf(t[:]) for t in wd]
                actT = [sb(f"actT{i}", [128, 2, SO], BF16, p2e) for i in range(2)]
                ACT_ = [[Buf(actT[i][:, :, nb * 512:(nb + 1) * 512]) for nb in range(4)] for i in range(2)]
                sgt = [sb(f"sgt{i}", [128, 256], F32, p2e) for i in range(2)]
                SGT = [Buf(t[:]) for t in sgt]
                att = [sb(f"att{i}", [128, 256], BF16, p2e) for i in range(2)]
                ATT = [Buf(t[:]) for t in att]
                NE = n_experts

                def load_w(e_):
                    sl_ = e_ % 2
                    gv = I["wgu"][e_].rearrange("(k p) c -> p k c", p=128)
                    kb.dma("gpsimd", wg[sl_][:, 0:4, :], gv[:, 0:4, :], outs=[WG[sl_]])
                    kb.dma("gpsimd", wg[sl_][:, 4:8, :], gv[:, 4:8, :], outs=[WG[sl_]])
                    kb.dma("gpsimd", wd[sl_][:], I["wdn"][e_].rearrange("(k p) c -> p k c", p=128), outs=[WD[sl_]])

                def load_wg(e_):
                    sl_ = e_ % 2
                    gv = I["wgu"][e_].rearrange("(k p) c -> p k c", p=128)
                    kb.dma("gpsimd", wg[sl_][:, 0:4, :], gv[:, 0:4, :], outs=[WG[sl_]])
                    kb.dma("gpsimd", wg[sl_][:, 4:8, :], gv[:, 4:8, :], outs=[WG[sl_]])

                def load_wd(e_):
                    sl_ = e_ % 2
                    kb.dma("gpsimd", wd[sl_][:], I["wdn"][e_].rearrange("(k p) c -> p k c", p=128), outs=[WD[sl_]])

                cnt = [0]
                pend = {}

                def GU(e_, tt):
                    sl_ = e_ % 2
                    bk = bank2()
                    for k in range(8):
                        kb.op("tensor", lambda e, k=k: e.matmul(ps[bk][:], lhsT=h2T[:, k, tt * 128:(tt + 1) * 128], rhs=wg[sl_][:, k, :], start=(k == 0), stop=(k == 7)),
                              outs=[PS[bk]], ins=[H2[tt // 4], WG[sl_]], mark=(k == 7))
                    cnt[0] += 1
                    i2 = cnt[0] % 2
                    kb.op("scalar", lambda e: e.activation(out=sgt[i2][:], in_=ps[bk][:, 0:256], func=AF.Silu), outs=[SGT[i2]], ins=[PS[bk]])
                    kb.op("vector", lambda e: e.scalar_tensor_tensor(out=att[i2][:], in0=ps[bk][:, 256:512], scalar=gw[:, tt, e_:e_ + 1], in1=sgt[i2][:],
                                                                     op0=ALU.mult, op1=ALU.mult), outs=[ATT[i2]], ins=[PS[bk], SGT[i2], GW])
                    pend[(e_, tt)] = i2

                def TR(e_, tt):
                    sl_ = e_ % 2
                    i2 = pend.pop((e_, tt))
                    for hc in range(2):
                        bt = bank2()
                        kb.op("tensor", lambda e, hc=hc, bt=bt: e.matmul(ps[bt][:, 0:128], lhsT=att[i2][:, hc * 128:(hc + 1) * 128], rhs=ident[:], start=True, stop=True),
                              outs=[PS[bt]], ins=[ATT[i2], CST])
                        kb.op("scalar", lambda e, hc=hc, bt=bt: e.copy(out=actT[sl_][:, hc, tt * 128:(tt + 1) * 128], in_=ps[bt][:, 0:128]),
                              outs=[ACT_[sl_][tt // 4]], ins=[PS[bt]])

                def DN(e_, nb, ms):
                    sl_ = e_ % 2
                    sl = slice(nb * 512, (nb + 1) * 512)
                    for m in ms:
                        bk = bank2()
                        for hc in range(2):
                            kb.op("tensor", lambda e, bk=bk, hc=hc, m=m: e.matmul(ps[bk][:], lhsT=wd[sl_][:, hc, m * 128:(m + 1) * 128], rhs=actT[sl_][:, hc, sl], start=(hc == 0), stop=(hc == 1)),
                                  outs=[PS[bk]], ins=[WD[sl_], ACT_[sl_][nb]], mark=(hc == 1))
                        kb.op("vector", lambda e, bk=bk, m=m: e.scalar_tensor_tensor(out=acc[:, m, sl], in0=ps[bk][:], scalar=g2c(m), in1=acc[:, m, sl], op0=ALU.mult, op1=ALU.add),
                              outs=[ACCB[nb]], ins=[PS[bk], ACCB[nb], MOD])

                load_wg(0)
                load_wd(0)
                for e_ in range(NE + 1):
                    if e_ + 1 < NE:
                        load_wg(e_ + 1)
                    for tt in range(16):
                        if e_ < NE:
                            GU(e_, tt)
                            if tt > 0:
                                TR(e_, tt - 1)
                        if e_ > 0:
                            DN(e_ - 1, tt // 4, (2 * (tt % 4), 2 * (tt % 4) + 1))
                    if e_ < NE:
                        TR(e_, 15)
                    if e_ + 1 < NE:
                        load_wd(e_ + 1)
                kb.barrier()
            if "x3T" in dbg:
                dump("x3T", acc[:], [128, 8, SO])
            sq2 = sb("sq2", [128, 8, 512], F32, p2)
            SQ2 = Buf(sq2[:])
            rstd2 = sb("rstd2", [128, 512], F32, p2)
            RS2 = Buf(rstd2[:])
            ot = [sb(f"ot{i}", [128, 8, 512], F32, p2) for i in range(2)]
            OTB = [Buf(t[:]) for t in ot]
            outT_v = outT.rearrange("(k p) t -> p k t", p=128)
            for nb in range(4):
                sl = slice(nb * 512, (nb + 1) * 512)
                kb.op("scalar", lambda e, sl=sl: e.activation(out=sq2[:], in_=acc[:, :, sl], func=AF.Square), outs=[SQ2], ins=[ACCB[nb]])
                bk = bank2()
                for k in range(8):
                    kb.op("tensor", lambda e, k=k, bk=bk: e.matmul(ps[bk][:], lhsT=onesf[:], rhs=sq2[:, k, :], start=(k == 0), stop=(k == 7)),
                          outs=[PS[bk]], ins=[SQ2, CST], mark=(k == 7))
                kb.op("scalar", lambda e, bk=bk: e.activation(out=rstd2[:], in_=ps[bk][:], func=AF.Sqrt, scale=1.0 / D, bias=EPS), outs=[RS2], ins=[PS[bk]])
                kb.op("vector", lambda e: e.reciprocal(out=rstd2[:], in_=rstd2[:]), outs=[RS2], ins=[RS2])
                o_ = ot[nb % 2]
                for k in range(8):
                    kb.op("vector", lambda e, k=k, o_=o_, sl=sl: e.scalar_tensor_tensor(out=o_[:, k, :], in0=acc[:, k, sl], scalar=fg[:, k:k + 1], in1=rstd2[:], op0=ALU.mult, op1=ALU.mult),
                          outs=[OTB[nb % 2]], ins=[ACCB[nb], FG, RS2])
                kb.dma("sync", outT_v[:, :, sl], o_[:], ins=[OTB[nb % 2]])
            kb.barrier()
        kb.barrier()
    return nc, dbg_out


def _prep_inputs(inp, core):
    b = core // 2
    half = core % 2
    f = lambda a: np.ascontiguousarray(a, dtype=np.float32)
    x = inp["x"][b]
    m = {}
    xT = f(x.T)
    m["xTo"] = f(xT[:, half * SO:(half + 1) * SO])
    if half == 0:
        xl = np.zeros((D, S), np.float32)
        xl[:, SO:] = xT[:, :SO]
        m["xT"] = xl
    else:
        m["xT"] = xT
    m["cT"] = f(inp["c"][b].reshape(8, 128).T)
    m["w_ada"] = f(inp["w_ada"][0])
    m["b_ada"] = f(inp["b_ada"][0].reshape(1, -1))
    m["n1g"] = f(inp["norm1_g"][0].reshape(8, 128).T)
    m["w_in"] = f(inp["w_in"][0])
    m["lbl"] = f(inp["hg_lb_logits"].reshape(2, 4, 128).transpose(2, 0, 1))
    m["hng"] = f(np.broadcast_to(inp["hg_norm_g"][0][None, :], (128, 512)))
    for s in ("k", "v"):
        m["peT" + s] = f(inp["cmp_pos_" + s][0].T)
        m["w1" + s] = f(inp["cmp_w1_" + s][0].reshape(32, 64, 256).transpose(1, 0, 2))
        m["b1" + s] = f(inp["cmp_b1_" + s][0].reshape(2, 128).T)
        m["w2" + s] = f(inp["cmp_w2_" + s][0].reshape(2, 128, 64).transpose(1, 0, 2))
    m["w_out"] = f(inp["w_out"][0])
    m["n2g"] = f(inp["norm2_g"][0].reshape(8, 128).T)
    m["rw"] = f(inp["router_w"][0].reshape(8, 128, 64).transpose(1, 0, 2))
    m["rbias"] = f(np.broadcast_to(inp["router_bias"][0][None, :], (128, 64)))
    m["fg"] = f(inp["final_g"].reshape(8, 128).T)
    return m


_SHARED = {}


def kernel(**inp):
    inp = {k: np.asarray(v) for k, v in inp.items()}
    nc, _ = build()
    wgu = np.ascontiguousarray(np.concatenate([inp["w_exp_gu"][0], inp["w_sh_gu"][0][None]], axis=0), dtype=np.float32)
    wdn = np.ascontiguousarray(np.concatenate([inp["w_exp_dn"][0], inp["w_sh_dn"][0][None]], axis=0), dtype=np.float32)
    in_maps = []
    for core in range(8):
        m = _prep_inputs(inp, core)
        m["wgu"] = wgu
        m["wdn"] = wdn
        m.update(_consts(core % 2))
        in_maps.append(m)
    res = run_bass_kernel_spmd(nc, in_maps, core_ids=list(range(8)))
    out = np.zeros((4, S, D), np.float32)
    for core in range(8):
        b, half = core // 2, core % 2
        out[b, half * SO:(half + 1) * SO, :] = res.results[core]["outT"].T
    return out
```

```python
import numpy as np
import os as _os0
import ml_dtypes
from contextlib import ExitStack
import concourse.bass as bass
import concourse.mybir as mybir
from concourse.bass_utils import run_bass_kernel_spmd

F32 = mybir.dt.float32
BF16 = mybir.dt.bfloat16
AF = mybir.ActivationFunctionType
ALU = mybir.AluOpType
AX = mybir.AxisListType

S = 4096
D = 1024
NT = 32
NB = 8
SO = 2048
EPS = 1e-6
NEG = -30000.0
NDS = 12
SEM_LIMIT = 2000
SAME_SYNC = not bool(int(_os0.environ.get("NOSAME", "0")))


class Buf:
    __slots__ = ("ap", "w", "r", "excl")

    def __init__(self, ap, excl=False):
        self.ap = ap
        self.w = None
        self.r = {}
        self.excl = excl

    def __getitem__(self, k):
        return self.ap[k]


class Eng:
    def __init__(self, name, h):
        self.name = name
        self.h = h
        self.sem = None
        self.count = 0
        self.epoch = 0
        self.waited = {}


class KB:
    def __init__(self, nc, es):
        self.nc = nc
        self.es = es
        self.engs = {n: Eng(n, getattr(nc, n)) for n in ("tensor", "vector", "scalar", "gpsimd", "sync")}
        for e in self.engs.values():
            self._new_sem(e)
        self.dsems = {q: [es.enter_context(nc.semaphore(f"d_{q}{i}")) for i in range(NDS)] for q in ("sync", "gpsimd")}
        self.dcnt = {q: [0] * NDS for q in ("sync", "gpsimd")}
        self.drr = {"sync": 0, "gpsimd": 0}
        self.nsem = 0

    def _new_sem(self, e):
        e.epoch += 1
        e.sem = self.es.enter_context(self.nc.semaphore(f"s_{e.name}_{e.epoch}"))
        e.count = 0

    def wait(self, eng, tk):
        key, sem, val = tk
        if eng.waited.get(key, 0) >= val:
            return
        eng.h.wait_ge(sem, val)
        eng.waited[key] = val

    def _deps(self, en, eng, outs, ins):
        need = {}

        def add(t):
            if t[3] == en and (en == "tensor" or not SAME_SYNC):
                return
            cur = need.get(t[0])
            if cur is None or cur[2] < t[2]:
                need[t[0]] = t

        for b in ins:
            if b.w is not None:
                add(b.w)
            if b.excl:
                for t in b.r.values():
                    if t[3] != en:
                        add(t)
        for b in outs:
            if b.w is not None:
                add(b.w)
            for t in b.r.values():
                add(t)
        for t in need.values():
            self.wait(eng, t[:3])

    def op(self, en, fn, outs=(), ins=(), mark=True):
        eng = self.engs[en]
        self._deps(en, eng, outs, ins)
        if eng.count >= SEM_LIMIT:
            self._new_sem(eng)
        inst = fn(eng.h)
        if mark:
            eng.count += 1
            inst.then_inc(eng.sem, 1)
            tk = ((en, eng.epoch), eng.sem, eng.count, en)
        else:
            tk = ((en, eng.epoch), eng.sem, eng.count + 1, en)
        for b in ins:
            b.r[tk[0]] = tk
        for b in outs:
            b.w = tk
            b.r = {}
        return tk

    def dma(self, q, out_ap, in_ap, outs=(), ins=()):
        eng = self.engs[q]
        i = self.drr[q]
        self.drr[q] = (i + 1) % NDS
        sem = self.dsems[q][i]
        key = ("d", q, i)
        if self.dcnt[q][i] > 0:
            self.wait(eng, (key, sem, self.dcnt[q][i]))
        self._deps("dma_" + q, eng, outs, ins)
        inst = eng.h.dma_start(out=out_ap, in_=in_ap)
        self.dcnt[q][i] += 16
        inst.then_inc(sem, 16)
        tk = (key, sem, self.dcnt[q][i], "dma_" + q)
        for b in ins:
            b.r[key] = tk
        for b in outs:
            b.w = tk
            b.r = {}
        return tk

    def barrier(self):
        for e in self.engs.values():
            for o in self.engs.values():
                if o is e or o.count == 0:
                    continue
                self.wait(e, ((o.name, o.epoch), o.sem, o.count))
            for q in ("sync", "gpsimd"):
                for i in range(NDS):
                    if self.dcnt[q][i] > 0:
                        self.wait(e, (("d", q, i), self.dsems[q][i], self.dcnt[q][i]))


def _consts(half):
    bf = ml_dtypes.bfloat16
    c = {}
    eye = np.eye(128, dtype=np.float32)
    c["ident"] = eye.astype(bf)
    c["onesf"] = np.ones((128, 128), np.float32)
    c["isel0"] = (eye * (1.0 if half == 0 else 0.0)).astype(bf)
    c["isel1"] = (eye * (1.0 if half == 1 else 0.0)).astype(bf)
    m = np.arange(128)
    sw = (m // 64) * 64 + ((m % 64) + 32) % 64
    ps = np.zeros((128, 128), np.float32)
    ps[sw, m] = 1.0
    c["pswap"] = ps.astype(bf)
    shift = 2048 if half == 0 else 0
    dd = np.arange(128) % 64
    i = dd % 32
    inv = 10000.0 ** (-(i.astype(np.float64)) / 32.0)
    tpos = (np.arange(S) - shift).astype(np.float64)
    ang = inv[:, None].astype(np.float32).astype(np.float64) * tpos[None, :]
    ang = ang.astype(np.float32).astype(np.float64)
    c["cosT"] = np.cos(ang).astype(np.float32)
    sg = np.where(dd < 32, -1.0, 1.0)[:, None]
    c["sinT"] = (np.sin(ang) * sg).astype(np.float32)
    vm = np.ones((128, 32), np.float32)
    if half == 0:
        vm[:, :16] = 0.0
    c["vmask"] = vm
    c["hmask"] = (m[:, None] <= m[None, :]).astype(np.float32).astype(bf)
    seg = np.ones((128, 512), np.float32)
    seg[:, ::128] = 0.0
    c["segm"] = seg
    r = np.arange(128)[:, None]
    qi = np.arange(512)[None, :]
    wb = np.zeros((8, 128, 512), np.float32)
    cb = np.zeros((4, 128, 512), np.float32)
    for j in range(8):
        kpos = -512 + 128 * j + r
        dlt = qi - kpos
        wb[j] = np.where((dlt >= 0) & (dlt < 512), 0.0, NEG)
    for j in range(4):
        kpos = 128 * j + r
        cb[j] = np.where(kpos <= qi, 0.0, NEG)
    c["wband"] = np.ascontiguousarray(wb.transpose(1, 0, 2)).astype(bf)
    c["causb"] = np.ascontiguousarray(cb.transpose(1, 0, 2)).astype(bf)
    wb4 = wb.copy()
    if half == 0:
        wb4[0:4] = NEG
    c["wband4"] = np.ascontiguousarray(wb4.transpose(1, 0, 2)).astype(bf)
    cm = np.zeros((8, 128, 512), np.float32)
    for qb in range(8):
        ct = 0 if qb < 4 else 1
        cc = 128 * ct + r
        qpos = 512 * qb + qi - shift
        tc = cc - shift // 16
        cm[qb] = np.where((16 * tc + 31 <= qpos) & (cc < 255) & (tc >= 0), 0.0, NEG)
    c["cmpb"] = np.ascontiguousarray(cm.transpose(1, 0, 2)).astype(bf)
    c["cmpb0"] = np.full((128, 512), NEG if half == 0 else 0.0, np.float32).astype(bf)
    ek = np.zeros((64, 32, 128), np.float32)
    for kt in range(32):
        ek[2 * kt, kt, :64] = 1.0
        ek[2 * kt + 1, kt, 64:] = 1.0
    c["ekt"] = np.concatenate([ek, ek], axis=0).astype(bf)
    add = np.zeros((128, 32, 64), np.float32)
    for qt in range(32):
        pos = 128 * qt + np.arange(128) - shift
        cur = pos // 64
        j = np.arange(64)[None, :] - shift // 64
        forced = (j == 0) | (j == cur[:, None]) | (j == cur[:, None] - 1)
        avail = (j <= cur[:, None]) & (j >= 0)
        add[:, qt, :] = np.where(avail & forced, 1e30, np.where(avail, 0.0, -1e30))
    c["seladd"] = add
    cs = np.arange(256)[:, None] * 16
    ss = np.arange(64)[None, :] * 64
    ov = np.clip(np.minimum(cs + 32, ss + 64) - np.maximum(cs, ss), 0, None).astype(np.float32) / 32.0
    ov[255] = 0.0
    c["ovl"] = np.ascontiguousarray(ov.reshape(2, 128, 64).transpose(1, 0, 2)).astype(bf)
    return c


CONST_SHAPES = {
    "ident": ([128, 128], BF16), "onesf": ([128, 128], F32), "isel0": ([128, 128], BF16), "isel1": ([128, 128], BF16),
    "pswap": ([128, 128], BF16), "cosT": ([128, S], F32), "sinT": ([128, S], F32), "hmask": ([128, 128], BF16),
    "segm": ([128, 512], F32), "wband": ([128, 8, 512], BF16), "causb": ([128, 4, 512], BF16),
    "cmpb": ([128, 8, 512], BF16), "ekt": ([128, 32, 128], BF16), "seladd": ([128, 32, 64], F32),
    "ovl": ([128, 2, 64], BF16), "vmask": ([128, 32], F32), "wband4": ([128, 8, 512], BF16), "cmpb0": ([128, 512], BF16),
}

IN_SHAPES = {
    "xT": [D, S], "xTo": [D, SO], "cT": [128, 8], "w_ada": [D, 6 * D], "b_ada": [1, 6 * D], "n1g": [128, 8],
    "w_in": [D, 3352], "lbl": [128, 2, 4], "hng": [128, 512],
    "peTk": [64, 32], "w1k": [64, 32, 256], "b1k": [128, 2], "w2k": [128, 2, 64],
    "peTv": [64, 32], "w1v": [64, 32, 256], "b1v": [128, 2], "w2v": [128, 2, 64],
    "w_out": [D, D], "n2g": [128, 8], "rw": [128, 8, 64], "rbias": [128, 64],
    "wgu": [65, D, 512], "wdn": [65, 256, D], "fg": [128, 8],
}


class _SkipNSA(Exception):
    pass


class _NSAScope(ExitStack):
    def __exit__(self, et, ev, tb):
        super().__exit__(None, None, None)
        return et is _SkipNSA


def build(stop_after=None, dbg=(), with_moe=True, enable_nsa=True, n_experts=65):
    nc = bass.Bass("TRN2", target_bir_lowering=False)
    I = {}
    for k, shp in IN_SHAPES.items():
        if not with_moe and k in ("wgu", "wdn"):
            continue
        I[k] = nc.dram_tensor(k, list(shp), F32, kind="ExternalInput").ap()
    for k, (shp, dt) in CONST_SHAPES.items():
        I[k] = nc.dram_tensor(k, list(shp), dt, kind="ExternalInput").ap()
    outT = nc.dram_tensor("outT", [D, SO], F32, kind="ExternalOutput").ap()
    dbg_out = {}
    with ExitStack() as es:
        kb = KB(nc, es)
        E = es.enter_context

        uid = [0]

        def sb(name, shape, dt=F32, stack=None):
            uid[0] += 1
            return (stack or es).enter_context(nc.sbuf_tensor(f"sb{uid[0]}_" + name, list(shape), dt))

        ps = [E(nc.psum_tensor(f"ps{i}", [128, 512], F32)) for i in range(8)]
        PS = [Buf(p[:], excl=True) for p in ps]

        def dump(name, ap, shape, dt=F32):
            t = nc.dram_tensor("dbg_" + name, list(shape), dt, kind="ExternalOutput").ap()
            dbg_out[name] = t
            kb.barrier()
            kb.dma("sync", t, ap)
            kb.barrier()

        ident = sb("ident", [128, 128], BF16)
        onesf = sb("onesf", [128, 128], F32)
        isel0 = sb("isel0", [128, 128], BF16)
        isel1 = sb("isel1", [128, 128], BF16)
        pswap = sb("pswap", [128, 128], BF16)
        hmask = sb("hmask", [128, 128], BF16)
        segm = sb("segm", [128, 512], F32)
        CST = Buf(ident[:])
        for nm, t in (("ident", ident), ("onesf", onesf), ("isel0", isel0), ("isel1", isel1), ("pswap", pswap),
                      ("hmask", hmask), ("segm", segm)):
            kb.dma("sync", t[:], I[nm], outs=[CST])
        modcol = sb("modcol", [128, 48], F32)
        a1 = sb("a1", [128, 8], F32)
        a2 = sb("a2", [128, 8], F32)
        MOD = Buf(modcol[:])
        oT = sb("oT", [128, 8, SO], BF16)
        OT = [[Buf(oT[:, j, s * 128:(s + 1) * 128]) for s in range(16)] for j in range(8)]

        with ExitStack() as p0:
            cT = sb("cT", [128, 8], F32, p0)
            cs = sb("cs", [128, 8], F32, p0)
            bada = sb("bada", [1, 6 * D], F32, p0)
            modrow = sb("modrow", [1, 6 * D], F32, p0)
            one1 = sb("one1", [1, 1], F32, p0)
            n1g = sb("n1g", [128, 8], F32, p0)
            n2g = sb("n2g", [128, 8], F32, p0)
            wab = [sb(f"wab{i}", [128, 8, 512], F32, p0) for i in range(2)]
            WAB = [Buf(w[:]) for w in wab]
            SM = Buf(cT[:])
            MR = Buf(modrow[:])
            kb.dma("sync", cT[:], I["cT"], outs=[SM])
            kb.dma("sync", bada[:], I["b_ada"], outs=[SM])
            kb.dma("sync", n1g[:], I["n1g"], outs=[SM])
            kb.dma("sync", n2g[:], I["n2g"], outs=[SM])
            kb.op("vector", lambda e: e.memset(one1[:], 1.0), outs=[SM])
            kb.op("scalar", lambda e: e.activation(out=cs[:], in_=cT[:], func=AF.Silu), outs=[SM], ins=[SM])
            wada_v = I["w_ada"].rearrange("(k p) c -> p k c", p=128)
            for cb in range(12):
                W = WAB[cb % 2]
                kb.dma("sync" if cb % 2 == 0 else "gpsimd", wab[cb % 2][:], wada_v[:, :, cb * 512:(cb + 1) * 512], outs=[W])
                P = PS[cb % 2]
                for k in range(8):
                    kb.op("tensor", lambda e, k=k, cb=cb: e.matmul(ps[cb % 2][0:1, :], lhsT=cs[:, k:k + 1], rhs=wab[cb % 2][:, k, :],
                                                                 start=(k == 0), stop=(k == 7)),
                          outs=[P], ins=[SM, W], mark=(k == 7))
                kb.op("vector", lambda e, cb=cb: e.tensor_tensor(out=modrow[0:1, cb * 512:(cb + 1) * 512], in0=ps[cb % 2][0:1, :],
                                                                  in1=bada[0:1, cb * 512:(cb + 1) * 512], op=ALU.add),
                      outs=[MR], ins=[P, SM])
            P = PS[2]
            for j in range(48):
                kb.op("tensor", lambda e, j=j: e.matmul(ps[2][:, j:j + 1], lhsT=modrow[0:1, j * 128:(j + 1) * 128], rhs=one1[0:1, 0:1],
                                                       start=True, stop=True), outs=[P], ins=[MR, SM], mark=(j == 47))
            kb.op("vector", lambda e: e.tensor_copy(out=modcol[:], in_=ps[2][:, 0:48]), outs=[MOD], ins=[P])
            kb.op("vector", lambda e: e.scalar_tensor_tensor(out=a1[:], in0=modcol[:, 8:16], scalar=1.0, in1=n1g[:], op0=ALU.add, op1=ALU.mult),
                  outs=[MOD], ins=[MOD, SM])
            kb.op("vector", lambda e: e.scalar_tensor_tensor(out=a2[:], in0=modcol[:, 32:40], scalar=1.0, in1=n2g[:], op0=ALU.add, op1=ALU.mult),
                  outs=[MOD], ins=[MOD, SM])
            if "mod" in dbg:
                dump("mod", modcol[:], [128, 48])
            kb.barrier()
        sh1 = lambda k: modcol[:, k:k + 1]
        g1c = lambda k: modcol[:, 16 + k:17 + k]
        sh2 = lambda k: modcol[:, 24 + k:25 + k]
        g2c = lambda k: modcol[:, 40 + k:41 + k]

        if stop_after == "p0":
            kb.barrier()
            return nc, dbg_out

        with ExitStack() as p1:
            hT = sb("hT", [128, 8, S], BF16, p1)
            HT = [Buf(hT[:, :, n * 512:(n + 1) * 512]) for n in range(NB)]
            with ExitStack() as p1a:
                xb = [sb(f"xb{i}", [128, 8, 512], F32, p1a) for i in range(2)]
                XB = [Buf(t[:]) for t in xb]
                sq = sb("sq", [128, 8, 512], F32, p1a)
                SQ = Buf(sq[:])
                rstd = sb("rstd", [128, 512], F32, p1a)
                RS = Buf(rstd[:])
                tmp = [sb(f"tmp{i}", [128, 512], F32, p1a) for i in range(2)]
                TMP = [Buf(t[:]) for t in tmp]
                xT_v = I["xT"].rearrange("(k p) t -> p k t", p=128)
                for n in range(NB):
                    X = XB[n % 2]
                    x_ = xb[n % 2]
                    kb.dma("sync" if n % 2 == 0 else "gpsimd", x_[:], xT_v[:, :, n * 512:(n + 1) * 512], outs=[X])
                    kb.op("scalar", lambda e, x_=x_: e.activation(out=sq[:], in_=x_[:], func=AF.Square), outs=[SQ], ins=[X])
                    P = PS[n % 2]
                    for k in range(8):
                        kb.op("tensor", lambda e, k=k, n=n: e.matmul(ps[n % 2][:], lhsT=onesf[:], rhs=sq[:, k, :], start=(k == 0), stop=(k == 7)),
                              outs=[P], ins=[SQ, CST], mark=(k == 7))
                    kb.op("scalar", lambda e, n=n: e.activation(out=rstd[:], in_=ps[n % 2][:], func=AF.Sqrt, scale=1.0 / D, bias=EPS),
                          outs=[RS], ins=[P])
                    kb.op("vector", lambda e: e.reciprocal(out=rstd[:], in_=rstd[:]), outs=[RS], ins=[RS])
                    for k in range(8):
                        T = TMP[k % 2]
                        t_ = tmp[k % 2]
                        kb.op("vector", lambda e, k=k, t_=t_, x_=x_: e.tensor_tensor(out=t_[:], in0=x_[:, k, :], in1=rstd[:], op=ALU.mult),
                              outs=[T], ins=[X, RS])
                        kb.op("scalar", lambda e, k=k, t_=t_, n=n: e.activation(out=hT[:, k, n * 512:(n + 1) * 512], in_=t_[:], func=AF.Identity,
                                                                            scale=a1[:, k:k + 1], bias=sh1(k)),
                              outs=[HT[n]], ins=[T, MOD])
                kb.barrier()
            if "hT" in dbg:
                dump("hT", hT[:], [128, 8, S], BF16)
            if stop_after == "p1a":
                kb.barrier()
                return nc, dbg_out

            rr = [0]

            def bank():
                rr[0] = (rr[0] + 1) % 8
                return rr[0]

            w_in_v = I["w_in"].rearrange("(k p) c -> p k c", p=128)

            with ExitStack() as ph:
                lbl = sb("lbl", [128, 2, 4], F32, ph)
                lb = sb("lb", [128, 4], F32, ph)
                oml = sb("oml", [128, 4], F32, ph)
                hng = sb("hng", [128, 512], F32, ph)
                HC = Buf(lbl[:])
                kb.dma("sync", lbl[:], I["lbl"], outs=[HC])
                kb.dma("sync", hng[:], I["hng"], outs=[HC])
                kb.op("vector", lambda e: e.tensor_tensor(out=lb[:], in0=lbl[:, 0, :], in1=lbl[:, 1, :], op=ALU.subtract), outs=[HC], ins=[HC])
                kb.op("scalar", lambda e: e.activation(out=lb[:], in_=lb[:], func=AF.Sigmoid), outs=[HC], ins=[HC])
                kb.op("vector", lambda e: e.tensor_scalar(out=oml[:], in0=lb[:], scalar1=-1.0, scalar2=1.0, op0=ALU.mult, op1=ALU.add), outs=[HC], ins=[HC])
                wq = sb("wq", [128, 8, 128], BF16, ph)
                wf = sb("wf", [128, 8, 128], BF16, ph)
                wig = sb("wig", [128, 8, 256], BF16, ph)
                WQ, WF, WIG = Buf(wq[:]), Buf(wf[:]), Buf(wig[:])
                Q1 = sb("Q1", [128, S], BF16, ph)
                Q2 = sb("Q2", [128, S], BF16, ph)
                Kt = sb("Kt", [128, S], BF16, ph)
                Kh = sb("Kh", [128, NT, 128], BF16, ph)
                Vh = sb("Vh", [128, NT, 128], BF16, ph)
                SGt = sb("SGt", [128, NT, 128], BF16, ph)
                ebl = sb("ebl", [128, NT], F32, ph)
                BQ = [Buf(Q1[:, n * 512:(n + 1) * 512]) for n in range(NB)]
                BKH = [Buf(Kh[:, t, :]) for t in range(NT)]
                BV = [Buf(Vh[:, t, :]) for t in range(NT)]
                tn = ["f", "lf", "b", "d1", "d2", "eb", "e1", "en1", "el", "k"]
                T2 = [{n_: sb(f"t{i}_" + n_, [128, 512], F32, ph) for n_ in tn} for i in range(2)]
                TB2 = [{n_: Buf(T2[i][n_][:]) for n_ in tn} for i in range(2)]
                khtb2 = [sb(f"khtb{i}", [128, 512], BF16, ph) for i in range(2)]
                KHTB2 = [Buf(t[:]) for t in khtb2]
                vmask = sb("vmask", [128, 32], F32, ph)
                kb.dma("sync", vmask[:], I["vmask"], outs=[HC])
                Sst = sb("Sst", [128, 128], F32, ph)
                SST = Buf(Sst[:])
                sbf = [sb(f"sbf{i}", [128, 128], BF16, ph) for i in range(2)]
                SBF = [Buf(t[:]) for t in sbf]
                atm = [sb(f"atm{i}", [128, 128], BF16, ph) for i in range(2)]
                ATM = [Buf(t[:]) for t in atm]
                for i in range(2):
                    kb.op("vector", lambda e, i=i: e.memset(atm[i][:], 0.0), outs=[ATM[i]])
                junk = sb("junk", [128, 128], F32, ph)
                JK = Buf(junk[:])
                ssq = [sb(f"ssq{i}", [128, 1], F32, ph) for i in range(2)]
                SSQ = [Buf(t[:]) for t in ssq]
                of = [sb(f"of{i}", [128, 128], F32, ph) for i in range(2)]
                OF = [Buf(t[:]) for t in of]
                obf = [sb(f"obf{i}", [128, 128], BF16, ph) for i in range(2)]
                OBF = [Buf(t[:]) for t in obf]
                v4 = lambda ap: ap.rearrange("p (c t) -> p c t", t=128)
                for hd in range(int(_os0.environ.get("NHEADS", "4"))):
                    c0 = hd * 128
                    kb.dma("gpsimd", wq[:], w_in_v[:, :, c0:c0 + 128], outs=[WQ])
                    kb.dma("gpsimd", wf[:], w_in_v[:, :, 512 + c0:512 + c0 + 128], outs=[WF])
                    kb.dma("gpsimd", wig[:, :, 0:128], w_in_v[:, :, 1024 + c0:1024 + c0 + 128], outs=[WIG])
                    kb.dma("gpsimd", wig[:, :, 128:256], w_in_v[:, :, 1536 + c0:1536 + c0 + 128], outs=[WIG])
                    for n in range(NB):
                        sl = slice(n * 512, (n + 1) * 512)
                        own = n >= 4
                        bq_, bf_ = bank(), bank()
                        if own:
                            for k in range(8):
                                kb.op("tensor", lambda e, k=k, bq_=bq_, sl=sl: e.matmul(ps[bq_][:], lhsT=wq[:, k, :], rhs=hT[:, k, sl], start=(k == 0), stop=(k == 7)),
                                      outs=[PS[bq_]], ins=[WQ, HT[n]], mark=(k == 7))
                        for k in range(8):
                            kb.op("tensor", lambda e, k=k, bf_=bf_, sl=sl: e.matmul(ps[bf_][:], lhsT=wf[:, k, :], rhs=hT[:, k, sl], start=(k == 0), stop=(k == 7)),
                                  outs=[PS[bf_]], ins=[WF, HT[n]], mark=(k == 7))
                        t = T2[n % 2]
                        TB = TB2[n % 2]
                        khtb = khtb2[n % 2]
                        KHTB = KHTB2[n % 2]
                        kb.op("scalar", lambda e, bf_=bf_: e.activation(out=t["f"][:], in_=ps[bf_][:], func=AF.Sigmoid), outs=[TB["f"]], ins=[PS[bf_]])
                        kb.op("vector", lambda e, hd=hd: e.tensor_scalar(out=t["f"][:], in0=t["f"][:], scalar1=oml[:, hd:hd + 1], scalar2=lb[:, hd:hd + 1],
                                                                     op0=ALU.mult, op1=ALU.add), outs=[TB["f"]], ins=[TB["f"], HC])
                        kb.op("scalar", lambda e: e.activation(out=t["lf"][:], in_=t["f"][:], func=AF.Ln), outs=[TB["lf"]], ins=[TB["f"]])
                        kb.op("gpsimd", lambda e: e.tensor_scalar(out=t["k"][:], in0=t["f"][:], scalar1=-1.0, scalar2=1.0, op0=ALU.mult, op1=ALU.add),
                              outs=[TB["k"]], ins=[TB["f"]])
                        kb.op("vector", lambda e: e.tensor_tensor_scan(out=t["b"][:], data0=segm[:], data1=t["lf"][:], initial=0.0, op0=ALU.mult, op1=ALU.add),
                              outs=[TB["b"]], ins=[TB["lf"], CST])
                        if own:
                            kb.op("vector", lambda e: e.tensor_tensor(out=v4(t["d1"][:]), in0=v4(t["b"][:]), in1=v4(t["b"][:])[:, :, 63:64].to_broadcast([128, 4, 128]),
                                                                      op=ALU.subtract), outs=[TB["d1"]], ins=[TB["b"]])
                        kb.op("vector", lambda e: e.tensor_tensor(out=v4(t["d2"][:]), in0=v4(t["b"][:])[:, :, 127:128].to_broadcast([128, 4, 128]), in1=v4(t["b"][:]),
                                                                  op=ALU.subtract), outs=[TB["d2"]], ins=[TB["b"]])
                        kb.op("scalar", lambda e: e.activation(out=t["eb"][:], in_=t["b"][:], func=AF.Exp), outs=[TB["eb"]], ins=[TB["b"]])
                        if own:
                            kb.op("scalar", lambda e: e.activation(out=t["e1"][:], in_=t["d1"][:], func=AF.Exp), outs=[TB["e1"]], ins=[TB["d1"]])
                            kb.op("scalar", lambda e: e.activation(out=t["en1"][:], in_=t["d1"][:], func=AF.Exp, scale=-1.0), outs=[TB["en1"]], ins=[TB["d1"]])
                        kb.op("scalar", lambda e: e.activation(out=t["el"][:], in_=t["d2"][:], func=AF.Exp), outs=[TB["el"]], ins=[TB["d2"]])
                        sc_ = 128.0 ** -0.5
                        if own:
                            kb.op("vector", lambda e, bq_=bq_, sl=sl: e.scalar_tensor_tensor(out=Q1[:, sl], in0=ps[bq_][:], scalar=sc_, in1=t["e1"][:], op0=ALU.mult, op1=ALU.mult),
                                  outs=[BQ[n]], ins=[PS[bq_], TB["e1"]])
                            kb.op("vector", lambda e, bq_=bq_, sl=sl: e.scalar_tensor_tensor(out=Q2[:, sl], in0=ps[bq_][:], scalar=sc_, in1=t["eb"][:], op0=ALU.mult, op1=ALU.mult),
                                  outs=[BQ[n]], ins=[PS[bq_], TB["eb"]])
                            kb.op("gpsimd", lambda e, sl=sl: e.tensor_tensor(out=Kt[:, sl], in0=t["k"][:], in1=t["en1"][:], op=ALU.mult), outs=[BQ[n]], ins=[TB["k"], TB["en1"]])
                        kb.op("gpsimd", lambda e: e.tensor_tensor(out=khtb[:], in0=t["k"][:], in1=t["el"][:], op=ALU.mult), outs=[KHTB], ins=[TB["k"], TB["el"]])
                        kb.op("gpsimd", lambda e, n=n: e.tensor_copy(out=ebl[:, 4 * n:4 * n + 4], in_=v4(t["eb"][:])[:, :, 127]), outs=[BQ[n]], ins=[TB["eb"]])
                        for tt in range(4 * n, 4 * n + 4):
                            bk = bank()
                            for k in range(8):
                                kb.op("tensor", lambda e, k=k, bk=bk, tt=tt: e.matmul(ps[bk][:, 0:256], lhsT=hT[:, k, tt * 128:(tt + 1) * 128], rhs=wig[:, k, :],
                                                                                     start=(k == 0), stop=(k == 7)),
                                      outs=[PS[bk]], ins=[WIG, HT[n]], mark=(k == 7))
                            kb.op("vector", lambda e, bk=bk, tt=tt: e.tensor_scalar(out=Vh[:, tt, :], in0=ps[bk][:, 0:128], scalar1=vmask[:, tt:tt + 1], scalar2=None, op0=ALU.mult),
                                  outs=[BV[tt]], ins=[PS[bk], HC])
                            kb.op("scalar", lambda e, bk=bk, tt=tt: e.activation(out=SGt[:, tt, :], in_=ps[bk][:, 128:256], func=AF.Silu), outs=[BV[tt]], ins=[PS[bk]])
                        for i in range(4):
                            bk = bank()
                            kb.op("tensor", lambda e, i=i, bk=bk: e.matmul(ps[bk][:, 0:128], lhsT=khtb[:, i * 128:(i + 1) * 128], rhs=ident[:], start=True, stop=True),
                                  outs=[PS[bk]], ins=[KHTB, CST])
                            kb.op("scalar", lambda e, i=i, bk=bk, n=n: e.copy(out=Kh[:, 4 * n + i, :], in_=ps[bk][:, 0:128]), outs=[BKH[4 * n + i]], ins=[PS[bk]])
                    kb.op("vector", lambda e: e.memset(Sst[:], 0.0), outs=[SST])
                    at_bank = {}
                    pending_b = []

                    def emit_at(c):
                        bk = bank()
                        at_bank[c] = bk
                        cs_ = slice(c * 128, (c + 1) * 128)
                        c0_ = c * 128
                        kb.op("tensor", lambda e: e.matmul(ps[bk][0:64, 0:64], lhsT=Kt[:, c0_:c0_ + 64], rhs=Q1[:, c0_:c0_ + 64], start=True, stop=True),
                              outs=[PS[bk]], ins=[BQ[c // 4]], mark=False)
                        kb.op("tensor", lambda e: e.matmul(ps[bk][:, 64:128], lhsT=Kt[:, cs_], rhs=Q1[:, c0_ + 64:c0_ + 128], start=True, stop=True),
                              outs=[PS[bk]], ins=[BQ[c // 4]])
                        kb.op("vector", lambda e: e.tensor_tensor(out=atm[c % 2][0:64, 0:64], in0=ps[bk][0:64, 0:64], in1=hmask[0:64, 0:64], op=ALU.mult),
                              outs=[ATM[c % 2]], ins=[PS[bk], CST])
                        kb.op("vector", lambda e: e.tensor_tensor(out=atm[c % 2][:, 64:128], in0=ps[bk][:, 64:128], in1=hmask[:, 64:128], op=ALU.mult),
                              outs=[ATM[c % 2]], ins=[PS[bk], CST])

                    for c in range(NT):
                        if c + 1 < NT and c + 1 >= 16:
                            emit_at(c + 1)
                        cs_ = slice(c * 128, (c + 1) * 128)
                        bd = bank()
                        kb.op("tensor", lambda e, bd=bd, c=c: e.matmul(ps[bd][:, 0:128], lhsT=Kh[:, c, :], rhs=Vh[:, c, :], start=True, stop=True),
                              outs=[PS[bd]], ins=[BKH[c], BV[c]])
                        if c >= 16:
                            bo = bank()
                            kb.op("tensor", lambda e, bo=bo, c=c: e.matmul(ps[bo][:, 0:128], lhsT=atm[c % 2][:], rhs=Vh[:, c, :], start=True, stop=False),
                                  outs=[PS[bo]], ins=[ATM[c % 2], BV[c]], mark=False)
                            kb.op("tensor", lambda e, bo=bo, c=c, cs_=cs_: e.matmul(ps[bo][:, 0:128], lhsT=Q2[:, cs_], rhs=sbf[(c - 1) % 2][:], start=False, stop=True),
                                  outs=[PS[bo]], ins=[BQ[c // 4], SBF[(c - 1) % 2]])
                        if c + 1 < NT:
                            kb.op("vector", lambda e, bd=bd, c=c: e.scalar_tensor_tensor(out=Sst[:], in0=Sst[:], scalar=ebl[:, c:c + 1], in1=ps[bd][:, 0:128],
                                                                                     op0=ALU.mult, op1=ALU.add), outs=[SST], ins=[SST, PS[bd], BQ[c // 4]])
                            if c >= 15:
                                kb.op("scalar", lambda e, c=c: e.copy(out=sbf[c % 2][:], in_=Sst[:]), outs=[SBF[c % 2]], ins=[SST])
                        if c < 16:
                            continue
                        i2 = c % 2
                        kb.op("gpsimd", lambda e, i2=i2: e.memset(ssq[i2][:], 0.0), outs=[SSQ[i2]])
                        kb.op("scalar", lambda e, bo=bo, i2=i2: e.activation(out=junk[:], in_=ps[bo][:, 0:128], func=AF.Square, accum_out=ssq[i2][:]),
                              outs=[JK, SSQ[i2]], ins=[PS[bo]])
                        kb.op("scalar", lambda e, i2=i2: e.activation(out=ssq[i2][:], in_=ssq[i2][:], func=AF.Sqrt, scale=1.0 / 128, bias=EPS), outs=[SSQ[i2]], ins=[SSQ[i2]])

                        def part_b(c=c, bo=bo, i2=i2, hd=hd, c0=c0):
                            kb.op("vector", lambda e: e.reciprocal(out=ssq[i2][:], in_=ssq[i2][:]), outs=[SSQ[i2]], ins=[SSQ[i2]])
                            kb.op("vector", lambda e: e.scalar_tensor_tensor(out=of[i2][:], in0=ps[bo][:, 0:128], scalar=ssq[i2][:, 0:1], in1=hng[:, c0:c0 + 128],
                                                                             op0=ALU.mult, op1=ALU.mult), outs=[OF[i2]], ins=[PS[bo], SSQ[i2], HC])
                            kb.op("gpsimd", lambda e: e.tensor_tensor(out=obf[i2][:], in0=of[i2][:], in1=SGt[:, c, :], op=ALU.mult), outs=[OBF[i2]], ins=[OF[i2], BV[c]])
                            bt = bank()
                            kb.op("tensor", lambda e: e.matmul(ps[bt][:, 0:128], lhsT=obf[i2][:], rhs=ident[:], start=True, stop=True),
                                  outs=[PS[bt]], ins=[OBF[i2], CST])
                            s_ = c - 16
                            kb.op("scalar", lambda e: e.copy(out=oT[:, hd, s_ * 128:(s_ + 1) * 128], in_=ps[bt][:, 0:128]), outs=[OT[hd][s_]], ins=[PS[bt]])

                        pending_b.append(part_b)
                        if len(pending_b) > 1:
                            pending_b.pop(0)()
                    while pending_b:
                        pending_b.pop(0)()
                kb.barrier()
            if "oT" in dbg:
                dump("oT", oT[:], [128, 8, SO], BF16)
            if stop_after == "p1b":
                kb.barrier()
                return nc, dbg_out

            SCL = 64.0 ** -0.5
            if not enable_nsa:
                for jf in range(4, 8):
                    kb.op("vector", lambda e, jf=jf: e.memset(oT[:, jf, :], 0.0), outs=OT[jf])
            with _NSAScope() as pn:
                if not enable_nsa:
                    raise _SkipNSA()
                ovl = sb("ovl", [128, 2, 64], BF16, pn)
                kb.dma("sync", ovl[:], I["ovl"], outs=[CST])
                KEe = sb("KEe", [128, S], BF16, pn)
                KEo = sb("KEo", [128, S], BF16, pn)
                ekt_v = I["ekt"].rearrange("p a b -> p (a b)")
                kb.dma("sync", KEe[64:128, :], ekt_v[64:128, :], outs=[CST])
                kb.dma("sync", KEo[0:64, :], ekt_v[0:64, :], outs=[CST])
                kwT = sb("kwT", [128, S], BF16, pn)
                kcvT = sb("kcvT", [128, S], BF16, pn)
                vs1 = sb("vs1", [128, NT, 80], BF16, pn)
                vw1 = sb("vw1", [128, NT, 80], BF16, pn)
                KS = Buf(kwT[:])
                kcmpT = sb("kcmpT", [128, 256], BF16, pn)
                vcmp1 = sb("vcmp1", [128, 2, 144], BF16, pn)
                KC = Buf(kcmpT[:])
                wk3 = sb("wk3", [128, 8, 384], BF16, pn)
                wv2 = sb("wv2", [128, 8, 128], BF16, pn)
                wqg = sb("wqg", [128, 8, 256], BF16, pn)
                wgt = sb("wgt", [128, 8, 12], BF16, pn)
                WN = Buf(wk3[:])
                cosb = sb("cosb", [128, 512], F32, pn)
                sinb = sb("sinb", [128, 512], F32, pn)
                CSB = Buf(cosb[:])
                rawb = sb("rawb", [128, 512], BF16, pn)
                RAWB = Buf(rawb[:])
                rt1 = sb("rt1", [128, 512], F32, pn)
                rt2 = sb("rt2", [128, 512], F32, pn)
                RT1, RT2 = Buf(rt1[:]), Buf(rt2[:])
                _padn = int(_os0.environ.get("PADN", "0"))
                if _padn:
                    _pad = sb("padn", [128, _padn], F32, pn)
                srr = [0]

                def sbank():
                    srr[0] = (srr[0] + 1) % 3
                    return srr[0]

                mrr = [0]

                def mbank():
                    return 7

                import os as _os
                _dbgmode = int(_os.environ.get("ROPEDBG", "0"))

                def rope_from(bk, dst_ap, dstbuf):
                    if _dbgmode == 1:
                        kb.op("scalar", lambda e: e.copy(out=dst_ap, in_=ps[bk][:]), outs=[dstbuf], ins=[PS[bk]])
                        return
                    if _dbgmode == 3:
                        kb.op("vector", lambda e: e.tensor_tensor(out=rt1[:], in0=ps[bk][:], in1=cosb[:], op=ALU.mult), outs=[RT1], ins=[PS[bk], CSB])
                        kb.op("gpsimd", lambda e: e.tensor_copy(out=dst_ap, in_=rt1[:]), outs=[dstbuf], ins=[RT1])
                        return
                    if _dbgmode == 4:
                        kb.op("scalar", lambda e: e.copy(out=rawb[:], in_=ps[bk][:]), outs=[RAWB], ins=[PS[bk]])
                        b2 = mbank()
                        kb.op("tensor", lambda e: e.matmul(ps[b2][:], lhsT=pswap[:], rhs=rawb[:], start=True, stop=True), outs=[PS[b2]], ins=[RAWB, CST])
                        kb.op("vector", lambda e: e.tensor_tensor(out=rt1[:], in0=ps[bk][:], in1=cosb[:], op=ALU.mult), outs=[RT1], ins=[PS[bk], CSB])
                        kb.op("vector", lambda e: e.tensor_tensor(out=rt2[:], in0=ps[b2][:], in1=sinb[:], op=ALU.mult), outs=[RT2], ins=[PS[b2], CSB])
                        kb.op("vector", lambda e: e.tensor_tensor(out=dst_ap, in0=rt1[:], in1=rt2[:], op=ALU.add), outs=[dstbuf], ins=[RT1, RT2])
                        return
                    if _dbgmode == 5:
                        kb.op("scalar", lambda e: e.copy(out=rawb[:], in_=ps[bk][:]), outs=[RAWB], ins=[PS[bk]])
                        b2 = mbank()
                        kb.op("tensor", lambda e: e.matmul(ps[b2][:], lhsT=pswap[:], rhs=rawb[:], start=True, stop=True), outs=[PS[b2]], ins=[RAWB, CST])
                        kb.op("vector", lambda e: e.tensor_tensor(out=rt1[:], in0=ps[bk][:], in1=cosb[:], op=ALU.mult), outs=[RT1], ins=[PS[bk], CSB])
                        kb.op("scalar", lambda e: e.copy(out=rt2[:], in_=ps[b2][:]), outs=[RT2], ins=[PS[b2]])
                        _sb = cosb if _os.environ.get("USECOS") else sinb
                        kb.op("vector", lambda e: e.tensor_tensor(out=rt2[:], in0=rt2[:], in1=_sb[:], op=ALU.mult), outs=[RT2], ins=[RT2, CSB])
                        kb.op("vector", lambda e: e.tensor_tensor(out=dst_ap, in0=rt1[:], in1=rt2[:], op=ALU.add), outs=[dstbuf], ins=[RT1, RT2])
                        return
                    if _dbgmode in (7, 8):
                        kb.op("scalar", lambda e: e.copy(out=rawb[:], in_=ps[bk][:]), outs=[RAWB], ins=[PS[bk]])
                        b2 = mbank()
                        kb.op("tensor", lambda e: e.matmul(ps[b2][:], lhsT=pswap[:], rhs=rawb[:], start=True, stop=True), outs=[PS[b2]], ins=[RAWB, CST])
                        kb.op("vector", lambda e: e.tensor_tensor(out=rt1[:], in0=ps[bk][:], in1=cosb[:], op=ALU.mult), outs=[RT1], ins=[PS[bk], CSB])
                        kb.op("scalar", lambda e: e.copy(out=rt2[:], in_=ps[b2][:]), outs=[RT2], ins=[PS[b2]])
                        kb.op("vector", lambda e: e.tensor_tensor(out=rt2[:], in0=rt2[:], in1=sinb[:], op=ALU.mult), outs=[RT2], ins=[RT2, CSB])
                        if _dbgmode == 8:
                            kb.op("vector", lambda e: e.tensor_tensor(out=rt1[:], in0=rt1[:], in1=rt2[:], op=ALU.add), outs=[RT1], ins=[RT1, RT2])
                        kb.op("scalar", lambda e: e.copy(out=dst_ap, in_=rt1[:]), outs=[dstbuf], ins=[RT1])
                        return
                    if _dbgmode in (9, 10):
                        kb.op("vector", lambda e: e.tensor_tensor(out=rt1[:], in0=ps[bk][:], in1=cosb[:], op=ALU.mult), outs=[RT1], ins=[PS[bk], CSB])
                        if _dbgmode == 9:
                            kb.op("scalar", lambda e: e.copy(out=rt2[:], in_=ps[bk][:]), outs=[RT2], ins=[PS[bk]])
                        else:
                            kb.op("vector", lambda e: e.tensor_tensor(out=rt2[:], in0=rt1[:], in1=cosb[:], op=ALU.mult), outs=[RT2], ins=[RT1, CSB])
                        kb.op("gpsimd", lambda e: e.tensor_copy(out=dst_ap, in_=rt1[:]), outs=[dstbuf], ins=[RT1])
                        return
                    if _dbgmode in (11, 12):
                        kb.op("scalar", lambda e: e.copy(out=rawb[:], in_=ps[bk][:]), outs=[RAWB], ins=[PS[bk]])
                        b2 = mbank()
                        kb.op("tensor", lambda e: e.matmul(ps[b2][:], lhsT=pswap[:], rhs=rawb[:], start=True, stop=True), outs=[PS[b2]], ins=[RAWB, CST])
                        kb.op("vector", lambda e: e.tensor_tensor(out=rt1[:], in0=ps[bk][:], in1=cosb[:], op=ALU.mult), outs=[RT1], ins=[PS[bk], CSB, RAWB])
                        kb.op("gpsimd", lambda e: e.tensor_copy(out=dst_ap, in_=rt1[:]), outs=[dstbuf], ins=[RT1])
                        if _dbgmode == 12:
                            return
                        kb.op("vector", lambda e: e.tensor_tensor(out=rt1[:], in0=ps[b2][:], in1=sinb[:], op=ALU.mult), outs=[RT1], ins=[PS[b2], CSB])
                        kb.op("gpsimd", lambda e: e.tensor_tensor(out=dst_ap, in0=dst_ap, in1=rt1[:], op=ALU.add), outs=[dstbuf], ins=[RT1, dstbuf])
                        return
                    if _dbgmode == 2:
                        kb.op("scalar", lambda e: e.copy(out=rawb[:], in_=ps[bk][:]), outs=[RAWB], ins=[PS[bk]])
                        b2 = mbank()
                        kb.op("tensor", lambda e: e.matmul(ps[b2][:], lhsT=pswap[:], rhs=rawb[:], start=True, stop=True), outs=[PS[b2]], ins=[RAWB, CST])
                        kb.op("scalar", lambda e: e.copy(out=dst_ap, in_=ps[b2][:]), outs=[dstbuf], ins=[PS[b2]])
                        return
                    kb.op("scalar", lambda e: e.copy(out=rawb[:], in_=ps[bk][:]), outs=[RAWB], ins=[PS[bk]])
                    b2 = mbank()
                    kb.op("tensor", lambda e: e.matmul(ps[b2][:], lhsT=pswap[:], rhs=rawb[:], start=True, stop=True), outs=[PS[b2]], ins=[RAWB, CST])
                    kb.op("vector", lambda e: e.tensor_tensor(out=rt1[:], in0=ps[bk][:], in1=cosb[:], op=ALU.mult), outs=[RT1], ins=[PS[bk], CSB])
                    kb.op("vector", lambda e: e.tensor_tensor(out=rt2[:], in0=ps[b2][:], in1=sinb[:], op=ALU.mult), outs=[RT2], ins=[PS[b2], CSB])
                    if isinstance(dst_ap, tuple):
                        kb.op("gpsimd", lambda e: e.tensor_tensor(out=dst_ap[0], in0=rt1[0:64, :], in1=rt2[0:64, :], op=ALU.add), outs=[dstbuf], ins=[RT1, RT2])
                        kb.op("gpsimd", lambda e: e.tensor_tensor(out=dst_ap[1], in0=rt1[64:128, :], in1=rt2[64:128, :], op=ALU.add), outs=[dstbuf], ins=[RT1, RT2])
                    else:
                        kb.op("gpsimd", lambda e: e.tensor_tensor(out=dst_ap, in0=rt1[:], in1=rt2[:], op=ALU.add), outs=[dstbuf], ins=[RT1, RT2])

                for g in range(2):
                    for j, cbase in enumerate((2560, 2688)):
                        kb.dma("gpsimd", wk3[:, :, j * 64:(j + 1) * 64], w_in_v[:, :, cbase + g * 64:cbase + g * 64 + 64], outs=[WN])
                    for j, cbase in enumerate((2816, 2816, 3072, 3072)):
                        kb.dma("gpsimd", wk3[:, :, 128 + j * 64:128 + (j + 1) * 64], w_in_v[:, :, cbase + g * 64:cbase + g * 64 + 64], outs=[WN])
                    for j, cbase in enumerate((2944, 3200)):
                        kb.dma("gpsimd", wv2[:, :, j * 64:(j + 1) * 64], w_in_v[:, :, cbase + g * 64:cbase + g * 64 + 64], outs=[WN])
                    kb.dma("gpsimd", wqg[:], w_in_v[:, :, 2048 + g * 256:2048 + (g + 1) * 256], outs=[WN])
                    kb.dma("gpsimd", wgt[:], w_in_v[:, :, 3328 + g * 12:3328 + (g + 1) * 12], outs=[WN])
                    kb.op("vector", lambda e: e.memset(vs1[:, :, 64:65], 1.0), outs=[KS])
                    kb.op("vector", lambda e: e.memset(vw1[:, :, 64:65], 1.0), outs=[KS])
                    if stop_after == "p1c_a":
                        dump("wk3", wk3[:], [128, 8, 384], BF16)
                        return nc, dbg_out
                    for n in range(NB):
                        sl = slice(n * 512, (n + 1) * 512)
                        kb.dma("sync", cosb[:], I["cosT"][:, sl], outs=[CSB])
                        kb.dma("sync", sinb[:], I["sinT"][:, sl], outs=[CSB])
                        for j in range(3):
                            bk = sbank()
                            for k in range(8):
                                kb.op("tensor", lambda e, k=k, bk=bk, j=j: e.matmul(ps[bk][:], lhsT=wk3[:, k, j * 128:(j + 1) * 128], rhs=hT[:, k, sl],
                                                                                    start=(k == 0), stop=(k == 7)), outs=[PS[bk]], ins=[WN, HT[n]], mark=(k == 7))
                            if j == 0:
                                kb.op("scalar", lambda e, bk=bk: e.copy(out=kcvT[:, sl], in_=ps[bk][:]), outs=[KS], ins=[PS[bk]])
                            else:
                                rope_from(bk, (KEe[0:64, sl], KEo[64:128, sl]) if j == 1 else kwT[:, sl], KS)
                        if stop_after == "p1c_b":
                                return nc, dbg_out
                        for i in range(4):
                            tt = 4 * n + i
                            bk = mbank()
                            for k in range(8):
                                kb.op("tensor", lambda e, k=k, bk=bk, tt=tt: e.matmul(ps[bk][:, 0:128], lhsT=hT[:, k, tt * 128:(tt + 1) * 128], rhs=wv2[:, k, :],
                                                                                     start=(k == 0), stop=(k == 7)), outs=[PS[bk]], ins=[WN, HT[n]], mark=(k == 7))
                            kb.op("scalar", lambda e, bk=bk, tt=tt: e.copy(out=vs1[:, tt, 0:64], in_=ps[bk][:, 0:64]), outs=[KS], ins=[PS[bk]])
                            kb.op("vector", lambda e, bk=bk, tt=tt: e.tensor_copy(out=vw1[:, tt, 0:64], in_=ps[bk][:, 64:128]), outs=[KS], ins=[PS[bk]])
                    if stop_after == "p1c_k":
                        dump("kcvT", kcvT[:], [128, S], BF16)
                        dump("vs1", vs1[:], [128, NT, 80], BF16)
                        return nc, dbg_out
                    with ExitStack() as pc:
                        w1kv = sb("w1kv", [128, 32, 256], BF16, pc)
                        peT = sb("peT", [128, 32], F32, pc)
                        peTb = sb("peTb", [128, 32], BF16, pc)
                        b1kv = sb("b1kv", [128, 4], F32, pc)
                        w2k2 = sb("w2k2", [128, 2, 128], BF16, pc)
                        w2v = sb("w2v", [128, 2, 64], BF16, pc)
                        hid = sb("hid", [128, 4, 256], BF16, pc)
                        beff = sb("beff", [128, 4], F32, pc)
                        gx = sb("gx", [128, 256], F32, pc)
                        gu = sb("gu", [128, 256], F32, pc)
                        gs = sb("gs", [128, 256], F32, pc)
                        CW = Buf(w1kv[:])
                        HID = Buf(hid[:])
                        GX = Buf(gx[:])
                        kb.dma("gpsimd", w1kv[0:64], I["w1k"], outs=[CW])
                        kb.dma("gpsimd", w1kv[64:128], I["w1v"], outs=[CW])
                        kb.dma("sync", peT[0:64], I["peTk"], outs=[CW])
                        kb.dma("sync", peT[64:128], I["peTv"], outs=[CW])
                        kb.dma("sync", b1kv[:, 0:2], I["b1k"], outs=[CW])
                        kb.dma("sync", b1kv[:, 2:4], I["b1v"], outs=[CW])
                        kb.dma("gpsimd", w2k2[:, :, 0:64], I["w2k"], outs=[CW])
                        kb.dma("gpsimd", w2k2[:, :, 64:128], I["w2k"], outs=[CW])
                        kb.dma("gpsimd", w2v[:], I["w2v"], outs=[CW])
                        kb.op("vector", lambda e: e.tensor_copy(out=peTb[:], in_=peT[:]), outs=[CW], ins=[CW])
                        kb.op("vector", lambda e: e.memset(hid[:], 0.0), outs=[HID])
                        for kv in range(2):
                            p0_ = kv * 64
                            for hc in range(2):
                                bk, bb = sbank(), mbank()
                                for l in range(32):
                                    kb.op("tensor", lambda e, l=l, bk=bk, hc=hc, p0_=p0_: e.matmul(ps[bk][:, 0:255], lhsT=w1kv[p0_:p0_ + 64, l, hc * 128:(hc + 1) * 128],
                                                                                                rhs=kcvT[p0_:p0_ + 64, l:l + 16 * 254 + 1:16], start=(l == 0), stop=(l == 31)),
                                          outs=[PS[bk]], ins=[CW, KS], mark=(l == 31))
                                for l in range(32):
                                    kb.op("tensor", lambda e, l=l, bb=bb, hc=hc, p0_=p0_: e.matmul(ps[bb][:, 0:1], lhsT=w1kv[p0_:p0_ + 64, l, hc * 128:(hc + 1) * 128],
                                                                                                rhs=peTb[p0_:p0_ + 64, l:l + 1], start=(l == 0), stop=(l == 31)),
                                          outs=[PS[bb]], ins=[CW], mark=(l == 31))
                                ci = kv * 2 + hc
                                kb.op("vector", lambda e, bb=bb, ci=ci: e.tensor_tensor(out=beff[:, ci:ci + 1], in0=ps[bb][:, 0:1], in1=b1kv[:, ci:ci + 1], op=ALU.add),
                                      outs=[GX], ins=[PS[bb], CW])
                                kb.op("vector", lambda e, bk=bk, ci=ci: e.tensor_scalar(out=gx[:, 0:255], in0=ps[bk][:, 0:255], scalar1=beff[:, ci:ci + 1], scalar2=None, op0=ALU.add),
                                      outs=[GX], ins=[PS[bk], GX])
                                kb.op("vector", lambda e: e.tensor_tensor(out=gu[:, 0:255], in0=gx[:, 0:255], in1=gx[:, 0:255], op=ALU.mult), outs=[GX], ins=[GX])
                                kb.op("vector", lambda e: e.tensor_scalar(out=gu[:, 0:255], in0=gu[:, 0:255], scalar1=0.044715, scalar2=1.0, op0=ALU.mult, op1=ALU.add), outs=[GX], ins=[GX])
                                kb.op("vector", lambda e: e.tensor_tensor(out=gu[:, 0:255], in0=gu[:, 0:255], in1=gx[:, 0:255], op=ALU.mult), outs=[GX], ins=[GX])
                                kb.op("scalar", lambda e: e.activation(out=gs[:, 0:255], in_=gu[:, 0:255], func=AF.Sigmoid, scale=1.5957691216057308), outs=[GX], ins=[GX])
                                kb.op("vector", lambda e, ci=ci: e.tensor_tensor(out=hid[:, ci, 0:255], in0=gx[:, 0:255], in1=gs[:, 0:255], op=ALU.mult), outs=[HID], ins=[GX])
                        bk = sbank()
                        for hc in range(2):
                            kb.op("tensor", lambda e, hc=hc, bk=bk: e.matmul(ps[bk][:, 0:256], lhsT=w2k2[:, hc, :], rhs=hid[:, hc, :], start=(hc == 0), stop=(hc == 1)),
                                  outs=[PS[bk]], ins=[CW, HID], mark=(hc == 1))
                        kb.op("scalar", lambda e, bk=bk: e.copy(out=kcmpT[:], in_=ps[bk][:, 0:256]), outs=[KC], ins=[PS[bk]])
                        for ct in range(2):
                            bk = sbank()
                            for hc in range(2):
                                kb.op("tensor", lambda e, hc=hc, bk=bk, ct=ct: e.matmul(ps[bk][:, 0:64], lhsT=hid[:, 2 + hc, ct * 128:(ct + 1) * 128], rhs=w2v[:, hc, :],
                                                                                       start=(hc == 0), stop=(hc == 1)), outs=[PS[bk]], ins=[CW, HID], mark=(hc == 1))
                            kb.op("scalar", lambda e, bk=bk, ct=ct: e.copy(out=vcmp1[:, ct, 0:64], in_=ps[bk][:, 0:64]), outs=[KC], ins=[PS[bk]])
                        kb.op("vector", lambda e: e.memset(vcmp1[:, :, 64:65], 1.0), outs=[KC])
                        kb.op("vector", lambda e: e.tensor_copy(out=vcmp1[:, :, 65:129], in_=ovl[:]), outs=[KC], ins=[CST])
                        kb.barrier()
                    if stop_after == "p1c_c":
                        dump("kcmpT", kcmpT[:], [128, 256], BF16)
                        dump("vcmp1", vcmp1[:], [128, 2, 144], BF16)
                        return nc, dbg_out
                    with ExitStack() as pq:
                        wband = sb("wband", [128, 8, 512], BF16, pq)
                        QC = Buf(wband[:])
                        kb.dma("sync", wband[:], I["wband"], outs=[QC])
                        qn = [[sb(f"qn{ch}{par}", [128, 512], BF16, pq) for par in range(2)] for ch in range(2)]
                        QN = Buf(qn[0][0][:])
                        qTb = sb("qTb", [128, 2, 512], BF16, pq)
                        qrTb = sb("qrTb", [128, 2, 512], BF16, pq)
                        QB_ = Buf(qTb[:])
                        QRB = Buf(qrTb[:])
                        gts = sb("gts", [128, 4, 12], F32, pq)
                        GTS = Buf(gts[:])
                        cmpbb = sb("cmpbb", [128, 512], BF16, pq)
                        CMB = Buf(cmpbb[:])
                        pt = [sb(f"pt{i}", [128, 512], BF16, pq) for i in range(3)]
                        PT = [Buf(t[:]) for t in pt]
                        onsa = sb("onsa", [128, 4, 256], F32, pq)
                        ONSA = Buf(onsa[:])
                        obn = sb("obn", [128, 4, 256], BF16, pq)
                        OBN = Buf(obn[:])
                        pslc = sb("pslc", [128, 4, 64], F32, pq)
                        PSLC = Buf(pslc[:])
                        sadd = sb("sadd", [128, 64], F32, pq)
                        SADD = Buf(sadd[:])
                        score = sb("score", [128, 64], F32, pq)
                        stmp = sb("stmp", [128, 64], F32, pq)
                        sel = sb("sel", [128, 64], F32, pq)
                        m8 = sb("m8", [128, 16], F32, pq)
                        negb4 = [sb(f"negb{i}", [128, 128], BF16, pq) for i in range(4)]
                        SEL = Buf(score[:])
                        negbT = sb("negbT", [128, 512], BF16, pq)
                        NBT = Buf(negbT[:])
                        rz = sb("rz", [128, 4], F32, pq)
                        RZ = Buf(rz[:])
                        accs = [ps[3][:, 0:129], ps[4][:, 0:129], ps[5][:, 0:129], ps[6][:, 0:129]]
                        ACC = [PS[3], PS[4], PS[5], PS[6]]
                        prr = [0]

                        def run_branch(steps):
                            LA = 2
                            n_ = len(steps)
                            for idx_ in range(n_ + LA):
                                if idx_ < n_:
                                    st = steps[idx_]
                                    bk = sbank()
                                    nm = len(st["s"])
                                    for idx, (l_, r_, insb) in enumerate(st["s"]):
                                        kb.op("tensor", lambda e, l_=l_, r_=r_, idx=idx, nm=nm, bk=bk: e.matmul(ps[bk][:], lhsT=l_, rhs=r_, start=(idx == 0), stop=(idx == nm - 1)),
                                              outs=[PS[bk]], ins=insb, mark=(idx == nm - 1))
                                    prr[0] = (prr[0] + 1) % 3
                                    pi = prr[0]
                                    kb.op("scalar", lambda e, bk=bk, pi=pi: e.activation(out=pt[pi][:], in_=ps[bk][:], func=AF.Exp, scale=SCL), outs=[PT[pi]], ins=[PS[bk]])
                                    st["pi"] = pi
                                if idx_ >= LA:
                                    prev = steps[idx_ - LA]
                                    pi = prev["pi"]
                                    for (i, rhs_ap, w_, st_, sp_) in prev["pv"]:
                                        kb.op("tensor", lambda e, i=i, rhs_ap=rhs_ap, w_=w_, st_=st_, sp_=sp_, pi=pi: e.matmul(accs[i][:, 0:w_], lhsT=pt[pi][:, i * 128:(i + 1) * 128], rhs=rhs_ap,
                                                                                                                 start=st_, stop=sp_),
                                              outs=[ACC[i]], ins=[PT[pi], KS, KC])

                        accsb = [sb(f"accsb{i}", [128, 132], F32, pq) for i in range(4)]
                        ACCSB = [Buf(t[:]) for t in accsb]

                        def finish(h, br, first):
                            zc = 64
                            wd_ = 129 if br == 0 else 65
                            for i in range(4):
                                kb.op("vector", lambda e, i=i: e.tensor_copy(out=accsb[i][:, 0:wd_], in_=accs[i][:, 0:wd_]), outs=[ACCSB[i]], ins=[ACC[i]])
                            for i in range(4):
                                kb.op("vector", lambda e, i=i: e.tensor_scalar(out=rz[:, i:i + 1], in0=accsb[i][:, zc:zc + 1], scalar1=1e-30, scalar2=None, op0=ALU.max),
                                      outs=[RZ], ins=[ACCSB[i]])
                            kb.op("vector", lambda e: e.reciprocal(out=rz[:], in_=rz[:]), outs=[RZ], ins=[RZ])
                            if br == 0:
                                for i in range(4):
                                    if h == 0:
                                        kb.op("vector", lambda e, i=i: e.tensor_scalar(out=pslc[:, i, :], in0=accsb[i][:, 65:129], scalar1=rz[:, i:i + 1], scalar2=None, op0=ALU.mult),
                                              outs=[PSLC], ins=[ACCSB[i], RZ])
                                    else:
                                        kb.op("vector", lambda e, i=i: e.scalar_tensor_tensor(out=pslc[:, i, :], in0=accsb[i][:, 65:129], scalar=rz[:, i:i + 1], in1=pslc[:, i, :],
                                                                                           op0=ALU.mult, op1=ALU.add), outs=[PSLC], ins=[ACCSB[i], RZ, PSLC])
                            kb.op("vector", lambda e: e.tensor_tensor(out=rz[:], in0=rz[:], in1=gts[:, :, h * 3 + br], op=ALU.mult), outs=[RZ], ins=[RZ, GTS])
                            for i in range(4):
                                dst = onsa[:, i, h * 64:(h + 1) * 64]
                                if first:
                                    kb.op("vector", lambda e, i=i, dst=dst: e.tensor_scalar(out=dst, in0=accsb[i][:, 0:64], scalar1=rz[:, i:i + 1], scalar2=None, op0=ALU.mult),
                                          outs=[ONSA], ins=[ACCSB[i], RZ])
                                else:
                                    kb.op("vector", lambda e, i=i, dst=dst: e.scalar_tensor_tensor(out=dst, in0=accsb[i][:, 0:64], scalar=rz[:, i:i + 1], in1=dst, op0=ALU.mult, op1=ALU.add),
                                          outs=[ONSA], ins=[ACCSB[i], RZ, ONSA])

                        wband4 = sb("wband4", [128, 8, 512], BF16, pq)
                        cmpb0 = sb("cmpb0", [128, 512], BF16, pq)
                        kb.dma("sync", wband4[:], I["wband4"], outs=[QC])
                        kb.dma("sync", cmpb0[:], I["cmpb0"], outs=[QC])
                        for qb in range(4, NB):
                            sl = slice(qb * 512, (qb + 1) * 512)
                            kb.dma("sync", cosb[:], I["cosT"][:, sl], outs=[CSB])
                            kb.dma("sync", sinb[:], I["sinT"][:, sl], outs=[CSB])
                            kb.dma("sync", cmpbb[:], I["cmpb"][:, qb, :], outs=[CMB])
                            for ch in range(2):
                                bk = sbank()
                                for k in range(8):
                                    kb.op("tensor", lambda e, k=k, bk=bk, ch=ch: e.matmul(ps[bk][:], lhsT=wqg[:, k, ch * 128:(ch + 1) * 128], rhs=hT[:, k, sl],
                                                                                         start=(k == 0), stop=(k == 7)), outs=[PS[bk]], ins=[WN, HT[qb]], mark=(k == 7))
                                kb.op("scalar", lambda e, bk=bk, ch=ch: e.copy(out=qTb[:, ch, :], in_=ps[bk][:]), outs=[QB_], ins=[PS[bk]])
                                rope_from(bk, qrTb[:, ch, :], QRB)
                            for i in range(4):
                                tt = 4 * qb + i
                                bk = mbank()
                                for k in range(8):
                                    kb.op("tensor", lambda e, k=k, bk=bk, tt=tt: e.matmul(ps[bk][:, 0:12], lhsT=hT[:, k, tt * 128:(tt + 1) * 128], rhs=wgt[:, k, :],
                                                                                         start=(k == 0), stop=(k == 7)), outs=[PS[bk]], ins=[WN, HT[qb]], mark=(k == 7))
                                kb.op("scalar", lambda e, bk=bk, i=i: e.activation(out=gts[:, i, :], in_=ps[bk][:, 0:12], func=AF.Sigmoid), outs=[GTS], ins=[PS[bk]])
                            if stop_after == "p1c_qa":
                                dump("onsa", onsa[:], [128, 4, 256])
                                return nc, dbg_out
                            ncts = 1 if qb < 4 else 2
                            for h in range(4):
                                ch, p0_ = h // 2, (h % 2) * 64
                                steps = []
                                for ct in range(ncts):
                                    smm = [(kcmpT[p0_:p0_ + 64, ct * 128:(ct + 1) * 128], qTb[p0_:p0_ + 64, ch, :], [KC, QB_])]
                                    if ct == ncts - 1:
                                        smm.append((ident[:], cmpbb[:], [CST, CMB]))
                                    else:
                                        smm.append((ident[:], cmpb0[:], [CST, QC]))
                                    pv = [(i, vcmp1[:, ct, 0:129], 129, ct == 0, ct == ncts - 1) for i in range(4)]
                                    steps.append({"s": smm, "pv": pv})
                                run_branch(steps)
                                finish(h, 0, True)
                            if stop_after == "p1c_qb":
                                dump("onsa", onsa[:], [128, 4, 256])
                                return nc, dbg_out
                            for i in range(4):
                                qt = 4 * qb + i
                                kb.dma("sync", sadd[:], I["seladd"][:, qt, :], outs=[SADD])
                                kb.op("vector", lambda e, i=i: e.tensor_tensor(out=score[:], in0=pslc[:, i, :], in1=sadd[:], op=ALU.add), outs=[SEL], ins=[PSLC, SADD])
                                kb.op("vector", lambda e: e.max(out=m8[:, 0:8], in_=score[:]), outs=[SEL], ins=[SEL])
                                kb.op("vector", lambda e: e.match_replace(out=stmp[:], in_to_replace=m8[:, 0:8], in_values=score[:], imm_value=-3e38), outs=[SEL], ins=[SEL])
                                kb.op("vector", lambda e: e.max(out=m8[:, 8:16], in_=stmp[:]), outs=[SEL], ins=[SEL])
                                kb.op("vector", lambda e: e.tensor_scalar(out=sel[:], in0=score[:], scalar1=m8[:, 15:16], scalar2=None, op0=ALU.is_ge), outs=[SEL], ins=[SEL])
                                kb.op("vector", lambda e: e.scalar_tensor_tensor(out=sel[:], in0=score[:], scalar=-1e29, in1=sel[:], op0=ALU.is_gt, op1=ALU.mult), outs=[SEL], ins=[SEL])
                                kb.op("vector", lambda e: e.tensor_scalar(out=negb4[i][:, 0:64], in0=sel[:], scalar1=-1.0, scalar2=-NEG, op0=ALU.add, op1=ALU.mult), outs=[SEL], ins=[SEL])
                                kb.op("vector", lambda e: e.tensor_scalar(out=negb4[i][:, 64:128], in0=sel[:], scalar1=-1.0, scalar2=-NEG, op0=ALU.add, op1=ALU.mult), outs=[SEL], ins=[SEL])
                            for h in range(4):
                                ch, p0_ = h // 2, (h % 2) * 64
                                steps = []
                                jmin = max(0, 4 - 4 * qb)
                                for j in range(jmin, 8):
                                    kt = 4 * qb - 4 + j
                                    smm = [(kwT[p0_:p0_ + 64, kt * 128:(kt + 1) * 128], qrTb[p0_:p0_ + 64, ch, :], [KS, QRB]),
                                           (ident[:], (wband4 if qb == 4 else wband)[:, j, :], [CST, QC])]
                                    pv = [(i, vw1[:, kt, 0:65], 65, j == max(i, jmin), j == i + 4) for i in range(4) if i <= j <= i + 4]
                                    steps.append({"s": smm, "pv": pv})
                                run_branch(steps)
                                finish(h, 2, False)
                            for i in range(4):
                                bk = mbank()
                                kb.op("tensor", lambda e, bk=bk, i=i: e.matmul(ps[bk][:, 0:128], lhsT=negb4[i][:], rhs=ident[:], start=True, stop=True), outs=[PS[bk]], ins=[SEL, CST])
                                kb.op("scalar", lambda e, bk=bk, i=i: e.copy(out=negbT[:, i * 128:(i + 1) * 128], in_=ps[bk][:, 0:128]), outs=[NBT], ins=[PS[bk]])
                            for ch in range(2):
                                kb.op("gpsimd", lambda e, ch=ch: e.tensor_copy(out=qn[ch][0][0:64, :], in_=qrTb[0:64, ch, :]), outs=[QN], ins=[QRB])
                                kb.op("gpsimd", lambda e, ch=ch: e.tensor_copy(out=qn[ch][0][64:128, :], in_=negbT[64:128, :]), outs=[QN], ins=[NBT])
                                kb.op("gpsimd", lambda e, ch=ch: e.tensor_copy(out=qn[ch][1][0:64, :], in_=negbT[0:64, :]), outs=[QN], ins=[NBT])
                                kb.op("gpsimd", lambda e, ch=ch: e.tensor_copy(out=qn[ch][1][64:128, :], in_=qrTb[64:128, ch, :]), outs=[QN], ins=[QRB])
                            for h in range(4):
                                ch, p0_ = h // 2, (h % 2) * 64
                                KE_ = KEe if h % 2 == 0 else KEo
                                steps = []
                                for kt in range(4 * qb + 4):
                                    smm = [(KE_[:, kt * 128:(kt + 1) * 128], qn[ch][h % 2][:], [KS, QN, CST])]
                                    if kt >= 4 * qb:
                                        smm.append((ident[:], wband[:, 4 + kt - 4 * qb, :], [CST, QC]))
                                    pv = [(i, vs1[:, kt, 0:65], 65, kt == 0, kt == 4 * qb + i) for i in range(4) if kt <= 4 * qb + i]
                                    steps.append({"s": smm, "pv": pv})
                                run_branch(steps)
                                finish(h, 1, False)
                            if stop_after == "p1c_q":
                                dump("onsa", onsa[:], [128, 4, 256])
                                dump("pslc", pslc[:], [128, 4, 64])
                                dump("negbT", negbT[:], [128, 512], BF16)
                                return nc, dbg_out
                            kb.op("gpsimd", lambda e: e.tensor_copy(out=obn[:], in_=onsa[:]), outs=[OBN], ins=[ONSA])
                            for i in range(4):
                                qt = 4 * qb + i
                                s_ = qt - 16
                                for ch in range(2):
                                    jf = 4 + g * 2 + ch
                                    bk = mbank()
                                    kb.op("tensor", lambda e, bk=bk, i=i, ch=ch: e.matmul(ps[bk][:, 0:128], lhsT=obn[:, i, ch * 128:(ch + 1) * 128], rhs=ident[:], start=True, stop=True),
                                          outs=[PS[bk]], ins=[OBN, CST])
                                    dst = oT[:, jf, s_ * 128:(s_ + 1) * 128]
                                    kb.op("scalar", lambda e, bk=bk, dst=dst: e.copy(out=dst, in_=ps[bk][:, 0:128]), outs=[OT[jf][s_]], ins=[PS[bk]])
                        kb.barrier()
                kb.barrier()
            if "oT2" in dbg:
                dump("oT2", oT[:], [128, 8, SO], BF16)
            if stop_after == "p1c":
                kb.barrier()
                return nc, dbg_out
        kb.barrier()
        with ExitStack() as p2:
            acc = sb("acc", [128, 8, SO], F32, p2)
            ACCB = [Buf(acc[:, :, nb * 512:(nb + 1) * 512]) for nb in range(4)]
            wo = sb("wo", [128, 8, D], BF16, p2)
            WO = Buf(wo[:])
            fg = sb("fg", [128, 8], F32, p2)
            FG = Buf(fg[:])
            kb.dma("sync", fg[:], I["fg"], outs=[FG])
            xTo_v = I["xTo"].rearrange("(k p) t -> p k t", p=128)
            for nb in range(4):
                kb.dma("sync", acc[:, :, nb * 512:(nb + 1) * 512], xTo_v[:, :, nb * 512:(nb + 1) * 512], outs=[ACCB[nb]])
            w_out_v = I["w_out"].rearrange("(k p) c -> p k c", p=128)
            for k in range(8):
                kb.dma("gpsimd", wo[:, k, :], w_out_v[:, k, :], outs=[WO])
            rr2 = [0]

            def bank2():
                rr2[0] = (rr2[0] + 1) % 8
                return rr2[0]

            for nb in range(4):
                sl = slice(nb * 512, (nb + 1) * 512)
                for m in range(8):
                    bk = bank2()
                    for k in range(8):
                        kb.op("tensor", lambda e, k=k, m=m, bk=bk, sl=sl: e.matmul(ps[bk][:], lhsT=wo[:, k, m * 128:(m + 1) * 128], rhs=oT[:, k, sl], start=(k == 0), stop=(k == 7)),
                              outs=[PS[bk]], ins=[WO] + [OT[k][s] for s in range(nb * 4, nb * 4 + 4)], mark=(k == 7))
                    kb.op("vector", lambda e, m=m, bk=bk, sl=sl: e.scalar_tensor_tensor(out=acc[:, m, sl], in0=ps[bk][:], scalar=g1c(m), in1=acc[:, m, sl], op0=ALU.mult, op1=ALU.add),
                          outs=[ACCB[nb]], ins=[PS[bk], ACCB[nb], MOD])
            if "x2T" in dbg:
                dump("x2T", acc[:], [128, 8, SO])
            h2T = sb("h2T", [128, 8, SO], BF16, p2)
            H2 = [Buf(h2T[:, :, nb * 512:(nb + 1) * 512]) for nb in range(4)]
            with ExitStack() as p2a:
                sqa = sb("sqa", [128, 8, 512], F32, p2a)
                SQA = Buf(sqa[:])
                rsa = sb("rsa", [128, 512], F32, p2a)
                RSA = Buf(rsa[:])
                tma = [sb(f"tma{i}", [128, 512], F32, p2a) for i in range(2)]
                TMA = [Buf(t[:]) for t in tma]
                for nb in range(4):
                    sl = slice(nb * 512, (nb + 1) * 512)
                    kb.op("scalar", lambda e, sl=sl: e.activation(out=sqa[:], in_=acc[:, :, sl], func=AF.Square), outs=[SQA], ins=[ACCB[nb]])
                    bk = bank2()
                    for k in range(8):
                        kb.op("tensor", lambda e, k=k, bk=bk: e.matmul(ps[bk][:], lhsT=onesf[:], rhs=sqa[:, k, :], start=(k == 0), stop=(k == 7)),
                              outs=[PS[bk]], ins=[SQA, CST], mark=(k == 7))
                    kb.op("scalar", lambda e, bk=bk: e.activation(out=rsa[:], in_=ps[bk][:], func=AF.Sqrt, scale=1.0 / D, bias=EPS), outs=[RSA], ins=[PS[bk]])
                    kb.op("vector", lambda e: e.reciprocal(out=rsa[:], in_=rsa[:]), outs=[RSA], ins=[RSA])
                    for k in range(8):
                        T = TMA[k % 2]
                        t_ = tma[k % 2]
                        kb.op("vector", lambda e, k=k, t_=t_, sl=sl: e.tensor_tensor(out=t_[:], in0=acc[:, k, sl], in1=rsa[:], op=ALU.mult), outs=[T], ins=[ACCB[nb], RSA])
                        kb.op("scalar", lambda e, k=k, t_=t_, sl=sl: e.activation(out=h2T[:, k, sl], in_=t_[:], func=AF.Identity, scale=a2[:, k:k + 1], bias=sh2(k)),
                              outs=[H2[nb]], ins=[T, MOD])
                kb.barrier()
            if "h2T" in dbg:
                dump("h2T", h2T[:], [128, 8, SO], BF16)
            gw = sb("gw", [128, 16, 65], F32, p2)
            GW = Buf(gw[:])
            kb.op("vector", lambda e: e.memset(gw[:], 1.0), outs=[GW])
            with ExitStack() as p2r:
                rwf = sb("rwf", [128, 8, 64], F32, p2r)
                rwb = sb("rwb", [128, 8, 64], BF16, p2r)
                rbias = sb("rbias", [128, 64], F32, p2r)
                RW = Buf(rwf[:])
                kb.dma("sync", rwf[:], I["rw"], outs=[RW])
                kb.dma("sync", rbias[:], I["rbias"], outs=[RW])
                kb.op("vector", lambda e: e.tensor_copy(out=rwb[:], in_=rwf[:]), outs=[RW], ins=[RW])
                scr = sb("scr", [128, 64], F32, p2r)
                chs = sb("chs", [128, 64], F32, p2r)
                eq = sb("eq", [128, 64], F32, p2r)
                chm = sb("chm", [128, 64], F32, p2r)
                m1 = sb("m1", [128, 8], F32, p2r)
                m2 = sb("m2", [128, 8], F32, p2r)
                gsm = sb("gsm", [128, 8], F32, p2r)
                g8 = sb("g8", [128, 8], F32, p2r)
                gmk = sb("gmk", [128, 8], F32, p2r)
                e8 = sb("e8", [128, 8], F32, p2r)
                ssum = sb("ssum", [128, 1], F32, p2r)
                RT = Buf(scr[:])
                v3 = lambda ap: ap.rearrange("p (g j) -> p g j", j=8)
                b3 = lambda ap: ap.rearrange("p (g o) -> p g o", o=1).to_broadcast([128, 8, 8])
                for tt in range(16):
                    bk = bank2()
                    for k in range(8):
                        kb.op("tensor", lambda e, k=k, bk=bk, tt=tt: e.matmul(ps[bk][:, 0:64], lhsT=h2T[:, k, tt * 128:(tt + 1) * 128], rhs=rwb[:, k, :], start=(k == 0), stop=(k == 7)),
                              outs=[PS[bk]], ins=[H2[tt // 4], RW], mark=(k == 7))
                    kb.op("scalar", lambda e, bk=bk: e.activation(out=scr[:], in_=ps[bk][:, 0:64], func=AF.Sigmoid), outs=[RT], ins=[PS[bk]])
                    V = lambda f: kb.op("vector", f, outs=[RT], ins=[RT, RW])
                    V(lambda e: e.tensor_tensor(out=chs[:], in0=scr[:], in1=rbias[:], op=ALU.add))
                    V(lambda e: e.tensor_reduce(out=m1[:], in_=v3(chs[:]), axis=AX.X, op=ALU.max))
                    V(lambda e: e.tensor_tensor(out=v3(eq[:]), in0=v3(chs[:]), in1=b3(m1[:]), op=ALU.is_equal))
                    V(lambda e: e.scalar_tensor_tensor(out=eq[:], in0=eq[:], scalar=-1e30, in1=chs[:], op0=ALU.mult, op1=ALU.add))
                    V(lambda e: e.tensor_reduce(out=m2[:], in_=v3(eq[:]), axis=AX.X, op=ALU.max))
                    V(lambda e: e.tensor_tensor(out=gsm[:], in0=m1[:], in1=m2[:], op=ALU.add))
                    V(lambda e: e.max(out=g8[:], in_=gsm[:]))
                    V(lambda e: e.tensor_scalar(out=gmk[:], in0=gsm[:], scalar1=g8[:, 3:4], scalar2=None, op0=ALU.is_ge))
                    V(lambda e: e.scalar_tensor_tensor(out=v3(chm[:]), in0=v3(chs[:]), scalar=10.0, in1=b3(gmk[:]), op0=ALU.add, op1=ALU.mult))
                    V(lambda e: e.max(out=e8[:], in_=chm[:]))
                    V(lambda e: e.tensor_scalar(out=eq[:], in0=chm[:], scalar1=e8[:, 7:8], scalar2=None, op0=ALU.is_ge))
                    V(lambda e: e.tensor_tensor(out=eq[:], in0=eq[:], in1=scr[:], op=ALU.mult))
                    V(lambda e: e.tensor_reduce(out=ssum[:], in_=eq[:], axis=AX.X, op=ALU.add))
                    V(lambda e: e.reciprocal(out=ssum[:], in_=ssum[:]))
                    kb.op("vector", lambda e, tt=tt: e.tensor_scalar(out=gw[:, tt, 0:64], in0=eq[:], scalar1=ssum[:, 0:1], scalar2=2.5, op0=ALU.mult, op1=ALU.mult),
                          outs=[GW], ins=[RT])
                kb.barrier()
            if "gw" in dbg:
                dump("gw", gw[:], [128, 16, 65])
            with ExitStack() as p2e:
                wg = [sb(f"wg{i}", [128, 8, 512], BF16, p2e) for i in range(2)]
                wd = [sb(f"wd{i}", [128, 2, D], BF16, p2e) for i in range(2)]
                WG = [Buf(t[:]) for t in wg]
                WD = [Buf(t[:]) for t in wd]
                actT = [sb(f"actT{i}", [128, 2, SO], BF16, p2e) for i in range(2)]
                ACT_ = [[Buf(actT[i][:, :, nb * 512:(nb + 1) * 512]) for nb in range(4)] for i in range(2)]
                sgt = [sb(f"sgt{i}", [128, 256], F32, p2e) for i in range(2)]
                SGT = [Buf(t[:]) for t in sgt]
                att = [sb(f"att{i}", [128, 256], BF16, p2e) for i in range(2)]
                ATT = [Buf(t[:]) for t in att]
                NE = n_experts

                def load_w(e_):
                    sl_ = e_ % 2
                    gv = I["wgu"][e_].rearrange("(k p) c -> p k c", p=128)
                    kb.dma("gpsimd", wg[sl_][:, 0:4, :], gv[:, 0:4, :], outs=[WG[sl_]])
                    kb.dma("gpsimd", wg[sl_][:, 4:8, :], gv[:, 4:8, :], outs=[WG[sl_]])
                    kb.dma("gpsimd", wd[sl_][:], I["wdn"][e_].rearrange("(k p) c -> p k c", p=128), outs=[WD[sl_]])

                def load_wg(e_):
                    sl_ = e_ % 2
                    gv = I["wgu"][e_].rearrange("(k p) c -> p k c", p=128)
                    kb.dma("gpsimd", wg[sl_][:, 0:4, :], gv[:, 0:4, :], outs=[WG[sl_]])
                    kb.dma("gpsimd", wg[sl_][:, 4:8, :], gv[:, 4:8, :], outs=[WG[sl_]])

                def load_wd(e_):
                    sl_ = e_ % 2
                    kb.dma("gpsimd", wd[sl_][:], I["wdn"][e_].rearrange("(k p) c -> p k c", p=128), outs=[WD[sl_]])

                cnt = [0]
                pend = {}

                def GU(e_, tt):
                    sl_ = e_ % 2
                    bk = bank2()
                    for k in range(8):
                        kb.op("tensor", lambda e, k=k: e.matmul(ps[bk][:], lhsT=h2T[:, k, tt * 128:(tt + 1) * 128], rhs=wg[sl_][:, k, :], start=(k == 0), stop=(k == 7)),
                              outs=[PS[bk]], ins=[H2[tt // 4], WG[sl_]], mark=(k == 7))
                    cnt[0] += 1
                    i2 = cnt[0] % 2
                    kb.op("scalar", lambda e: e.activation(out=sgt[i2][:], in_=ps[bk][:, 0:256], func=AF.Silu), outs=[SGT[i2]], ins=[PS[bk]])
                    kb.op("vector", lambda e: e.scalar_tensor_tensor(out=att[i2][:], in0=ps[bk][:, 256:512], scalar=gw[:, tt, e_:e_ + 1], in1=sgt[i2][:],
                                                                     op0=ALU.mult, op1=ALU.mult), outs=[ATT[i2]], ins=[PS[bk], SGT[i2], GW])
                    pend[(e_, tt)] = i2

                def TR(e_, tt):
                    sl_ = e_ % 2
                    i2 = pend.pop((e_, tt))
                    for hc in range(2):
                        bt = bank2()
                        kb.op("tensor", lambda e, hc=hc, bt=bt: e.matmul(ps[bt][:, 0:128], lhsT=att[i2][:, hc * 128:(hc + 1) * 128], rhs=ident[:], start=True, stop=True),
                              outs=[PS[bt]], ins=[ATT[i2], CST])
                        kb.op("scalar", lambda e, hc=hc, bt=bt: e.copy(out=actT[sl_][:, hc, tt * 128:(tt + 1) * 128], in_=ps[bt][:, 0:128]),
                              outs=[ACT_[sl_][tt // 4]], ins=[PS[bt]])

                def DN(e_, nb, ms):
                    sl_ = e_ % 2
                    sl = slice(nb * 512, (nb + 1) * 512)
                    for m in ms:
                        bk = bank2()
                        for hc in range(2):
                            kb.op("tensor", lambda e, bk=bk, hc=hc, m=m: e.matmul(ps[bk][:], lhsT=wd[sl_][:, hc, m * 128:(m + 1) * 128], rhs=actT[sl_][:, hc, sl], start=(hc == 0), stop=(hc == 1)),
                                  outs=[PS[bk]], ins=[WD[sl_], ACT_[sl_][nb]], mark=(hc == 1))
                        kb.op("vector", lambda e, bk=bk, m=m: e.scalar_tensor_tensor(out=acc[:, m, sl], in0=ps[bk][:], scalar=g2c(m), in1=acc[:, m, sl], op0=ALU.mult, op1=ALU.add),
                              outs=[ACCB[nb]], ins=[PS[bk], ACCB[nb], MOD])

                load_wg(0)
                load_wd(0)
                for e_ in range(NE + 1):
                    if e_ + 1 < NE:
                        load_wg(e_ + 1)
                    for tt in range(16):
                        if e_ < NE:
                            GU(e_, tt)
                            if tt > 0:
                                TR(e_, tt - 1)
                        if e_ > 0:
                            DN(e_ - 1, tt // 4, (2 * (tt % 4), 2 * (tt % 4) + 1))
                    if e_ < NE:
                        TR(e_, 15)
                    if e_ + 1 < NE:
                        load_wd(e_ + 1)
                kb.barrier()
            if "x3T" in dbg:
                dump("x3T", acc[:], [128, 8, SO])
            sq2 = sb("sq2", [128, 8, 512], F32, p2)
            SQ2 = Buf(sq2[:])
            rstd2 = sb("rstd2", [128, 512], F32, p2)
            RS2 = Buf(rstd2[:])
            ot = [sb(f"ot{i}", [128, 8, 512], F32, p2) for i in range(2)]
            OTB = [Buf(t[:]) for t in ot]
            outT_v = outT.rearrange("(k p) t -> p k t", p=128)
            for nb in range(4):
                sl = slice(nb * 512, (nb + 1) * 512)
                kb.op("scalar", lambda e, sl=sl: e.activation(out=sq2[:], in_=acc[:, :, sl], func=AF.Square), outs=[SQ2], ins=[ACCB[nb]])
                bk = bank2()
                for k in range(8):
                    kb.op("tensor", lambda e, k=k, bk=bk: e.matmul(ps[bk][:], lhsT=onesf[:], rhs=sq2[:, k, :], start=(k == 0), stop=(k == 7)),
                          outs=[PS[bk]], ins=[SQ2, CST], mark=(k == 7))
                kb.op("scalar", lambda e, bk=bk: e.activation(out=rstd2[:], in_=ps[bk][:], func=AF.Sqrt, scale=1.0 / D, bias=EPS), outs=[RS2], ins=[PS[bk]])
                kb.op("vector", lambda e: e.reciprocal(out=rstd2[:], in_=rstd2[:]), outs=[RS2], ins=[RS2])
                o_ = ot[nb % 2]
                for k in range(8):
                    kb.op("vector", lambda e, k=k, o_=o_, sl=sl: e.scalar_tensor_tensor(out=o_[:, k, :], in0=acc[:, k, sl], scalar=fg[:, k:k + 1], in1=rstd2[:], op0=ALU.mult, op1=ALU.mult),
                          outs=[OTB[nb % 2]], ins=[ACCB[nb], FG, RS2])
                kb.dma("sync", outT_v[:, :, sl], o_[:], ins=[OTB[nb % 2]])
            kb.barrier()
        kb.barrier()
    return nc, dbg_out


def _prep_inputs(inp, core):
    b = core // 2
    half = core % 2
    f = lambda a: np.ascontiguousarray(a, dtype=np.float32)
    x = inp["x"][b]
    m = {}
    xT = f(x.T)
    m["xTo"] = f(xT[:, half * SO:(half + 1) * SO])
    if half == 0:
        xl = np.zeros((D, S), np.float32)
        xl[:, SO:] = xT[:, :SO]
        m["xT"] = xl
    else:
        m["xT"] = xT
    m["cT"] = f(inp["c"][b].reshape(8, 128).T)
    m["w_ada"] = f(inp["w_ada"][0])
    m["b_ada"] = f(inp["b_ada"][0].reshape(1, -1))
    m["n1g"] = f(inp["norm1_g"][0].reshape(8, 128).T)
    m["w_in"] = f(inp["w_in"][0])
    m["lbl"] = f(inp["hg_lb_logits"].reshape(2, 4, 128).transpose(2, 0, 1))
    m["hng"] = f(np.broadcast_to(inp["hg_norm_g"][0][None, :], (128, 512)))
    for s in ("k", "v"):
        m["peT" + s] = f(inp["cmp_pos_" + s][0].T)
        m["w1" + s] = f(inp["cmp_w1_" + s][0].reshape(32, 64, 256).transpose(1, 0, 2))
        m["b1" + s] = f(inp["cmp_b1_" + s][0].reshape(2, 128).T)
        m["w2" + s] = f(inp["cmp_w2_" + s][0].reshape(2, 128, 64).transpose(1, 0, 2))
    m["w_out"] = f(inp["w_out"][0])
    m["n2g"] = f(inp["norm2_g"][0].reshape(8, 128).T)
    m["rw"] = f(inp["router_w"][0].reshape(8, 128, 64).transpose(1, 0, 2))
    m["rbias"] = f(np.broadcast_to(inp["router_bias"][0][None, :], (128, 64)))
    m["fg"] = f(inp["final_g"].reshape(8, 128).T)
    return m


_SHARED = {}


def kernel(**inp):
    inp = {k: np.asarray(v) for k, v in inp.items()}
    nc, _ = build()
    wgu = np.ascontiguousarray(np.concatenate([inp["w_exp_gu"][0], inp["w_sh_gu"][0][None]], axis=0), dtype=np.float32)
    wdn = np.ascontiguousarray(np.concatenate([inp["w_exp_dn"][0], inp["w_sh_dn"][0][None]], axis=0), dtype=np.float32)
    in_maps = []
    for core in range(8):
        m = _prep_inputs(inp, core)
        m["wgu"] = wgu
        m["wdn"] = wdn
        m.update(_consts(core % 2))
        in_maps.append(m)
    res = run_bass_kernel_spmd(nc, in_maps, core_ids=list(range(8)))
    out = np.zeros((4, S, D), np.float32)
    for core in range(8):
        b, half = core // 2, core % 2
        out[b, half * SO:(half + 1) * SO, :] = res.results[core]["outT"].T
    return out
```
